# Optimizing a Trainium2 kernel written in Bass

```python
import math
import jax
import jax.numpy as jnp
from jax import lax
import numpy as np


D_MODEL = 1024
BATCH = 16
SEQ = 2048
DEPTH = 2

CTX_LEN = 256
GRID_W = 64
EPS = 1e-6
N_BRANCH = 3
BRANCH_WIDTH = 512
A_HEADS = 8
A_HEAD_DIM = BRANCH_WIDTH // A_HEADS
DECAY_LORA = 64
AAA_LORA = 64
GATE_LORA = 128
LN_X_EPS = 64e-5
B_HEADS = 4
B_HEAD_DIM = BRANCH_WIDTH // B_HEADS
SHORT_CONV = 5
GDN_CHUNK = 64
DW_CONV = 31
N_EXPERTS = 16
N_GROUPS = 4
EXP_PER_GROUP = N_EXPERTS // N_GROUPS
TOP_K = 2
D_EXPERT = 512
MOE_BLOCK = 128

A_COLS = 3 * BRANCH_WIDTH + 2 * DECAY_LORA + 2 * AAA_LORA + GATE_LORA
B_COLS = 4 * BRANCH_WIDTH + 4 * B_HEADS
C_COLS = 2 * BRANCH_WIDTH
G_COLS = N_BRANCH * D_MODEL
N_IN = A_COLS + B_COLS + C_COLS + G_COLS
A_SPLITS = [BRANCH_WIDTH, 2 * BRANCH_WIDTH, 3 * BRANCH_WIDTH, 3 * BRANCH_WIDTH + 2 * DECAY_LORA, 3 * BRANCH_WIDTH + 2 * DECAY_LORA + 2 * AAA_LORA]
B_SPLITS = [3 * BRANCH_WIDTH, 4 * BRANCH_WIDTH, 4 * BRANCH_WIDTH + 2 * B_HEADS]
IN_SPLITS = [A_COLS, A_COLS + B_COLS, A_COLS + B_COLS + C_COLS]

kernel_name = 'hybrid_rwkv7_gdn_conformer_moe_dit'


def rmsnorm(x, g):
    xf = x.astype(jnp.float32)
    y = xf * lax.rsqrt(jnp.mean(xf * xf, axis=-1, keepdims=True) + EPS)
    return (y * g).astype(x.dtype)


def layernorm(x, g, b, eps=EPS):
    xf = x.astype(jnp.float32)
    mu = jnp.mean(xf, axis=-1, keepdims=True)
    var = jnp.mean(jnp.square(xf - mu), axis=-1, keepdims=True)
    return ((xf - mu) * lax.rsqrt(var + eps) * g + b).astype(x.dtype)


def modulate(x, g, shift, scale):
    return rmsnorm(x, g) * (1 + scale) + shift


def heads(t, n):
    return t.reshape(t.shape[:-1] + (n, t.shape[-1] // n))


def l2norm(t):
    tf = t.astype(jnp.float32)
    return tf * lax.rsqrt(jnp.sum(tf * tf, axis=-1, keepdims=True) + EPS)


def dwconv(x, w):
    K, C = w.shape
    return lax.conv_general_dilated(x, w[:, None, :].astype(x.dtype), window_strides=(1,),
                                    padding=[(K // 2, K // 2)],
                                    dimension_numbers=('NWC', 'WIO', 'NWC'),
                                    feature_group_count=C)


def centred_shift(p, mu):
    prev = jnp.pad(p[:, :-1], ((0, 0), (1, 0), (0, 0)))
    nxt = jnp.pad(p[:, 1:], ((0, 0), (0, 1), (0, 0)))
    return p + mu[0] * (prev - p) + mu[1] * (nxt - p)


def run_bidirectional(scan_fn, ctx_args, lat_args, state0):
    y_ctx, y_lat = [], []
    for d in range(2):
        orient = (lambda t: t) if d == 0 else (lambda t: jnp.flip(t, axis=1))
        yc, s_ctx = scan_fn(state0, *[orient(t) for t in ctx_args[d]])
        yl, _ = scan_fn(s_ctx, *[orient(t) for t in lat_args[d]])
        y_ctx.append(orient(yc))
        y_lat.append(orient(yl))
    return y_ctx[0] + y_ctx[1], y_lat[0] + y_lat[1]


def rwkv_prepare(pa, mu, w0, w2, a0, a2, g2, k_k, k_a):
    pa = centred_shift(pa, mu)
    r, k, v, xw, xa, xg = jnp.split(pa, A_SPLITS, axis=-1)
    bsz, T, _ = r.shape
    xw = xw.reshape(bsz, T, 2, DECAY_LORA)
    xa = xa.reshape(bsz, T, 2, AAA_LORA)
    w_log = -jax.nn.softplus(-(w0 + jnp.einsum('btnr,nrc->btnc', jnp.tanh(xw), w2)).astype(jnp.float32)) - 0.5
    decay = jnp.exp(-jnp.exp(w_log))
    a = jax.nn.sigmoid(a0 + jnp.einsum('btnr,nrc->btnc', xa, a2))
    g = jax.nn.sigmoid(xg) @ g2
    kk = l2norm(heads(k * k_k, A_HEADS))
    k_eff = k[:, :, None] * (1 + (a - 1) * k_a)
    r_h, k_h, v_h = heads(r, A_HEADS), heads(k, A_HEADS), heads(v, A_HEADS)
    dec_h, keff_h, a_h = heads(decay, A_HEADS), heads(k_eff, A_HEADS), heads(a, A_HEADS)
    dir_args = [(r_h, dec_h[:, :, d], keff_h[:, :, d], v_h, kk, a_h[:, :, d]) for d in range(2)]
    return dir_args, (r_h, k_h, v_h, g)


def rwkv7_scan(S0, r, w, k, v, kk, a):
    def step(S, xs):
        r_t, w_t, k_t, v_t, kk_t, a_t = xs
        S = (S * w_t[:, :, None, :]
             - jnp.einsum('bhvk,bhk->bhv', S, kk_t)[..., None] * (kk_t * a_t)[:, :, None, :]
             + v_t[..., None] * k_t[:, :, None, :])
        return S, jnp.einsum('bhvk,bhk->bhv', S, r_t)
    xs = tuple(jnp.moveaxis(t.astype(jnp.float32), 1, 0) for t in (r, w, k, v, kk, a))
    S, y = lax.scan(step, S0, xs)
    return jnp.moveaxis(y, 0, 1), S


def rwkv_output(y, r_h, k_h, v_h, g, r_k, ln_g, ln_b):
    bsz, T = y.shape[:2]
    yn = layernorm(y, ln_g.reshape(A_HEADS, A_HEAD_DIM), ln_b.reshape(A_HEADS, A_HEAD_DIM), LN_X_EPS)
    bonus = jnp.sum(r_h * k_h * r_k, axis=-1, keepdims=True) * v_h
    return (yn + bonus).reshape(bsz, T, BRANCH_WIDTH) * g


def gdn_prepare(pb, conv_w, A_log, dt_bias):
    qkv, z, a_in, b_in = jnp.split(pb, B_SPLITS, axis=-1)
    qkv = jax.nn.silu(dwconv(qkv, conv_w))
    q, k, v = jnp.split(qkv, 3, axis=-1)
    bsz, T, _ = q.shape
    q = l2norm(heads(q, B_HEADS)) * B_HEAD_DIM ** -0.5
    k = l2norm(heads(k, B_HEADS))
    v = heads(v, B_HEADS)
    g = -jnp.exp(A_log.astype(jnp.float32)) * jax.nn.softplus(a_in.reshape(bsz, T, 2, B_HEADS).astype(jnp.float32) + dt_bias)
    beta = jax.nn.sigmoid(b_in.reshape(bsz, T, 2, B_HEADS))
    dir_args = [(q, k, v, g[:, :, d], beta[:, :, d]) for d in range(2)]
    return dir_args, z


def gdn_chunked(S0, q, k, v, g, beta):
    q, k, v, g, beta = (t.astype(jnp.float32) for t in (q, k, v, g, beta))
    bsz, T, H, dk = q.shape
    dv = v.shape[-1]
    n = T // GDN_CHUNK

    def chunked(t):
        t = t.reshape((bsz, n, GDN_CHUNK, H) + t.shape[3:])
        return jnp.moveaxis(jnp.swapaxes(t, 2, 3), 1, 0)

    q, k, v, g, beta = (chunked(t) for t in (q, k, v, g, beta))
    gc = jnp.cumsum(g, axis=-1)
    idx = jnp.arange(GDN_CHUNK)
    lower_incl = idx[:, None] >= idx[None, :]
    lower_strict = idx[:, None] > idx[None, :]
    diff = gc[..., :, None] - gc[..., None, :]
    decay_incl = jnp.where(lower_incl, jnp.exp(jnp.where(lower_incl, diff, 0.0)), 0.0)
    decay_strict = jnp.where(lower_strict, decay_incl, 0.0)
    kb = k * beta[..., None]
    L = jnp.einsum('nbhid,nbhjd->nbhij', kb, k) * decay_strict
    rhs = jnp.concatenate([v * beta[..., None], kb * jnp.exp(gc)[..., None]], axis=-1)
    sol = lax.linalg.triangular_solve(L + jnp.eye(GDN_CHUNK, dtype=jnp.float32), rhs,
                                      left_side=True, lower=True, unit_diagonal=True)
    u, w = sol[..., :dv], sol[..., dv:]
    attn = jnp.einsum('nbhid,nbhjd->nbhij', q, k) * decay_incl
    q_dec = q * jnp.exp(gc)[..., None]
    k_dec = k * jnp.exp(gc[..., -1:] - gc)[..., None]
    g_last = jnp.exp(gc[..., -1])

    def step(S, xs):
        u_c, w_c, attn_c, qd_c, kd_c, gl_c = xs
        v_new = u_c - jnp.einsum('bhik,bhkv->bhiv', w_c, S)
        o = jnp.einsum('bhik,bhkv->bhiv', qd_c, S) + jnp.einsum('bhij,bhjv->bhiv', attn_c, v_new)
        S = S * gl_c[..., None, None] + jnp.einsum('bhik,bhiv->bhkv', kd_c, v_new)
        return S, o

    S, o = lax.scan(step, S0, (u, w, attn, q_dec, k_dec, g_last))
    o = jnp.swapaxes(jnp.moveaxis(o, 0, 1), 2, 3).reshape(bsz, T, H, dv)
    return o, S


def gdn_output(o, z, norm_g):
    bsz, T = o.shape[:2]
    return (rmsnorm(o, norm_g) * jax.nn.silu(heads(z, B_HEADS))).reshape(bsz, T, BRANCH_WIDTH)


def conformer_branch(pc, dw, dw_b, ln_g, ln_b, on_grid):
    val, gate = jnp.split(pc, 2, axis=-1)
    u = val * jax.nn.sigmoid(gate)
    if on_grid:
        bsz, T, C = u.shape
        rows = T // GRID_W
        half = C // 2
        grid = u.reshape(bsz, rows, GRID_W, C)
        along_w = dwconv(grid[..., :half].reshape(bsz * rows, GRID_W, half), dw[:, :half])
        along_h = dwconv(jnp.swapaxes(grid[..., half:], 1, 2).reshape(bsz * GRID_W, rows, C - half), dw[:, half:])
        along_w = along_w.reshape(bsz, rows, GRID_W, half)
        along_h = jnp.swapaxes(along_h.reshape(bsz, GRID_W, rows, C - half), 1, 2)
        y = jnp.concatenate([along_w, along_h], axis=-1).reshape(bsz, T, C)
    else:
        y = dwconv(u, dw)
    return jax.nn.silu(layernorm(y + dw_b, ln_g, ln_b))


def merge_branches(ya, yb, yc, pg, w_branch, w_out):
    y = jnp.stack([ya, yb, yc], axis=2).astype(pg.dtype)
    up = jnp.einsum('btnc,ncd->btnd', y, w_branch)
    gates = jax.nn.sigmoid(pg.reshape(pg.shape[:-1] + (N_BRANCH, D_MODEL)))
    return jnp.sum(gates * up, axis=2) @ w_out


def mixer_sublayer(h, hc, with_ctx_out, w_in, rwkv_p, gdn_p, conf_p, w_branch, w_out):
    mu, w0, w2, a0, a2, g2, k_k, k_a, r_k, lnx_g, lnx_b = rwkv_p
    conv_w, A_log, dt_bias, gdn_g = gdn_p
    bsz = h.shape[0]
    p = h @ w_in
    pa, pb, pc, pg = jnp.split(p, IN_SPLITS, axis=-1)
    ctx_cols = N_IN if with_ctx_out else A_COLS + B_COLS
    p_ctx = hc @ w_in[:, :ctx_cols]
    pa_c, pb_c = p_ctx[..., :A_COLS], p_ctx[..., A_COLS:A_COLS + B_COLS]
    rw = (mu, w0, w2, a0, a2, g2, k_k, k_a)
    a_args, a_aux = rwkv_prepare(pa, *rw)
    a_args_c, a_aux_c = rwkv_prepare(pa_c, *rw)
    ya_c, ya = run_bidirectional(rwkv7_scan, a_args_c, a_args,
                                 jnp.zeros((bsz, A_HEADS, A_HEAD_DIM, A_HEAD_DIM), jnp.float32))
    b_args, z = gdn_prepare(pb, conv_w, A_log, dt_bias)
    b_args_c, z_c = gdn_prepare(pb_c, conv_w, A_log, dt_bias)
    yb_c, yb = run_bidirectional(gdn_chunked, b_args_c, b_args,
                                 jnp.zeros((bsz, B_HEADS, B_HEAD_DIM, B_HEAD_DIM), jnp.float32))
    mix = merge_branches(rwkv_output(ya, *a_aux, r_k, lnx_g, lnx_b), gdn_output(yb, z, gdn_g),
                         conformer_branch(pc, *conf_p, True), pg, w_branch, w_out)
    if not with_ctx_out:
        return mix, None
    pc_c = p_ctx[..., A_COLS + B_COLS:A_COLS + B_COLS + C_COLS]
    pg_c = p_ctx[..., A_COLS + B_COLS + C_COLS:]
    mix_c = merge_branches(rwkv_output(ya_c, *a_aux_c, r_k, lnx_g, lnx_b), gdn_output(yb_c, z_c, gdn_g),
                           conformer_branch(pc_c, *conf_p, False), pg_c, w_branch, w_out)
    return mix, mix_c


def moe_ffn(h, w_router, router_bias, w_gate, w_up, w_down):
    bsz, T, D = h.shape
    xf = h.reshape(bsz * T, D)
    N = xf.shape[0]
    scores = jax.nn.sigmoid(xf.astype(jnp.float32) @ w_router.astype(jnp.float32))
    sel = (scores + router_bias.astype(jnp.float32)).reshape(N, N_GROUPS, EXP_PER_GROUP)
    group_score = jnp.sum(lax.top_k(sel, TOP_K)[0], axis=-1)
    grp = jnp.argmax(group_score, axis=-1)
    sel_in_grp = jnp.take_along_axis(sel, grp[:, None, None], axis=1)[:, 0]
    _, local = lax.top_k(sel_in_grp, TOP_K)
    experts = grp[:, None] * EXP_PER_GROUP + local
    wts = jnp.take_along_axis(scores, experts, axis=1)
    wts = wts / jnp.sum(wts, axis=-1, keepdims=True)
    A = N * TOP_K
    flat_e = experts.reshape(A)
    flat_tok = jnp.repeat(jnp.arange(N, dtype=jnp.int32), TOP_K)
    flat_w = wts.reshape(A)
    order = jnp.argsort(flat_e)
    e_sorted = flat_e[order]
    sizes = jnp.bincount(flat_e, length=N_EXPERTS)
    starts = jnp.cumsum(sizes) - sizes
    padded = (sizes + MOE_BLOCK - 1) // MOE_BLOCK * MOE_BLOCK
    pad_ends = jnp.cumsum(padded)
    pad_starts = pad_ends - padded
    dest = pad_starts[e_sorted] + jnp.arange(A, dtype=jnp.int32) - starts[e_sorted]
    n_blocks = -(-A // MOE_BLOCK) + N_EXPERTS
    rows = n_blocks * MOE_BLOCK
    row_tok = jnp.full((rows,), N, jnp.int32).at[dest].set(flat_tok[order])
    row_w = jnp.zeros((rows,), jnp.float32).at[dest].set(flat_w[order])
    block_e = jnp.minimum(jnp.searchsorted(pad_ends, jnp.arange(n_blocks) * MOE_BLOCK, side='right'), N_EXPERTS - 1)
    x_pad = jnp.concatenate([xf, jnp.zeros((1, D), xf.dtype)], axis=0)
    xb = x_pad[row_tok].reshape(n_blocks, MOE_BLOCK, D)

    def expert_block(args):
        xblk, e = args
        return (jax.nn.silu(xblk @ w_gate[e]) * (xblk @ w_up[e])) @ w_down[e]

    yb = lax.map(expert_block, (xb, block_e)).reshape(rows, D)
    y = jnp.zeros((N + 1, D), yb.dtype).at[row_tok].add(yb * row_w[:, None].astype(yb.dtype))
    return y[:N].reshape(bsz, T, D)


def setup_inputs(seed: int = 0) -> dict:
    key = jax.random.key(seed)
    keys = jax.random.split(key, 36)
    L, D = DEPTH, D_MODEL

    def nrm(i, shape, scale):
        return jax.random.normal(keys[i], shape, jnp.float32) * scale

    def uni(i, shape, lo, hi):
        return jax.random.uniform(keys[i], shape, jnp.float32, lo, hi)

    dt = jnp.exp(uni(23, (L, 2, B_HEADS), math.log(1e-3), math.log(1e-1)))
    return {
        'x': nrm(0, (BATCH, SEQ, D), 1.0),
        'c': nrm(1, (BATCH, D), 1.0),
        'ctx': nrm(2, (BATCH, CTX_LEN, D), 1.0),
        'c_ctx': nrm(3, (D,), 1.0),
        'w_ada': nrm(4, (L, D, 6 * D), 0.5 * D ** -0.5),
        'b_ada': nrm(5, (L, 6 * D), 0.02),
        'norm_mix': 1.0 + nrm(6, (L, D), 0.02),
        'norm_ffn': 1.0 + nrm(7, (L, D), 0.02),
        'norm_final': 1.0 + nrm(8, (D,), 0.02),
        'w_in': nrm(9, (L, D, N_IN), D ** -0.5),
        'rwkv_mu': uni(10, (L, 2, A_COLS), 0.0, 0.5),
        'rwkv_w0': uni(11, (L, 2, BRANCH_WIDTH), -6.0, 0.0),
        'rwkv_w2': nrm(12, (L, 2, DECAY_LORA, BRANCH_WIDTH), 0.1 * DECAY_LORA ** -0.5),
        'rwkv_a0': uni(13, (L, 2, BRANCH_WIDTH), -1.0, 1.0),
        'rwkv_a2': nrm(14, (L, 2, AAA_LORA, BRANCH_WIDTH), 0.5 * AAA_LORA ** -0.5),
        'rwkv_g2': nrm(15, (L, GATE_LORA, BRANCH_WIDTH), GATE_LORA ** -0.5),
        'rwkv_kk': 0.85 + nrm(16, (L, BRANCH_WIDTH), 0.02),
        'rwkv_ka': 1.0 + nrm(17, (L, BRANCH_WIDTH), 0.02),
        'rwkv_rk': nrm(18, (L, A_HEADS, A_HEAD_DIM), 0.1),
        'rwkv_ln_g': 1.0 + nrm(19, (L, BRANCH_WIDTH), 0.02),
        'rwkv_ln_b': nrm(20, (L, BRANCH_WIDTH), 0.02),
        'gdn_conv': nrm(21, (L, SHORT_CONV, 3 * BRANCH_WIDTH), SHORT_CONV ** -0.5),
        'gdn_A_log': jnp.log(uni(22, (L, 2, B_HEADS), 1.0, 16.0)),
        'gdn_dt_bias': dt + jnp.log(-jnp.expm1(-dt)),
        'gdn_norm': 1.0 + nrm(24, (L, B_HEAD_DIM), 0.02),
        'conf_dw': nrm(25, (L, DW_CONV, BRANCH_WIDTH), DW_CONV ** -0.5),
        'conf_dw_b': nrm(26, (L, BRANCH_WIDTH), 0.02),
        'conf_ln_g': 1.0 + nrm(27, (L, BRANCH_WIDTH), 0.02),
        'conf_ln_b': nrm(28, (L, BRANCH_WIDTH), 0.02),
        'w_branch': nrm(29, (L, N_BRANCH, BRANCH_WIDTH, D), BRANCH_WIDTH ** -0.5),
        'w_out': nrm(30, (L, D, D), D ** -0.5),
        'w_router': nrm(31, (D, N_EXPERTS), D ** -0.5),
        'router_bias': nrm(32, (N_EXPERTS,), 0.01),
        'w_e_gate': nrm(33, (L, N_EXPERTS, D, D_EXPERT), D ** -0.5),
        'w_e_up': nrm(34, (L, N_EXPERTS, D, D_EXPERT), D ** -0.5),
        'w_e_down': nrm(35, (L, N_EXPERTS, D_EXPERT, D), D_EXPERT ** -0.5),
    }


def reference(x, c, ctx, c_ctx, w_ada, b_ada, norm_mix, norm_ffn, norm_final, w_in,
              rwkv_mu, rwkv_w0, rwkv_w2, rwkv_a0, rwkv_a2, rwkv_g2, rwkv_kk, rwkv_ka, rwkv_rk,
              rwkv_ln_g, rwkv_ln_b, gdn_conv, gdn_A_log, gdn_dt_bias, gdn_norm,
              conf_dw, conf_dw_b, conf_ln_g, conf_ln_b, w_branch, w_out,
              w_router, router_bias, w_e_gate, w_e_up, w_e_down):
    silu_c = jax.nn.silu(c)
    silu_cc = jax.nn.silu(c_ctx)
    xc = ctx
    for l in range(DEPTH):
        with_ctx_out = l < DEPTH - 1
        mod = jnp.split((silu_c @ w_ada[l] + b_ada[l])[:, None, :], 6, axis=-1)
        mod_c = jnp.split(silu_cc @ w_ada[l] + b_ada[l], 6, axis=-1)
        h = modulate(x, norm_mix[l], mod[0], mod[1])
        hc = modulate(xc, norm_mix[l], mod_c[0], mod_c[1])
        rwkv_p = (rwkv_mu[l], rwkv_w0[l], rwkv_w2[l], rwkv_a0[l], rwkv_a2[l], rwkv_g2[l],
                  rwkv_kk[l], rwkv_ka[l], rwkv_rk[l], rwkv_ln_g[l], rwkv_ln_b[l])
        gdn_p = (gdn_conv[l], gdn_A_log[l], gdn_dt_bias[l], gdn_norm[l])
        conf_p = (conf_dw[l], conf_dw_b[l], conf_ln_g[l], conf_ln_b[l])
        mix, mix_c = mixer_sublayer(h, hc, with_ctx_out, w_in[l], rwkv_p, gdn_p, conf_p, w_branch[l], w_out[l])
        x = x + mod[2] * mix
        x = x + mod[5] * moe_ffn(modulate(x, norm_ffn[l], mod[3], mod[4]),
                                 w_router, router_bias, w_e_gate[l], w_e_up[l], w_e_down[l])
        if with_ctx_out:
            xc = xc + mod_c[2] * mix_c
            xc = xc + mod_c[5] * moe_ffn(modulate(xc, norm_ffn[l], mod_c[3], mod_c[4]),
                                         w_router, router_bias, w_e_gate[l], w_e_up[l], w_e_down[l])
    return rmsnorm(x, norm_final)
```

```python
import numpy as np
from contextlib import ExitStack
import concourse.bass as bass
import concourse.mybir as mybir
from concourse.bass_utils import run_bass_kernel_spmd

F32 = mybir.dt.float32
BF16 = mybir.dt.bfloat16
ALU = mybir.AluOpType
AF = mybir.ActivationFunctionType
AX = mybir.AxisListType

D = 1024
S = 2
TC = 256
TL = 2048
T = TC + TL
NT = S * T
L = 2
NIN = 8080
NDS = 24


class Dep:
    __slots__ = ("w", "r")

    def __init__(self):
        self.w = None
        self.r = {}


class Tl:
    def __init__(self, t):
        self.t = t
        self.d = Dep()

    def __getitem__(self, k):
        return self.t[k]


class Eng:
    def __init__(self, name, be, sem):
        self.key = name
        self.be = be
        self.sem = sem
        self.n = 0
        self.waited = {}


def _d(x):
    return x.d if hasattr(x, "d") else x


class KB:
    def __init__(self, dbg=()):
        self.nc = nc = bass.Bass("TRN2", target_bir_lowering=False)
        self.es = ExitStack()
        self.dbg = set(dbg)
        e = self.es.enter_context
        self.pe = Eng("pe", nc.tensor, e(nc.semaphore("s_pe")))
        self.act = Eng("act", nc.scalar, e(nc.semaphore("s_act")))
        self.dve = Eng("dve", nc.vector, e(nc.semaphore("s_dve")))
        self.pool = Eng("pool", nc.gpsimd, e(nc.semaphore("s_pool")))
        self.sp = Eng("sp", nc.sync, e(nc.semaphore("s_sp")))
        self.engs = [self.pe, self.act, self.dve, self.pool, self.sp]
        self.dsem = [e(nc.semaphore(f"s_d{i}")) for i in range(NDS)]
        self.dcnt = [0] * NDS
        self.drr = 0
        self.ddeps = {}
        self.ntile = 0

    def sb(self, shape, dt=F32, es=None):
        self.ntile += 1
        t = (es or self.es).enter_context(self.nc.sbuf_tensor(f"t{self.ntile}", list(shape), dt))
        return Tl(t)

    def psum(self, shape, dt=F32, es=None):
        self.ntile += 1
        t = (es or self.es).enter_context(self.nc.psum_tensor(f"p{self.ntile}", list(shape), dt))
        return Tl(t)

    def dram(self, name, shape, dt=F32, kind=None):
        if kind is None:
            kind = "ExternalOutput" if name in self.dbg else "Internal"
        return self.nc.dram_tensor(name, list(shape), dt, kind=kind).ap()

    def dd(self, *key):
        d = self.ddeps.get(key)
        if d is None:
            d = self.ddeps[key] = Dep()
        return d

    def dr(self, name, n0, w):
        return [self.dd(name, i) for i in range(n0 // 128, (n0 + w + 127) // 128)]

    def _sync(self, E, R, W):
        need = {}

        def upd(tok):
            k, sem, val = tok
            if k not in need or need[k][1] < val:
                need[k] = (sem, val)

        for d in R:
            d = _d(d)
            if d.w:
                upd(d.w)
        for d in W:
            d = _d(d)
            if d.w:
                upd(d.w)
            for k, (sem, val) in d.r.items():
                if k != E.key:
                    upd((k, sem, val))
        for k, (sem, val) in need.items():
            if k == E.key and E is self.pe:
                continue
            if E.waited.get(k, 0) < val:
                E.be.wait_ge(sem, val)
                E.waited[k] = val

    def _mark(self, tok, R, W):
        k, sem, val = tok
        for d in R:
            _d(d).r[k] = (sem, val)
        for d in W:
            d = _d(d)
            d.w = tok
            d.r = {}

    def op(self, E, fn, R, W):
        self._sync(E, R, W)
        ins = fn()
        E.n += 1
        ins.then_inc(E.sem, 1)
        self._mark((E.key, E.sem, E.n), R, W)

    def dma(self, out, in_, R, W, Q=None, **kw):
        Q = Q or self.sp
        self._sync(Q, R, W)
        s = self.drr
        self.drr = (s + 1) % NDS
        sem = self.dsem[s]
        k = ("d", s)
        if self.dcnt[s] > 0 and Q.waited.get(k, 0) < 16 * self.dcnt[s]:
            Q.be.wait_ge(sem, 16 * self.dcnt[s])
            Q.waited[k] = 16 * self.dcnt[s]
        Q.be.dma_start(out=out, in_=in_, **kw).then_inc(sem, 16)
        self.dcnt[s] += 1
        self._mark((k, sem, 16 * self.dcnt[s]), R, W)

    def barrier(self):
        for E in self.engs:
            for E2 in self.engs:
                if E2 is not E and E2.n > 0 and E.waited.get(E2.key, 0) < E2.n:
                    E.be.wait_ge(E2.sem, E2.n)
                    E.waited[E2.key] = E2.n
            for s in range(NDS):
                k = ("d", s)
                if self.dcnt[s] > 0 and E.waited.get(k, 0) < 16 * self.dcnt[s]:
                    E.be.wait_ge(self.dsem[s], 16 * self.dcnt[s])
                    E.waited[k] = 16 * self.dcnt[s]

    def mm(self, out, lhsT, rhs, start, stop, R, W):
        self.op(self.pe, lambda: self.nc.tensor.matmul(out, lhsT=lhsT, rhs=rhs, start=start, stop=stop), R, W)

    def tr(self, out, in_, ident, R, W):
        self.op(self.pe, lambda: self.nc.tensor.transpose(out, in_, ident), R, W)

    def actf(self, out, in_, func, R, W, bias=None, scale=None):
        kw = {}
        if bias is not None:
            kw["bias"] = bias
        if scale is not None:
            kw["scale"] = scale
        self.op(self.act, lambda: self.nc.scalar.activation(out=out, in_=in_, func=func, **kw), R, W)

    def ts(self, E, out, in0, s1, s2, op0, op1, R, W):
        if op1 is None:
            self.op(E, lambda: E.be.tensor_scalar(out=out, in0=in0, scalar1=s1, scalar2=None, op0=op0), R, W)
        else:
            self.op(E, lambda: E.be.tensor_scalar(out=out, in0=in0, scalar1=s1, scalar2=s2, op0=op0, op1=op1), R, W)

    def tt(self, E, out, in0, in1, op, R, W):
        self.op(E, lambda: E.be.tensor_tensor(out=out, in0=in0, in1=in1, op=op), R, W)

    def stt(self, E, out, in0, scalar, in1, op0, op1, R, W):
        self.op(E, lambda: E.be.scalar_tensor_tensor(out=out, in0=in0, scalar=scalar, in1=in1, op0=op0, op1=op1), R, W)

    def cp(self, E, out, in_, R, W):
        if E is self.act:
            self.op(E, lambda: self.nc.scalar.copy(out=out, in_=in_), R, W)
        else:
            self.op(E, lambda: E.be.tensor_copy(out=out, in_=in_), R, W)

    def recip(self, out, in_, R, W):
        self.op(self.dve, lambda: self.nc.vector.reciprocal(out=out, in_=in_), R, W)

    def memset(self, E, ap, v, W):
        self.op(E, lambda: E.be.memset(ap, v), [], W)


class PF:
    def __init__(self):
        self.cols = {}
        self.n = 0

    def add(self, name, nch):
        self.cols[name] = (self.n, nch)
        self.n += nch
        return self.cols[name][0]


def pf_layout():
    pf = PF()
    for nm, nch in [("norm_mix", 8), ("norm_ffn", 8), ("b_ada", 48), ("mu0", 15), ("mu1", 15),
                    ("w0_0", 4), ("w0_1", 4), ("a0_0", 4), ("a0_1", 4), ("kk", 4), ("ka", 4), ("rk", 4),
                    ("ln_g", 4), ("ln_b", 4), ("gconv", 60), ("gnorm", 1), ("alog", 1), ("dtb", 1),
                    ("cdw", 124), ("cdwb", 4), ("clng", 4), ("clnb", 4), ("norm_final", 8)]:
        pf.add(nm, nch)
    return pf


def _fm(v, nch):
    return np.ascontiguousarray(np.asarray(v, np.float32).reshape(nch, 128).T)


def pack_pf(inp, l):
    pf = pf_layout()
    out = np.zeros((128, pf.n), np.float32)

    def put(nm, arr):
        o, n = pf.cols[nm]
        out[:, o:o + n] = arr

    put("norm_mix", _fm(inp["norm_mix"][l], 8))
    put("norm_ffn", _fm(inp["norm_ffn"][l], 8))
    put("b_ada", _fm(inp["b_ada"][l], 48))
    put("mu0", _fm(inp["rwkv_mu"][l, 0], 15))
    put("mu1", _fm(inp["rwkv_mu"][l, 1], 15))
    for d in range(2):
        put(f"w0_{d}", _fm(inp["rwkv_w0"][l, d], 4))
        put(f"a0_{d}", _fm(inp["rwkv_a0"][l, d], 4))
    put("kk", _fm(inp["rwkv_kk"][l], 4))
    put("ka", _fm(inp["rwkv_ka"][l], 4))
    put("rk", _fm(inp["rwkv_rk"][l].reshape(-1), 4))
    put("ln_g", _fm(inp["rwkv_ln_g"][l], 4))
    put("ln_b", _fm(inp["rwkv_ln_b"][l], 4))
    gc = np.concatenate([_fm(inp["gdn_conv"][l, k], 12) for k in range(5)], axis=1)
    put("gconv", gc)
    put("gnorm", _fm(inp["gdn_norm"][l], 1))
    al = np.zeros((128, 1), np.float32); al[0:8, 0] = np.asarray(inp["gdn_A_log"][l]).reshape(-1)
    db = np.zeros((128, 1), np.float32); db[0:8, 0] = np.asarray(inp["gdn_dt_bias"][l]).reshape(-1)
    put("alog", al)
    put("dtb", db)
    cd = np.concatenate([_fm(inp["conf_dw"][l, k], 4) for k in range(31)], axis=1)
    put("cdw", cd)
    put("cdwb", _fm(inp["conf_dw_b"][l], 4))
    put("clng", _fm(inp["conf_ln_g"][l], 4))
    put("clnb", _fm(inp["conf_ln_b"][l], 4))
    put("norm_final", _fm(inp["norm_final"], 8))
    return out


EPS = 1e-6


class Prog(KB):
    def __init__(self, dbg=(), stop=None, nlayers=L):
        super().__init__(dbg)
        self.stop = stop
        self.nlayers = nlayers
        nc = self.nc
        self.pf = pf_layout()

        def inp(name, shape):
            return nc.dram_tensor(name, list(shape), F32, kind="ExternalInput").ap()

        self.x = inp("x", [S, TL, D])
        self.ctx = inp("ctx", [S, TC, D])
        self.cvecT = inp("cvecT", [D, 3])
        self.pfp = inp("pfp", [L, 128, self.pf.n])
        self.w_ada = inp("w_ada", [L, D, 6 * D])
        self.w_in = inp("w_in", [L, D, NIN])
        self.rw2 = inp("rwkv_w2", [L, 2, 64, 512])
        self.ra2 = inp("rwkv_a2", [L, 2, 64, 512])
        self.rg2 = inp("rwkv_g2", [L, 128, 512])
        self.w_branch = inp("w_branch", [L, 3, 512, D])
        self.w_out = inp("w_out", [L, D, D])
        self.w_router = inp("w_router", [D, 16])
        self.rbias = inp("rbias", [128, 16])
        self.weg = inp("w_e_gate", [L, 16, D, 512])
        self.weu = inp("w_e_up", [L, 16, D, 512])
        self.wed = inp("w_e_down", [L, 16, 512, D])
        self.y = nc.dram_tensor("y", [S, TL, D], F32, kind="ExternalOutput").ap()
        self.XT = self.dram("XT", [D, NT])
        self.XTv = self.XT.rearrange("(c p) n -> p c n", p=128)
        self.PT = self.dram("PT", [NIN, NT])
        self.ps = [self.psum([128, 512], F32) for _ in range(8)]
        self.ident = self.sb([128, 128], F32)
        self.identb = self.sb([128, 128], BF16)
        self.ones = self.sb([128, 128], F32)
        self.PFt = [self.sb([128, self.pf.n], F32) for _ in range(L)]
        self.scT = self.sb([128, 8, 3], F32)
        self.MOD = [self.sb([128, 48, 3], F32) for _ in range(L)]
        self.GM = [self.sb([128, 8, 3], F32) for _ in range(L)]
        self.GF = [self.sb([128, 8, 3], F32) for _ in range(L)]

    def pfv(self, l, name, c=None, n=1):
        o, nch = self.pf.cols[name]
        if c is None:
            return self.PFt[l][:, o:o + nch]
        return self.PFt[l][:, o + c:o + c + n]

    def consts(self):
        nc = self.nc
        P = self.pool
        self.memset(P, self.ident[:, :], 0.0, [self.ident])
        self.op(P, lambda: nc.gpsimd.affine_select(out=self.ident[:, :], in_=self.ident[:, :], pattern=[[-1, 128]],
                                                   compare_op=ALU.not_equal, fill=1.0, base=0, channel_multiplier=1),
                [self.ident], [self.ident])
        self.cp(P, self.identb[:, :], self.ident[:, :], [self.ident], [self.identb])
        self.memset(P, self.ones[:, :], 1.0, [self.ones])
        for l in range(L):
            self.dma(self.PFt[l][:, :], self.pfp[l], [], [self.PFt[l]])
        cv = self.sb([128, 8, 3], F32)
        self.dma(cv[:, :, :], self.cvecT.rearrange("(c p) r -> p c r", p=128), [], [cv])
        self.actf(self.scT[:, :, :], cv[:, :, :], AF.Silu, [cv], [self.scT])

    def phase0(self):
        with ExitStack() as es:
            xin = [self.sb([128, 1024], F32, es) for _ in range(2)]
            xo = [self.sb([128, 8, 128], F32, es) for _ in range(2)]
            i = 0
            for s in range(S):
                for j in range(T // 128):
                    n0 = s * T + j * 128
                    src = self.ctx[s, j * 128:(j + 1) * 128, :] if j < 2 else self.x[s, (j - 2) * 128:(j - 1) * 128, :]
                    a = xin[i % 2]
                    o = xo[i % 2]
                    self.dma(a[:, :], src, [], [a])
                    for c in range(8):
                        pb = self.ps[(i % 2) * 2 + c // 4]
                        self.tr(pb[:, (c % 4) * 128:(c % 4 + 1) * 128], a[:, c * 128:(c + 1) * 128], self.ident[:, :],
                                [a, self.ident], [pb])
                    for hf in range(2):
                        pb = self.ps[(i % 2) * 2 + hf]
                        self.cp(self.act if hf == 0 else self.dve, o[:, hf * 4:(hf + 1) * 4, :],
                                pb[:, :].rearrange("p (c t) -> p c t", c=4), [pb], [o])
                    self.dma(self.XTv[:, :, n0:n0 + 128], o[:, :, :], [o], self.dr("XT", n0, 128))
                    i += 1
            self.barrier()

    def phaseA(self, l):
        with ExitStack() as es:
            wa = [self.sb([128, 8, 768], F32, es) for _ in range(2)]
            pm = self.ps[0]
            wav = self.w_ada[l].rearrange("(k p) n -> p k n", p=128)
            for mg in range(8):
                w = wa[mg % 2]
                for q in range(4):
                    self.dma(w[:, q * 2:(q + 1) * 2, :], wav[:, q * 2:(q + 1) * 2, mg * 768:(mg + 1) * 768], [], [w])
                for m in range(6):
                    mm_ = mg * 6 + m
                    for k in range(8):
                        self.mm(pm[:, mm_ * 3:(mm_ + 1) * 3], w[:, k, m * 128:(m + 1) * 128], self.scT[:, k, :], k == 0, k == 7,
                                [w, self.scT], [pm])
            mod = self.MOD[l]
            self.tt(self.dve, mod[:, :, :], pm[:, 0:144].rearrange("p (m r) -> p m r", r=3),
                    self.pfv(l, "b_ada").unsqueeze(2).to_broadcast([128, 48, 3]), ALU.add, [pm, self.PFt[l]], [mod])
            for (G, mi, nm) in ((self.GM[l], 1, "norm_mix"), (self.GF[l], 4, "norm_ffn")):
                self.ts(self.dve, G[:, :, :], mod[:, mi * 8:(mi + 1) * 8, :], 1.0, None, ALU.add, None, [mod], [G])
                self.dump(f"G1{l}{mi}", G, G[:, :, :], [128, 8, 3])
                self.tt(self.dve, G[:, :, :], G[:, :, :], self.pfv(l, nm).unsqueeze(2).to_broadcast([128, 8, 3]), ALU.mult,
                        [G, self.PFt[l]], [G])
            self.dump(f"MOD{l}", mod, mod[:, :, :], [128, 48, 3])
            self.dump(f"GM{l}", self.GM[l], self.GM[l][:, :, :], [128, 8, 3])
            self.barrier()

    def tiles(self):
        out = []
        for s in range(S):
            out.append((s * T, TC, 2))
            for j in range(TL // 512):
                out.append((s * T + TC + j * 512, 512, s))
        return out

    def modulate_tile(self, es_bufs, n0, w, r, G, shift_col, mod, out_fn):
        xt, sq, rs, tmp = es_bufs
        psS = self.ps[7]
        self.dma(xt[:, :, :w], self.XTv[:, :, n0:n0 + w], self.dr("XT", n0, w), [xt])
        self.actf(sq[:, :, :w], xt[:, :, :w], AF.Square, [xt], [sq])
        for c in range(8):
            self.mm(psS[:, :w], self.ones[:, :], sq[:, c, :w], c == 0, c == 7, [self.ones, sq], [psS])
        self.ts(self.dve, rs[:, :w], psS[:, :w], 1.0 / D, EPS, ALU.mult, ALU.add, [psS], [rs])
        self.actf(rs[:, :w], rs[:, :w], AF.Sqrt, [rs], [rs])
        self.recip(rs[:, :w], rs[:, :w], [rs], [rs])
        return xt, rs

    def phaseB(self, l):
        with ExitStack() as es:
            hT = self.sb([128, 8, NT], BF16, es)
            with ExitStack() as es1:
                xts = [self.sb([128, 8, 512], F32, es1) for _ in range(2)]
                sq = self.sb([128, 8, 512], F32, es1)
                rs = self.sb([128, 512], F32, es1)
                tmps = [self.sb([128, 512], F32, es1) for _ in range(2)]
                for i, (n0, w, r) in enumerate(self.tiles()):
                    xt, _ = self.modulate_tile((xts[i % 2], sq, rs, None), n0, w, r, None, None, None, None)
                    for c in range(8):
                        tmp = tmps[c % 2]
                        self.stt(self.dve, tmp[:, :w], xt[:, c, :w], self.GM[l][:, c, r:r + 1], rs[:, :w], ALU.mult, ALU.mult,
                                 [xt, rs, self.GM[l]], [tmp])
                        self.actf(hT[:, c, n0:n0 + w], tmp[:, :w], AF.Identity, [tmp, self.MOD[l]], [hT],
                                  bias=self.MOD[l][:, c, r:r + 1], scale=1.0)
                if "HT" in self.dbg:
                    hd = self.dram("HT", [128, 8, NT], BF16)
                    self.dma(hd, hT[:, :, :], [hT], [self.dd("HTd")])
                self.barrier()
            wbf = [self.sb([128, 8, 1024], BF16, es) for _ in range(2)]
            ost = [self.sb([128, 512], F32, es) for _ in range(4)]
            no = 0
            for g in range(8):
                c0 = g * 1024
                cw = min(1024, NIN - c0)
                wb = wbf[g % 2]
                self.dma(wb[:, :, :cw], self.w_in[l][:, c0:c0 + cw].rearrange("(k p) n -> p k n", p=128), [], [wb], Q=self.pool)
                nm = (cw + 127) // 128
                for tt_ in range(NT // 512):
                    n0 = tt_ * 512
                    for m in range(nm):
                        mw = min(128, cw - m * 128)
                        pb = self.ps[no % 6]
                        for k in range(8):
                            self.mm(pb[:mw, :], wb[:, k, m * 128:m * 128 + mw], hT[:, k, n0:n0 + 512], k == 0, k == 7,
                                    [wb, hT], [pb])
                        o = ost[no % 4]
                        self.cp(self.act if no % 2 == 0 else self.dve, o[:mw, :], pb[:mw, :], [pb], [o])
                        self.dma(self.PT[c0 + m * 128:c0 + m * 128 + mw, n0:n0 + 512], o[:mw, :], [o],
                                 [self.dd("PT", (c0 + m * 128) // 128, j) for j in range(n0 // 128, n0 // 128 + 4)])
                        no += 1
            self.barrier()

    def dump(self, name, tile, ap, shape, dt=F32):
        if name in self.dbg:
            d = self.dram(name, shape, dt)
            self.dma(d, ap, [tile], [self.dd(name)])

    def finish(self):
        self.barrier()

    def build(self):
        self.consts()
        self.phase0()
        if self.stop == "0":
            return self.finish()
        for l in range(self.nlayers):
            self.phaseA(l)
            if self.stop == f"A{l}":
                return self.finish()
            self.phaseB(l)
            if self.stop == f"B{l}":
                return self.finish()
        self.finish()


def make_in_maps(inp):
    ncores = 8
    pfp = np.stack([pack_pf(inp, l) for l in range(L)])
    rbias = np.ascontiguousarray(np.broadcast_to(np.asarray(inp["router_bias"], np.float32)[None, :], (128, 16)))
    maps = []
    for i in range(ncores):
        cv = np.stack([inp["c"][2 * i], inp["c"][2 * i + 1], inp["c_ctx"]], axis=1).astype(np.float32)
        m = {
            "x": np.ascontiguousarray(inp["x"][2 * i:2 * i + 2]),
            "ctx": np.ascontiguousarray(inp["ctx"][2 * i:2 * i + 2]),
            "cvecT": np.ascontiguousarray(cv),
            "pfp": pfp, "rbias": rbias,
        }
        for k in ("w_ada", "w_in", "rwkv_w2", "rwkv_a2", "rwkv_g2", "w_branch", "w_out", "w_router",
                  "w_e_gate", "w_e_up", "w_e_down"):
            m[k] = np.ascontiguousarray(inp[k], dtype=np.float32)
        maps.append(m)
    return maps


def kernel(**inputs):
    inp = {k: np.asarray(v) for k, v in inputs.items()}
    prog = Prog2()
    prog.build()
    maps = make_in_maps(inp)
    res = run_bass_kernel_spmd(prog.nc, maps, core_ids=list(range(8)))
    return np.concatenate([r["y"] for r in res.results], axis=0).astype(np.float32)


CDEC = 0.6065306597126334
NJ = T // 128


def seg_bounds(j):
    return (j == 0 or j == 2), (j == 1 or j == NJ - 1)


class StopBuild(Exception):
    pass


class Prog2(Prog):
    cut = None

    def ck(self, n):
        if self.cut == n:
            raise StopBuild()

    def __init__(self, **kw):
        super().__init__(**kw)
        self.PTv = self.PT[0:8064, :].rearrange("(c p) n -> p c n", p=128)
        self.RKQ = self.dram("RKQ", [S, 2, NJ, 128, 4 * 256], BF16)
        self.RMAT = self.dram("RMAT", [S, 2, NJ, 128, 8 * 512], BF16)
        self.RBC = self.dram("RBC", [S, 2, NJ, 128, 1024], BF16)
        self.RV = self.dram("RV", [S, NJ, 128, 512], BF16)
        self.RPC = self.dram("RPC", [S, 2, NJ, 128, 8], F32)
        self.GAs = self.dram("GAs", [512, NT])
        self.BON = self.dram("BON", [512, NT])
        self.YA = self.dram("YA", [2, NT, 512])
        self.bones = self.sb([128, 128], F32)
        self.MU = self.sb([128, 256], F32)
        self.ML = self.sb([128, 256], F32)
        self.RM = self.sb([128, 128], F32)

    def consts(self):
        super().consts()
        nc = self.nc
        P = self.pool
        self.memset(P, self.bones[:, :], 0.0, [self.bones])
        self.memset(P, self.bones[0:64, 0:64], 1.0, [self.bones])
        self.memset(P, self.bones[64:128, 64:128], 1.0, [self.bones])
        self.memset(P, self.RM[:, :], 1.0, [self.RM])
        self.memset(P, self.RM[:, 0:1], 0.0, [self.RM])
        self.memset(P, self.RM[:, 64:65], 0.0, [self.RM])
        for (Mt, off, cmp_, sg) in ((self.MU, 0, ALU.is_gt, 1), (self.MU, 128, ALU.is_ge, 1), (self.ML, 0, ALU.is_gt, -1), (self.ML, 128, ALU.is_ge, -1)):
            sl = Mt[:, off:off + 128]
            self.memset(P, sl, 1.0, [Mt])
            self.op(P, lambda sl=sl, cmp_=cmp_, sg=sg: nc.gpsimd.affine_select(out=sl, in_=sl, pattern=[[sg, 128]], compare_op=cmp_, fill=0.0,
                                                                              base=0, channel_multiplier=-sg), [Mt], [Mt])
            self.memset(P, Mt[0:64, off + 64:off + 128], 0.0, [Mt])
            self.memset(P, Mt[64:128, off:off + 64], 0.0, [Mt])

    def load_halo(self, dst, c0, nch, s, j, hw):
        n0 = s * T + j * 128
        lb, rb = seg_bounds(j)
        lo = 0 if lb else hw
        hi = 0 if rb else hw
        if lb:
            self.memset(self.pool, dst[:, :, 0:hw], 0.0, [dst])
        if rb:
            self.memset(self.pool, dst[:, :, 128 + hw:128 + 2 * hw], 0.0, [dst])
        deps = [self.dd("PT", c, i) for c in range(c0, c0 + nch) for i in range((n0 - lo) // 128, (n0 + 128 + hi - 1) // 128 + 1)]
        self.dma(dst[:, :, hw - lo:hw + 128 + hi], self.PTv[:, c0:c0 + nch, n0 - lo:n0 + 128 + hi], deps, [dst])

    def inverse(self, X1, NN, MAT, h, Wt, At, Bt, pA, pB_, pC):
        V, G = self.dve, self.pool
        W, A, B = Wt[0], At[0], Bt[0]
        self.tt(G, W[:, :], self.identb[:, :], X1[:, 0:128], ALU.subtract, [self.identb, X1], [W])
        self.mm(pA[:, 0:128], X1[:, 0:128], NN[:, :], True, True, [X1, NN], [pA])
        self.mm(pB_[:, 0:128], NN[:, :], X1[:, 0:128], True, True, [X1, NN], [pB_])
        self.cp(self.act, A[:, :], pA[:, 0:128], [pA], [A])
        self.cp(self.act, B[:, :], pB_[:, 0:128], [pB_], [B])
        for it in range(5):
            W2, A2, B2 = Wt[(it + 1) % 2], At[(it + 1) % 2], Bt[(it + 1) % 2]
            self.mm(pC[:, 0:128], A[:, :], W[:, :], True, True, [A, W], [pC])
            if it < 4:
                self.mm(pA[:, 0:128], B[:, :], A[:, :], True, True, [A, B], [pA])
                self.mm(pB_[:, 0:128], A[:, :], B[:, :], True, True, [A, B], [pB_])
            dstW = W2[:, :] if it < 4 else MAT[:, h, 0:128]
            self.tt(V, dstW, W[:, :], pC[:, 0:128], ALU.add, [W, pC], [W2 if it < 4 else MAT])
            if it < 4:
                self.cp(self.act, A2[:, :], pA[:, 0:128], [pA], [A2])
                self.cp(self.act, B2[:, :], pB_[:, 0:128], [pB_], [B2])
            W, A, B = W2, A2, B2

    def inverse_batch(self, X1a, NNa, MAT, nh, Wt, At, Bt):
        V, G = self.dve, self.pool
        nb = nh // 4
        psW, psA, psB = self.ps[0:nb], self.ps[2:2 + nb], self.ps[4:4 + nb]

        def reg(pl, h):
            return pl[h // 4][:, (h % 4) * 128:(h % 4 + 1) * 128]

        def bv(p):
            return p[:, :].rearrange("p (h t) -> p h t", h=4)
        W, A, B = Wt[0], At[0], Bt[0]
        self.tt(G, W[:, :, :], self.identb[:, :].unsqueeze(1).to_broadcast([128, nh, 128]), X1a[:, :, 0:128], ALU.subtract, [self.identb, X1a], [W])
        for h in range(nh):
            self.mm(reg(psA, h), X1a[:, h, 0:128], NNa[:, h, :], True, True, [X1a, NNa], [psA[h // 4]])
        for h in range(nh):
            self.mm(reg(psB, h), NNa[:, h, :], X1a[:, h, 0:128], True, True, [X1a, NNa], [psB[h // 4]])
        for b in range(nb):
            self.cp(self.act, A[:, 4 * b:4 * b + 4, :], bv(psA[b]), [psA[b]], [A])
            self.cp(V, B[:, 4 * b:4 * b + 4, :], bv(psB[b]), [psB[b]], [B])
        for it in range(5):
            W2, A2, B2 = Wt[(it + 1) % 2], At[(it + 1) % 2], Bt[(it + 1) % 2]
            for h in range(nh):
                self.mm(reg(psW, h), A[:, h, :], W[:, h, :], True, True, [A, W], [psW[h // 4]])
            if it < 4:
                for h in range(nh):
                    self.mm(reg(psA, h), B[:, h, :], A[:, h, :], True, True, [A, B], [psA[h // 4]])
                for h in range(nh):
                    self.mm(reg(psB, h), A[:, h, :], B[:, h, :], True, True, [A, B], [psB[h // 4]])
            for b in range(nb):
                if it < 4:
                    self.tt(V, W2[:, 4 * b:4 * b + 4, :], W[:, 4 * b:4 * b + 4, :], bv(psW[b]), ALU.add, [W, psW[b]], [W2])
                else:
                    self.tt(V, MAT[:, 4 * b:4 * b + 4, 0:128], W[:, 4 * b:4 * b + 4, :], bv(psW[b]), ALU.add, [W, psW[b]], [MAT])
            if it < 4:
                for b in range(nb):
                    self.cp(self.act, A2[:, 4 * b:4 * b + 4, :], bv(psA[b]), [psA[b]], [A2])
                    self.cp(V, B2[:, 4 * b:4 * b + 4, :], bv(psB[b]), [psB[b]], [B2])
            W, A, B = W2, A2, B2

    def rwkv_prep(self, l):
        nc = self.nc
        V, G = self.dve, self.pool
        with ExitStack() as es:
            def t(shape, dt=F32):
                return self.sb(shape, dt, es)
            wtmp = t([128, 512])
            w2b, a2b, g2b = t([128, 512], BF16), t([128, 512], BF16), t([128, 512], BF16)
            for (src, dstb) in ((self.rw2[l].rearrange("d r c -> (d r) c"), w2b), (self.ra2[l].rearrange("d r c -> (d r) c"), a2b), (self.rg2[l], g2b)):
                self.dma(wtmp[:, :], src, [], [wtmp])
                self.cp(V, dstb[:, :], wtmp[:, :], [wtmp], [dstb])
            PFl = self.PFt[l]
            c0t = t([128, 15])
            self.tt(V, c0t[:, :], self.pfv(l, "mu0"), self.pfv(l, "mu1"), ALU.add, [PFl], [c0t])
            self.ts(V, c0t[:, :], c0t[:, :], -1.0, 1.0, ALU.mult, ALU.add, [c0t], [c0t])
            omka = t([128, 4])
            self.ts(V, omka[:, :], self.pfv(l, "ka"), -1.0, 1.0, ALU.mult, ALU.add, [PFl], [omka])

            def bc(ap, n):
                return ap.unsqueeze(2).to_broadcast([128, n, 128])

            pa = t([128, 15, 130])
            sh = t([128, 15, 128])
            t1 = t([128, 15, 128])
            twb, xab, sgb = t([128, 128], BF16), t([128, 128], BF16), t([128, 128], BF16)
            SW = [t([128, 4, 128]) for _ in range(2)]
            AA = [t([128, 4, 128]) for _ in range(2)]
            ga = t([128, 4, 128])
            kx, kk, tq, bon = t([128, 4, 128]), t([128, 4, 128]), t([128, 4, 128]), t([128, 4, 128])
            CS, EX, IN_, tmpa, tmpb = (t([128, 4, 128]) for _ in range(5))
            e1, e2, e3 = (t([128, 4, 128]) for _ in range(3))
            pc = t([128, 8])
            KQ = t([128, 4, 256], BF16)
            BTb, CTb = t([128, 4, 128], BF16), t([128, 4, 128], BF16)
            vb = t([128, 4, 128], BF16)
            BC = t([128, 1024], BF16)
            Vt = t([128, 512], BF16)
            MAT = t([128, 8, 512], BF16)
            X1a = t([128, 8, 256], BF16)
            NNa = t([128, 8, 128], BF16)
            BTz, CTz = t([128, 8, 128], BF16), t([128, 8, 128], BF16)
            self.memset(G, BTz[:, :, :], 0.0, [BTz])
            self.memset(G, CTz[:, :, :], 0.0, [CTz])
            Wt = [t([128, 8, 128], BF16) for _ in range(2)]
            At = [t([128, 8, 128], BF16) for _ in range(2)]
            Bt = [t([128, 8, 128], BF16) for _ in range(2)]
            ps = self.ps
            for s in range(S):
                for j in range(NJ):
                    n0 = s * T + j * 128
                    self.ck(1000 + s * NJ + j)
                    self.load_halo(pa, 0, 15, s, j, 1)
                    self.ck(1)
                    self.tt(V, sh[:, :, :], pa[:, :, 1:129], bc(c0t[:, :], 15), ALU.mult, [pa, c0t], [sh])
                    self.tt(G, t1[:, :, :], pa[:, :, 0:128], bc(self.pfv(l, "mu0"), 15), ALU.mult, [pa, PFl], [t1])
                    self.tt(V, sh[:, :, :], sh[:, :, :], t1[:, :, :], ALU.add, [sh, t1], [sh])
                    self.tt(G, t1[:, :, :], pa[:, :, 2:130], bc(self.pfv(l, "mu1"), 15), ALU.mult, [pa, PFl], [t1])
                    self.tt(V, sh[:, :, :], sh[:, :, :], t1[:, :, :], ALU.add, [sh, t1], [sh])
                    self.ck(2)
                    r_, k_, v_ = sh[:, 0:4, :], sh[:, 4:8, :], sh[:, 8:12, :]
                    self.actf(twb[:, :], sh[:, 12, :], AF.Tanh, [sh], [twb])
                    self.cp(self.act, xab[:, :], sh[:, 13, :], [sh], [xab])
                    self.actf(sgb[:, :], sh[:, 14, :], AF.Sigmoid, [sh], [sgb])
                    self.ck(3)
                    for d in range(2):
                        for (wb_, xin, dst, bname, pb) in ((w2b, twb, SW[d], f"w0_{d}", ps[0]), (a2b, xab, AA[d], f"a0_{d}", ps[1])):
                            for m in range(4):
                                self.mm(pb[:, m * 128:(m + 1) * 128], wb_[d * 64:(d + 1) * 64, m * 128:(m + 1) * 128],
                                        xin[d * 64:(d + 1) * 64, :], True, True, [wb_, xin], [pb])
                            for m in range(4):
                                self.actf(dst[:, m, :], pb[:, m * 128:(m + 1) * 128], AF.Sigmoid, [pb, PFl], [dst],
                                          bias=self.pfv(l, bname, m), scale=1.0)
                    for m in range(4):
                        self.mm(ps[2][:, m * 128:(m + 1) * 128], g2b[:, m * 128:(m + 1) * 128], sgb[:, :], True, True, [g2b, sgb], [ps[2]])
                    self.cp(self.act, ga[:, :, :], ps[2][:, :].rearrange("p (m t) -> p m t", m=4), [ps[2]], [ga])
                    self.dma(self.GAs.rearrange("(m p) n -> p m n", p=128)[:, :, n0:n0 + 128], ga[:, :, :], [ga], [self.dd("GAs", n0 // 128)])
                    self.ck(4)
                    self.tt(V, kx[:, :, :], k_, bc(self.pfv(l, "kk"), 4), ALU.mult, [sh, PFl], [kx])
                    self.tt(G, tq[:, :, :], kx[:, :, :], kx[:, :, :], ALU.mult, [kx], [tq])
                    for m in range(4):
                        self.mm(ps[3][:, m * 128:(m + 1) * 128], self.bones[:, :], tq[:, m, :], True, True, [self.bones, tq], [ps[3]])
                    self.ts(V, tq[:, :, :], ps[3][:, :].rearrange("p (m t) -> p m t", m=4), EPS, None, ALU.add, None, [ps[3]], [tq])
                    self.actf(tq[:, :, :], tq[:, :, :], AF.Sqrt, [tq], [tq])
                    self.recip(tq[:, :, :], tq[:, :, :], [tq], [tq])
                    self.tt(V, kk[:, :, :], kx[:, :, :], tq[:, :, :], ALU.mult, [kx, tq], [kk])
                    self.ck(5)
                    self.tt(G, bon[:, :, :], r_, k_, ALU.mult, [sh], [bon])
                    self.tt(G, bon[:, :, :], bon[:, :, :], bc(self.pfv(l, "rk"), 4), ALU.mult, [bon, PFl], [bon])
                    for m in range(4):
                        self.mm(ps[4][:, m * 128:(m + 1) * 128], self.bones[:, :], bon[:, m, :], True, True, [self.bones, bon], [ps[4]])
                    self.tt(V, bon[:, :, :], ps[4][:, :].rearrange("p (m t) -> p m t", m=4), v_, ALU.mult, [ps[4], sh], [bon])
                    self.dma(self.BON.rearrange("(m p) n -> p m n", p=128)[:, :, n0:n0 + 128], bon[:, :, :], [bon], [self.dd("BON", n0 // 128)])
                    self.ck(6)
                    self.cp(G, vb[:, :, :], v_, [sh], [vb])
                    pbv = ps[5][:, :].bitcast(BF16)
                    for m in range(4):
                        self.tr(pbv[:, m * 128:(m + 1) * 128], vb[:, m, :], self.identb[:, :], [vb, self.identb], [ps[5]])
                    self.cp(self.act, Vt[:, :], pbv[:, 0:512], [ps[5]], [Vt])
                    self.dma(self.RV[s, j], Vt[:, :], [Vt], [self.dd("RV", s, j)])
                    for d in range(2):
                        self.ck(7)
                        self.tt(V, tmpa[:, :, :], AA[d][:, :, :], bc(self.pfv(l, "ka"), 4), ALU.mult, [AA[d], PFl], [tmpa])
                        self.tt(V, tmpa[:, :, :], tmpa[:, :, :], bc(omka[:, :], 4), ALU.add, [tmpa, omka], [tmpa])
                        self.tt(V, tmpa[:, :, :], tmpa[:, :, :], k_, ALU.mult, [tmpa, sh], [tmpa])
                        self.tt(G, tmpb[:, :, :], kk[:, :, :], AA[d][:, :, :], ALU.mult, [kk, AA[d]], [tmpb])
                        self.ck(8)
                        for m in range(4):
                            self.op(V, lambda m=m: nc.vector.tensor_tensor_scan(out=CS[:, m, :], data0=self.RM[:, :], data1=SW[d][:, m, :],
                                                                               initial=0.0, op0=ALU.mult, op1=ALU.add),
                                    [self.RM, SW[d]], [CS])
                        CSv = CS[:, :, :].rearrange("p m (c t) -> p m c t", t=64)
                        if d == 0:
                            self.tt(V, EX[:, :, :], CS[:, :, :], SW[d][:, :, :], ALU.subtract, [CS, SW[d]], [EX])
                            incl = CS
                        else:
                            tot = CSv[:, :, :, 63:64].to_broadcast([128, 4, 2, 64])
                            self.tt(V, EX[:, :, :].rearrange("p m (c t) -> p m c t", t=64), tot, CSv, ALU.subtract, [CS], [EX])
                            self.tt(V, IN_[:, :, :], EX[:, :, :], SW[d][:, :, :], ALU.add, [EX, SW[d]], [IN_])
                            incl = IN_
                        self.ck(9)
                        self.actf(e1[:, :, :], EX[:, :, :], AF.Exp, [EX], [e1], scale=-CDEC)
                        self.actf(e2[:, :, :], incl[:, :, :], AF.Exp, [incl], [e2], scale=CDEC)
                        self.actf(e3[:, :, :], incl[:, :, :], AF.Exp, [incl], [e3], scale=-CDEC)
                        self.actf(pc[:, :].rearrange("p (m c) -> p m c", c=2), CSv[:, :, :, 63], AF.Exp, [CS], [pc], scale=-CDEC)
                        self.dma(self.RPC[s, d, j], pc[:, :], [pc], [self.dd("RPC", s, d, j)])
                        self.tt(V, KQ[:, :, 0:128], kk[:, :, :], e1[:, :, :], ALU.mult, [kk, e1], [KQ])
                        self.tt(G, KQ[:, :, 128:256], r_, e3[:, :, :], ALU.mult, [sh, e3], [KQ])
                        self.tt(V, BTb[:, :, :], tmpb[:, :, :], e2[:, :, :], ALU.mult, [tmpb, e2], [BTb])
                        self.tt(G, CTb[:, :, :], tmpa[:, :, :], e2[:, :, :], ALU.mult, [tmpa, e2], [CTb])
                        self.dma(self.RKQ[s, d, j], KQ[:, :, :].rearrange("p m t -> p (m t)"), [KQ], [self.dd("RKQ", s, d, j)])
                        self.ck(10)
                        pbb = ps[6][:, :].bitcast(BF16)
                        for m in range(4):
                            self.tr(pbb[:, m * 128:(m + 1) * 128], BTb[:, m, :], self.identb[:, :], [BTb, self.identb], [ps[6]])
                            self.tr(pbb[:, 512 + m * 128:512 + (m + 1) * 128], CTb[:, m, :], self.identb[:, :], [CTb, self.identb], [ps[6]])
                        self.cp(self.act, BC[:, :], pbb[:, :], [ps[6]], [BC])
                        self.dma(self.RBC[s, d, j], BC[:, :], [BC], [self.dd("RBC", s, d, j)])
                        self.ck(11)
                        Ms, Mn = (self.MU, self.ML) if d == 0 else (self.ML, self.MU)
                        for e_ in range(2):
                            rows = slice(e_ * 64, e_ * 64 + 64)
                            self.cp(G, BTz[:, :, :].rearrange("p (m e) t -> p m e t", e=2)[rows, :, e_, :], BTb[rows, :, :], [BTb], [BTz])
                            self.cp(self.act, CTz[:, :, :].rearrange("p (m e) t -> p m e t", e=2)[rows, :, e_, :], CTb[rows, :, :], [CTb], [CTz])
                        for h in range(8):
                            m = h // 2
                            self.mm(ps[h // 2][:, (h % 2) * 256:(h % 2 + 1) * 256], BTz[:, h, :], KQ[:, m, :], True, True, [BTz, KQ], [ps[h // 2]])
                        for h in range(8):
                            m = h // 2
                            self.mm(ps[4 + h // 2][:, (h % 2) * 256:(h % 2 + 1) * 256], CTz[:, h, :], KQ[:, m, :], True, True, [CTz, KQ], [ps[4 + h // 2]])
                        Msb = Ms[:, :].unsqueeze(1).to_broadcast([128, 2, 256])
                        for b in range(4):
                            self.tt(V, X1a[:, 2 * b:2 * b + 2, :], ps[b][:, :].rearrange("p (h t) -> p h t", h=2), Msb, ALU.mult, [ps[b], Ms], [X1a])
                            self.tt(V, MAT[:, 2 * b:2 * b + 2, 256:512], ps[4 + b][:, :].rearrange("p (h t) -> p h t", h=2), Msb, ALU.mult, [ps[4 + b], Ms], [MAT])
                        for h in range(8):
                            m = h // 2
                            self.mm(ps[h // 4][:, (h % 4) * 128:(h % 4 + 1) * 128], KQ[:, m, 0:128], BTz[:, h, :], True, True, [BTz, KQ], [ps[h // 4]])
                        Mnb = Mn[:, 0:128].unsqueeze(1).to_broadcast([128, 4, 128])
                        for b in range(2):
                            self.tt(V, NNa[:, 4 * b:4 * b + 4, :], ps[b][:, :].rearrange("p (h t) -> p h t", h=4), Mnb, ALU.mult, [ps[b], Mn], [NNa])
                        self.cp(G, MAT[:, :, 128:256], X1a[:, :, 128:256], [X1a], [MAT])
                        self.inverse_batch(X1a, NNa, MAT, 8, Wt, At, Bt)
                        self.ck(12)
                        self.dma(self.RMAT[s, d, j], MAT[:, :, :].rearrange("p h t -> p (h t)"), [MAT], [self.dd("RMAT", s, d, j)])
            self.barrier()

    def gdn_setup(self):
        self.GKQ = self.dram("GKQ", [S, 2, NJ, 128, 4 * 256], BF16)
        self.GMAT = self.dram("GMAT", [S, 2, NJ, 128, 4 * 512], BF16)
        self.GBC = self.dram("GBC", [S, 2, NJ, 128, 1024], BF16)
        self.GV = self.dram("GV", [S, NJ, 128, 512], BF16)
        self.GPC = self.dram("GPC", [S, 2, NJ, 128, 8], F32)
        self.YB = self.dram("YB", [2, NT, 512])
        self.SEL = self.sb([16, 16, 128], F32)
        self.selc = self.sb([128, 1], F32)
        self.onec = self.sb([128, 1], F32)
        nc = self.nc
        P = self.pool
        for i in range(16):
            self.cp(P, self.SEL[0:16, i, :], self.ident[0:16, i:i + 1].to_broadcast([16, 128]), [self.ident], [self.SEL])
        self.memset(P, self.onec[:, :], 1.0, [self.onec])
        self.memset(P, self.selc[:, :], 1.0, [self.selc])
        self.op(P, lambda: nc.gpsimd.affine_select(out=self.selc[:, :], in_=self.selc[:, :], pattern=[[0, 1]], compare_op=ALU.is_ge, fill=0.0,
                                                   base=-4, channel_multiplier=1), [self.selc], [self.selc])

    def gdn_prep(self, l):
        nc = self.nc
        V, G = self.dve, self.pool
        ps = self.ps
        with ExitStack() as es:
            def t(shape, dt=F32):
                return self.sb(shape, dt, es)
            PFl = self.PFt[l]

            def bc(ap, n):
                return ap.unsqueeze(2).to_broadcast([128, n, 128])
            negA = t([128, 1])
            self.actf(negA[:, :], self.pfv(l, "alog"), AF.Exp, [PFl], [negA])
            self.ts(V, negA[:, :], negA[:, :], -1.0, None, ALU.mult, None, [negA], [negA])
            qkv = t([128, 12, 132])
            cv, t2 = t([128, 12, 128]), t([128, 12, 128])
            sq = t([128, 8, 128])
            kq = t([128, 4, 256])
            kqb = t([128, 4, 256], BF16)
            kb = t([128, 4, 128], BF16)
            vb = t([128, 4, 128], BF16)
            ab, x1, Xg, SIG, gcf, gcr = (t([16, 128]) for _ in range(6))
            X2, E2 = t([16, 256]), t([16, 256])
            TOTb, Etot = t([16, 128]), t([16, 2])
            TS = t([128, 80])
            negg, sB, sC, dd_ = t([128, 8]), t([128, 8]), t([128, 8]), t([128, 8])
            gm4 = t([128, 4, 256])
            KQd = t([128, 4, 256], BF16)
            BC = t([128, 1024], BF16)
            Vt = t([128, 512], BF16)
            MAT = t([128, 4, 512], BF16)
            pc = t([128, 8])
            X1a = t([128, 4, 256], BF16)
            NNa = t([128, 4, 128], BF16)
            Wt = [t([128, 4, 128], BF16) for _ in range(2)]
            At = [t([128, 4, 128], BF16) for _ in range(2)]
            Bt = [t([128, 4, 128], BF16) for _ in range(2)]
            abv = self.PT[3968:3984, :]
            for s in range(S):
                for j in range(NJ):
                    n0 = s * T + j * 128
                    self.load_halo(qkv, 15, 12, s, j, 2)
                    gw = self.pfv(l, "gconv")
                    self.tt(V, cv[:, :, :], qkv[:, :, 0:128], bc(gw[:, 0:12], 12), ALU.mult, [qkv, PFl], [cv])
                    for k in range(1, 5):
                        self.tt(G, t2[:, :, :], qkv[:, :, k:k + 128], bc(gw[:, k * 12:(k + 1) * 12], 12), ALU.mult, [qkv, PFl], [t2])
                        self.tt(V, cv[:, :, :], cv[:, :, :], t2[:, :, :], ALU.add, [cv, t2], [cv])
                    self.actf(cv[:, :, :], cv[:, :, :], AF.Silu, [cv], [cv])
                    self.tt(G, sq[:, :, :], cv[:, 0:8, :], cv[:, 0:8, :], ALU.mult, [cv], [sq])
                    for c in range(8):
                        pb = ps[c // 4]
                        self.mm(pb[:, (c % 4) * 128:(c % 4 + 1) * 128], self.ones[:, :], sq[:, c, :], True, True, [self.ones, sq], [pb])
                    for hf in range(2):
                        self.ts(V, sq[:, hf * 4:(hf + 1) * 4, :], ps[hf][:, :].rearrange("p (c t) -> p c t", c=4), EPS, None, ALU.add, None, [ps[hf]], [sq])
                    self.actf(sq[:, :, :], sq[:, :, :], AF.Sqrt, [sq], [sq])
                    self.recip(sq[:, :, :], sq[:, :, :], [sq], [sq])
                    self.tt(V, kq[:, :, 0:128], cv[:, 4:8, :], sq[:, 4:8, :], ALU.mult, [cv, sq], [kq])
                    self.stt(V, kq[:, :, 128:256], cv[:, 0:4, :], 128.0 ** -0.5, sq[:, 0:4, :], ALU.mult, ALU.mult, [cv, sq], [kq])
                    self.cp(G, kqb[:, :, :], kq[:, :, :], [kq], [kqb])
                    self.cp(G, kb[:, :, :], kq[:, :, 0:128], [kq], [kb])
                    self.cp(G, vb[:, :, :], cv[:, 8:12, :], [cv], [vb])
                    pbv = ps[5][:, :].bitcast(BF16)
                    for m in range(4):
                        self.tr(pbv[:, m * 128:(m + 1) * 128], vb[:, m, :], self.identb[:, :], [vb, self.identb], [ps[5]])
                    self.cp(self.act, Vt[:, :], pbv[:, 0:512], [ps[5]], [Vt])
                    self.dma(self.GV[s, j], Vt[:, :], [Vt], [self.dd("GV", s, j)])
                    pbk = ps[6][:, :].bitcast(BF16)
                    for m in range(4):
                        self.tr(pbk[:, m * 128:(m + 1) * 128], kb[:, m, :], self.identb[:, :], [kb, self.identb], [ps[6]])
                    self.dma(ab[0:16, :], abv[:, n0:n0 + 128], [], [ab])
                    self.actf(x1[0:16, :], ab[0:16, :], AF.Exp, [ab, PFl], [x1], bias=self.pfv(l, "dtb")[0:16, :], scale=1.0)
                    self.actf(x1[0:16, :], x1[0:16, :], AF.Ln, [x1, self.onec], [x1], bias=self.onec[0:16, :], scale=1.0)
                    self.ts(V, Xg[0:16, :], x1[0:16, :], negA[0:16, :], None, ALU.mult, None, [x1, negA], [Xg])
                    self.actf(SIG[0:16, :], ab[0:16, :], AF.Sigmoid, [ab], [SIG])
                    self.op(V, lambda: nc.vector.tensor_tensor_scan(out=gcf[0:16, :], data0=self.RM[0:16, :], data1=Xg[0:16, :], initial=0.0,
                                                                    op0=ALU.mult, op1=ALU.add), [self.RM, Xg], [gcf])
                    gcfv = gcf[0:16, :].rearrange("p (c t) -> p c t", t=64)
                    totb = gcfv[:, :, 63:64].to_broadcast([16, 2, 64])
                    self.cp(V, TOTb[0:16, :].rearrange("p (c t) -> p c t", t=64), totb, [gcf], [TOTb])
                    self.tt(V, gcr[0:16, :], TOTb[0:16, :], gcf[0:16, :], ALU.subtract, [TOTb, gcf], [gcr])
                    self.tt(V, X2[0:16, 128:256], gcr[0:16, :], Xg[0:16, :], ALU.add, [gcr, Xg], [X2])
                    self.tt(V, X2[0:16, 128:256], X2[0:16, 128:256], gcf[0:16, :], ALU.subtract, [X2, gcf], [X2])
                    self.stt(V, X2[0:16, 128:256], X2[0:16, 128:256], self.selc[0:16, :], gcf[0:16, :], ALU.mult, ALU.add, [X2, self.selc, gcf], [X2])
                    self.tt(V, X2[0:16, 0:128], X2[0:16, 128:256], Xg[0:16, :], ALU.subtract, [X2, Xg], [X2])
                    self.actf(E2[0:16, :], X2[0:16, :], AF.Exp, [X2], [E2])
                    self.actf(Etot[0:16, :], gcfv[:, :, 63], AF.Exp, [gcf], [Etot])
                    pT = ps[7]
                    for q_, src in enumerate((Xg[0:16, :], SIG[0:16, :], X2[0:16, 0:128], X2[0:16, 128:256], TOTb[0:16, :])):
                        self.tr(pT[:, q_ * 16:(q_ + 1) * 16], src, self.ident[0:16, 0:16], [Xg, SIG, X2, TOTb, self.ident], [pT])
                    self.cp(V, TS[:, :], pT[:, 0:80], [pT], [TS])
                    self.actf(negg[:, :], TS[:, 0:8], AF.Exp, [TS], [negg], scale=-1.0)
                    self.tt(V, dd_[:, :], TS[:, 64:72], TS[:, 32:40], ALU.subtract, [TS], [dd_])
                    self.actf(sB[:, :], dd_[:, :], AF.Exp, [dd_], [sB])
                    self.tt(V, sB[:, :], sB[:, :], TS[:, 24:32], ALU.mult, [sB, TS], [sB])
                    self.tt(V, dd_[:, :], TS[:, 64:72], TS[:, 48:56], ALU.subtract, [TS], [dd_])
                    self.actf(sC[:, :], dd_[:, :], AF.Exp, [dd_], [sC])
                    self.tt(V, sC[:, :], sC[:, :], TS[:, 24:32], ALU.mult, [sC, TS], [sC])
                    for d in range(2):
                        Ms = self.MU if d == 0 else self.ML
                        for h in range(4):
                            i = d * 4 + h
                            self.mm(ps[h // 2][:, (h % 2) * 256:(h % 2 + 1) * 256], self.SEL[0:16, i, :], X2[0:16, :], True, True, [self.SEL, X2], [ps[h // 2]])
                            self.mm(ps[2 + h // 2][:, (h % 2) * 256:(h % 2 + 1) * 256], self.SEL[0:16, i, :], E2[0:16, :], True, True, [self.SEL, E2], [ps[2 + h // 2]])
                            self.mm(ps[7][:, 128 + h * 2:128 + (h + 1) * 2], self.SEL[0:16, i, :], Etot[0:16, :], True, True, [self.SEL, Etot], [ps[7]])
                        self.cp(self.act, pc[:, :], ps[7][:, 128:136], [ps[7]], [pc])
                        v2 = lambda p: p[:, :].rearrange("p (h t) -> p h t", h=2)
                        for b in range(2):
                            self.tt(V, gm4[:, 2 * b:2 * b + 2, :], v2(ps[b]), TS[:, 32 + d * 4 + 2 * b:32 + d * 4 + 2 * b + 2].unsqueeze(2).to_broadcast([128, 2, 256]),
                                    ALU.subtract, [ps[b], TS], [gm4])
                            self.tt(V, KQd[:, 2 * b:2 * b + 2, :], kq[:, 2 * b:2 * b + 2, :], v2(ps[2 + b]), ALU.mult, [kq, ps[2 + b]], [KQd])
                        self.ts(G, gm4[:, :, :], gm4[:, :, :], 0.0, None, ALU.min, None, [gm4], [gm4])
                        self.actf(gm4[:, :, :], gm4[:, :, :], AF.Exp, [gm4], [gm4])
                        self.tt(G, gm4[:, :, :], gm4[:, :, :], Ms[:, :].unsqueeze(1).to_broadcast([128, 4, 256]), ALU.mult, [gm4, Ms], [gm4])
                        for h in range(4):
                            self.mm(ps[4 + h // 2][:, (h % 2) * 256:(h % 2 + 1) * 256], kb[:, h, :], kqb[:, h, :], True, True, [kb, kqb], [ps[4 + h // 2]])
                        for b in range(2):
                            self.tt(V, gm4[:, 2 * b:2 * b + 2, :], gm4[:, 2 * b:2 * b + 2, :], v2(ps[4 + b]), ALU.mult, [gm4, ps[4 + b]], [gm4])
                        self.tt(V, X1a[:, :, :], gm4[:, :, :], TS[:, 24 + d * 4:28 + d * 4].unsqueeze(2).to_broadcast([128, 4, 256]), ALU.mult, [gm4, TS], [X1a])
                        self.tt(V, MAT[:, :, 256:512], X1a[:, :, :], negg[:, d * 4:d * 4 + 4].unsqueeze(2).to_broadcast([128, 4, 256]), ALU.mult, [X1a, negg], [MAT])
                        self.cp(G, MAT[:, :, 128:256], X1a[:, :, 128:256], [X1a], [MAT])
                        pN = ps[5][:, :].bitcast(BF16)
                        for h in range(4):
                            self.tr(pN[:, h * 128:(h + 1) * 128], X1a[:, h, 0:128], self.identb[:, :], [X1a, self.identb], [ps[5]])
                        self.cp(self.act, NNa[:, :, :], pN[:, 0:512].rearrange("p (h t) -> p h t", h=4), [ps[5]], [NNa])
                        self.inverse_batch(X1a, NNa, MAT, 4, Wt, At, Bt)
                        pbk4 = pbk[:, 0:512].rearrange("p (h t) -> p h t", h=4)
                        self.tt(V, BC[:, 0:512].rearrange("p (h t) -> p h t", h=4), pbk4, sB[:, d * 4:d * 4 + 4].unsqueeze(2).to_broadcast([128, 4, 128]), ALU.mult, [ps[6], sB], [BC])
                        self.tt(V, BC[:, 512:1024].rearrange("p (h t) -> p h t", h=4), pbk4, sC[:, d * 4:d * 4 + 4].unsqueeze(2).to_broadcast([128, 4, 128]), ALU.mult, [ps[6], sC], [BC])
                        self.dma(self.GKQ[s, d, j], KQd[:, :, :].rearrange("p m t -> p (m t)"), [KQd], [self.dd("GKQ", s, d, j)])
                        self.dma(self.GMAT[s, d, j], MAT[:, :, :].rearrange("p h t -> p (h t)"), [MAT], [self.dd("GMAT", s, d, j)])
                        self.dma(self.GBC[s, d, j], BC[:, :], [BC], [self.dd("GBC", s, d, j)])
                        self.dma(self.GPC[s, d, j], pc[:, :], [pc], [self.dd("GPC", s, d, j)])
            self.barrier()

    def conf_setup(self):
        self.RCs = self.dram("RCs", [512, NT], BF16)
        self.RAs = self.dram("RAs", [512, NT], BF16)
        self.RBs = self.dram("RBs", [512, NT], BF16)

    def conformer(self, l):
        V, G = self.dve, self.pool
        ps = self.ps
        PFl = self.PFt[l]
        valv = self.PT[3984:3984 + 512, :].rearrange("(c p) n -> p c n", p=128)
        gatv = self.PT[4496:4496 + 512, :].rearrange("(c p) n -> p c n", p=128)
        RCv = self.RCs.rearrange("(c p) n -> p c n", p=128)
        with ExitStack() as es:
            def t(shape, dt=F32):
                return self.sb(shape, dt, es)
            u = t([128, 4, TL])
            gt = t([128, 4, TL])
            o = t([128, 4, TL])
            sq = t([128, 4, 512])
            mu, rs, var = t([128, 512]), t([128, 512]), t([128, 512])
            ob = t([128, 4, 512], BF16)
            cw = self.pfv(l, "cdw")
            for s in range(S):
                for (seg0, W_) in ((0, TC), (TC, TL)):
                    n0 = s * T + seg0
                    for c in range(4):
                        self.dma(u[:, c, :W_], valv[:, c, n0:n0 + W_], [], [u])
                        self.dma(gt[:, c, :W_], gatv[:, c, n0:n0 + W_], [], [gt])
                    self.actf(gt[:, :, :W_], gt[:, :, :W_], AF.Sigmoid, [gt], [gt])
                    self.tt(V, u[:, :, :W_], u[:, :, :W_], gt[:, :, :W_], ALU.mult, [u, gt], [u])
                    for c in range(4):
                        def wk(k):
                            return cw[:, k * 4 + c:k * 4 + c + 1]
                        self.ts(V, o[:, c, :W_], u[:, c, :W_], wk(15), None, ALU.mult, None, [u, PFl], [o])
                        for k in range(31):
                            dlt = k - 15
                            if dlt == 0:
                                continue
                            if seg0 == 0:
                                lo, hi = max(0, -dlt), min(W_, W_ - dlt)
                                self.stt(V, o[:, c, lo:hi], u[:, c, lo + dlt:hi + dlt], wk(k), o[:, c, lo:hi], ALU.mult, ALU.add, [u, PFl, o], [o])
                            elif c < 2:
                                uv = u[:, c, :].rearrange("p (r w) -> p r w", w=64)
                                ov = o[:, c, :].rearrange("p (r w) -> p r w", w=64)
                                lo, hi = max(0, -dlt), min(64, 64 - dlt)
                                self.stt(V, ov[:, :, lo:hi], uv[:, :, lo + dlt:hi + dlt], wk(k), ov[:, :, lo:hi], ALU.mult, ALU.add, [u, PFl, o], [o])
                            else:
                                uv = u[:, c, :].rearrange("p (r w) -> p r w", w=64)
                                ov = o[:, c, :].rearrange("p (r w) -> p r w", w=64)
                                lo, hi = max(0, -dlt), min(32, 32 - dlt)
                                self.stt(V, ov[:, lo:hi, :], uv[:, lo + dlt:hi + dlt, :], wk(k), ov[:, lo:hi, :], ALU.mult, ALU.add, [u, PFl, o], [o])
                        self.ts(V, o[:, c, :W_], o[:, c, :W_], self.pfv(l, "cdwb", c), None, ALU.add, None, [o, PFl], [o])
                    for t0 in range(0, W_, 512):
                        w = min(512, W_ - t0)
                        self.tt(G, sq[:, :, :w], o[:, :, t0:t0 + w], o[:, :, t0:t0 + w], ALU.mult, [o], [sq])
                        for c in range(4):
                            self.mm(ps[0][:, :w], self.ones[:, :], o[:, c, t0:t0 + w], c == 0, c == 3, [self.ones, o], [ps[0]])
                        for c in range(4):
                            self.mm(ps[1][:, :w], self.ones[:, :], sq[:, c, :w], c == 0, c == 3, [self.ones, sq], [ps[1]])
                        self.ts(V, mu[:, :w], ps[0][:, :w], 1.0 / 512, None, ALU.mult, None, [ps[0]], [mu])
                        self.tt(V, var[:, :w], mu[:, :w], mu[:, :w], ALU.mult, [mu], [var])
                        self.stt(V, var[:, :w], ps[1][:, :w], 1.0 / 512, var[:, :w], ALU.mult, ALU.subtract, [ps[1], var], [var])
                        self.ts(V, var[:, :w], var[:, :w], EPS, None, ALU.add, None, [var], [var])
                        self.actf(var[:, :w], var[:, :w], AF.Sqrt, [var], [var])
                        self.recip(rs[:, :w], var[:, :w], [var], [rs])
                        for c in range(4):
                            self.tt(V, sq[:, c, :w], o[:, c, t0:t0 + w], mu[:, :w], ALU.subtract, [o, mu], [sq])
                            self.tt(V, sq[:, c, :w], sq[:, c, :w], rs[:, :w], ALU.mult, [sq, rs], [sq])
                            self.ts(V, sq[:, c, :w], sq[:, c, :w], self.pfv(l, "clng", c), self.pfv(l, "clnb", c), ALU.mult, ALU.add, [sq, PFl], [sq])
                        self.actf(ob[:, :, :w], sq[:, :, :w], AF.Silu, [sq], [ob])
                        self.dma(RCv[:, :, n0 + t0:n0 + t0 + w], ob[:, :, :w], [ob], [self.dd("RCs", (n0 + t0) // 128)])
            self.barrier()

    def red(self, E, out, in_, op, R, W):
        self.op(E, lambda: E.be.tensor_reduce(out=out, in_=in_, axis=AX.X, op=op), R, W)

    def merge(self, l):
        V, G = self.dve, self.pool
        ps = self.ps
        PFl = self.PFt[l]
        with ExitStack() as es:
            def t(shape, dt=F32):
                return self.sb(shape, dt, es)
            wbr = t([128, 12, 1024], BF16)
            wo = t([128, 8, 1024], BF16)
            for n in range(3):
                self.dma(wbr[:, n * 4:(n + 1) * 4, :], self.w_branch[l, n].rearrange("(k p) n -> p k n", p=128), [], [wbr], Q=self.pool)
            self.dma(wo[:, :, :], self.w_out[l].rearrange("(k p) n -> p k n", p=128), [], [wo], Q=self.pool)
            pgv = [self.PT[5008 + n * 1024:5008 + (n + 1) * 1024, :].rearrange("(c p) n -> p c n", p=128) for n in range(3)]
            fm4 = lambda A: A.rearrange("(c p) n -> p c n", p=128)
            y0, y1, ysq = t([128, 512]), t([128, 512]), t([128, 512])
            m1, m2, m3 = t([128, 8]), t([128, 8]), t([128, 8])
            raf, bon, ga, zt = (t([128, 4, 128]) for _ in range(4))
            Rb = [t([128, 4, 128], BF16) for _ in range(3)]
            pg = t([128, 8, 128])
            macc, tmpm = t([128, 8, 128]), t([128, 8, 128])
            mb = t([128, 8, 128], BF16)
            xt = t([128, 8, 128])
            for s in range(S):
                for j in range(NJ):
                    n0 = s * T + j * 128
                    r = 2 if j < 2 else s
                    for br in range(2):
                        DY, nm, nh, dvv, eps_ = ((self.YA, "R", 8, 64, 64e-5), (self.YB, "G", 4, 128, EPS))[br]
                        self.dma(y0[:, :], DY[0, n0:n0 + 128, :], [self.dd(nm + "Y", 0, n0 // 128)], [y0])
                        self.dma(y1[:, :], DY[1, n0:n0 + 128, :], [self.dd(nm + "Y", 1, n0 // 128)], [y1])
                        self.tt(V, y0[:, :], y0[:, :], y1[:, :], ALU.add, [y0, y1], [y0])
                        yv = y0[:, :].rearrange("p (h d) -> p h d", d=dvv)
                        self.tt(G, ysq[:, :], y0[:, :], y0[:, :], ALU.mult, [y0], [ysq])
                        self.red(V, m2[:, 0:nh], ysq[:, :].rearrange("p (h d) -> p h d", d=dvv), ALU.add, [ysq], [m2])
                        if br == 0:
                            self.red(V, m1[:, 0:nh], yv, ALU.add, [y0], [m1])
                            self.ts(V, m1[:, 0:nh], m1[:, 0:nh], 1.0 / dvv, None, ALU.mult, None, [m1], [m1])
                            self.tt(V, m3[:, 0:nh], m1[:, 0:nh], m1[:, 0:nh], ALU.mult, [m1], [m3])
                            self.stt(V, m2[:, 0:nh], m2[:, 0:nh], 1.0 / dvv, m3[:, 0:nh], ALU.mult, ALU.subtract, [m2, m3], [m2])
                            self.ts(V, m2[:, 0:nh], m2[:, 0:nh], eps_, None, ALU.add, None, [m2], [m2])
                            self.tt(V, yv, yv, m1[:, 0:nh].unsqueeze(2).to_broadcast([128, nh, dvv]), ALU.subtract, [y0, m1], [y0])
                        else:
                            self.ts(V, m2[:, 0:nh], m2[:, 0:nh], 1.0 / dvv, eps_, ALU.mult, ALU.add, [m2], [m2])
                        self.actf(m2[:, 0:nh], m2[:, 0:nh], AF.Sqrt, [m2], [m2])
                        self.recip(m2[:, 0:nh], m2[:, 0:nh], [m2], [m2])
                        self.tt(V, yv, yv, m2[:, 0:nh].unsqueeze(2).to_broadcast([128, nh, dvv]), ALU.mult, [y0, m2], [y0])
                        pb = ps[br]
                        for c in range(4):
                            self.tr(pb[:, c * 128:(c + 1) * 128], y0[:, c * 128:(c + 1) * 128], self.ident[:, :], [y0, self.ident], [pb])
                        pbv = pb[:, :].rearrange("p (c t) -> p c t", c=4)
                        if br == 0:
                            for c in range(4):
                                self.ts(V, raf[:, c, :], pbv[:, c, :], self.pfv(l, "ln_g", c), self.pfv(l, "ln_b", c), ALU.mult, ALU.add, [pb, PFl], [raf])
                            self.dma(bon[:, :, :], fm4(self.BON)[:, :, n0:n0 + 128], [self.dd("BON", n0 // 128)], [bon])
                            self.dma(ga[:, :, :], fm4(self.GAs)[:, :, n0:n0 + 128], [self.dd("GAs", n0 // 128)], [ga])
                            self.tt(V, raf[:, :, :], raf[:, :, :], bon[:, :, :], ALU.add, [raf, bon], [raf])
                            self.tt(V, Rb[0][:, :, :], raf[:, :, :], ga[:, :, :], ALU.mult, [raf, ga], [Rb[0]])
                        else:
                            self.dma(zt[:, :, :], self.PTv[:, 27:31, n0:n0 + 128], [], [zt])
                            self.actf(zt[:, :, :], zt[:, :, :], AF.Silu, [zt], [zt])
                            self.stt(V, Rb[1][:, :, :], pbv, self.pfv(l, "gnorm", 0), zt[:, :, :], ALU.mult, ALU.mult, [pb, PFl, zt], [Rb[1]])
                    self.dma(Rb[2][:, :, :], fm4(self.RCs)[:, :, n0:n0 + 128], [self.dd("RCs", n0 // 128)], [Rb[2]])
                    for n in range(3):
                        self.dma(pg[:, :, :], pgv[n][:, :, n0:n0 + 128], [], [pg])
                        self.actf(pg[:, :, :], pg[:, :, :], AF.Sigmoid, [pg], [pg])
                        for m in range(8):
                            pb = ps[2 + m // 4]
                            for k in range(4):
                                self.mm(pb[:, (m % 4) * 128:(m % 4 + 1) * 128], wbr[:, n * 4 + k, m * 128:(m + 1) * 128], Rb[n][:, k, :], k == 0, k == 3,
                                        [wbr, Rb[n]], [pb])
                        for hf in range(2):
                            pbv = ps[2 + hf][:, :].rearrange("p (c t) -> p c t", c=4)
                            dst = macc if n == 0 else tmpm
                            self.tt(V, dst[:, hf * 4:(hf + 1) * 4, :], pbv, pg[:, hf * 4:(hf + 1) * 4, :], ALU.mult, [ps[2 + hf], pg], [dst])
                        if n > 0:
                            self.tt(G, macc[:, :, :], macc[:, :, :], tmpm[:, :, :], ALU.add, [macc, tmpm], [macc])
                    self.cp(G, mb[:, :, :], macc[:, :, :], [macc], [mb])
                    self.dma(xt[:, :, :], self.XTv[:, :, n0:n0 + 128], self.dr("XT", n0, 128), [xt])
                    for m in range(8):
                        pb = ps[4 + m // 4]
                        for k in range(8):
                            self.mm(pb[:, (m % 4) * 128:(m % 4 + 1) * 128], wo[:, k, m * 128:(m + 1) * 128], mb[:, k, :], k == 0, k == 7, [wo, mb], [pb])
                    for m in range(8):
                        pb = ps[4 + m // 4]
                        self.stt(V, xt[:, m, :], pb[:, (m % 4) * 128:(m % 4 + 1) * 128], self.MOD[l][:, 16 + m, r:r + 1], xt[:, m, :], ALU.mult, ALU.add,
                                 [pb, self.MOD[l], xt], [xt])
                    self.dma(self.XTv[:, :, n0:n0 + 128], xt[:, :, :], [xt], self.dr("XT", n0, 128))
                    if f"XM{l}" in self.dbg:
                        pass
            self.barrier()

    def moe(self, l):
        V, G = self.dve, self.pool
        ps = self.ps
        with ExitStack() as es:
            def t(shape, dt=F32, e_=None):
                return self.sb(shape, dt, e_ or es)
            hT = t([128, 8, NT], BF16)
            WTf = t([16, NT])
            wr = t([128, 8, 16])
            rb = t([128, 16])
            self.dma(wr[:, :, :], self.w_router.rearrange("(k p) e -> p k e", p=128), [], [wr])
            self.dma(rb[:, :], self.rbias, [], [rb])
            with ExitStack() as es1:
                xts = [t([128, 8, 512], F32, es1) for _ in range(2)]
                sq = t([128, 8, 512], F32, es1)
                rs = t([128, 512], F32, es1)
                hf = t([128, 8, 512], F32, es1)
                sc, sel, sel2, eq, cm, wts = (t([128, 16], F32, es1) for _ in range(6))
                m1, m2, gs, gsel = (t([128, 4], F32, es1) for _ in range(4))
                gmx, wsum = t([128, 1], F32, es1), t([128, 1], F32, es1)
                v4 = lambda a: a[:, :].rearrange("p (g j) -> p g j", j=4)
                b4 = lambda a: a[:, :].unsqueeze(2).to_broadcast([128, 4, 4])
                for i, (n0, w, r) in enumerate(self.tiles()):
                    xt, _ = self.modulate_tile((xts[i % 2], sq, rs, None), n0, w, r, None, None, None, None)
                    for c in range(8):
                        self.stt(V, hf[:, c, :w], xt[:, c, :w], self.GF[l][:, c, r:r + 1], rs[:, :w], ALU.mult, ALU.mult, [xt, rs, self.GF[l]], [hf])
                        self.actf(hf[:, c, :w], hf[:, c, :w], AF.Identity, [hf, self.MOD[l]], [hf], bias=self.MOD[l][:, 24 + c, r:r + 1], scale=1.0)
                    self.cp(G, hT[:, :, n0:n0 + w], hf[:, :, :w], [hf], [hT])
                    for q in range(w // 128):
                        pR = ps[6]
                        for c in range(8):
                            self.mm(pR[:, 0:16], hf[:, c, q * 128:(q + 1) * 128], wr[:, c, :], c == 0, c == 7, [hf, wr], [pR])
                        self.actf(sc[:, :], pR[:, 0:16], AF.Sigmoid, [pR], [sc])
                        self.tt(V, sel[:, :], sc[:, :], rb[:, :], ALU.add, [sc, rb], [sel])
                        self.red(V, m1[:, :], v4(sel), ALU.max, [sel], [m1])
                        self.tt(V, v4(eq), v4(sel), b4(m1), ALU.is_equal, [sel, m1], [eq])
                        self.stt(V, sel2[:, :], eq[:, :], -1e9, sel[:, :], ALU.mult, ALU.add, [eq, sel], [sel2])
                        self.red(V, m2[:, :], v4(sel2), ALU.max, [sel2], [m2])
                        self.tt(V, gs[:, :], m1[:, :], m2[:, :], ALU.add, [m1, m2], [gs])
                        self.red(V, gmx[:, :], gs[:, :], ALU.max, [gs], [gmx])
                        self.ts(V, gsel[:, :], gs[:, :], gmx[:, 0:1], None, ALU.is_equal, None, [gs, gmx], [gsel])
                        self.tt(V, v4(cm), v4(sel), b4(m2), ALU.is_ge, [sel, m2], [cm])
                        self.tt(V, v4(cm), v4(cm), b4(gsel), ALU.mult, [cm, gsel], [cm])
                        self.tt(V, wts[:, :], sc[:, :], cm[:, :], ALU.mult, [sc, cm], [wts])
                        self.red(V, wsum[:, :], wts[:, :], ALU.add, [wts], [wsum])
                        self.recip(wsum[:, :], wsum[:, :], [wsum], [wsum])
                        self.ts(V, wts[:, :], wts[:, :], wsum[:, 0:1], None, ALU.mult, None, [wts, wsum], [wts])
                        pT = ps[7]
                        self.tr(pT[0:16, 0:128], wts[:, :], self.ident[:, :], [wts, self.ident], [pT])
                        self.cp(self.act, WTf[0:16, n0 + q * 128:n0 + (q + 1) * 128], pT[0:16, 0:128], [pT], [WTf])
                self.barrier()
            TG = 1152
            TW = 384
            yacc = t([128, 8, TG])
            wgb = [t([128, 8, 512], BF16) for _ in range(2)]
            wub = [t([128, 8, 512], BF16) for _ in range(2)]
            wdb = [t([128, 4, 1024], BF16) for _ in range(2)]
            wtb = t([128, TW])
            sg = [t([128, TW]) for _ in range(2)]
            actb = t([128, 4, TW], BF16)
            xt2 = t([128, 8, 128])
            for g in range(NT // TG):
                for e in range(16):
                    gb, ub, db = wgb[e % 2], wub[e % 2], wdb[e % 2]
                    for (src, dstt) in ((self.weg[l, e], gb), (self.weu[l, e], ub), (self.wed[l, e], db)):
                        self.dma(dstt[:, :, :], src.rearrange("(k p) n -> p k n", p=128), [], [dstt], Q=self.pool)
                    for tt_ in range(TG // TW):
                        n0 = g * TG + tt_ * TW
                        self.mm(ps[4][:, :TW], self.SEL[0:16, e, :], WTf[0:16, n0:n0 + TW], True, True, [self.SEL, WTf], [ps[4]])
                        self.cp(self.act, wtb[:, :], ps[4][:, :TW], [ps[4]], [wtb])
                        for hc in range(4):
                            pg_, pu_ = ps[(hc % 2) * 2], ps[(hc % 2) * 2 + 1]
                            for k in range(8):
                                self.mm(pg_[:, :TW], gb[:, k, hc * 128:(hc + 1) * 128], hT[:, k, n0:n0 + TW], k == 0, k == 7, [gb, hT], [pg_])
                            for k in range(8):
                                self.mm(pu_[:, :TW], ub[:, k, hc * 128:(hc + 1) * 128], hT[:, k, n0:n0 + TW], k == 0, k == 7, [ub, hT], [pu_])
                            sg_ = sg[hc % 2]
                            self.actf(sg_[:, :], pg_[:, :TW], AF.Silu, [pg_], [sg_])
                            self.tt(V, sg_[:, :], sg_[:, :], pu_[:, :TW], ALU.mult, [sg_, pu_], [sg_])
                            self.tt(G, actb[:, hc, :], sg_[:, :], wtb[:, :], ALU.mult, [sg_, wtb], [actb])
                        for m in range(8):
                            pd = ps[4 + m % 4]
                            for hc in range(4):
                                self.mm(pd[:, :TW], db[:, hc, m * 128:(m + 1) * 128], actb[:, hc, :], hc == 0, hc == 3, [db, actb], [pd])
                            dst = yacc[:, m, tt_ * TW:(tt_ + 1) * TW]
                            if e == 0:
                                self.cp(self.act, dst, pd[:, :TW], [pd], [yacc])
                            else:
                                self.tt(V, dst, dst, pd[:, :TW], ALU.add, [yacc, pd], [yacc])
                for p_ in range(TG // 128):
                    n0 = g * TG + p_ * 128
                    tq = n0 % T
                    r = 2 if tq < TC else n0 // T
                    self.dma(xt2[:, :, :], self.XTv[:, :, n0:n0 + 128], self.dr("XT", n0, 128), [xt2])
                    for m in range(8):
                        self.stt(V, xt2[:, m, :], yacc[:, m, p_ * 128:(p_ + 1) * 128], self.MOD[l][:, 40 + m, r:r + 1], xt2[:, m, :], ALU.mult, ALU.add,
                                 [yacc, self.MOD[l], xt2], [xt2])
                    self.dma(self.XTv[:, :, n0:n0 + 128], xt2[:, :, :], [xt2], self.dr("XT", n0, 128))
            self.barrier()

    def final(self):
        V, G = self.dve, self.pool
        ps = self.ps
        with ExitStack() as es:
            def t(shape, dt=F32):
                return self.sb(shape, dt, es)
            xts = [t([128, 8, 128]) for _ in range(2)]
            sq = t([128, 8, 128])
            rs = t([128, 128])
            tmp = t([128, 8, 128])
            os_ = [t([128, 1024]) for _ in range(2)]
            i = 0
            for s in range(S):
                for j in range(2, NJ):
                    n0 = s * T + j * 128
                    xt = xts[i % 2]; o = os_[i % 2]; i += 1
                    self.dma(xt[:, :, :], self.XTv[:, :, n0:n0 + 128], self.dr("XT", n0, 128), [xt])
                    self.actf(sq[:, :, :], xt[:, :, :], AF.Square, [xt], [sq])
                    for c in range(8):
                        self.mm(ps[0][:, 0:128], self.ones[:, :], sq[:, c, :], c == 0, c == 7, [self.ones, sq], [ps[0]])
                    self.ts(V, rs[:, :], ps[0][:, 0:128], 1.0 / D, EPS, ALU.mult, ALU.add, [ps[0]], [rs])
                    self.actf(rs[:, :], rs[:, :], AF.Sqrt, [rs], [rs])
                    self.recip(rs[:, :], rs[:, :], [rs], [rs])
                    for c in range(8):
                        self.stt(V, tmp[:, c, :], xt[:, c, :], self.pfv(0, "norm_final", c), rs[:, :], ALU.mult, ALU.mult, [xt, rs, self.PFt[0]], [tmp])
                    for c in range(8):
                        pb = ps[1 + c // 4]
                        self.tr(pb[:, (c % 4) * 128:(c % 4 + 1) * 128], tmp[:, c, :], self.ident[:, :], [tmp, self.ident], [pb])
                    self.cp(self.act, o[:, 0:512], ps[1][:, :], [ps[1]], [o])
                    self.cp(V, o[:, 512:1024], ps[2][:, :], [ps[2]], [o])
                    self.dma(self.y[s, (j - 2) * 128:(j - 1) * 128, :], o[:, :], [o], [self.dd("y", s, j)])
            self.barrier()

    def rwkv_scan(self, l, gdn=False):
        V, G = self.dve, self.pool
        ps = self.ps
        nh = 4 if gdn else 8
        dv = 512 // nh
        DKQ, DMAT, DBC, DV_, DPC, DY, nm = ((self.GKQ, self.GMAT, self.GBC, self.GV, self.GPC, self.YB, 'G') if gdn else (self.RKQ, self.RMAT, self.RBC, self.RV, self.RPC, self.YA, 'R'))
        with ExitStack() as es:
            def t(shape, dt=F32):
                return self.sb(shape, dt, es)
            chains = [(s, d) for s in range(S) for d in range(2)]
            order = {0: list(range(NJ)), 1: [1, 0] + list(range(NJ - 1, 1, -1))}
            bufs = []
            for _ in chains:
                ld = [(t([128, 4, 256], BF16), t([128, nh, 512], BF16), t([128, 1024], BF16), t([128, 512], BF16), t([128, 8])) for _ in range(2)]
                bufs.append(dict(ld=ld, H=t([128, 4, dv]), Hz=t([128, nh, dv], BF16), Rn=t([128, nh, dv], BF16),
                                 Ubz=[t([128, nh, dv], BF16) for _ in range(2)], Vz=[t([128, 512], BF16) for _ in range(2)],
                                 Yt=t([128, 512])))
            for i in range(NJ):
                for ci, (s, d) in enumerate(chains):
                    j = order[d][i]
                    n0 = s * T + j * 128
                    b = bufs[ci]
                    KQ, MAT, BC, Vt, pc = b["ld"][i % 2]
                    H, Hz, Rn, Ubz, Vz, Yt = b["H"], b["Hz"], b["Rn"], b["Ubz"], b["Vz"], b["Yt"]
                    self.dma(KQ[:, :, :].rearrange("p m t -> p (m t)"), DKQ[s, d, j], [self.dd(nm + "KQ", s, d, j)], [KQ])
                    self.dma(MAT[:, :, :].rearrange("p h t -> p (h t)"), DMAT[s, d, j], [self.dd(nm + "MAT", s, d, j)], [MAT])
                    self.dma(BC[:, :], DBC[s, d, j], [self.dd(nm + "BC", s, d, j)], [BC])
                    self.dma(Vt[:, :], DV_[s, j], [self.dd(nm + "V", s, j)], [Vt])
                    self.dma(pc[:, :], DPC[s, d, j], [self.dd(nm + "PC", s, d, j)], [pc])
                    if i == 0:
                        self.memset(G, H[:, :, :], 0.0, [H])
                        self.memset(G, Hz[:, :, :], 0.0, [Hz])
                        self.memset(G, Rn[:, :, :], 0.0, [Rn])
                        for c in range(2):
                            self.memset(G, Ubz[c][:, :, :], 0.0, [Ubz[c]])
                            self.memset(G, Vz[c][:, :], 0.0, [Vz[c]])
                    for c in range(2):
                        self.cp(G, Vz[c][c * 64:c * 64 + 64, :], Vt[c * 64:c * 64 + 64, :], [Vt], [Vz[c]])
                    pA, pB = ps[2 * ci], ps[2 * ci + 1]
                    pAv = pA[:, :].rearrange("p (h v) -> p h v", v=dv)
                    pBv = pB[:, :].rearrange("p (h v) -> p h v", v=dv)
                    if not gdn:
                        pBe = pB[:, :].rearrange("p (m e v) -> p m e v", e=2, v=64)
                        Hze = Hz[:, :, :].rearrange("p (m e) v -> p m e v", e=2)
                    pcv = pc[:, :].rearrange("p (m c) -> p m c", c=2)
                    for c in ([0, 1] if d == 0 else [1, 0]):
                        cs = slice(c * 64, c * 64 + 64)
                        Ub = Ubz[c]
                        for h in range(nh):
                            m = h if gdn else h // 2
                            self.mm(pAv[:, h, :], KQ[:, m, 0:128], Hz[:, h, :], True, False, [KQ, Hz], [pA])
                            self.mm(pAv[:, h, :], MAT[:, h, 256:384], Vt[:, h * dv:(h + 1) * dv], False, True, [MAT, Vt], [pA])
                        self.ts(V, Rn[cs, :, :], pAv[cs, :, :], -1.0, None, ALU.mult, None, [pA], [Rn])
                        for h in range(nh):
                            self.mm(pBv[:, h, :], MAT[:, h, 0:128], Rn[:, h, :], True, True, [MAT, Rn], [pB])
                        self.cp(self.act, Ub[cs, :, :], pBv[cs, :, :], [pB], [Ub])
                        for h in range(nh):
                            m = h if gdn else h // 2
                            self.mm(pAv[:, h, :], KQ[:, m, 128:256], Hz[:, h, :], True, False, [KQ, Hz], [pA])
                            self.mm(pAv[:, h, :], MAT[:, h, 128:256], Ub[:, h, :], False, False, [MAT, Ub], [pA])
                            self.mm(pAv[:, h, :], MAT[:, h, 384:512], Vt[:, h * dv:(h + 1) * dv], False, True, [MAT, Vt], [pA])
                        self.cp(V, Yt[cs, :], pA[cs, :], [pA], [Yt])
                        for h in range(nh):
                            m = h if gdn else h // 2
                            self.mm(pBv[:, h, :], BC[:, m * 128:(m + 1) * 128], Ub[:, h, :], True, False, [BC, Ub], [pB])
                            self.mm(pBv[:, h, :], BC[:, 512 + m * 128:512 + (m + 1) * 128], Vz[c][:, h * dv:(h + 1) * dv], False, True, [BC, Vz[c]], [pB])
                        if gdn:
                            self.tt(V, H[:, :, :], H[:, :, :], pcv[:, :, c:c + 1].to_broadcast([128, 4, dv]), ALU.mult, [H, pc], [H])
                            self.tt(V, H[:, :, :], H[:, :, :], pBv[:, :, :], ALU.add, [H, pB], [H])
                            self.cp(self.act, Hz[:, :, :], H[:, :, :], [H], [Hz])
                        else:
                            for e in range(2):
                                rows = slice(e * 64, e * 64 + 64)
                                self.tt(V, H[rows, :, :], H[rows, :, :], pBe[rows, :, e, :], ALU.add, [H, pB], [H])
                            self.tt(V, H[:, :, :], H[:, :, :], pcv[:, :, c:c + 1].to_broadcast([128, 4, 64]), ALU.mult, [H, pc], [H])
                            for e in range(2):
                                rows = slice(e * 64, e * 64 + 64)
                                self.cp(self.act, Hze[rows, :, e, :], H[rows, :, :], [H], [Hz])
                    self.dma(DY[d, n0:n0 + 128, :], Yt[:, :], [Yt], [self.dd(nm + "Y", d, n0 // 128)])
            self.barrier()

    def build(self):
        try:
            self.build_()
        except StopBuild:
            self.es2 = None
            self.finish()

    def build_(self):
        self.consts()
        self.gdn_setup()
        self.conf_setup()
        self.phase0()
        if self.stop == "0":
            return self.finish()
        for l in range(self.nlayers):
            self.phaseA(l)
            if self.stop == f"A{l}":
                return self.finish()
            self.phaseB(l)
            if self.stop == f"B{l}":
                return self.finish()
            self.rwkv_prep(l)
            if self.stop == f"C{l}":
                return self.finish()
            self.rwkv_scan(l)
            if self.stop == f"D{l}":
                return self.finish()
            self.gdn_prep(l)
            if self.stop == f"E{l}":
                return self.finish()
            self.rwkv_scan(l, gdn=True)
            if self.stop == f"F{l}":
                return self.finish()
            self.conformer(l)
            if self.stop == f"G{l}":
                return self.finish()
            self.merge(l)
            if self.stop == f"H{l}":
                return self.finish()
            self.moe(l)
            if self.stop == f"I{l}":
                return self.finish()
        self.final()
        self.finish()
```

```python
import threading
import numpy as np
from contextlib import ExitStack
import concourse.bass as bass
import concourse.mybir as mybir
from concourse.bass_utils import run_bass_kernel_spmd

F32 = mybir.dt.float32
BF16 = mybir.dt.bfloat16
ALU = mybir.AluOpType
AF = mybir.ActivationFunctionType
AX = mybir.AxisListType

D = 1024
S = 2
TC = 256
TL = 2048
T = TC + TL
NT = S * T
L = 2
NIN = 8080
NDS = 24


class Dep:
    __slots__ = ("w", "r")

    def __init__(self):
        self.w = None
        self.r = {}


class Tl:
    def __init__(self, t):
        self.t = t
        self.d = Dep()

    def __getitem__(self, k):
        return self.t[k]


class Eng:
    def __init__(self, name, be, sem):
        self.key = name
        self.be = be
        self.sem = sem
        self.n = 0
        self.waited = {}


def _d(x):
    return x.d if hasattr(x, "d") else x


class KB:
    def __init__(self, dbg=()):
        self.nc = nc = bass.Bass("TRN2", target_bir_lowering=False)
        self.es = ExitStack()
        self.dbg = set(dbg)
        e = self.es.enter_context
        self.pe = Eng("pe", nc.tensor, e(nc.semaphore("s_pe")))
        self.act = Eng("act", nc.scalar, e(nc.semaphore("s_act")))
        self.dve = Eng("dve", nc.vector, e(nc.semaphore("s_dve")))
        self.pool = Eng("pool", nc.gpsimd, e(nc.semaphore("s_pool")))
        self.sp = Eng("sp", nc.sync, e(nc.semaphore("s_sp")))
        self.engs = [self.pe, self.act, self.dve, self.pool, self.sp]
        self.dsem = [e(nc.semaphore(f"s_d{i}")) for i in range(NDS)]
        self.dcnt = [0] * NDS
        self.drr = 0
        self.ddeps = {}
        self.ntile = 0
        self.yielders = {}

    def sb(self, shape, dt=F32, es=None):
        self.ntile += 1
        t = (es or self.es).enter_context(self.nc.sbuf_tensor(f"t{self.ntile}", list(shape), dt))
        return Tl(t)

    def psum(self, shape, dt=F32, es=None):
        self.ntile += 1
        t = (es or self.es).enter_context(self.nc.psum_tensor(f"p{self.ntile}", list(shape), dt))
        return Tl(t)

    def dram(self, name, shape, dt=F32, kind=None):
        if kind is None:
            kind = "ExternalOutput" if name in self.dbg else "Internal"
        return self.nc.dram_tensor(name, list(shape), dt, kind=kind).ap()

    def dd(self, *key):
        d = self.ddeps.get(key)
        if d is None:
            d = self.ddeps[key] = Dep()
        return d

    def dr(self, name, n0, w):
        return [self.dd(name, i) for i in range(n0 // 128, (n0 + w + 127) // 128)]

    def _sync(self, E, R, W):
        need = {}

        def upd(tok):
            k, sem, val = tok
            if k not in need or need[k][1] < val:
                need[k] = (sem, val)

        for d in R:
            d = _d(d)
            if d.w:
                upd(d.w)
        for d in W:
            d = _d(d)
            if d.w:
                upd(d.w)
            for k, (sem, val) in d.r.items():
                if k != E.key:
                    upd((k, sem, val))
        for k, (sem, val) in need.items():
            if k == E.key and E is self.pe:
                continue
            if E.waited.get(k, 0) < val:
                E.be.wait_ge(sem, val)
                E.waited[k] = val

    def _mark(self, tok, R, W):
        k, sem, val = tok
        for d in R:
            _d(d).r[k] = (sem, val)
        for d in W:
            d = _d(d)
            d.w = tok
            d.r = {}

    def op(self, E, fn, R, W):
        self._sync(E, R, W)
        ins = fn()
        E.n += 1
        ins.then_inc(E.sem, 1)
        self._mark((E.key, E.sem, E.n), R, W)
        self._yield()

    def dma(self, out, in_, R, W, Q=None, **kw):
        Q = Q or self.sp
        self._sync(Q, R, W)
        s = self.drr
        self.drr = (s + 1) % NDS
        sem = self.dsem[s]
        k = ("d", s)
        if self.dcnt[s] > 0 and Q.waited.get(k, 0) < 16 * self.dcnt[s]:
            Q.be.wait_ge(sem, 16 * self.dcnt[s])
            Q.waited[k] = 16 * self.dcnt[s]
        Q.be.dma_start(out=out, in_=in_, **kw).then_inc(sem, 16)
        self.dcnt[s] += 1
        self._mark((k, sem, 16 * self.dcnt[s]), R, W)
        self._yield()

    def _yield(self):
        if self.yielders:
            y = self.yielders.get(threading.get_ident())
            if y:
                y()

    def run_interleaved(self, fns):
        n = len(fns)
        state = {"turn": 0, "done": [False] * n, "exc": None}
        cv = threading.Condition()

        def advance(i):
            for k in range(1, n + 1):
                nx = (i + k) % n
                if not state["done"][nx]:
                    state["turn"] = nx
                    break
            else:
                state["turn"] = -1
            cv.notify_all()

        def yielder(i):
            def y():
                with cv:
                    advance(i)
                    while state["turn"] != i:
                        cv.wait()
            return y

        def worker(i):
            with cv:
                while state["turn"] != i:
                    cv.wait()
            self.yielders[threading.get_ident()] = yielder(i)
            try:
                fns[i]()
            except BaseException as e:
                state["exc"] = e
            finally:
                self.yielders.pop(threading.get_ident(), None)
                with cv:
                    state["done"][i] = True
                    advance(i)

        ths = [threading.Thread(target=worker, args=(i,)) for i in range(n)]
        for th in ths:
            th.start()
        for th in ths:
            th.join()
        if state["exc"] is not None:
            raise state["exc"]

    def barrier(self):
        for E in self.engs:
            for E2 in self.engs:
                if E2 is not E and E2.n > 0 and E.waited.get(E2.key, 0) < E2.n:
                    E.be.wait_ge(E2.sem, E2.n)
                    E.waited[E2.key] = E2.n
            for s in range(NDS):
                k = ("d", s)
                if self.dcnt[s] > 0 and E.waited.get(k, 0) < 16 * self.dcnt[s]:
                    E.be.wait_ge(self.dsem[s], 16 * self.dcnt[s])
                    E.waited[k] = 16 * self.dcnt[s]

    def mm(self, out, lhsT, rhs, start, stop, R, W):
        self.op(self.pe, lambda: self.nc.tensor.matmul(out, lhsT=lhsT, rhs=rhs, start=start, stop=stop), R, W)

    def tr(self, out, in_, ident, R, W):
        self.op(self.pe, lambda: self.nc.tensor.transpose(out, in_, ident), R, W)

    def actf(self, out, in_, func, R, W, bias=None, scale=None):
        kw = {}
        if bias is not None:
            kw["bias"] = bias
        if scale is not None:
            kw["scale"] = scale
        self.op(self.act, lambda: self.nc.scalar.activation(out=out, in_=in_, func=func, **kw), R, W)

    def ts(self, E, out, in0, s1, s2, op0, op1, R, W):
        if op1 is None:
            self.op(E, lambda: E.be.tensor_scalar(out=out, in0=in0, scalar1=s1, scalar2=None, op0=op0), R, W)
        else:
            self.op(E, lambda: E.be.tensor_scalar(out=out, in0=in0, scalar1=s1, scalar2=s2, op0=op0, op1=op1), R, W)

    def tt(self, E, out, in0, in1, op, R, W):
        self.op(E, lambda: E.be.tensor_tensor(out=out, in0=in0, in1=in1, op=op), R, W)

    def stt(self, E, out, in0, scalar, in1, op0, op1, R, W):
        self.op(E, lambda: E.be.scalar_tensor_tensor(out=out, in0=in0, scalar=scalar, in1=in1, op0=op0, op1=op1), R, W)

    def cp(self, E, out, in_, R, W):
        if E is self.act:
            self.op(E, lambda: self.nc.scalar.copy(out=out, in_=in_), R, W)
        else:
            self.op(E, lambda: E.be.tensor_copy(out=out, in_=in_), R, W)

    def recip(self, out, in_, R, W):
        self.op(self.dve, lambda: self.nc.vector.reciprocal(out=out, in_=in_), R, W)

    def memset(self, E, ap, v, W):
        self.op(E, lambda: E.be.memset(ap, v), [], W)


class PF:
    def __init__(self):
        self.cols = {}
        self.n = 0

    def add(self, name, nch):
        self.cols[name] = (self.n, nch)
        self.n += nch
        return self.cols[name][0]


def pf_layout():
    pf = PF()
    for nm, nch in [("norm_mix", 8), ("norm_ffn", 8), ("b_ada", 48), ("mu0", 15), ("mu1", 15),
                    ("w0_0", 4), ("w0_1", 4), ("a0_0", 4), ("a0_1", 4), ("kk", 4), ("ka", 4), ("rk", 4),
                    ("ln_g", 4), ("ln_b", 4), ("gconv", 60), ("gnorm", 1), ("alog", 1), ("dtb", 1),
                    ("cdw", 124), ("cdwb", 4), ("clng", 4), ("clnb", 4), ("norm_final", 8)]:
        pf.add(nm, nch)
    return pf


def _fm(v, nch):
    return np.ascontiguousarray(np.asarray(v, np.float32).reshape(nch, 128).T)


def pack_pf(inp, l):
    pf = pf_layout()
    out = np.zeros((128, pf.n), np.float32)

    def put(nm, arr):
        o, n = pf.cols[nm]
        out[:, o:o + n] = arr

    put("norm_mix", _fm(inp["norm_mix"][l], 8))
    put("norm_ffn", _fm(inp["norm_ffn"][l], 8))
    put("b_ada", _fm(inp["b_ada"][l], 48))
    put("mu0", _fm(inp["rwkv_mu"][l, 0], 15))
    put("mu1", _fm(inp["rwkv_mu"][l, 1], 15))
    for d in range(2):
        put(f"w0_{d}", _fm(inp["rwkv_w0"][l, d], 4))
        put(f"a0_{d}", _fm(inp["rwkv_a0"][l, d], 4))
    put("kk", _fm(inp["rwkv_kk"][l], 4))
    put("ka", _fm(inp["rwkv_ka"][l], 4))
    put("rk", _fm(inp["rwkv_rk"][l].reshape(-1), 4))
    put("ln_g", _fm(inp["rwkv_ln_g"][l], 4))
    put("ln_b", _fm(inp["rwkv_ln_b"][l], 4))
    gc = np.concatenate([_fm(inp["gdn_conv"][l, k], 12) for k in range(5)], axis=1)
    put("gconv", gc)
    put("gnorm", _fm(inp["gdn_norm"][l], 1))
    al = np.zeros((128, 1), np.float32); al[0:8, 0] = np.asarray(inp["gdn_A_log"][l]).reshape(-1)
    db = np.zeros((128, 1), np.float32); db[0:8, 0] = np.asarray(inp["gdn_dt_bias"][l]).reshape(-1)
    put("alog", al)
    put("dtb", db)
    cd = np.concatenate([_fm(inp["conf_dw"][l, k], 4) for k in range(31)], axis=1)
    put("cdw", cd)
    put("cdwb", _fm(inp["conf_dw_b"][l], 4))
    put("clng", _fm(inp["conf_ln_g"][l], 4))
    put("clnb", _fm(inp["conf_ln_b"][l], 4))
    put("norm_final", _fm(inp["norm_final"], 8))
    return out


EPS = 1e-6


class Prog(KB):
    def __init__(self, dbg=(), stop=None, nlayers=L):
        super().__init__(dbg)
        self.stop = stop
        self.nlayers = nlayers
        nc = self.nc
        self.pf = pf_layout()

        def inp(name, shape):
            return nc.dram_tensor(name, list(shape), F32, kind="ExternalInput").ap()

        self.x = inp("x", [S, TL, D])
        self.ctx = inp("ctx", [S, TC, D])
        self.cvecT = inp("cvecT", [D, 3])
        self.pfp = inp("pfp", [L, 128, self.pf.n])
        self.w_ada = inp("w_ada", [L, D, 6 * D])
        self.w_in = inp("w_in", [L, D, NIN])
        self.rw2 = inp("rwkv_w2", [L, 2, 64, 512])
        self.ra2 = inp("rwkv_a2", [L, 2, 64, 512])
        self.rg2 = inp("rwkv_g2", [L, 128, 512])
        self.w_branch = inp("w_branch", [L, 3, 512, D])
        self.w_out = inp("w_out", [L, D, D])
        self.w_router = inp("w_router", [D, 16])
        self.rbias = inp("rbias", [128, 16])
        self.weg = inp("w_e_gate", [L, 16, D, 512])
        self.weu = inp("w_e_up", [L, 16, D, 512])
        self.wed = inp("w_e_down", [L, 16, 512, D])
        self.y = nc.dram_tensor("y", [S, TL, D], F32, kind="ExternalOutput").ap()
        self.XT = self.dram("XT", [D, NT])
        self.XTv = self.XT.rearrange("(c p) n -> p c n", p=128)
        self.PT = self.dram("PT", [NIN, NT])
        self.ps = [self.psum([128, 512], F32) for _ in range(8)]
        self.ident = self.sb([128, 128], F32)
        self.identb = self.sb([128, 128], BF16)
        self.ones = self.sb([128, 128], F32)
        self.PFt = [self.sb([128, self.pf.n], F32) for _ in range(L)]
        self.scT = self.sb([128, 8, 3], F32)
        self.MOD = [self.sb([128, 48, 3], F32) for _ in range(L)]
        self.GM = [self.sb([128, 8, 3], F32) for _ in range(L)]
        self.GF = [self.sb([128, 8, 3], F32) for _ in range(L)]

    def pfv(self, l, name, c=None, n=1):
        o, nch = self.pf.cols[name]
        if c is None:
            return self.PFt[l][:, o:o + nch]
        return self.PFt[l][:, o + c:o + c + n]

    def consts(self):
        nc = self.nc
        P = self.pool
        self.memset(P, self.ident[:, :], 0.0, [self.ident])
        self.op(P, lambda: nc.gpsimd.affine_select(out=self.ident[:, :], in_=self.ident[:, :], pattern=[[-1, 128]],
                                                   compare_op=ALU.not_equal, fill=1.0, base=0, channel_multiplier=1),
                [self.ident], [self.ident])
        self.cp(P, self.identb[:, :], self.ident[:, :], [self.ident], [self.identb])
        self.memset(P, self.ones[:, :], 1.0, [self.ones])
        for l in range(L):
            self.dma(self.PFt[l][:, :], self.pfp[l], [], [self.PFt[l]])
        cv = self.sb([128, 8, 3], F32)
        self.dma(cv[:, :, :], self.cvecT.rearrange("(c p) r -> p c r", p=128), [], [cv])
        self.actf(self.scT[:, :, :], cv[:, :, :], AF.Silu, [cv], [self.scT])

    def phase0(self):
        with ExitStack() as es:
            xin = [self.sb([128, 1024], F32, es) for _ in range(2)]
            xo = [self.sb([128, 8, 128], F32, es) for _ in range(2)]
            i = 0
            for s in range(S):
                for j in range(T // 128):
                    n0 = s * T + j * 128
                    src = self.ctx[s, j * 128:(j + 1) * 128, :] if j < 2 else self.x[s, (j - 2) * 128:(j - 1) * 128, :]
                    a = xin[i % 2]
                    o = xo[i % 2]
                    self.dma(a[:, :], src, [], [a])
                    for c in range(8):
                        pb = self.ps[(i % 2) * 2 + c // 4]
                        self.tr(pb[:, (c % 4) * 128:(c % 4 + 1) * 128], a[:, c * 128:(c + 1) * 128], self.ident[:, :],
                                [a, self.ident], [pb])
                    for hf in range(2):
                        pb = self.ps[(i % 2) * 2 + hf]
                        self.cp(self.act if hf == 0 else self.dve, o[:, hf * 4:(hf + 1) * 4, :],
                                pb[:, :].rearrange("p (c t) -> p c t", c=4), [pb], [o])
                    self.dma(self.XTv[:, :, n0:n0 + 128], o[:, :, :], [o], self.dr("XT", n0, 128))
                    i += 1
            self.barrier()

    def phaseA(self, l):
        with ExitStack() as es:
            wa = [self.sb([128, 8, 768], F32, es) for _ in range(2)]
            pm = self.ps[0]
            wav = self.w_ada[l].rearrange("(k p) n -> p k n", p=128)
            for mg in range(8):
                w = wa[mg % 2]
                for q in range(4):
                    self.dma(w[:, q * 2:(q + 1) * 2, :], wav[:, q * 2:(q + 1) * 2, mg * 768:(mg + 1) * 768], [], [w])
                for m in range(6):
                    mm_ = mg * 6 + m
                    for k in range(8):
                        self.mm(pm[:, mm_ * 3:(mm_ + 1) * 3], w[:, k, m * 128:(m + 1) * 128], self.scT[:, k, :], k == 0, k == 7,
                                [w, self.scT], [pm])
            mod = self.MOD[l]
            self.tt(self.dve, mod[:, :, :], pm[:, 0:144].rearrange("p (m r) -> p m r", r=3),
                    self.pfv(l, "b_ada").unsqueeze(2).to_broadcast([128, 48, 3]), ALU.add, [pm, self.PFt[l]], [mod])
            for (G, mi, nm) in ((self.GM[l], 1, "norm_mix"), (self.GF[l], 4, "norm_ffn")):
                self.ts(self.dve, G[:, :, :], mod[:, mi * 8:(mi + 1) * 8, :], 1.0, None, ALU.add, None, [mod], [G])
                self.dump(f"G1{l}{mi}", G, G[:, :, :], [128, 8, 3])
                self.tt(self.dve, G[:, :, :], G[:, :, :], self.pfv(l, nm).unsqueeze(2).to_broadcast([128, 8, 3]), ALU.mult,
                        [G, self.PFt[l]], [G])
            self.dump(f"MOD{l}", mod, mod[:, :, :], [128, 48, 3])
            self.dump(f"GM{l}", self.GM[l], self.GM[l][:, :, :], [128, 8, 3])
            self.barrier()

    def tiles(self):
        out = []
        for s in range(S):
            out.append((s * T, TC, 2))
            for j in range(TL // 512):
                out.append((s * T + TC + j * 512, 512, s))
        return out

    def modulate_tile(self, es_bufs, n0, w, r, G, shift_col, mod, out_fn):
        xt, sq, rs, tmp = es_bufs
        psS = self.ps[7]
        self.dma(xt[:, :, :w], self.XTv[:, :, n0:n0 + w], self.dr("XT", n0, w), [xt])
        self.actf(sq[:, :, :w], xt[:, :, :w], AF.Square, [xt], [sq])
        for c in range(8):
            self.mm(psS[:, :w], self.ones[:, :], sq[:, c, :w], c == 0, c == 7, [self.ones, sq], [psS])
        self.ts(self.dve, rs[:, :w], psS[:, :w], 1.0 / D, EPS, ALU.mult, ALU.add, [psS], [rs])
        self.actf(rs[:, :w], rs[:, :w], AF.Sqrt, [rs], [rs])
        self.recip(rs[:, :w], rs[:, :w], [rs], [rs])
        return xt, rs

    def phaseB(self, l):
        with ExitStack() as es:
            hT = self.sb([128, 8, NT], BF16, es)
            with ExitStack() as es1:
                xts = [self.sb([128, 8, 512], F32, es1) for _ in range(2)]
                sq = self.sb([128, 8, 512], F32, es1)
                rs = self.sb([128, 512], F32, es1)
                tmps = [self.sb([128, 512], F32, es1) for _ in range(2)]
                for i, (n0, w, r) in enumerate(self.tiles()):
                    xt, _ = self.modulate_tile((xts[i % 2], sq, rs, None), n0, w, r, None, None, None, None)
                    for c in range(8):
                        tmp = tmps[c % 2]
                        self.stt(self.dve, tmp[:, :w], xt[:, c, :w], self.GM[l][:, c, r:r + 1], rs[:, :w], ALU.mult, ALU.mult,
                                 [xt, rs, self.GM[l]], [tmp])
                        self.actf(hT[:, c, n0:n0 + w], tmp[:, :w], AF.Identity, [tmp, self.MOD[l]], [hT],
                                  bias=self.MOD[l][:, c, r:r + 1], scale=1.0)
                if "HT" in self.dbg:
                    hd = self.dram("HT", [128, 8, NT], BF16)
                    self.dma(hd, hT[:, :, :], [hT], [self.dd("HTd")])
                self.barrier()
            wbf = [self.sb([128, 8, 1024], BF16, es) for _ in range(2)]
            ost = [self.sb([128, 512], F32, es) for _ in range(4)]
            no = 0
            def loadw(g):
                c0 = g * 1024
                cw = min(1024, NIN - c0)
                self.dma(wbf[g % 2][:, :, :cw], self.w_in[l][:, c0:c0 + cw].rearrange("(k p) n -> p k n", p=128), [], [wbf[g % 2]], Q=self.pool)
            loadw(0)
            for g in range(8):
                c0 = g * 1024
                cw = min(1024, NIN - c0)
                wb = wbf[g % 2]
                if g < 7:
                    loadw(g + 1)
                nm = (cw + 127) // 128
                for tt_ in range(NT // 512):
                    n0 = tt_ * 512
                    for m in range(nm):
                        mw = min(128, cw - m * 128)
                        pb = self.ps[no % 6]
                        for k in range(8):
                            self.mm(pb[:mw, :], wb[:, k, m * 128:m * 128 + mw], hT[:, k, n0:n0 + 512], k == 0, k == 7,
                                    [wb, hT], [pb])
                        o = ost[no % 4]
                        self.cp(self.act if no % 2 == 0 else self.dve, o[:mw, :], pb[:mw, :], [pb], [o])
                        self.dma(self.PT[c0 + m * 128:c0 + m * 128 + mw, n0:n0 + 512], o[:mw, :], [o],
                                 [self.dd("PT", (c0 + m * 128) // 128, j) for j in range(n0 // 128, n0 // 128 + 4)])
                        no += 1
            self.barrier()

    def dump(self, name, tile, ap, shape, dt=F32):
        if name in self.dbg:
            d = self.dram(name, shape, dt)
            self.dma(d, ap, [tile], [self.dd(name)])

    def finish(self):
        self.barrier()

    def build(self):
        self.consts()
        self.phase0()
        if self.stop == "0":
            return self.finish()
        for l in range(self.nlayers):
            self.phaseA(l)
            if self.stop == f"A{l}":
                return self.finish()
            self.phaseB(l)
            if self.stop == f"B{l}":
                return self.finish()
        self.finish()


def make_in_maps(inp):
    ncores = 8
    pfp = np.stack([pack_pf(inp, l) for l in range(L)])
    rbias = np.ascontiguousarray(np.broadcast_to(np.asarray(inp["router_bias"], np.float32)[None, :], (128, 16)))
    maps = []
    for i in range(ncores):
        cv = np.stack([inp["c"][2 * i], inp["c"][2 * i + 1], inp["c_ctx"]], axis=1).astype(np.float32)
        m = {
            "x": np.ascontiguousarray(inp["x"][2 * i:2 * i + 2]),
            "ctx": np.ascontiguousarray(inp["ctx"][2 * i:2 * i + 2]),
            "cvecT": np.ascontiguousarray(cv),
            "pfp": pfp, "rbias": rbias,
        }
        for k in ("w_ada", "w_in", "rwkv_w2", "rwkv_a2", "rwkv_g2", "w_branch", "w_out", "w_router",
                  "w_e_gate", "w_e_up", "w_e_down"):
            m[k] = np.ascontiguousarray(inp[k], dtype=np.float32)
        maps.append(m)
    return maps


def kernel(**inputs):
    inp = {k: np.asarray(v) for k, v in inputs.items()}
    prog = Prog2()
    prog.build()
    maps = make_in_maps(inp)
    res = run_bass_kernel_spmd(prog.nc, maps, core_ids=list(range(8)))
    return np.concatenate([r["y"] for r in res.results], axis=0).astype(np.float32)


CDEC = 0.6065306597126334
NJ = T // 128


def seg_bounds(j):
    return (j == 0 or j == 2), (j == 1 or j == NJ - 1)


class StopBuild(Exception):
    pass


class Prog2(Prog):
    cut = None

    def ck(self, n):
        if self.cut == n:
            raise StopBuild()

    def __init__(self, **kw):
        super().__init__(**kw)
        self.PTv = self.PT[0:8064, :].rearrange("(c p) n -> p c n", p=128)
        self.RKQ = self.dram("RKQ", [S, 2, NJ, 128, 4 * 256], BF16)
        self.RMAT = self.dram("RMAT", [S, 2, NJ, 128, 8 * 512], BF16)
        self.RBC = self.dram("RBC", [S, 2, NJ, 128, 1024], BF16)
        self.RV = self.dram("RV", [S, NJ, 128, 512], BF16)
        self.RPC = self.dram("RPC", [S, 2, NJ, 128, 8], F32)
        self.GAs = self.dram("GAs", [512, NT])
        self.BON = self.dram("BON", [512, NT])
        self.YA = self.dram("YA", [2, NT, 512])
        self.bones = self.sb([128, 128], F32)
        self.MU = self.sb([128, 256], F32)
        self.ML = self.sb([128, 256], F32)
        self.RM = self.sb([128, 128], F32)

    def consts(self):
        super().consts()
        nc = self.nc
        P = self.pool
        self.memset(P, self.bones[:, :], 0.0, [self.bones])
        self.memset(P, self.bones[0:64, 0:64], 1.0, [self.bones])
        self.memset(P, self.bones[64:128, 64:128], 1.0, [self.bones])
        self.memset(P, self.RM[:, :], 1.0, [self.RM])
        self.memset(P, self.RM[:, 0:1], 0.0, [self.RM])
        self.memset(P, self.RM[:, 64:65], 0.0, [self.RM])
        for (Mt, off, cmp_, sg) in ((self.MU, 0, ALU.is_gt, 1), (self.MU, 128, ALU.is_ge, 1), (self.ML, 0, ALU.is_gt, -1), (self.ML, 128, ALU.is_ge, -1)):
            sl = Mt[:, off:off + 128]
            self.memset(P, sl, 1.0, [Mt])
            self.op(P, lambda sl=sl, cmp_=cmp_, sg=sg: nc.gpsimd.affine_select(out=sl, in_=sl, pattern=[[sg, 128]], compare_op=cmp_, fill=0.0,
                                                                              base=0, channel_multiplier=-sg), [Mt], [Mt])
            self.memset(P, Mt[0:64, off + 64:off + 128], 0.0, [Mt])
            self.memset(P, Mt[64:128, off:off + 64], 0.0, [Mt])

    def load_halo(self, dst, c0, nch, s, j, hw):
        n0 = s * T + j * 128
        lb, rb = seg_bounds(j)
        lo = 0 if lb else hw
        hi = 0 if rb else hw
        if lb:
            self.memset(self.pool, dst[:, :, 0:hw], 0.0, [dst])
        if rb:
            self.memset(self.pool, dst[:, :, 128 + hw:128 + 2 * hw], 0.0, [dst])
        deps = [self.dd("PT", c, i) for c in range(c0, c0 + nch) for i in range((n0 - lo) // 128, (n0 + 128 + hi - 1) // 128 + 1)]
        self.dma(dst[:, :, hw - lo:hw + 128 + hi], self.PTv[:, c0:c0 + nch, n0 - lo:n0 + 128 + hi], deps, [dst])

    def inverse(self, X1, NN, MAT, h, Wt, At, Bt, pA, pB_, pC):
        V, G = self.dve, self.pool
        W, A, B = Wt[0], At[0], Bt[0]
        self.tt(G, W[:, :], self.identb[:, :], X1[:, 0:128], ALU.subtract, [self.identb, X1], [W])
        self.mm(pA[:, 0:128], X1[:, 0:128], NN[:, :], True, True, [X1, NN], [pA])
        self.mm(pB_[:, 0:128], NN[:, :], X1[:, 0:128], True, True, [X1, NN], [pB_])
        self.cp(self.act, A[:, :], pA[:, 0:128], [pA], [A])
        self.cp(self.act, B[:, :], pB_[:, 0:128], [pB_], [B])
        for it in range(5):
            W2, A2, B2 = Wt[(it + 1) % 2], At[(it + 1) % 2], Bt[(it + 1) % 2]
            self.mm(pC[:, 0:128], A[:, :], W[:, :], True, True, [A, W], [pC])
            if it < 4:
                self.mm(pA[:, 0:128], B[:, :], A[:, :], True, True, [A, B], [pA])
                self.mm(pB_[:, 0:128], A[:, :], B[:, :], True, True, [A, B], [pB_])
            dstW = W2[:, :] if it < 4 else MAT[:, h, 0:128]
            self.tt(V, dstW, W[:, :], pC[:, 0:128], ALU.add, [W, pC], [W2 if it < 4 else MAT])
            if it < 4:
                self.cp(self.act, A2[:, :], pA[:, 0:128], [pA], [A2])
                self.cp(self.act, B2[:, :], pB_[:, 0:128], [pB_], [B2])
            W, A, B = W2, A2, B2

    def inverse_batch(self, X1a, NNa, MAT, nh, Wt, At, Bt):
        V, G = self.dve, self.pool
        nb = nh // 4
        psW, psA, psB = self.ps[0:nb], self.ps[2:2 + nb], self.ps[4:4 + nb]

        def reg(pl, h):
            return pl[h // 4][:, (h % 4) * 128:(h % 4 + 1) * 128]

        def bv(p):
            return p[:, :].rearrange("p (h t) -> p h t", h=4)
        W, A, B = Wt[0], At[0], Bt[0]
        self.tt(G, W[:, :, :], self.identb[:, :].unsqueeze(1).to_broadcast([128, nh, 128]), X1a[:, :, 0:128], ALU.subtract, [self.identb, X1a], [W])
        for h in range(nh):
            self.mm(reg(psA, h), X1a[:, h, 0:128], NNa[:, h, :], True, True, [X1a, NNa], [psA[h // 4]])
        for h in range(nh):
            self.mm(reg(psB, h), NNa[:, h, :], X1a[:, h, 0:128], True, True, [X1a, NNa], [psB[h // 4]])
        for b in range(nb):
            self.cp(self.act, A[:, 4 * b:4 * b + 4, :], bv(psA[b]), [psA[b]], [A])
            self.cp(V, B[:, 4 * b:4 * b + 4, :], bv(psB[b]), [psB[b]], [B])
        for it in range(5):
            W2, A2, B2 = Wt[(it + 1) % 2], At[(it + 1) % 2], Bt[(it + 1) % 2]
            for h in range(nh):
                self.mm(reg(psW, h), A[:, h, :], W[:, h, :], True, True, [A, W], [psW[h // 4]])
            if it < 4:
                for h in range(nh):
                    self.mm(reg(psA, h), B[:, h, :], A[:, h, :], True, True, [A, B], [psA[h // 4]])
                for h in range(nh):
                    self.mm(reg(psB, h), A[:, h, :], B[:, h, :], True, True, [A, B], [psB[h // 4]])
            for b in range(nb):
                if it < 4:
                    self.tt(V, W2[:, 4 * b:4 * b + 4, :], W[:, 4 * b:4 * b + 4, :], bv(psW[b]), ALU.add, [W, psW[b]], [W2])
                else:
                    self.tt(V, MAT[:, 4 * b:4 * b + 4, 0:128], W[:, 4 * b:4 * b + 4, :], bv(psW[b]), ALU.add, [W, psW[b]], [MAT])
            if it < 4:
                for b in range(nb):
                    self.cp(self.act, A2[:, 4 * b:4 * b + 4, :], bv(psA[b]), [psA[b]], [A2])
                    self.cp(V, B2[:, 4 * b:4 * b + 4, :], bv(psB[b]), [psB[b]], [B2])
            W, A, B = W2, A2, B2

    def rwkv_prep(self, l):
        nc = self.nc
        V, G = self.dve, self.pool
        with ExitStack() as es:
            def t(shape, dt=F32):
                return self.sb(shape, dt, es)
            wtmp = t([128, 512])
            w2b, a2b, g2b = t([128, 512], BF16), t([128, 512], BF16), t([128, 512], BF16)
            for (src, dstb) in ((self.rw2[l].rearrange("d r c -> (d r) c"), w2b), (self.ra2[l].rearrange("d r c -> (d r) c"), a2b), (self.rg2[l], g2b)):
                self.dma(wtmp[:, :], src, [], [wtmp])
                self.cp(V, dstb[:, :], wtmp[:, :], [wtmp], [dstb])
            PFl = self.PFt[l]
            c0t = t([128, 15])
            self.tt(V, c0t[:, :], self.pfv(l, "mu0"), self.pfv(l, "mu1"), ALU.add, [PFl], [c0t])
            self.ts(V, c0t[:, :], c0t[:, :], -1.0, 1.0, ALU.mult, ALU.add, [c0t], [c0t])
            omka = t([128, 4])
            self.ts(V, omka[:, :], self.pfv(l, "ka"), -1.0, 1.0, ALU.mult, ALU.add, [PFl], [omka])

            def bc(ap, n):
                return ap.unsqueeze(2).to_broadcast([128, n, 128])

            def stream(s):
                pa = t([128, 15, 130])
                sh = t([128, 15, 128])
                twb, xab, sgb = t([128, 128], BF16), t([128, 128], BF16), t([128, 128], BF16)
                SW = [t([128, 4, 128]) for _ in range(2)]
                AA = [t([128, 4, 128]) for _ in range(2)]
                ga = t([128, 4, 128])
                kx, kk, tq, bon = t([128, 4, 128]), t([128, 4, 128]), t([128, 4, 128]), t([128, 4, 128])
                CS, EX, tmpa, tmpb = (t([128, 4, 128]) for _ in range(4))
                e1, e2, e3 = (t([128, 4, 128]) for _ in range(3))
                pc = t([128, 8])
                KQ = t([128, 4, 256], BF16)
                BTb, CTb = t([128, 4, 128], BF16), t([128, 4, 128], BF16)
                vb = t([128, 4, 128], BF16)
                BC = t([128, 1024], BF16)
                Vt = t([128, 512], BF16)
                MAT = t([128, 8, 512], BF16)
                X1a = t([128, 8, 256], BF16)
                NNa = t([128, 8, 128], BF16)
                BTz, CTz = t([128, 8, 128], BF16), t([128, 8, 128], BF16)
                self.memset(G, BTz[:, :, :], 0.0, [BTz])
                self.memset(G, CTz[:, :, :], 0.0, [CTz])
                Wt = [t([128, 8, 128], BF16) for _ in range(2)]
                At = [t([128, 8, 128], BF16) for _ in range(2)]
                Bt = [t([128, 8, 128], BF16) for _ in range(2)]
                ps = self.ps
                for j in range(NJ):
                    n0 = s * T + j * 128
                    self.ck(1000 + s * NJ + j)
                    self.load_halo(pa, 0, 15, s, j, 1)
                    self.ck(1)
                    self.tt(V, sh[:, :, :], pa[:, :, 1:129], bc(c0t[:, :], 15), ALU.mult, [pa, c0t], [sh])
                    for c in range(15):
                        self.stt(V, sh[:, c, :], pa[:, c, 0:128], self.pfv(l, "mu0", c), sh[:, c, :], ALU.mult, ALU.add, [pa, PFl, sh], [sh])
                        self.stt(V, sh[:, c, :], pa[:, c, 2:130], self.pfv(l, "mu1", c), sh[:, c, :], ALU.mult, ALU.add, [pa, PFl, sh], [sh])
                    self.ck(2)
                    r_, k_, v_ = sh[:, 0:4, :], sh[:, 4:8, :], sh[:, 8:12, :]
                    self.actf(twb[:, :], sh[:, 12, :], AF.Tanh, [sh], [twb])
                    self.cp(self.act, xab[:, :], sh[:, 13, :], [sh], [xab])
                    self.actf(sgb[:, :], sh[:, 14, :], AF.Sigmoid, [sh], [sgb])
                    self.ck(3)
                    for d in range(2):
                        for (wb_, xin, dst, bname, pb) in ((w2b, twb, SW[d], f"w0_{d}", ps[0]), (a2b, xab, AA[d], f"a0_{d}", ps[1])):
                            for m in range(4):
                                self.mm(pb[:, m * 128:(m + 1) * 128], wb_[d * 64:(d + 1) * 64, m * 128:(m + 1) * 128],
                                        xin[d * 64:(d + 1) * 64, :], True, True, [wb_, xin], [pb])
                            for m in range(4):
                                self.actf(dst[:, m, :], pb[:, m * 128:(m + 1) * 128], AF.Sigmoid, [pb, PFl], [dst],
                                          bias=self.pfv(l, bname, m), scale=1.0)
                    for m in range(4):
                        self.mm(ps[2][:, m * 128:(m + 1) * 128], g2b[:, m * 128:(m + 1) * 128], sgb[:, :], True, True, [g2b, sgb], [ps[2]])
                    self.cp(self.act, ga[:, :, :], ps[2][:, :].rearrange("p (m t) -> p m t", m=4), [ps[2]], [ga])
                    self.dma(self.GAs.rearrange("(m p) n -> p m n", p=128)[:, :, n0:n0 + 128], ga[:, :, :], [ga], [self.dd("GAs", n0 // 128)])
                    self.ck(4)
                    self.tt(V, kx[:, :, :], k_, bc(self.pfv(l, "kk"), 4), ALU.mult, [sh, PFl], [kx])
                    self.tt(G, tq[:, :, :], kx[:, :, :], kx[:, :, :], ALU.mult, [kx], [tq])
                    for m in range(4):
                        self.mm(ps[3][:, m * 128:(m + 1) * 128], self.bones[:, :], tq[:, m, :], True, True, [self.bones, tq], [ps[3]])
                    self.ts(V, tq[:, :, :], ps[3][:, :].rearrange("p (m t) -> p m t", m=4), EPS, None, ALU.add, None, [ps[3]], [tq])
                    self.actf(tq[:, :, :], tq[:, :, :], AF.Sqrt, [tq], [tq])
                    self.recip(tq[:, :, :], tq[:, :, :], [tq], [tq])
                    self.tt(V, kk[:, :, :], kx[:, :, :], tq[:, :, :], ALU.mult, [kx, tq], [kk])
                    self.ck(5)
                    self.tt(G, bon[:, :, :], r_, k_, ALU.mult, [sh], [bon])
                    self.tt(G, bon[:, :, :], bon[:, :, :], bc(self.pfv(l, "rk"), 4), ALU.mult, [bon, PFl], [bon])
                    for m in range(4):
                        self.mm(ps[4][:, m * 128:(m + 1) * 128], self.bones[:, :], bon[:, m, :], True, True, [self.bones, bon], [ps[4]])
                    self.tt(V, bon[:, :, :], ps[4][:, :].rearrange("p (m t) -> p m t", m=4), v_, ALU.mult, [ps[4], sh], [bon])
                    self.dma(self.BON.rearrange("(m p) n -> p m n", p=128)[:, :, n0:n0 + 128], bon[:, :, :], [bon], [self.dd("BON", n0 // 128)])
                    self.ck(6)
                    self.cp(G, vb[:, :, :], v_, [sh], [vb])
                    pbv = ps[5][:, :].bitcast(BF16)
                    for m in range(4):
                        self.tr(pbv[:, m * 128:(m + 1) * 128], vb[:, m, :], self.identb[:, :], [vb, self.identb], [ps[5]])
                    self.cp(self.act, Vt[:, :], pbv[:, 0:512], [ps[5]], [Vt])
                    self.dma(self.RV[s, j], Vt[:, :], [Vt], [self.dd("RV", s, j)])
                    for d in range(2):
                        self.ck(7)
                        self.tt(V, tmpa[:, :, :], AA[d][:, :, :], bc(self.pfv(l, "ka"), 4), ALU.mult, [AA[d], PFl], [tmpa])
                        self.tt(V, tmpa[:, :, :], tmpa[:, :, :], bc(omka[:, :], 4), ALU.add, [tmpa, omka], [tmpa])
                        self.tt(V, tmpa[:, :, :], tmpa[:, :, :], k_, ALU.mult, [tmpa, sh], [tmpa])
                        self.tt(G, tmpb[:, :, :], kk[:, :, :], AA[d][:, :, :], ALU.mult, [kk, AA[d]], [tmpb])
                        self.ck(8)
                        for m in range(4):
                            self.op(V, lambda m=m: nc.vector.tensor_tensor_scan(out=CS[:, m, :], data0=self.RM[:, :], data1=SW[d][:, m, :],
                                                                               initial=0.0, op0=ALU.mult, op1=ALU.add),
                                    [self.RM, SW[d]], [CS])
                        CSv = CS[:, :, :].rearrange("p m (c t) -> p m c t", t=64)
                        if d == 0:
                            self.tt(V, EX[:, :, :], CS[:, :, :], SW[d][:, :, :], ALU.subtract, [CS, SW[d]], [EX])
                            incl = CS
                        else:
                            tot = CSv[:, :, :, 63:64].to_broadcast([128, 4, 2, 64])
                            self.tt(V, EX[:, :, :].rearrange("p m (c t) -> p m c t", t=64), tot, CSv, ALU.subtract, [CS], [EX])
                            self.tt(V, e3[:, :, :], EX[:, :, :], SW[d][:, :, :], ALU.add, [EX, SW[d]], [e3])
                            incl = e3
                        self.ck(9)
                        self.actf(e1[:, :, :], EX[:, :, :], AF.Exp, [EX], [e1], scale=-CDEC)
                        self.actf(e2[:, :, :], incl[:, :, :], AF.Exp, [incl], [e2], scale=CDEC)
                        self.actf(e3[:, :, :], incl[:, :, :], AF.Exp, [incl], [e3], scale=-CDEC)
                        self.actf(pc[:, :].rearrange("p (m c) -> p m c", c=2), CSv[:, :, :, 63], AF.Exp, [CS], [pc], scale=-CDEC)
                        self.dma(self.RPC[s, d, j], pc[:, :], [pc], [self.dd("RPC", s, d, j)])
                        self.tt(V, KQ[:, :, 0:128], kk[:, :, :], e1[:, :, :], ALU.mult, [kk, e1], [KQ])
                        self.tt(G, KQ[:, :, 128:256], r_, e3[:, :, :], ALU.mult, [sh, e3], [KQ])
                        self.tt(V, BTb[:, :, :], tmpb[:, :, :], e2[:, :, :], ALU.mult, [tmpb, e2], [BTb])
                        self.tt(G, CTb[:, :, :], tmpa[:, :, :], e2[:, :, :], ALU.mult, [tmpa, e2], [CTb])
                        self.dma(self.RKQ[s, d, j], KQ[:, :, :].rearrange("p m t -> p (m t)"), [KQ], [self.dd("RKQ", s, d, j)])
                        self.ck(10)
                        pbb = ps[6][:, :].bitcast(BF16)
                        for m in range(4):
                            self.tr(pbb[:, m * 128:(m + 1) * 128], BTb[:, m, :], self.identb[:, :], [BTb, self.identb], [ps[6]])
                            self.tr(pbb[:, 512 + m * 128:512 + (m + 1) * 128], CTb[:, m, :], self.identb[:, :], [CTb, self.identb], [ps[6]])
                        self.cp(self.act, BC[:, :], pbb[:, :], [ps[6]], [BC])
                        self.dma(self.RBC[s, d, j], BC[:, :], [BC], [self.dd("RBC", s, d, j)])
                        self.ck(11)
                        Ms, Mn = (self.MU, self.ML) if d == 0 else (self.ML, self.MU)
                        for e_ in range(2):
                            rows = slice(e_ * 64, e_ * 64 + 64)
                            self.cp(G, BTz[:, :, :].rearrange("p (m e) t -> p m e t", e=2)[rows, :, e_, :], BTb[rows, :, :], [BTb], [BTz])
                            self.cp(self.act, CTz[:, :, :].rearrange("p (m e) t -> p m e t", e=2)[rows, :, e_, :], CTb[rows, :, :], [CTb], [CTz])
                        for h in range(8):
                            m = h // 2
                            self.mm(ps[h // 2][:, (h % 2) * 256:(h % 2 + 1) * 256], BTz[:, h, :], KQ[:, m, :], True, True, [BTz, KQ], [ps[h // 2]])
                        for h in range(8):
                            m = h // 2
                            self.mm(ps[4 + h // 2][:, (h % 2) * 256:(h % 2 + 1) * 256], CTz[:, h, :], KQ[:, m, :], True, True, [CTz, KQ], [ps[4 + h // 2]])
                        Msb = Ms[:, :].unsqueeze(1).to_broadcast([128, 2, 256])
                        for b in range(4):
                            self.tt(V, X1a[:, 2 * b:2 * b + 2, :], ps[b][:, :].rearrange("p (h t) -> p h t", h=2), Msb, ALU.mult, [ps[b], Ms], [X1a])
                            self.tt(V, MAT[:, 2 * b:2 * b + 2, 256:512], ps[4 + b][:, :].rearrange("p (h t) -> p h t", h=2), Msb, ALU.mult, [ps[4 + b], Ms], [MAT])
                        for h in range(8):
                            m = h // 2
                            self.mm(ps[h // 4][:, (h % 4) * 128:(h % 4 + 1) * 128], KQ[:, m, 0:128], BTz[:, h, :], True, True, [BTz, KQ], [ps[h // 4]])
                        Mnb = Mn[:, 0:128].unsqueeze(1).to_broadcast([128, 4, 128])
                        for b in range(2):
                            self.tt(V, NNa[:, 4 * b:4 * b + 4, :], ps[b][:, :].rearrange("p (h t) -> p h t", h=4), Mnb, ALU.mult, [ps[b], Mn], [NNa])
                        self.cp(G, MAT[:, :, 128:256], X1a[:, :, 128:256], [X1a], [MAT])
                        self.inverse_batch(X1a, NNa, MAT, 8, Wt, At, Bt)
                        self.ck(12)
                        self.dma(self.RMAT[s, d, j], MAT[:, :, :].rearrange("p h t -> p (h t)"), [MAT], [self.dd("RMAT", s, d, j)])
            stream(0)
            stream(1)
            self.barrier()

    def gdn_setup(self):
        self.GKQ = self.dram("GKQ", [S, 2, NJ, 128, 4 * 256], BF16)
        self.GMAT = self.dram("GMAT", [S, 2, NJ, 128, 4 * 512], BF16)
        self.GBC = self.dram("GBC", [S, 2, NJ, 128, 1024], BF16)
        self.GV = self.dram("GV", [S, NJ, 128, 512], BF16)
        self.GPC = self.dram("GPC", [S, 2, NJ, 128, 8], F32)
        self.YB = self.dram("YB", [2, NT, 512])
        self.SEL = self.sb([16, 16, 128], F32)
        self.selc = self.sb([128, 1], F32)
        self.onec = self.sb([128, 1], F32)
        nc = self.nc
        P = self.pool
        for i in range(16):
            self.cp(P, self.SEL[0:16, i, :], self.ident[0:16, i:i + 1].to_broadcast([16, 128]), [self.ident], [self.SEL])
        self.memset(P, self.onec[:, :], 1.0, [self.onec])
        self.memset(P, self.selc[:, :], 1.0, [self.selc])
        self.op(P, lambda: nc.gpsimd.affine_select(out=self.selc[:, :], in_=self.selc[:, :], pattern=[[0, 1]], compare_op=ALU.is_ge, fill=0.0,
                                                   base=-4, channel_multiplier=1), [self.selc], [self.selc])

    def gdn_prep(self, l):
        nc = self.nc
        V, G = self.dve, self.pool
        ps = self.ps
        with ExitStack() as es:
            def t(shape, dt=F32):
                return self.sb(shape, dt, es)
            PFl = self.PFt[l]

            def bc(ap, n):
                return ap.unsqueeze(2).to_broadcast([128, n, 128])
            negA = t([128, 1])
            self.actf(negA[:, :], self.pfv(l, "alog"), AF.Exp, [PFl], [negA])
            self.ts(V, negA[:, :], negA[:, :], -1.0, None, ALU.mult, None, [negA], [negA])
            def stream(s):
                qkv = t([128, 12, 132])
                cv, t2 = t([128, 12, 128]), t([128, 12, 128])
                sq = t([128, 8, 128])
                kq = t([128, 4, 256])
                kqb = t([128, 4, 256], BF16)
                kb = t([128, 4, 128], BF16)
                vb = t([128, 4, 128], BF16)
                ab, x1, Xg, SIG, gcf, gcr = (t([16, 128]) for _ in range(6))
                X2, E2 = t([16, 256]), t([16, 256])
                TOTb, Etot = t([16, 128]), t([16, 2])
                TS = t([128, 80])
                negg, sB, sC, dd_ = t([128, 8]), t([128, 8]), t([128, 8]), t([128, 8])
                gm4 = t([128, 4, 256])
                KQd = t([128, 4, 256], BF16)
                BC = t([128, 1024], BF16)
                Vt = t([128, 512], BF16)
                MAT = t([128, 4, 512], BF16)
                pc = t([128, 8])
                X1a = t([128, 4, 256], BF16)
                NNa = t([128, 4, 128], BF16)
                Wt = [t([128, 4, 128], BF16) for _ in range(2)]
                At = [t([128, 4, 128], BF16) for _ in range(2)]
                Bt = [t([128, 4, 128], BF16) for _ in range(2)]
                abv = self.PT[3968:3984, :]
                for j in range(NJ):
                    n0 = s * T + j * 128
                    self.load_halo(qkv, 15, 12, s, j, 2)
                    gw = self.pfv(l, "gconv")
                    self.tt(V, cv[:, :, :], qkv[:, :, 0:128], bc(gw[:, 0:12], 12), ALU.mult, [qkv, PFl], [cv])
                    for k in range(1, 5):
                        self.tt(G, t2[:, :, :], qkv[:, :, k:k + 128], bc(gw[:, k * 12:(k + 1) * 12], 12), ALU.mult, [qkv, PFl], [t2])
                        self.tt(V, cv[:, :, :], cv[:, :, :], t2[:, :, :], ALU.add, [cv, t2], [cv])
                    self.actf(cv[:, :, :], cv[:, :, :], AF.Silu, [cv], [cv])
                    self.tt(G, sq[:, :, :], cv[:, 0:8, :], cv[:, 0:8, :], ALU.mult, [cv], [sq])
                    for c in range(8):
                        pb = ps[c // 4]
                        self.mm(pb[:, (c % 4) * 128:(c % 4 + 1) * 128], self.ones[:, :], sq[:, c, :], True, True, [self.ones, sq], [pb])
                    for hf in range(2):
                        self.ts(V, sq[:, hf * 4:(hf + 1) * 4, :], ps[hf][:, :].rearrange("p (c t) -> p c t", c=4), EPS, None, ALU.add, None, [ps[hf]], [sq])
                    self.actf(sq[:, :, :], sq[:, :, :], AF.Sqrt, [sq], [sq])
                    self.recip(sq[:, :, :], sq[:, :, :], [sq], [sq])
                    self.tt(V, kq[:, :, 0:128], cv[:, 4:8, :], sq[:, 4:8, :], ALU.mult, [cv, sq], [kq])
                    self.stt(V, kq[:, :, 128:256], cv[:, 0:4, :], 128.0 ** -0.5, sq[:, 0:4, :], ALU.mult, ALU.mult, [cv, sq], [kq])
                    self.cp(G, kqb[:, :, :], kq[:, :, :], [kq], [kqb])
                    self.cp(G, kb[:, :, :], kq[:, :, 0:128], [kq], [kb])
                    self.cp(G, vb[:, :, :], cv[:, 8:12, :], [cv], [vb])
                    pbv = ps[5][:, :].bitcast(BF16)
                    for m in range(4):
                        self.tr(pbv[:, m * 128:(m + 1) * 128], vb[:, m, :], self.identb[:, :], [vb, self.identb], [ps[5]])
                    self.cp(self.act, Vt[:, :], pbv[:, 0:512], [ps[5]], [Vt])
                    self.dma(self.GV[s, j], Vt[:, :], [Vt], [self.dd("GV", s, j)])
                    pbk = ps[6][:, :].bitcast(BF16)
                    for m in range(4):
                        self.tr(pbk[:, m * 128:(m + 1) * 128], kb[:, m, :], self.identb[:, :], [kb, self.identb], [ps[6]])
                    self.dma(ab[0:16, :], abv[:, n0:n0 + 128], [], [ab])
                    self.actf(x1[0:16, :], ab[0:16, :], AF.Exp, [ab, PFl], [x1], bias=self.pfv(l, "dtb")[0:16, :], scale=1.0)
                    self.actf(x1[0:16, :], x1[0:16, :], AF.Ln, [x1, self.onec], [x1], bias=self.onec[0:16, :], scale=1.0)
                    self.ts(V, Xg[0:16, :], x1[0:16, :], negA[0:16, :], None, ALU.mult, None, [x1, negA], [Xg])
                    self.actf(SIG[0:16, :], ab[0:16, :], AF.Sigmoid, [ab], [SIG])
                    self.op(V, lambda: nc.vector.tensor_tensor_scan(out=gcf[0:16, :], data0=self.RM[0:16, :], data1=Xg[0:16, :], initial=0.0,
                                                                    op0=ALU.mult, op1=ALU.add), [self.RM, Xg], [gcf])
                    gcfv = gcf[0:16, :].rearrange("p (c t) -> p c t", t=64)
                    totb = gcfv[:, :, 63:64].to_broadcast([16, 2, 64])
                    self.cp(V, TOTb[0:16, :].rearrange("p (c t) -> p c t", t=64), totb, [gcf], [TOTb])
                    self.tt(V, gcr[0:16, :], TOTb[0:16, :], gcf[0:16, :], ALU.subtract, [TOTb, gcf], [gcr])
                    self.tt(V, X2[0:16, 128:256], gcr[0:16, :], Xg[0:16, :], ALU.add, [gcr, Xg], [X2])
                    self.tt(V, X2[0:16, 128:256], X2[0:16, 128:256], gcf[0:16, :], ALU.subtract, [X2, gcf], [X2])
                    self.stt(V, X2[0:16, 128:256], X2[0:16, 128:256], self.selc[0:16, :], gcf[0:16, :], ALU.mult, ALU.add, [X2, self.selc, gcf], [X2])
                    self.tt(V, X2[0:16, 0:128], X2[0:16, 128:256], Xg[0:16, :], ALU.subtract, [X2, Xg], [X2])
                    self.actf(E2[0:16, :], X2[0:16, :], AF.Exp, [X2], [E2])
                    self.actf(Etot[0:16, :], gcfv[:, :, 63], AF.Exp, [gcf], [Etot])
                    pT = ps[7]
                    for q_, src in enumerate((Xg[0:16, :], SIG[0:16, :], X2[0:16, 0:128], X2[0:16, 128:256], TOTb[0:16, :])):
                        self.tr(pT[:, q_ * 16:(q_ + 1) * 16], src, self.ident[0:16, 0:16], [Xg, SIG, X2, TOTb, self.ident], [pT])
                    self.cp(V, TS[:, :], pT[:, 0:80], [pT], [TS])
                    self.actf(negg[:, :], TS[:, 0:8], AF.Exp, [TS], [negg], scale=-1.0)
                    self.tt(V, dd_[:, :], TS[:, 64:72], TS[:, 32:40], ALU.subtract, [TS], [dd_])
                    self.actf(sB[:, :], dd_[:, :], AF.Exp, [dd_], [sB])
                    self.tt(V, sB[:, :], sB[:, :], TS[:, 24:32], ALU.mult, [sB, TS], [sB])
                    self.tt(V, dd_[:, :], TS[:, 64:72], TS[:, 48:56], ALU.subtract, [TS], [dd_])
                    self.actf(sC[:, :], dd_[:, :], AF.Exp, [dd_], [sC])
                    self.tt(V, sC[:, :], sC[:, :], TS[:, 24:32], ALU.mult, [sC, TS], [sC])
                    for d in range(2):
                        Ms = self.MU if d == 0 else self.ML
                        for h in range(4):
                            i = d * 4 + h
                            self.mm(ps[h // 2][:, (h % 2) * 256:(h % 2 + 1) * 256], self.SEL[0:16, i, :], X2[0:16, :], True, True, [self.SEL, X2], [ps[h // 2]])
                            self.mm(ps[2 + h // 2][:, (h % 2) * 256:(h % 2 + 1) * 256], self.SEL[0:16, i, :], E2[0:16, :], True, True, [self.SEL, E2], [ps[2 + h // 2]])
                            self.mm(ps[7][:, 128 + h * 2:128 + (h + 1) * 2], self.SEL[0:16, i, :], Etot[0:16, :], True, True, [self.SEL, Etot], [ps[7]])
                        self.cp(self.act, pc[:, :], ps[7][:, 128:136], [ps[7]], [pc])
                        v2 = lambda p: p[:, :].rearrange("p (h t) -> p h t", h=2)
                        for b in range(2):
                            self.tt(V, gm4[:, 2 * b:2 * b + 2, :], v2(ps[b]), TS[:, 32 + d * 4 + 2 * b:32 + d * 4 + 2 * b + 2].unsqueeze(2).to_broadcast([128, 2, 256]),
                                    ALU.subtract, [ps[b], TS], [gm4])
                            self.tt(V, KQd[:, 2 * b:2 * b + 2, :], kq[:, 2 * b:2 * b + 2, :], v2(ps[2 + b]), ALU.mult, [kq, ps[2 + b]], [KQd])
                        self.ts(G, gm4[:, :, :], gm4[:, :, :], 0.0, None, ALU.min, None, [gm4], [gm4])
                        self.actf(gm4[:, :, :], gm4[:, :, :], AF.Exp, [gm4], [gm4])
                        self.tt(G, gm4[:, :, :], gm4[:, :, :], Ms[:, :].unsqueeze(1).to_broadcast([128, 4, 256]), ALU.mult, [gm4, Ms], [gm4])
                        for h in range(4):
                            self.mm(ps[4 + h // 2][:, (h % 2) * 256:(h % 2 + 1) * 256], kb[:, h, :], kqb[:, h, :], True, True, [kb, kqb], [ps[4 + h // 2]])
                        for b in range(2):
                            self.tt(V, gm4[:, 2 * b:2 * b + 2, :], gm4[:, 2 * b:2 * b + 2, :], v2(ps[4 + b]), ALU.mult, [gm4, ps[4 + b]], [gm4])
                        self.tt(V, X1a[:, :, :], gm4[:, :, :], TS[:, 24 + d * 4:28 + d * 4].unsqueeze(2).to_broadcast([128, 4, 256]), ALU.mult, [gm4, TS], [X1a])
                        self.tt(V, MAT[:, :, 256:512], X1a[:, :, :], negg[:, d * 4:d * 4 + 4].unsqueeze(2).to_broadcast([128, 4, 256]), ALU.mult, [X1a, negg], [MAT])
                        self.cp(G, MAT[:, :, 128:256], X1a[:, :, 128:256], [X1a], [MAT])
                        pN = ps[5][:, :].bitcast(BF16)
                        for h in range(4):
                            self.tr(pN[:, h * 128:(h + 1) * 128], X1a[:, h, 0:128], self.identb[:, :], [X1a, self.identb], [ps[5]])
                        self.cp(self.act, NNa[:, :, :], pN[:, 0:512].rearrange("p (h t) -> p h t", h=4), [ps[5]], [NNa])
                        self.inverse_batch(X1a, NNa, MAT, 4, Wt, At, Bt)
                        pbk4 = pbk[:, 0:512].rearrange("p (h t) -> p h t", h=4)
                        self.tt(V, BC[:, 0:512].rearrange("p (h t) -> p h t", h=4), pbk4, sB[:, d * 4:d * 4 + 4].unsqueeze(2).to_broadcast([128, 4, 128]), ALU.mult, [ps[6], sB], [BC])
                        self.tt(V, BC[:, 512:1024].rearrange("p (h t) -> p h t", h=4), pbk4, sC[:, d * 4:d * 4 + 4].unsqueeze(2).to_broadcast([128, 4, 128]), ALU.mult, [ps[6], sC], [BC])
                        self.dma(self.GKQ[s, d, j], KQd[:, :, :].rearrange("p m t -> p (m t)"), [KQd], [self.dd("GKQ", s, d, j)])
                        self.dma(self.GMAT[s, d, j], MAT[:, :, :].rearrange("p h t -> p (h t)"), [MAT], [self.dd("GMAT", s, d, j)])
                        self.dma(self.GBC[s, d, j], BC[:, :], [BC], [self.dd("GBC", s, d, j)])
                        self.dma(self.GPC[s, d, j], pc[:, :], [pc], [self.dd("GPC", s, d, j)])
            stream(0)
            stream(1)
            self.barrier()

    def conf_setup(self):
        self.RCs = self.dram("RCs", [512, NT], BF16)
        self.RAs = self.dram("RAs", [512, NT], BF16)
        self.RBs = self.dram("RBs", [512, NT], BF16)

    def conformer(self, l):
        V, G = self.dve, self.pool
        ps = self.ps
        PFl = self.PFt[l]
        valv = self.PT[3984:3984 + 512, :].rearrange("(c p) n -> p c n", p=128)
        gatv = self.PT[4496:4496 + 512, :].rearrange("(c p) n -> p c n", p=128)
        RCv = self.RCs.rearrange("(c p) n -> p c n", p=128)
        with ExitStack() as es:
            def t(shape, dt=F32):
                return self.sb(shape, dt, es)
            u = t([128, 4, TL])
            gt = t([128, 4, TL])
            o = t([128, 4, TL])
            sq = t([128, 4, 512])
            mu, rs, var = t([128, 512]), t([128, 512]), t([128, 512])
            ob = t([128, 4, 512], BF16)
            cw = self.pfv(l, "cdw")
            for s in range(S):
                for (seg0, W_) in ((0, TC), (TC, TL)):
                    n0 = s * T + seg0
                    for c in range(4):
                        self.dma(u[:, c, :W_], valv[:, c, n0:n0 + W_], [], [u])
                        self.dma(gt[:, c, :W_], gatv[:, c, n0:n0 + W_], [], [gt])
                    self.actf(gt[:, :, :W_], gt[:, :, :W_], AF.Sigmoid, [gt], [gt])
                    self.tt(V, u[:, :, :W_], u[:, :, :W_], gt[:, :, :W_], ALU.mult, [u, gt], [u])
                    for c in range(4):
                        def wk(k):
                            return cw[:, k * 4 + c:k * 4 + c + 1]
                        self.ts(V, o[:, c, :W_], u[:, c, :W_], wk(15), None, ALU.mult, None, [u, PFl], [o])
                        for k in range(31):
                            dlt = k - 15
                            if dlt == 0:
                                continue
                            if seg0 == 0:
                                lo, hi = max(0, -dlt), min(W_, W_ - dlt)
                                self.stt(V, o[:, c, lo:hi], u[:, c, lo + dlt:hi + dlt], wk(k), o[:, c, lo:hi], ALU.mult, ALU.add, [u, PFl, o], [o])
                            elif c < 2:
                                uv = u[:, c, :].rearrange("p (r w) -> p r w", w=64)
                                ov = o[:, c, :].rearrange("p (r w) -> p r w", w=64)
                                lo, hi = max(0, -dlt), min(64, 64 - dlt)
                                self.stt(V, ov[:, :, lo:hi], uv[:, :, lo + dlt:hi + dlt], wk(k), ov[:, :, lo:hi], ALU.mult, ALU.add, [u, PFl, o], [o])
                            else:
                                uv = u[:, c, :].rearrange("p (r w) -> p r w", w=64)
                                ov = o[:, c, :].rearrange("p (r w) -> p r w", w=64)
                                lo, hi = max(0, -dlt), min(32, 32 - dlt)
                                self.stt(V, ov[:, lo:hi, :], uv[:, lo + dlt:hi + dlt, :], wk(k), ov[:, lo:hi, :], ALU.mult, ALU.add, [u, PFl, o], [o])
                        self.ts(V, o[:, c, :W_], o[:, c, :W_], self.pfv(l, "cdwb", c), None, ALU.add, None, [o, PFl], [o])
                    for t0 in range(0, W_, 512):
                        w = min(512, W_ - t0)
                        self.tt(G, sq[:, :, :w], o[:, :, t0:t0 + w], o[:, :, t0:t0 + w], ALU.mult, [o], [sq])
                        for c in range(4):
                            self.mm(ps[0][:, :w], self.ones[:, :], o[:, c, t0:t0 + w], c == 0, c == 3, [self.ones, o], [ps[0]])
                        for c in range(4):
                            self.mm(ps[1][:, :w], self.ones[:, :], sq[:, c, :w], c == 0, c == 3, [self.ones, sq], [ps[1]])
                        self.ts(V, mu[:, :w], ps[0][:, :w], 1.0 / 512, None, ALU.mult, None, [ps[0]], [mu])
                        self.tt(V, var[:, :w], mu[:, :w], mu[:, :w], ALU.mult, [mu], [var])
                        self.stt(V, var[:, :w], ps[1][:, :w], 1.0 / 512, var[:, :w], ALU.mult, ALU.subtract, [ps[1], var], [var])
                        self.ts(V, var[:, :w], var[:, :w], EPS, None, ALU.add, None, [var], [var])
                        self.actf(var[:, :w], var[:, :w], AF.Sqrt, [var], [var])
                        self.recip(rs[:, :w], var[:, :w], [var], [rs])
                        for c in range(4):
                            self.tt(V, sq[:, c, :w], o[:, c, t0:t0 + w], mu[:, :w], ALU.subtract, [o, mu], [sq])
                            self.tt(V, sq[:, c, :w], sq[:, c, :w], rs[:, :w], ALU.mult, [sq, rs], [sq])
                            self.ts(V, sq[:, c, :w], sq[:, c, :w], self.pfv(l, "clng", c), self.pfv(l, "clnb", c), ALU.mult, ALU.add, [sq, PFl], [sq])
                        self.actf(ob[:, :, :w], sq[:, :, :w], AF.Silu, [sq], [ob])
                        self.dma(RCv[:, :, n0 + t0:n0 + t0 + w], ob[:, :, :w], [ob], [self.dd("RCs", (n0 + t0) // 128)])
            self.barrier()

    def red(self, E, out, in_, op, R, W):
        self.op(E, lambda: E.be.tensor_reduce(out=out, in_=in_, axis=AX.X, op=op), R, W)

    def merge(self, l):
        V, G = self.dve, self.pool
        ps = self.ps
        PFl = self.PFt[l]
        with ExitStack() as es:
            def t(shape, dt=F32):
                return self.sb(shape, dt, es)
            wbr = t([128, 12, 1024], BF16)
            wo = t([128, 8, 1024], BF16)
            for n in range(3):
                self.dma(wbr[:, n * 4:(n + 1) * 4, :], self.w_branch[l, n].rearrange("(k p) n -> p k n", p=128), [], [wbr], Q=self.pool)
            self.dma(wo[:, :, :], self.w_out[l].rearrange("(k p) n -> p k n", p=128), [], [wo], Q=self.pool)
            pgv = [self.PT[5008 + n * 1024:5008 + (n + 1) * 1024, :].rearrange("(c p) n -> p c n", p=128) for n in range(3)]
            fm4 = lambda A: A.rearrange("(c p) n -> p c n", p=128)
            y0, y1, ysq = t([128, 512]), t([128, 512]), t([128, 512])
            m1, m2, m3 = t([128, 8]), t([128, 8]), t([128, 8])
            raf, bon, ga, zt = (t([128, 4, 128]) for _ in range(4))
            Rb = [t([128, 4, 128], BF16) for _ in range(3)]
            pg = t([128, 8, 128])
            macc, tmpm = t([128, 8, 128]), t([128, 8, 128])
            mb = t([128, 8, 128], BF16)
            xt = t([128, 8, 128])
            for s in range(S):
                for j in range(NJ):
                    n0 = s * T + j * 128
                    r = 2 if j < 2 else s
                    for br in range(2):
                        DY, nm, nh, dvv, eps_ = ((self.YA, "R", 8, 64, 64e-5), (self.YB, "G", 4, 128, EPS))[br]
                        self.dma(y0[:, :], DY[0, n0:n0 + 128, :], [self.dd(nm + "Y", 0, n0 // 128)], [y0])
                        self.dma(y1[:, :], DY[1, n0:n0 + 128, :], [self.dd(nm + "Y", 1, n0 // 128)], [y1])
                        self.tt(V, y0[:, :], y0[:, :], y1[:, :], ALU.add, [y0, y1], [y0])
                        yv = y0[:, :].rearrange("p (h d) -> p h d", d=dvv)
                        self.tt(G, ysq[:, :], y0[:, :], y0[:, :], ALU.mult, [y0], [ysq])
                        self.red(V, m2[:, 0:nh], ysq[:, :].rearrange("p (h d) -> p h d", d=dvv), ALU.add, [ysq], [m2])
                        if br == 0:
                            self.red(V, m1[:, 0:nh], yv, ALU.add, [y0], [m1])
                            self.ts(V, m1[:, 0:nh], m1[:, 0:nh], 1.0 / dvv, None, ALU.mult, None, [m1], [m1])
                            self.tt(V, m3[:, 0:nh], m1[:, 0:nh], m1[:, 0:nh], ALU.mult, [m1], [m3])
                            self.stt(V, m2[:, 0:nh], m2[:, 0:nh], 1.0 / dvv, m3[:, 0:nh], ALU.mult, ALU.subtract, [m2, m3], [m2])
                            self.ts(V, m2[:, 0:nh], m2[:, 0:nh], eps_, None, ALU.add, None, [m2], [m2])
                            self.tt(V, yv, yv, m1[:, 0:nh].unsqueeze(2).to_broadcast([128, nh, dvv]), ALU.subtract, [y0, m1], [y0])
                        else:
                            self.ts(V, m2[:, 0:nh], m2[:, 0:nh], 1.0 / dvv, eps_, ALU.mult, ALU.add, [m2], [m2])
                        self.actf(m2[:, 0:nh], m2[:, 0:nh], AF.Sqrt, [m2], [m2])
                        self.recip(m2[:, 0:nh], m2[:, 0:nh], [m2], [m2])
                        self.tt(V, yv, yv, m2[:, 0:nh].unsqueeze(2).to_broadcast([128, nh, dvv]), ALU.mult, [y0, m2], [y0])
                        pb = ps[br]
                        for c in range(4):
                            self.tr(pb[:, c * 128:(c + 1) * 128], y0[:, c * 128:(c + 1) * 128], self.ident[:, :], [y0, self.ident], [pb])
                        pbv = pb[:, :].rearrange("p (c t) -> p c t", c=4)
                        if br == 0:
                            for c in range(4):
                                self.ts(V, raf[:, c, :], pbv[:, c, :], self.pfv(l, "ln_g", c), self.pfv(l, "ln_b", c), ALU.mult, ALU.add, [pb, PFl], [raf])
                            self.dma(bon[:, :, :], fm4(self.BON)[:, :, n0:n0 + 128], [self.dd("BON", n0 // 128)], [bon])
                            self.dma(ga[:, :, :], fm4(self.GAs)[:, :, n0:n0 + 128], [self.dd("GAs", n0 // 128)], [ga])
                            self.tt(V, raf[:, :, :], raf[:, :, :], bon[:, :, :], ALU.add, [raf, bon], [raf])
                            self.tt(V, Rb[0][:, :, :], raf[:, :, :], ga[:, :, :], ALU.mult, [raf, ga], [Rb[0]])
                        else:
                            self.dma(zt[:, :, :], self.PTv[:, 27:31, n0:n0 + 128], [], [zt])
                            self.actf(zt[:, :, :], zt[:, :, :], AF.Silu, [zt], [zt])
                            self.stt(V, Rb[1][:, :, :], pbv, self.pfv(l, "gnorm", 0), zt[:, :, :], ALU.mult, ALU.mult, [pb, PFl, zt], [Rb[1]])
                    self.dma(Rb[2][:, :, :], fm4(self.RCs)[:, :, n0:n0 + 128], [self.dd("RCs", n0 // 128)], [Rb[2]])
                    for n in range(3):
                        self.dma(pg[:, :, :], pgv[n][:, :, n0:n0 + 128], [], [pg])
                        self.actf(pg[:, :, :], pg[:, :, :], AF.Sigmoid, [pg], [pg])
                        for m in range(8):
                            pb = ps[2 + m // 4]
                            for k in range(4):
                                self.mm(pb[:, (m % 4) * 128:(m % 4 + 1) * 128], wbr[:, n * 4 + k, m * 128:(m + 1) * 128], Rb[n][:, k, :], k == 0, k == 3,
                                        [wbr, Rb[n]], [pb])
                        for hf in range(2):
                            pbv = ps[2 + hf][:, :].rearrange("p (c t) -> p c t", c=4)
                            dst = macc if n == 0 else tmpm
                            self.tt(V, dst[:, hf * 4:(hf + 1) * 4, :], pbv, pg[:, hf * 4:(hf + 1) * 4, :], ALU.mult, [ps[2 + hf], pg], [dst])
                        if n > 0:
                            self.tt(G, macc[:, :, :], macc[:, :, :], tmpm[:, :, :], ALU.add, [macc, tmpm], [macc])
                    self.cp(G, mb[:, :, :], macc[:, :, :], [macc], [mb])
                    self.dma(xt[:, :, :], self.XTv[:, :, n0:n0 + 128], self.dr("XT", n0, 128), [xt])
                    for m in range(8):
                        pb = ps[4 + m // 4]
                        for k in range(8):
                            self.mm(pb[:, (m % 4) * 128:(m % 4 + 1) * 128], wo[:, k, m * 128:(m + 1) * 128], mb[:, k, :], k == 0, k == 7, [wo, mb], [pb])
                    for m in range(8):
                        pb = ps[4 + m // 4]
                        self.stt(V, xt[:, m, :], pb[:, (m % 4) * 128:(m % 4 + 1) * 128], self.MOD[l][:, 16 + m, r:r + 1], xt[:, m, :], ALU.mult, ALU.add,
                                 [pb, self.MOD[l], xt], [xt])
                    self.dma(self.XTv[:, :, n0:n0 + 128], xt[:, :, :], [xt], self.dr("XT", n0, 128))
                    if f"XM{l}" in self.dbg:
                        pass
            self.barrier()

    def moe(self, l):
        V, G = self.dve, self.pool
        ps = self.ps
        with ExitStack() as es:
            def t(shape, dt=F32, e_=None):
                return self.sb(shape, dt, e_ or es)
            hT = t([128, 8, NT], BF16)
            WTf = t([16, NT])
            wr = t([128, 8, 16])
            rb = t([128, 16])
            self.dma(wr[:, :, :], self.w_router.rearrange("(k p) e -> p k e", p=128), [], [wr])
            self.dma(rb[:, :], self.rbias, [], [rb])
            with ExitStack() as es1:
                xts = [t([128, 8, 512], F32, es1) for _ in range(2)]
                sq = t([128, 8, 512], F32, es1)
                rs = t([128, 512], F32, es1)
                hf = t([128, 8, 512], F32, es1)
                sc, sel, sel2, eq, cm, wts = (t([128, 16], F32, es1) for _ in range(6))
                m1, m2, gs, gsel = (t([128, 4], F32, es1) for _ in range(4))
                gmx, wsum = t([128, 1], F32, es1), t([128, 1], F32, es1)
                v4 = lambda a: a[:, :].rearrange("p (g j) -> p g j", j=4)
                b4 = lambda a: a[:, :].unsqueeze(2).to_broadcast([128, 4, 4])
                for i, (n0, w, r) in enumerate(self.tiles()):
                    xt, _ = self.modulate_tile((xts[i % 2], sq, rs, None), n0, w, r, None, None, None, None)
                    for c in range(8):
                        self.stt(V, hf[:, c, :w], xt[:, c, :w], self.GF[l][:, c, r:r + 1], rs[:, :w], ALU.mult, ALU.mult, [xt, rs, self.GF[l]], [hf])
                        self.actf(hf[:, c, :w], hf[:, c, :w], AF.Identity, [hf, self.MOD[l]], [hf], bias=self.MOD[l][:, 24 + c, r:r + 1], scale=1.0)
                    self.cp(G, hT[:, :, n0:n0 + w], hf[:, :, :w], [hf], [hT])
                    for q in range(w // 128):
                        pR = ps[6]
                        for c in range(8):
                            self.mm(pR[:, 0:16], hf[:, c, q * 128:(q + 1) * 128], wr[:, c, :], c == 0, c == 7, [hf, wr], [pR])
                        self.actf(sc[:, :], pR[:, 0:16], AF.Sigmoid, [pR], [sc])
                        self.tt(V, sel[:, :], sc[:, :], rb[:, :], ALU.add, [sc, rb], [sel])
                        self.red(V, m1[:, :], v4(sel), ALU.max, [sel], [m1])
                        self.tt(V, v4(eq), v4(sel), b4(m1), ALU.is_equal, [sel, m1], [eq])
                        self.stt(V, sel2[:, :], eq[:, :], -1e9, sel[:, :], ALU.mult, ALU.add, [eq, sel], [sel2])
                        self.red(V, m2[:, :], v4(sel2), ALU.max, [sel2], [m2])
                        self.tt(V, gs[:, :], m1[:, :], m2[:, :], ALU.add, [m1, m2], [gs])
                        self.red(V, gmx[:, :], gs[:, :], ALU.max, [gs], [gmx])
                        self.ts(V, gsel[:, :], gs[:, :], gmx[:, 0:1], None, ALU.is_equal, None, [gs, gmx], [gsel])
                        self.tt(V, v4(cm), v4(sel), b4(m2), ALU.is_ge, [sel, m2], [cm])
                        self.tt(V, v4(cm), v4(cm), b4(gsel), ALU.mult, [cm, gsel], [cm])
                        self.tt(V, wts[:, :], sc[:, :], cm[:, :], ALU.mult, [sc, cm], [wts])
                        self.red(V, wsum[:, :], wts[:, :], ALU.add, [wts], [wsum])
                        self.recip(wsum[:, :], wsum[:, :], [wsum], [wsum])
                        self.ts(V, wts[:, :], wts[:, :], wsum[:, 0:1], None, ALU.mult, None, [wts, wsum], [wts])
                        pT = ps[7]
                        self.tr(pT[0:16, 0:128], wts[:, :], self.ident[:, :], [wts, self.ident], [pT])
                        self.cp(self.act, WTf[0:16, n0 + q * 128:n0 + (q + 1) * 128], pT[0:16, 0:128], [pT], [WTf])
                self.barrier()
            TG = 1152
            TW = 384
            yacc = t([128, 8, TG])
            wgb = [t([128, 8, 512], BF16) for _ in range(2)]
            wub = [t([128, 8, 512], BF16) for _ in range(2)]
            wdb = [t([128, 4, 1024], BF16) for _ in range(2)]
            wtb = t([128, TW])
            sg = [t([128, TW]) for _ in range(2)]
            actb = t([128, 4, TW], BF16)
            xt2 = t([128, 8, 128])
            def loadw(e):
                for (src, dstt) in ((self.weg[l, e], wgb[e % 2]), (self.weu[l, e], wub[e % 2]), (self.wed[l, e], wdb[e % 2])):
                    self.dma(dstt[:, :, :], src.rearrange("(k p) n -> p k n", p=128), [], [dstt], Q=self.pool)
            loadw(0)
            for g in range(NT // TG):
                for e in range(16):
                    gb, ub, db = wgb[e % 2], wub[e % 2], wdb[e % 2]
                    if not (g == NT // TG - 1 and e == 15):
                        loadw((e + 1) % 16)
                    for tt_ in range(TG // TW):
                        n0 = g * TG + tt_ * TW
                        self.mm(ps[4][:, :TW], self.SEL[0:16, e, :], WTf[0:16, n0:n0 + TW], True, True, [self.SEL, WTf], [ps[4]])
                        self.cp(self.act, wtb[:, :], ps[4][:, :TW], [ps[4]], [wtb])
                        for hc in range(4):
                            pg_, pu_ = ps[(hc % 2) * 2], ps[(hc % 2) * 2 + 1]
                            for k in range(8):
                                self.mm(pg_[:, :TW], gb[:, k, hc * 128:(hc + 1) * 128], hT[:, k, n0:n0 + TW], k == 0, k == 7, [gb, hT], [pg_])
                            for k in range(8):
                                self.mm(pu_[:, :TW], ub[:, k, hc * 128:(hc + 1) * 128], hT[:, k, n0:n0 + TW], k == 0, k == 7, [ub, hT], [pu_])
                            sg_ = sg[hc % 2]
                            self.actf(sg_[:, :], pg_[:, :TW], AF.Silu, [pg_], [sg_])
                            self.tt(V, sg_[:, :], sg_[:, :], pu_[:, :TW], ALU.mult, [sg_, pu_], [sg_])
                            self.tt(G, actb[:, hc, :], sg_[:, :], wtb[:, :], ALU.mult, [sg_, wtb], [actb])
                        for m in range(8):
                            pd = ps[4 + m % 4]
                            for hc in range(4):
                                self.mm(pd[:, :TW], db[:, hc, m * 128:(m + 1) * 128], actb[:, hc, :], hc == 0, hc == 3, [db, actb], [pd])
                            dst = yacc[:, m, tt_ * TW:(tt_ + 1) * TW]
                            if e == 0:
                                self.cp(self.act, dst, pd[:, :TW], [pd], [yacc])
                            else:
                                self.tt(V, dst, dst, pd[:, :TW], ALU.add, [yacc, pd], [yacc])
                for p_ in range(TG // 128):
                    n0 = g * TG + p_ * 128
                    tq = n0 % T
                    r = 2 if tq < TC else n0 // T
                    self.dma(xt2[:, :, :], self.XTv[:, :, n0:n0 + 128], self.dr("XT", n0, 128), [xt2])
                    for m in range(8):
                        self.stt(V, xt2[:, m, :], yacc[:, m, p_ * 128:(p_ + 1) * 128], self.MOD[l][:, 40 + m, r:r + 1], xt2[:, m, :], ALU.mult, ALU.add,
                                 [yacc, self.MOD[l], xt2], [xt2])
                    self.dma(self.XTv[:, :, n0:n0 + 128], xt2[:, :, :], [xt2], self.dr("XT", n0, 128))
            self.barrier()

    def final(self):
        V, G = self.dve, self.pool
        ps = self.ps
        with ExitStack() as es:
            def t(shape, dt=F32):
                return self.sb(shape, dt, es)
            xts = [t([128, 8, 128]) for _ in range(2)]
            sq = t([128, 8, 128])
            rs = t([128, 128])
            tmp = t([128, 8, 128])
            os_ = [t([128, 1024]) for _ in range(2)]
            i = 0
            for s in range(S):
                for j in range(2, NJ):
                    n0 = s * T + j * 128
                    xt = xts[i % 2]; o = os_[i % 2]; i += 1
                    self.dma(xt[:, :, :], self.XTv[:, :, n0:n0 + 128], self.dr("XT", n0, 128), [xt])
                    self.actf(sq[:, :, :], xt[:, :, :], AF.Square, [xt], [sq])
                    for c in range(8):
                        self.mm(ps[0][:, 0:128], self.ones[:, :], sq[:, c, :], c == 0, c == 7, [self.ones, sq], [ps[0]])
                    self.ts(V, rs[:, :], ps[0][:, 0:128], 1.0 / D, EPS, ALU.mult, ALU.add, [ps[0]], [rs])
                    self.actf(rs[:, :], rs[:, :], AF.Sqrt, [rs], [rs])
                    self.recip(rs[:, :], rs[:, :], [rs], [rs])
                    for c in range(8):
                        self.stt(V, tmp[:, c, :], xt[:, c, :], self.pfv(0, "norm_final", c), rs[:, :], ALU.mult, ALU.mult, [xt, rs, self.PFt[0]], [tmp])
                    for c in range(8):
                        pb = ps[1 + c // 4]
                        self.tr(pb[:, (c % 4) * 128:(c % 4 + 1) * 128], tmp[:, c, :], self.ident[:, :], [tmp, self.ident], [pb])
                    self.cp(self.act, o[:, 0:512], ps[1][:, :], [ps[1]], [o])
                    self.cp(V, o[:, 512:1024], ps[2][:, :], [ps[2]], [o])
                    self.dma(self.y[s, (j - 2) * 128:(j - 1) * 128, :], o[:, :], [o], [self.dd("y", s, j)])
            self.barrier()

    def rwkv_scan(self, l, gdn=False):
        V, G = self.dve, self.pool
        ps = self.ps
        nh = 4 if gdn else 8
        dv = 512 // nh
        DKQ, DMAT, DBC, DV_, DPC, DY, nm = ((self.GKQ, self.GMAT, self.GBC, self.GV, self.GPC, self.YB, 'G') if gdn else (self.RKQ, self.RMAT, self.RBC, self.RV, self.RPC, self.YA, 'R'))
        with ExitStack() as es:
            def t(shape, dt=F32):
                return self.sb(shape, dt, es)
            chains = [(s, d) for s in range(S) for d in range(2)]
            order = {0: list(range(NJ)), 1: [1, 0] + list(range(NJ - 1, 1, -1))}
            bufs = []
            for _ in chains:
                ld = [(t([128, 4, 256], BF16), t([128, nh, 512], BF16), t([128, 1024], BF16), t([128, 512], BF16), t([128, 8])) for _ in range(2)]
                bufs.append(dict(ld=ld, H=t([128, 4, dv]), Hz=t([128, nh, dv], BF16), Rn=t([128, nh, dv], BF16),
                                 Ubz=[t([128, nh, dv], BF16) for _ in range(2)], Vz=[t([128, 512], BF16) for _ in range(2)],
                                 Yt=t([128, 512])))
            for i in range(NJ):
                for ci, (s, d) in enumerate(chains):
                    j = order[d][i]
                    n0 = s * T + j * 128
                    b = bufs[ci]
                    KQ, MAT, BC, Vt, pc = b["ld"][i % 2]
                    H, Hz, Rn, Ubz, Vz, Yt = b["H"], b["Hz"], b["Rn"], b["Ubz"], b["Vz"], b["Yt"]
                    self.dma(KQ[:, :, :].rearrange("p m t -> p (m t)"), DKQ[s, d, j], [self.dd(nm + "KQ", s, d, j)], [KQ])
                    self.dma(MAT[:, :, :].rearrange("p h t -> p (h t)"), DMAT[s, d, j], [self.dd(nm + "MAT", s, d, j)], [MAT])
                    self.dma(BC[:, :], DBC[s, d, j], [self.dd(nm + "BC", s, d, j)], [BC])
                    self.dma(Vt[:, :], DV_[s, j], [self.dd(nm + "V", s, j)], [Vt])
                    self.dma(pc[:, :], DPC[s, d, j], [self.dd(nm + "PC", s, d, j)], [pc])
                    if i == 0:
                        self.memset(G, H[:, :, :], 0.0, [H])
                        self.memset(G, Hz[:, :, :], 0.0, [Hz])
                        self.memset(G, Rn[:, :, :], 0.0, [Rn])
                        for c in range(2):
                            self.memset(G, Ubz[c][:, :, :], 0.0, [Ubz[c]])
                            self.memset(G, Vz[c][:, :], 0.0, [Vz[c]])
                    for c in range(2):
                        self.cp(G, Vz[c][c * 64:c * 64 + 64, :], Vt[c * 64:c * 64 + 64, :], [Vt], [Vz[c]])
                    pA, pB = ps[2 * ci], ps[2 * ci + 1]
                    pAv = pA[:, :].rearrange("p (h v) -> p h v", v=dv)
                    pBv = pB[:, :].rearrange("p (h v) -> p h v", v=dv)
                    if not gdn:
                        pBe = pB[:, :].rearrange("p (m e v) -> p m e v", e=2, v=64)
                        Hze = Hz[:, :, :].rearrange("p (m e) v -> p m e v", e=2)
                    pcv = pc[:, :].rearrange("p (m c) -> p m c", c=2)
                    for c in ([0, 1] if d == 0 else [1, 0]):
                        cs = slice(c * 64, c * 64 + 64)
                        Ub = Ubz[c]
                        for h in range(nh):
                            m = h if gdn else h // 2
                            self.mm(pAv[:, h, :], KQ[:, m, 0:128], Hz[:, h, :], True, False, [KQ, Hz], [pA])
                            self.mm(pAv[:, h, :], MAT[:, h, 256:384], Vt[:, h * dv:(h + 1) * dv], False, True, [MAT, Vt], [pA])
                        self.ts(V, Rn[cs, :, :], pAv[cs, :, :], -1.0, None, ALU.mult, None, [pA], [Rn])
                        for h in range(nh):
                            self.mm(pBv[:, h, :], MAT[:, h, 0:128], Rn[:, h, :], True, True, [MAT, Rn], [pB])
                        self.cp(self.act, Ub[cs, :, :], pBv[cs, :, :], [pB], [Ub])
                        for h in range(nh):
                            m = h if gdn else h // 2
                            self.mm(pAv[:, h, :], KQ[:, m, 128:256], Hz[:, h, :], True, False, [KQ, Hz], [pA])
                            self.mm(pAv[:, h, :], MAT[:, h, 128:256], Ub[:, h, :], False, False, [MAT, Ub], [pA])
                            self.mm(pAv[:, h, :], MAT[:, h, 384:512], Vt[:, h * dv:(h + 1) * dv], False, True, [MAT, Vt], [pA])
                        self.cp(V, Yt[cs, :], pA[cs, :], [pA], [Yt])
                        for h in range(nh):
                            m = h if gdn else h // 2
                            self.mm(pBv[:, h, :], BC[:, m * 128:(m + 1) * 128], Ub[:, h, :], True, False, [BC, Ub], [pB])
                            self.mm(pBv[:, h, :], BC[:, 512 + m * 128:512 + (m + 1) * 128], Vz[c][:, h * dv:(h + 1) * dv], False, True, [BC, Vz[c]], [pB])
                        if gdn:
                            self.tt(V, H[:, :, :], H[:, :, :], pcv[:, :, c:c + 1].to_broadcast([128, 4, dv]), ALU.mult, [H, pc], [H])
                            self.tt(V, H[:, :, :], H[:, :, :], pBv[:, :, :], ALU.add, [H, pB], [H])
                            self.cp(self.act, Hz[:, :, :], H[:, :, :], [H], [Hz])
                        else:
                            for e in range(2):
                                rows = slice(e * 64, e * 64 + 64)
                                self.tt(V, H[rows, :, :], H[rows, :, :], pBe[rows, :, e, :], ALU.add, [H, pB], [H])
                            self.tt(V, H[:, :, :], H[:, :, :], pcv[:, :, c:c + 1].to_broadcast([128, 4, 64]), ALU.mult, [H, pc], [H])
                            for e in range(2):
                                rows = slice(e * 64, e * 64 + 64)
                                self.cp(self.act, Hze[rows, :, e, :], H[rows, :, :], [H], [Hz])
                    self.dma(DY[d, n0:n0 + 128, :], Yt[:, :], [Yt], [self.dd(nm + "Y", d, n0 // 128)])
            self.barrier()

    def build(self):
        try:
            self.build_()
        except StopBuild:
            self.es2 = None
            self.finish()

    def build_(self):
        self.consts()
        self.gdn_setup()
        self.conf_setup()
        self.phase0()
        if self.stop == "0":
            return self.finish()
        for l in range(self.nlayers):
            self.phaseA(l)
            if self.stop == f"A{l}":
                return self.finish()
            self.phaseB(l)
            if self.stop == f"B{l}":
                return self.finish()
            self.rwkv_prep(l)
            if self.stop == f"C{l}":
                return self.finish()
            self.rwkv_scan(l)
            if self.stop == f"D{l}":
                return self.finish()
            self.gdn_prep(l)
            if self.stop == f"E{l}":
                return self.finish()
            self.rwkv_scan(l, gdn=True)
            if self.stop == f"F{l}":
                return self.finish()
            self.conformer(l)
            if self.stop == f"G{l}":
                return self.finish()
            self.merge(l)
            if self.stop == f"H{l}":
                return self.finish()
            self.moe(l)
            if self.stop == f"I{l}":
                return self.finish()
        self.final()
        self.finish()
```

```python
import threading
import numpy as np
from contextlib import ExitStack
import concourse.bass as bass
import concourse.mybir as mybir
from concourse.bass_utils import run_bass_kernel_spmd

F32 = mybir.dt.float32
BF16 = mybir.dt.bfloat16
ALU = mybir.AluOpType
AF = mybir.ActivationFunctionType
AX = mybir.AxisListType

D = 1024
S = 2
TC = 256
TL = 2048
T = TC + TL
NT = S * T
L = 2
NIN = 8080
NDS = 24


class Dep:
    __slots__ = ("w", "r")

    def __init__(self):
        self.w = None
        self.r = {}


class Tl:
    def __init__(self, t):
        self.t = t
        self.d = Dep()

    def __getitem__(self, k):
        return self.t[k]


class Eng:
    def __init__(self, name, be, sem):
        self.key = name
        self.be = be
        self.sem = sem
        self.n = 0
        self.waited = {}


def _d(x):
    return x.d if hasattr(x, "d") else x


class KB:
    def __init__(self, dbg=()):
        self.nc = nc = bass.Bass("TRN2", target_bir_lowering=False)
        self.es = ExitStack()
        self.dbg = set(dbg)
        e = self.es.enter_context
        self.pe = Eng("pe", nc.tensor, e(nc.semaphore("s_pe")))
        self.act = Eng("act", nc.scalar, e(nc.semaphore("s_act")))
        self.dve = Eng("dve", nc.vector, e(nc.semaphore("s_dve")))
        self.pool = Eng("pool", nc.gpsimd, e(nc.semaphore("s_pool")))
        self.sp = Eng("sp", nc.sync, e(nc.semaphore("s_sp")))
        self.engs = [self.pe, self.act, self.dve, self.pool, self.sp]
        self.dsem = [e(nc.semaphore(f"s_d{i}")) for i in range(NDS)]
        self.dcnt = [0] * NDS
        self.drr = 0
        self.ddeps = {}
        self.ntile = 0
        self.yielders = {}

    def sb(self, shape, dt=F32, es=None):
        self.ntile += 1
        t = (es or self.es).enter_context(self.nc.sbuf_tensor(f"t{self.ntile}", list(shape), dt))
        return Tl(t)

    def psum(self, shape, dt=F32, es=None):
        self.ntile += 1
        t = (es or self.es).enter_context(self.nc.psum_tensor(f"p{self.ntile}", list(shape), dt))
        return Tl(t)

    def dram(self, name, shape, dt=F32, kind=None):
        if kind is None:
            kind = "ExternalOutput" if name in self.dbg else "Internal"
        return self.nc.dram_tensor(name, list(shape), dt, kind=kind).ap()

    def dd(self, *key):
        d = self.ddeps.get(key)
        if d is None:
            d = self.ddeps[key] = Dep()
        return d

    def dr(self, name, n0, w):
        return [self.dd(name, i) for i in range(n0 // 128, (n0 + w + 127) // 128)]

    def _sync(self, E, R, W):
        need = {}

        def upd(tok):
            k, sem, val = tok
            if k not in need or need[k][1] < val:
                need[k] = (sem, val)

        for d in R:
            d = _d(d)
            if d.w:
                upd(d.w)
        for d in W:
            d = _d(d)
            if d.w:
                upd(d.w)
            for k, (sem, val) in d.r.items():
                if k != E.key:
                    upd((k, sem, val))
        for k, (sem, val) in need.items():
            if k == E.key and E is self.pe:
                continue
            if E.waited.get(k, 0) < val:
                E.be.wait_ge(sem, val)
                E.waited[k] = val

    def _mark(self, tok, R, W):
        k, sem, val = tok
        for d in R:
            _d(d).r[k] = (sem, val)
        for d in W:
            d = _d(d)
            d.w = tok
            d.r = {}

    def op(self, E, fn, R, W):
        self._sync(E, R, W)
        ins = fn()
        E.n += 1
        ins.then_inc(E.sem, 1)
        self._mark((E.key, E.sem, E.n), R, W)
        self._yield()

    def dma(self, out, in_, R, W, Q=None, **kw):
        Q = Q or self.sp
        self._sync(Q, R, W)
        s = self.drr
        self.drr = (s + 1) % NDS
        sem = self.dsem[s]
        k = ("d", s)
        if self.dcnt[s] > 0 and Q.waited.get(k, 0) < 16 * self.dcnt[s]:
            Q.be.wait_ge(sem, 16 * self.dcnt[s])
            Q.waited[k] = 16 * self.dcnt[s]
        Q.be.dma_start(out=out, in_=in_, **kw).then_inc(sem, 16)
        self.dcnt[s] += 1
        self._mark((k, sem, 16 * self.dcnt[s]), R, W)
        self._yield()

    def _yield(self):
        if self.yielders:
            y = self.yielders.get(threading.get_ident())
            if y:
                y()

    def run_interleaved(self, fns):
        n = len(fns)
        state = {"turn": 0, "done": [False] * n, "exc": None}
        cv = threading.Condition()

        def advance(i):
            for k in range(1, n + 1):
                nx = (i + k) % n
                if not state["done"][nx]:
                    state["turn"] = nx
                    break
            else:
                state["turn"] = -1
            cv.notify_all()

        def yielder(i):
            def y():
                with cv:
                    advance(i)
                    while state["turn"] != i:
                        cv.wait()
            return y

        def worker(i):
            with cv:
                while state["turn"] != i:
                    cv.wait()
            self.yielders[threading.get_ident()] = yielder(i)
            try:
                fns[i]()
            except BaseException as e:
                state["exc"] = e
            finally:
                self.yielders.pop(threading.get_ident(), None)
                with cv:
                    state["done"][i] = True
                    advance(i)

        ths = [threading.Thread(target=worker, args=(i,)) for i in range(n)]
        for th in ths:
            th.start()
        for th in ths:
            th.join()
        if state["exc"] is not None:
            raise state["exc"]

    def barrier(self):
        for E in self.engs:
            for E2 in self.engs:
                if E2 is not E and E2.n > 0 and E.waited.get(E2.key, 0) < E2.n:
                    E.be.wait_ge(E2.sem, E2.n)
                    E.waited[E2.key] = E2.n
            for s in range(NDS):
                k = ("d", s)
                if self.dcnt[s] > 0 and E.waited.get(k, 0) < 16 * self.dcnt[s]:
                    E.be.wait_ge(self.dsem[s], 16 * self.dcnt[s])
                    E.waited[k] = 16 * self.dcnt[s]

    def mm(self, out, lhsT, rhs, start, stop, R, W):
        self.op(self.pe, lambda: self.nc.tensor.matmul(out, lhsT=lhsT, rhs=rhs, start=start, stop=stop), R, W)

    def tr(self, out, in_, ident, R, W):
        self.op(self.pe, lambda: self.nc.tensor.transpose(out, in_, ident), R, W)

    def actf(self, out, in_, func, R, W, bias=None, scale=None):
        kw = {}
        if bias is not None:
            kw["bias"] = bias
        if scale is not None:
            kw["scale"] = scale
        self.op(self.act, lambda: self.nc.scalar.activation(out=out, in_=in_, func=func, **kw), R, W)

    def ts(self, E, out, in0, s1, s2, op0, op1, R, W):
        if op1 is None:
            self.op(E, lambda: E.be.tensor_scalar(out=out, in0=in0, scalar1=s1, scalar2=None, op0=op0), R, W)
        else:
            self.op(E, lambda: E.be.tensor_scalar(out=out, in0=in0, scalar1=s1, scalar2=s2, op0=op0, op1=op1), R, W)

    def tt(self, E, out, in0, in1, op, R, W):
        self.op(E, lambda: E.be.tensor_tensor(out=out, in0=in0, in1=in1, op=op), R, W)

    def stt(self, E, out, in0, scalar, in1, op0, op1, R, W):
        self.op(E, lambda: E.be.scalar_tensor_tensor(out=out, in0=in0, scalar=scalar, in1=in1, op0=op0, op1=op1), R, W)

    def cp(self, E, out, in_, R, W):
        if E is self.act:
            self.op(E, lambda: self.nc.scalar.copy(out=out, in_=in_), R, W)
        else:
            self.op(E, lambda: E.be.tensor_copy(out=out, in_=in_), R, W)

    def recip(self, out, in_, R, W):
        self.op(self.dve, lambda: self.nc.vector.reciprocal(out=out, in_=in_), R, W)

    def memset(self, E, ap, v, W):
        self.op(E, lambda: E.be.memset(ap, v), [], W)


class PF:
    def __init__(self):
        self.cols = {}
        self.n = 0

    def add(self, name, nch):
        self.cols[name] = (self.n, nch)
        self.n += nch
        return self.cols[name][0]


def pf_layout():
    pf = PF()
    for nm, nch in [("norm_mix", 8), ("norm_ffn", 8), ("b_ada", 48), ("mu0", 15), ("mu1", 15),
                    ("w0_0", 4), ("w0_1", 4), ("a0_0", 4), ("a0_1", 4), ("kk", 4), ("ka", 4), ("rk", 4),
                    ("ln_g", 4), ("ln_b", 4), ("gconv", 60), ("gnorm", 1), ("alog", 1), ("dtb", 1),
                    ("cdw", 124), ("cdwb", 4), ("clng", 4), ("clnb", 4), ("norm_final", 8)]:
        pf.add(nm, nch)
    return pf


def _fm(v, nch):
    return np.ascontiguousarray(np.asarray(v, np.float32).reshape(nch, 128).T)


def pack_pf(inp, l):
    pf = pf_layout()
    out = np.zeros((128, pf.n), np.float32)

    def put(nm, arr):
        o, n = pf.cols[nm]
        out[:, o:o + n] = arr

    put("norm_mix", _fm(inp["norm_mix"][l], 8))
    put("norm_ffn", _fm(inp["norm_ffn"][l], 8))
    put("b_ada", _fm(inp["b_ada"][l], 48))
    put("mu0", _fm(inp["rwkv_mu"][l, 0], 15))
    put("mu1", _fm(inp["rwkv_mu"][l, 1], 15))
    for d in range(2):
        put(f"w0_{d}", _fm(inp["rwkv_w0"][l, d], 4))
        put(f"a0_{d}", _fm(inp["rwkv_a0"][l, d], 4))
    put("kk", _fm(inp["rwkv_kk"][l], 4))
    put("ka", _fm(inp["rwkv_ka"][l], 4))
    put("rk", _fm(inp["rwkv_rk"][l].reshape(-1), 4))
    put("ln_g", _fm(inp["rwkv_ln_g"][l], 4))
    put("ln_b", _fm(inp["rwkv_ln_b"][l], 4))
    gc = np.concatenate([_fm(inp["gdn_conv"][l, k], 12) for k in range(5)], axis=1)
    put("gconv", gc)
    put("gnorm", _fm(inp["gdn_norm"][l], 1))
    al = np.zeros((128, 1), np.float32); al[0:8, 0] = np.asarray(inp["gdn_A_log"][l]).reshape(-1)
    db = np.zeros((128, 1), np.float32); db[0:8, 0] = np.asarray(inp["gdn_dt_bias"][l]).reshape(-1)
    put("alog", al)
    put("dtb", db)
    cd = np.concatenate([_fm(inp["conf_dw"][l, k], 4) for k in range(31)], axis=1)
    put("cdw", cd)
    put("cdwb", _fm(inp["conf_dw_b"][l], 4))
    put("clng", _fm(inp["conf_ln_g"][l], 4))
    put("clnb", _fm(inp["conf_ln_b"][l], 4))
    put("norm_final", _fm(inp["norm_final"], 8))
    return out


EPS = 1e-6


class Prog(KB):
    def __init__(self, dbg=(), stop=None, nlayers=L):
        super().__init__(dbg)
        self.stop = stop
        self.nlayers = nlayers
        nc = self.nc
        self.pf = pf_layout()

        def inp(name, shape):
            return nc.dram_tensor(name, list(shape), F32, kind="ExternalInput").ap()

        self.x = inp("x", [S, TL, D])
        self.ctx = inp("ctx", [S, TC, D])
        self.cvecT = inp("cvecT", [D, 3])
        self.pfp = inp("pfp", [L, 128, self.pf.n])
        self.w_ada = inp("w_ada", [L, D, 6 * D])
        self.w_in = inp("w_in", [L, D, NIN])
        self.rw2 = inp("rwkv_w2", [L, 2, 64, 512])
        self.ra2 = inp("rwkv_a2", [L, 2, 64, 512])
        self.rg2 = inp("rwkv_g2", [L, 128, 512])
        self.w_branch = inp("w_branch", [L, 3, 512, D])
        self.w_out = inp("w_out", [L, D, D])
        self.w_router = inp("w_router", [D, 16])
        self.rbias = inp("rbias", [128, 16])
        self.weg = inp("w_e_gate", [L, 16, D, 512])
        self.weu = inp("w_e_up", [L, 16, D, 512])
        self.wed = inp("w_e_down", [L, 16, 512, D])
        self.y = nc.dram_tensor("y", [S, TL, D], F32, kind="ExternalOutput").ap()
        self.XT = self.dram("XT", [D, NT])
        self.XTv = self.XT.rearrange("(c p) n -> p c n", p=128)
        self.PT = self.dram("PT", [NIN, NT])
        self.ps = [self.psum([128, 512], F32) for _ in range(8)]
        self.ident = self.sb([128, 128], F32)
        self.identb = self.sb([128, 128], BF16)
        self.ones = self.sb([128, 128], F32)
        self.PFt = [self.sb([128, self.pf.n], F32) for _ in range(L)]
        self.scT = self.sb([128, 8, 3], F32)
        self.MOD = [self.sb([128, 48, 3], F32) for _ in range(L)]
        self.GM = [self.sb([128, 8, 3], F32) for _ in range(L)]
        self.GF = [self.sb([128, 8, 3], F32) for _ in range(L)]

    def pfv(self, l, name, c=None, n=1):
        o, nch = self.pf.cols[name]
        if c is None:
            return self.PFt[l][:, o:o + nch]
        return self.PFt[l][:, o + c:o + c + n]

    def consts(self):
        nc = self.nc
        P = self.pool
        self.memset(P, self.ident[:, :], 0.0, [self.ident])
        self.op(P, lambda: nc.gpsimd.affine_select(out=self.ident[:, :], in_=self.ident[:, :], pattern=[[-1, 128]],
                                                   compare_op=ALU.not_equal, fill=1.0, base=0, channel_multiplier=1),
                [self.ident], [self.ident])
        self.cp(P, self.identb[:, :], self.ident[:, :], [self.ident], [self.identb])
        self.memset(P, self.ones[:, :], 1.0, [self.ones])
        for l in range(L):
            self.dma(self.PFt[l][:, :], self.pfp[l], [], [self.PFt[l]])
        cv = self.sb([128, 8, 3], F32)
        self.dma(cv[:, :, :], self.cvecT.rearrange("(c p) r -> p c r", p=128), [], [cv])
        self.actf(self.scT[:, :, :], cv[:, :, :], AF.Silu, [cv], [self.scT])

    def phase0(self):
        with ExitStack() as es:
            xin = [self.sb([128, 1024], F32, es) for _ in range(2)]
            xo = [self.sb([128, 8, 128], F32, es) for _ in range(2)]
            i = 0
            for s in range(S):
                for j in range(T // 128):
                    n0 = s * T + j * 128
                    src = self.ctx[s, j * 128:(j + 1) * 128, :] if j < 2 else self.x[s, (j - 2) * 128:(j - 1) * 128, :]
                    a = xin[i % 2]
                    o = xo[i % 2]
                    self.dma(a[:, :], src, [], [a])
                    for c in range(8):
                        pb = self.ps[(i % 2) * 2 + c // 4]
                        self.tr(pb[:, (c % 4) * 128:(c % 4 + 1) * 128], a[:, c * 128:(c + 1) * 128], self.ident[:, :],
                                [a, self.ident], [pb])
                    for hf in range(2):
                        pb = self.ps[(i % 2) * 2 + hf]
                        self.cp(self.act if hf == 0 else self.dve, o[:, hf * 4:(hf + 1) * 4, :],
                                pb[:, :].rearrange("p (c t) -> p c t", c=4), [pb], [o])
                    self.dma(self.XTv[:, :, n0:n0 + 128], o[:, :, :], [o], self.dr("XT", n0, 128))
                    i += 1
            self.barrier()

    def phaseA(self, l):
        with ExitStack() as es:
            wa = [self.sb([128, 8, 768], F32, es) for _ in range(2)]
            pm = self.ps[0]
            wav = self.w_ada[l].rearrange("(k p) n -> p k n", p=128)
            for mg in range(8):
                w = wa[mg % 2]
                for q in range(4):
                    self.dma(w[:, q * 2:(q + 1) * 2, :], wav[:, q * 2:(q + 1) * 2, mg * 768:(mg + 1) * 768], [], [w])
                for m in range(6):
                    mm_ = mg * 6 + m
                    for k in range(8):
                        self.mm(pm[:, mm_ * 3:(mm_ + 1) * 3], w[:, k, m * 128:(m + 1) * 128], self.scT[:, k, :], k == 0, k == 7,
                                [w, self.scT], [pm])
            mod = self.MOD[l]
            self.tt(self.dve, mod[:, :, :], pm[:, 0:144].rearrange("p (m r) -> p m r", r=3),
                    self.pfv(l, "b_ada").unsqueeze(2).to_broadcast([128, 48, 3]), ALU.add, [pm, self.PFt[l]], [mod])
            for (G, mi, nm) in ((self.GM[l], 1, "norm_mix"), (self.GF[l], 4, "norm_ffn")):
                self.ts(self.dve, G[:, :, :], mod[:, mi * 8:(mi + 1) * 8, :], 1.0, None, ALU.add, None, [mod], [G])
                self.dump(f"G1{l}{mi}", G, G[:, :, :], [128, 8, 3])
                self.tt(self.dve, G[:, :, :], G[:, :, :], self.pfv(l, nm).unsqueeze(2).to_broadcast([128, 8, 3]), ALU.mult,
                        [G, self.PFt[l]], [G])
            self.dump(f"MOD{l}", mod, mod[:, :, :], [128, 48, 3])
            self.dump(f"GM{l}", self.GM[l], self.GM[l][:, :, :], [128, 8, 3])
            self.barrier()

    def tiles(self):
        out = []
        for s in range(S):
            out.append((s * T, TC, 2))
            for j in range(TL // 512):
                out.append((s * T + TC + j * 512, 512, s))
        return out

    def modulate_tile(self, es_bufs, n0, w, r, G, shift_col, mod, out_fn):
        xt, sq, rs, tmp = es_bufs
        psS = self.ps[7]
        self.dma(xt[:, :, :w], self.XTv[:, :, n0:n0 + w], self.dr("XT", n0, w), [xt])
        self.actf(sq[:, :, :w], xt[:, :, :w], AF.Square, [xt], [sq])
        for c in range(8):
            self.mm(psS[:, :w], self.ones[:, :], sq[:, c, :w], c == 0, c == 7, [self.ones, sq], [psS])
        self.ts(self.dve, rs[:, :w], psS[:, :w], 1.0 / D, EPS, ALU.mult, ALU.add, [psS], [rs])
        self.actf(rs[:, :w], rs[:, :w], AF.Sqrt, [rs], [rs])
        self.recip(rs[:, :w], rs[:, :w], [rs], [rs])
        return xt, rs

    def phaseB(self, l):
        with ExitStack() as es:
            hT = self.sb([128, 8, NT], BF16, es)
            with ExitStack() as es1:
                xts = [self.sb([128, 8, 512], F32, es1) for _ in range(2)]
                sq = self.sb([128, 8, 512], F32, es1)
                rs = self.sb([128, 512], F32, es1)
                tmps = [self.sb([128, 512], F32, es1) for _ in range(2)]
                for i, (n0, w, r) in enumerate(self.tiles()):
                    xt, _ = self.modulate_tile((xts[i % 2], sq, rs, None), n0, w, r, None, None, None, None)
                    for c in range(8):
                        tmp = tmps[c % 2]
                        self.stt(self.dve, tmp[:, :w], xt[:, c, :w], self.GM[l][:, c, r:r + 1], rs[:, :w], ALU.mult, ALU.mult,
                                 [xt, rs, self.GM[l]], [tmp])
                        self.actf(hT[:, c, n0:n0 + w], tmp[:, :w], AF.Identity, [tmp, self.MOD[l]], [hT],
                                  bias=self.MOD[l][:, c, r:r + 1], scale=1.0)
                if "HT" in self.dbg:
                    hd = self.dram("HT", [128, 8, NT], BF16)
                    self.dma(hd, hT[:, :, :], [hT], [self.dd("HTd")])
                self.barrier()
            wbf = [self.sb([128, 8, 1024], BF16, es) for _ in range(2)]
            ost = [self.sb([128, 512], F32, es) for _ in range(4)]
            no = 0
            def loadw(g):
                c0 = g * 1024
                cw = min(1024, NIN - c0)
                self.dma(wbf[g % 2][:, :, :cw], self.w_in[l][:, c0:c0 + cw].rearrange("(k p) n -> p k n", p=128), [], [wbf[g % 2]], Q=self.pool)
            loadw(0)
            for g in range(8):
                c0 = g * 1024
                cw = min(1024, NIN - c0)
                wb = wbf[g % 2]
                if g < 7:
                    loadw(g + 1)
                nm = (cw + 127) // 128
                for tt_ in range(NT // 512):
                    n0 = tt_ * 512
                    for m in range(nm):
                        mw = min(128, cw - m * 128)
                        pb = self.ps[no % 6]
                        for k in range(8):
                            self.mm(pb[:mw, :], wb[:, k, m * 128:m * 128 + mw], hT[:, k, n0:n0 + 512], k == 0, k == 7,
                                    [wb, hT], [pb])
                        o = ost[no % 4]
                        self.cp(self.act if no % 2 == 0 else self.dve, o[:mw, :], pb[:mw, :], [pb], [o])
                        self.dma(self.PT[c0 + m * 128:c0 + m * 128 + mw, n0:n0 + 512], o[:mw, :], [o],
                                 [self.dd("PT", (c0 + m * 128) // 128, j) for j in range(n0 // 128, n0 // 128 + 4)])
                        no += 1
            self.barrier()

    def dump(self, name, tile, ap, shape, dt=F32):
        if name in self.dbg:
            d = self.dram(name, shape, dt)
            self.dma(d, ap, [tile], [self.dd(name)])

    def finish(self):
        self.barrier()

    def build(self):
        self.consts()
        self.phase0()
        if self.stop == "0":
            return self.finish()
        for l in range(self.nlayers):
            self.phaseA(l)
            if self.stop == f"A{l}":
                return self.finish()
            self.phaseB(l)
            if self.stop == f"B{l}":
                return self.finish()
        self.finish()


def make_in_maps(inp):
    ncores = 8
    pfp = np.stack([pack_pf(inp, l) for l in range(L)])
    rbias = np.ascontiguousarray(np.broadcast_to(np.asarray(inp["router_bias"], np.float32)[None, :], (128, 16)))
    maps = []
    for i in range(ncores):
        cv = np.stack([inp["c"][2 * i], inp["c"][2 * i + 1], inp["c_ctx"]], axis=1).astype(np.float32)
        m = {
            "x": np.ascontiguousarray(inp["x"][2 * i:2 * i + 2]),
            "ctx": np.ascontiguousarray(inp["ctx"][2 * i:2 * i + 2]),
            "cvecT": np.ascontiguousarray(cv),
            "pfp": pfp, "rbias": rbias,
        }
        for k in ("w_ada", "w_in", "rwkv_w2", "rwkv_a2", "rwkv_g2", "w_branch", "w_out", "w_router",
                  "w_e_gate", "w_e_up", "w_e_down"):
            m[k] = np.ascontiguousarray(inp[k], dtype=np.float32)
        maps.append(m)
    return maps


def kernel(**inputs):
    inp = {k: np.asarray(v) for k, v in inputs.items()}
    prog = Prog2()
    prog.build()
    maps = make_in_maps(inp)
    res = run_bass_kernel_spmd(prog.nc, maps, core_ids=list(range(8)))
    return np.concatenate([r["y"] for r in res.results], axis=0).astype(np.float32)


CDEC = 0.6065306597126334
NJ = T // 128


def seg_bounds(j):
    return (j == 0 or j == 2), (j == 1 or j == NJ - 1)


class StopBuild(Exception):
    pass


class Prog2(Prog):
    cut = None

    def ck(self, n):
        if self.cut == n:
            raise StopBuild()

    def __init__(self, **kw):
        super().__init__(**kw)
        self.PTv = self.PT[0:8064, :].rearrange("(c p) n -> p c n", p=128)
        self.RKQ = self.dram("RKQ", [S, 2, NJ, 128, 4 * 256], BF16)
        self.RMAT = self.dram("RMAT", [S, 2, NJ, 128, 8 * 512], BF16)
        self.RBC = self.dram("RBC", [S, 2, NJ, 128, 1024], BF16)
        self.RV = self.dram("RV", [S, NJ, 128, 512], BF16)
        self.RPC = self.dram("RPC", [S, 2, NJ, 128, 8], F32)
        self.GAs = self.dram("GAs", [512, NT])
        self.BON = self.dram("BON", [512, NT])
        self.YA = self.dram("YA", [2, NT, 512])
        self.bones = self.sb([128, 128], F32)
        self.MU = self.sb([128, 256], F32)
        self.ML = self.sb([128, 256], F32)
        self.RM = self.sb([128, 128], F32)

    def consts(self):
        super().consts()
        nc = self.nc
        P = self.pool
        self.memset(P, self.bones[:, :], 0.0, [self.bones])
        self.memset(P, self.bones[0:64, 0:64], 1.0, [self.bones])
        self.memset(P, self.bones[64:128, 64:128], 1.0, [self.bones])
        self.memset(P, self.RM[:, :], 1.0, [self.RM])
        self.memset(P, self.RM[:, 0:1], 0.0, [self.RM])
        self.memset(P, self.RM[:, 64:65], 0.0, [self.RM])
        for (Mt, off, cmp_, sg) in ((self.MU, 0, ALU.is_gt, 1), (self.MU, 128, ALU.is_ge, 1), (self.ML, 0, ALU.is_gt, -1), (self.ML, 128, ALU.is_ge, -1)):
            sl = Mt[:, off:off + 128]
            self.memset(P, sl, 1.0, [Mt])
            self.op(P, lambda sl=sl, cmp_=cmp_, sg=sg: nc.gpsimd.affine_select(out=sl, in_=sl, pattern=[[sg, 128]], compare_op=cmp_, fill=0.0,
                                                                              base=0, channel_multiplier=-sg), [Mt], [Mt])
            self.memset(P, Mt[0:64, off + 64:off + 128], 0.0, [Mt])
            self.memset(P, Mt[64:128, off:off + 64], 0.0, [Mt])

    def load_halo(self, dst, c0, nch, s, j, hw):
        n0 = s * T + j * 128
        lb, rb = seg_bounds(j)
        lo = 0 if lb else hw
        hi = 0 if rb else hw
        if lb:
            self.memset(self.pool, dst[:, :, 0:hw], 0.0, [dst])
        if rb:
            self.memset(self.pool, dst[:, :, 128 + hw:128 + 2 * hw], 0.0, [dst])
        deps = [self.dd("PT", c, i) for c in range(c0, c0 + nch) for i in range((n0 - lo) // 128, (n0 + 128 + hi - 1) // 128 + 1)]
        self.dma(dst[:, :, hw - lo:hw + 128 + hi], self.PTv[:, c0:c0 + nch, n0 - lo:n0 + 128 + hi], deps, [dst])

    def inverse(self, X1, NN, MAT, h, Wt, At, Bt, pA, pB_, pC):
        V, G = self.dve, self.pool
        W, A, B = Wt[0], At[0], Bt[0]
        self.tt(G, W[:, :], self.identb[:, :], X1[:, 0:128], ALU.subtract, [self.identb, X1], [W])
        self.mm(pA[:, 0:128], X1[:, 0:128], NN[:, :], True, True, [X1, NN], [pA])
        self.mm(pB_[:, 0:128], NN[:, :], X1[:, 0:128], True, True, [X1, NN], [pB_])
        self.cp(self.act, A[:, :], pA[:, 0:128], [pA], [A])
        self.cp(self.act, B[:, :], pB_[:, 0:128], [pB_], [B])
        for it in range(5):
            W2, A2, B2 = Wt[(it + 1) % 2], At[(it + 1) % 2], Bt[(it + 1) % 2]
            self.mm(pC[:, 0:128], A[:, :], W[:, :], True, True, [A, W], [pC])
            if it < 4:
                self.mm(pA[:, 0:128], B[:, :], A[:, :], True, True, [A, B], [pA])
                self.mm(pB_[:, 0:128], A[:, :], B[:, :], True, True, [A, B], [pB_])
            dstW = W2[:, :] if it < 4 else MAT[:, h, 0:128]
            self.tt(V, dstW, W[:, :], pC[:, 0:128], ALU.add, [W, pC], [W2 if it < 4 else MAT])
            if it < 4:
                self.cp(self.act, A2[:, :], pA[:, 0:128], [pA], [A2])
                self.cp(self.act, B2[:, :], pB_[:, 0:128], [pB_], [B2])
            W, A, B = W2, A2, B2

    def inverse_batch(self, X1a, NNa, MAT, h0, banks, Wt, At, Bt):
        V, G = self.dve, self.pool
        psW, psA, psB = banks
        hs = slice(h0, h0 + 4)

        def reg(p, i):
            return p[:, i * 128:(i + 1) * 128]

        def bv(p):
            return p[:, :].rearrange("p (h t) -> p h t", h=4)
        W, A, B = Wt[0], At[0], Bt[0]
        self.tt(G, W[:, :, :], self.identb[:, :].unsqueeze(1).to_broadcast([128, 4, 128]), X1a[:, hs, 0:128], ALU.subtract, [self.identb, X1a], [W])
        for i in range(4):
            self.mm(reg(psA, i), X1a[:, h0 + i, 0:128], NNa[:, h0 + i, :], True, True, [X1a, NNa], [psA])
        for i in range(4):
            self.mm(reg(psB, i), NNa[:, h0 + i, :], X1a[:, h0 + i, 0:128], True, True, [X1a, NNa], [psB])
        self.cp(self.act, A[:, :, :], bv(psA), [psA], [A])
        self.cp(V, B[:, :, :], bv(psB), [psB], [B])
        for it in range(5):
            W2, A2, B2 = Wt[(it + 1) % 2], At[(it + 1) % 2], Bt[(it + 1) % 2]
            for i in range(4):
                self.mm(reg(psW, i), A[:, i, :], W[:, i, :], True, True, [A, W], [psW])
            if it < 4:
                for i in range(4):
                    self.mm(reg(psA, i), B[:, i, :], A[:, i, :], True, True, [A, B], [psA])
                for i in range(4):
                    self.mm(reg(psB, i), A[:, i, :], B[:, i, :], True, True, [A, B], [psB])
                self.tt(V, W2[:, :, :], W[:, :, :], bv(psW), ALU.add, [W, psW], [W2])
                self.cp(self.act, A2[:, :, :], bv(psA), [psA], [A2])
                self.cp(V, B2[:, :, :], bv(psB), [psB], [B2])
            else:
                self.tt(V, MAT[:, hs, 0:128], W[:, :, :], bv(psW), ALU.add, [W, psW], [MAT])
            W, A, B = W2, A2, B2

    def rwkv_prep(self, l):
        nc = self.nc
        V, G = self.dve, self.pool
        with ExitStack() as es:
            def t(shape, dt=F32):
                return self.sb(shape, dt, es)
            wtmp = t([128, 512])
            w2b, a2b, g2b = t([128, 512], BF16), t([128, 512], BF16), t([128, 512], BF16)
            for (src, dstb) in ((self.rw2[l].rearrange("d r c -> (d r) c"), w2b), (self.ra2[l].rearrange("d r c -> (d r) c"), a2b), (self.rg2[l], g2b)):
                self.dma(wtmp[:, :], src, [], [wtmp])
                self.cp(V, dstb[:, :], wtmp[:, :], [wtmp], [dstb])
            PFl = self.PFt[l]
            c0t = t([128, 15])
            self.tt(V, c0t[:, :], self.pfv(l, "mu0"), self.pfv(l, "mu1"), ALU.add, [PFl], [c0t])
            self.ts(V, c0t[:, :], c0t[:, :], -1.0, 1.0, ALU.mult, ALU.add, [c0t], [c0t])
            omka = t([128, 4])
            self.ts(V, omka[:, :], self.pfv(l, "ka"), -1.0, 1.0, ALU.mult, ALU.add, [PFl], [omka])

            def bc(ap, n):
                return ap.unsqueeze(2).to_broadcast([128, n, 128])

            def stream(s):
                pa = t([128, 15, 130])
                sh = t([128, 15, 128])
                twb, xab, sgb = t([128, 128], BF16), t([128, 128], BF16), t([128, 128], BF16)
                SW = [t([128, 4, 128]) for _ in range(2)]
                AA = [t([128, 4, 128]) for _ in range(2)]
                ga = t([128, 4, 128])
                kx, kk, tq, bon = t([128, 4, 128]), t([128, 4, 128]), t([128, 4, 128]), t([128, 4, 128])
                CS, EX, tmpa, tmpb = (t([128, 4, 128]) for _ in range(4))
                e1, e2, e3 = (t([128, 4, 128]) for _ in range(3))
                pc = t([128, 8])
                KQ = t([128, 4, 256], BF16)
                BTb, CTb = t([128, 4, 128], BF16), t([128, 4, 128], BF16)
                vb = t([128, 4, 128], BF16)
                BC = t([128, 1024], BF16)
                Vt = t([128, 512], BF16)
                MAT = t([128, 8, 512], BF16)
                X1a = t([128, 8, 256], BF16)
                NNa = t([128, 8, 128], BF16)
                BTz, CTz = t([128, 8, 128], BF16), t([128, 8, 128], BF16)
                self.memset(G, BTz[:, :, :], 0.0, [BTz])
                self.memset(G, CTz[:, :, :], 0.0, [CTz])
                Wt = [t([128, 4, 128], BF16) for _ in range(2)]
                At = [t([128, 4, 128], BF16) for _ in range(2)]
                Bt = [t([128, 4, 128], BF16) for _ in range(2)]
                Wt2 = [t([128, 4, 128], BF16) for _ in range(2)]
                At2 = [t([128, 4, 128], BF16) for _ in range(2)]
                Bt2 = [t([128, 4, 128], BF16) for _ in range(2)]
                B = self.ps[4 * s:4 * s + 4]
                for j in range(NJ):
                    n0 = s * T + j * 128
                    self.ck(1000 + s * NJ + j)
                    self.load_halo(pa, 0, 15, s, j, 1)
                    self.ck(1)
                    self.tt(V, sh[:, :, :], pa[:, :, 1:129], bc(c0t[:, :], 15), ALU.mult, [pa, c0t], [sh])
                    for c in range(15):
                        self.stt(V, sh[:, c, :], pa[:, c, 0:128], self.pfv(l, "mu0", c), sh[:, c, :], ALU.mult, ALU.add, [pa, PFl, sh], [sh])
                        self.stt(V, sh[:, c, :], pa[:, c, 2:130], self.pfv(l, "mu1", c), sh[:, c, :], ALU.mult, ALU.add, [pa, PFl, sh], [sh])
                    self.ck(2)
                    r_, k_, v_ = sh[:, 0:4, :], sh[:, 4:8, :], sh[:, 8:12, :]
                    self.actf(twb[:, :], sh[:, 12, :], AF.Tanh, [sh], [twb])
                    self.cp(self.act, xab[:, :], sh[:, 13, :], [sh], [xab])
                    self.actf(sgb[:, :], sh[:, 14, :], AF.Sigmoid, [sh], [sgb])
                    self.ck(3)
                    for d in range(2):
                        for (wb_, xin, dst, bname, pb) in ((w2b, twb, SW[d], f"w0_{d}", B[0]), (a2b, xab, AA[d], f"a0_{d}", B[1])):
                            for m in range(4):
                                self.mm(pb[:, m * 128:(m + 1) * 128], wb_[d * 64:(d + 1) * 64, m * 128:(m + 1) * 128],
                                        xin[d * 64:(d + 1) * 64, :], True, True, [wb_, xin], [pb])
                            for m in range(4):
                                self.actf(dst[:, m, :], pb[:, m * 128:(m + 1) * 128], AF.Sigmoid, [pb, PFl], [dst],
                                          bias=self.pfv(l, bname, m), scale=1.0)
                    for m in range(4):
                        self.mm(B[2][:, m * 128:(m + 1) * 128], g2b[:, m * 128:(m + 1) * 128], sgb[:, :], True, True, [g2b, sgb], [B[2]])
                    self.cp(self.act, ga[:, :, :], B[2][:, :].rearrange("p (m t) -> p m t", m=4), [B[2]], [ga])
                    self.dma(self.GAs.rearrange("(m p) n -> p m n", p=128)[:, :, n0:n0 + 128], ga[:, :, :], [ga], [self.dd("GAs", n0 // 128)])
                    self.ck(4)
                    self.tt(V, kx[:, :, :], k_, bc(self.pfv(l, "kk"), 4), ALU.mult, [sh, PFl], [kx])
                    self.tt(G, tq[:, :, :], kx[:, :, :], kx[:, :, :], ALU.mult, [kx], [tq])
                    for m in range(4):
                        self.mm(B[3][:, m * 128:(m + 1) * 128], self.bones[:, :], tq[:, m, :], True, True, [self.bones, tq], [B[3]])
                    self.ts(V, tq[:, :, :], B[3][:, :].rearrange("p (m t) -> p m t", m=4), EPS, None, ALU.add, None, [B[3]], [tq])
                    self.actf(tq[:, :, :], tq[:, :, :], AF.Sqrt, [tq], [tq])
                    self.recip(tq[:, :, :], tq[:, :, :], [tq], [tq])
                    self.tt(V, kk[:, :, :], kx[:, :, :], tq[:, :, :], ALU.mult, [kx, tq], [kk])
                    self.ck(5)
                    self.tt(G, bon[:, :, :], r_, k_, ALU.mult, [sh], [bon])
                    self.tt(G, bon[:, :, :], bon[:, :, :], bc(self.pfv(l, "rk"), 4), ALU.mult, [bon, PFl], [bon])
                    for m in range(4):
                        self.mm(B[0][:, m * 128:(m + 1) * 128], self.bones[:, :], bon[:, m, :], True, True, [self.bones, bon], [B[0]])
                    self.tt(V, bon[:, :, :], B[0][:, :].rearrange("p (m t) -> p m t", m=4), v_, ALU.mult, [B[0], sh], [bon])
                    self.dma(self.BON.rearrange("(m p) n -> p m n", p=128)[:, :, n0:n0 + 128], bon[:, :, :], [bon], [self.dd("BON", n0 // 128)])
                    self.ck(6)
                    self.cp(G, vb[:, :, :], v_, [sh], [vb])
                    pbv = B[1][:, :].bitcast(BF16)
                    for m in range(4):
                        self.tr(pbv[:, m * 128:(m + 1) * 128], vb[:, m, :], self.identb[:, :], [vb, self.identb], [B[1]])
                    self.cp(self.act, Vt[:, :], pbv[:, 0:512], [B[1]], [Vt])
                    self.dma(self.RV[s, j], Vt[:, :], [Vt], [self.dd("RV", s, j)])
                    for d in range(2):
                        self.ck(7)
                        self.tt(V, tmpa[:, :, :], AA[d][:, :, :], bc(self.pfv(l, "ka"), 4), ALU.mult, [AA[d], PFl], [tmpa])
                        self.tt(V, tmpa[:, :, :], tmpa[:, :, :], bc(omka[:, :], 4), ALU.add, [tmpa, omka], [tmpa])
                        self.tt(V, tmpa[:, :, :], tmpa[:, :, :], k_, ALU.mult, [tmpa, sh], [tmpa])
                        self.tt(G, tmpb[:, :, :], kk[:, :, :], AA[d][:, :, :], ALU.mult, [kk, AA[d]], [tmpb])
                        self.ck(8)
                        for m in range(4):
                            self.op(V, lambda m=m: nc.vector.tensor_tensor_scan(out=CS[:, m, :], data0=self.RM[:, :], data1=SW[d][:, m, :],
                                                                               initial=0.0, op0=ALU.mult, op1=ALU.add),
                                    [self.RM, SW[d]], [CS])
                        CSv = CS[:, :, :].rearrange("p m (c t) -> p m c t", t=64)
                        if d == 0:
                            self.tt(V, EX[:, :, :], CS[:, :, :], SW[d][:, :, :], ALU.subtract, [CS, SW[d]], [EX])
                            incl = CS
                        else:
                            tot = CSv[:, :, :, 63:64].to_broadcast([128, 4, 2, 64])
                            self.tt(V, EX[:, :, :].rearrange("p m (c t) -> p m c t", t=64), tot, CSv, ALU.subtract, [CS], [EX])
                            self.tt(V, e3[:, :, :], EX[:, :, :], SW[d][:, :, :], ALU.add, [EX, SW[d]], [e3])
                            incl = e3
                        self.ck(9)
                        self.actf(e1[:, :, :], EX[:, :, :], AF.Exp, [EX], [e1], scale=-CDEC)
                        self.actf(e2[:, :, :], incl[:, :, :], AF.Exp, [incl], [e2], scale=CDEC)
                        self.actf(e3[:, :, :], incl[:, :, :], AF.Exp, [incl], [e3], scale=-CDEC)
                        self.actf(pc[:, :].rearrange("p (m c) -> p m c", c=2), CSv[:, :, :, 63], AF.Exp, [CS], [pc], scale=-CDEC)
                        self.dma(self.RPC[s, d, j], pc[:, :], [pc], [self.dd("RPC", s, d, j)])
                        self.tt(V, KQ[:, :, 0:128], kk[:, :, :], e1[:, :, :], ALU.mult, [kk, e1], [KQ])
                        self.tt(G, KQ[:, :, 128:256], r_, e3[:, :, :], ALU.mult, [sh, e3], [KQ])
                        self.tt(V, BTb[:, :, :], tmpb[:, :, :], e2[:, :, :], ALU.mult, [tmpb, e2], [BTb])
                        self.tt(G, CTb[:, :, :], tmpa[:, :, :], e2[:, :, :], ALU.mult, [tmpa, e2], [CTb])
                        self.dma(self.RKQ[s, d, j], KQ[:, :, :].rearrange("p m t -> p (m t)"), [KQ], [self.dd("RKQ", s, d, j)])
                        self.ck(10)
                        pbb = B[2][:, :].bitcast(BF16)
                        for m in range(4):
                            self.tr(pbb[:, m * 128:(m + 1) * 128], BTb[:, m, :], self.identb[:, :], [BTb, self.identb], [B[2]])
                            self.tr(pbb[:, 512 + m * 128:512 + (m + 1) * 128], CTb[:, m, :], self.identb[:, :], [CTb, self.identb], [B[2]])
                        self.cp(self.act, BC[:, :], pbb[:, :], [B[2]], [BC])
                        self.dma(self.RBC[s, d, j], BC[:, :], [BC], [self.dd("RBC", s, d, j)])
                        self.ck(11)
                        Ms, Mn = (self.MU, self.ML) if d == 0 else (self.ML, self.MU)
                        for e_ in range(2):
                            rows = slice(e_ * 64, e_ * 64 + 64)
                            self.cp(G, BTz[:, :, :].rearrange("p (m e) t -> p m e t", e=2)[rows, :, e_, :], BTb[rows, :, :], [BTb], [BTz])
                            self.cp(self.act, CTz[:, :, :].rearrange("p (m e) t -> p m e t", e=2)[rows, :, e_, :], CTb[rows, :, :], [CTb], [CTz])
                        Msb = Ms[:, :].unsqueeze(1).to_broadcast([128, 2, 256])
                        v2 = lambda p: p[:, :].rearrange("p (h t) -> p h t", h=2)
                        for h in range(8):
                            self.mm(B[h // 2][:, (h % 2) * 256:(h % 2 + 1) * 256], BTz[:, h, :], KQ[:, h // 2, :], True, True, [BTz, KQ], [B[h // 2]])
                        for q in range(4):
                            self.tt(V, X1a[:, 2 * q:2 * q + 2, :], v2(B[q]), Msb, ALU.mult, [B[q], Ms], [X1a])
                        for h in range(8):
                            self.mm(B[h // 2][:, (h % 2) * 256:(h % 2 + 1) * 256], CTz[:, h, :], KQ[:, h // 2, :], True, True, [CTz, KQ], [B[h // 2]])
                        for q in range(4):
                            self.tt(V, MAT[:, 2 * q:2 * q + 2, 256:512], v2(B[q]), Msb, ALU.mult, [B[q], Ms], [MAT])
                        for h in range(8):
                            self.mm(B[h // 4][:, (h % 4) * 128:(h % 4 + 1) * 128], KQ[:, h // 2, 0:128], BTz[:, h, :], True, True, [BTz, KQ], [B[h // 4]])
                        Mnb = Mn[:, 0:128].unsqueeze(1).to_broadcast([128, 4, 128])
                        for q in range(2):
                            self.tt(V, NNa[:, 4 * q:4 * q + 4, :], B[q][:, :].rearrange("p (h t) -> p h t", h=4), Mnb, ALU.mult, [B[q], Mn], [NNa])
                        self.cp(G, MAT[:, :, 128:256], X1a[:, :, 128:256], [X1a], [MAT])
                        self.inverse_batch(X1a, NNa, MAT, 0, (B[0], B[1], B[2]), Wt, At, Bt)
                        self.inverse_batch(X1a, NNa, MAT, 4, (B[3], B[1], B[2]), Wt2, At2, Bt2)
                        self.ck(12)
                        self.dma(self.RMAT[s, d, j], MAT[:, :, :].rearrange("p h t -> p (h t)"), [MAT], [self.dd("RMAT", s, d, j)])
            self.run_interleaved([lambda: stream(0), lambda: stream(1)])
            self.barrier()

    def gdn_setup(self):
        self.GKQ = self.dram("GKQ", [S, 2, NJ, 128, 4 * 256], BF16)
        self.GMAT = self.dram("GMAT", [S, 2, NJ, 128, 4 * 512], BF16)
        self.GBC = self.dram("GBC", [S, 2, NJ, 128, 1024], BF16)
        self.GV = self.dram("GV", [S, NJ, 128, 512], BF16)
        self.GPC = self.dram("GPC", [S, 2, NJ, 128, 8], F32)
        self.YB = self.dram("YB", [2, NT, 512])
        self.SEL = self.sb([16, 16, 128], F32)
        self.selc = self.sb([128, 1], F32)
        self.onec = self.sb([128, 1], F32)
        nc = self.nc
        P = self.pool
        for i in range(16):
            self.cp(P, self.SEL[0:16, i, :], self.ident[0:16, i:i + 1].to_broadcast([16, 128]), [self.ident], [self.SEL])
        self.memset(P, self.onec[:, :], 1.0, [self.onec])
        self.memset(P, self.selc[:, :], 1.0, [self.selc])
        self.op(P, lambda: nc.gpsimd.affine_select(out=self.selc[:, :], in_=self.selc[:, :], pattern=[[0, 1]], compare_op=ALU.is_ge, fill=0.0,
                                                   base=-4, channel_multiplier=1), [self.selc], [self.selc])

    def gdn_prep(self, l):
        nc = self.nc
        V, G = self.dve, self.pool
        ps = self.ps
        with ExitStack() as es:
            def t(shape, dt=F32):
                return self.sb(shape, dt, es)
            PFl = self.PFt[l]

            def bc(ap, n):
                return ap.unsqueeze(2).to_broadcast([128, n, 128])
            negA = t([128, 1])
            self.actf(negA[:, :], self.pfv(l, "alog"), AF.Exp, [PFl], [negA])
            self.ts(V, negA[:, :], negA[:, :], -1.0, None, ALU.mult, None, [negA], [negA])
            def stream(s):
                B = self.ps[4 * s:4 * s + 4]
                qkv = t([128, 12, 132])
                cv, t2 = t([128, 12, 128]), t([128, 12, 128])
                sq = t([128, 8, 128])
                kq = t([128, 4, 256])
                kqb = t([128, 4, 256], BF16)
                kb = t([128, 4, 128], BF16)
                vb = t([128, 4, 128], BF16)
                ab, x1, Xg, SIG, gcf, gcr = (t([16, 128]) for _ in range(6))
                X2, E2 = t([16, 256]), t([16, 256])
                TOTb, Etot = t([16, 128]), t([16, 2])
                TS = t([128, 80])
                negg, sB, sC, dd_ = t([128, 8]), t([128, 8]), t([128, 8]), t([128, 8])
                gm4 = t([128, 4, 256])
                KQd = t([128, 4, 256], BF16)
                BCd = [t([128, 1024], BF16) for _ in range(2)]
                Vt = t([128, 512], BF16)
                MAT = t([128, 4, 512], BF16)
                pc = t([128, 8])
                X1a = t([128, 4, 256], BF16)
                NNa = t([128, 4, 128], BF16)
                Wt = [t([128, 4, 128], BF16) for _ in range(2)]
                At = [t([128, 4, 128], BF16) for _ in range(2)]
                Bt = [t([128, 4, 128], BF16) for _ in range(2)]
                abv = self.PT[3968:3984, :]
                for j in range(NJ):
                    n0 = s * T + j * 128
                    self.load_halo(qkv, 15, 12, s, j, 2)
                    gw = self.pfv(l, "gconv")
                    self.tt(V, cv[:, :, :], qkv[:, :, 0:128], bc(gw[:, 0:12], 12), ALU.mult, [qkv, PFl], [cv])
                    for k in range(1, 5):
                        self.tt(G, t2[:, :, :], qkv[:, :, k:k + 128], bc(gw[:, k * 12:(k + 1) * 12], 12), ALU.mult, [qkv, PFl], [t2])
                        self.tt(V, cv[:, :, :], cv[:, :, :], t2[:, :, :], ALU.add, [cv, t2], [cv])
                    self.actf(cv[:, :, :], cv[:, :, :], AF.Silu, [cv], [cv])
                    self.tt(G, sq[:, :, :], cv[:, 0:8, :], cv[:, 0:8, :], ALU.mult, [cv], [sq])
                    for c in range(8):
                        pb = B[c // 4]
                        self.mm(pb[:, (c % 4) * 128:(c % 4 + 1) * 128], self.ones[:, :], sq[:, c, :], True, True, [self.ones, sq], [pb])
                    for hf in range(2):
                        self.ts(V, sq[:, hf * 4:(hf + 1) * 4, :], B[hf][:, :].rearrange("p (c t) -> p c t", c=4), EPS, None, ALU.add, None, [B[hf]], [sq])
                    self.actf(sq[:, :, :], sq[:, :, :], AF.Sqrt, [sq], [sq])
                    self.recip(sq[:, :, :], sq[:, :, :], [sq], [sq])
                    self.tt(V, kq[:, :, 0:128], cv[:, 4:8, :], sq[:, 4:8, :], ALU.mult, [cv, sq], [kq])
                    self.stt(V, kq[:, :, 128:256], cv[:, 0:4, :], 128.0 ** -0.5, sq[:, 0:4, :], ALU.mult, ALU.mult, [cv, sq], [kq])
                    self.cp(G, kqb[:, :, :], kq[:, :, :], [kq], [kqb])
                    self.cp(G, kb[:, :, :], kq[:, :, 0:128], [kq], [kb])
                    self.cp(G, vb[:, :, :], cv[:, 8:12, :], [cv], [vb])
                    pbv = B[2][:, :].bitcast(BF16)
                    for m in range(4):
                        self.tr(pbv[:, m * 128:(m + 1) * 128], vb[:, m, :], self.identb[:, :], [vb, self.identb], [B[2]])
                    self.cp(self.act, Vt[:, :], pbv[:, 0:512], [B[2]], [Vt])
                    self.dma(self.GV[s, j], Vt[:, :], [Vt], [self.dd("GV", s, j)])
                    pbk = B[3][:, :].bitcast(BF16)
                    for m in range(4):
                        self.tr(pbk[:, m * 128:(m + 1) * 128], kb[:, m, :], self.identb[:, :], [kb, self.identb], [B[3]])
                    self.dma(ab[0:16, :], abv[:, n0:n0 + 128], [], [ab])
                    self.actf(x1[0:16, :], ab[0:16, :], AF.Exp, [ab, PFl], [x1], bias=self.pfv(l, "dtb")[0:16, :], scale=1.0)
                    self.actf(x1[0:16, :], x1[0:16, :], AF.Ln, [x1, self.onec], [x1], bias=self.onec[0:16, :], scale=1.0)
                    self.ts(V, Xg[0:16, :], x1[0:16, :], negA[0:16, :], None, ALU.mult, None, [x1, negA], [Xg])
                    self.actf(SIG[0:16, :], ab[0:16, :], AF.Sigmoid, [ab], [SIG])
                    self.op(V, lambda: nc.vector.tensor_tensor_scan(out=gcf[0:16, :], data0=self.RM[0:16, :], data1=Xg[0:16, :], initial=0.0,
                                                                    op0=ALU.mult, op1=ALU.add), [self.RM, Xg], [gcf])
                    gcfv = gcf[0:16, :].rearrange("p (c t) -> p c t", t=64)
                    totb = gcfv[:, :, 63:64].to_broadcast([16, 2, 64])
                    self.cp(V, TOTb[0:16, :].rearrange("p (c t) -> p c t", t=64), totb, [gcf], [TOTb])
                    self.tt(V, gcr[0:16, :], TOTb[0:16, :], gcf[0:16, :], ALU.subtract, [TOTb, gcf], [gcr])
                    self.tt(V, X2[0:16, 128:256], gcr[0:16, :], Xg[0:16, :], ALU.add, [gcr, Xg], [X2])
                    self.tt(V, X2[0:16, 128:256], X2[0:16, 128:256], gcf[0:16, :], ALU.subtract, [X2, gcf], [X2])
                    self.stt(V, X2[0:16, 128:256], X2[0:16, 128:256], self.selc[0:16, :], gcf[0:16, :], ALU.mult, ALU.add, [X2, self.selc, gcf], [X2])
                    self.tt(V, X2[0:16, 0:128], X2[0:16, 128:256], Xg[0:16, :], ALU.subtract, [X2, Xg], [X2])
                    self.actf(E2[0:16, :], X2[0:16, :], AF.Exp, [X2], [E2])
                    self.actf(Etot[0:16, :], gcfv[:, :, 63], AF.Exp, [gcf], [Etot])
                    pT = B[2]
                    for q_, src in enumerate((Xg[0:16, :], SIG[0:16, :], X2[0:16, 0:128], X2[0:16, 128:256], TOTb[0:16, :])):
                        self.tr(pT[:, q_ * 16:(q_ + 1) * 16], src, self.ident[0:16, 0:16], [Xg, SIG, X2, TOTb, self.ident], [pT])
                    self.cp(V, TS[:, :], pT[:, 0:80], [pT], [TS])
                    self.actf(negg[:, :], TS[:, 0:8], AF.Exp, [TS], [negg], scale=-1.0)
                    self.tt(V, dd_[:, :], TS[:, 64:72], TS[:, 32:40], ALU.subtract, [TS], [dd_])
                    self.actf(sB[:, :], dd_[:, :], AF.Exp, [dd_], [sB])
                    self.tt(V, sB[:, :], sB[:, :], TS[:, 24:32], ALU.mult, [sB, TS], [sB])
                    self.tt(V, dd_[:, :], TS[:, 64:72], TS[:, 48:56], ALU.subtract, [TS], [dd_])
                    self.actf(sC[:, :], dd_[:, :], AF.Exp, [dd_], [sC])
                    self.tt(V, sC[:, :], sC[:, :], TS[:, 24:32], ALU.mult, [sC, TS], [sC])
                    pbk4 = pbk[:, 0:512].rearrange("p (h t) -> p h t", h=4)
                    for d in range(2):
                        self.tt(V, BCd[d][:, 0:512].rearrange("p (h t) -> p h t", h=4), pbk4, sB[:, d * 4:d * 4 + 4].unsqueeze(2).to_broadcast([128, 4, 128]), ALU.mult, [B[3], sB], [BCd[d]])
                        self.tt(V, BCd[d][:, 512:1024].rearrange("p (h t) -> p h t", h=4), pbk4, sC[:, d * 4:d * 4 + 4].unsqueeze(2).to_broadcast([128, 4, 128]), ALU.mult, [B[3], sC], [BCd[d]])
                    for d in range(2):
                        Ms = self.MU if d == 0 else self.ML
                        BC = BCd[d]
                        for h in range(4):
                            i = d * 4 + h
                            self.mm(B[h // 2][:, (h % 2) * 256:(h % 2 + 1) * 256], self.SEL[0:16, i, :], X2[0:16, :], True, True, [self.SEL, X2], [B[h // 2]])
                            self.mm(B[2 + h // 2][:, (h % 2) * 256:(h % 2 + 1) * 256], self.SEL[0:16, i, :], E2[0:16, :], True, True, [self.SEL, E2], [B[2 + h // 2]])
                        v2 = lambda p: p[:, :].rearrange("p (h t) -> p h t", h=2)
                        for q in range(2):
                            self.tt(V, gm4[:, 2 * q:2 * q + 2, :], v2(B[q]), TS[:, 32 + d * 4 + 2 * q:32 + d * 4 + 2 * q + 2].unsqueeze(2).to_broadcast([128, 2, 256]),
                                    ALU.subtract, [B[q], TS], [gm4])
                            self.tt(V, KQd[:, 2 * q:2 * q + 2, :], kq[:, 2 * q:2 * q + 2, :], v2(B[2 + q]), ALU.mult, [kq, B[2 + q]], [KQd])
                        for h in range(4):
                            self.mm(B[2][:, h * 2:(h + 1) * 2], self.SEL[0:16, d * 4 + h, :], Etot[0:16, :], True, True, [self.SEL, Etot], [B[2]])
                        self.cp(self.act, pc[:, :], B[2][:, 0:8], [B[2]], [pc])
                        self.ts(G, gm4[:, :, :], gm4[:, :, :], 0.0, None, ALU.min, None, [gm4], [gm4])
                        self.actf(gm4[:, :, :], gm4[:, :, :], AF.Exp, [gm4], [gm4])
                        self.tt(G, gm4[:, :, :], gm4[:, :, :], Ms[:, :].unsqueeze(1).to_broadcast([128, 4, 256]), ALU.mult, [gm4, Ms], [gm4])
                        for h in range(4):
                            self.mm(B[h // 2][:, (h % 2) * 256:(h % 2 + 1) * 256], kb[:, h, :], kqb[:, h, :], True, True, [kb, kqb], [B[h // 2]])
                        for q in range(2):
                            self.tt(V, gm4[:, 2 * q:2 * q + 2, :], gm4[:, 2 * q:2 * q + 2, :], v2(B[q]), ALU.mult, [gm4, B[q]], [gm4])
                        self.tt(V, X1a[:, :, :], gm4[:, :, :], TS[:, 24 + d * 4:28 + d * 4].unsqueeze(2).to_broadcast([128, 4, 256]), ALU.mult, [gm4, TS], [X1a])
                        self.tt(V, MAT[:, :, 256:512], X1a[:, :, :], negg[:, d * 4:d * 4 + 4].unsqueeze(2).to_broadcast([128, 4, 256]), ALU.mult, [X1a, negg], [MAT])
                        self.cp(G, MAT[:, :, 128:256], X1a[:, :, 128:256], [X1a], [MAT])
                        pN = B[3][:, :].bitcast(BF16)
                        for h in range(4):
                            self.tr(pN[:, h * 128:(h + 1) * 128], X1a[:, h, 0:128], self.identb[:, :], [X1a, self.identb], [B[3]])
                        self.cp(self.act, NNa[:, :, :], pN[:, 0:512].rearrange("p (h t) -> p h t", h=4), [B[3]], [NNa])
                        self.inverse_batch(X1a, NNa, MAT, 0, (B[0], B[1], B[2]), Wt, At, Bt)
                        self.dma(self.GKQ[s, d, j], KQd[:, :, :].rearrange("p m t -> p (m t)"), [KQd], [self.dd("GKQ", s, d, j)])
                        self.dma(self.GMAT[s, d, j], MAT[:, :, :].rearrange("p h t -> p (h t)"), [MAT], [self.dd("GMAT", s, d, j)])
                        self.dma(self.GBC[s, d, j], BC[:, :], [BC], [self.dd("GBC", s, d, j)])
                        self.dma(self.GPC[s, d, j], pc[:, :], [pc], [self.dd("GPC", s, d, j)])
            self.run_interleaved([lambda: stream(0), lambda: stream(1)])
            self.barrier()

    def conf_setup(self):
        self.RCs = self.dram("RCs", [512, NT], BF16)
        self.RAs = self.dram("RAs", [512, NT], BF16)
        self.RBs = self.dram("RBs", [512, NT], BF16)

    def conformer(self, l):
        V, G = self.dve, self.pool
        ps = self.ps
        PFl = self.PFt[l]
        valv = self.PT[3984:3984 + 512, :].rearrange("(c p) n -> p c n", p=128)
        gatv = self.PT[4496:4496 + 512, :].rearrange("(c p) n -> p c n", p=128)
        RCv = self.RCs.rearrange("(c p) n -> p c n", p=128)
        with ExitStack() as es:
            def t(shape, dt=F32):
                return self.sb(shape, dt, es)
            u = t([128, 4, TL])
            gt = t([128, 4, TL])
            o = t([128, 4, TL])
            sq = t([128, 4, 512])
            mu, rs, var = t([128, 512]), t([128, 512]), t([128, 512])
            ob = t([128, 4, 512], BF16)
            cw = self.pfv(l, "cdw")
            for s in range(S):
                for (seg0, W_) in ((0, TC), (TC, TL)):
                    n0 = s * T + seg0
                    for c in range(4):
                        self.dma(u[:, c, :W_], valv[:, c, n0:n0 + W_], [], [u])
                        self.dma(gt[:, c, :W_], gatv[:, c, n0:n0 + W_], [], [gt])
                    self.actf(gt[:, :, :W_], gt[:, :, :W_], AF.Sigmoid, [gt], [gt])
                    self.tt(V, u[:, :, :W_], u[:, :, :W_], gt[:, :, :W_], ALU.mult, [u, gt], [u])
                    for c in range(4):
                        def wk(k):
                            return cw[:, k * 4 + c:k * 4 + c + 1]
                        self.ts(V, o[:, c, :W_], u[:, c, :W_], wk(15), None, ALU.mult, None, [u, PFl], [o])
                        for k in range(31):
                            dlt = k - 15
                            if dlt == 0:
                                continue
                            if seg0 == 0:
                                lo, hi = max(0, -dlt), min(W_, W_ - dlt)
                                self.stt(V, o[:, c, lo:hi], u[:, c, lo + dlt:hi + dlt], wk(k), o[:, c, lo:hi], ALU.mult, ALU.add, [u, PFl, o], [o])
                            elif c < 2:
                                uv = u[:, c, :].rearrange("p (r w) -> p r w", w=64)
                                ov = o[:, c, :].rearrange("p (r w) -> p r w", w=64)
                                lo, hi = max(0, -dlt), min(64, 64 - dlt)
                                self.stt(V, ov[:, :, lo:hi], uv[:, :, lo + dlt:hi + dlt], wk(k), ov[:, :, lo:hi], ALU.mult, ALU.add, [u, PFl, o], [o])
                            else:
                                uv = u[:, c, :].rearrange("p (r w) -> p r w", w=64)
                                ov = o[:, c, :].rearrange("p (r w) -> p r w", w=64)
                                lo, hi = max(0, -dlt), min(32, 32 - dlt)
                                self.stt(V, ov[:, lo:hi, :], uv[:, lo + dlt:hi + dlt, :], wk(k), ov[:, lo:hi, :], ALU.mult, ALU.add, [u, PFl, o], [o])
                        self.ts(V, o[:, c, :W_], o[:, c, :W_], self.pfv(l, "cdwb", c), None, ALU.add, None, [o, PFl], [o])
                    for t0 in range(0, W_, 512):
                        w = min(512, W_ - t0)
                        self.tt(G, sq[:, :, :w], o[:, :, t0:t0 + w], o[:, :, t0:t0 + w], ALU.mult, [o], [sq])
                        for c in range(4):
                            self.mm(ps[0][:, :w], self.ones[:, :], o[:, c, t0:t0 + w], c == 0, c == 3, [self.ones, o], [ps[0]])
                        for c in range(4):
                            self.mm(ps[1][:, :w], self.ones[:, :], sq[:, c, :w], c == 0, c == 3, [self.ones, sq], [ps[1]])
                        self.ts(V, mu[:, :w], ps[0][:, :w], 1.0 / 512, None, ALU.mult, None, [ps[0]], [mu])
                        self.tt(V, var[:, :w], mu[:, :w], mu[:, :w], ALU.mult, [mu], [var])
                        self.stt(V, var[:, :w], ps[1][:, :w], 1.0 / 512, var[:, :w], ALU.mult, ALU.subtract, [ps[1], var], [var])
                        self.ts(V, var[:, :w], var[:, :w], EPS, None, ALU.add, None, [var], [var])
                        self.actf(var[:, :w], var[:, :w], AF.Sqrt, [var], [var])
                        self.recip(rs[:, :w], var[:, :w], [var], [rs])
                        for c in range(4):
                            self.tt(V, sq[:, c, :w], o[:, c, t0:t0 + w], mu[:, :w], ALU.subtract, [o, mu], [sq])
                            self.tt(V, sq[:, c, :w], sq[:, c, :w], rs[:, :w], ALU.mult, [sq, rs], [sq])
                            self.ts(V, sq[:, c, :w], sq[:, c, :w], self.pfv(l, "clng", c), self.pfv(l, "clnb", c), ALU.mult, ALU.add, [sq, PFl], [sq])
                        self.actf(ob[:, :, :w], sq[:, :, :w], AF.Silu, [sq], [ob])
                        self.dma(RCv[:, :, n0 + t0:n0 + t0 + w], ob[:, :, :w], [ob], [self.dd("RCs", (n0 + t0) // 128)])
            self.barrier()

    def red(self, E, out, in_, op, R, W):
        self.op(E, lambda: E.be.tensor_reduce(out=out, in_=in_, axis=AX.X, op=op), R, W)

    def merge(self, l):
        V, G = self.dve, self.pool
        ps = self.ps
        PFl = self.PFt[l]
        with ExitStack() as es:
            def t(shape, dt=F32):
                return self.sb(shape, dt, es)
            wbr = t([128, 12, 1024], BF16)
            wo = t([128, 8, 1024], BF16)
            for n in range(3):
                self.dma(wbr[:, n * 4:(n + 1) * 4, :], self.w_branch[l, n].rearrange("(k p) n -> p k n", p=128), [], [wbr], Q=self.pool)
            self.dma(wo[:, :, :], self.w_out[l].rearrange("(k p) n -> p k n", p=128), [], [wo], Q=self.pool)
            pgv = [self.PT[5008 + n * 1024:5008 + (n + 1) * 1024, :].rearrange("(c p) n -> p c n", p=128) for n in range(3)]
            fm4 = lambda A: A.rearrange("(c p) n -> p c n", p=128)
            y0, y1, ysq = t([128, 512]), t([128, 512]), t([128, 512])
            m1, m2, m3 = t([128, 8]), t([128, 8]), t([128, 8])
            raf, bon, ga, zt = (t([128, 4, 128]) for _ in range(4))
            Rb = [t([128, 4, 128], BF16) for _ in range(3)]
            pg = t([128, 8, 128])
            macc, tmpm = t([128, 8, 128]), t([128, 8, 128])
            mb = t([128, 8, 128], BF16)
            xt = t([128, 8, 128])
            for s in range(S):
                for j in range(NJ):
                    n0 = s * T + j * 128
                    r = 2 if j < 2 else s
                    for br in range(2):
                        DY, nm, nh, dvv, eps_ = ((self.YA, "R", 8, 64, 64e-5), (self.YB, "G", 4, 128, EPS))[br]
                        self.dma(y0[:, :], DY[0, n0:n0 + 128, :], [self.dd(nm + "Y", 0, n0 // 128)], [y0])
                        self.dma(y1[:, :], DY[1, n0:n0 + 128, :], [self.dd(nm + "Y", 1, n0 // 128)], [y1])
                        self.tt(V, y0[:, :], y0[:, :], y1[:, :], ALU.add, [y0, y1], [y0])
                        yv = y0[:, :].rearrange("p (h d) -> p h d", d=dvv)
                        self.tt(G, ysq[:, :], y0[:, :], y0[:, :], ALU.mult, [y0], [ysq])
                        self.red(V, m2[:, 0:nh], ysq[:, :].rearrange("p (h d) -> p h d", d=dvv), ALU.add, [ysq], [m2])
                        if br == 0:
                            self.red(V, m1[:, 0:nh], yv, ALU.add, [y0], [m1])
                            self.ts(V, m1[:, 0:nh], m1[:, 0:nh], 1.0 / dvv, None, ALU.mult, None, [m1], [m1])
                            self.tt(V, m3[:, 0:nh], m1[:, 0:nh], m1[:, 0:nh], ALU.mult, [m1], [m3])
                            self.stt(V, m2[:, 0:nh], m2[:, 0:nh], 1.0 / dvv, m3[:, 0:nh], ALU.mult, ALU.subtract, [m2, m3], [m2])
                            self.ts(V, m2[:, 0:nh], m2[:, 0:nh], eps_, None, ALU.add, None, [m2], [m2])
                            self.tt(V, yv, yv, m1[:, 0:nh].unsqueeze(2).to_broadcast([128, nh, dvv]), ALU.subtract, [y0, m1], [y0])
                        else:
                            self.ts(V, m2[:, 0:nh], m2[:, 0:nh], 1.0 / dvv, eps_, ALU.mult, ALU.add, [m2], [m2])
                        self.actf(m2[:, 0:nh], m2[:, 0:nh], AF.Sqrt, [m2], [m2])
                        self.recip(m2[:, 0:nh], m2[:, 0:nh], [m2], [m2])
                        self.tt(V, yv, yv, m2[:, 0:nh].unsqueeze(2).to_broadcast([128, nh, dvv]), ALU.mult, [y0, m2], [y0])
                        pb = ps[br]
                        for c in range(4):
                            self.tr(pb[:, c * 128:(c + 1) * 128], y0[:, c * 128:(c + 1) * 128], self.ident[:, :], [y0, self.ident], [pb])
                        pbv = pb[:, :].rearrange("p (c t) -> p c t", c=4)
                        if br == 0:
                            for c in range(4):
                                self.ts(V, raf[:, c, :], pbv[:, c, :], self.pfv(l, "ln_g", c), self.pfv(l, "ln_b", c), ALU.mult, ALU.add, [pb, PFl], [raf])
                            self.dma(bon[:, :, :], fm4(self.BON)[:, :, n0:n0 + 128], [self.dd("BON", n0 // 128)], [bon])
                            self.dma(ga[:, :, :], fm4(self.GAs)[:, :, n0:n0 + 128], [self.dd("GAs", n0 // 128)], [ga])
                            self.tt(V, raf[:, :, :], raf[:, :, :], bon[:, :, :], ALU.add, [raf, bon], [raf])
                            self.tt(V, Rb[0][:, :, :], raf[:, :, :], ga[:, :, :], ALU.mult, [raf, ga], [Rb[0]])
                        else:
                            self.dma(zt[:, :, :], self.PTv[:, 27:31, n0:n0 + 128], [], [zt])
                            self.actf(zt[:, :, :], zt[:, :, :], AF.Silu, [zt], [zt])
                            self.stt(V, Rb[1][:, :, :], pbv, self.pfv(l, "gnorm", 0), zt[:, :, :], ALU.mult, ALU.mult, [pb, PFl, zt], [Rb[1]])
                    self.dma(Rb[2][:, :, :], fm4(self.RCs)[:, :, n0:n0 + 128], [self.dd("RCs", n0 // 128)], [Rb[2]])
                    for n in range(3):
                        self.dma(pg[:, :, :], pgv[n][:, :, n0:n0 + 128], [], [pg])
                        self.actf(pg[:, :, :], pg[:, :, :], AF.Sigmoid, [pg], [pg])
                        for m in range(8):
                            pb = ps[2 + m // 4]
                            for k in range(4):
                                self.mm(pb[:, (m % 4) * 128:(m % 4 + 1) * 128], wbr[:, n * 4 + k, m * 128:(m + 1) * 128], Rb[n][:, k, :], k == 0, k == 3,
                                        [wbr, Rb[n]], [pb])
                        for hf in range(2):
                            pbv = ps[2 + hf][:, :].rearrange("p (c t) -> p c t", c=4)
                            dst = macc if n == 0 else tmpm
                            self.tt(V, dst[:, hf * 4:(hf + 1) * 4, :], pbv, pg[:, hf * 4:(hf + 1) * 4, :], ALU.mult, [ps[2 + hf], pg], [dst])
                        if n > 0:
                            self.tt(G, macc[:, :, :], macc[:, :, :], tmpm[:, :, :], ALU.add, [macc, tmpm], [macc])
                    self.cp(G, mb[:, :, :], macc[:, :, :], [macc], [mb])
                    self.dma(xt[:, :, :], self.XTv[:, :, n0:n0 + 128], self.dr("XT", n0, 128), [xt])
                    for m in range(8):
                        pb = ps[4 + m // 4]
                        for k in range(8):
                            self.mm(pb[:, (m % 4) * 128:(m % 4 + 1) * 128], wo[:, k, m * 128:(m + 1) * 128], mb[:, k, :], k == 0, k == 7, [wo, mb], [pb])
                    for m in range(8):
                        pb = ps[4 + m // 4]
                        self.stt(V, xt[:, m, :], pb[:, (m % 4) * 128:(m % 4 + 1) * 128], self.MOD[l][:, 16 + m, r:r + 1], xt[:, m, :], ALU.mult, ALU.add,
                                 [pb, self.MOD[l], xt], [xt])
                    self.dma(self.XTv[:, :, n0:n0 + 128], xt[:, :, :], [xt], self.dr("XT", n0, 128))
                    if f"XM{l}" in self.dbg:
                        pass
            self.barrier()

    def moe(self, l):
        V, G = self.dve, self.pool
        ps = self.ps
        with ExitStack() as es:
            def t(shape, dt=F32, e_=None):
                return self.sb(shape, dt, e_ or es)
            hT = t([128, 8, NT], BF16)
            WTf = t([16, NT])
            wr = t([128, 8, 16])
            rb = t([128, 16])
            self.dma(wr[:, :, :], self.w_router.rearrange("(k p) e -> p k e", p=128), [], [wr])
            self.dma(rb[:, :], self.rbias, [], [rb])
            with ExitStack() as es1:
                xts = [t([128, 8, 512], F32, es1) for _ in range(2)]
                sq = t([128, 8, 512], F32, es1)
                rs = t([128, 512], F32, es1)
                hf = t([128, 8, 512], F32, es1)
                sc, sel, sel2, eq, cm, wts = (t([128, 16], F32, es1) for _ in range(6))
                m1, m2, gs, gsel = (t([128, 4], F32, es1) for _ in range(4))
                gmx, wsum = t([128, 1], F32, es1), t([128, 1], F32, es1)
                v4 = lambda a: a[:, :].rearrange("p (g j) -> p g j", j=4)
                b4 = lambda a: a[:, :].unsqueeze(2).to_broadcast([128, 4, 4])
                for i, (n0, w, r) in enumerate(self.tiles()):
                    xt, _ = self.modulate_tile((xts[i % 2], sq, rs, None), n0, w, r, None, None, None, None)
                    for c in range(8):
                        self.stt(V, hf[:, c, :w], xt[:, c, :w], self.GF[l][:, c, r:r + 1], rs[:, :w], ALU.mult, ALU.mult, [xt, rs, self.GF[l]], [hf])
                        self.actf(hf[:, c, :w], hf[:, c, :w], AF.Identity, [hf, self.MOD[l]], [hf], bias=self.MOD[l][:, 24 + c, r:r + 1], scale=1.0)
                    self.cp(G, hT[:, :, n0:n0 + w], hf[:, :, :w], [hf], [hT])
                    for q in range(w // 128):
                        pR = ps[6]
                        for c in range(8):
                            self.mm(pR[:, 0:16], hf[:, c, q * 128:(q + 1) * 128], wr[:, c, :], c == 0, c == 7, [hf, wr], [pR])
                        self.actf(sc[:, :], pR[:, 0:16], AF.Sigmoid, [pR], [sc])
                        self.tt(V, sel[:, :], sc[:, :], rb[:, :], ALU.add, [sc, rb], [sel])
                        self.red(V, m1[:, :], v4(sel), ALU.max, [sel], [m1])
                        self.tt(V, v4(eq), v4(sel), b4(m1), ALU.is_equal, [sel, m1], [eq])
                        self.stt(V, sel2[:, :], eq[:, :], -1e9, sel[:, :], ALU.mult, ALU.add, [eq, sel], [sel2])
                        self.red(V, m2[:, :], v4(sel2), ALU.max, [sel2], [m2])
                        self.tt(V, gs[:, :], m1[:, :], m2[:, :], ALU.add, [m1, m2], [gs])
                        self.red(V, gmx[:, :], gs[:, :], ALU.max, [gs], [gmx])
                        self.ts(V, gsel[:, :], gs[:, :], gmx[:, 0:1], None, ALU.is_equal, None, [gs, gmx], [gsel])
                        self.tt(V, v4(cm), v4(sel), b4(m2), ALU.is_ge, [sel, m2], [cm])
                        self.tt(V, v4(cm), v4(cm), b4(gsel), ALU.mult, [cm, gsel], [cm])
                        self.tt(V, wts[:, :], sc[:, :], cm[:, :], ALU.mult, [sc, cm], [wts])
                        self.red(V, wsum[:, :], wts[:, :], ALU.add, [wts], [wsum])
                        self.recip(wsum[:, :], wsum[:, :], [wsum], [wsum])
                        self.ts(V, wts[:, :], wts[:, :], wsum[:, 0:1], None, ALU.mult, None, [wts, wsum], [wts])
                        pT = ps[7]
                        self.tr(pT[0:16, 0:128], wts[:, :], self.ident[:, :], [wts, self.ident], [pT])
                        self.cp(self.act, WTf[0:16, n0 + q * 128:n0 + (q + 1) * 128], pT[0:16, 0:128], [pT], [WTf])
                self.barrier()
            TG = 1152
            TW = 384
            yacc = t([128, 8, TG])
            wgb = [t([128, 8, 512], BF16) for _ in range(2)]
            wub = [t([128, 8, 512], BF16) for _ in range(2)]
            wdb = [t([128, 4, 1024], BF16) for _ in range(2)]
            wtb = t([128, TW])
            sg = [t([128, TW]) for _ in range(2)]
            actb = t([128, 4, TW], BF16)
            xt2 = t([128, 8, 128])
            def loadw(e):
                for (src, dstt) in ((self.weg[l, e], wgb[e % 2]), (self.weu[l, e], wub[e % 2]), (self.wed[l, e], wdb[e % 2])):
                    self.dma(dstt[:, :, :], src.rearrange("(k p) n -> p k n", p=128), [], [dstt], Q=self.pool)
            loadw(0)
            for g in range(NT // TG):
                for e in range(16):
                    gb, ub, db = wgb[e % 2], wub[e % 2], wdb[e % 2]
                    if not (g == NT // TG - 1 and e == 15):
                        loadw((e + 1) % 16)
                    for tt_ in range(TG // TW):
                        n0 = g * TG + tt_ * TW
                        self.mm(ps[4][:, :TW], self.SEL[0:16, e, :], WTf[0:16, n0:n0 + TW], True, True, [self.SEL, WTf], [ps[4]])
                        self.cp(self.act, wtb[:, :], ps[4][:, :TW], [ps[4]], [wtb])
                        for hc in range(4):
                            pg_, pu_ = ps[(hc % 2) * 2], ps[(hc % 2) * 2 + 1]
                            for k in range(8):
                                self.mm(pg_[:, :TW], gb[:, k, hc * 128:(hc + 1) * 128], hT[:, k, n0:n0 + TW], k == 0, k == 7, [gb, hT], [pg_])
                            for k in range(8):
                                self.mm(pu_[:, :TW], ub[:, k, hc * 128:(hc + 1) * 128], hT[:, k, n0:n0 + TW], k == 0, k == 7, [ub, hT], [pu_])
                            sg_ = sg[hc % 2]
                            self.actf(sg_[:, :], pg_[:, :TW], AF.Silu, [pg_], [sg_])
                            self.tt(V, sg_[:, :], sg_[:, :], pu_[:, :TW], ALU.mult, [sg_, pu_], [sg_])
                            self.tt(G, actb[:, hc, :], sg_[:, :], wtb[:, :], ALU.mult, [sg_, wtb], [actb])
                        for m in range(8):
                            pd = ps[4 + m % 4]
                            for hc in range(4):
                                self.mm(pd[:, :TW], db[:, hc, m * 128:(m + 1) * 128], actb[:, hc, :], hc == 0, hc == 3, [db, actb], [pd])
                            dst = yacc[:, m, tt_ * TW:(tt_ + 1) * TW]
                            if e == 0:
                                self.cp(self.act, dst, pd[:, :TW], [pd], [yacc])
                            else:
                                self.tt(V, dst, dst, pd[:, :TW], ALU.add, [yacc, pd], [yacc])
                for p_ in range(TG // 128):
                    n0 = g * TG + p_ * 128
                    tq = n0 % T
                    r = 2 if tq < TC else n0 // T
                    self.dma(xt2[:, :, :], self.XTv[:, :, n0:n0 + 128], self.dr("XT", n0, 128), [xt2])
                    for m in range(8):
                        self.stt(V, xt2[:, m, :], yacc[:, m, p_ * 128:(p_ + 1) * 128], self.MOD[l][:, 40 + m, r:r + 1], xt2[:, m, :], ALU.mult, ALU.add,
                                 [yacc, self.MOD[l], xt2], [xt2])
                    self.dma(self.XTv[:, :, n0:n0 + 128], xt2[:, :, :], [xt2], self.dr("XT", n0, 128))
            self.barrier()

    def final(self):
        V, G = self.dve, self.pool
        ps = self.ps
        with ExitStack() as es:
            def t(shape, dt=F32):
                return self.sb(shape, dt, es)
            xts = [t([128, 8, 128]) for _ in range(2)]
            sq = t([128, 8, 128])
            rs = t([128, 128])
            tmp = t([128, 8, 128])
            os_ = [t([128, 1024]) for _ in range(2)]
            i = 0
            for s in range(S):
                for j in range(2, NJ):
                    n0 = s * T + j * 128
                    xt = xts[i % 2]; o = os_[i % 2]; i += 1
                    self.dma(xt[:, :, :], self.XTv[:, :, n0:n0 + 128], self.dr("XT", n0, 128), [xt])
                    self.actf(sq[:, :, :], xt[:, :, :], AF.Square, [xt], [sq])
                    for c in range(8):
                        self.mm(ps[0][:, 0:128], self.ones[:, :], sq[:, c, :], c == 0, c == 7, [self.ones, sq], [ps[0]])
                    self.ts(V, rs[:, :], ps[0][:, 0:128], 1.0 / D, EPS, ALU.mult, ALU.add, [ps[0]], [rs])
                    self.actf(rs[:, :], rs[:, :], AF.Sqrt, [rs], [rs])
                    self.recip(rs[:, :], rs[:, :], [rs], [rs])
                    for c in range(8):
                        self.stt(V, tmp[:, c, :], xt[:, c, :], self.pfv(0, "norm_final", c), rs[:, :], ALU.mult, ALU.mult, [xt, rs, self.PFt[0]], [tmp])
                    for c in range(8):
                        pb = ps[1 + c // 4]
                        self.tr(pb[:, (c % 4) * 128:(c % 4 + 1) * 128], tmp[:, c, :], self.ident[:, :], [tmp, self.ident], [pb])
                    self.cp(self.act, o[:, 0:512], ps[1][:, :], [ps[1]], [o])
                    self.cp(V, o[:, 512:1024], ps[2][:, :], [ps[2]], [o])
                    self.dma(self.y[s, (j - 2) * 128:(j - 1) * 128, :], o[:, :], [o], [self.dd("y", s, j)])
            self.barrier()

    def rwkv_scan(self, l, gdn=False):
        V, G = self.dve, self.pool
        ps = self.ps
        nh = 4 if gdn else 8
        dv = 512 // nh
        DKQ, DMAT, DBC, DV_, DPC, DY, nm = ((self.GKQ, self.GMAT, self.GBC, self.GV, self.GPC, self.YB, 'G') if gdn else (self.RKQ, self.RMAT, self.RBC, self.RV, self.RPC, self.YA, 'R'))
        with ExitStack() as es:
            def t(shape, dt=F32):
                return self.sb(shape, dt, es)
            chains = [(s, d) for s in range(S) for d in range(2)]
            order = {0: list(range(NJ)), 1: [1, 0] + list(range(NJ - 1, 1, -1))}
            bufs = []
            for _ in chains:
                ld = [(t([128, 4, 256], BF16), t([128, nh, 512], BF16), t([128, 1024], BF16), t([128, 512], BF16), t([128, 8])) for _ in range(2)]
                bufs.append(dict(ld=ld, H=t([128, 4, dv]), Hz=t([128, nh, dv], BF16), Rn=t([128, nh, dv], BF16),
                                 Ubz=[t([128, nh, dv], BF16) for _ in range(2)], Vz=[t([128, 512], BF16) for _ in range(2)],
                                 Yt=t([128, 512])))
            for i in range(NJ):
                for ci, (s, d) in enumerate(chains):
                    j = order[d][i]
                    n0 = s * T + j * 128
                    b = bufs[ci]
                    KQ, MAT, BC, Vt, pc = b["ld"][i % 2]
                    H, Hz, Rn, Ubz, Vz, Yt = b["H"], b["Hz"], b["Rn"], b["Ubz"], b["Vz"], b["Yt"]
                    self.dma(KQ[:, :, :].rearrange("p m t -> p (m t)"), DKQ[s, d, j], [self.dd(nm + "KQ", s, d, j)], [KQ])
                    self.dma(MAT[:, :, :].rearrange("p h t -> p (h t)"), DMAT[s, d, j], [self.dd(nm + "MAT", s, d, j)], [MAT])
                    self.dma(BC[:, :], DBC[s, d, j], [self.dd(nm + "BC", s, d, j)], [BC])
                    self.dma(Vt[:, :], DV_[s, j], [self.dd(nm + "V", s, j)], [Vt])
                    self.dma(pc[:, :], DPC[s, d, j], [self.dd(nm + "PC", s, d, j)], [pc])
                    if i == 0:
                        self.memset(G, H[:, :, :], 0.0, [H])
                        self.memset(G, Hz[:, :, :], 0.0, [Hz])
                        self.memset(G, Rn[:, :, :], 0.0, [Rn])
                        for c in range(2):
                            self.memset(G, Ubz[c][:, :, :], 0.0, [Ubz[c]])
                            self.memset(G, Vz[c][:, :], 0.0, [Vz[c]])
                    for c in range(2):
                        self.cp(G, Vz[c][c * 64:c * 64 + 64, :], Vt[c * 64:c * 64 + 64, :], [Vt], [Vz[c]])
                    pA, pB = ps[2 * ci], ps[2 * ci + 1]
                    pAv = pA[:, :].rearrange("p (h v) -> p h v", v=dv)
                    pBv = pB[:, :].rearrange("p (h v) -> p h v", v=dv)
                    if not gdn:
                        pBe = pB[:, :].rearrange("p (m e v) -> p m e v", e=2, v=64)
                        Hze = Hz[:, :, :].rearrange("p (m e) v -> p m e v", e=2)
                    pcv = pc[:, :].rearrange("p (m c) -> p m c", c=2)
                    for c in ([0, 1] if d == 0 else [1, 0]):
                        cs = slice(c * 64, c * 64 + 64)
                        Ub = Ubz[c]
                        for h in range(nh):
                            m = h if gdn else h // 2
                            self.mm(pAv[:, h, :], KQ[:, m, 0:128], Hz[:, h, :], True, False, [KQ, Hz], [pA])
                            self.mm(pAv[:, h, :], MAT[:, h, 256:384], Vt[:, h * dv:(h + 1) * dv], False, True, [MAT, Vt], [pA])
                        self.ts(V, Rn[cs, :, :], pAv[cs, :, :], -1.0, None, ALU.mult, None, [pA], [Rn])
                        for h in range(nh):
                            self.mm(pBv[:, h, :], MAT[:, h, 0:128], Rn[:, h, :], True, True, [MAT, Rn], [pB])
                        self.cp(self.act, Ub[cs, :, :], pBv[cs, :, :], [pB], [Ub])
                        for h in range(nh):
                            m = h if gdn else h // 2
                            self.mm(pAv[:, h, :], KQ[:, m, 128:256], Hz[:, h, :], True, False, [KQ, Hz], [pA])
                            self.mm(pAv[:, h, :], MAT[:, h, 128:256], Ub[:, h, :], False, False, [MAT, Ub], [pA])
                            self.mm(pAv[:, h, :], MAT[:, h, 384:512], Vt[:, h * dv:(h + 1) * dv], False, True, [MAT, Vt], [pA])
                        self.cp(V, Yt[cs, :], pA[cs, :], [pA], [Yt])
                        for h in range(nh):
                            m = h if gdn else h // 2
                            self.mm(pBv[:, h, :], BC[:, m * 128:(m + 1) * 128], Ub[:, h, :], True, False, [BC, Ub], [pB])
                            self.mm(pBv[:, h, :], BC[:, 512 + m * 128:512 + (m + 1) * 128], Vz[c][:, h * dv:(h + 1) * dv], False, True, [BC, Vz[c]], [pB])
                        if gdn:
                            self.tt(V, H[:, :, :], H[:, :, :], pcv[:, :, c:c + 1].to_broadcast([128, 4, dv]), ALU.mult, [H, pc], [H])
                            self.tt(V, H[:, :, :], H[:, :, :], pBv[:, :, :], ALU.add, [H, pB], [H])
                            self.cp(self.act, Hz[:, :, :], H[:, :, :], [H], [Hz])
                        else:
                            for e in range(2):
                                rows = slice(e * 64, e * 64 + 64)
                                self.tt(V, H[rows, :, :], H[rows, :, :], pBe[rows, :, e, :], ALU.add, [H, pB], [H])
                            self.tt(V, H[:, :, :], H[:, :, :], pcv[:, :, c:c + 1].to_broadcast([128, 4, 64]), ALU.mult, [H, pc], [H])
                            for e in range(2):
                                rows = slice(e * 64, e * 64 + 64)
                                self.cp(self.act, Hze[rows, :, e, :], H[rows, :, :], [H], [Hz])
                    self.dma(DY[d, n0:n0 + 128, :], Yt[:, :], [Yt], [self.dd(nm + "Y", d, n0 // 128)])
            self.barrier()

    def build(self):
        try:
            self.build_()
        except StopBuild:
            self.es2 = None
            self.finish()

    def build_(self):
        self.consts()
        self.gdn_setup()
        self.conf_setup()
        self.phase0()
        if self.stop == "0":
            return self.finish()
        for l in range(self.nlayers):
            self.phaseA(l)
            if self.stop == f"A{l}":
                return self.finish()
            self.phaseB(l)
            if self.stop == f"B{l}":
                return self.finish()
            self.rwkv_prep(l)
            if self.stop == f"C{l}":
                return self.finish()
            self.rwkv_scan(l)
            if self.stop == f"D{l}":
                return self.finish()
            self.gdn_prep(l)
            if self.stop == f"E{l}":
                return self.finish()
            self.rwkv_scan(l, gdn=True)
            if self.stop == f"F{l}":
                return self.finish()
            self.conformer(l)
            if self.stop == f"G{l}":
                return self.finish()
            self.merge(l)
            if self.stop == f"H{l}":
                return self.finish()
            self.moe(l)
            if self.stop == f"I{l}":
                return self.finish()
        self.final()
        self.finish()
```

```python
import threading
import numpy as np
from contextlib import ExitStack
import concourse.bass as bass
import concourse.mybir as mybir
from concourse.bass_utils import run_bass_kernel_spmd

F32 = mybir.dt.float32
BF16 = mybir.dt.bfloat16
ALU = mybir.AluOpType
AF = mybir.ActivationFunctionType
AX = mybir.AxisListType

D = 1024
S = 2
TC = 256
TL = 2048
T = TC + TL
NT = S * T
L = 2
NIN = 8080
NDS = 24
NDS_SW = 8


class Dep:
    __slots__ = ("w", "r")

    def __init__(self):
        self.w = None
        self.r = {}


class Tl:
    def __init__(self, t):
        self.t = t
        self.d = Dep()

    def __getitem__(self, k):
        return self.t[k]


class Eng:
    def __init__(self, name, be, sem):
        self.key = name
        self.be = be
        self.sem = sem
        self.n = 0
        self.waited = {}


def _d(x):
    return x.d if hasattr(x, "d") else x


class KB:
    def __init__(self, dbg=()):
        self.nc = nc = bass.Bass("TRN2", target_bir_lowering=False)
        self.es = ExitStack()
        self.dbg = set(dbg)
        e = self.es.enter_context
        self.pe = Eng("pe", nc.tensor, e(nc.semaphore("s_pe")))
        self.act = Eng("act", nc.scalar, e(nc.semaphore("s_act")))
        self.dve = Eng("dve", nc.vector, e(nc.semaphore("s_dve")))
        self.pool = Eng("pool", nc.gpsimd, e(nc.semaphore("s_pool")))
        self.sp = Eng("sp", nc.sync, e(nc.semaphore("s_sp")))
        self.engs = [self.pe, self.act, self.dve, self.pool, self.sp]
        self.dsem = [e(nc.semaphore(f"s_d{i}")) for i in range(NDS + NDS_SW)]
        self.dcnt = [0] * (NDS + NDS_SW)
        self.drr = 0
        self.drr_sw = 0
        self.ddeps = {}
        self.ntile = 0
        self.yielders = {}

    def sb(self, shape, dt=F32, es=None):
        self.ntile += 1
        t = (es or self.es).enter_context(self.nc.sbuf_tensor(f"t{self.ntile}", list(shape), dt))
        return Tl(t)

    def psum(self, shape, dt=F32, es=None):
        self.ntile += 1
        t = (es or self.es).enter_context(self.nc.psum_tensor(f"p{self.ntile}", list(shape), dt))
        return Tl(t)

    def dram(self, name, shape, dt=F32, kind=None):
        if kind is None:
            kind = "ExternalOutput" if name in self.dbg else "Internal"
        return self.nc.dram_tensor(name, list(shape), dt, kind=kind).ap()

    def dd(self, *key):
        d = self.ddeps.get(key)
        if d is None:
            d = self.ddeps[key] = Dep()
        return d

    def dr(self, name, n0, w):
        return [self.dd(name, i) for i in range(n0 // 128, (n0 + w + 127) // 128)]

    def _sync(self, E, R, W):
        need = {}

        def upd(tok):
            k, sem, val = tok
            if k not in need or need[k][1] < val:
                need[k] = (sem, val)

        for d in R:
            d = _d(d)
            if d.w:
                upd(d.w)
        for d in W:
            d = _d(d)
            if d.w:
                upd(d.w)
            for k, (sem, val) in d.r.items():
                if k != E.key:
                    upd((k, sem, val))
        for k, (sem, val) in need.items():
            if k == E.key and E is self.pe:
                continue
            if E.waited.get(k, 0) < val:
                E.be.wait_ge(sem, val)
                E.waited[k] = val

    def _mark(self, tok, R, W):
        k, sem, val = tok
        for d in R:
            _d(d).r[k] = (sem, val)
        for d in W:
            d = _d(d)
            d.w = tok
            d.r = {}

    def op(self, E, fn, R, W):
        self._sync(E, R, W)
        ins = fn()
        E.n += 1
        ins.then_inc(E.sem, 1)
        self._mark((E.key, E.sem, E.n), R, W)
        self._yield()

    def dma(self, out, in_, R, W, Q=None, **kw):
        Q = Q or self.sp
        self._sync(Q, R, W)
        sw = Q is self.pool
        if sw:
            s = NDS + self.drr_sw
            self.drr_sw = (self.drr_sw + 1) % NDS_SW
        else:
            s = self.drr
            self.drr = (s + 1) % NDS
        sem = self.dsem[s]
        k = ("d", s)
        if self.dcnt[s] > 0 and Q.waited.get(k, 0) < 16 * self.dcnt[s]:
            Q.be.wait_ge(sem, 16 * self.dcnt[s])
            Q.waited[k] = 16 * self.dcnt[s]
        Q.be.dma_start(out=out, in_=in_, **kw).then_inc(sem, 16)
        self.dcnt[s] += 1
        self._mark((k, sem, 16 * self.dcnt[s]), R, W)
        self._yield()

    def _yield(self):
        if self.yielders:
            y = self.yielders.get(threading.get_ident())
            if y:
                y()

    def run_interleaved(self, fns):
        n = len(fns)
        state = {"turn": 0, "done": [False] * n, "exc": None}
        cv = threading.Condition()

        def advance(i):
            for k in range(1, n + 1):
                nx = (i + k) % n
                if not state["done"][nx]:
                    state["turn"] = nx
                    break
            else:
                state["turn"] = -1
            cv.notify_all()

        def yielder(i):
            def y():
                with cv:
                    advance(i)
                    while state["turn"] != i:
                        cv.wait()
            return y

        def worker(i):
            with cv:
                while state["turn"] != i:
                    cv.wait()
            self.yielders[threading.get_ident()] = yielder(i)
            try:
                fns[i]()
            except BaseException as e:
                state["exc"] = e
            finally:
                self.yielders.pop(threading.get_ident(), None)
                with cv:
                    state["done"][i] = True
                    advance(i)

        ths = [threading.Thread(target=worker, args=(i,)) for i in range(n)]
        for th in ths:
            th.start()
        for th in ths:
            th.join()
        if state["exc"] is not None:
            raise state["exc"]

    def barrier(self):
        for E in self.engs:
            for E2 in self.engs:
                if E2 is not E and E2.n > 0 and E.waited.get(E2.key, 0) < E2.n:
                    E.be.wait_ge(E2.sem, E2.n)
                    E.waited[E2.key] = E2.n
            for s in range(NDS + NDS_SW):
                k = ("d", s)
                if self.dcnt[s] > 0 and E.waited.get(k, 0) < 16 * self.dcnt[s]:
                    E.be.wait_ge(self.dsem[s], 16 * self.dcnt[s])
                    E.waited[k] = 16 * self.dcnt[s]

    def mm(self, out, lhsT, rhs, start, stop, R, W):
        self.op(self.pe, lambda: self.nc.tensor.matmul(out, lhsT=lhsT, rhs=rhs, start=start, stop=stop), R, W)

    def tr(self, out, in_, ident, R, W):
        self.op(self.pe, lambda: self.nc.tensor.transpose(out, in_, ident), R, W)

    def actf(self, out, in_, func, R, W, bias=None, scale=None):
        kw = {}
        if bias is not None:
            kw["bias"] = bias
        if scale is not None:
            kw["scale"] = scale
        self.op(self.act, lambda: self.nc.scalar.activation(out=out, in_=in_, func=func, **kw), R, W)

    def ts(self, E, out, in0, s1, s2, op0, op1, R, W):
        if op1 is None:
            self.op(E, lambda: E.be.tensor_scalar(out=out, in0=in0, scalar1=s1, scalar2=None, op0=op0), R, W)
        else:
            self.op(E, lambda: E.be.tensor_scalar(out=out, in0=in0, scalar1=s1, scalar2=s2, op0=op0, op1=op1), R, W)

    def tt(self, E, out, in0, in1, op, R, W):
        self.op(E, lambda: E.be.tensor_tensor(out=out, in0=in0, in1=in1, op=op), R, W)

    def stt(self, E, out, in0, scalar, in1, op0, op1, R, W):
        self.op(E, lambda: E.be.scalar_tensor_tensor(out=out, in0=in0, scalar=scalar, in1=in1, op0=op0, op1=op1), R, W)

    def cp(self, E, out, in_, R, W):
        if E is self.act:
            self.op(E, lambda: self.nc.scalar.copy(out=out, in_=in_), R, W)
        else:
            self.op(E, lambda: E.be.tensor_copy(out=out, in_=in_), R, W)

    def recip(self, out, in_, R, W):
        self.op(self.dve, lambda: self.nc.vector.reciprocal(out=out, in_=in_), R, W)

    def memset(self, E, ap, v, W):
        self.op(E, lambda: E.be.memset(ap, v), [], W)


class PF:
    def __init__(self):
        self.cols = {}
        self.n = 0

    def add(self, name, nch):
        self.cols[name] = (self.n, nch)
        self.n += nch
        return self.cols[name][0]


def pf_layout():
    pf = PF()
    for nm, nch in [("norm_mix", 8), ("norm_ffn", 8), ("b_ada", 48), ("mu0", 15), ("mu1", 15),
                    ("w0_0", 4), ("w0_1", 4), ("a0_0", 4), ("a0_1", 4), ("kk", 4), ("ka", 4), ("rk", 4),
                    ("ln_g", 4), ("ln_b", 4), ("gconv", 60), ("gnorm", 1), ("alog", 1), ("dtb", 1),
                    ("cdw", 124), ("cdwb", 4), ("clng", 4), ("clnb", 4), ("norm_final", 8)]:
        pf.add(nm, nch)
    return pf


def _fm(v, nch):
    return np.ascontiguousarray(np.asarray(v, np.float32).reshape(nch, 128).T)


def pack_pf(inp, l):
    pf = pf_layout()
    out = np.zeros((128, pf.n), np.float32)

    def put(nm, arr):
        o, n = pf.cols[nm]
        out[:, o:o + n] = arr

    put("norm_mix", _fm(inp["norm_mix"][l], 8))
    put("norm_ffn", _fm(inp["norm_ffn"][l], 8))
    put("b_ada", _fm(inp["b_ada"][l], 48))
    put("mu0", _fm(inp["rwkv_mu"][l, 0], 15))
    put("mu1", _fm(inp["rwkv_mu"][l, 1], 15))
    for d in range(2):
        put(f"w0_{d}", _fm(inp["rwkv_w0"][l, d], 4))
        put(f"a0_{d}", _fm(inp["rwkv_a0"][l, d], 4))
    put("kk", _fm(inp["rwkv_kk"][l], 4))
    put("ka", _fm(inp["rwkv_ka"][l], 4))
    put("rk", _fm(inp["rwkv_rk"][l].reshape(-1), 4))
    put("ln_g", _fm(inp["rwkv_ln_g"][l], 4))
    put("ln_b", _fm(inp["rwkv_ln_b"][l], 4))
    gc = np.concatenate([_fm(inp["gdn_conv"][l, k], 12) for k in range(5)], axis=1)
    put("gconv", gc)
    put("gnorm", _fm(inp["gdn_norm"][l], 1))
    al = np.zeros((128, 1), np.float32); al[0:8, 0] = np.asarray(inp["gdn_A_log"][l]).reshape(-1)
    db = np.zeros((128, 1), np.float32); db[0:8, 0] = np.asarray(inp["gdn_dt_bias"][l]).reshape(-1)
    put("alog", al)
    put("dtb", db)
    cd = np.concatenate([_fm(inp["conf_dw"][l, k], 4) for k in range(31)], axis=1)
    put("cdw", cd)
    put("cdwb", _fm(inp["conf_dw_b"][l], 4))
    put("clng", _fm(inp["conf_ln_g"][l], 4))
    put("clnb", _fm(inp["conf_ln_b"][l], 4))
    put("norm_final", _fm(inp["norm_final"], 8))
    return out


EPS = 1e-6


class Prog(KB):
    def __init__(self, dbg=(), stop=None, nlayers=L):
        super().__init__(dbg)
        self.stop = stop
        self.nlayers = nlayers
        nc = self.nc
        self.pf = pf_layout()

        def inp(name, shape):
            return nc.dram_tensor(name, list(shape), F32, kind="ExternalInput").ap()

        self.x = inp("x", [S, TL, D])
        self.ctx = inp("ctx", [S, TC, D])
        self.cvecT = inp("cvecT", [D, 3])
        self.pfp = inp("pfp", [L, 128, self.pf.n])
        self.w_ada = inp("w_ada", [L, D, 6 * D])
        self.w_in = inp("w_in", [L, D, NIN])
        self.rw2 = inp("rwkv_w2", [L, 2, 64, 512])
        self.ra2 = inp("rwkv_a2", [L, 2, 64, 512])
        self.rg2 = inp("rwkv_g2", [L, 128, 512])
        self.w_branch = inp("w_branch", [L, 3, 512, D])
        self.w_out = inp("w_out", [L, D, D])
        self.w_router = inp("w_router", [D, 16])
        self.rbias = inp("rbias", [128, 16])
        self.weg = inp("w_e_gate", [L, 16, D, 512])
        self.weu = inp("w_e_up", [L, 16, D, 512])
        self.wed = inp("w_e_down", [L, 16, 512, D])
        self.y = nc.dram_tensor("y", [S, TL, D], F32, kind="ExternalOutput").ap()
        self.XT = self.dram("XT", [D, NT])
        self.XTv = self.XT.rearrange("(c p) n -> p c n", p=128)
        self.PT = self.dram("PT", [NIN, NT])
        self.ps = [self.psum([128, 512], F32) for _ in range(8)]
        self.ident = self.sb([128, 128], F32)
        self.identb = self.sb([128, 128], BF16)
        self.ones = self.sb([128, 128], F32)
        self.PFt = [self.sb([128, self.pf.n], F32) for _ in range(L)]
        self.scT = self.sb([128, 8, 3], F32)
        self.MOD = [self.sb([128, 48, 3], F32) for _ in range(L)]
        self.GM = [self.sb([128, 8, 3], F32) for _ in range(L)]
        self.GF = [self.sb([128, 8, 3], F32) for _ in range(L)]

    def pfv(self, l, name, c=None, n=1):
        o, nch = self.pf.cols[name]
        if c is None:
            return self.PFt[l][:, o:o + nch]
        return self.PFt[l][:, o + c:o + c + n]

    def consts(self):
        nc = self.nc
        P = self.pool
        self.memset(P, self.ident[:, :], 0.0, [self.ident])
        self.op(P, lambda: nc.gpsimd.affine_select(out=self.ident[:, :], in_=self.ident[:, :], pattern=[[-1, 128]],
                                                   compare_op=ALU.not_equal, fill=1.0, base=0, channel_multiplier=1),
                [self.ident], [self.ident])
        self.cp(P, self.identb[:, :], self.ident[:, :], [self.ident], [self.identb])
        self.memset(P, self.ones[:, :], 1.0, [self.ones])
        for l in range(L):
            self.dma(self.PFt[l][:, :], self.pfp[l], [], [self.PFt[l]])
        cv = self.sb([128, 8, 3], F32)
        self.dma(cv[:, :, :], self.cvecT.rearrange("(c p) r -> p c r", p=128), [], [cv])
        self.actf(self.scT[:, :, :], cv[:, :, :], AF.Silu, [cv], [self.scT])

    def phase0(self):
        with ExitStack() as es:
            xin = [self.sb([128, 1024], F32, es) for _ in range(2)]
            xo = [self.sb([128, 8, 128], F32, es) for _ in range(2)]
            i = 0
            for s in range(S):
                for j in range(T // 128):
                    n0 = s * T + j * 128
                    src = self.ctx[s, j * 128:(j + 1) * 128, :] if j < 2 else self.x[s, (j - 2) * 128:(j - 1) * 128, :]
                    a = xin[i % 2]
                    o = xo[i % 2]
                    self.dma(a[:, :], src, [], [a])
                    for c in range(8):
                        pb = self.ps[(i % 2) * 2 + c // 4]
                        self.tr(pb[:, (c % 4) * 128:(c % 4 + 1) * 128], a[:, c * 128:(c + 1) * 128], self.ident[:, :],
                                [a, self.ident], [pb])
                    for hf in range(2):
                        pb = self.ps[(i % 2) * 2 + hf]
                        self.cp(self.act if hf == 0 else self.dve, o[:, hf * 4:(hf + 1) * 4, :],
                                pb[:, :].rearrange("p (c t) -> p c t", c=4), [pb], [o])
                    self.dma(self.XTv[:, :, n0:n0 + 128], o[:, :, :], [o], self.dr("XT", n0, 128))
                    i += 1
            self.barrier()

    def phaseA(self, l):
        with ExitStack() as es:
            wa = [self.sb([128, 8, 768], F32, es) for _ in range(2)]
            pm = self.ps[0]
            wav = self.w_ada[l].rearrange("(k p) n -> p k n", p=128)
            for mg in range(8):
                w = wa[mg % 2]
                for q in range(4):
                    self.dma(w[:, q * 2:(q + 1) * 2, :], wav[:, q * 2:(q + 1) * 2, mg * 768:(mg + 1) * 768], [], [w])
                for m in range(6):
                    mm_ = mg * 6 + m
                    for k in range(8):
                        self.mm(pm[:, mm_ * 3:(mm_ + 1) * 3], w[:, k, m * 128:(m + 1) * 128], self.scT[:, k, :], k == 0, k == 7,
                                [w, self.scT], [pm])
            mod = self.MOD[l]
            self.tt(self.dve, mod[:, :, :], pm[:, 0:144].rearrange("p (m r) -> p m r", r=3),
                    self.pfv(l, "b_ada").unsqueeze(2).to_broadcast([128, 48, 3]), ALU.add, [pm, self.PFt[l]], [mod])
            for (G, mi, nm) in ((self.GM[l], 1, "norm_mix"), (self.GF[l], 4, "norm_ffn")):
                self.ts(self.dve, G[:, :, :], mod[:, mi * 8:(mi + 1) * 8, :], 1.0, None, ALU.add, None, [mod], [G])
                self.dump(f"G1{l}{mi}", G, G[:, :, :], [128, 8, 3])
                self.tt(self.dve, G[:, :, :], G[:, :, :], self.pfv(l, nm).unsqueeze(2).to_broadcast([128, 8, 3]), ALU.mult,
                        [G, self.PFt[l]], [G])
            self.dump(f"MOD{l}", mod, mod[:, :, :], [128, 48, 3])
            self.dump(f"GM{l}", self.GM[l], self.GM[l][:, :, :], [128, 8, 3])
            self.barrier()

    def tiles(self):
        out = []
        for s in range(S):
            out.append((s * T, TC, 2))
            for j in range(TL // 512):
                out.append((s * T + TC + j * 512, 512, s))
        return out

    def modulate_tile(self, es_bufs, n0, w, r, G, shift_col, mod, out_fn):
        xt, sq, rs, tmp = es_bufs
        psS = self.ps[7]
        self.dma(xt[:, :, :w], self.XTv[:, :, n0:n0 + w], self.dr("XT", n0, w), [xt])
        self.actf(sq[:, :, :w], xt[:, :, :w], AF.Square, [xt], [sq])
        for c in range(8):
            self.mm(psS[:, :w], self.ones[:, :], sq[:, c, :w], c == 0, c == 7, [self.ones, sq], [psS])
        self.ts(self.dve, rs[:, :w], psS[:, :w], 1.0 / D, EPS, ALU.mult, ALU.add, [psS], [rs])
        self.actf(rs[:, :w], rs[:, :w], AF.Sqrt, [rs], [rs])
        self.recip(rs[:, :w], rs[:, :w], [rs], [rs])
        return xt, rs

    def phaseB(self, l):
        with ExitStack() as es:
            hT = self.sb([128, 8, NT], BF16, es)
            with ExitStack() as es1:
                xts = [self.sb([128, 8, 512], F32, es1) for _ in range(2)]
                sq = self.sb([128, 8, 512], F32, es1)
                rs = self.sb([128, 512], F32, es1)
                tmps = [self.sb([128, 512], F32, es1) for _ in range(2)]
                for i, (n0, w, r) in enumerate(self.tiles()):
                    xt, _ = self.modulate_tile((xts[i % 2], sq, rs, None), n0, w, r, None, None, None, None)
                    for c in range(8):
                        tmp = tmps[c % 2]
                        self.stt(self.dve, tmp[:, :w], xt[:, c, :w], self.GM[l][:, c, r:r + 1], rs[:, :w], ALU.mult, ALU.mult,
                                 [xt, rs, self.GM[l]], [tmp])
                        self.actf(hT[:, c, n0:n0 + w], tmp[:, :w], AF.Identity, [tmp, self.MOD[l]], [hT],
                                  bias=self.MOD[l][:, c, r:r + 1], scale=1.0)
                if "HT" in self.dbg:
                    hd = self.dram("HT", [128, 8, NT], BF16)
                    self.dma(hd, hT[:, :, :], [hT], [self.dd("HTd")])
                self.barrier()
            wbf = [self.sb([128, 8, 1024], BF16, es) for _ in range(2)]
            ost = [self.sb([128, 512], F32, es) for _ in range(4)]
            no = 0
            def loadw(g):
                c0 = g * 1024
                cw = min(1024, NIN - c0)
                self.dma(wbf[g % 2][:, :, :cw], self.w_in[l][:, c0:c0 + cw].rearrange("(k p) n -> p k n", p=128), [], [wbf[g % 2]], Q=self.pool)
            loadw(0)
            for g in range(8):
                c0 = g * 1024
                cw = min(1024, NIN - c0)
                wb = wbf[g % 2]
                if g < 7:
                    loadw(g + 1)
                nm = (cw + 127) // 128
                for tt_ in range(NT // 512):
                    n0 = tt_ * 512
                    for m in range(nm):
                        mw = min(128, cw - m * 128)
                        pb = self.ps[no % 6]
                        for k in range(8):
                            self.mm(pb[:mw, :], wb[:, k, m * 128:m * 128 + mw], hT[:, k, n0:n0 + 512], k == 0, k == 7,
                                    [wb, hT], [pb])
                        o = ost[no % 4]
                        self.cp(self.act if no % 2 == 0 else self.dve, o[:mw, :], pb[:mw, :], [pb], [o])
                        self.dma(self.PT[c0 + m * 128:c0 + m * 128 + mw, n0:n0 + 512], o[:mw, :], [o],
                                 [self.dd("PT", (c0 + m * 128) // 128, j) for j in range(n0 // 128, n0 // 128 + 4)])
                        no += 1
            self.barrier()

    def dump(self, name, tile, ap, shape, dt=F32):
        if name in self.dbg:
            d = self.dram(name, shape, dt)
            self.dma(d, ap, [tile], [self.dd(name)])

    def finish(self):
        self.barrier()

    def build(self):
        self.consts()
        self.phase0()
        if self.stop == "0":
            return self.finish()
        for l in range(self.nlayers):
            self.phaseA(l)
            if self.stop == f"A{l}":
                return self.finish()
            self.phaseB(l)
            if self.stop == f"B{l}":
                return self.finish()
        self.finish()


def make_in_maps(inp):
    ncores = 8
    pfp = np.stack([pack_pf(inp, l) for l in range(L)])
    rbias = np.ascontiguousarray(np.broadcast_to(np.asarray(inp["router_bias"], np.float32)[None, :], (128, 16)))
    maps = []
    for i in range(ncores):
        cv = np.stack([inp["c"][2 * i], inp["c"][2 * i + 1], inp["c_ctx"]], axis=1).astype(np.float32)
        m = {
            "x": np.ascontiguousarray(inp["x"][2 * i:2 * i + 2]),
            "ctx": np.ascontiguousarray(inp["ctx"][2 * i:2 * i + 2]),
            "cvecT": np.ascontiguousarray(cv),
            "pfp": pfp, "rbias": rbias,
        }
        for k in ("w_ada", "w_in", "rwkv_w2", "rwkv_a2", "rwkv_g2", "w_branch", "w_out", "w_router",
                  "w_e_gate", "w_e_up", "w_e_down"):
            m[k] = np.ascontiguousarray(inp[k], dtype=np.float32)
        maps.append(m)
    return maps


def kernel(**inputs):
    inp = {k: np.asarray(v) for k, v in inputs.items()}
    prog = Prog2()
    prog.build()
    maps = make_in_maps(inp)
    res = run_bass_kernel_spmd(prog.nc, maps, core_ids=list(range(8)))
    return np.concatenate([r["y"] for r in res.results], axis=0).astype(np.float32)


CDEC = 0.6065306597126334
NJ = T // 128


def seg_bounds(j):
    return (j == 0 or j == 2), (j == 1 or j == NJ - 1)


class StopBuild(Exception):
    pass


class Prog2(Prog):
    cut = None

    def ck(self, n):
        if self.cut == n:
            raise StopBuild()

    def __init__(self, **kw):
        super().__init__(**kw)
        self.PTv = self.PT[0:8064, :].rearrange("(c p) n -> p c n", p=128)
        self.RKQ = self.dram("RKQ", [S, 2, NJ, 128, 4 * 256], BF16)
        self.RMAT = self.dram("RMAT", [S, 2, NJ, 128, 8 * 512], BF16)
        self.RBC = self.dram("RBC", [S, 2, NJ, 128, 1024], BF16)
        self.RV = self.dram("RV", [S, NJ, 128, 512], BF16)
        self.RPC = self.dram("RPC", [S, 2, NJ, 128, 8], F32)
        self.GAs = self.dram("GAs", [512, NT])
        self.BON = self.dram("BON", [512, NT])
        self.YA = self.dram("YA", [2, NT, 512])
        self.bones = self.sb([128, 128], F32)
        self.MU = self.sb([128, 256], F32)
        self.ML = self.sb([128, 256], F32)
        self.RM = self.sb([128, 128], F32)

    def consts(self):
        super().consts()
        nc = self.nc
        P = self.pool
        self.memset(P, self.bones[:, :], 0.0, [self.bones])
        self.memset(P, self.bones[0:64, 0:64], 1.0, [self.bones])
        self.memset(P, self.bones[64:128, 64:128], 1.0, [self.bones])
        self.memset(P, self.RM[:, :], 1.0, [self.RM])
        self.memset(P, self.RM[:, 0:1], 0.0, [self.RM])
        self.memset(P, self.RM[:, 64:65], 0.0, [self.RM])
        for (Mt, off, cmp_, sg) in ((self.MU, 0, ALU.is_gt, 1), (self.MU, 128, ALU.is_ge, 1), (self.ML, 0, ALU.is_gt, -1), (self.ML, 128, ALU.is_ge, -1)):
            sl = Mt[:, off:off + 128]
            self.memset(P, sl, 1.0, [Mt])
            self.op(P, lambda sl=sl, cmp_=cmp_, sg=sg: nc.gpsimd.affine_select(out=sl, in_=sl, pattern=[[sg, 128]], compare_op=cmp_, fill=0.0,
                                                                              base=0, channel_multiplier=-sg), [Mt], [Mt])
            self.memset(P, Mt[0:64, off + 64:off + 128], 0.0, [Mt])
            self.memset(P, Mt[64:128, off:off + 64], 0.0, [Mt])

    def load_halo(self, dst, c0, nch, s, j, hw):
        n0 = s * T + j * 128
        lb, rb = seg_bounds(j)
        lo = 0 if lb else hw
        hi = 0 if rb else hw
        if lb:
            self.memset(self.pool, dst[:, :, 0:hw], 0.0, [dst])
        if rb:
            self.memset(self.pool, dst[:, :, 128 + hw:128 + 2 * hw], 0.0, [dst])
        deps = [self.dd("PT", c, i) for c in range(c0, c0 + nch) for i in range((n0 - lo) // 128, (n0 + 128 + hi - 1) // 128 + 1)]
        self.dma(dst[:, :, hw - lo:hw + 128 + hi], self.PTv[:, c0:c0 + nch, n0 - lo:n0 + 128 + hi], deps, [dst])

    def inverse(self, X1, NN, MAT, h, Wt, At, Bt, pA, pB_, pC):
        V, G = self.dve, self.pool
        W, A, B = Wt[0], At[0], Bt[0]
        self.tt(G, W[:, :], self.identb[:, :], X1[:, 0:128], ALU.subtract, [self.identb, X1], [W])
        self.mm(pA[:, 0:128], X1[:, 0:128], NN[:, :], True, True, [X1, NN], [pA])
        self.mm(pB_[:, 0:128], NN[:, :], X1[:, 0:128], True, True, [X1, NN], [pB_])
        self.cp(self.act, A[:, :], pA[:, 0:128], [pA], [A])
        self.cp(self.act, B[:, :], pB_[:, 0:128], [pB_], [B])
        for it in range(5):
            W2, A2, B2 = Wt[(it + 1) % 2], At[(it + 1) % 2], Bt[(it + 1) % 2]
            self.mm(pC[:, 0:128], A[:, :], W[:, :], True, True, [A, W], [pC])
            if it < 4:
                self.mm(pA[:, 0:128], B[:, :], A[:, :], True, True, [A, B], [pA])
                self.mm(pB_[:, 0:128], A[:, :], B[:, :], True, True, [A, B], [pB_])
            dstW = W2[:, :] if it < 4 else MAT[:, h, 0:128]
            self.tt(V, dstW, W[:, :], pC[:, 0:128], ALU.add, [W, pC], [W2 if it < 4 else MAT])
            if it < 4:
                self.cp(self.act, A2[:, :], pA[:, 0:128], [pA], [A2])
                self.cp(self.act, B2[:, :], pB_[:, 0:128], [pB_], [B2])
            W, A, B = W2, A2, B2

    def inverse_batch(self, X1a, NNa, MAT, h0, banks, Wt, At, Bt):
        V, G = self.dve, self.pool
        psW, psA, psB = banks
        hs = slice(h0, h0 + 4)

        def reg(p, i):
            return p[:, i * 128:(i + 1) * 128]

        def bv(p):
            return p[:, :].rearrange("p (h t) -> p h t", h=4)
        W, A, B = Wt[0], At[0], Bt[0]
        self.tt(G, W[:, :, :], self.identb[:, :].unsqueeze(1).to_broadcast([128, 4, 128]), X1a[:, hs, 0:128], ALU.subtract, [self.identb, X1a], [W])
        for i in range(4):
            self.mm(reg(psA, i), X1a[:, h0 + i, 0:128], NNa[:, h0 + i, :], True, True, [X1a, NNa], [psA])
        for i in range(4):
            self.mm(reg(psB, i), NNa[:, h0 + i, :], X1a[:, h0 + i, 0:128], True, True, [X1a, NNa], [psB])
        self.cp(self.act, A[:, :, :], bv(psA), [psA], [A])
        self.cp(self.act, B[:, :, :], bv(psB), [psB], [B])
        for it in range(5):
            W2, A2, B2 = Wt[(it + 1) % 2], At[(it + 1) % 2], Bt[(it + 1) % 2]
            for i in range(4):
                self.mm(reg(psW, i), A[:, i, :], W[:, i, :], True, True, [A, W], [psW])
            if it < 4:
                for i in range(4):
                    self.mm(reg(psA, i), B[:, i, :], A[:, i, :], True, True, [A, B], [psA])
                for i in range(4):
                    self.mm(reg(psB, i), A[:, i, :], B[:, i, :], True, True, [A, B], [psB])
                self.tt(V, W2[:, :, :], W[:, :, :], bv(psW), ALU.add, [W, psW], [W2])
                self.cp(self.act, A2[:, :, :], bv(psA), [psA], [A2])
                self.cp(self.act, B2[:, :, :], bv(psB), [psB], [B2])
            else:
                self.tt(V, MAT[:, hs, 0:128], W[:, :, :], bv(psW), ALU.add, [W, psW], [MAT])
            W, A, B = W2, A2, B2

    def rwkv_prep(self, l):
        nc = self.nc
        V, G = self.dve, self.pool
        with ExitStack() as es:
            def t(shape, dt=F32):
                return self.sb(shape, dt, es)
            wtmp = t([128, 512])
            w2b, a2b, g2b = t([128, 512], BF16), t([128, 512], BF16), t([128, 512], BF16)
            for (src, dstb) in ((self.rw2[l].rearrange("d r c -> (d r) c"), w2b), (self.ra2[l].rearrange("d r c -> (d r) c"), a2b), (self.rg2[l], g2b)):
                self.dma(wtmp[:, :], src, [], [wtmp])
                self.cp(V, dstb[:, :], wtmp[:, :], [wtmp], [dstb])
            PFl = self.PFt[l]
            c0t = t([128, 15])
            self.tt(V, c0t[:, :], self.pfv(l, "mu0"), self.pfv(l, "mu1"), ALU.add, [PFl], [c0t])
            self.ts(V, c0t[:, :], c0t[:, :], -1.0, 1.0, ALU.mult, ALU.add, [c0t], [c0t])
            omka = t([128, 4])
            self.ts(V, omka[:, :], self.pfv(l, "ka"), -1.0, 1.0, ALU.mult, ALU.add, [PFl], [omka])

            def bc(ap, n):
                return ap.unsqueeze(2).to_broadcast([128, n, 128])

            def stream(s):
                pa = t([128, 15, 130])
                sh = t([128, 15, 128])
                twb, xab, sgb = t([128, 128], BF16), t([128, 128], BF16), t([128, 128], BF16)
                SW = [t([128, 4, 128]) for _ in range(2)]
                AA = [t([128, 4, 128]) for _ in range(2)]
                ga = t([128, 4, 128])
                kx, kk, tq, bon = t([128, 4, 128]), t([128, 4, 128]), t([128, 4, 128]), t([128, 4, 128])
                CS, EX, tmpa, tmpb = (t([128, 4, 128]) for _ in range(4))
                e1, e2, e3 = (t([128, 4, 128]) for _ in range(3))
                pc = t([128, 8])
                KQ = t([128, 4, 256], BF16)
                BTb, CTb = t([128, 4, 128], BF16), t([128, 4, 128], BF16)
                vb = t([128, 4, 128], BF16)
                BC = t([128, 1024], BF16)
                Vt = t([128, 512], BF16)
                MAT = t([128, 8, 512], BF16)
                X1a = t([128, 8, 256], BF16)
                NNa = t([128, 8, 128], BF16)
                BTz, CTz = t([128, 8, 128], BF16), t([128, 8, 128], BF16)
                self.memset(G, BTz[:, :, :], 0.0, [BTz])
                self.memset(G, CTz[:, :, :], 0.0, [CTz])
                Wt = [t([128, 4, 128], BF16) for _ in range(2)]
                At = [t([128, 4, 128], BF16) for _ in range(2)]
                Bt = [t([128, 4, 128], BF16) for _ in range(2)]
                Wt2 = [t([128, 4, 128], BF16) for _ in range(2)]
                At2 = [t([128, 4, 128], BF16) for _ in range(2)]
                Bt2 = [t([128, 4, 128], BF16) for _ in range(2)]
                B = self.ps[4 * s:4 * s + 4]
                for j in range(NJ):
                    n0 = s * T + j * 128
                    self.ck(1000 + s * NJ + j)
                    self.load_halo(pa, 0, 15, s, j, 1)
                    self.ck(1)
                    self.tt(V, sh[:, :, :], pa[:, :, 1:129], bc(c0t[:, :], 15), ALU.mult, [pa, c0t], [sh])
                    for c in range(15):
                        self.stt(V, sh[:, c, :], pa[:, c, 0:128], self.pfv(l, "mu0", c), sh[:, c, :], ALU.mult, ALU.add, [pa, PFl, sh], [sh])
                        self.stt(V, sh[:, c, :], pa[:, c, 2:130], self.pfv(l, "mu1", c), sh[:, c, :], ALU.mult, ALU.add, [pa, PFl, sh], [sh])
                    self.ck(2)
                    r_, k_, v_ = sh[:, 0:4, :], sh[:, 4:8, :], sh[:, 8:12, :]
                    self.actf(twb[:, :], sh[:, 12, :], AF.Tanh, [sh], [twb])
                    self.cp(self.act, xab[:, :], sh[:, 13, :], [sh], [xab])
                    self.actf(sgb[:, :], sh[:, 14, :], AF.Sigmoid, [sh], [sgb])
                    self.ck(3)
                    for d in range(2):
                        for (wb_, xin, dst, bname, pb) in ((w2b, twb, SW[d], f"w0_{d}", B[0]), (a2b, xab, AA[d], f"a0_{d}", B[1])):
                            for m in range(4):
                                self.mm(pb[:, m * 128:(m + 1) * 128], wb_[d * 64:(d + 1) * 64, m * 128:(m + 1) * 128],
                                        xin[d * 64:(d + 1) * 64, :], True, True, [wb_, xin], [pb])
                            for m in range(4):
                                self.actf(dst[:, m, :], pb[:, m * 128:(m + 1) * 128], AF.Sigmoid, [pb, PFl], [dst],
                                          bias=self.pfv(l, bname, m), scale=1.0)
                    for m in range(4):
                        self.mm(B[2][:, m * 128:(m + 1) * 128], g2b[:, m * 128:(m + 1) * 128], sgb[:, :], True, True, [g2b, sgb], [B[2]])
                    self.cp(self.act, ga[:, :, :], B[2][:, :].rearrange("p (m t) -> p m t", m=4), [B[2]], [ga])
                    self.dma(self.GAs.rearrange("(m p) n -> p m n", p=128)[:, :, n0:n0 + 128], ga[:, :, :], [ga], [self.dd("GAs", n0 // 128)])
                    self.ck(4)
                    self.tt(V, kx[:, :, :], k_, bc(self.pfv(l, "kk"), 4), ALU.mult, [sh, PFl], [kx])
                    self.tt(G, tq[:, :, :], kx[:, :, :], kx[:, :, :], ALU.mult, [kx], [tq])
                    for m in range(4):
                        self.mm(B[3][:, m * 128:(m + 1) * 128], self.bones[:, :], tq[:, m, :], True, True, [self.bones, tq], [B[3]])
                    self.ts(V, tq[:, :, :], B[3][:, :].rearrange("p (m t) -> p m t", m=4), EPS, None, ALU.add, None, [B[3]], [tq])
                    self.actf(tq[:, :, :], tq[:, :, :], AF.Sqrt, [tq], [tq])
                    self.recip(tq[:, :, :], tq[:, :, :], [tq], [tq])
                    self.tt(V, kk[:, :, :], kx[:, :, :], tq[:, :, :], ALU.mult, [kx, tq], [kk])
                    self.ck(5)
                    self.tt(G, bon[:, :, :], r_, k_, ALU.mult, [sh], [bon])
                    self.tt(G, bon[:, :, :], bon[:, :, :], bc(self.pfv(l, "rk"), 4), ALU.mult, [bon, PFl], [bon])
                    for m in range(4):
                        self.mm(B[0][:, m * 128:(m + 1) * 128], self.bones[:, :], bon[:, m, :], True, True, [self.bones, bon], [B[0]])
                    self.tt(V, bon[:, :, :], B[0][:, :].rearrange("p (m t) -> p m t", m=4), v_, ALU.mult, [B[0], sh], [bon])
                    self.dma(self.BON.rearrange("(m p) n -> p m n", p=128)[:, :, n0:n0 + 128], bon[:, :, :], [bon], [self.dd("BON", n0 // 128)])
                    self.ck(6)
                    self.cp(G, vb[:, :, :], v_, [sh], [vb])
                    pbv = B[1][:, :].bitcast(BF16)
                    for m in range(4):
                        self.tr(pbv[:, m * 128:(m + 1) * 128], vb[:, m, :], self.identb[:, :], [vb, self.identb], [B[1]])
                    self.cp(self.act, Vt[:, :], pbv[:, 0:512], [B[1]], [Vt])
                    self.dma(self.RV[s, j], Vt[:, :], [Vt], [self.dd("RV", s, j)])
                    for d in range(2):
                        self.ck(7)
                        self.tt(V, tmpa[:, :, :], AA[d][:, :, :], bc(self.pfv(l, "ka"), 4), ALU.mult, [AA[d], PFl], [tmpa])
                        self.tt(V, tmpa[:, :, :], tmpa[:, :, :], bc(omka[:, :], 4), ALU.add, [tmpa, omka], [tmpa])
                        self.tt(V, tmpa[:, :, :], tmpa[:, :, :], k_, ALU.mult, [tmpa, sh], [tmpa])
                        self.tt(G, tmpb[:, :, :], kk[:, :, :], AA[d][:, :, :], ALU.mult, [kk, AA[d]], [tmpb])
                        self.ck(8)
                        for m in range(4):
                            self.op(V, lambda m=m: nc.vector.tensor_tensor_scan(out=CS[:, m, :], data0=self.RM[:, :], data1=SW[d][:, m, :],
                                                                               initial=0.0, op0=ALU.mult, op1=ALU.add),
                                    [self.RM, SW[d]], [CS])
                        CSv = CS[:, :, :].rearrange("p m (c t) -> p m c t", t=64)
                        if d == 0:
                            self.tt(V, EX[:, :, :], CS[:, :, :], SW[d][:, :, :], ALU.subtract, [CS, SW[d]], [EX])
                            incl = CS
                        else:
                            tot = CSv[:, :, :, 63:64].to_broadcast([128, 4, 2, 64])
                            self.tt(V, EX[:, :, :].rearrange("p m (c t) -> p m c t", t=64), tot, CSv, ALU.subtract, [CS], [EX])
                            self.tt(V, e3[:, :, :], EX[:, :, :], SW[d][:, :, :], ALU.add, [EX, SW[d]], [e3])
                            incl = e3
                        self.ck(9)
                        self.actf(e1[:, :, :], EX[:, :, :], AF.Exp, [EX], [e1], scale=-CDEC)
                        self.actf(e2[:, :, :], incl[:, :, :], AF.Exp, [incl], [e2], scale=CDEC)
                        self.actf(e3[:, :, :], incl[:, :, :], AF.Exp, [incl], [e3], scale=-CDEC)
                        self.actf(pc[:, :].rearrange("p (m c) -> p m c", c=2), CSv[:, :, :, 63], AF.Exp, [CS], [pc], scale=-CDEC)
                        self.dma(self.RPC[s, d, j], pc[:, :], [pc], [self.dd("RPC", s, d, j)])
                        self.tt(V, KQ[:, :, 0:128], kk[:, :, :], e1[:, :, :], ALU.mult, [kk, e1], [KQ])
                        self.tt(G, KQ[:, :, 128:256], r_, e3[:, :, :], ALU.mult, [sh, e3], [KQ])
                        self.tt(V, BTb[:, :, :], tmpb[:, :, :], e2[:, :, :], ALU.mult, [tmpb, e2], [BTb])
                        self.tt(G, CTb[:, :, :], tmpa[:, :, :], e2[:, :, :], ALU.mult, [tmpa, e2], [CTb])
                        self.dma(self.RKQ[s, d, j], KQ[:, :, :].rearrange("p m t -> p (m t)"), [KQ], [self.dd("RKQ", s, d, j)])
                        self.ck(10)
                        pbb = B[2][:, :].bitcast(BF16)
                        for m in range(4):
                            self.tr(pbb[:, m * 128:(m + 1) * 128], BTb[:, m, :], self.identb[:, :], [BTb, self.identb], [B[2]])
                            self.tr(pbb[:, 512 + m * 128:512 + (m + 1) * 128], CTb[:, m, :], self.identb[:, :], [CTb, self.identb], [B[2]])
                        self.cp(self.act, BC[:, :], pbb[:, :], [B[2]], [BC])
                        self.dma(self.RBC[s, d, j], BC[:, :], [BC], [self.dd("RBC", s, d, j)])
                        self.ck(11)
                        Ms, Mn = (self.MU, self.ML) if d == 0 else (self.ML, self.MU)
                        for e_ in range(2):
                            rows = slice(e_ * 64, e_ * 64 + 64)
                            self.cp(G, BTz[:, :, :].rearrange("p (m e) t -> p m e t", e=2)[rows, :, e_, :], BTb[rows, :, :], [BTb], [BTz])
                            self.cp(self.act, CTz[:, :, :].rearrange("p (m e) t -> p m e t", e=2)[rows, :, e_, :], CTb[rows, :, :], [CTb], [CTz])
                        Msb = Ms[:, :].unsqueeze(1).to_broadcast([128, 2, 256])
                        v2 = lambda p: p[:, :].rearrange("p (h t) -> p h t", h=2)
                        for h in range(8):
                            self.mm(B[h // 2][:, (h % 2) * 256:(h % 2 + 1) * 256], BTz[:, h, :], KQ[:, h // 2, :], True, True, [BTz, KQ], [B[h // 2]])
                        for q in range(4):
                            self.tt(V, X1a[:, 2 * q:2 * q + 2, :], v2(B[q]), Msb, ALU.mult, [B[q], Ms], [X1a])
                        for h in range(8):
                            self.mm(B[h // 2][:, (h % 2) * 256:(h % 2 + 1) * 256], CTz[:, h, :], KQ[:, h // 2, :], True, True, [CTz, KQ], [B[h // 2]])
                        for q in range(4):
                            self.tt(V, MAT[:, 2 * q:2 * q + 2, 256:512], v2(B[q]), Msb, ALU.mult, [B[q], Ms], [MAT])
                        for h in range(8):
                            self.mm(B[h // 4][:, (h % 4) * 128:(h % 4 + 1) * 128], KQ[:, h // 2, 0:128], BTz[:, h, :], True, True, [BTz, KQ], [B[h // 4]])
                        Mnb = Mn[:, 0:128].unsqueeze(1).to_broadcast([128, 4, 128])
                        for q in range(2):
                            self.tt(V, NNa[:, 4 * q:4 * q + 4, :], B[q][:, :].rearrange("p (h t) -> p h t", h=4), Mnb, ALU.mult, [B[q], Mn], [NNa])
                        self.cp(G, MAT[:, :, 128:256], X1a[:, :, 128:256], [X1a], [MAT])
                        self.inverse_batch(X1a, NNa, MAT, 0, (B[0], B[1], B[2]), Wt, At, Bt)
                        self.inverse_batch(X1a, NNa, MAT, 4, (B[3], B[1], B[2]), Wt2, At2, Bt2)
                        self.ck(12)
                        self.dma(self.RMAT[s, d, j], MAT[:, :, :].rearrange("p h t -> p (h t)"), [MAT], [self.dd("RMAT", s, d, j)])
            self.run_interleaved([lambda: stream(0), lambda: stream(1)])
            self.barrier()

    def gdn_setup(self):
        self.GKQ = self.dram("GKQ", [S, 2, NJ, 128, 4 * 256], BF16)
        self.GMAT = self.dram("GMAT", [S, 2, NJ, 128, 4 * 512], BF16)
        self.GBC = self.dram("GBC", [S, 2, NJ, 128, 1024], BF16)
        self.GV = self.dram("GV", [S, NJ, 128, 512], BF16)
        self.GPC = self.dram("GPC", [S, 2, NJ, 128, 8], F32)
        self.YB = self.dram("YB", [2, NT, 512])
        self.SEL = self.sb([16, 16, 128], F32)
        self.selc = self.sb([128, 1], F32)
        self.onec = self.sb([128, 1], F32)
        nc = self.nc
        P = self.pool
        for i in range(16):
            self.cp(P, self.SEL[0:16, i, :], self.ident[0:16, i:i + 1].to_broadcast([16, 128]), [self.ident], [self.SEL])
        self.memset(P, self.onec[:, :], 1.0, [self.onec])
        self.memset(P, self.selc[:, :], 1.0, [self.selc])
        self.op(P, lambda: nc.gpsimd.affine_select(out=self.selc[:, :], in_=self.selc[:, :], pattern=[[0, 1]], compare_op=ALU.is_ge, fill=0.0,
                                                   base=-4, channel_multiplier=1), [self.selc], [self.selc])

    def gdn_prep(self, l):
        nc = self.nc
        V, G = self.dve, self.pool
        ps = self.ps
        with ExitStack() as es:
            def t(shape, dt=F32):
                return self.sb(shape, dt, es)
            PFl = self.PFt[l]

            def bc(ap, n):
                return ap.unsqueeze(2).to_broadcast([128, n, 128])
            negA = t([128, 1])
            self.actf(negA[:, :], self.pfv(l, "alog"), AF.Exp, [PFl], [negA])
            self.ts(V, negA[:, :], negA[:, :], -1.0, None, ALU.mult, None, [negA], [negA])
            def stream(s):
                B = self.ps[4 * s:4 * s + 4]
                qkv = t([128, 12, 132])
                cv, t2 = t([128, 12, 128]), t([128, 12, 128])
                sq = t([128, 8, 128])
                kq = t([128, 4, 256])
                kqb = t([128, 4, 256], BF16)
                kb = t([128, 4, 128], BF16)
                vb = t([128, 4, 128], BF16)
                ab, x1, Xg, SIG, gcf, gcr = (t([16, 128]) for _ in range(6))
                X2, E2 = t([16, 256]), t([16, 256])
                TOTb, Etot = t([16, 128]), t([16, 2])
                TS = t([128, 80])
                negg, sB, sC, dd_ = t([128, 8]), t([128, 8]), t([128, 8]), t([128, 8])
                gm4 = t([128, 4, 256])
                KQd = t([128, 4, 256], BF16)
                BCd = [t([128, 1024], BF16) for _ in range(2)]
                Vt = t([128, 512], BF16)
                MAT = t([128, 4, 512], BF16)
                pc = t([128, 8])
                X1a = t([128, 4, 256], BF16)
                NNa = t([128, 4, 128], BF16)
                Wt = [t([128, 4, 128], BF16) for _ in range(2)]
                At = [t([128, 4, 128], BF16) for _ in range(2)]
                Bt = [t([128, 4, 128], BF16) for _ in range(2)]
                abv = self.PT[3968:3984, :]
                for j in range(NJ):
                    n0 = s * T + j * 128
                    self.load_halo(qkv, 15, 12, s, j, 2)
                    gw = self.pfv(l, "gconv")
                    self.tt(V, cv[:, :, :], qkv[:, :, 0:128], bc(gw[:, 0:12], 12), ALU.mult, [qkv, PFl], [cv])
                    for k in range(1, 5):
                        self.tt(G, t2[:, :, :], qkv[:, :, k:k + 128], bc(gw[:, k * 12:(k + 1) * 12], 12), ALU.mult, [qkv, PFl], [t2])
                        self.tt(V, cv[:, :, :], cv[:, :, :], t2[:, :, :], ALU.add, [cv, t2], [cv])
                    self.actf(cv[:, :, :], cv[:, :, :], AF.Silu, [cv], [cv])
                    self.tt(G, sq[:, :, :], cv[:, 0:8, :], cv[:, 0:8, :], ALU.mult, [cv], [sq])
                    for c in range(8):
                        pb = B[c // 4]
                        self.mm(pb[:, (c % 4) * 128:(c % 4 + 1) * 128], self.ones[:, :], sq[:, c, :], True, True, [self.ones, sq], [pb])
                    for hf in range(2):
                        self.ts(V, sq[:, hf * 4:(hf + 1) * 4, :], B[hf][:, :].rearrange("p (c t) -> p c t", c=4), EPS, None, ALU.add, None, [B[hf]], [sq])
                    self.actf(sq[:, :, :], sq[:, :, :], AF.Sqrt, [sq], [sq])
                    self.recip(sq[:, :, :], sq[:, :, :], [sq], [sq])
                    self.tt(V, kq[:, :, 0:128], cv[:, 4:8, :], sq[:, 4:8, :], ALU.mult, [cv, sq], [kq])
                    self.stt(V, kq[:, :, 128:256], cv[:, 0:4, :], 128.0 ** -0.5, sq[:, 0:4, :], ALU.mult, ALU.mult, [cv, sq], [kq])
                    self.cp(G, kqb[:, :, :], kq[:, :, :], [kq], [kqb])
                    self.cp(G, kb[:, :, :], kq[:, :, 0:128], [kq], [kb])
                    self.cp(G, vb[:, :, :], cv[:, 8:12, :], [cv], [vb])
                    pbv = B[2][:, :].bitcast(BF16)
                    for m in range(4):
                        self.tr(pbv[:, m * 128:(m + 1) * 128], vb[:, m, :], self.identb[:, :], [vb, self.identb], [B[2]])
                    self.cp(self.act, Vt[:, :], pbv[:, 0:512], [B[2]], [Vt])
                    self.dma(self.GV[s, j], Vt[:, :], [Vt], [self.dd("GV", s, j)])
                    pbk = B[3][:, :].bitcast(BF16)
                    for m in range(4):
                        self.tr(pbk[:, m * 128:(m + 1) * 128], kb[:, m, :], self.identb[:, :], [kb, self.identb], [B[3]])
                    self.dma(ab[0:16, :], abv[:, n0:n0 + 128], [], [ab])
                    self.actf(x1[0:16, :], ab[0:16, :], AF.Exp, [ab, PFl], [x1], bias=self.pfv(l, "dtb")[0:16, :], scale=1.0)
                    self.actf(x1[0:16, :], x1[0:16, :], AF.Ln, [x1, self.onec], [x1], bias=self.onec[0:16, :], scale=1.0)
                    self.ts(V, Xg[0:16, :], x1[0:16, :], negA[0:16, :], None, ALU.mult, None, [x1, negA], [Xg])
                    self.actf(SIG[0:16, :], ab[0:16, :], AF.Sigmoid, [ab], [SIG])
                    self.op(V, lambda: nc.vector.tensor_tensor_scan(out=gcf[0:16, :], data0=self.RM[0:16, :], data1=Xg[0:16, :], initial=0.0,
                                                                    op0=ALU.mult, op1=ALU.add), [self.RM, Xg], [gcf])
                    gcfv = gcf[0:16, :].rearrange("p (c t) -> p c t", t=64)
                    totb = gcfv[:, :, 63:64].to_broadcast([16, 2, 64])
                    self.cp(V, TOTb[0:16, :].rearrange("p (c t) -> p c t", t=64), totb, [gcf], [TOTb])
                    self.tt(V, gcr[0:16, :], TOTb[0:16, :], gcf[0:16, :], ALU.subtract, [TOTb, gcf], [gcr])
                    self.tt(V, X2[0:16, 128:256], gcr[0:16, :], Xg[0:16, :], ALU.add, [gcr, Xg], [X2])
                    self.tt(V, X2[0:16, 128:256], X2[0:16, 128:256], gcf[0:16, :], ALU.subtract, [X2, gcf], [X2])
                    self.stt(V, X2[0:16, 128:256], X2[0:16, 128:256], self.selc[0:16, :], gcf[0:16, :], ALU.mult, ALU.add, [X2, self.selc, gcf], [X2])
                    self.tt(V, X2[0:16, 0:128], X2[0:16, 128:256], Xg[0:16, :], ALU.subtract, [X2, Xg], [X2])
                    self.actf(E2[0:16, :], X2[0:16, :], AF.Exp, [X2], [E2])
                    self.actf(Etot[0:16, :], gcfv[:, :, 63], AF.Exp, [gcf], [Etot])
                    pT = B[2]
                    for q_, src in enumerate((Xg[0:16, :], SIG[0:16, :], X2[0:16, 0:128], X2[0:16, 128:256], TOTb[0:16, :])):
                        self.tr(pT[:, q_ * 16:(q_ + 1) * 16], src, self.ident[0:16, 0:16], [Xg, SIG, X2, TOTb, self.ident], [pT])
                    self.cp(V, TS[:, :], pT[:, 0:80], [pT], [TS])
                    self.actf(negg[:, :], TS[:, 0:8], AF.Exp, [TS], [negg], scale=-1.0)
                    self.tt(V, dd_[:, :], TS[:, 64:72], TS[:, 32:40], ALU.subtract, [TS], [dd_])
                    self.actf(sB[:, :], dd_[:, :], AF.Exp, [dd_], [sB])
                    self.tt(V, sB[:, :], sB[:, :], TS[:, 24:32], ALU.mult, [sB, TS], [sB])
                    self.tt(V, dd_[:, :], TS[:, 64:72], TS[:, 48:56], ALU.subtract, [TS], [dd_])
                    self.actf(sC[:, :], dd_[:, :], AF.Exp, [dd_], [sC])
                    self.tt(V, sC[:, :], sC[:, :], TS[:, 24:32], ALU.mult, [sC, TS], [sC])
                    pbk4 = pbk[:, 0:512].rearrange("p (h t) -> p h t", h=4)
                    for d in range(2):
                        self.tt(V, BCd[d][:, 0:512].rearrange("p (h t) -> p h t", h=4), pbk4, sB[:, d * 4:d * 4 + 4].unsqueeze(2).to_broadcast([128, 4, 128]), ALU.mult, [B[3], sB], [BCd[d]])
                        self.tt(V, BCd[d][:, 512:1024].rearrange("p (h t) -> p h t", h=4), pbk4, sC[:, d * 4:d * 4 + 4].unsqueeze(2).to_broadcast([128, 4, 128]), ALU.mult, [B[3], sC], [BCd[d]])
                    for d in range(2):
                        Ms = self.MU if d == 0 else self.ML
                        BC = BCd[d]
                        for h in range(4):
                            i = d * 4 + h
                            self.mm(B[h // 2][:, (h % 2) * 256:(h % 2 + 1) * 256], self.SEL[0:16, i, :], X2[0:16, :], True, True, [self.SEL, X2], [B[h // 2]])
                            self.mm(B[2 + h // 2][:, (h % 2) * 256:(h % 2 + 1) * 256], self.SEL[0:16, i, :], E2[0:16, :], True, True, [self.SEL, E2], [B[2 + h // 2]])
                        v2 = lambda p: p[:, :].rearrange("p (h t) -> p h t", h=2)
                        for q in range(2):
                            self.tt(V, gm4[:, 2 * q:2 * q + 2, :], v2(B[q]), TS[:, 32 + d * 4 + 2 * q:32 + d * 4 + 2 * q + 2].unsqueeze(2).to_broadcast([128, 2, 256]),
                                    ALU.subtract, [B[q], TS], [gm4])
                            self.tt(V, KQd[:, 2 * q:2 * q + 2, :], kq[:, 2 * q:2 * q + 2, :], v2(B[2 + q]), ALU.mult, [kq, B[2 + q]], [KQd])
                        for h in range(4):
                            self.mm(B[2][:, h * 2:(h + 1) * 2], self.SEL[0:16, d * 4 + h, :], Etot[0:16, :], True, True, [self.SEL, Etot], [B[2]])
                        self.cp(self.act, pc[:, :], B[2][:, 0:8], [B[2]], [pc])
                        self.ts(G, gm4[:, :, :], gm4[:, :, :], 0.0, None, ALU.min, None, [gm4], [gm4])
                        self.actf(gm4[:, :, :], gm4[:, :, :], AF.Exp, [gm4], [gm4])
                        self.tt(G, gm4[:, :, :], gm4[:, :, :], Ms[:, :].unsqueeze(1).to_broadcast([128, 4, 256]), ALU.mult, [gm4, Ms], [gm4])
                        for h in range(4):
                            self.mm(B[h // 2][:, (h % 2) * 256:(h % 2 + 1) * 256], kb[:, h, :], kqb[:, h, :], True, True, [kb, kqb], [B[h // 2]])
                        for q in range(2):
                            self.tt(V, gm4[:, 2 * q:2 * q + 2, :], gm4[:, 2 * q:2 * q + 2, :], v2(B[q]), ALU.mult, [gm4, B[q]], [gm4])
                        self.tt(V, X1a[:, :, :], gm4[:, :, :], TS[:, 24 + d * 4:28 + d * 4].unsqueeze(2).to_broadcast([128, 4, 256]), ALU.mult, [gm4, TS], [X1a])
                        self.tt(V, MAT[:, :, 256:512], X1a[:, :, :], negg[:, d * 4:d * 4 + 4].unsqueeze(2).to_broadcast([128, 4, 256]), ALU.mult, [X1a, negg], [MAT])
                        self.cp(G, MAT[:, :, 128:256], X1a[:, :, 128:256], [X1a], [MAT])
                        pN = B[3][:, :].bitcast(BF16)
                        for h in range(4):
                            self.tr(pN[:, h * 128:(h + 1) * 128], X1a[:, h, 0:128], self.identb[:, :], [X1a, self.identb], [B[3]])
                        self.cp(self.act, NNa[:, :, :], pN[:, 0:512].rearrange("p (h t) -> p h t", h=4), [B[3]], [NNa])
                        self.inverse_batch(X1a, NNa, MAT, 0, (B[0], B[1], B[2]), Wt, At, Bt)
                        self.dma(self.GKQ[s, d, j], KQd[:, :, :].rearrange("p m t -> p (m t)"), [KQd], [self.dd("GKQ", s, d, j)])
                        self.dma(self.GMAT[s, d, j], MAT[:, :, :].rearrange("p h t -> p (h t)"), [MAT], [self.dd("GMAT", s, d, j)])
                        self.dma(self.GBC[s, d, j], BC[:, :], [BC], [self.dd("GBC", s, d, j)])
                        self.dma(self.GPC[s, d, j], pc[:, :], [pc], [self.dd("GPC", s, d, j)])
            self.run_interleaved([lambda: stream(0), lambda: stream(1)])
            self.barrier()

    def conf_setup(self):
        self.RCs = self.dram("RCs", [512, NT], BF16)
        self.RAs = self.dram("RAs", [512, NT], BF16)
        self.RBs = self.dram("RBs", [512, NT], BF16)

    def conformer(self, l):
        V, G = self.dve, self.pool
        ps = self.ps
        PFl = self.PFt[l]
        valv = self.PT[3984:3984 + 512, :].rearrange("(c p) n -> p c n", p=128)
        gatv = self.PT[4496:4496 + 512, :].rearrange("(c p) n -> p c n", p=128)
        RCv = self.RCs.rearrange("(c p) n -> p c n", p=128)
        with ExitStack() as es:
            def t(shape, dt=F32):
                return self.sb(shape, dt, es)
            u = t([128, 4, TL])
            gt = t([128, 4, TL])
            o = t([128, 4, TL])
            sq = t([128, 4, 512])
            mu, rs, var = t([128, 512]), t([128, 512]), t([128, 512])
            ob = t([128, 4, 512], BF16)
            cw = self.pfv(l, "cdw")
            for s in range(S):
                for (seg0, W_) in ((0, TC), (TC, TL)):
                    n0 = s * T + seg0
                    for c in range(4):
                        self.dma(u[:, c, :W_], valv[:, c, n0:n0 + W_], [], [u])
                        self.dma(gt[:, c, :W_], gatv[:, c, n0:n0 + W_], [], [gt])
                    self.actf(gt[:, :, :W_], gt[:, :, :W_], AF.Sigmoid, [gt], [gt])
                    self.tt(V, u[:, :, :W_], u[:, :, :W_], gt[:, :, :W_], ALU.mult, [u, gt], [u])
                    for c in range(4):
                        def wk(k):
                            return cw[:, k * 4 + c:k * 4 + c + 1]
                        self.ts(V, o[:, c, :W_], u[:, c, :W_], wk(15), None, ALU.mult, None, [u, PFl], [o])
                        for k in range(31):
                            dlt = k - 15
                            if dlt == 0:
                                continue
                            if seg0 == 0:
                                lo, hi = max(0, -dlt), min(W_, W_ - dlt)
                                self.stt(V, o[:, c, lo:hi], u[:, c, lo + dlt:hi + dlt], wk(k), o[:, c, lo:hi], ALU.mult, ALU.add, [u, PFl, o], [o])
                            elif c < 2:
                                uv = u[:, c, :].rearrange("p (r w) -> p r w", w=64)
                                ov = o[:, c, :].rearrange("p (r w) -> p r w", w=64)
                                lo, hi = max(0, -dlt), min(64, 64 - dlt)
                                self.stt(V, ov[:, :, lo:hi], uv[:, :, lo + dlt:hi + dlt], wk(k), ov[:, :, lo:hi], ALU.mult, ALU.add, [u, PFl, o], [o])
                            else:
                                uv = u[:, c, :].rearrange("p (r w) -> p r w", w=64)
                                ov = o[:, c, :].rearrange("p (r w) -> p r w", w=64)
                                lo, hi = max(0, -dlt), min(32, 32 - dlt)
                                self.stt(V, ov[:, lo:hi, :], uv[:, lo + dlt:hi + dlt, :], wk(k), ov[:, lo:hi, :], ALU.mult, ALU.add, [u, PFl, o], [o])
                        self.ts(V, o[:, c, :W_], o[:, c, :W_], self.pfv(l, "cdwb", c), None, ALU.add, None, [o, PFl], [o])
                    for t0 in range(0, W_, 512):
                        w = min(512, W_ - t0)
                        self.tt(G, sq[:, :, :w], o[:, :, t0:t0 + w], o[:, :, t0:t0 + w], ALU.mult, [o], [sq])
                        for c in range(4):
                            self.mm(ps[0][:, :w], self.ones[:, :], o[:, c, t0:t0 + w], c == 0, c == 3, [self.ones, o], [ps[0]])
                        for c in range(4):
                            self.mm(ps[1][:, :w], self.ones[:, :], sq[:, c, :w], c == 0, c == 3, [self.ones, sq], [ps[1]])
                        self.ts(V, mu[:, :w], ps[0][:, :w], 1.0 / 512, None, ALU.mult, None, [ps[0]], [mu])
                        self.tt(V, var[:, :w], mu[:, :w], mu[:, :w], ALU.mult, [mu], [var])
                        self.stt(V, var[:, :w], ps[1][:, :w], 1.0 / 512, var[:, :w], ALU.mult, ALU.subtract, [ps[1], var], [var])
                        self.ts(V, var[:, :w], var[:, :w], EPS, None, ALU.add, None, [var], [var])
                        self.actf(var[:, :w], var[:, :w], AF.Sqrt, [var], [var])
                        self.recip(rs[:, :w], var[:, :w], [var], [rs])
                        for c in range(4):
                            self.tt(V, sq[:, c, :w], o[:, c, t0:t0 + w], mu[:, :w], ALU.subtract, [o, mu], [sq])
                            self.tt(V, sq[:, c, :w], sq[:, c, :w], rs[:, :w], ALU.mult, [sq, rs], [sq])
                            self.ts(V, sq[:, c, :w], sq[:, c, :w], self.pfv(l, "clng", c), self.pfv(l, "clnb", c), ALU.mult, ALU.add, [sq, PFl], [sq])
                        self.actf(ob[:, :, :w], sq[:, :, :w], AF.Silu, [sq], [ob])
                        self.dma(RCv[:, :, n0 + t0:n0 + t0 + w], ob[:, :, :w], [ob], [self.dd("RCs", (n0 + t0) // 128)])
            self.barrier()

    def red(self, E, out, in_, op, R, W):
        self.op(E, lambda: E.be.tensor_reduce(out=out, in_=in_, axis=AX.X, op=op), R, W)

    def merge(self, l):
        V, G = self.dve, self.pool
        ps = self.ps
        PFl = self.PFt[l]
        with ExitStack() as es:
            def t(shape, dt=F32):
                return self.sb(shape, dt, es)
            wbr = t([128, 12, 1024], BF16)
            wo = t([128, 8, 1024], BF16)
            for n in range(3):
                self.dma(wbr[:, n * 4:(n + 1) * 4, :], self.w_branch[l, n].rearrange("(k p) n -> p k n", p=128), [], [wbr], Q=self.pool)
            self.dma(wo[:, :, :], self.w_out[l].rearrange("(k p) n -> p k n", p=128), [], [wo], Q=self.pool)
            pgv = [self.PT[5008 + n * 1024:5008 + (n + 1) * 1024, :].rearrange("(c p) n -> p c n", p=128) for n in range(3)]
            fm4 = lambda A: A.rearrange("(c p) n -> p c n", p=128)
            def stream(s):
                B = self.ps[4 * s:4 * s + 4]
                y0, y1, ysq = t([128, 512]), t([128, 512]), t([128, 512])
                m1, m2, m3 = t([128, 8]), t([128, 8]), t([128, 8])
                raf, bon, ga, zt = (t([128, 4, 128]) for _ in range(4))
                Rb = [t([128, 4, 128], BF16) for _ in range(3)]
                pg = t([128, 8, 128])
                macc, tmpm = t([128, 8, 128]), t([128, 8, 128])
                mb = t([128, 8, 128], BF16)
                xt = t([128, 8, 128])
                for j in range(NJ):
                    n0 = s * T + j * 128
                    r = 2 if j < 2 else s
                    for br in range(2):
                        DY, nm, nh, dvv, eps_ = ((self.YA, "R", 8, 64, 64e-5), (self.YB, "G", 4, 128, EPS))[br]
                        self.dma(y0[:, :], DY[0, n0:n0 + 128, :], [self.dd(nm + "Y", 0, n0 // 128)], [y0])
                        self.dma(y1[:, :], DY[1, n0:n0 + 128, :], [self.dd(nm + "Y", 1, n0 // 128)], [y1])
                        self.tt(V, y0[:, :], y0[:, :], y1[:, :], ALU.add, [y0, y1], [y0])
                        yv = y0[:, :].rearrange("p (h d) -> p h d", d=dvv)
                        self.tt(G, ysq[:, :], y0[:, :], y0[:, :], ALU.mult, [y0], [ysq])
                        self.red(V, m2[:, 0:nh], ysq[:, :].rearrange("p (h d) -> p h d", d=dvv), ALU.add, [ysq], [m2])
                        if br == 0:
                            self.red(V, m1[:, 0:nh], yv, ALU.add, [y0], [m1])
                            self.ts(V, m1[:, 0:nh], m1[:, 0:nh], 1.0 / dvv, None, ALU.mult, None, [m1], [m1])
                            self.tt(V, m3[:, 0:nh], m1[:, 0:nh], m1[:, 0:nh], ALU.mult, [m1], [m3])
                            self.stt(V, m2[:, 0:nh], m2[:, 0:nh], 1.0 / dvv, m3[:, 0:nh], ALU.mult, ALU.subtract, [m2, m3], [m2])
                            self.ts(V, m2[:, 0:nh], m2[:, 0:nh], eps_, None, ALU.add, None, [m2], [m2])
                            self.tt(V, yv, yv, m1[:, 0:nh].unsqueeze(2).to_broadcast([128, nh, dvv]), ALU.subtract, [y0, m1], [y0])
                        else:
                            self.ts(V, m2[:, 0:nh], m2[:, 0:nh], 1.0 / dvv, eps_, ALU.mult, ALU.add, [m2], [m2])
                        self.actf(m2[:, 0:nh], m2[:, 0:nh], AF.Sqrt, [m2], [m2])
                        self.recip(m2[:, 0:nh], m2[:, 0:nh], [m2], [m2])
                        self.tt(V, yv, yv, m2[:, 0:nh].unsqueeze(2).to_broadcast([128, nh, dvv]), ALU.mult, [y0, m2], [y0])
                        pb = B[br]
                        for c in range(4):
                            self.tr(pb[:, c * 128:(c + 1) * 128], y0[:, c * 128:(c + 1) * 128], self.ident[:, :], [y0, self.ident], [pb])
                        pbv = pb[:, :].rearrange("p (c t) -> p c t", c=4)
                        if br == 0:
                            for c in range(4):
                                self.ts(V, raf[:, c, :], pbv[:, c, :], self.pfv(l, "ln_g", c), self.pfv(l, "ln_b", c), ALU.mult, ALU.add, [pb, PFl], [raf])
                            self.dma(bon[:, :, :], fm4(self.BON)[:, :, n0:n0 + 128], [self.dd("BON", n0 // 128)], [bon])
                            self.dma(ga[:, :, :], fm4(self.GAs)[:, :, n0:n0 + 128], [self.dd("GAs", n0 // 128)], [ga])
                            self.tt(V, raf[:, :, :], raf[:, :, :], bon[:, :, :], ALU.add, [raf, bon], [raf])
                            self.tt(V, Rb[0][:, :, :], raf[:, :, :], ga[:, :, :], ALU.mult, [raf, ga], [Rb[0]])
                        else:
                            self.dma(zt[:, :, :], self.PTv[:, 27:31, n0:n0 + 128], [], [zt])
                            self.actf(zt[:, :, :], zt[:, :, :], AF.Silu, [zt], [zt])
                            self.stt(V, Rb[1][:, :, :], pbv, self.pfv(l, "gnorm", 0), zt[:, :, :], ALU.mult, ALU.mult, [pb, PFl, zt], [Rb[1]])
                    self.dma(Rb[2][:, :, :], fm4(self.RCs)[:, :, n0:n0 + 128], [self.dd("RCs", n0 // 128)], [Rb[2]])
                    for n in range(3):
                        self.dma(pg[:, :, :], pgv[n][:, :, n0:n0 + 128], [], [pg])
                        self.actf(pg[:, :, :], pg[:, :, :], AF.Sigmoid, [pg], [pg])
                        for m in range(8):
                            pb = B[2 + m // 4]
                            for k in range(4):
                                self.mm(pb[:, (m % 4) * 128:(m % 4 + 1) * 128], wbr[:, n * 4 + k, m * 128:(m + 1) * 128], Rb[n][:, k, :], k == 0, k == 3,
                                        [wbr, Rb[n]], [pb])
                        for hf in range(2):
                            pbv = B[2 + hf][:, :].rearrange("p (c t) -> p c t", c=4)
                            dst = macc if n == 0 else tmpm
                            self.tt(V, dst[:, hf * 4:(hf + 1) * 4, :], pbv, pg[:, hf * 4:(hf + 1) * 4, :], ALU.mult, [B[2 + hf], pg], [dst])
                        if n > 0:
                            self.tt(G, macc[:, :, :], macc[:, :, :], tmpm[:, :, :], ALU.add, [macc, tmpm], [macc])
                    self.cp(G, mb[:, :, :], macc[:, :, :], [macc], [mb])
                    self.dma(xt[:, :, :], self.XTv[:, :, n0:n0 + 128], self.dr("XT", n0, 128), [xt])
                    for m in range(8):
                        pb = B[m // 4]
                        for k in range(8):
                            self.mm(pb[:, (m % 4) * 128:(m % 4 + 1) * 128], wo[:, k, m * 128:(m + 1) * 128], mb[:, k, :], k == 0, k == 7, [wo, mb], [pb])
                    for m in range(8):
                        pb = B[m // 4]
                        self.stt(V, xt[:, m, :], pb[:, (m % 4) * 128:(m % 4 + 1) * 128], self.MOD[l][:, 16 + m, r:r + 1], xt[:, m, :], ALU.mult, ALU.add,
                                 [pb, self.MOD[l], xt], [xt])
                    self.dma(self.XTv[:, :, n0:n0 + 128], xt[:, :, :], [xt], self.dr("XT", n0, 128))
                    if f"XM{l}" in self.dbg:
                        pass
            self.run_interleaved([lambda: stream(0), lambda: stream(1)])
            self.barrier()

    def moe(self, l):
        V, G = self.dve, self.pool
        ps = self.ps
        with ExitStack() as es:
            def t(shape, dt=F32, e_=None):
                return self.sb(shape, dt, e_ or es)
            hT = t([128, 8, NT], BF16)
            WTf = t([16, NT])
            wr = t([128, 8, 16])
            rb = t([128, 16])
            self.dma(wr[:, :, :], self.w_router.rearrange("(k p) e -> p k e", p=128), [], [wr])
            self.dma(rb[:, :], self.rbias, [], [rb])
            with ExitStack() as es1:
                xts = [t([128, 8, 512], F32, es1) for _ in range(2)]
                sq = t([128, 8, 512], F32, es1)
                rs = t([128, 512], F32, es1)
                hf = t([128, 8, 512], F32, es1)
                sc, sel, sel2, eq, cm, wts = (t([128, 16], F32, es1) for _ in range(6))
                m1, m2, gs, gsel = (t([128, 4], F32, es1) for _ in range(4))
                gmx, wsum = t([128, 1], F32, es1), t([128, 1], F32, es1)
                v4 = lambda a: a[:, :].rearrange("p (g j) -> p g j", j=4)
                b4 = lambda a: a[:, :].unsqueeze(2).to_broadcast([128, 4, 4])
                for i, (n0, w, r) in enumerate(self.tiles()):
                    xt, _ = self.modulate_tile((xts[i % 2], sq, rs, None), n0, w, r, None, None, None, None)
                    for c in range(8):
                        self.stt(V, hf[:, c, :w], xt[:, c, :w], self.GF[l][:, c, r:r + 1], rs[:, :w], ALU.mult, ALU.mult, [xt, rs, self.GF[l]], [hf])
                        self.actf(hf[:, c, :w], hf[:, c, :w], AF.Identity, [hf, self.MOD[l]], [hf], bias=self.MOD[l][:, 24 + c, r:r + 1], scale=1.0)
                    self.cp(G, hT[:, :, n0:n0 + w], hf[:, :, :w], [hf], [hT])
                    for q in range(w // 128):
                        pR = ps[6]
                        for c in range(8):
                            self.mm(pR[:, 0:16], hf[:, c, q * 128:(q + 1) * 128], wr[:, c, :], c == 0, c == 7, [hf, wr], [pR])
                        self.actf(sc[:, :], pR[:, 0:16], AF.Sigmoid, [pR], [sc])
                        self.tt(V, sel[:, :], sc[:, :], rb[:, :], ALU.add, [sc, rb], [sel])
                        self.red(V, m1[:, :], v4(sel), ALU.max, [sel], [m1])
                        self.tt(V, v4(eq), v4(sel), b4(m1), ALU.is_equal, [sel, m1], [eq])
                        self.stt(V, sel2[:, :], eq[:, :], -1e9, sel[:, :], ALU.mult, ALU.add, [eq, sel], [sel2])
                        self.red(V, m2[:, :], v4(sel2), ALU.max, [sel2], [m2])
                        self.tt(V, gs[:, :], m1[:, :], m2[:, :], ALU.add, [m1, m2], [gs])
                        self.red(V, gmx[:, :], gs[:, :], ALU.max, [gs], [gmx])
                        self.ts(V, gsel[:, :], gs[:, :], gmx[:, 0:1], None, ALU.is_equal, None, [gs, gmx], [gsel])
                        self.tt(V, v4(cm), v4(sel), b4(m2), ALU.is_ge, [sel, m2], [cm])
                        self.tt(V, v4(cm), v4(cm), b4(gsel), ALU.mult, [cm, gsel], [cm])
                        self.tt(V, wts[:, :], sc[:, :], cm[:, :], ALU.mult, [sc, cm], [wts])
                        self.red(V, wsum[:, :], wts[:, :], ALU.add, [wts], [wsum])
                        self.recip(wsum[:, :], wsum[:, :], [wsum], [wsum])
                        self.ts(V, wts[:, :], wts[:, :], wsum[:, 0:1], None, ALU.mult, None, [wts, wsum], [wts])
                        pT = ps[7]
                        self.tr(pT[0:16, 0:128], wts[:, :], self.ident[:, :], [wts, self.ident], [pT])
                        self.cp(self.act, WTf[0:16, n0 + q * 128:n0 + (q + 1) * 128], pT[0:16, 0:128], [pT], [WTf])
                self.barrier()
            TG = 1152
            TW = 384
            yacc = t([128, 8, TG])
            wgb = [t([128, 8, 512], BF16) for _ in range(2)]
            wub = [t([128, 8, 512], BF16) for _ in range(2)]
            wdb = [t([128, 4, 1024], BF16) for _ in range(2)]
            wtb = t([128, TW])
            sg = [t([128, TW]) for _ in range(2)]
            actb = t([128, 4, TW], BF16)
            xt2 = t([128, 8, 128])
            def loadw(e):
                for (src, dstt) in ((self.weg[l, e], wgb[e % 2]), (self.weu[l, e], wub[e % 2]), (self.wed[l, e], wdb[e % 2])):
                    self.dma(dstt[:, :, :], src.rearrange("(k p) n -> p k n", p=128), [], [dstt], Q=self.pool)
            loadw(0)
            for g in range(NT // TG):
                for e in range(16):
                    gb, ub, db = wgb[e % 2], wub[e % 2], wdb[e % 2]
                    if not (g == NT // TG - 1 and e == 15):
                        loadw((e + 1) % 16)
                    for tt_ in range(TG // TW):
                        n0 = g * TG + tt_ * TW
                        self.mm(ps[4][:, :TW], self.SEL[0:16, e, :], WTf[0:16, n0:n0 + TW], True, True, [self.SEL, WTf], [ps[4]])
                        self.cp(self.act, wtb[:, :], ps[4][:, :TW], [ps[4]], [wtb])
                        for hc in range(4):
                            pg_, pu_ = ps[(hc % 2) * 2], ps[(hc % 2) * 2 + 1]
                            for k in range(8):
                                self.mm(pg_[:, :TW], gb[:, k, hc * 128:(hc + 1) * 128], hT[:, k, n0:n0 + TW], k == 0, k == 7, [gb, hT], [pg_])
                            for k in range(8):
                                self.mm(pu_[:, :TW], ub[:, k, hc * 128:(hc + 1) * 128], hT[:, k, n0:n0 + TW], k == 0, k == 7, [ub, hT], [pu_])
                            sg_ = sg[hc % 2]
                            self.actf(sg_[:, :], pg_[:, :TW], AF.Silu, [pg_], [sg_])
                            self.tt(V, sg_[:, :], sg_[:, :], pu_[:, :TW], ALU.mult, [sg_, pu_], [sg_])
                            self.tt(G, actb[:, hc, :], sg_[:, :], wtb[:, :], ALU.mult, [sg_, wtb], [actb])
                        for m in range(8):
                            pd = ps[4 + m % 4]
                            for hc in range(4):
                                self.mm(pd[:, :TW], db[:, hc, m * 128:(m + 1) * 128], actb[:, hc, :], hc == 0, hc == 3, [db, actb], [pd])
                            dst = yacc[:, m, tt_ * TW:(tt_ + 1) * TW]
                            if e == 0:
                                self.cp(self.act, dst, pd[:, :TW], [pd], [yacc])
                            else:
                                self.tt(V, dst, dst, pd[:, :TW], ALU.add, [yacc, pd], [yacc])
                for p_ in range(TG // 128):
                    n0 = g * TG + p_ * 128
                    tq = n0 % T
                    r = 2 if tq < TC else n0 // T
                    self.dma(xt2[:, :, :], self.XTv[:, :, n0:n0 + 128], self.dr("XT", n0, 128), [xt2])
                    for m in range(8):
                        self.stt(V, xt2[:, m, :], yacc[:, m, p_ * 128:(p_ + 1) * 128], self.MOD[l][:, 40 + m, r:r + 1], xt2[:, m, :], ALU.mult, ALU.add,
                                 [yacc, self.MOD[l], xt2], [xt2])
                    self.dma(self.XTv[:, :, n0:n0 + 128], xt2[:, :, :], [xt2], self.dr("XT", n0, 128))
            self.barrier()

    def final(self):
        V, G = self.dve, self.pool
        ps = self.ps
        with ExitStack() as es:
            def t(shape, dt=F32):
                return self.sb(shape, dt, es)
            xts = [t([128, 8, 128]) for _ in range(2)]
            sq = t([128, 8, 128])
            rs = t([128, 128])
            tmp = t([128, 8, 128])
            os_ = [t([128, 1024]) for _ in range(2)]
            i = 0
            for s in range(S):
                for j in range(2, NJ):
                    n0 = s * T + j * 128
                    xt = xts[i % 2]; o = os_[i % 2]; i += 1
                    self.dma(xt[:, :, :], self.XTv[:, :, n0:n0 + 128], self.dr("XT", n0, 128), [xt])
                    self.actf(sq[:, :, :], xt[:, :, :], AF.Square, [xt], [sq])
                    for c in range(8):
                        self.mm(ps[0][:, 0:128], self.ones[:, :], sq[:, c, :], c == 0, c == 7, [self.ones, sq], [ps[0]])
                    self.ts(V, rs[:, :], ps[0][:, 0:128], 1.0 / D, EPS, ALU.mult, ALU.add, [ps[0]], [rs])
                    self.actf(rs[:, :], rs[:, :], AF.Sqrt, [rs], [rs])
                    self.recip(rs[:, :], rs[:, :], [rs], [rs])
                    for c in range(8):
                        self.stt(V, tmp[:, c, :], xt[:, c, :], self.pfv(0, "norm_final", c), rs[:, :], ALU.mult, ALU.mult, [xt, rs, self.PFt[0]], [tmp])
                    for c in range(8):
                        pb = ps[1 + c // 4]
                        self.tr(pb[:, (c % 4) * 128:(c % 4 + 1) * 128], tmp[:, c, :], self.ident[:, :], [tmp, self.ident], [pb])
                    self.cp(self.act, o[:, 0:512], ps[1][:, :], [ps[1]], [o])
                    self.cp(V, o[:, 512:1024], ps[2][:, :], [ps[2]], [o])
                    self.dma(self.y[s, (j - 2) * 128:(j - 1) * 128, :], o[:, :], [o], [self.dd("y", s, j)])
            self.barrier()

    def rwkv_scan(self, l, gdn=False):
        V, G = self.dve, self.pool
        ps = self.ps
        nh = 4 if gdn else 8
        dv = 512 // nh
        DKQ, DMAT, DBC, DV_, DPC, DY, nm = ((self.GKQ, self.GMAT, self.GBC, self.GV, self.GPC, self.YB, 'G') if gdn else (self.RKQ, self.RMAT, self.RBC, self.RV, self.RPC, self.YA, 'R'))
        with ExitStack() as es:
            def t(shape, dt=F32):
                return self.sb(shape, dt, es)
            chains = [(s, d) for s in range(S) for d in range(2)]
            order = {0: list(range(NJ)), 1: [1, 0] + list(range(NJ - 1, 1, -1))}
            bufs = []
            for _ in chains:
                ld = [(t([128, 4, 256], BF16), t([128, nh, 512], BF16), t([128, 1024], BF16), t([128, 512], BF16), t([128, 8])) for _ in range(2)]
                bufs.append(dict(ld=ld, H=t([128, 4, dv]), Hz=t([128, nh, dv], BF16), Rn=t([128, nh, dv], BF16),
                                 Ubz=[t([128, nh, dv], BF16) for _ in range(2)], Vz=[t([128, 512], BF16) for _ in range(2)],
                                 Yt=t([128, 512])))
            for i in range(NJ):
                for ci, (s, d) in enumerate(chains):
                    j = order[d][i]
                    n0 = s * T + j * 128
                    b = bufs[ci]
                    KQ, MAT, BC, Vt, pc = b["ld"][i % 2]
                    H, Hz, Rn, Ubz, Vz, Yt = b["H"], b["Hz"], b["Rn"], b["Ubz"], b["Vz"], b["Yt"]
                    self.dma(KQ[:, :, :].rearrange("p m t -> p (m t)"), DKQ[s, d, j], [self.dd(nm + "KQ", s, d, j)], [KQ])
                    self.dma(MAT[:, :, :].rearrange("p h t -> p (h t)"), DMAT[s, d, j], [self.dd(nm + "MAT", s, d, j)], [MAT])
                    self.dma(BC[:, :], DBC[s, d, j], [self.dd(nm + "BC", s, d, j)], [BC])
                    self.dma(Vt[:, :], DV_[s, j], [self.dd(nm + "V", s, j)], [Vt])
                    self.dma(pc[:, :], DPC[s, d, j], [self.dd(nm + "PC", s, d, j)], [pc])
                    if i == 0:
                        self.memset(G, H[:, :, :], 0.0, [H])
                        self.memset(G, Hz[:, :, :], 0.0, [Hz])
                        self.memset(G, Rn[:, :, :], 0.0, [Rn])
                        for c in range(2):
                            self.memset(G, Ubz[c][:, :, :], 0.0, [Ubz[c]])
                            self.memset(G, Vz[c][:, :], 0.0, [Vz[c]])
                    for c in range(2):
                        self.cp(G, Vz[c][c * 64:c * 64 + 64, :], Vt[c * 64:c * 64 + 64, :], [Vt], [Vz[c]])
                    pA, pB = ps[2 * ci], ps[2 * ci + 1]
                    pAv = pA[:, :].rearrange("p (h v) -> p h v", v=dv)
                    pBv = pB[:, :].rearrange("p (h v) -> p h v", v=dv)
                    if not gdn:
                        pBe = pB[:, :].rearrange("p (m e v) -> p m e v", e=2, v=64)
                        Hze = Hz[:, :, :].rearrange("p (m e) v -> p m e v", e=2)
                    pcv = pc[:, :].rearrange("p (m c) -> p m c", c=2)
                    for c in ([0, 1] if d == 0 else [1, 0]):
                        cs = slice(c * 64, c * 64 + 64)
                        Ub = Ubz[c]
                        for h in range(nh):
                            m = h if gdn else h // 2
                            self.mm(pAv[:, h, :], KQ[:, m, 0:128], Hz[:, h, :], True, False, [KQ, Hz], [pA])
                            self.mm(pAv[:, h, :], MAT[:, h, 256:384], Vt[:, h * dv:(h + 1) * dv], False, True, [MAT, Vt], [pA])
                        self.ts(V, Rn[cs, :, :], pAv[cs, :, :], -1.0, None, ALU.mult, None, [pA], [Rn])
                        for h in range(nh):
                            self.mm(pBv[:, h, :], MAT[:, h, 0:128], Rn[:, h, :], True, True, [MAT, Rn], [pB])
                        self.cp(self.act, Ub[cs, :, :], pBv[cs, :, :], [pB], [Ub])
                        for h in range(nh):
                            m = h if gdn else h // 2
                            self.mm(pAv[:, h, :], KQ[:, m, 128:256], Hz[:, h, :], True, False, [KQ, Hz], [pA])
                            self.mm(pAv[:, h, :], MAT[:, h, 128:256], Ub[:, h, :], False, False, [MAT, Ub], [pA])
                            self.mm(pAv[:, h, :], MAT[:, h, 384:512], Vt[:, h * dv:(h + 1) * dv], False, True, [MAT, Vt], [pA])
                        self.cp(V, Yt[cs, :], pA[cs, :], [pA], [Yt])
                        for h in range(nh):
                            m = h if gdn else h // 2
                            self.mm(pBv[:, h, :], BC[:, m * 128:(m + 1) * 128], Ub[:, h, :], True, False, [BC, Ub], [pB])
                            self.mm(pBv[:, h, :], BC[:, 512 + m * 128:512 + (m + 1) * 128], Vz[c][:, h * dv:(h + 1) * dv], False, True, [BC, Vz[c]], [pB])
                        if gdn:
                            self.tt(V, H[:, :, :], H[:, :, :], pcv[:, :, c:c + 1].to_broadcast([128, 4, dv]), ALU.mult, [H, pc], [H])
                            self.tt(V, H[:, :, :], H[:, :, :], pBv[:, :, :], ALU.add, [H, pB], [H])
                            self.cp(self.act, Hz[:, :, :], H[:, :, :], [H], [Hz])
                        else:
                            for e in range(2):
                                rows = slice(e * 64, e * 64 + 64)
                                self.tt(V, H[rows, :, :], H[rows, :, :], pBe[rows, :, e, :], ALU.add, [H, pB], [H])
                            self.tt(V, H[:, :, :], H[:, :, :], pcv[:, :, c:c + 1].to_broadcast([128, 4, 64]), ALU.mult, [H, pc], [H])
                            for e in range(2):
                                rows = slice(e * 64, e * 64 + 64)
                                self.cp(self.act, Hze[rows, :, e, :], H[rows, :, :], [H], [Hz])
                    self.dma(DY[d, n0:n0 + 128, :], Yt[:, :], [Yt], [self.dd(nm + "Y", d, n0 // 128)])
            self.barrier()

    def build(self):
        try:
            self.build_()
        except StopBuild:
            self.es2 = None
            self.finish()

    def build_(self):
        self.consts()
        self.gdn_setup()
        self.conf_setup()
        self.phase0()
        if self.stop == "0":
            return self.finish()
        for l in range(self.nlayers):
            self.phaseA(l)
            if self.stop == f"A{l}":
                return self.finish()
            self.phaseB(l)
            if self.stop == f"B{l}":
                return self.finish()
            self.rwkv_prep(l)
            if self.stop == f"C{l}":
                return self.finish()
            self.rwkv_scan(l)
            if self.stop == f"D{l}":
                return self.finish()
            self.gdn_prep(l)
            if self.stop == f"E{l}":
                return self.finish()
            self.rwkv_scan(l, gdn=True)
            if self.stop == f"F{l}":
                return self.finish()
            self.conformer(l)
            if self.stop == f"G{l}":
                return self.finish()
            self.merge(l)
            if self.stop == f"H{l}":
                return self.finish()
            self.moe(l)
            if self.stop == f"I{l}":
                return self.finish()
        self.final()
        self.finish()
```

```python
import threading
import numpy as np
from contextlib import ExitStack
import concourse.bass as bass
import concourse.mybir as mybir
from concourse.bass_utils import run_bass_kernel_spmd

F32 = mybir.dt.float32
BF16 = mybir.dt.bfloat16
ALU = mybir.AluOpType
AF = mybir.ActivationFunctionType
AX = mybir.AxisListType

D = 1024
S = 2
TC = 256
TL = 2048
T = TC + TL
NT = S * T
L = 2
NIN = 8080
NDS = 24
NDS_SW = 8


class Dep:
    __slots__ = ("w", "r")

    def __init__(self):
        self.w = None
        self.r = {}


class Tl:
    def __init__(self, t):
        self.t = t
        self.d = Dep()

    def __getitem__(self, k):
        return self.t[k]


class Eng:
    def __init__(self, name, be, sem):
        self.key = name
        self.be = be
        self.sem = sem
        self.n = 0
        self.waited = {}


def _d(x):
    return x.d if hasattr(x, "d") else x


class KB:
    def __init__(self, dbg=()):
        self.nc = nc = bass.Bass("TRN2", target_bir_lowering=False)
        self.es = ExitStack()
        self.dbg = set(dbg)
        e = self.es.enter_context
        self.pe = Eng("pe", nc.tensor, e(nc.semaphore("s_pe")))
        self.act = Eng("act", nc.scalar, e(nc.semaphore("s_act")))
        self.dve = Eng("dve", nc.vector, e(nc.semaphore("s_dve")))
        self.pool = Eng("pool", nc.gpsimd, e(nc.semaphore("s_pool")))
        self.sp = Eng("sp", nc.sync, e(nc.semaphore("s_sp")))
        self.engs = [self.pe, self.act, self.dve, self.pool, self.sp]
        self.dsem = [e(nc.semaphore(f"s_d{i}")) for i in range(NDS + NDS_SW)]
        self.dcnt = [0] * (NDS + NDS_SW)
        self.drr = 0
        self.drr_sw = 0
        self.ddeps = {}
        self.ntile = 0
        self.yielders = {}

    def sb(self, shape, dt=F32, es=None):
        self.ntile += 1
        t = (es or self.es).enter_context(self.nc.sbuf_tensor(f"t{self.ntile}", list(shape), dt))
        return Tl(t)

    def psum(self, shape, dt=F32, es=None):
        self.ntile += 1
        t = (es or self.es).enter_context(self.nc.psum_tensor(f"p{self.ntile}", list(shape), dt))
        return Tl(t)

    def dram(self, name, shape, dt=F32, kind=None):
        if kind is None:
            kind = "ExternalOutput" if name in self.dbg else "Internal"
        return self.nc.dram_tensor(name, list(shape), dt, kind=kind).ap()

    def dd(self, *key):
        d = self.ddeps.get(key)
        if d is None:
            d = self.ddeps[key] = Dep()
        return d

    def dr(self, name, n0, w):
        return [self.dd(name, i) for i in range(n0 // 128, (n0 + w + 127) // 128)]

    def _sync(self, E, R, W):
        need = {}

        def upd(tok):
            k, sem, val = tok
            if k not in need or need[k][1] < val:
                need[k] = (sem, val)

        for d in R:
            d = _d(d)
            if d.w:
                upd(d.w)
        for d in W:
            d = _d(d)
            if d.w:
                upd(d.w)
            for k, (sem, val) in d.r.items():
                if k != E.key:
                    upd((k, sem, val))
        for k, (sem, val) in need.items():
            if k == E.key and E is self.pe:
                continue
            if E.waited.get(k, 0) < val:
                E.be.wait_ge(sem, val)
                E.waited[k] = val

    def _mark(self, tok, R, W):
        k, sem, val = tok
        for d in R:
            _d(d).r[k] = (sem, val)
        for d in W:
            d = _d(d)
            d.w = tok
            d.r = {}

    def op(self, E, fn, R, W):
        self._sync(E, R, W)
        ins = fn()
        E.n += 1
        ins.then_inc(E.sem, 1)
        self._mark((E.key, E.sem, E.n), R, W)
        self._yield()

    def dma(self, out, in_, R, W, Q=None, **kw):
        Q = Q or self.sp
        self._sync(Q, R, W)
        sw = Q is self.pool
        if sw:
            s = NDS + self.drr_sw
            self.drr_sw = (self.drr_sw + 1) % NDS_SW
        else:
            s = self.drr
            self.drr = (s + 1) % NDS
        sem = self.dsem[s]
        k = ("d", s)
        if self.dcnt[s] > 0 and Q.waited.get(k, 0) < 16 * self.dcnt[s]:
            Q.be.wait_ge(sem, 16 * self.dcnt[s])
            Q.waited[k] = 16 * self.dcnt[s]
        Q.be.dma_start(out=out, in_=in_, **kw).then_inc(sem, 16)
        self.dcnt[s] += 1
        self._mark((k, sem, 16 * self.dcnt[s]), R, W)
        self._yield()

    def _yield(self):
        if self.yielders:
            y = self.yielders.get(threading.get_ident())
            if y:
                y()

    def run_interleaved(self, fns):
        n = len(fns)
        state = {"turn": 0, "done": [False] * n, "exc": None}
        cv = threading.Condition()

        def advance(i):
            for k in range(1, n + 1):
                nx = (i + k) % n
                if not state["done"][nx]:
                    state["turn"] = nx
                    break
            else:
                state["turn"] = -1
            cv.notify_all()

        def yielder(i):
            def y():
                with cv:
                    advance(i)
                    while state["turn"] != i:
                        cv.wait()
            return y

        def worker(i):
            with cv:
                while state["turn"] != i:
                    cv.wait()
            self.yielders[threading.get_ident()] = yielder(i)
            try:
                fns[i]()
            except BaseException as e:
                state["exc"] = e
            finally:
                self.yielders.pop(threading.get_ident(), None)
                with cv:
                    state["done"][i] = True
                    advance(i)

        ths = [threading.Thread(target=worker, args=(i,)) for i in range(n)]
        for th in ths:
            th.start()
        for th in ths:
            th.join()
        if state["exc"] is not None:
            raise state["exc"]

    def barrier(self):
        for E in self.engs:
            for E2 in self.engs:
                if E2 is not E and E2.n > 0 and E.waited.get(E2.key, 0) < E2.n:
                    E.be.wait_ge(E2.sem, E2.n)
                    E.waited[E2.key] = E2.n
            for s in range(NDS + NDS_SW):
                k = ("d", s)
                if self.dcnt[s] > 0 and E.waited.get(k, 0) < 16 * self.dcnt[s]:
                    E.be.wait_ge(self.dsem[s], 16 * self.dcnt[s])
                    E.waited[k] = 16 * self.dcnt[s]

    def mm(self, out, lhsT, rhs, start, stop, R, W):
        self.op(self.pe, lambda: self.nc.tensor.matmul(out, lhsT=lhsT, rhs=rhs, start=start, stop=stop), R, W)

    def tr(self, out, in_, ident, R, W):
        self.op(self.pe, lambda: self.nc.tensor.transpose(out, in_, ident), R, W)

    def actf(self, out, in_, func, R, W, bias=None, scale=None):
        kw = {}
        if bias is not None:
            kw["bias"] = bias
        if scale is not None:
            kw["scale"] = scale
        self.op(self.act, lambda: self.nc.scalar.activation(out=out, in_=in_, func=func, **kw), R, W)

    def ts(self, E, out, in0, s1, s2, op0, op1, R, W):
        if op1 is None:
            self.op(E, lambda: E.be.tensor_scalar(out=out, in0=in0, scalar1=s1, scalar2=None, op0=op0), R, W)
        else:
            self.op(E, lambda: E.be.tensor_scalar(out=out, in0=in0, scalar1=s1, scalar2=s2, op0=op0, op1=op1), R, W)

    def tt(self, E, out, in0, in1, op, R, W):
        self.op(E, lambda: E.be.tensor_tensor(out=out, in0=in0, in1=in1, op=op), R, W)

    def stt(self, E, out, in0, scalar, in1, op0, op1, R, W):
        self.op(E, lambda: E.be.scalar_tensor_tensor(out=out, in0=in0, scalar=scalar, in1=in1, op0=op0, op1=op1), R, W)

    def cp(self, E, out, in_, R, W):
        if E is self.act:
            self.op(E, lambda: self.nc.scalar.copy(out=out, in_=in_), R, W)
        else:
            self.op(E, lambda: E.be.tensor_copy(out=out, in_=in_), R, W)

    def recip(self, out, in_, R, W):
        self.op(self.dve, lambda: self.nc.vector.reciprocal(out=out, in_=in_), R, W)

    def memset(self, E, ap, v, W):
        self.op(E, lambda: E.be.memset(ap, v), [], W)


class PF:
    def __init__(self):
        self.cols = {}
        self.n = 0

    def add(self, name, nch):
        self.cols[name] = (self.n, nch)
        self.n += nch
        return self.cols[name][0]


def pf_layout():
    pf = PF()
    for nm, nch in [("norm_mix", 8), ("norm_ffn", 8), ("b_ada", 48), ("mu0", 15), ("mu1", 15),
                    ("w0_0", 4), ("w0_1", 4), ("a0_0", 4), ("a0_1", 4), ("kk", 4), ("ka", 4), ("rk", 4),
                    ("ln_g", 4), ("ln_b", 4), ("gconv", 60), ("gnorm", 1), ("alog", 1), ("dtb", 1),
                    ("cdw", 124), ("cdwb", 4), ("clng", 4), ("clnb", 4), ("norm_final", 8)]:
        pf.add(nm, nch)
    return pf


def _fm(v, nch):
    return np.ascontiguousarray(np.asarray(v, np.float32).reshape(nch, 128).T)


def pack_pf(inp, l):
    pf = pf_layout()
    out = np.zeros((128, pf.n), np.float32)

    def put(nm, arr):
        o, n = pf.cols[nm]
        out[:, o:o + n] = arr

    put("norm_mix", _fm(inp["norm_mix"][l], 8))
    put("norm_ffn", _fm(inp["norm_ffn"][l], 8))
    put("b_ada", _fm(inp["b_ada"][l], 48))
    put("mu0", _fm(inp["rwkv_mu"][l, 0], 15))
    put("mu1", _fm(inp["rwkv_mu"][l, 1], 15))
    for d in range(2):
        put(f"w0_{d}", _fm(inp["rwkv_w0"][l, d], 4))
        put(f"a0_{d}", _fm(inp["rwkv_a0"][l, d], 4))
    put("kk", _fm(inp["rwkv_kk"][l], 4))
    put("ka", _fm(inp["rwkv_ka"][l], 4))
    put("rk", _fm(inp["rwkv_rk"][l].reshape(-1), 4))
    put("ln_g", _fm(inp["rwkv_ln_g"][l], 4))
    put("ln_b", _fm(inp["rwkv_ln_b"][l], 4))
    gc = np.concatenate([_fm(inp["gdn_conv"][l, k], 12) for k in range(5)], axis=1)
    put("gconv", gc)
    put("gnorm", _fm(inp["gdn_norm"][l], 1))
    al = np.zeros((128, 1), np.float32); al[0:8, 0] = np.asarray(inp["gdn_A_log"][l]).reshape(-1)
    db = np.zeros((128, 1), np.float32); db[0:8, 0] = np.asarray(inp["gdn_dt_bias"][l]).reshape(-1)
    put("alog", al)
    put("dtb", db)
    cd = np.concatenate([_fm(inp["conf_dw"][l, k], 4) for k in range(31)], axis=1)
    put("cdw", cd)
    put("cdwb", _fm(inp["conf_dw_b"][l], 4))
    put("clng", _fm(inp["conf_ln_g"][l], 4))
    put("clnb", _fm(inp["conf_ln_b"][l], 4))
    put("norm_final", _fm(inp["norm_final"], 8))
    return out


EPS = 1e-6


class Prog(KB):
    def __init__(self, dbg=(), stop=None, nlayers=L):
        super().__init__(dbg)
        self.stop = stop
        self.nlayers = nlayers
        nc = self.nc
        self.pf = pf_layout()

        def inp(name, shape):
            return nc.dram_tensor(name, list(shape), F32, kind="ExternalInput").ap()

        self.x = inp("x", [S, TL, D])
        self.ctx = inp("ctx", [S, TC, D])
        self.cvecT = inp("cvecT", [D, 3])
        self.pfp = inp("pfp", [L, 128, self.pf.n])
        self.w_ada = inp("w_ada", [L, D, 6 * D])
        self.w_in = inp("w_in", [L, D, NIN])
        self.rw2 = inp("rwkv_w2", [L, 2, 64, 512])
        self.ra2 = inp("rwkv_a2", [L, 2, 64, 512])
        self.rg2 = inp("rwkv_g2", [L, 128, 512])
        self.w_branch = inp("w_branch", [L, 3, 512, D])
        self.w_out = inp("w_out", [L, D, D])
        self.w_router = inp("w_router", [D, 16])
        self.rbias = inp("rbias", [128, 16])
        self.weg = inp("w_e_gate", [L, 16, D, 512])
        self.weu = inp("w_e_up", [L, 16, D, 512])
        self.wed = inp("w_e_down", [L, 16, 512, D])
        self.y = nc.dram_tensor("y", [S, TL, D], F32, kind="ExternalOutput").ap()
        self.XT = self.dram("XT", [D, NT])
        self.XTv = self.XT.rearrange("(c p) n -> p c n", p=128)
        self.PT = self.dram("PT", [NIN, NT])
        self.ps = [self.psum([128, 512], F32) for _ in range(8)]
        self.ident = self.sb([128, 128], F32)
        self.identb = self.sb([128, 128], BF16)
        self.ones = self.sb([128, 128], F32)
        self.PFt = [self.sb([128, self.pf.n], F32) for _ in range(L)]
        self.scT = self.sb([128, 8, 3], F32)
        self.MOD = [self.sb([128, 48, 3], F32) for _ in range(L)]
        self.GM = [self.sb([128, 8, 3], F32) for _ in range(L)]
        self.GF = [self.sb([128, 8, 3], F32) for _ in range(L)]

    def pfv(self, l, name, c=None, n=1):
        o, nch = self.pf.cols[name]
        if c is None:
            return self.PFt[l][:, o:o + nch]
        return self.PFt[l][:, o + c:o + c + n]

    def consts(self):
        nc = self.nc
        P = self.pool
        self.memset(P, self.ident[:, :], 0.0, [self.ident])
        self.op(P, lambda: nc.gpsimd.affine_select(out=self.ident[:, :], in_=self.ident[:, :], pattern=[[-1, 128]],
                                                   compare_op=ALU.not_equal, fill=1.0, base=0, channel_multiplier=1),
                [self.ident], [self.ident])
        self.cp(P, self.identb[:, :], self.ident[:, :], [self.ident], [self.identb])
        self.memset(P, self.ones[:, :], 1.0, [self.ones])
        for l in range(L):
            self.dma(self.PFt[l][:, :], self.pfp[l], [], [self.PFt[l]])
        cv = self.sb([128, 8, 3], F32)
        self.dma(cv[:, :, :], self.cvecT.rearrange("(c p) r -> p c r", p=128), [], [cv])
        self.actf(self.scT[:, :, :], cv[:, :, :], AF.Silu, [cv], [self.scT])

    def phase0(self):
        with ExitStack() as es:
            xin = [self.sb([128, 1024], F32, es) for _ in range(2)]
            xo = [self.sb([128, 8, 128], F32, es) for _ in range(2)]
            i = 0
            for s in range(S):
                for j in range(T // 128):
                    n0 = s * T + j * 128
                    src = self.ctx[s, j * 128:(j + 1) * 128, :] if j < 2 else self.x[s, (j - 2) * 128:(j - 1) * 128, :]
                    a = xin[i % 2]
                    o = xo[i % 2]
                    self.dma(a[:, :], src, [], [a])
                    for c in range(8):
                        pb = self.ps[(i % 2) * 2 + c // 4]
                        self.tr(pb[:, (c % 4) * 128:(c % 4 + 1) * 128], a[:, c * 128:(c + 1) * 128], self.ident[:, :],
                                [a, self.ident], [pb])
                    for hf in range(2):
                        pb = self.ps[(i % 2) * 2 + hf]
                        self.cp(self.act if hf == 0 else self.dve, o[:, hf * 4:(hf + 1) * 4, :],
                                pb[:, :].rearrange("p (c t) -> p c t", c=4), [pb], [o])
                    self.dma(self.XTv[:, :, n0:n0 + 128], o[:, :, :], [o], self.dr("XT", n0, 128))
                    i += 1
            self.barrier()

    def phaseA(self, l):
        with ExitStack() as es:
            wa = [self.sb([128, 8, 768], F32, es) for _ in range(2)]
            pm = self.ps[0]
            wav = self.w_ada[l].rearrange("(k p) n -> p k n", p=128)
            for mg in range(8):
                w = wa[mg % 2]
                for q in range(4):
                    self.dma(w[:, q * 2:(q + 1) * 2, :], wav[:, q * 2:(q + 1) * 2, mg * 768:(mg + 1) * 768], [], [w])
                for m in range(6):
                    mm_ = mg * 6 + m
                    for k in range(8):
                        self.mm(pm[:, mm_ * 3:(mm_ + 1) * 3], w[:, k, m * 128:(m + 1) * 128], self.scT[:, k, :], k == 0, k == 7,
                                [w, self.scT], [pm])
            mod = self.MOD[l]
            self.tt(self.dve, mod[:, :, :], pm[:, 0:144].rearrange("p (m r) -> p m r", r=3),
                    self.pfv(l, "b_ada").unsqueeze(2).to_broadcast([128, 48, 3]), ALU.add, [pm, self.PFt[l]], [mod])
            for (G, mi, nm) in ((self.GM[l], 1, "norm_mix"), (self.GF[l], 4, "norm_ffn")):
                self.ts(self.dve, G[:, :, :], mod[:, mi * 8:(mi + 1) * 8, :], 1.0, None, ALU.add, None, [mod], [G])
                self.dump(f"G1{l}{mi}", G, G[:, :, :], [128, 8, 3])
                self.tt(self.dve, G[:, :, :], G[:, :, :], self.pfv(l, nm).unsqueeze(2).to_broadcast([128, 8, 3]), ALU.mult,
                        [G, self.PFt[l]], [G])
            self.dump(f"MOD{l}", mod, mod[:, :, :], [128, 48, 3])
            self.dump(f"GM{l}", self.GM[l], self.GM[l][:, :, :], [128, 8, 3])
            self.barrier()

    def tiles(self):
        out = []
        for s in range(S):
            out.append((s * T, TC, 2))
            for j in range(TL // 512):
                out.append((s * T + TC + j * 512, 512, s))
        return out

    def modulate_tile(self, es_bufs, n0, w, r, G, shift_col, mod, out_fn):
        xt, sq, rs, tmp = es_bufs
        psS = self.ps[7]
        self.dma(xt[:, :, :w], self.XTv[:, :, n0:n0 + w], self.dr("XT", n0, w), [xt])
        self.actf(sq[:, :, :w], xt[:, :, :w], AF.Square, [xt], [sq])
        for c in range(8):
            self.mm(psS[:, :w], self.ones[:, :], sq[:, c, :w], c == 0, c == 7, [self.ones, sq], [psS])
        self.ts(self.dve, rs[:, :w], psS[:, :w], 1.0 / D, EPS, ALU.mult, ALU.add, [psS], [rs])
        self.actf(rs[:, :w], rs[:, :w], AF.Sqrt, [rs], [rs])
        self.recip(rs[:, :w], rs[:, :w], [rs], [rs])
        return xt, rs

    def phaseB(self, l):
        with ExitStack() as es:
            hT = self.sb([128, 8, NT], BF16, es)
            with ExitStack() as es1:
                xts = [self.sb([128, 8, 512], F32, es1) for _ in range(2)]
                sq = self.sb([128, 8, 512], F32, es1)
                rs = self.sb([128, 512], F32, es1)
                tmps = [self.sb([128, 512], F32, es1) for _ in range(2)]
                for i, (n0, w, r) in enumerate(self.tiles()):
                    xt, _ = self.modulate_tile((xts[i % 2], sq, rs, None), n0, w, r, None, None, None, None)
                    for c in range(8):
                        tmp = tmps[c % 2]
                        self.stt(self.dve, tmp[:, :w], xt[:, c, :w], self.GM[l][:, c, r:r + 1], rs[:, :w], ALU.mult, ALU.mult,
                                 [xt, rs, self.GM[l]], [tmp])
                        self.actf(hT[:, c, n0:n0 + w], tmp[:, :w], AF.Identity, [tmp, self.MOD[l]], [hT],
                                  bias=self.MOD[l][:, c, r:r + 1], scale=1.0)
                if "HT" in self.dbg:
                    hd = self.dram("HT", [128, 8, NT], BF16)
                    self.dma(hd, hT[:, :, :], [hT], [self.dd("HTd")])
                self.barrier()
            wbf = [self.sb([128, 8, 1024], BF16, es) for _ in range(2)]
            ost = [self.sb([128, 512], F32, es) for _ in range(4)]
            no = 0
            def loadw(g):
                c0 = g * 1024
                cw = min(1024, NIN - c0)
                self.dma(wbf[g % 2][:, :, :cw], self.w_in[l][:, c0:c0 + cw].rearrange("(k p) n -> p k n", p=128), [], [wbf[g % 2]], Q=self.pool)
            loadw(0)
            for g in range(8):
                c0 = g * 1024
                cw = min(1024, NIN - c0)
                wb = wbf[g % 2]
                if g < 7:
                    loadw(g + 1)
                nm = (cw + 127) // 128
                for tt_ in range(NT // 512):
                    n0 = tt_ * 512
                    for m in range(nm):
                        mw = min(128, cw - m * 128)
                        pb = self.ps[no % 6]
                        for k in range(8):
                            self.mm(pb[:mw, :], wb[:, k, m * 128:m * 128 + mw], hT[:, k, n0:n0 + 512], k == 0, k == 7,
                                    [wb, hT], [pb])
                        o = ost[no % 4]
                        self.cp(self.act if no % 2 == 0 else self.dve, o[:mw, :], pb[:mw, :], [pb], [o])
                        self.dma(self.PT[c0 + m * 128:c0 + m * 128 + mw, n0:n0 + 512], o[:mw, :], [o],
                                 [self.dd("PT", (c0 + m * 128) // 128, j) for j in range(n0 // 128, n0 // 128 + 4)])
                        no += 1
            self.barrier()

    def dump(self, name, tile, ap, shape, dt=F32):
        if name in self.dbg:
            d = self.dram(name, shape, dt)
            self.dma(d, ap, [tile], [self.dd(name)])

    def finish(self):
        self.barrier()

    def build(self):
        self.consts()
        self.phase0()
        if self.stop == "0":
            return self.finish()
        for l in range(self.nlayers):
            self.phaseA(l)
            if self.stop == f"A{l}":
                return self.finish()
            self.phaseB(l)
            if self.stop == f"B{l}":
                return self.finish()
        self.finish()


def make_in_maps(inp):
    ncores = 8
    pfp = np.stack([pack_pf(inp, l) for l in range(L)])
    rbias = np.ascontiguousarray(np.broadcast_to(np.asarray(inp["router_bias"], np.float32)[None, :], (128, 16)))
    maps = []
    for i in range(ncores):
        cv = np.stack([inp["c"][2 * i], inp["c"][2 * i + 1], inp["c_ctx"]], axis=1).astype(np.float32)
        m = {
            "x": np.ascontiguousarray(inp["x"][2 * i:2 * i + 2]),
            "ctx": np.ascontiguousarray(inp["ctx"][2 * i:2 * i + 2]),
            "cvecT": np.ascontiguousarray(cv),
            "pfp": pfp, "rbias": rbias,
        }
        for k in ("w_ada", "w_in", "rwkv_w2", "rwkv_a2", "rwkv_g2", "w_branch", "w_out", "w_router",
                  "w_e_gate", "w_e_up", "w_e_down"):
            m[k] = np.ascontiguousarray(inp[k], dtype=np.float32)
        maps.append(m)
    return maps


def kernel(**inputs):
    inp = {k: np.asarray(v) for k, v in inputs.items()}
    prog = Prog2()
    prog.build()
    maps = make_in_maps(inp)
    res = run_bass_kernel_spmd(prog.nc, maps, core_ids=list(range(8)))
    return np.concatenate([r["y"] for r in res.results], axis=0).astype(np.float32)


CDEC = 0.6065306597126334
NJ = T // 128


def seg_bounds(j):
    return (j == 0 or j == 2), (j == 1 or j == NJ - 1)


class StopBuild(Exception):
    pass


class Prog2(Prog):
    cut = None

    def ck(self, n):
        if self.cut == n:
            raise StopBuild()

    def __init__(self, **kw):
        super().__init__(**kw)
        self.PTv = self.PT[0:8064, :].rearrange("(c p) n -> p c n", p=128)
        self.RKQ = self.dram("RKQ", [S, 2, NJ, 128, 4 * 256], BF16)
        self.RMAT = self.dram("RMAT", [S, 2, NJ, 128, 8 * 512], BF16)
        self.RBC = self.dram("RBC", [S, 2, NJ, 128, 1024], BF16)
        self.RV = self.dram("RV", [S, NJ, 128, 512], BF16)
        self.RPC = self.dram("RPC", [S, 2, NJ, 128, 8], F32)
        self.GAs = self.dram("GAs", [512, NT])
        self.BON = self.dram("BON", [512, NT])
        self.YA = self.dram("YA", [2, NT, 512])
        self.bones = self.sb([128, 128], F32)
        self.MU = self.sb([128, 256], F32)
        self.ML = self.sb([128, 256], F32)
        self.RM = self.sb([128, 128], F32)

    def consts(self):
        super().consts()
        nc = self.nc
        P = self.pool
        self.memset(P, self.bones[:, :], 0.0, [self.bones])
        self.memset(P, self.bones[0:64, 0:64], 1.0, [self.bones])
        self.memset(P, self.bones[64:128, 64:128], 1.0, [self.bones])
        self.memset(P, self.RM[:, :], 1.0, [self.RM])
        self.memset(P, self.RM[:, 0:1], 0.0, [self.RM])
        self.memset(P, self.RM[:, 64:65], 0.0, [self.RM])
        for (Mt, off, cmp_, sg) in ((self.MU, 0, ALU.is_gt, 1), (self.MU, 128, ALU.is_ge, 1), (self.ML, 0, ALU.is_gt, -1), (self.ML, 128, ALU.is_ge, -1)):
            sl = Mt[:, off:off + 128]
            self.memset(P, sl, 1.0, [Mt])
            self.op(P, lambda sl=sl, cmp_=cmp_, sg=sg: nc.gpsimd.affine_select(out=sl, in_=sl, pattern=[[sg, 128]], compare_op=cmp_, fill=0.0,
                                                                              base=0, channel_multiplier=-sg), [Mt], [Mt])
            self.memset(P, Mt[0:64, off + 64:off + 128], 0.0, [Mt])
            self.memset(P, Mt[64:128, off:off + 64], 0.0, [Mt])

    def load_halo(self, dst, c0, nch, s, j, hw):
        n0 = s * T + j * 128
        lb, rb = seg_bounds(j)
        lo = 0 if lb else hw
        hi = 0 if rb else hw
        if lb:
            self.memset(self.pool, dst[:, :, 0:hw], 0.0, [dst])
        if rb:
            self.memset(self.pool, dst[:, :, 128 + hw:128 + 2 * hw], 0.0, [dst])
        deps = [self.dd("PT", c, i) for c in range(c0, c0 + nch) for i in range((n0 - lo) // 128, (n0 + 128 + hi - 1) // 128 + 1)]
        self.dma(dst[:, :, hw - lo:hw + 128 + hi], self.PTv[:, c0:c0 + nch, n0 - lo:n0 + 128 + hi], deps, [dst])

    def inverse(self, X1, NN, MAT, h, Wt, At, Bt, pA, pB_, pC):
        V, G = self.dve, self.pool
        W, A, B = Wt[0], At[0], Bt[0]
        self.tt(G, W[:, :], self.identb[:, :], X1[:, 0:128], ALU.subtract, [self.identb, X1], [W])
        self.mm(pA[:, 0:128], X1[:, 0:128], NN[:, :], True, True, [X1, NN], [pA])
        self.mm(pB_[:, 0:128], NN[:, :], X1[:, 0:128], True, True, [X1, NN], [pB_])
        self.cp(self.act, A[:, :], pA[:, 0:128], [pA], [A])
        self.cp(self.act, B[:, :], pB_[:, 0:128], [pB_], [B])
        for it in range(5):
            W2, A2, B2 = Wt[(it + 1) % 2], At[(it + 1) % 2], Bt[(it + 1) % 2]
            self.mm(pC[:, 0:128], A[:, :], W[:, :], True, True, [A, W], [pC])
            if it < 4:
                self.mm(pA[:, 0:128], B[:, :], A[:, :], True, True, [A, B], [pA])
                self.mm(pB_[:, 0:128], A[:, :], B[:, :], True, True, [A, B], [pB_])
            dstW = W2[:, :] if it < 4 else MAT[:, h, 0:128]
            self.tt(V, dstW, W[:, :], pC[:, 0:128], ALU.add, [W, pC], [W2 if it < 4 else MAT])
            if it < 4:
                self.cp(self.act, A2[:, :], pA[:, 0:128], [pA], [A2])
                self.cp(self.act, B2[:, :], pB_[:, 0:128], [pB_], [B2])
            W, A, B = W2, A2, B2

    def inverse_batch(self, X1a, NNa, MAT, h0, banks, Wt, At, Bt):
        V, G = self.dve, self.pool
        psW, psA, psB = banks
        hs = slice(h0, h0 + 4)

        def reg(p, i):
            return p[:, i * 128:(i + 1) * 128]

        def bv(p):
            return p[:, :].rearrange("p (h t) -> p h t", h=4)
        W, A, B = Wt[0], At[0], Bt[0]
        self.tt(G, W[:, :, :], self.identb[:, :].unsqueeze(1).to_broadcast([128, 4, 128]), X1a[:, hs, 0:128], ALU.subtract, [self.identb, X1a], [W])
        for i in range(4):
            self.mm(reg(psA, i), X1a[:, h0 + i, 0:128], NNa[:, h0 + i, :], True, True, [X1a, NNa], [psA])
        for i in range(4):
            self.mm(reg(psB, i), NNa[:, h0 + i, :], X1a[:, h0 + i, 0:128], True, True, [X1a, NNa], [psB])
        self.cp(self.act, A[:, :, :], bv(psA), [psA], [A])
        self.cp(self.act, B[:, :, :], bv(psB), [psB], [B])
        for it in range(5):
            W2, A2, B2 = Wt[(it + 1) % 2], At[(it + 1) % 2], Bt[(it + 1) % 2]
            for i in range(4):
                self.mm(reg(psW, i), A[:, i, :], W[:, i, :], True, True, [A, W], [psW])
            if it < 4:
                for i in range(4):
                    self.mm(reg(psA, i), B[:, i, :], A[:, i, :], True, True, [A, B], [psA])
                for i in range(4):
                    self.mm(reg(psB, i), A[:, i, :], B[:, i, :], True, True, [A, B], [psB])
                self.tt(V, W2[:, :, :], W[:, :, :], bv(psW), ALU.add, [W, psW], [W2])
                self.cp(self.act, A2[:, :, :], bv(psA), [psA], [A2])
                self.cp(self.act, B2[:, :, :], bv(psB), [psB], [B2])
            else:
                self.tt(V, MAT[:, hs, 0:128], W[:, :, :], bv(psW), ALU.add, [W, psW], [MAT])
            W, A, B = W2, A2, B2

    def rwkv_prep(self, l):
        nc = self.nc
        V, G = self.dve, self.pool
        with ExitStack() as es:
            def t(shape, dt=F32):
                return self.sb(shape, dt, es)
            wtmp = t([128, 512])
            w2b, a2b, g2b = t([128, 512], BF16), t([128, 512], BF16), t([128, 512], BF16)
            for (src, dstb) in ((self.rw2[l].rearrange("d r c -> (d r) c"), w2b), (self.ra2[l].rearrange("d r c -> (d r) c"), a2b), (self.rg2[l], g2b)):
                self.dma(wtmp[:, :], src, [], [wtmp])
                self.cp(V, dstb[:, :], wtmp[:, :], [wtmp], [dstb])
            PFl = self.PFt[l]
            c0t = t([128, 15])
            self.tt(V, c0t[:, :], self.pfv(l, "mu0"), self.pfv(l, "mu1"), ALU.add, [PFl], [c0t])
            self.ts(V, c0t[:, :], c0t[:, :], -1.0, 1.0, ALU.mult, ALU.add, [c0t], [c0t])
            omka = t([128, 4])
            self.ts(V, omka[:, :], self.pfv(l, "ka"), -1.0, 1.0, ALU.mult, ALU.add, [PFl], [omka])

            def bc(ap, n):
                return ap.unsqueeze(2).to_broadcast([128, n, 128])

            def stream(s):
                pa = t([128, 15, 130])
                sh = t([128, 15, 128])
                twb, xab, sgb = t([128, 128], BF16), t([128, 128], BF16), t([128, 128], BF16)
                SW = [t([128, 4, 128]) for _ in range(2)]
                AA = [t([128, 4, 128]) for _ in range(2)]
                ga = t([128, 4, 128])
                kx, kk, tq, bon = t([128, 4, 128]), t([128, 4, 128]), t([128, 4, 128]), t([128, 4, 128])
                CS, EX, tmpa, tmpb = (t([128, 4, 128]) for _ in range(4))
                e1, e2, e3 = (t([128, 4, 128]) for _ in range(3))
                pc = t([128, 8])
                KQ = t([128, 4, 256], BF16)
                BTb, CTb = t([128, 4, 128], BF16), t([128, 4, 128], BF16)
                vb = t([128, 4, 128], BF16)
                BC = t([128, 1024], BF16)
                Vt = t([128, 512], BF16)
                MAT = t([128, 8, 512], BF16)
                X1a = t([128, 8, 256], BF16)
                NNa = t([128, 8, 128], BF16)
                BTz, CTz = t([128, 8, 128], BF16), t([128, 8, 128], BF16)
                self.memset(G, BTz[:, :, :], 0.0, [BTz])
                self.memset(G, CTz[:, :, :], 0.0, [CTz])
                Wt = [t([128, 4, 128], BF16) for _ in range(2)]
                At = [t([128, 4, 128], BF16) for _ in range(2)]
                Bt = [t([128, 4, 128], BF16) for _ in range(2)]
                Wt2 = [t([128, 4, 128], BF16) for _ in range(2)]
                At2 = [t([128, 4, 128], BF16) for _ in range(2)]
                Bt2 = [t([128, 4, 128], BF16) for _ in range(2)]
                B = self.ps[4 * s:4 * s + 4]
                for j in range(NJ):
                    n0 = s * T + j * 128
                    self.ck(1000 + s * NJ + j)
                    self.load_halo(pa, 0, 15, s, j, 1)
                    self.ck(1)
                    self.tt(V, sh[:, :, :], pa[:, :, 1:129], bc(c0t[:, :], 15), ALU.mult, [pa, c0t], [sh])
                    for c in range(15):
                        self.stt(V, sh[:, c, :], pa[:, c, 0:128], self.pfv(l, "mu0", c), sh[:, c, :], ALU.mult, ALU.add, [pa, PFl, sh], [sh])
                        self.stt(V, sh[:, c, :], pa[:, c, 2:130], self.pfv(l, "mu1", c), sh[:, c, :], ALU.mult, ALU.add, [pa, PFl, sh], [sh])
                    self.ck(2)
                    r_, k_, v_ = sh[:, 0:4, :], sh[:, 4:8, :], sh[:, 8:12, :]
                    self.actf(twb[:, :], sh[:, 12, :], AF.Tanh, [sh], [twb])
                    self.cp(self.act, xab[:, :], sh[:, 13, :], [sh], [xab])
                    self.actf(sgb[:, :], sh[:, 14, :], AF.Sigmoid, [sh], [sgb])
                    self.ck(3)
                    for d in range(2):
                        for (wb_, xin, dst, bname, pb) in ((w2b, twb, SW[d], f"w0_{d}", B[0]), (a2b, xab, AA[d], f"a0_{d}", B[1])):
                            for m in range(4):
                                self.mm(pb[:, m * 128:(m + 1) * 128], wb_[d * 64:(d + 1) * 64, m * 128:(m + 1) * 128],
                                        xin[d * 64:(d + 1) * 64, :], True, True, [wb_, xin], [pb])
                            for m in range(4):
                                self.actf(dst[:, m, :], pb[:, m * 128:(m + 1) * 128], AF.Sigmoid, [pb, PFl], [dst],
                                          bias=self.pfv(l, bname, m), scale=1.0)
                    for m in range(4):
                        self.mm(B[2][:, m * 128:(m + 1) * 128], g2b[:, m * 128:(m + 1) * 128], sgb[:, :], True, True, [g2b, sgb], [B[2]])
                    self.cp(self.act, ga[:, :, :], B[2][:, :].rearrange("p (m t) -> p m t", m=4), [B[2]], [ga])
                    self.dma(self.GAs.rearrange("(m p) n -> p m n", p=128)[:, :, n0:n0 + 128], ga[:, :, :], [ga], [self.dd("GAs", n0 // 128)])
                    self.ck(4)
                    self.tt(V, kx[:, :, :], k_, bc(self.pfv(l, "kk"), 4), ALU.mult, [sh, PFl], [kx])
                    self.tt(G, tq[:, :, :], kx[:, :, :], kx[:, :, :], ALU.mult, [kx], [tq])
                    for m in range(4):
                        self.mm(B[3][:, m * 128:(m + 1) * 128], self.bones[:, :], tq[:, m, :], True, True, [self.bones, tq], [B[3]])
                    self.ts(V, tq[:, :, :], B[3][:, :].rearrange("p (m t) -> p m t", m=4), EPS, None, ALU.add, None, [B[3]], [tq])
                    self.actf(tq[:, :, :], tq[:, :, :], AF.Sqrt, [tq], [tq])
                    self.recip(tq[:, :, :], tq[:, :, :], [tq], [tq])
                    self.tt(V, kk[:, :, :], kx[:, :, :], tq[:, :, :], ALU.mult, [kx, tq], [kk])
                    self.ck(5)
                    self.tt(G, bon[:, :, :], r_, k_, ALU.mult, [sh], [bon])
                    self.tt(G, bon[:, :, :], bon[:, :, :], bc(self.pfv(l, "rk"), 4), ALU.mult, [bon, PFl], [bon])
                    for m in range(4):
                        self.mm(B[0][:, m * 128:(m + 1) * 128], self.bones[:, :], bon[:, m, :], True, True, [self.bones, bon], [B[0]])
                    self.tt(V, bon[:, :, :], B[0][:, :].rearrange("p (m t) -> p m t", m=4), v_, ALU.mult, [B[0], sh], [bon])
                    self.dma(self.BON.rearrange("(m p) n -> p m n", p=128)[:, :, n0:n0 + 128], bon[:, :, :], [bon], [self.dd("BON", n0 // 128)])
                    self.ck(6)
                    self.cp(G, vb[:, :, :], v_, [sh], [vb])
                    pbv = B[1][:, :].bitcast(BF16)
                    for m in range(4):
                        self.tr(pbv[:, m * 128:(m + 1) * 128], vb[:, m, :], self.identb[:, :], [vb, self.identb], [B[1]])
                    self.cp(self.act, Vt[:, :], pbv[:, 0:512], [B[1]], [Vt])
                    self.dma(self.RV[s, j], Vt[:, :], [Vt], [self.dd("RV", s, j)])
                    for d in range(2):
                        self.ck(7)
                        self.tt(V, tmpa[:, :, :], AA[d][:, :, :], bc(self.pfv(l, "ka"), 4), ALU.mult, [AA[d], PFl], [tmpa])
                        self.tt(V, tmpa[:, :, :], tmpa[:, :, :], bc(omka[:, :], 4), ALU.add, [tmpa, omka], [tmpa])
                        self.tt(V, tmpa[:, :, :], tmpa[:, :, :], k_, ALU.mult, [tmpa, sh], [tmpa])
                        self.tt(G, tmpb[:, :, :], kk[:, :, :], AA[d][:, :, :], ALU.mult, [kk, AA[d]], [tmpb])
                        self.ck(8)
                        for m in range(4):
                            self.op(V, lambda m=m: nc.vector.tensor_tensor_scan(out=CS[:, m, :], data0=self.RM[:, :], data1=SW[d][:, m, :],
                                                                               initial=0.0, op0=ALU.mult, op1=ALU.add),
                                    [self.RM, SW[d]], [CS])
                        CSv = CS[:, :, :].rearrange("p m (c t) -> p m c t", t=64)
                        if d == 0:
                            self.tt(V, EX[:, :, :], CS[:, :, :], SW[d][:, :, :], ALU.subtract, [CS, SW[d]], [EX])
                            incl = CS
                        else:
                            tot = CSv[:, :, :, 63:64].to_broadcast([128, 4, 2, 64])
                            self.tt(V, EX[:, :, :].rearrange("p m (c t) -> p m c t", t=64), tot, CSv, ALU.subtract, [CS], [EX])
                            self.tt(V, e3[:, :, :], EX[:, :, :], SW[d][:, :, :], ALU.add, [EX, SW[d]], [e3])
                            incl = e3
                        self.ck(9)
                        self.actf(e1[:, :, :], EX[:, :, :], AF.Exp, [EX], [e1], scale=-CDEC)
                        self.actf(e2[:, :, :], incl[:, :, :], AF.Exp, [incl], [e2], scale=CDEC)
                        self.actf(e3[:, :, :], incl[:, :, :], AF.Exp, [incl], [e3], scale=-CDEC)
                        self.actf(pc[:, :].rearrange("p (m c) -> p m c", c=2), CSv[:, :, :, 63], AF.Exp, [CS], [pc], scale=-CDEC)
                        self.dma(self.RPC[s, d, j], pc[:, :], [pc], [self.dd("RPC", s, d, j)])
                        self.tt(V, KQ[:, :, 0:128], kk[:, :, :], e1[:, :, :], ALU.mult, [kk, e1], [KQ])
                        self.tt(G, KQ[:, :, 128:256], r_, e3[:, :, :], ALU.mult, [sh, e3], [KQ])
                        self.tt(V, BTb[:, :, :], tmpb[:, :, :], e2[:, :, :], ALU.mult, [tmpb, e2], [BTb])
                        self.tt(G, CTb[:, :, :], tmpa[:, :, :], e2[:, :, :], ALU.mult, [tmpa, e2], [CTb])
                        self.dma(self.RKQ[s, d, j], KQ[:, :, :].rearrange("p m t -> p (m t)"), [KQ], [self.dd("RKQ", s, d, j)])
                        self.ck(10)
                        pbb = B[2][:, :].bitcast(BF16)
                        for m in range(4):
                            self.tr(pbb[:, m * 128:(m + 1) * 128], BTb[:, m, :], self.identb[:, :], [BTb, self.identb], [B[2]])
                            self.tr(pbb[:, 512 + m * 128:512 + (m + 1) * 128], CTb[:, m, :], self.identb[:, :], [CTb, self.identb], [B[2]])
                        self.cp(self.act, BC[:, :], pbb[:, :], [B[2]], [BC])
                        self.dma(self.RBC[s, d, j], BC[:, :], [BC], [self.dd("RBC", s, d, j)])
                        self.ck(11)
                        Ms, Mn = (self.MU, self.ML) if d == 0 else (self.ML, self.MU)
                        for e_ in range(2):
                            rows = slice(e_ * 64, e_ * 64 + 64)
                            self.cp(G, BTz[:, :, :].rearrange("p (m e) t -> p m e t", e=2)[rows, :, e_, :], BTb[rows, :, :], [BTb], [BTz])
                            self.cp(self.act, CTz[:, :, :].rearrange("p (m e) t -> p m e t", e=2)[rows, :, e_, :], CTb[rows, :, :], [CTb], [CTz])
                        Msb = Ms[:, :].unsqueeze(1).to_broadcast([128, 2, 256])
                        v2 = lambda p: p[:, :].rearrange("p (h t) -> p h t", h=2)
                        for h in range(8):
                            self.mm(B[h // 2][:, (h % 2) * 256:(h % 2 + 1) * 256], BTz[:, h, :], KQ[:, h // 2, :], True, True, [BTz, KQ], [B[h // 2]])
                        for q in range(4):
                            self.tt(V, X1a[:, 2 * q:2 * q + 2, :], v2(B[q]), Msb, ALU.mult, [B[q], Ms], [X1a])
                        for h in range(8):
                            self.mm(B[h // 2][:, (h % 2) * 256:(h % 2 + 1) * 256], CTz[:, h, :], KQ[:, h // 2, :], True, True, [CTz, KQ], [B[h // 2]])
                        for q in range(4):
                            self.tt(V, MAT[:, 2 * q:2 * q + 2, 256:512], v2(B[q]), Msb, ALU.mult, [B[q], Ms], [MAT])
                        for h in range(8):
                            self.mm(B[h // 4][:, (h % 4) * 128:(h % 4 + 1) * 128], KQ[:, h // 2, 0:128], BTz[:, h, :], True, True, [BTz, KQ], [B[h // 4]])
                        Mnb = Mn[:, 0:128].unsqueeze(1).to_broadcast([128, 4, 128])
                        for q in range(2):
                            self.tt(V, NNa[:, 4 * q:4 * q + 4, :], B[q][:, :].rearrange("p (h t) -> p h t", h=4), Mnb, ALU.mult, [B[q], Mn], [NNa])
                        self.cp(G, MAT[:, :, 128:256], X1a[:, :, 128:256], [X1a], [MAT])
                        self.inverse_batch(X1a, NNa, MAT, 0, (B[0], B[1], B[2]), Wt, At, Bt)
                        self.inverse_batch(X1a, NNa, MAT, 4, (B[3], B[1], B[2]), Wt2, At2, Bt2)
                        self.ck(12)
                        self.dma(self.RMAT[s, d, j], MAT[:, :, :].rearrange("p h t -> p (h t)"), [MAT], [self.dd("RMAT", s, d, j)])
            self.run_interleaved([lambda: stream(0), lambda: stream(1)])
            self.barrier()

    def gdn_setup(self):
        self.GKQ = self.dram("GKQ", [S, 2, NJ, 128, 4 * 256], BF16)
        self.GMAT = self.dram("GMAT", [S, 2, NJ, 128, 4 * 512], BF16)
        self.GBC = self.dram("GBC", [S, 2, NJ, 128, 1024], BF16)
        self.GV = self.dram("GV", [S, NJ, 128, 512], BF16)
        self.GPC = self.dram("GPC", [S, 2, NJ, 128, 8], F32)
        self.YB = self.dram("YB", [2, NT, 512])
        self.SEL = self.sb([16, 16, 128], F32)
        self.selc = self.sb([128, 1], F32)
        self.onec = self.sb([128, 1], F32)
        nc = self.nc
        P = self.pool
        for i in range(16):
            self.cp(P, self.SEL[0:16, i, :], self.ident[0:16, i:i + 1].to_broadcast([16, 128]), [self.ident], [self.SEL])
        self.memset(P, self.onec[:, :], 1.0, [self.onec])
        self.memset(P, self.selc[:, :], 1.0, [self.selc])
        self.op(P, lambda: nc.gpsimd.affine_select(out=self.selc[:, :], in_=self.selc[:, :], pattern=[[0, 1]], compare_op=ALU.is_ge, fill=0.0,
                                                   base=-4, channel_multiplier=1), [self.selc], [self.selc])

    def gdn_prep(self, l):
        nc = self.nc
        V, G = self.dve, self.pool
        ps = self.ps
        with ExitStack() as es:
            def t(shape, dt=F32):
                return self.sb(shape, dt, es)
            PFl = self.PFt[l]

            def bc(ap, n):
                return ap.unsqueeze(2).to_broadcast([128, n, 128])
            negA = t([128, 1])
            self.actf(negA[:, :], self.pfv(l, "alog"), AF.Exp, [PFl], [negA])
            self.ts(V, negA[:, :], negA[:, :], -1.0, None, ALU.mult, None, [negA], [negA])
            def stream(s):
                B = self.ps[4 * s:4 * s + 4]
                qkv = t([128, 12, 132])
                cv, t2 = t([128, 12, 128]), t([128, 12, 128])
                sq = t([128, 8, 128])
                kq = t([128, 4, 256])
                kqb = t([128, 4, 256], BF16)
                kb = t([128, 4, 128], BF16)
                vb = t([128, 4, 128], BF16)
                ab, x1, Xg, SIG, gcf, gcr = (t([16, 128]) for _ in range(6))
                X2, E2 = t([16, 256]), t([16, 256])
                TOTb, Etot = t([16, 128]), t([16, 2])
                TS = t([128, 80])
                negg, sB, sC, dd_ = t([128, 8]), t([128, 8]), t([128, 8]), t([128, 8])
                gm4 = t([128, 4, 256])
                KQd = t([128, 4, 256], BF16)
                BCd = [t([128, 1024], BF16) for _ in range(2)]
                Vt = t([128, 512], BF16)
                MAT = t([128, 4, 512], BF16)
                pc = t([128, 8])
                X1a = t([128, 4, 256], BF16)
                NNa = t([128, 4, 128], BF16)
                Wt = [t([128, 4, 128], BF16) for _ in range(2)]
                At = [t([128, 4, 128], BF16) for _ in range(2)]
                Bt = [t([128, 4, 128], BF16) for _ in range(2)]
                abv = self.PT[3968:3984, :]
                for j in range(NJ):
                    n0 = s * T + j * 128
                    self.load_halo(qkv, 15, 12, s, j, 2)
                    gw = self.pfv(l, "gconv")
                    self.tt(V, cv[:, :, :], qkv[:, :, 0:128], bc(gw[:, 0:12], 12), ALU.mult, [qkv, PFl], [cv])
                    for k in range(1, 5):
                        self.tt(G, t2[:, :, :], qkv[:, :, k:k + 128], bc(gw[:, k * 12:(k + 1) * 12], 12), ALU.mult, [qkv, PFl], [t2])
                        self.tt(V, cv[:, :, :], cv[:, :, :], t2[:, :, :], ALU.add, [cv, t2], [cv])
                    self.actf(cv[:, :, :], cv[:, :, :], AF.Silu, [cv], [cv])
                    self.tt(G, sq[:, :, :], cv[:, 0:8, :], cv[:, 0:8, :], ALU.mult, [cv], [sq])
                    for c in range(8):
                        pb = B[c // 4]
                        self.mm(pb[:, (c % 4) * 128:(c % 4 + 1) * 128], self.ones[:, :], sq[:, c, :], True, True, [self.ones, sq], [pb])
                    for hf in range(2):
                        self.ts(V, sq[:, hf * 4:(hf + 1) * 4, :], B[hf][:, :].rearrange("p (c t) -> p c t", c=4), EPS, None, ALU.add, None, [B[hf]], [sq])
                    self.actf(sq[:, :, :], sq[:, :, :], AF.Sqrt, [sq], [sq])
                    self.recip(sq[:, :, :], sq[:, :, :], [sq], [sq])
                    self.tt(V, kq[:, :, 0:128], cv[:, 4:8, :], sq[:, 4:8, :], ALU.mult, [cv, sq], [kq])
                    self.stt(V, kq[:, :, 128:256], cv[:, 0:4, :], 128.0 ** -0.5, sq[:, 0:4, :], ALU.mult, ALU.mult, [cv, sq], [kq])
                    self.cp(G, kqb[:, :, :], kq[:, :, :], [kq], [kqb])
                    self.cp(G, kb[:, :, :], kq[:, :, 0:128], [kq], [kb])
                    self.cp(G, vb[:, :, :], cv[:, 8:12, :], [cv], [vb])
                    pbv = B[2][:, :].bitcast(BF16)
                    for m in range(4):
                        self.tr(pbv[:, m * 128:(m + 1) * 128], vb[:, m, :], self.identb[:, :], [vb, self.identb], [B[2]])
                    self.cp(self.act, Vt[:, :], pbv[:, 0:512], [B[2]], [Vt])
                    self.dma(self.GV[s, j], Vt[:, :], [Vt], [self.dd("GV", s, j)])
                    pbk = B[3][:, :].bitcast(BF16)
                    for m in range(4):
                        self.tr(pbk[:, m * 128:(m + 1) * 128], kb[:, m, :], self.identb[:, :], [kb, self.identb], [B[3]])
                    self.dma(ab[0:16, :], abv[:, n0:n0 + 128], [], [ab])
                    self.actf(x1[0:16, :], ab[0:16, :], AF.Exp, [ab, PFl], [x1], bias=self.pfv(l, "dtb")[0:16, :], scale=1.0)
                    self.actf(x1[0:16, :], x1[0:16, :], AF.Ln, [x1, self.onec], [x1], bias=self.onec[0:16, :], scale=1.0)
                    self.ts(V, Xg[0:16, :], x1[0:16, :], negA[0:16, :], None, ALU.mult, None, [x1, negA], [Xg])
                    self.actf(SIG[0:16, :], ab[0:16, :], AF.Sigmoid, [ab], [SIG])
                    self.op(V, lambda: nc.vector.tensor_tensor_scan(out=gcf[0:16, :], data0=self.RM[0:16, :], data1=Xg[0:16, :], initial=0.0,
                                                                    op0=ALU.mult, op1=ALU.add), [self.RM, Xg], [gcf])
                    gcfv = gcf[0:16, :].rearrange("p (c t) -> p c t", t=64)
                    totb = gcfv[:, :, 63:64].to_broadcast([16, 2, 64])
                    self.cp(V, TOTb[0:16, :].rearrange("p (c t) -> p c t", t=64), totb, [gcf], [TOTb])
                    self.tt(V, gcr[0:16, :], TOTb[0:16, :], gcf[0:16, :], ALU.subtract, [TOTb, gcf], [gcr])
                    self.tt(V, X2[0:16, 128:256], gcr[0:16, :], Xg[0:16, :], ALU.add, [gcr, Xg], [X2])
                    self.tt(V, X2[0:16, 128:256], X2[0:16, 128:256], gcf[0:16, :], ALU.subtract, [X2, gcf], [X2])
                    self.stt(V, X2[0:16, 128:256], X2[0:16, 128:256], self.selc[0:16, :], gcf[0:16, :], ALU.mult, ALU.add, [X2, self.selc, gcf], [X2])
                    self.tt(V, X2[0:16, 0:128], X2[0:16, 128:256], Xg[0:16, :], ALU.subtract, [X2, Xg], [X2])
                    self.actf(E2[0:16, :], X2[0:16, :], AF.Exp, [X2], [E2])
                    self.actf(Etot[0:16, :], gcfv[:, :, 63], AF.Exp, [gcf], [Etot])
                    pT = B[2]
                    for q_, src in enumerate((Xg[0:16, :], SIG[0:16, :], X2[0:16, 0:128], X2[0:16, 128:256], TOTb[0:16, :])):
                        self.tr(pT[:, q_ * 16:(q_ + 1) * 16], src, self.ident[0:16, 0:16], [Xg, SIG, X2, TOTb, self.ident], [pT])
                    self.cp(V, TS[:, :], pT[:, 0:80], [pT], [TS])
                    self.actf(negg[:, :], TS[:, 0:8], AF.Exp, [TS], [negg], scale=-1.0)
                    self.tt(V, dd_[:, :], TS[:, 64:72], TS[:, 32:40], ALU.subtract, [TS], [dd_])
                    self.actf(sB[:, :], dd_[:, :], AF.Exp, [dd_], [sB])
                    self.tt(V, sB[:, :], sB[:, :], TS[:, 24:32], ALU.mult, [sB, TS], [sB])
                    self.tt(V, dd_[:, :], TS[:, 64:72], TS[:, 48:56], ALU.subtract, [TS], [dd_])
                    self.actf(sC[:, :], dd_[:, :], AF.Exp, [dd_], [sC])
                    self.tt(V, sC[:, :], sC[:, :], TS[:, 24:32], ALU.mult, [sC, TS], [sC])
                    pbk4 = pbk[:, 0:512].rearrange("p (h t) -> p h t", h=4)
                    for d in range(2):
                        self.tt(V, BCd[d][:, 0:512].rearrange("p (h t) -> p h t", h=4), pbk4, sB[:, d * 4:d * 4 + 4].unsqueeze(2).to_broadcast([128, 4, 128]), ALU.mult, [B[3], sB], [BCd[d]])
                        self.tt(V, BCd[d][:, 512:1024].rearrange("p (h t) -> p h t", h=4), pbk4, sC[:, d * 4:d * 4 + 4].unsqueeze(2).to_broadcast([128, 4, 128]), ALU.mult, [B[3], sC], [BCd[d]])
                    for d in range(2):
                        Ms = self.MU if d == 0 else self.ML
                        BC = BCd[d]
                        for h in range(4):
                            i = d * 4 + h
                            self.mm(B[h // 2][:, (h % 2) * 256:(h % 2 + 1) * 256], self.SEL[0:16, i, :], X2[0:16, :], True, True, [self.SEL, X2], [B[h // 2]])
                            self.mm(B[2 + h // 2][:, (h % 2) * 256:(h % 2 + 1) * 256], self.SEL[0:16, i, :], E2[0:16, :], True, True, [self.SEL, E2], [B[2 + h // 2]])
                        v2 = lambda p: p[:, :].rearrange("p (h t) -> p h t", h=2)
                        for q in range(2):
                            self.tt(V, gm4[:, 2 * q:2 * q + 2, :], v2(B[q]), TS[:, 32 + d * 4 + 2 * q:32 + d * 4 + 2 * q + 2].unsqueeze(2).to_broadcast([128, 2, 256]),
                                    ALU.subtract, [B[q], TS], [gm4])
                            self.tt(V, KQd[:, 2 * q:2 * q + 2, :], kq[:, 2 * q:2 * q + 2, :], v2(B[2 + q]), ALU.mult, [kq, B[2 + q]], [KQd])
                        for h in range(4):
                            self.mm(B[2][:, h * 2:(h + 1) * 2], self.SEL[0:16, d * 4 + h, :], Etot[0:16, :], True, True, [self.SEL, Etot], [B[2]])
                        self.cp(self.act, pc[:, :], B[2][:, 0:8], [B[2]], [pc])
                        self.ts(G, gm4[:, :, :], gm4[:, :, :], 0.0, None, ALU.min, None, [gm4], [gm4])
                        self.actf(gm4[:, :, :], gm4[:, :, :], AF.Exp, [gm4], [gm4])
                        self.tt(G, gm4[:, :, :], gm4[:, :, :], Ms[:, :].unsqueeze(1).to_broadcast([128, 4, 256]), ALU.mult, [gm4, Ms], [gm4])
                        for h in range(4):
                            self.mm(B[h // 2][:, (h % 2) * 256:(h % 2 + 1) * 256], kb[:, h, :], kqb[:, h, :], True, True, [kb, kqb], [B[h // 2]])
                        for q in range(2):
                            self.tt(V, gm4[:, 2 * q:2 * q + 2, :], gm4[:, 2 * q:2 * q + 2, :], v2(B[q]), ALU.mult, [gm4, B[q]], [gm4])
                        self.tt(V, X1a[:, :, :], gm4[:, :, :], TS[:, 24 + d * 4:28 + d * 4].unsqueeze(2).to_broadcast([128, 4, 256]), ALU.mult, [gm4, TS], [X1a])
                        self.tt(V, MAT[:, :, 256:512], X1a[:, :, :], negg[:, d * 4:d * 4 + 4].unsqueeze(2).to_broadcast([128, 4, 256]), ALU.mult, [X1a, negg], [MAT])
                        self.cp(G, MAT[:, :, 128:256], X1a[:, :, 128:256], [X1a], [MAT])
                        pN = B[3][:, :].bitcast(BF16)
                        for h in range(4):
                            self.tr(pN[:, h * 128:(h + 1) * 128], X1a[:, h, 0:128], self.identb[:, :], [X1a, self.identb], [B[3]])
                        self.cp(self.act, NNa[:, :, :], pN[:, 0:512].rearrange("p (h t) -> p h t", h=4), [B[3]], [NNa])
                        self.inverse_batch(X1a, NNa, MAT, 0, (B[0], B[1], B[2]), Wt, At, Bt)
                        self.dma(self.GKQ[s, d, j], KQd[:, :, :].rearrange("p m t -> p (m t)"), [KQd], [self.dd("GKQ", s, d, j)])
                        self.dma(self.GMAT[s, d, j], MAT[:, :, :].rearrange("p h t -> p (h t)"), [MAT], [self.dd("GMAT", s, d, j)])
                        self.dma(self.GBC[s, d, j], BC[:, :], [BC], [self.dd("GBC", s, d, j)])
                        self.dma(self.GPC[s, d, j], pc[:, :], [pc], [self.dd("GPC", s, d, j)])
            self.run_interleaved([lambda: stream(0), lambda: stream(1)])
            self.barrier()

    def conf_setup(self):
        self.RCs = self.dram("RCs", [512, NT], BF16)
        self.RAs = self.dram("RAs", [512, NT], BF16)
        self.RBs = self.dram("RBs", [512, NT], BF16)

    def conformer(self, l):
        V, G = self.dve, self.pool
        ps = self.ps
        PFl = self.PFt[l]
        valv = self.PT[3984:3984 + 512, :].rearrange("(c p) n -> p c n", p=128)
        gatv = self.PT[4496:4496 + 512, :].rearrange("(c p) n -> p c n", p=128)
        RCv = self.RCs.rearrange("(c p) n -> p c n", p=128)
        with ExitStack() as es:
            def t(shape, dt=F32):
                return self.sb(shape, dt, es)
            u = t([128, 4, TL])
            gt = t([128, 4, TL])
            o = t([128, 4, TL])
            sq = t([128, 4, 512])
            mu, rs, var = t([128, 512]), t([128, 512]), t([128, 512])
            ob = t([128, 4, 512], BF16)
            cw = self.pfv(l, "cdw")
            for s in range(S):
                for (seg0, W_) in ((0, TC), (TC, TL)):
                    n0 = s * T + seg0
                    for c in range(4):
                        self.dma(u[:, c, :W_], valv[:, c, n0:n0 + W_], [], [u])
                        self.dma(gt[:, c, :W_], gatv[:, c, n0:n0 + W_], [], [gt])
                    self.actf(gt[:, :, :W_], gt[:, :, :W_], AF.Sigmoid, [gt], [gt])
                    self.tt(V, u[:, :, :W_], u[:, :, :W_], gt[:, :, :W_], ALU.mult, [u, gt], [u])
                    for c in range(4):
                        def wk(k):
                            return cw[:, k * 4 + c:k * 4 + c + 1]
                        self.ts(V, o[:, c, :W_], u[:, c, :W_], wk(15), None, ALU.mult, None, [u, PFl], [o])
                        for k in range(31):
                            dlt = k - 15
                            if dlt == 0:
                                continue
                            if seg0 == 0:
                                lo, hi = max(0, -dlt), min(W_, W_ - dlt)
                                self.stt(V, o[:, c, lo:hi], u[:, c, lo + dlt:hi + dlt], wk(k), o[:, c, lo:hi], ALU.mult, ALU.add, [u, PFl, o], [o])
                            elif c < 2:
                                uv = u[:, c, :].rearrange("p (r w) -> p r w", w=64)
                                ov = o[:, c, :].rearrange("p (r w) -> p r w", w=64)
                                lo, hi = max(0, -dlt), min(64, 64 - dlt)
                                self.stt(V, ov[:, :, lo:hi], uv[:, :, lo + dlt:hi + dlt], wk(k), ov[:, :, lo:hi], ALU.mult, ALU.add, [u, PFl, o], [o])
                            else:
                                uv = u[:, c, :].rearrange("p (r w) -> p r w", w=64)
                                ov = o[:, c, :].rearrange("p (r w) -> p r w", w=64)
                                lo, hi = max(0, -dlt), min(32, 32 - dlt)
                                self.stt(V, ov[:, lo:hi, :], uv[:, lo + dlt:hi + dlt, :], wk(k), ov[:, lo:hi, :], ALU.mult, ALU.add, [u, PFl, o], [o])
                        self.ts(V, o[:, c, :W_], o[:, c, :W_], self.pfv(l, "cdwb", c), None, ALU.add, None, [o, PFl], [o])
                    for t0 in range(0, W_, 512):
                        w = min(512, W_ - t0)
                        self.tt(G, sq[:, :, :w], o[:, :, t0:t0 + w], o[:, :, t0:t0 + w], ALU.mult, [o], [sq])
                        for c in range(4):
                            self.mm(ps[0][:, :w], self.ones[:, :], o[:, c, t0:t0 + w], c == 0, c == 3, [self.ones, o], [ps[0]])
                        for c in range(4):
                            self.mm(ps[1][:, :w], self.ones[:, :], sq[:, c, :w], c == 0, c == 3, [self.ones, sq], [ps[1]])
                        self.ts(V, mu[:, :w], ps[0][:, :w], 1.0 / 512, None, ALU.mult, None, [ps[0]], [mu])
                        self.tt(V, var[:, :w], mu[:, :w], mu[:, :w], ALU.mult, [mu], [var])
                        self.stt(V, var[:, :w], ps[1][:, :w], 1.0 / 512, var[:, :w], ALU.mult, ALU.subtract, [ps[1], var], [var])
                        self.ts(V, var[:, :w], var[:, :w], EPS, None, ALU.add, None, [var], [var])
                        self.actf(var[:, :w], var[:, :w], AF.Sqrt, [var], [var])
                        self.recip(rs[:, :w], var[:, :w], [var], [rs])
                        for c in range(4):
                            self.tt(V, sq[:, c, :w], o[:, c, t0:t0 + w], mu[:, :w], ALU.subtract, [o, mu], [sq])
                            self.tt(V, sq[:, c, :w], sq[:, c, :w], rs[:, :w], ALU.mult, [sq, rs], [sq])
                            self.ts(V, sq[:, c, :w], sq[:, c, :w], self.pfv(l, "clng", c), self.pfv(l, "clnb", c), ALU.mult, ALU.add, [sq, PFl], [sq])
                        self.actf(ob[:, :, :w], sq[:, :, :w], AF.Silu, [sq], [ob])
                        self.dma(RCv[:, :, n0 + t0:n0 + t0 + w], ob[:, :, :w], [ob], [self.dd("RCs", (n0 + t0) // 128)])
            self.barrier()

    def red(self, E, out, in_, op, R, W):
        self.op(E, lambda: E.be.tensor_reduce(out=out, in_=in_, axis=AX.X, op=op), R, W)

    def merge(self, l):
        V, G = self.dve, self.pool
        ps = self.ps
        PFl = self.PFt[l]
        with ExitStack() as es:
            def t(shape, dt=F32):
                return self.sb(shape, dt, es)
            wbr = t([128, 12, 1024], BF16)
            wo = t([128, 8, 1024], BF16)
            for n in range(3):
                self.dma(wbr[:, n * 4:(n + 1) * 4, :], self.w_branch[l, n].rearrange("(k p) n -> p k n", p=128), [], [wbr], Q=self.pool)
            self.dma(wo[:, :, :], self.w_out[l].rearrange("(k p) n -> p k n", p=128), [], [wo], Q=self.pool)
            pgv = [self.PT[5008 + n * 1024:5008 + (n + 1) * 1024, :].rearrange("(c p) n -> p c n", p=128) for n in range(3)]
            fm4 = lambda A: A.rearrange("(c p) n -> p c n", p=128)
            def stream(s):
                B = self.ps[4 * s:4 * s + 4]
                y0, y1, ysq = t([128, 512]), t([128, 512]), t([128, 512])
                m1, m2, m3 = t([128, 8]), t([128, 8]), t([128, 8])
                raf, bon, ga, zt = (t([128, 4, 128]) for _ in range(4))
                Rb = [t([128, 4, 128], BF16) for _ in range(3)]
                pg = t([128, 8, 128])
                macc, tmpm = t([128, 8, 128]), t([128, 8, 128])
                mb = t([128, 8, 128], BF16)
                xt = t([128, 8, 128])
                for j in range(NJ):
                    n0 = s * T + j * 128
                    r = 2 if j < 2 else s
                    for br in range(2):
                        DY, nm, nh, dvv, eps_ = ((self.YA, "R", 8, 64, 64e-5), (self.YB, "G", 4, 128, EPS))[br]
                        self.dma(y0[:, :], DY[0, n0:n0 + 128, :], [self.dd(nm + "Y", 0, n0 // 128)], [y0])
                        self.dma(y1[:, :], DY[1, n0:n0 + 128, :], [self.dd(nm + "Y", 1, n0 // 128)], [y1])
                        self.tt(V, y0[:, :], y0[:, :], y1[:, :], ALU.add, [y0, y1], [y0])
                        yv = y0[:, :].rearrange("p (h d) -> p h d", d=dvv)
                        self.tt(G, ysq[:, :], y0[:, :], y0[:, :], ALU.mult, [y0], [ysq])
                        self.red(V, m2[:, 0:nh], ysq[:, :].rearrange("p (h d) -> p h d", d=dvv), ALU.add, [ysq], [m2])
                        if br == 0:
                            self.red(V, m1[:, 0:nh], yv, ALU.add, [y0], [m1])
                            self.ts(V, m1[:, 0:nh], m1[:, 0:nh], 1.0 / dvv, None, ALU.mult, None, [m1], [m1])
                            self.tt(V, m3[:, 0:nh], m1[:, 0:nh], m1[:, 0:nh], ALU.mult, [m1], [m3])
                            self.stt(V, m2[:, 0:nh], m2[:, 0:nh], 1.0 / dvv, m3[:, 0:nh], ALU.mult, ALU.subtract, [m2, m3], [m2])
                            self.ts(V, m2[:, 0:nh], m2[:, 0:nh], eps_, None, ALU.add, None, [m2], [m2])
                            self.tt(V, yv, yv, m1[:, 0:nh].unsqueeze(2).to_broadcast([128, nh, dvv]), ALU.subtract, [y0, m1], [y0])
                        else:
                            self.ts(V, m2[:, 0:nh], m2[:, 0:nh], 1.0 / dvv, eps_, ALU.mult, ALU.add, [m2], [m2])
                        self.actf(m2[:, 0:nh], m2[:, 0:nh], AF.Sqrt, [m2], [m2])
                        self.recip(m2[:, 0:nh], m2[:, 0:nh], [m2], [m2])
                        self.tt(V, yv, yv, m2[:, 0:nh].unsqueeze(2).to_broadcast([128, nh, dvv]), ALU.mult, [y0, m2], [y0])
                        pb = B[br]
                        for c in range(4):
                            self.tr(pb[:, c * 128:(c + 1) * 128], y0[:, c * 128:(c + 1) * 128], self.ident[:, :], [y0, self.ident], [pb])
                        pbv = pb[:, :].rearrange("p (c t) -> p c t", c=4)
                        if br == 0:
                            for c in range(4):
                                self.ts(V, raf[:, c, :], pbv[:, c, :], self.pfv(l, "ln_g", c), self.pfv(l, "ln_b", c), ALU.mult, ALU.add, [pb, PFl], [raf])
                            self.dma(bon[:, :, :], fm4(self.BON)[:, :, n0:n0 + 128], [self.dd("BON", n0 // 128)], [bon])
                            self.dma(ga[:, :, :], fm4(self.GAs)[:, :, n0:n0 + 128], [self.dd("GAs", n0 // 128)], [ga])
                            self.tt(V, raf[:, :, :], raf[:, :, :], bon[:, :, :], ALU.add, [raf, bon], [raf])
                            self.tt(V, Rb[0][:, :, :], raf[:, :, :], ga[:, :, :], ALU.mult, [raf, ga], [Rb[0]])
                        else:
                            self.dma(zt[:, :, :], self.PTv[:, 27:31, n0:n0 + 128], [], [zt])
                            self.actf(zt[:, :, :], zt[:, :, :], AF.Silu, [zt], [zt])
                            self.stt(V, Rb[1][:, :, :], pbv, self.pfv(l, "gnorm", 0), zt[:, :, :], ALU.mult, ALU.mult, [pb, PFl, zt], [Rb[1]])
                    self.dma(Rb[2][:, :, :], fm4(self.RCs)[:, :, n0:n0 + 128], [self.dd("RCs", n0 // 128)], [Rb[2]])
                    for n in range(3):
                        self.dma(pg[:, :, :], pgv[n][:, :, n0:n0 + 128], [], [pg])
                        self.actf(pg[:, :, :], pg[:, :, :], AF.Sigmoid, [pg], [pg])
                        for m in range(8):
                            pb = B[2 + m // 4]
                            for k in range(4):
                                self.mm(pb[:, (m % 4) * 128:(m % 4 + 1) * 128], wbr[:, n * 4 + k, m * 128:(m + 1) * 128], Rb[n][:, k, :], k == 0, k == 3,
                                        [wbr, Rb[n]], [pb])
                        for hf in range(2):
                            pbv = B[2 + hf][:, :].rearrange("p (c t) -> p c t", c=4)
                            dst = macc if n == 0 else tmpm
                            self.tt(V, dst[:, hf * 4:(hf + 1) * 4, :], pbv, pg[:, hf * 4:(hf + 1) * 4, :], ALU.mult, [B[2 + hf], pg], [dst])
                        if n > 0:
                            self.tt(G, macc[:, :, :], macc[:, :, :], tmpm[:, :, :], ALU.add, [macc, tmpm], [macc])
                    self.cp(G, mb[:, :, :], macc[:, :, :], [macc], [mb])
                    self.dma(xt[:, :, :], self.XTv[:, :, n0:n0 + 128], self.dr("XT", n0, 128), [xt])
                    for m in range(8):
                        pb = B[m // 4]
                        for k in range(8):
                            self.mm(pb[:, (m % 4) * 128:(m % 4 + 1) * 128], wo[:, k, m * 128:(m + 1) * 128], mb[:, k, :], k == 0, k == 7, [wo, mb], [pb])
                    for m in range(8):
                        pb = B[m // 4]
                        self.stt(V, xt[:, m, :], pb[:, (m % 4) * 128:(m % 4 + 1) * 128], self.MOD[l][:, 16 + m, r:r + 1], xt[:, m, :], ALU.mult, ALU.add,
                                 [pb, self.MOD[l], xt], [xt])
                    self.dma(self.XTv[:, :, n0:n0 + 128], xt[:, :, :], [xt], self.dr("XT", n0, 128))
                    if f"XM{l}" in self.dbg:
                        pass
            self.run_interleaved([lambda: stream(0), lambda: stream(1)])
            self.barrier()

    def moe(self, l):
        V, G = self.dve, self.pool
        ps = self.ps
        with ExitStack() as es:
            def t(shape, dt=F32, e_=None):
                return self.sb(shape, dt, e_ or es)
            hT = t([128, 8, NT], BF16)
            WTf = t([16, NT])
            wr = t([128, 8, 16])
            rb = t([128, 16])
            self.dma(wr[:, :, :], self.w_router.rearrange("(k p) e -> p k e", p=128), [], [wr])
            self.dma(rb[:, :], self.rbias, [], [rb])
            with ExitStack() as es1:
                xts = [t([128, 8, 512], F32, es1) for _ in range(2)]
                sq = t([128, 8, 512], F32, es1)
                rs = t([128, 512], F32, es1)
                hf = t([128, 8, 512], F32, es1)
                sc, sel, sel2, eq, cm, wts = (t([128, 16], F32, es1) for _ in range(6))
                m1, m2, gs, gsel = (t([128, 4], F32, es1) for _ in range(4))
                gmx, wsum = t([128, 1], F32, es1), t([128, 1], F32, es1)
                v4 = lambda a: a[:, :].rearrange("p (g j) -> p g j", j=4)
                b4 = lambda a: a[:, :].unsqueeze(2).to_broadcast([128, 4, 4])
                for i, (n0, w, r) in enumerate(self.tiles()):
                    xt, _ = self.modulate_tile((xts[i % 2], sq, rs, None), n0, w, r, None, None, None, None)
                    for c in range(8):
                        self.stt(V, hf[:, c, :w], xt[:, c, :w], self.GF[l][:, c, r:r + 1], rs[:, :w], ALU.mult, ALU.mult, [xt, rs, self.GF[l]], [hf])
                        self.actf(hf[:, c, :w], hf[:, c, :w], AF.Identity, [hf, self.MOD[l]], [hf], bias=self.MOD[l][:, 24 + c, r:r + 1], scale=1.0)
                    self.cp(G, hT[:, :, n0:n0 + w], hf[:, :, :w], [hf], [hT])
                    for q in range(w // 128):
                        pR = ps[6]
                        for c in range(8):
                            self.mm(pR[:, 0:16], hf[:, c, q * 128:(q + 1) * 128], wr[:, c, :], c == 0, c == 7, [hf, wr], [pR])
                        self.actf(sc[:, :], pR[:, 0:16], AF.Sigmoid, [pR], [sc])
                        self.tt(V, sel[:, :], sc[:, :], rb[:, :], ALU.add, [sc, rb], [sel])
                        self.red(V, m1[:, :], v4(sel), ALU.max, [sel], [m1])
                        self.tt(V, v4(eq), v4(sel), b4(m1), ALU.is_equal, [sel, m1], [eq])
                        self.stt(V, sel2[:, :], eq[:, :], -1e9, sel[:, :], ALU.mult, ALU.add, [eq, sel], [sel2])
                        self.red(V, m2[:, :], v4(sel2), ALU.max, [sel2], [m2])
                        self.tt(V, gs[:, :], m1[:, :], m2[:, :], ALU.add, [m1, m2], [gs])
                        self.red(V, gmx[:, :], gs[:, :], ALU.max, [gs], [gmx])
                        self.ts(V, gsel[:, :], gs[:, :], gmx[:, 0:1], None, ALU.is_equal, None, [gs, gmx], [gsel])
                        self.tt(V, v4(cm), v4(sel), b4(m2), ALU.is_ge, [sel, m2], [cm])
                        self.tt(V, v4(cm), v4(cm), b4(gsel), ALU.mult, [cm, gsel], [cm])
                        self.tt(V, wts[:, :], sc[:, :], cm[:, :], ALU.mult, [sc, cm], [wts])
                        self.red(V, wsum[:, :], wts[:, :], ALU.add, [wts], [wsum])
                        self.recip(wsum[:, :], wsum[:, :], [wsum], [wsum])
                        self.ts(V, wts[:, :], wts[:, :], wsum[:, 0:1], None, ALU.mult, None, [wts, wsum], [wts])
                        pT = ps[7]
                        self.tr(pT[0:16, 0:128], wts[:, :], self.ident[:, :], [wts, self.ident], [pT])
                        self.cp(self.act, WTf[0:16, n0 + q * 128:n0 + (q + 1) * 128], pT[0:16, 0:128], [pT], [WTf])
                self.barrier()
            TG = 1152
            TW = 384
            yacc = t([128, 8, TG])
            wgb = [t([128, 8, 512], BF16) for _ in range(2)]
            wub = [t([128, 8, 512], BF16) for _ in range(2)]
            wdb = [t([128, 4, 1024], BF16) for _ in range(2)]
            wtb = t([128, TW])
            sg = [t([128, TW]) for _ in range(2)]
            actb = t([128, 4, TW], BF16)
            xt2 = t([128, 8, 128])
            def loadw(e):
                for (src, dstt) in ((self.weg[l, e], wgb[e % 2]), (self.weu[l, e], wub[e % 2]), (self.wed[l, e], wdb[e % 2])):
                    self.dma(dstt[:, :, :], src.rearrange("(k p) n -> p k n", p=128), [], [dstt], Q=self.pool)
            loadw(0)
            for g in range(NT // TG):
                for e in range(16):
                    gb, ub, db = wgb[e % 2], wub[e % 2], wdb[e % 2]
                    if not (g == NT // TG - 1 and e == 15):
                        loadw((e + 1) % 16)
                    for tt_ in range(TG // TW):
                        n0 = g * TG + tt_ * TW
                        self.mm(ps[4][:, :TW], self.SEL[0:16, e, :], WTf[0:16, n0:n0 + TW], True, True, [self.SEL, WTf], [ps[4]])
                        self.cp(self.act, wtb[:, :], ps[4][:, :TW], [ps[4]], [wtb])
                        for hc in range(4):
                            pg_, pu_ = ps[(hc % 2) * 2], ps[(hc % 2) * 2 + 1]
                            for k in range(8):
                                self.mm(pg_[:, :TW], gb[:, k, hc * 128:(hc + 1) * 128], hT[:, k, n0:n0 + TW], k == 0, k == 7, [gb, hT], [pg_])
                            for k in range(8):
                                self.mm(pu_[:, :TW], ub[:, k, hc * 128:(hc + 1) * 128], hT[:, k, n0:n0 + TW], k == 0, k == 7, [ub, hT], [pu_])
                            sg_ = sg[hc % 2]
                            self.actf(sg_[:, :], pg_[:, :TW], AF.Silu, [pg_], [sg_])
                            self.tt(V, sg_[:, :], sg_[:, :], pu_[:, :TW], ALU.mult, [sg_, pu_], [sg_])
                            self.tt(G, actb[:, hc, :], sg_[:, :], wtb[:, :], ALU.mult, [sg_, wtb], [actb])
                        for m in range(8):
                            pd = ps[4 + m % 4]
                            for hc in range(4):
                                self.mm(pd[:, :TW], db[:, hc, m * 128:(m + 1) * 128], actb[:, hc, :], hc == 0, hc == 3, [db, actb], [pd])
                            dst = yacc[:, m, tt_ * TW:(tt_ + 1) * TW]
                            if e == 0:
                                self.cp(self.act, dst, pd[:, :TW], [pd], [yacc])
                            else:
                                self.tt(V, dst, dst, pd[:, :TW], ALU.add, [yacc, pd], [yacc])
                for p_ in range(TG // 128):
                    n0 = g * TG + p_ * 128
                    tq = n0 % T
                    r = 2 if tq < TC else n0 // T
                    self.dma(xt2[:, :, :], self.XTv[:, :, n0:n0 + 128], self.dr("XT", n0, 128), [xt2])
                    for m in range(8):
                        self.stt(V, xt2[:, m, :], yacc[:, m, p_ * 128:(p_ + 1) * 128], self.MOD[l][:, 40 + m, r:r + 1], xt2[:, m, :], ALU.mult, ALU.add,
                                 [yacc, self.MOD[l], xt2], [xt2])
                    self.dma(self.XTv[:, :, n0:n0 + 128], xt2[:, :, :], [xt2], self.dr("XT", n0, 128))
            self.barrier()

    def final(self):
        V, G = self.dve, self.pool
        ps = self.ps
        with ExitStack() as es:
            def t(shape, dt=F32):
                return self.sb(shape, dt, es)
            xts = [t([128, 8, 128]) for _ in range(2)]
            sq = t([128, 8, 128])
            rs = t([128, 128])
            tmp = t([128, 8, 128])
            os_ = [t([128, 1024]) for _ in range(2)]
            i = 0
            for s in range(S):
                for j in range(2, NJ):
                    n0 = s * T + j * 128
                    xt = xts[i % 2]; o = os_[i % 2]; i += 1
                    self.dma(xt[:, :, :], self.XTv[:, :, n0:n0 + 128], self.dr("XT", n0, 128), [xt])
                    self.actf(sq[:, :, :], xt[:, :, :], AF.Square, [xt], [sq])
                    for c in range(8):
                        self.mm(ps[0][:, 0:128], self.ones[:, :], sq[:, c, :], c == 0, c == 7, [self.ones, sq], [ps[0]])
                    self.ts(V, rs[:, :], ps[0][:, 0:128], 1.0 / D, EPS, ALU.mult, ALU.add, [ps[0]], [rs])
                    self.actf(rs[:, :], rs[:, :], AF.Sqrt, [rs], [rs])
                    self.recip(rs[:, :], rs[:, :], [rs], [rs])
                    for c in range(8):
                        self.stt(V, tmp[:, c, :], xt[:, c, :], self.pfv(0, "norm_final", c), rs[:, :], ALU.mult, ALU.mult, [xt, rs, self.PFt[0]], [tmp])
                    for c in range(8):
                        pb = ps[1 + c // 4]
                        self.tr(pb[:, (c % 4) * 128:(c % 4 + 1) * 128], tmp[:, c, :], self.ident[:, :], [tmp, self.ident], [pb])
                    self.cp(self.act, o[:, 0:512], ps[1][:, :], [ps[1]], [o])
                    self.cp(V, o[:, 512:1024], ps[2][:, :], [ps[2]], [o])
                    self.dma(self.y[s, (j - 2) * 128:(j - 1) * 128, :], o[:, :], [o], [self.dd("y", s, j)])
            self.barrier()

    def rwkv_scan(self, l, gdn=False):
        V, G = self.dve, self.pool
        ps = self.ps
        nh = 4 if gdn else 8
        dv = 512 // nh
        DKQ, DMAT, DBC, DV_, DPC, DY, nm = ((self.GKQ, self.GMAT, self.GBC, self.GV, self.GPC, self.YB, 'G') if gdn else (self.RKQ, self.RMAT, self.RBC, self.RV, self.RPC, self.YA, 'R'))
        with ExitStack() as es:
            def t(shape, dt=F32):
                return self.sb(shape, dt, es)
            chains = [(s, d) for s in range(S) for d in range(2)]
            order = {0: list(range(NJ)), 1: [1, 0] + list(range(NJ - 1, 1, -1))}
            bufs = []
            for _ in chains:
                ld = [(t([128, 4, 256], BF16), t([128, nh, 512], BF16), t([128, 1024], BF16), t([128, 512], BF16), t([128, 8])) for _ in range(2)]
                bufs.append(dict(ld=ld, H=t([128, 4, dv]), Hz=t([128, nh, dv], BF16), Rn=t([128, nh, dv], BF16),
                                 Ubz=[t([128, nh, dv], BF16) for _ in range(2)], Vz=[t([128, 512], BF16) for _ in range(2)],
                                 Yt=t([128, 512])))
            def chain(ci, s, d):
                for i in range(NJ):
                    j = order[d][i]
                    n0 = s * T + j * 128
                    b = bufs[ci]
                    KQ, MAT, BC, Vt, pc = b["ld"][i % 2]
                    H, Hz, Rn, Ubz, Vz, Yt = b["H"], b["Hz"], b["Rn"], b["Ubz"], b["Vz"], b["Yt"]
                    self.dma(KQ[:, :, :].rearrange("p m t -> p (m t)"), DKQ[s, d, j], [self.dd(nm + "KQ", s, d, j)], [KQ])
                    self.dma(MAT[:, :, :].rearrange("p h t -> p (h t)"), DMAT[s, d, j], [self.dd(nm + "MAT", s, d, j)], [MAT])
                    self.dma(BC[:, :], DBC[s, d, j], [self.dd(nm + "BC", s, d, j)], [BC])
                    self.dma(Vt[:, :], DV_[s, j], [self.dd(nm + "V", s, j)], [Vt])
                    self.dma(pc[:, :], DPC[s, d, j], [self.dd(nm + "PC", s, d, j)], [pc])
                    if i == 0:
                        self.memset(G, H[:, :, :], 0.0, [H])
                        self.memset(G, Hz[:, :, :], 0.0, [Hz])
                        self.memset(G, Rn[:, :, :], 0.0, [Rn])
                        for c in range(2):
                            self.memset(G, Ubz[c][:, :, :], 0.0, [Ubz[c]])
                            self.memset(G, Vz[c][:, :], 0.0, [Vz[c]])
                    for c in range(2):
                        self.cp(G, Vz[c][c * 64:c * 64 + 64, :], Vt[c * 64:c * 64 + 64, :], [Vt], [Vz[c]])
                    pA, pB = ps[2 * ci], ps[2 * ci + 1]
                    pAv = pA[:, :].rearrange("p (h v) -> p h v", v=dv)
                    pBv = pB[:, :].rearrange("p (h v) -> p h v", v=dv)
                    if not gdn:
                        pBe = pB[:, :].rearrange("p (m e v) -> p m e v", e=2, v=64)
                        Hze = Hz[:, :, :].rearrange("p (m e) v -> p m e v", e=2)
                    pcv = pc[:, :].rearrange("p (m c) -> p m c", c=2)
                    for c in ([0, 1] if d == 0 else [1, 0]):
                        cs = slice(c * 64, c * 64 + 64)
                        Ub = Ubz[c]
                        for h in range(nh):
                            m = h if gdn else h // 2
                            self.mm(pAv[:, h, :], KQ[:, m, 0:128], Hz[:, h, :], True, False, [KQ, Hz], [pA])
                            self.mm(pAv[:, h, :], MAT[:, h, 256:384], Vt[:, h * dv:(h + 1) * dv], False, True, [MAT, Vt], [pA])
                        self.ts(V, Rn[cs, :, :], pAv[cs, :, :], -1.0, None, ALU.mult, None, [pA], [Rn])
                        for h in range(nh):
                            self.mm(pBv[:, h, :], MAT[:, h, 0:128], Rn[:, h, :], True, True, [MAT, Rn], [pB])
                        self.cp(self.act, Ub[cs, :, :], pBv[cs, :, :], [pB], [Ub])
                        for h in range(nh):
                            m = h if gdn else h // 2
                            self.mm(pAv[:, h, :], KQ[:, m, 128:256], Hz[:, h, :], True, False, [KQ, Hz], [pA])
                            self.mm(pAv[:, h, :], MAT[:, h, 128:256], Ub[:, h, :], False, False, [MAT, Ub], [pA])
                            self.mm(pAv[:, h, :], MAT[:, h, 384:512], Vt[:, h * dv:(h + 1) * dv], False, True, [MAT, Vt], [pA])
                        self.cp(V, Yt[cs, :], pA[cs, :], [pA], [Yt])
                        for h in range(nh):
                            m = h if gdn else h // 2
                            self.mm(pBv[:, h, :], BC[:, m * 128:(m + 1) * 128], Ub[:, h, :], True, False, [BC, Ub], [pB])
                            self.mm(pBv[:, h, :], BC[:, 512 + m * 128:512 + (m + 1) * 128], Vz[c][:, h * dv:(h + 1) * dv], False, True, [BC, Vz[c]], [pB])
                        if gdn:
                            self.tt(V, H[:, :, :], H[:, :, :], pcv[:, :, c:c + 1].to_broadcast([128, 4, dv]), ALU.mult, [H, pc], [H])
                            self.tt(V, H[:, :, :], H[:, :, :], pBv[:, :, :], ALU.add, [H, pB], [H])
                            self.cp(self.act, Hz[:, :, :], H[:, :, :], [H], [Hz])
                        else:
                            for e in range(2):
                                rows = slice(e * 64, e * 64 + 64)
                                self.tt(V, H[rows, :, :], H[rows, :, :], pBe[rows, :, e, :], ALU.add, [H, pB], [H])
                            self.tt(V, H[:, :, :], H[:, :, :], pcv[:, :, c:c + 1].to_broadcast([128, 4, 64]), ALU.mult, [H, pc], [H])
                            for e in range(2):
                                rows = slice(e * 64, e * 64 + 64)
                                self.cp(self.act, Hze[rows, :, e, :], H[rows, :, :], [H], [Hz])
                    self.dma(DY[d, n0:n0 + 128, :], Yt[:, :], [Yt], [self.dd(nm + "Y", d, n0 // 128)])
            self.run_interleaved([(lambda ci=ci, s=s, d=d: chain(ci, s, d)) for ci, (s, d) in enumerate(chains)])
            self.barrier()

    def build(self):
        try:
            self.build_()
        except StopBuild:
            self.es2 = None
            self.finish()

    def build_(self):
        self.consts()
        self.gdn_setup()
        self.conf_setup()
        self.phase0()
        if self.stop == "0":
            return self.finish()
        for l in range(self.nlayers):
            self.phaseA(l)
            if self.stop == f"A{l}":
                return self.finish()
            self.phaseB(l)
            if self.stop == f"B{l}":
                return self.finish()
            self.rwkv_prep(l)
            if self.stop == f"C{l}":
                return self.finish()
            self.rwkv_scan(l)
            if self.stop == f"D{l}":
                return self.finish()
            self.gdn_prep(l)
            if self.stop == f"E{l}":
                return self.finish()
            self.rwkv_scan(l, gdn=True)
            if self.stop == f"F{l}":
                return self.finish()
            self.conformer(l)
            if self.stop == f"G{l}":
                return self.finish()
            self.merge(l)
            if self.stop == f"H{l}":
                return self.finish()
            self.moe(l)
            if self.stop == f"I{l}":
                return self.finish()
        self.final()
        self.finish()
```

```python
import threading
import numpy as np
from contextlib import ExitStack
import concourse.bass as bass
import concourse.mybir as mybir
from concourse.bass_utils import run_bass_kernel_spmd

F32 = mybir.dt.float32
BF16 = mybir.dt.bfloat16
ALU = mybir.AluOpType
AF = mybir.ActivationFunctionType
AX = mybir.AxisListType

D = 1024
S = 2
TC = 256
TL = 2048
T = TC + TL
NT = S * T
L = 2
NIN = 8080
NDS = 24
NDS_SW = 8


class Dep:
    __slots__ = ("w", "r")

    def __init__(self):
        self.w = None
        self.r = {}


class Tl:
    def __init__(self, t):
        self.t = t
        self.d = Dep()

    def __getitem__(self, k):
        return self.t[k]


class Eng:
    def __init__(self, name, be, sem):
        self.key = name
        self.be = be
        self.sem = sem
        self.n = 0
        self.waited = {}


def _d(x):
    return x.d if hasattr(x, "d") else x


class KB:
    def __init__(self, dbg=()):
        self.nc = nc = bass.Bass("TRN2", target_bir_lowering=False)
        self.es = ExitStack()
        self.dbg = set(dbg)
        e = self.es.enter_context
        self.pe = Eng("pe", nc.tensor, e(nc.semaphore("s_pe")))
        self.act = Eng("act", nc.scalar, e(nc.semaphore("s_act")))
        self.dve = Eng("dve", nc.vector, e(nc.semaphore("s_dve")))
        self.pool = Eng("pool", nc.gpsimd, e(nc.semaphore("s_pool")))
        self.sp = Eng("sp", nc.sync, e(nc.semaphore("s_sp")))
        self.engs = [self.pe, self.act, self.dve, self.pool, self.sp]
        self.dsem = [e(nc.semaphore(f"s_d{i}")) for i in range(NDS + NDS_SW)]
        self.dcnt = [0] * (NDS + NDS_SW)
        self.drr = 0
        self.drr_sw = 0
        self.ddeps = {}
        self.ntile = 0
        self.yielders = {}

    def sb(self, shape, dt=F32, es=None):
        self.ntile += 1
        t = (es or self.es).enter_context(self.nc.sbuf_tensor(f"t{self.ntile}", list(shape), dt))
        return Tl(t)

    def psum(self, shape, dt=F32, es=None):
        self.ntile += 1
        t = (es or self.es).enter_context(self.nc.psum_tensor(f"p{self.ntile}", list(shape), dt))
        return Tl(t)

    def dram(self, name, shape, dt=F32, kind=None):
        if kind is None:
            kind = "ExternalOutput" if name in self.dbg else "Internal"
        return self.nc.dram_tensor(name, list(shape), dt, kind=kind).ap()

    def dd(self, *key):
        d = self.ddeps.get(key)
        if d is None:
            d = self.ddeps[key] = Dep()
        return d

    def dr(self, name, n0, w):
        return [self.dd(name, i) for i in range(n0 // 128, (n0 + w + 127) // 128)]

    def _sync(self, E, R, W):
        need = {}

        def upd(tok):
            k, sem, val = tok
            if k not in need or need[k][1] < val:
                need[k] = (sem, val)

        for d in R:
            d = _d(d)
            if d.w:
                upd(d.w)
        for d in W:
            d = _d(d)
            if d.w:
                upd(d.w)
            for k, (sem, val) in d.r.items():
                if k != E.key:
                    upd((k, sem, val))
        for k, (sem, val) in need.items():
            if k == E.key and E is self.pe:
                continue
            if E.waited.get(k, 0) < val:
                E.be.wait_ge(sem, val)
                E.waited[k] = val

    def _mark(self, tok, R, W):
        k, sem, val = tok
        for d in R:
            _d(d).r[k] = (sem, val)
        for d in W:
            d = _d(d)
            d.w = tok
            d.r = {}

    def op(self, E, fn, R, W):
        self._sync(E, R, W)
        ins = fn()
        E.n += 1
        ins.then_inc(E.sem, 1)
        self._mark((E.key, E.sem, E.n), R, W)
        self._yield()

    def dma(self, out, in_, R, W, Q=None, **kw):
        Q = Q or self.sp
        self._sync(Q, R, W)
        sw = Q is self.pool
        if sw:
            s = NDS + self.drr_sw
            self.drr_sw = (self.drr_sw + 1) % NDS_SW
        else:
            s = self.drr
            self.drr = (s + 1) % NDS
        sem = self.dsem[s]
        k = ("d", s)
        if self.dcnt[s] > 0 and Q.waited.get(k, 0) < 16 * self.dcnt[s]:
            Q.be.wait_ge(sem, 16 * self.dcnt[s])
            Q.waited[k] = 16 * self.dcnt[s]
        Q.be.dma_start(out=out, in_=in_, **kw).then_inc(sem, 16)
        self.dcnt[s] += 1
        self._mark((k, sem, 16 * self.dcnt[s]), R, W)
        self._yield()

    def _yield(self):
        if self.yielders:
            y = self.yielders.get(threading.get_ident())
            if y:
                y()

    def run_interleaved(self, fns):
        n = len(fns)
        state = {"turn": 0, "done": [False] * n, "exc": None}
        cv = threading.Condition()

        def advance(i):
            for k in range(1, n + 1):
                nx = (i + k) % n
                if not state["done"][nx]:
                    state["turn"] = nx
                    break
            else:
                state["turn"] = -1
            cv.notify_all()

        def yielder(i):
            def y():
                with cv:
                    advance(i)
                    while state["turn"] != i:
                        cv.wait()
            return y

        def worker(i):
            with cv:
                while state["turn"] != i:
                    cv.wait()
            self.yielders[threading.get_ident()] = yielder(i)
            try:
                fns[i]()
            except BaseException as e:
                state["exc"] = e
            finally:
                self.yielders.pop(threading.get_ident(), None)
                with cv:
                    state["done"][i] = True
                    advance(i)

        ths = [threading.Thread(target=worker, args=(i,)) for i in range(n)]
        for th in ths:
            th.start()
        for th in ths:
            th.join()
        if state["exc"] is not None:
            raise state["exc"]

    def barrier(self):
        for E in self.engs:
            for E2 in self.engs:
                if E2 is not E and E2.n > 0 and E.waited.get(E2.key, 0) < E2.n:
                    E.be.wait_ge(E2.sem, E2.n)
                    E.waited[E2.key] = E2.n
            for s in range(NDS + NDS_SW):
                k = ("d", s)
                if self.dcnt[s] > 0 and E.waited.get(k, 0) < 16 * self.dcnt[s]:
                    E.be.wait_ge(self.dsem[s], 16 * self.dcnt[s])
                    E.waited[k] = 16 * self.dcnt[s]

    def mm(self, out, lhsT, rhs, start, stop, R, W):
        self.op(self.pe, lambda: self.nc.tensor.matmul(out, lhsT=lhsT, rhs=rhs, start=start, stop=stop), R, W)

    def tr(self, out, in_, ident, R, W):
        self.op(self.pe, lambda: self.nc.tensor.transpose(out, in_, ident), R, W)

    def actf(self, out, in_, func, R, W, bias=None, scale=None):
        kw = {}
        if bias is not None:
            kw["bias"] = bias
        if scale is not None:
            kw["scale"] = scale
        self.op(self.act, lambda: self.nc.scalar.activation(out=out, in_=in_, func=func, **kw), R, W)

    def ts(self, E, out, in0, s1, s2, op0, op1, R, W):
        if op1 is None:
            self.op(E, lambda: E.be.tensor_scalar(out=out, in0=in0, scalar1=s1, scalar2=None, op0=op0), R, W)
        else:
            self.op(E, lambda: E.be.tensor_scalar(out=out, in0=in0, scalar1=s1, scalar2=s2, op0=op0, op1=op1), R, W)

    def tt(self, E, out, in0, in1, op, R, W):
        self.op(E, lambda: E.be.tensor_tensor(out=out, in0=in0, in1=in1, op=op), R, W)

    def stt(self, E, out, in0, scalar, in1, op0, op1, R, W):
        self.op(E, lambda: E.be.scalar_tensor_tensor(out=out, in0=in0, scalar=scalar, in1=in1, op0=op0, op1=op1), R, W)

    def cp(self, E, out, in_, R, W):
        if E is self.act:
            self.op(E, lambda: self.nc.scalar.copy(out=out, in_=in_), R, W)
        else:
            self.op(E, lambda: E.be.tensor_copy(out=out, in_=in_), R, W)

    def recip(self, out, in_, R, W):
        self.op(self.dve, lambda: self.nc.vector.reciprocal(out=out, in_=in_), R, W)

    def memset(self, E, ap, v, W):
        self.op(E, lambda: E.be.memset(ap, v), [], W)


class PF:
    def __init__(self):
        self.cols = {}
        self.n = 0

    def add(self, name, nch):
        self.cols[name] = (self.n, nch)
        self.n += nch
        return self.cols[name][0]


def pf_layout():
    pf = PF()
    for nm, nch in [("norm_mix", 8), ("norm_ffn", 8), ("b_ada", 48), ("mu0", 15), ("mu1", 15),
                    ("w0_0", 4), ("w0_1", 4), ("a0_0", 4), ("a0_1", 4), ("kk", 4), ("ka", 4), ("rk", 4),
                    ("ln_g", 4), ("ln_b", 4), ("gconv", 60), ("gnorm", 1), ("alog", 1), ("dtb", 1),
                    ("cdw", 124), ("cdwb", 4), ("clng", 4), ("clnb", 4), ("norm_final", 8)]:
        pf.add(nm, nch)
    return pf


def _fm(v, nch):
    return np.ascontiguousarray(np.asarray(v, np.float32).reshape(nch, 128).T)


def pack_pf(inp, l):
    pf = pf_layout()
    out = np.zeros((128, pf.n), np.float32)

    def put(nm, arr):
        o, n = pf.cols[nm]
        out[:, o:o + n] = arr

    put("norm_mix", _fm(inp["norm_mix"][l], 8))
    put("norm_ffn", _fm(inp["norm_ffn"][l], 8))
    put("b_ada", _fm(inp["b_ada"][l], 48))
    put("mu0", _fm(inp["rwkv_mu"][l, 0], 15))
    put("mu1", _fm(inp["rwkv_mu"][l, 1], 15))
    for d in range(2):
        put(f"w0_{d}", _fm(inp["rwkv_w0"][l, d], 4))
        put(f"a0_{d}", _fm(inp["rwkv_a0"][l, d], 4))
    put("kk", _fm(inp["rwkv_kk"][l], 4))
    put("ka", _fm(inp["rwkv_ka"][l], 4))
    put("rk", _fm(inp["rwkv_rk"][l].reshape(-1), 4))
    put("ln_g", _fm(inp["rwkv_ln_g"][l], 4))
    put("ln_b", _fm(inp["rwkv_ln_b"][l], 4))
    gc = np.concatenate([_fm(inp["gdn_conv"][l, k], 12) for k in range(5)], axis=1)
    put("gconv", gc)
    put("gnorm", _fm(inp["gdn_norm"][l], 1))
    al = np.zeros((128, 1), np.float32); al[0:8, 0] = np.asarray(inp["gdn_A_log"][l]).reshape(-1)
    db = np.zeros((128, 1), np.float32); db[0:8, 0] = np.asarray(inp["gdn_dt_bias"][l]).reshape(-1)
    put("alog", al)
    put("dtb", db)
    cd = np.concatenate([_fm(inp["conf_dw"][l, k], 4) for k in range(31)], axis=1)
    put("cdw", cd)
    put("cdwb", _fm(inp["conf_dw_b"][l], 4))
    put("clng", _fm(inp["conf_ln_g"][l], 4))
    put("clnb", _fm(inp["conf_ln_b"][l], 4))
    put("norm_final", _fm(inp["norm_final"], 8))
    return out


EPS = 1e-6


class Prog(KB):
    def __init__(self, dbg=(), stop=None, nlayers=L):
        super().__init__(dbg)
        self.stop = stop
        self.nlayers = nlayers
        nc = self.nc
        self.pf = pf_layout()

        def inp(name, shape):
            return nc.dram_tensor(name, list(shape), F32, kind="ExternalInput").ap()

        self.x = inp("x", [S, TL, D])
        self.ctx = inp("ctx", [S, TC, D])
        self.cvecT = inp("cvecT", [D, 3])
        self.pfp = inp("pfp", [L, 128, self.pf.n])
        self.w_ada = inp("w_ada", [L, D, 6 * D])
        self.w_in = inp("w_in", [L, D, NIN])
        self.rw2 = inp("rwkv_w2", [L, 2, 64, 512])
        self.ra2 = inp("rwkv_a2", [L, 2, 64, 512])
        self.rg2 = inp("rwkv_g2", [L, 128, 512])
        self.w_branch = inp("w_branch", [L, 3, 512, D])
        self.w_out = inp("w_out", [L, D, D])
        self.w_router = inp("w_router", [D, 16])
        self.rbias = inp("rbias", [128, 16])
        self.weg = inp("w_e_gate", [L, 16, D, 512])
        self.weu = inp("w_e_up", [L, 16, D, 512])
        self.wed = inp("w_e_down", [L, 16, 512, D])
        self.y = nc.dram_tensor("y", [S, TL, D], F32, kind="ExternalOutput").ap()
        self.XT = self.dram("XT", [D, NT])
        self.XTv = self.XT.rearrange("(c p) n -> p c n", p=128)
        self.PT = self.dram("PT", [NIN, NT])
        self.ps = [self.psum([128, 512], F32) for _ in range(8)]
        self.ident = self.sb([128, 128], F32)
        self.identb = self.sb([128, 128], BF16)
        self.ones = self.sb([128, 128], F32)
        self.PFt = [self.sb([128, self.pf.n], F32) for _ in range(L)]
        self.scT = self.sb([128, 8, 3], F32)
        self.MOD = [self.sb([128, 48, 3], F32) for _ in range(L)]
        self.GM = [self.sb([128, 8, 3], F32) for _ in range(L)]
        self.GF = [self.sb([128, 8, 3], F32) for _ in range(L)]

    def pfv(self, l, name, c=None, n=1):
        o, nch = self.pf.cols[name]
        if c is None:
            return self.PFt[l][:, o:o + nch]
        return self.PFt[l][:, o + c:o + c + n]

    def consts(self):
        nc = self.nc
        P = self.pool
        self.memset(P, self.ident[:, :], 0.0, [self.ident])
        self.op(P, lambda: nc.gpsimd.affine_select(out=self.ident[:, :], in_=self.ident[:, :], pattern=[[-1, 128]],
                                                   compare_op=ALU.not_equal, fill=1.0, base=0, channel_multiplier=1),
                [self.ident], [self.ident])
        self.cp(P, self.identb[:, :], self.ident[:, :], [self.ident], [self.identb])
        self.memset(P, self.ones[:, :], 1.0, [self.ones])
        for l in range(L):
            self.dma(self.PFt[l][:, :], self.pfp[l], [], [self.PFt[l]])
        cv = self.sb([128, 8, 3], F32)
        self.dma(cv[:, :, :], self.cvecT.rearrange("(c p) r -> p c r", p=128), [], [cv])
        self.actf(self.scT[:, :, :], cv[:, :, :], AF.Silu, [cv], [self.scT])

    def phase0(self):
        with ExitStack() as es:
            xin = [self.sb([128, 1024], F32, es) for _ in range(2)]
            xo = [self.sb([128, 8, 128], F32, es) for _ in range(2)]
            i = 0
            for s in range(S):
                for j in range(T // 128):
                    n0 = s * T + j * 128
                    src = self.ctx[s, j * 128:(j + 1) * 128, :] if j < 2 else self.x[s, (j - 2) * 128:(j - 1) * 128, :]
                    a = xin[i % 2]
                    o = xo[i % 2]
                    self.dma(a[:, :], src, [], [a])
                    for c in range(8):
                        pb = self.ps[(i % 2) * 2 + c // 4]
                        self.tr(pb[:, (c % 4) * 128:(c % 4 + 1) * 128], a[:, c * 128:(c + 1) * 128], self.ident[:, :],
                                [a, self.ident], [pb])
                    for hf in range(2):
                        pb = self.ps[(i % 2) * 2 + hf]
                        self.cp(self.act if hf == 0 else self.dve, o[:, hf * 4:(hf + 1) * 4, :],
                                pb[:, :].rearrange("p (c t) -> p c t", c=4), [pb], [o])
                    self.dma(self.XTv[:, :, n0:n0 + 128], o[:, :, :], [o], self.dr("XT", n0, 128))
                    i += 1
            self.barrier()

    def phaseA(self, l):
        with ExitStack() as es:
            wa = [self.sb([128, 8, 768], F32, es) for _ in range(2)]
            pm = self.ps[0]
            wav = self.w_ada[l].rearrange("(k p) n -> p k n", p=128)
            for mg in range(8):
                w = wa[mg % 2]
                for q in range(4):
                    self.dma(w[:, q * 2:(q + 1) * 2, :], wav[:, q * 2:(q + 1) * 2, mg * 768:(mg + 1) * 768], [], [w])
                for m in range(6):
                    mm_ = mg * 6 + m
                    for k in range(8):
                        self.mm(pm[:, mm_ * 3:(mm_ + 1) * 3], w[:, k, m * 128:(m + 1) * 128], self.scT[:, k, :], k == 0, k == 7,
                                [w, self.scT], [pm])
            mod = self.MOD[l]
            self.tt(self.dve, mod[:, :, :], pm[:, 0:144].rearrange("p (m r) -> p m r", r=3),
                    self.pfv(l, "b_ada").unsqueeze(2).to_broadcast([128, 48, 3]), ALU.add, [pm, self.PFt[l]], [mod])
            for (G, mi, nm) in ((self.GM[l], 1, "norm_mix"), (self.GF[l], 4, "norm_ffn")):
                self.ts(self.dve, G[:, :, :], mod[:, mi * 8:(mi + 1) * 8, :], 1.0, None, ALU.add, None, [mod], [G])
                self.dump(f"G1{l}{mi}", G, G[:, :, :], [128, 8, 3])
                self.tt(self.dve, G[:, :, :], G[:, :, :], self.pfv(l, nm).unsqueeze(2).to_broadcast([128, 8, 3]), ALU.mult,
                        [G, self.PFt[l]], [G])
            self.dump(f"MOD{l}", mod, mod[:, :, :], [128, 48, 3])
            self.dump(f"GM{l}", self.GM[l], self.GM[l][:, :, :], [128, 8, 3])
            self.barrier()

    def tiles(self):
        out = []
        for s in range(S):
            out.append((s * T, TC, 2))
            for j in range(TL // 512):
                out.append((s * T + TC + j * 512, 512, s))
        return out

    def modulate_tile(self, es_bufs, n0, w, r, G, shift_col, mod, out_fn):
        xt, sq, rs, tmp = es_bufs
        psS = self.ps[7]
        self.dma(xt[:, :, :w], self.XTv[:, :, n0:n0 + w], self.dr("XT", n0, w), [xt])
        self.actf(sq[:, :, :w], xt[:, :, :w], AF.Square, [xt], [sq])
        for c in range(8):
            self.mm(psS[:, :w], self.ones[:, :], sq[:, c, :w], c == 0, c == 7, [self.ones, sq], [psS])
        self.ts(self.dve, rs[:, :w], psS[:, :w], 1.0 / D, EPS, ALU.mult, ALU.add, [psS], [rs])
        self.actf(rs[:, :w], rs[:, :w], AF.Sqrt, [rs], [rs])
        self.recip(rs[:, :w], rs[:, :w], [rs], [rs])
        return xt, rs

    def phaseB(self, l):
        with ExitStack() as es:
            hT = self.sb([128, 8, NT], BF16, es)
            with ExitStack() as es1:
                xts = [self.sb([128, 8, 512], F32, es1) for _ in range(2)]
                sq = self.sb([128, 8, 512], F32, es1)
                rs = self.sb([128, 512], F32, es1)
                tmps = [self.sb([128, 512], F32, es1) for _ in range(2)]
                for i, (n0, w, r) in enumerate(self.tiles()):
                    xt, _ = self.modulate_tile((xts[i % 2], sq, rs, None), n0, w, r, None, None, None, None)
                    for c in range(8):
                        tmp = tmps[c % 2]
                        self.stt(self.dve, tmp[:, :w], xt[:, c, :w], self.GM[l][:, c, r:r + 1], rs[:, :w], ALU.mult, ALU.mult,
                                 [xt, rs, self.GM[l]], [tmp])
                        self.actf(hT[:, c, n0:n0 + w], tmp[:, :w], AF.Identity, [tmp, self.MOD[l]], [hT],
                                  bias=self.MOD[l][:, c, r:r + 1], scale=1.0)
                if "HT" in self.dbg:
                    hd = self.dram("HT", [128, 8, NT], BF16)
                    self.dma(hd, hT[:, :, :], [hT], [self.dd("HTd")])
                self.barrier()
            wbf = [self.sb([128, 8, 1024], BF16, es) for _ in range(2)]
            ost = [self.sb([128, 512], F32, es) for _ in range(4)]
            no = 0
            def loadw(g):
                c0 = g * 1024
                cw = min(1024, NIN - c0)
                self.dma(wbf[g % 2][:, :, :cw], self.w_in[l][:, c0:c0 + cw].rearrange("(k p) n -> p k n", p=128), [], [wbf[g % 2]], Q=self.pool)
            loadw(0)
            for g in range(8):
                c0 = g * 1024
                cw = min(1024, NIN - c0)
                wb = wbf[g % 2]
                if g < 7:
                    loadw(g + 1)
                nm = (cw + 127) // 128
                for tt_ in range(NT // 512):
                    n0 = tt_ * 512
                    for m in range(nm):
                        mw = min(128, cw - m * 128)
                        pb = self.ps[no % 6]
                        for k in range(8):
                            self.mm(pb[:mw, :], wb[:, k, m * 128:m * 128 + mw], hT[:, k, n0:n0 + 512], k == 0, k == 7,
                                    [wb, hT], [pb])
                        o = ost[no % 4]
                        self.cp(self.act if no % 2 == 0 else self.dve, o[:mw, :], pb[:mw, :], [pb], [o])
                        self.dma(self.PT[c0 + m * 128:c0 + m * 128 + mw, n0:n0 + 512], o[:mw, :], [o],
                                 [self.dd("PT", (c0 + m * 128) // 128, j) for j in range(n0 // 128, n0 // 128 + 4)])
                        no += 1
            self.barrier()

    def dump(self, name, tile, ap, shape, dt=F32):
        if name in self.dbg:
            d = self.dram(name, shape, dt)
            self.dma(d, ap, [tile], [self.dd(name)])

    def finish(self):
        self.barrier()

    def build(self):
        self.consts()
        self.phase0()
        if self.stop == "0":
            return self.finish()
        for l in range(self.nlayers):
            self.phaseA(l)
            if self.stop == f"A{l}":
                return self.finish()
            self.phaseB(l)
            if self.stop == f"B{l}":
                return self.finish()
        self.finish()


def make_in_maps(inp):
    ncores = 8
    pfp = np.stack([pack_pf(inp, l) for l in range(L)])
    rbias = np.ascontiguousarray(np.broadcast_to(np.asarray(inp["router_bias"], np.float32)[None, :], (128, 16)))
    maps = []
    for i in range(ncores):
        cv = np.stack([inp["c"][2 * i], inp["c"][2 * i + 1], inp["c_ctx"]], axis=1).astype(np.float32)
        m = {
            "x": np.ascontiguousarray(inp["x"][2 * i:2 * i + 2]),
            "ctx": np.ascontiguousarray(inp["ctx"][2 * i:2 * i + 2]),
            "cvecT": np.ascontiguousarray(cv),
            "pfp": pfp, "rbias": rbias,
        }
        for k in ("w_ada", "w_in", "rwkv_w2", "rwkv_a2", "rwkv_g2", "w_branch", "w_out", "w_router",
                  "w_e_gate", "w_e_up", "w_e_down"):
            m[k] = np.ascontiguousarray(inp[k], dtype=np.float32)
        maps.append(m)
    return maps


def kernel(**inputs):
    inp = {k: np.asarray(v) for k, v in inputs.items()}
    prog = Prog2()
    prog.build()
    maps = make_in_maps(inp)
    res = run_bass_kernel_spmd(prog.nc, maps, core_ids=list(range(8)))
    return np.concatenate([r["y"] for r in res.results], axis=0).astype(np.float32)


CDEC = 0.6065306597126334
NJ = T // 128


def seg_bounds(j):
    return (j == 0 or j == 2), (j == 1 or j == NJ - 1)


class StopBuild(Exception):
    pass


class Prog2(Prog):
    cut = None

    def ck(self, n):
        if self.cut == n:
            raise StopBuild()

    def __init__(self, **kw):
        super().__init__(**kw)
        self.PTv = self.PT[0:8064, :].rearrange("(c p) n -> p c n", p=128)
        self.RKQ = self.dram("RKQ", [S, 2, NJ, 128, 4 * 256], BF16)
        self.RMAT = self.dram("RMAT", [S, 2, NJ, 128, 8 * 512], BF16)
        self.RBC = self.dram("RBC", [S, 2, NJ, 128, 1024], BF16)
        self.RV = self.dram("RV", [S, NJ, 128, 512], BF16)
        self.RPC = self.dram("RPC", [S, 2, NJ, 128, 8], F32)
        self.GAs = self.dram("GAs", [512, NT])
        self.BON = self.dram("BON", [512, NT])
        self.YA = self.dram("YA", [2, NT, 512])
        self.bones = self.sb([128, 128], F32)
        self.MU = self.sb([128, 256], F32)
        self.ML = self.sb([128, 256], F32)
        self.RM = self.sb([128, 128], F32)

    def consts(self):
        super().consts()
        nc = self.nc
        P = self.pool
        self.memset(P, self.bones[:, :], 0.0, [self.bones])
        self.memset(P, self.bones[0:64, 0:64], 1.0, [self.bones])
        self.memset(P, self.bones[64:128, 64:128], 1.0, [self.bones])
        self.memset(P, self.RM[:, :], 1.0, [self.RM])
        self.memset(P, self.RM[:, 0:1], 0.0, [self.RM])
        self.memset(P, self.RM[:, 64:65], 0.0, [self.RM])
        for (Mt, off, cmp_, sg) in ((self.MU, 0, ALU.is_gt, 1), (self.MU, 128, ALU.is_ge, 1), (self.ML, 0, ALU.is_gt, -1), (self.ML, 128, ALU.is_ge, -1)):
            sl = Mt[:, off:off + 128]
            self.memset(P, sl, 1.0, [Mt])
            self.op(P, lambda sl=sl, cmp_=cmp_, sg=sg: nc.gpsimd.affine_select(out=sl, in_=sl, pattern=[[sg, 128]], compare_op=cmp_, fill=0.0,
                                                                              base=0, channel_multiplier=-sg), [Mt], [Mt])
            self.memset(P, Mt[0:64, off + 64:off + 128], 0.0, [Mt])
            self.memset(P, Mt[64:128, off:off + 64], 0.0, [Mt])

    def load_halo(self, dst, c0, nch, s, j, hw):
        n0 = s * T + j * 128
        lb, rb = seg_bounds(j)
        lo = 0 if lb else hw
        hi = 0 if rb else hw
        if lb:
            self.memset(self.pool, dst[:, :, 0:hw], 0.0, [dst])
        if rb:
            self.memset(self.pool, dst[:, :, 128 + hw:128 + 2 * hw], 0.0, [dst])
        deps = [self.dd("PT", c, i) for c in range(c0, c0 + nch) for i in range((n0 - lo) // 128, (n0 + 128 + hi - 1) // 128 + 1)]
        self.dma(dst[:, :, hw - lo:hw + 128 + hi], self.PTv[:, c0:c0 + nch, n0 - lo:n0 + 128 + hi], deps, [dst])

    def inverse(self, X1, NN, MAT, h, Wt, At, Bt, pA, pB_, pC):
        V, G = self.dve, self.pool
        W, A, B = Wt[0], At[0], Bt[0]
        self.tt(G, W[:, :], self.identb[:, :], X1[:, 0:128], ALU.subtract, [self.identb, X1], [W])
        self.mm(pA[:, 0:128], X1[:, 0:128], NN[:, :], True, True, [X1, NN], [pA])
        self.mm(pB_[:, 0:128], NN[:, :], X1[:, 0:128], True, True, [X1, NN], [pB_])
        self.cp(self.act, A[:, :], pA[:, 0:128], [pA], [A])
        self.cp(self.act, B[:, :], pB_[:, 0:128], [pB_], [B])
        for it in range(5):
            W2, A2, B2 = Wt[(it + 1) % 2], At[(it + 1) % 2], Bt[(it + 1) % 2]
            self.mm(pC[:, 0:128], A[:, :], W[:, :], True, True, [A, W], [pC])
            if it < 4:
                self.mm(pA[:, 0:128], B[:, :], A[:, :], True, True, [A, B], [pA])
                self.mm(pB_[:, 0:128], A[:, :], B[:, :], True, True, [A, B], [pB_])
            dstW = W2[:, :] if it < 4 else MAT[:, h, 0:128]
            self.tt(V, dstW, W[:, :], pC[:, 0:128], ALU.add, [W, pC], [W2 if it < 4 else MAT])
            if it < 4:
                self.cp(self.act, A2[:, :], pA[:, 0:128], [pA], [A2])
                self.cp(self.act, B2[:, :], pB_[:, 0:128], [pB_], [B2])
            W, A, B = W2, A2, B2

    def inverse_batch(self, X1a, NNa, MAT, h0, banks, Wt, At, Bt):
        V, G = self.dve, self.pool
        psW, psA, psB = banks
        hs = slice(h0, h0 + 4)

        def reg(p, i):
            return p[:, i * 128:(i + 1) * 128]

        def bv(p):
            return p[:, :].rearrange("p (h t) -> p h t", h=4)
        W, A, B = Wt[0], At[0], Bt[0]
        self.tt(G, W[:, :, :], self.identb[:, :].unsqueeze(1).to_broadcast([128, 4, 128]), X1a[:, hs, 0:128], ALU.subtract, [self.identb, X1a], [W])
        for i in range(4):
            self.mm(reg(psA, i), X1a[:, h0 + i, 0:128], NNa[:, h0 + i, :], True, True, [X1a, NNa], [psA])
        for i in range(4):
            self.mm(reg(psB, i), NNa[:, h0 + i, :], X1a[:, h0 + i, 0:128], True, True, [X1a, NNa], [psB])
        self.cp(self.act, A[:, :, :], bv(psA), [psA], [A])
        self.cp(self.act, B[:, :, :], bv(psB), [psB], [B])
        for it in range(5):
            W2, A2, B2 = Wt[(it + 1) % 2], At[(it + 1) % 2], Bt[(it + 1) % 2]
            for i in range(4):
                self.mm(reg(psW, i), A[:, i, :], W[:, i, :], True, True, [A, W], [psW])
            if it < 4:
                for i in range(4):
                    self.mm(reg(psA, i), B[:, i, :], A[:, i, :], True, True, [A, B], [psA])
                for i in range(4):
                    self.mm(reg(psB, i), A[:, i, :], B[:, i, :], True, True, [A, B], [psB])
                self.tt(V, W2[:, :, :], W[:, :, :], bv(psW), ALU.add, [W, psW], [W2])
                self.cp(self.act, A2[:, :, :], bv(psA), [psA], [A2])
                self.cp(self.act, B2[:, :, :], bv(psB), [psB], [B2])
            else:
                self.tt(V, MAT[:, hs, 0:128], W[:, :, :], bv(psW), ALU.add, [W, psW], [MAT])
            W, A, B = W2, A2, B2

    def rwkv_prep(self, l):
        nc = self.nc
        V, G = self.dve, self.pool
        with ExitStack() as es:
            def t(shape, dt=F32):
                return self.sb(shape, dt, es)
            wtmp = t([128, 512])
            w2b, a2b, g2b = t([128, 512], BF16), t([128, 512], BF16), t([128, 512], BF16)
            for (src, dstb) in ((self.rw2[l].rearrange("d r c -> (d r) c"), w2b), (self.ra2[l].rearrange("d r c -> (d r) c"), a2b), (self.rg2[l], g2b)):
                self.dma(wtmp[:, :], src, [], [wtmp])
                self.cp(V, dstb[:, :], wtmp[:, :], [wtmp], [dstb])
            PFl = self.PFt[l]
            c0t = t([128, 15])
            self.tt(V, c0t[:, :], self.pfv(l, "mu0"), self.pfv(l, "mu1"), ALU.add, [PFl], [c0t])
            self.ts(V, c0t[:, :], c0t[:, :], -1.0, 1.0, ALU.mult, ALU.add, [c0t], [c0t])
            omka = t([128, 4])
            self.ts(V, omka[:, :], self.pfv(l, "ka"), -1.0, 1.0, ALU.mult, ALU.add, [PFl], [omka])

            def bc(ap, n):
                return ap.unsqueeze(2).to_broadcast([128, n, 128])

            def stream(s):
                pa = t([128, 15, 130])
                sh = t([128, 15, 128])
                twb, xab, sgb = t([128, 128], BF16), t([128, 128], BF16), t([128, 128], BF16)
                SW = [t([128, 4, 128]) for _ in range(2)]
                AA = [t([128, 4, 128]) for _ in range(2)]
                ga = t([128, 4, 128])
                kx, kk, tq, bon = t([128, 4, 128]), t([128, 4, 128]), t([128, 4, 128]), t([128, 4, 128])
                CS, EX, tmpa, tmpb = (t([128, 4, 128]) for _ in range(4))
                e1, e2, e3 = (t([128, 4, 128]) for _ in range(3))
                pc = t([128, 8])
                KQ = t([128, 4, 256], BF16)
                BTb, CTb = t([128, 4, 128], BF16), t([128, 4, 128], BF16)
                vb = t([128, 4, 128], BF16)
                BC = t([128, 1024], BF16)
                Vt = t([128, 512], BF16)
                MAT = t([128, 8, 512], BF16)
                X1a = t([128, 8, 256], BF16)
                NNa = t([128, 8, 128], BF16)
                BTz, CTz = t([128, 8, 128], BF16), t([128, 8, 128], BF16)
                self.memset(G, BTz[:, :, :], 0.0, [BTz])
                self.memset(G, CTz[:, :, :], 0.0, [CTz])
                Wt = [t([128, 4, 128], BF16) for _ in range(2)]
                At = [t([128, 4, 128], BF16) for _ in range(2)]
                Bt = [t([128, 4, 128], BF16) for _ in range(2)]
                Wt2 = [t([128, 4, 128], BF16) for _ in range(2)]
                At2 = [t([128, 4, 128], BF16) for _ in range(2)]
                Bt2 = [t([128, 4, 128], BF16) for _ in range(2)]
                B = self.ps[4 * s:4 * s + 4]
                for j in range(NJ):
                    n0 = s * T + j * 128
                    self.ck(1000 + s * NJ + j)
                    self.load_halo(pa, 0, 15, s, j, 1)
                    self.ck(1)
                    self.tt(V, sh[:, :, :], pa[:, :, 1:129], bc(c0t[:, :], 15), ALU.mult, [pa, c0t], [sh])
                    for c in range(15):
                        self.stt(V, sh[:, c, :], pa[:, c, 0:128], self.pfv(l, "mu0", c), sh[:, c, :], ALU.mult, ALU.add, [pa, PFl, sh], [sh])
                        self.stt(V, sh[:, c, :], pa[:, c, 2:130], self.pfv(l, "mu1", c), sh[:, c, :], ALU.mult, ALU.add, [pa, PFl, sh], [sh])
                    self.ck(2)
                    r_, k_, v_ = sh[:, 0:4, :], sh[:, 4:8, :], sh[:, 8:12, :]
                    self.actf(twb[:, :], sh[:, 12, :], AF.Tanh, [sh], [twb])
                    self.cp(self.act, xab[:, :], sh[:, 13, :], [sh], [xab])
                    self.actf(sgb[:, :], sh[:, 14, :], AF.Sigmoid, [sh], [sgb])
                    self.ck(3)
                    for d in range(2):
                        for (wb_, xin, dst, bname, pb) in ((w2b, twb, SW[d], f"w0_{d}", B[0]), (a2b, xab, AA[d], f"a0_{d}", B[1])):
                            for m in range(4):
                                self.mm(pb[:, m * 128:(m + 1) * 128], wb_[d * 64:(d + 1) * 64, m * 128:(m + 1) * 128],
                                        xin[d * 64:(d + 1) * 64, :], True, True, [wb_, xin], [pb])
                            for m in range(4):
                                self.actf(dst[:, m, :], pb[:, m * 128:(m + 1) * 128], AF.Sigmoid, [pb, PFl], [dst],
                                          bias=self.pfv(l, bname, m), scale=1.0)
                    for m in range(4):
                        self.mm(B[2][:, m * 128:(m + 1) * 128], g2b[:, m * 128:(m + 1) * 128], sgb[:, :], True, True, [g2b, sgb], [B[2]])
                    self.cp(self.act, ga[:, :, :], B[2][:, :].rearrange("p (m t) -> p m t", m=4), [B[2]], [ga])
                    self.dma(self.GAs.rearrange("(m p) n -> p m n", p=128)[:, :, n0:n0 + 128], ga[:, :, :], [ga], [self.dd("GAs", n0 // 128)])
                    self.ck(4)
                    self.tt(V, kx[:, :, :], k_, bc(self.pfv(l, "kk"), 4), ALU.mult, [sh, PFl], [kx])
                    self.tt(G, tq[:, :, :], kx[:, :, :], kx[:, :, :], ALU.mult, [kx], [tq])
                    for m in range(4):
                        self.mm(B[3][:, m * 128:(m + 1) * 128], self.bones[:, :], tq[:, m, :], True, True, [self.bones, tq], [B[3]])
                    self.ts(V, tq[:, :, :], B[3][:, :].rearrange("p (m t) -> p m t", m=4), EPS, None, ALU.add, None, [B[3]], [tq])
                    self.actf(tq[:, :, :], tq[:, :, :], AF.Sqrt, [tq], [tq])
                    self.recip(tq[:, :, :], tq[:, :, :], [tq], [tq])
                    self.tt(V, kk[:, :, :], kx[:, :, :], tq[:, :, :], ALU.mult, [kx, tq], [kk])
                    self.ck(5)
                    self.tt(G, bon[:, :, :], r_, k_, ALU.mult, [sh], [bon])
                    self.tt(G, bon[:, :, :], bon[:, :, :], bc(self.pfv(l, "rk"), 4), ALU.mult, [bon, PFl], [bon])
                    for m in range(4):
                        self.mm(B[0][:, m * 128:(m + 1) * 128], self.bones[:, :], bon[:, m, :], True, True, [self.bones, bon], [B[0]])
                    self.tt(V, bon[:, :, :], B[0][:, :].rearrange("p (m t) -> p m t", m=4), v_, ALU.mult, [B[0], sh], [bon])
                    self.dma(self.BON.rearrange("(m p) n -> p m n", p=128)[:, :, n0:n0 + 128], bon[:, :, :], [bon], [self.dd("BON", n0 // 128)])
                    self.ck(6)
                    self.cp(G, vb[:, :, :], v_, [sh], [vb])
                    pbv = B[1][:, :].bitcast(BF16)
                    for m in range(4):
                        self.tr(pbv[:, m * 128:(m + 1) * 128], vb[:, m, :], self.identb[:, :], [vb, self.identb], [B[1]])
                    self.cp(self.act, Vt[:, :], pbv[:, 0:512], [B[1]], [Vt])
                    self.dma(self.RV[s, j], Vt[:, :], [Vt], [self.dd("RV", s, j)])
                    for d in range(2):
                        self.ck(7)
                        self.tt(V, tmpa[:, :, :], AA[d][:, :, :], bc(self.pfv(l, "ka"), 4), ALU.mult, [AA[d], PFl], [tmpa])
                        self.tt(V, tmpa[:, :, :], tmpa[:, :, :], bc(omka[:, :], 4), ALU.add, [tmpa, omka], [tmpa])
                        self.tt(V, tmpa[:, :, :], tmpa[:, :, :], k_, ALU.mult, [tmpa, sh], [tmpa])
                        self.tt(G, tmpb[:, :, :], kk[:, :, :], AA[d][:, :, :], ALU.mult, [kk, AA[d]], [tmpb])
                        self.ck(8)
                        for m in range(4):
                            self.op(V, lambda m=m: nc.vector.tensor_tensor_scan(out=CS[:, m, :], data0=self.RM[:, :], data1=SW[d][:, m, :],
                                                                               initial=0.0, op0=ALU.mult, op1=ALU.add),
                                    [self.RM, SW[d]], [CS])
                        CSv = CS[:, :, :].rearrange("p m (c t) -> p m c t", t=64)
                        if d == 0:
                            self.tt(V, EX[:, :, :], CS[:, :, :], SW[d][:, :, :], ALU.subtract, [CS, SW[d]], [EX])
                            incl = CS
                        else:
                            tot = CSv[:, :, :, 63:64].to_broadcast([128, 4, 2, 64])
                            self.tt(V, EX[:, :, :].rearrange("p m (c t) -> p m c t", t=64), tot, CSv, ALU.subtract, [CS], [EX])
                            self.tt(V, e3[:, :, :], EX[:, :, :], SW[d][:, :, :], ALU.add, [EX, SW[d]], [e3])
                            incl = e3
                        self.ck(9)
                        self.actf(e1[:, :, :], EX[:, :, :], AF.Exp, [EX], [e1], scale=-CDEC)
                        self.actf(e2[:, :, :], incl[:, :, :], AF.Exp, [incl], [e2], scale=CDEC)
                        self.actf(e3[:, :, :], incl[:, :, :], AF.Exp, [incl], [e3], scale=-CDEC)
                        self.actf(pc[:, :].rearrange("p (m c) -> p m c", c=2), CSv[:, :, :, 63], AF.Exp, [CS], [pc], scale=-CDEC)
                        self.dma(self.RPC[s, d, j], pc[:, :], [pc], [self.dd("RPC", s, d, j)])
                        self.tt(V, KQ[:, :, 0:128], kk[:, :, :], e1[:, :, :], ALU.mult, [kk, e1], [KQ])
                        self.tt(G, KQ[:, :, 128:256], r_, e3[:, :, :], ALU.mult, [sh, e3], [KQ])
                        self.tt(V, BTb[:, :, :], tmpb[:, :, :], e2[:, :, :], ALU.mult, [tmpb, e2], [BTb])
                        self.tt(G, CTb[:, :, :], tmpa[:, :, :], e2[:, :, :], ALU.mult, [tmpa, e2], [CTb])
                        self.dma(self.RKQ[s, d, j], KQ[:, :, :].rearrange("p m t -> p (m t)"), [KQ], [self.dd("RKQ", s, d, j)])
                        self.ck(10)
                        pbb = B[2][:, :].bitcast(BF16)
                        for m in range(4):
                            self.tr(pbb[:, m * 128:(m + 1) * 128], BTb[:, m, :], self.identb[:, :], [BTb, self.identb], [B[2]])
                            self.tr(pbb[:, 512 + m * 128:512 + (m + 1) * 128], CTb[:, m, :], self.identb[:, :], [CTb, self.identb], [B[2]])
                        self.cp(self.act, BC[:, :], pbb[:, :], [B[2]], [BC])
                        self.dma(self.RBC[s, d, j], BC[:, :], [BC], [self.dd("RBC", s, d, j)])
                        self.ck(11)
                        Ms, Mn = (self.MU, self.ML) if d == 0 else (self.ML, self.MU)
                        for e_ in range(2):
                            rows = slice(e_ * 64, e_ * 64 + 64)
                            self.cp(G, BTz[:, :, :].rearrange("p (m e) t -> p m e t", e=2)[rows, :, e_, :], BTb[rows, :, :], [BTb], [BTz])
                            self.cp(self.act, CTz[:, :, :].rearrange("p (m e) t -> p m e t", e=2)[rows, :, e_, :], CTb[rows, :, :], [CTb], [CTz])
                        Msb = Ms[:, :].unsqueeze(1).to_broadcast([128, 2, 256])
                        v2 = lambda p: p[:, :].rearrange("p (h t) -> p h t", h=2)
                        for h in range(8):
                            self.mm(B[h // 2][:, (h % 2) * 256:(h % 2 + 1) * 256], BTz[:, h, :], KQ[:, h // 2, :], True, True, [BTz, KQ], [B[h // 2]])
                        for q in range(4):
                            self.tt(V, X1a[:, 2 * q:2 * q + 2, :], v2(B[q]), Msb, ALU.mult, [B[q], Ms], [X1a])
                        for h in range(8):
                            self.mm(B[h // 2][:, (h % 2) * 256:(h % 2 + 1) * 256], CTz[:, h, :], KQ[:, h // 2, :], True, True, [CTz, KQ], [B[h // 2]])
                        for q in range(4):
                            self.tt(V, MAT[:, 2 * q:2 * q + 2, 256:512], v2(B[q]), Msb, ALU.mult, [B[q], Ms], [MAT])
                        for h in range(8):
                            self.mm(B[h // 4][:, (h % 4) * 128:(h % 4 + 1) * 128], KQ[:, h // 2, 0:128], BTz[:, h, :], True, True, [BTz, KQ], [B[h // 4]])
                        Mnb = Mn[:, 0:128].unsqueeze(1).to_broadcast([128, 4, 128])
                        for q in range(2):
                            self.tt(V, NNa[:, 4 * q:4 * q + 4, :], B[q][:, :].rearrange("p (h t) -> p h t", h=4), Mnb, ALU.mult, [B[q], Mn], [NNa])
                        self.cp(G, MAT[:, :, 128:256], X1a[:, :, 128:256], [X1a], [MAT])
                        self.inverse_batch(X1a, NNa, MAT, 0, (B[0], B[1], B[2]), Wt, At, Bt)
                        self.inverse_batch(X1a, NNa, MAT, 4, (B[3], B[1], B[2]), Wt2, At2, Bt2)
                        self.ck(12)
                        self.dma(self.RMAT[s, d, j], MAT[:, :, :].rearrange("p h t -> p (h t)"), [MAT], [self.dd("RMAT", s, d, j)])
            self.run_interleaved([lambda: stream(0), lambda: stream(1)])
            self.barrier()

    def gdn_setup(self):
        self.GKQ = self.dram("GKQ", [S, 2, NJ, 128, 4 * 256], BF16)
        self.GMAT = self.dram("GMAT", [S, 2, NJ, 128, 4 * 512], BF16)
        self.GBC = self.dram("GBC", [S, 2, NJ, 128, 1024], BF16)
        self.GV = self.dram("GV", [S, NJ, 128, 512], BF16)
        self.GPC = self.dram("GPC", [S, 2, NJ, 128, 8], F32)
        self.YB = self.dram("YB", [2, NT, 512])
        self.SEL = self.sb([16, 16, 128], F32)
        self.selc = self.sb([128, 1], F32)
        self.onec = self.sb([128, 1], F32)
        nc = self.nc
        P = self.pool
        for i in range(16):
            self.cp(P, self.SEL[0:16, i, :], self.ident[0:16, i:i + 1].to_broadcast([16, 128]), [self.ident], [self.SEL])
        self.memset(P, self.onec[:, :], 1.0, [self.onec])
        self.memset(P, self.selc[:, :], 1.0, [self.selc])
        self.op(P, lambda: nc.gpsimd.affine_select(out=self.selc[:, :], in_=self.selc[:, :], pattern=[[0, 1]], compare_op=ALU.is_ge, fill=0.0,
                                                   base=-4, channel_multiplier=1), [self.selc], [self.selc])

    def gdn_prep(self, l):
        nc = self.nc
        V, G = self.dve, self.pool
        ps = self.ps
        with ExitStack() as es:
            def t(shape, dt=F32):
                return self.sb(shape, dt, es)
            PFl = self.PFt[l]

            def bc(ap, n):
                return ap.unsqueeze(2).to_broadcast([128, n, 128])
            negA = t([128, 1])
            self.actf(negA[:, :], self.pfv(l, "alog"), AF.Exp, [PFl], [negA])
            self.ts(V, negA[:, :], negA[:, :], -1.0, None, ALU.mult, None, [negA], [negA])
            def stream(s):
                B = self.ps[4 * s:4 * s + 4]
                qkv = t([128, 12, 132])
                cv, t2 = t([128, 12, 128]), t([128, 12, 128])
                sq = t([128, 8, 128])
                kq = t([128, 4, 256])
                kqb = t([128, 4, 256], BF16)
                kb = t([128, 4, 128], BF16)
                vb = t([128, 4, 128], BF16)
                ab, x1, Xg, SIG, gcf, gcr = (t([16, 128]) for _ in range(6))
                X2, E2 = t([16, 256]), t([16, 256])
                TOTb, Etot = t([16, 128]), t([16, 2])
                TS = t([128, 80])
                negg, sB, sC, dd_ = t([128, 8]), t([128, 8]), t([128, 8]), t([128, 8])
                gm4 = t([128, 4, 256])
                KQd = t([128, 4, 256], BF16)
                BCd = [t([128, 1024], BF16) for _ in range(2)]
                Vt = t([128, 512], BF16)
                MAT = t([128, 4, 512], BF16)
                pc = t([128, 8])
                X1a = t([128, 4, 256], BF16)
                NNa = t([128, 4, 128], BF16)
                Wt = [t([128, 4, 128], BF16) for _ in range(2)]
                At = [t([128, 4, 128], BF16) for _ in range(2)]
                Bt = [t([128, 4, 128], BF16) for _ in range(2)]
                abv = self.PT[3968:3984, :]
                for j in range(NJ):
                    n0 = s * T + j * 128
                    self.load_halo(qkv, 15, 12, s, j, 2)
                    gw = self.pfv(l, "gconv")
                    self.tt(V, cv[:, :, :], qkv[:, :, 0:128], bc(gw[:, 0:12], 12), ALU.mult, [qkv, PFl], [cv])
                    for k in range(1, 5):
                        self.tt(G, t2[:, :, :], qkv[:, :, k:k + 128], bc(gw[:, k * 12:(k + 1) * 12], 12), ALU.mult, [qkv, PFl], [t2])
                        self.tt(V, cv[:, :, :], cv[:, :, :], t2[:, :, :], ALU.add, [cv, t2], [cv])
                    self.actf(cv[:, :, :], cv[:, :, :], AF.Silu, [cv], [cv])
                    self.tt(G, sq[:, :, :], cv[:, 0:8, :], cv[:, 0:8, :], ALU.mult, [cv], [sq])
                    for c in range(8):
                        pb = B[c // 4]
                        self.mm(pb[:, (c % 4) * 128:(c % 4 + 1) * 128], self.ones[:, :], sq[:, c, :], True, True, [self.ones, sq], [pb])
                    for hf in range(2):
                        self.ts(V, sq[:, hf * 4:(hf + 1) * 4, :], B[hf][:, :].rearrange("p (c t) -> p c t", c=4), EPS, None, ALU.add, None, [B[hf]], [sq])
                    self.actf(sq[:, :, :], sq[:, :, :], AF.Sqrt, [sq], [sq])
                    self.recip(sq[:, :, :], sq[:, :, :], [sq], [sq])
                    self.tt(V, kq[:, :, 0:128], cv[:, 4:8, :], sq[:, 4:8, :], ALU.mult, [cv, sq], [kq])
                    self.stt(V, kq[:, :, 128:256], cv[:, 0:4, :], 128.0 ** -0.5, sq[:, 0:4, :], ALU.mult, ALU.mult, [cv, sq], [kq])
                    self.cp(G, kqb[:, :, :], kq[:, :, :], [kq], [kqb])
                    self.cp(G, kb[:, :, :], kq[:, :, 0:128], [kq], [kb])
                    self.cp(G, vb[:, :, :], cv[:, 8:12, :], [cv], [vb])
                    pbv = B[2][:, :].bitcast(BF16)
                    for m in range(4):
                        self.tr(pbv[:, m * 128:(m + 1) * 128], vb[:, m, :], self.identb[:, :], [vb, self.identb], [B[2]])
                    self.cp(self.act, Vt[:, :], pbv[:, 0:512], [B[2]], [Vt])
                    self.dma(self.GV[s, j], Vt[:, :], [Vt], [self.dd("GV", s, j)])
                    pbk = B[3][:, :].bitcast(BF16)
                    for m in range(4):
                        self.tr(pbk[:, m * 128:(m + 1) * 128], kb[:, m, :], self.identb[:, :], [kb, self.identb], [B[3]])
                    self.dma(ab[0:16, :], abv[:, n0:n0 + 128], [], [ab])
                    self.actf(x1[0:16, :], ab[0:16, :], AF.Exp, [ab, PFl], [x1], bias=self.pfv(l, "dtb")[0:16, :], scale=1.0)
                    self.actf(x1[0:16, :], x1[0:16, :], AF.Ln, [x1, self.onec], [x1], bias=self.onec[0:16, :], scale=1.0)
                    self.ts(V, Xg[0:16, :], x1[0:16, :], negA[0:16, :], None, ALU.mult, None, [x1, negA], [Xg])
                    self.actf(SIG[0:16, :], ab[0:16, :], AF.Sigmoid, [ab], [SIG])
                    self.op(V, lambda: nc.vector.tensor_tensor_scan(out=gcf[0:16, :], data0=self.RM[0:16, :], data1=Xg[0:16, :], initial=0.0,
                                                                    op0=ALU.mult, op1=ALU.add), [self.RM, Xg], [gcf])
                    gcfv = gcf[0:16, :].rearrange("p (c t) -> p c t", t=64)
                    totb = gcfv[:, :, 63:64].to_broadcast([16, 2, 64])
                    self.cp(V, TOTb[0:16, :].rearrange("p (c t) -> p c t", t=64), totb, [gcf], [TOTb])
                    self.tt(V, gcr[0:16, :], TOTb[0:16, :], gcf[0:16, :], ALU.subtract, [TOTb, gcf], [gcr])
                    self.tt(V, X2[0:16, 128:256], gcr[0:16, :], Xg[0:16, :], ALU.add, [gcr, Xg], [X2])
                    self.tt(V, X2[0:16, 128:256], X2[0:16, 128:256], gcf[0:16, :], ALU.subtract, [X2, gcf], [X2])
                    self.stt(V, X2[0:16, 128:256], X2[0:16, 128:256], self.selc[0:16, :], gcf[0:16, :], ALU.mult, ALU.add, [X2, self.selc, gcf], [X2])
                    self.tt(V, X2[0:16, 0:128], X2[0:16, 128:256], Xg[0:16, :], ALU.subtract, [X2, Xg], [X2])
                    self.actf(E2[0:16, :], X2[0:16, :], AF.Exp, [X2], [E2])
                    self.actf(Etot[0:16, :], gcfv[:, :, 63], AF.Exp, [gcf], [Etot])
                    pT = B[2]
                    for q_, src in enumerate((Xg[0:16, :], SIG[0:16, :], X2[0:16, 0:128], X2[0:16, 128:256], TOTb[0:16, :])):
                        self.tr(pT[:, q_ * 16:(q_ + 1) * 16], src, self.ident[0:16, 0:16], [Xg, SIG, X2, TOTb, self.ident], [pT])
                    self.cp(V, TS[:, :], pT[:, 0:80], [pT], [TS])
                    self.actf(negg[:, :], TS[:, 0:8], AF.Exp, [TS], [negg], scale=-1.0)
                    self.tt(V, dd_[:, :], TS[:, 64:72], TS[:, 32:40], ALU.subtract, [TS], [dd_])
                    self.actf(sB[:, :], dd_[:, :], AF.Exp, [dd_], [sB])
                    self.tt(V, sB[:, :], sB[:, :], TS[:, 24:32], ALU.mult, [sB, TS], [sB])
                    self.tt(V, dd_[:, :], TS[:, 64:72], TS[:, 48:56], ALU.subtract, [TS], [dd_])
                    self.actf(sC[:, :], dd_[:, :], AF.Exp, [dd_], [sC])
                    self.tt(V, sC[:, :], sC[:, :], TS[:, 24:32], ALU.mult, [sC, TS], [sC])
                    pbk4 = pbk[:, 0:512].rearrange("p (h t) -> p h t", h=4)
                    for d in range(2):
                        self.tt(V, BCd[d][:, 0:512].rearrange("p (h t) -> p h t", h=4), pbk4, sB[:, d * 4:d * 4 + 4].unsqueeze(2).to_broadcast([128, 4, 128]), ALU.mult, [B[3], sB], [BCd[d]])
                        self.tt(V, BCd[d][:, 512:1024].rearrange("p (h t) -> p h t", h=4), pbk4, sC[:, d * 4:d * 4 + 4].unsqueeze(2).to_broadcast([128, 4, 128]), ALU.mult, [B[3], sC], [BCd[d]])
                    for d in range(2):
                        Ms = self.MU if d == 0 else self.ML
                        BC = BCd[d]
                        for h in range(4):
                            i = d * 4 + h
                            self.mm(B[h // 2][:, (h % 2) * 256:(h % 2 + 1) * 256], self.SEL[0:16, i, :], X2[0:16, :], True, True, [self.SEL, X2], [B[h // 2]])
                            self.mm(B[2 + h // 2][:, (h % 2) * 256:(h % 2 + 1) * 256], self.SEL[0:16, i, :], E2[0:16, :], True, True, [self.SEL, E2], [B[2 + h // 2]])
                        v2 = lambda p: p[:, :].rearrange("p (h t) -> p h t", h=2)
                        for q in range(2):
                            self.tt(V, gm4[:, 2 * q:2 * q + 2, :], v2(B[q]), TS[:, 32 + d * 4 + 2 * q:32 + d * 4 + 2 * q + 2].unsqueeze(2).to_broadcast([128, 2, 256]),
                                    ALU.subtract, [B[q], TS], [gm4])
                            self.tt(V, KQd[:, 2 * q:2 * q + 2, :], kq[:, 2 * q:2 * q + 2, :], v2(B[2 + q]), ALU.mult, [kq, B[2 + q]], [KQd])
                        for h in range(4):
                            self.mm(B[2][:, h * 2:(h + 1) * 2], self.SEL[0:16, d * 4 + h, :], Etot[0:16, :], True, True, [self.SEL, Etot], [B[2]])
                        self.cp(self.act, pc[:, :], B[2][:, 0:8], [B[2]], [pc])
                        self.ts(G, gm4[:, :, :], gm4[:, :, :], 0.0, None, ALU.min, None, [gm4], [gm4])
                        self.actf(gm4[:, :, :], gm4[:, :, :], AF.Exp, [gm4], [gm4])
                        self.tt(G, gm4[:, :, :], gm4[:, :, :], Ms[:, :].unsqueeze(1).to_broadcast([128, 4, 256]), ALU.mult, [gm4, Ms], [gm4])
                        for h in range(4):
                            self.mm(B[h // 2][:, (h % 2) * 256:(h % 2 + 1) * 256], kb[:, h, :], kqb[:, h, :], True, True, [kb, kqb], [B[h // 2]])
                        for q in range(2):
                            self.tt(V, gm4[:, 2 * q:2 * q + 2, :], gm4[:, 2 * q:2 * q + 2, :], v2(B[q]), ALU.mult, [gm4, B[q]], [gm4])
                        self.tt(V, X1a[:, :, :], gm4[:, :, :], TS[:, 24 + d * 4:28 + d * 4].unsqueeze(2).to_broadcast([128, 4, 256]), ALU.mult, [gm4, TS], [X1a])
                        self.tt(V, MAT[:, :, 256:512], X1a[:, :, :], negg[:, d * 4:d * 4 + 4].unsqueeze(2).to_broadcast([128, 4, 256]), ALU.mult, [X1a, negg], [MAT])
                        self.cp(G, MAT[:, :, 128:256], X1a[:, :, 128:256], [X1a], [MAT])
                        pN = B[3][:, :].bitcast(BF16)
                        for h in range(4):
                            self.tr(pN[:, h * 128:(h + 1) * 128], X1a[:, h, 0:128], self.identb[:, :], [X1a, self.identb], [B[3]])
                        self.cp(self.act, NNa[:, :, :], pN[:, 0:512].rearrange("p (h t) -> p h t", h=4), [B[3]], [NNa])
                        self.inverse_batch(X1a, NNa, MAT, 0, (B[0], B[1], B[2]), Wt, At, Bt)
                        self.dma(self.GKQ[s, d, j], KQd[:, :, :].rearrange("p m t -> p (m t)"), [KQd], [self.dd("GKQ", s, d, j)])
                        self.dma(self.GMAT[s, d, j], MAT[:, :, :].rearrange("p h t -> p (h t)"), [MAT], [self.dd("GMAT", s, d, j)])
                        self.dma(self.GBC[s, d, j], BC[:, :], [BC], [self.dd("GBC", s, d, j)])
                        self.dma(self.GPC[s, d, j], pc[:, :], [pc], [self.dd("GPC", s, d, j)])
            self.run_interleaved([lambda: stream(0), lambda: stream(1)])
            self.barrier()

    def conf_setup(self):
        self.RCs = self.dram("RCs", [512, NT], BF16)
        self.RAs = self.dram("RAs", [512, NT], BF16)
        self.RBs = self.dram("RBs", [512, NT], BF16)

    def conformer(self, l):
        V, G = self.dve, self.pool
        ps = self.ps
        PFl = self.PFt[l]
        valv = self.PT[3984:3984 + 512, :].rearrange("(c p) n -> p c n", p=128)
        gatv = self.PT[4496:4496 + 512, :].rearrange("(c p) n -> p c n", p=128)
        RCv = self.RCs.rearrange("(c p) n -> p c n", p=128)
        with ExitStack() as es:
            def t(shape, dt=F32):
                return self.sb(shape, dt, es)
            DW = t([128, 124, 128], BF16)
            cw = self.pfv(l, "cdw")
            for q in range(124):
                self.ts(V, DW[:, q, :], self.identb[:, :], cw[:, q:q + 1], None, ALU.mult, None, [self.identb, PFl], [DW])
            upW = t([128, 2, 32, 94], BF16)
            upH = t([128, 2, 62, 64], BF16)
            upC = t([128, 4, 286], BF16)
            self.memset(G, upW[:, :, :, :], 0.0, [upW])
            self.memset(G, upH[:, :, :, :], 0.0, [upH])
            self.memset(G, upC[:, :, :], 0.0, [upC])
            vl = [t([128, TL]) for _ in range(2)]
            gl = [t([128, TL]) for _ in range(2)]
            o = t([128, 4, TL])
            sq = t([128, 4, 512])
            mu, rs, var = t([128, 512]), t([128, 512]), t([128, 512])
            ob = t([128, 4, 512], BF16)
            nb = 0
            for s in range(S):
                for (seg0, W_) in ((0, TC), (TC, TL)):
                    n0 = s * T + seg0
                    for c in range(4):
                        v_, g_ = vl[c % 2], gl[c % 2]
                        self.dma(v_[:, :W_], valv[:, c, n0:n0 + W_], [], [v_])
                        self.dma(g_[:, :W_], gatv[:, c, n0:n0 + W_], [], [g_])
                        self.actf(g_[:, :W_], g_[:, :W_], AF.Sigmoid, [g_], [g_])
                        if seg0 == 0:
                            self.tt(V, upC[:, c, 15:15 + W_], v_[:, :W_], g_[:, :W_], ALU.mult, [v_, g_], [upC])
                        elif c < 2:
                            self.tt(V, upW[:, c, :, 15:79], v_[:, :].rearrange("p (r w) -> p r w", w=64), g_[:, :].rearrange("p (r w) -> p r w", w=64),
                                    ALU.mult, [v_, g_], [upW])
                        else:
                            self.tt(V, upH[:, c - 2, 15:47, :], v_[:, :].rearrange("p (r w) -> p r w", w=64), g_[:, :].rearrange("p (r w) -> p r w", w=64),
                                    ALU.mult, [v_, g_], [upH])
                    for c in range(4):
                        if seg0 == 0:
                            pb = ps[nb % 4]; nb += 1
                            for k in range(31):
                                self.mm(pb[:, 0:W_], DW[:, k * 4 + c, :], upC[:, c, k:k + W_], k == 0, k == 30, [DW, upC], [pb])
                            self.actf(o[:, c, 0:W_], pb[:, 0:W_], AF.Identity, [pb, PFl], [o], bias=self.pfv(l, "cdwb", c), scale=1.0)
                        else:
                            for rb_ in range(4):
                                pb = ps[nb % 4]; nb += 1
                                for k in range(31):
                                    if c < 2:
                                        rhs = upW[:, c, rb_ * 8:(rb_ + 1) * 8, k:k + 64]
                                        outp = pb[:, :].rearrange("p (r w) -> p r w", w=64)
                                    else:
                                        rhs = upH[:, c - 2, rb_ * 8 + k:rb_ * 8 + k + 8, :].rearrange("p r w -> p (r w)")
                                        outp = pb[:, :]
                                    self.mm(outp, DW[:, k * 4 + c, :], rhs, k == 0, k == 30, [DW, upW if c < 2 else upH], [pb])
                                self.actf(o[:, c, rb_ * 512:(rb_ + 1) * 512], pb[:, :], AF.Identity, [pb, PFl], [o], bias=self.pfv(l, "cdwb", c), scale=1.0)
                    for t0 in range(0, W_, 512):
                        w = min(512, W_ - t0)
                        self.tt(G, sq[:, :, :w], o[:, :, t0:t0 + w], o[:, :, t0:t0 + w], ALU.mult, [o], [sq])
                        for c in range(4):
                            self.mm(ps[4][:, :w], self.ones[:, :], o[:, c, t0:t0 + w], c == 0, c == 3, [self.ones, o], [ps[4]])
                        for c in range(4):
                            self.mm(ps[5][:, :w], self.ones[:, :], sq[:, c, :w], c == 0, c == 3, [self.ones, sq], [ps[5]])
                        self.ts(V, mu[:, :w], ps[4][:, :w], 1.0 / 512, None, ALU.mult, None, [ps[4]], [mu])
                        self.tt(V, var[:, :w], mu[:, :w], mu[:, :w], ALU.mult, [mu], [var])
                        self.stt(V, var[:, :w], ps[5][:, :w], 1.0 / 512, var[:, :w], ALU.mult, ALU.subtract, [ps[5], var], [var])
                        self.ts(V, var[:, :w], var[:, :w], EPS, None, ALU.add, None, [var], [var])
                        self.actf(var[:, :w], var[:, :w], AF.Sqrt, [var], [var])
                        self.recip(rs[:, :w], var[:, :w], [var], [rs])
                        for c in range(4):
                            self.tt(V, sq[:, c, :w], o[:, c, t0:t0 + w], mu[:, :w], ALU.subtract, [o, mu], [sq])
                            self.tt(G, sq[:, c, :w], sq[:, c, :w], rs[:, :w], ALU.mult, [sq, rs], [sq])
                            self.ts(V, sq[:, c, :w], sq[:, c, :w], self.pfv(l, "clng", c), self.pfv(l, "clnb", c), ALU.mult, ALU.add, [sq, PFl], [sq])
                        self.actf(ob[:, :, :w], sq[:, :, :w], AF.Silu, [sq], [ob])
                        self.dma(RCv[:, :, n0 + t0:n0 + t0 + w], ob[:, :, :w], [ob], [self.dd("RCs", (n0 + t0) // 128)])
            self.barrier()

    def red(self, E, out, in_, op, R, W):
        self.op(E, lambda: E.be.tensor_reduce(out=out, in_=in_, axis=AX.X, op=op), R, W)

    def merge(self, l):
        V, G = self.dve, self.pool
        ps = self.ps
        PFl = self.PFt[l]
        with ExitStack() as es:
            def t(shape, dt=F32):
                return self.sb(shape, dt, es)
            wbr = t([128, 12, 1024], BF16)
            wo = t([128, 8, 1024], BF16)
            for n in range(3):
                self.dma(wbr[:, n * 4:(n + 1) * 4, :], self.w_branch[l, n].rearrange("(k p) n -> p k n", p=128), [], [wbr], Q=self.pool)
            self.dma(wo[:, :, :], self.w_out[l].rearrange("(k p) n -> p k n", p=128), [], [wo], Q=self.pool)
            pgv = [self.PT[5008 + n * 1024:5008 + (n + 1) * 1024, :].rearrange("(c p) n -> p c n", p=128) for n in range(3)]
            fm4 = lambda A: A.rearrange("(c p) n -> p c n", p=128)
            def stream(s):
                B = self.ps[4 * s:4 * s + 4]
                y0, y1, ysq = t([128, 512]), t([128, 512]), t([128, 512])
                m1, m2, m3 = t([128, 8]), t([128, 8]), t([128, 8])
                raf, bon, ga, zt = (t([128, 4, 128]) for _ in range(4))
                Rb = [t([128, 4, 128], BF16) for _ in range(3)]
                pg = t([128, 8, 128])
                macc, tmpm = t([128, 8, 128]), t([128, 8, 128])
                mb = t([128, 8, 128], BF16)
                xt = t([128, 8, 128])
                for j in range(NJ):
                    n0 = s * T + j * 128
                    r = 2 if j < 2 else s
                    for br in range(2):
                        DY, nm, nh, dvv, eps_ = ((self.YA, "R", 8, 64, 64e-5), (self.YB, "G", 4, 128, EPS))[br]
                        self.dma(y0[:, :], DY[0, n0:n0 + 128, :], [self.dd(nm + "Y", 0, n0 // 128)], [y0])
                        self.dma(y1[:, :], DY[1, n0:n0 + 128, :], [self.dd(nm + "Y", 1, n0 // 128)], [y1])
                        self.tt(V, y0[:, :], y0[:, :], y1[:, :], ALU.add, [y0, y1], [y0])
                        yv = y0[:, :].rearrange("p (h d) -> p h d", d=dvv)
                        self.tt(G, ysq[:, :], y0[:, :], y0[:, :], ALU.mult, [y0], [ysq])
                        self.red(V, m2[:, 0:nh], ysq[:, :].rearrange("p (h d) -> p h d", d=dvv), ALU.add, [ysq], [m2])
                        if br == 0:
                            self.red(V, m1[:, 0:nh], yv, ALU.add, [y0], [m1])
                            self.ts(V, m1[:, 0:nh], m1[:, 0:nh], 1.0 / dvv, None, ALU.mult, None, [m1], [m1])
                            self.tt(V, m3[:, 0:nh], m1[:, 0:nh], m1[:, 0:nh], ALU.mult, [m1], [m3])
                            self.stt(V, m2[:, 0:nh], m2[:, 0:nh], 1.0 / dvv, m3[:, 0:nh], ALU.mult, ALU.subtract, [m2, m3], [m2])
                            self.ts(V, m2[:, 0:nh], m2[:, 0:nh], eps_, None, ALU.add, None, [m2], [m2])
                            self.tt(V, yv, yv, m1[:, 0:nh].unsqueeze(2).to_broadcast([128, nh, dvv]), ALU.subtract, [y0, m1], [y0])
                        else:
                            self.ts(V, m2[:, 0:nh], m2[:, 0:nh], 1.0 / dvv, eps_, ALU.mult, ALU.add, [m2], [m2])
                        self.actf(m2[:, 0:nh], m2[:, 0:nh], AF.Sqrt, [m2], [m2])
                        self.recip(m2[:, 0:nh], m2[:, 0:nh], [m2], [m2])
                        self.tt(V, yv, yv, m2[:, 0:nh].unsqueeze(2).to_broadcast([128, nh, dvv]), ALU.mult, [y0, m2], [y0])
                        pb = B[br]
                        for c in range(4):
                            self.tr(pb[:, c * 128:(c + 1) * 128], y0[:, c * 128:(c + 1) * 128], self.ident[:, :], [y0, self.ident], [pb])
                        pbv = pb[:, :].rearrange("p (c t) -> p c t", c=4)
                        if br == 0:
                            for c in range(4):
                                self.ts(V, raf[:, c, :], pbv[:, c, :], self.pfv(l, "ln_g", c), self.pfv(l, "ln_b", c), ALU.mult, ALU.add, [pb, PFl], [raf])
                            self.dma(bon[:, :, :], fm4(self.BON)[:, :, n0:n0 + 128], [self.dd("BON", n0 // 128)], [bon])
                            self.dma(ga[:, :, :], fm4(self.GAs)[:, :, n0:n0 + 128], [self.dd("GAs", n0 // 128)], [ga])
                            self.tt(V, raf[:, :, :], raf[:, :, :], bon[:, :, :], ALU.add, [raf, bon], [raf])
                            self.tt(V, Rb[0][:, :, :], raf[:, :, :], ga[:, :, :], ALU.mult, [raf, ga], [Rb[0]])
                        else:
                            self.dma(zt[:, :, :], self.PTv[:, 27:31, n0:n0 + 128], [], [zt])
                            self.actf(zt[:, :, :], zt[:, :, :], AF.Silu, [zt], [zt])
                            self.stt(V, Rb[1][:, :, :], pbv, self.pfv(l, "gnorm", 0), zt[:, :, :], ALU.mult, ALU.mult, [pb, PFl, zt], [Rb[1]])
                    self.dma(Rb[2][:, :, :], fm4(self.RCs)[:, :, n0:n0 + 128], [self.dd("RCs", n0 // 128)], [Rb[2]])
                    for n in range(3):
                        self.dma(pg[:, :, :], pgv[n][:, :, n0:n0 + 128], [], [pg])
                        self.actf(pg[:, :, :], pg[:, :, :], AF.Sigmoid, [pg], [pg])
                        for m in range(8):
                            pb = B[2 + m // 4]
                            for k in range(4):
                                self.mm(pb[:, (m % 4) * 128:(m % 4 + 1) * 128], wbr[:, n * 4 + k, m * 128:(m + 1) * 128], Rb[n][:, k, :], k == 0, k == 3,
                                        [wbr, Rb[n]], [pb])
                        for hf in range(2):
                            pbv = B[2 + hf][:, :].rearrange("p (c t) -> p c t", c=4)
                            dst = macc if n == 0 else tmpm
                            self.tt(V, dst[:, hf * 4:(hf + 1) * 4, :], pbv, pg[:, hf * 4:(hf + 1) * 4, :], ALU.mult, [B[2 + hf], pg], [dst])
                        if n > 0:
                            self.tt(G, macc[:, :, :], macc[:, :, :], tmpm[:, :, :], ALU.add, [macc, tmpm], [macc])
                    self.cp(G, mb[:, :, :], macc[:, :, :], [macc], [mb])
                    self.dma(xt[:, :, :], self.XTv[:, :, n0:n0 + 128], self.dr("XT", n0, 128), [xt])
                    for m in range(8):
                        pb = B[m // 4]
                        for k in range(8):
                            self.mm(pb[:, (m % 4) * 128:(m % 4 + 1) * 128], wo[:, k, m * 128:(m + 1) * 128], mb[:, k, :], k == 0, k == 7, [wo, mb], [pb])
                    for m in range(8):
                        pb = B[m // 4]
                        self.stt(V, xt[:, m, :], pb[:, (m % 4) * 128:(m % 4 + 1) * 128], self.MOD[l][:, 16 + m, r:r + 1], xt[:, m, :], ALU.mult, ALU.add,
                                 [pb, self.MOD[l], xt], [xt])
                    self.dma(self.XTv[:, :, n0:n0 + 128], xt[:, :, :], [xt], self.dr("XT", n0, 128))
                    if f"XM{l}" in self.dbg:
                        pass
            self.run_interleaved([lambda: stream(0), lambda: stream(1)])
            self.barrier()

    def moe(self, l):
        V, G = self.dve, self.pool
        ps = self.ps
        with ExitStack() as es:
            def t(shape, dt=F32, e_=None):
                return self.sb(shape, dt, e_ or es)
            hT = t([128, 8, NT], BF16)
            WTf = t([16, NT])
            wr = t([128, 8, 16])
            rb = t([128, 16])
            self.dma(wr[:, :, :], self.w_router.rearrange("(k p) e -> p k e", p=128), [], [wr])
            self.dma(rb[:, :], self.rbias, [], [rb])
            with ExitStack() as es1:
                xts = [t([128, 8, 512], F32, es1) for _ in range(2)]
                sq = t([128, 8, 512], F32, es1)
                rs = t([128, 512], F32, es1)
                hf = t([128, 8, 512], F32, es1)
                sc, sel, sel2, eq, cm, wts = (t([128, 16], F32, es1) for _ in range(6))
                m1, m2, gs, gsel = (t([128, 4], F32, es1) for _ in range(4))
                gmx, wsum = t([128, 1], F32, es1), t([128, 1], F32, es1)
                v4 = lambda a: a[:, :].rearrange("p (g j) -> p g j", j=4)
                b4 = lambda a: a[:, :].unsqueeze(2).to_broadcast([128, 4, 4])
                for i, (n0, w, r) in enumerate(self.tiles()):
                    xt, _ = self.modulate_tile((xts[i % 2], sq, rs, None), n0, w, r, None, None, None, None)
                    for c in range(8):
                        self.stt(V, hf[:, c, :w], xt[:, c, :w], self.GF[l][:, c, r:r + 1], rs[:, :w], ALU.mult, ALU.mult, [xt, rs, self.GF[l]], [hf])
                        self.actf(hf[:, c, :w], hf[:, c, :w], AF.Identity, [hf, self.MOD[l]], [hf], bias=self.MOD[l][:, 24 + c, r:r + 1], scale=1.0)
                    self.cp(G, hT[:, :, n0:n0 + w], hf[:, :, :w], [hf], [hT])
                    for q in range(w // 128):
                        pR = ps[6]
                        for c in range(8):
                            self.mm(pR[:, 0:16], hf[:, c, q * 128:(q + 1) * 128], wr[:, c, :], c == 0, c == 7, [hf, wr], [pR])
                        self.actf(sc[:, :], pR[:, 0:16], AF.Sigmoid, [pR], [sc])
                        self.tt(V, sel[:, :], sc[:, :], rb[:, :], ALU.add, [sc, rb], [sel])
                        self.red(V, m1[:, :], v4(sel), ALU.max, [sel], [m1])
                        self.tt(V, v4(eq), v4(sel), b4(m1), ALU.is_equal, [sel, m1], [eq])
                        self.stt(V, sel2[:, :], eq[:, :], -1e9, sel[:, :], ALU.mult, ALU.add, [eq, sel], [sel2])
                        self.red(V, m2[:, :], v4(sel2), ALU.max, [sel2], [m2])
                        self.tt(V, gs[:, :], m1[:, :], m2[:, :], ALU.add, [m1, m2], [gs])
                        self.red(V, gmx[:, :], gs[:, :], ALU.max, [gs], [gmx])
                        self.ts(V, gsel[:, :], gs[:, :], gmx[:, 0:1], None, ALU.is_equal, None, [gs, gmx], [gsel])
                        self.tt(V, v4(cm), v4(sel), b4(m2), ALU.is_ge, [sel, m2], [cm])
                        self.tt(V, v4(cm), v4(cm), b4(gsel), ALU.mult, [cm, gsel], [cm])
                        self.tt(V, wts[:, :], sc[:, :], cm[:, :], ALU.mult, [sc, cm], [wts])
                        self.red(V, wsum[:, :], wts[:, :], ALU.add, [wts], [wsum])
                        self.recip(wsum[:, :], wsum[:, :], [wsum], [wsum])
                        self.ts(V, wts[:, :], wts[:, :], wsum[:, 0:1], None, ALU.mult, None, [wts, wsum], [wts])
                        pT = ps[7]
                        self.tr(pT[0:16, 0:128], wts[:, :], self.ident[:, :], [wts, self.ident], [pT])
                        self.cp(self.act, WTf[0:16, n0 + q * 128:n0 + (q + 1) * 128], pT[0:16, 0:128], [pT], [WTf])
                self.barrier()
            TG = 1152
            TW = 384
            yacc = t([128, 8, TG])
            wgb = [t([128, 8, 512], BF16) for _ in range(2)]
            wub = [t([128, 8, 512], BF16) for _ in range(2)]
            wdb = [t([128, 4, 1024], BF16) for _ in range(2)]
            wtb = t([128, TW])
            sg = [t([128, TW]) for _ in range(2)]
            actb = t([128, 4, TW], BF16)
            xt2 = t([128, 8, 128])
            def loadw(e):
                for (src, dstt) in ((self.weg[l, e], wgb[e % 2]), (self.weu[l, e], wub[e % 2]), (self.wed[l, e], wdb[e % 2])):
                    self.dma(dstt[:, :, :], src.rearrange("(k p) n -> p k n", p=128), [], [dstt], Q=self.pool)
            loadw(0)
            for g in range(NT // TG):
                for e in range(16):
                    gb, ub, db = wgb[e % 2], wub[e % 2], wdb[e % 2]
                    if not (g == NT // TG - 1 and e == 15):
                        loadw((e + 1) % 16)
                    for tt_ in range(TG // TW):
                        n0 = g * TG + tt_ * TW
                        self.mm(ps[4][:, :TW], self.SEL[0:16, e, :], WTf[0:16, n0:n0 + TW], True, True, [self.SEL, WTf], [ps[4]])
                        self.cp(self.act, wtb[:, :], ps[4][:, :TW], [ps[4]], [wtb])
                        for hc in range(4):
                            pg_, pu_ = ps[(hc % 2) * 2], ps[(hc % 2) * 2 + 1]
                            for k in range(8):
                                self.mm(pg_[:, :TW], gb[:, k, hc * 128:(hc + 1) * 128], hT[:, k, n0:n0 + TW], k == 0, k == 7, [gb, hT], [pg_])
                            for k in range(8):
                                self.mm(pu_[:, :TW], ub[:, k, hc * 128:(hc + 1) * 128], hT[:, k, n0:n0 + TW], k == 0, k == 7, [ub, hT], [pu_])
                            sg_ = sg[hc % 2]
                            self.actf(sg_[:, :], pg_[:, :TW], AF.Silu, [pg_], [sg_])
                            self.tt(V, sg_[:, :], sg_[:, :], pu_[:, :TW], ALU.mult, [sg_, pu_], [sg_])
                            self.tt(G, actb[:, hc, :], sg_[:, :], wtb[:, :], ALU.mult, [sg_, wtb], [actb])
                        for m in range(8):
                            pd = ps[4 + m % 4]
                            for hc in range(4):
                                self.mm(pd[:, :TW], db[:, hc, m * 128:(m + 1) * 128], actb[:, hc, :], hc == 0, hc == 3, [db, actb], [pd])
                            dst = yacc[:, m, tt_ * TW:(tt_ + 1) * TW]
                            if e == 0:
                                self.cp(self.act, dst, pd[:, :TW], [pd], [yacc])
                            else:
                                self.tt(V, dst, dst, pd[:, :TW], ALU.add, [yacc, pd], [yacc])
                for p_ in range(TG // 128):
                    n0 = g * TG + p_ * 128
                    tq = n0 % T
                    r = 2 if tq < TC else n0 // T
                    self.dma(xt2[:, :, :], self.XTv[:, :, n0:n0 + 128], self.dr("XT", n0, 128), [xt2])
                    for m in range(8):
                        self.stt(V, xt2[:, m, :], yacc[:, m, p_ * 128:(p_ + 1) * 128], self.MOD[l][:, 40 + m, r:r + 1], xt2[:, m, :], ALU.mult, ALU.add,
                                 [yacc, self.MOD[l], xt2], [xt2])
                    self.dma(self.XTv[:, :, n0:n0 + 128], xt2[:, :, :], [xt2], self.dr("XT", n0, 128))
            self.barrier()

    def final(self):
        V, G = self.dve, self.pool
        ps = self.ps
        with ExitStack() as es:
            def t(shape, dt=F32):
                return self.sb(shape, dt, es)
            xts = [t([128, 8, 128]) for _ in range(2)]
            sq = t([128, 8, 128])
            rs = t([128, 128])
            tmp = t([128, 8, 128])
            os_ = [t([128, 1024]) for _ in range(2)]
            i = 0
            for s in range(S):
                for j in range(2, NJ):
                    n0 = s * T + j * 128
                    xt = xts[i % 2]; o = os_[i % 2]; i += 1
                    self.dma(xt[:, :, :], self.XTv[:, :, n0:n0 + 128], self.dr("XT", n0, 128), [xt])
                    self.actf(sq[:, :, :], xt[:, :, :], AF.Square, [xt], [sq])
                    for c in range(8):
                        self.mm(ps[0][:, 0:128], self.ones[:, :], sq[:, c, :], c == 0, c == 7, [self.ones, sq], [ps[0]])
                    self.ts(V, rs[:, :], ps[0][:, 0:128], 1.0 / D, EPS, ALU.mult, ALU.add, [ps[0]], [rs])
                    self.actf(rs[:, :], rs[:, :], AF.Sqrt, [rs], [rs])
                    self.recip(rs[:, :], rs[:, :], [rs], [rs])
                    for c in range(8):
                        self.stt(V, tmp[:, c, :], xt[:, c, :], self.pfv(0, "norm_final", c), rs[:, :], ALU.mult, ALU.mult, [xt, rs, self.PFt[0]], [tmp])
                    for c in range(8):
                        pb = ps[1 + c // 4]
                        self.tr(pb[:, (c % 4) * 128:(c % 4 + 1) * 128], tmp[:, c, :], self.ident[:, :], [tmp, self.ident], [pb])
                    self.cp(self.act, o[:, 0:512], ps[1][:, :], [ps[1]], [o])
                    self.cp(V, o[:, 512:1024], ps[2][:, :], [ps[2]], [o])
                    self.dma(self.y[s, (j - 2) * 128:(j - 1) * 128, :], o[:, :], [o], [self.dd("y", s, j)])
            self.barrier()

    def rwkv_scan(self, l, gdn=False):
        V, G = self.dve, self.pool
        ps = self.ps
        nh = 4 if gdn else 8
        dv = 512 // nh
        DKQ, DMAT, DBC, DV_, DPC, DY, nm = ((self.GKQ, self.GMAT, self.GBC, self.GV, self.GPC, self.YB, 'G') if gdn else (self.RKQ, self.RMAT, self.RBC, self.RV, self.RPC, self.YA, 'R'))
        with ExitStack() as es:
            def t(shape, dt=F32):
                return self.sb(shape, dt, es)
            chains = [(s, d) for s in range(S) for d in range(2)]
            order = {0: list(range(NJ)), 1: [1, 0] + list(range(NJ - 1, 1, -1))}
            bufs = []
            for _ in chains:
                ld = [(t([128, 4, 256], BF16), t([128, nh, 512], BF16), t([128, 1024], BF16), t([128, 512], BF16), t([128, 8])) for _ in range(2)]
                bufs.append(dict(ld=ld, H=t([128, 4, dv]), Hz=t([128, nh, dv], BF16), Rn=t([128, nh, dv], BF16),
                                 Ubz=[t([128, nh, dv], BF16) for _ in range(2)], Vz=[t([128, 512], BF16) for _ in range(2)],
                                 Yt=t([128, 512])))
            def chain(ci, s, d):
                for i in range(NJ):
                    j = order[d][i]
                    n0 = s * T + j * 128
                    b = bufs[ci]
                    KQ, MAT, BC, Vt, pc = b["ld"][i % 2]
                    H, Hz, Rn, Ubz, Vz, Yt = b["H"], b["Hz"], b["Rn"], b["Ubz"], b["Vz"], b["Yt"]
                    self.dma(KQ[:, :, :].rearrange("p m t -> p (m t)"), DKQ[s, d, j], [self.dd(nm + "KQ", s, d, j)], [KQ])
                    self.dma(MAT[:, :, :].rearrange("p h t -> p (h t)"), DMAT[s, d, j], [self.dd(nm + "MAT", s, d, j)], [MAT])
                    self.dma(BC[:, :], DBC[s, d, j], [self.dd(nm + "BC", s, d, j)], [BC])
                    self.dma(Vt[:, :], DV_[s, j], [self.dd(nm + "V", s, j)], [Vt])
                    self.dma(pc[:, :], DPC[s, d, j], [self.dd(nm + "PC", s, d, j)], [pc])
                    if i == 0:
                        self.memset(G, H[:, :, :], 0.0, [H])
                        self.memset(G, Hz[:, :, :], 0.0, [Hz])
                        self.memset(G, Rn[:, :, :], 0.0, [Rn])
                        for c in range(2):
                            self.memset(G, Ubz[c][:, :, :], 0.0, [Ubz[c]])
                            self.memset(G, Vz[c][:, :], 0.0, [Vz[c]])
                    for c in range(2):
                        self.cp(G, Vz[c][c * 64:c * 64 + 64, :], Vt[c * 64:c * 64 + 64, :], [Vt], [Vz[c]])
                    pA, pB = ps[2 * ci], ps[2 * ci + 1]
                    pAv = pA[:, :].rearrange("p (h v) -> p h v", v=dv)
                    pBv = pB[:, :].rearrange("p (h v) -> p h v", v=dv)
                    if not gdn:
                        pBe = pB[:, :].rearrange("p (m e v) -> p m e v", e=2, v=64)
                        Hze = Hz[:, :, :].rearrange("p (m e) v -> p m e v", e=2)
                    pcv = pc[:, :].rearrange("p (m c) -> p m c", c=2)
                    for c in ([0, 1] if d == 0 else [1, 0]):
                        cs = slice(c * 64, c * 64 + 64)
                        Ub = Ubz[c]
                        for h in range(nh):
                            m = h if gdn else h // 2
                            self.mm(pAv[:, h, :], KQ[:, m, 0:128], Hz[:, h, :], True, False, [KQ, Hz], [pA])
                            self.mm(pAv[:, h, :], MAT[:, h, 256:384], Vt[:, h * dv:(h + 1) * dv], False, True, [MAT, Vt], [pA])
                        self.ts(V, Rn[cs, :, :], pAv[cs, :, :], -1.0, None, ALU.mult, None, [pA], [Rn])
                        for h in range(nh):
                            self.mm(pBv[:, h, :], MAT[:, h, 0:128], Rn[:, h, :], True, True, [MAT, Rn], [pB])
                        self.cp(self.act, Ub[cs, :, :], pBv[cs, :, :], [pB], [Ub])
                        for h in range(nh):
                            m = h if gdn else h // 2
                            self.mm(pAv[:, h, :], KQ[:, m, 128:256], Hz[:, h, :], True, False, [KQ, Hz], [pA])
                            self.mm(pAv[:, h, :], MAT[:, h, 128:256], Ub[:, h, :], False, False, [MAT, Ub], [pA])
                            self.mm(pAv[:, h, :], MAT[:, h, 384:512], Vt[:, h * dv:(h + 1) * dv], False, True, [MAT, Vt], [pA])
                        self.cp(V, Yt[cs, :], pA[cs, :], [pA], [Yt])
                        for h in range(nh):
                            m = h if gdn else h // 2
                            self.mm(pBv[:, h, :], BC[:, m * 128:(m + 1) * 128], Ub[:, h, :], True, False, [BC, Ub], [pB])
                            self.mm(pBv[:, h, :], BC[:, 512 + m * 128:512 + (m + 1) * 128], Vz[c][:, h * dv:(h + 1) * dv], False, True, [BC, Vz[c]], [pB])
                        if gdn:
                            self.tt(V, H[:, :, :], H[:, :, :], pcv[:, :, c:c + 1].to_broadcast([128, 4, dv]), ALU.mult, [H, pc], [H])
                            self.tt(V, H[:, :, :], H[:, :, :], pBv[:, :, :], ALU.add, [H, pB], [H])
                            self.cp(self.act, Hz[:, :, :], H[:, :, :], [H], [Hz])
                        else:
                            for e in range(2):
                                rows = slice(e * 64, e * 64 + 64)
                                self.tt(V, H[rows, :, :], H[rows, :, :], pBe[rows, :, e, :], ALU.add, [H, pB], [H])
                            self.tt(V, H[:, :, :], H[:, :, :], pcv[:, :, c:c + 1].to_broadcast([128, 4, 64]), ALU.mult, [H, pc], [H])
                            for e in range(2):
                                rows = slice(e * 64, e * 64 + 64)
                                self.cp(self.act, Hze[rows, :, e, :], H[rows, :, :], [H], [Hz])
                    self.dma(DY[d, n0:n0 + 128, :], Yt[:, :], [Yt], [self.dd(nm + "Y", d, n0 // 128)])
            self.run_interleaved([(lambda ci=ci, s=s, d=d: chain(ci, s, d)) for ci, (s, d) in enumerate(chains)])
            self.barrier()

    def build(self):
        try:
            self.build_()
        except StopBuild:
            self.es2 = None
            self.finish()

    def build_(self):
        self.consts()
        self.gdn_setup()
        self.conf_setup()
        self.phase0()
        if self.stop == "0":
            return self.finish()
        for l in range(self.nlayers):
            self.phaseA(l)
            if self.stop == f"A{l}":
                return self.finish()
            self.phaseB(l)
            if self.stop == f"B{l}":
                return self.finish()
            self.rwkv_prep(l)
            if self.stop == f"C{l}":
                return self.finish()
            self.rwkv_scan(l)
            if self.stop == f"D{l}":
                return self.finish()
            self.gdn_prep(l)
            if self.stop == f"E{l}":
                return self.finish()
            self.rwkv_scan(l, gdn=True)
            if self.stop == f"F{l}":
                return self.finish()
            self.conformer(l)
            if self.stop == f"G{l}":
                return self.finish()
            self.merge(l)
            if self.stop == f"H{l}":
                return self.finish()
            self.moe(l)
            if self.stop == f"I{l}":
                return self.finish()
        self.final()
        self.finish()
```

```python
import threading
import numpy as np
from contextlib import ExitStack
import concourse.bass as bass
import concourse.mybir as mybir
from concourse.bass_utils import run_bass_kernel_spmd

F32 = mybir.dt.float32
BF16 = mybir.dt.bfloat16
ALU = mybir.AluOpType
AF = mybir.ActivationFunctionType
AX = mybir.AxisListType

D = 1024
S = 2
TC = 256
TL = 2048
T = TC + TL
NT = S * T
L = 2
NIN = 8080
NDS = 24
NDS_SW = 8


class Dep:
    __slots__ = ("w", "r")

    def __init__(self):
        self.w = None
        self.r = {}


class Tl:
    def __init__(self, t):
        self.t = t
        self.d = Dep()

    def __getitem__(self, k):
        return self.t[k]


class Eng:
    def __init__(self, name, be, sem):
        self.key = name
        self.be = be
        self.sem = sem
        self.n = 0
        self.waited = {}


def _d(x):
    return x.d if hasattr(x, "d") else x


class KB:
    def __init__(self, dbg=()):
        self.nc = nc = bass.Bass("TRN2", target_bir_lowering=False)
        self.es = ExitStack()
        self.dbg = set(dbg)
        e = self.es.enter_context
        self.pe = Eng("pe", nc.tensor, e(nc.semaphore("s_pe")))
        self.act = Eng("act", nc.scalar, e(nc.semaphore("s_act")))
        self.dve = Eng("dve", nc.vector, e(nc.semaphore("s_dve")))
        self.pool = Eng("pool", nc.gpsimd, e(nc.semaphore("s_pool")))
        self.sp = Eng("sp", nc.sync, e(nc.semaphore("s_sp")))
        self.engs = [self.pe, self.act, self.dve, self.pool, self.sp]
        self.dsem = [e(nc.semaphore(f"s_d{i}")) for i in range(NDS + NDS_SW)]
        self.dcnt = [0] * (NDS + NDS_SW)
        self.drr = 0
        self.drr_sw = 0
        self.ddeps = {}
        self.ntile = 0
        self.yielders = {}

    def sb(self, shape, dt=F32, es=None):
        self.ntile += 1
        t = (es or self.es).enter_context(self.nc.sbuf_tensor(f"t{self.ntile}", list(shape), dt))
        return Tl(t)

    def psum(self, shape, dt=F32, es=None):
        self.ntile += 1
        t = (es or self.es).enter_context(self.nc.psum_tensor(f"p{self.ntile}", list(shape), dt))
        return Tl(t)

    def dram(self, name, shape, dt=F32, kind=None):
        if kind is None:
            kind = "ExternalOutput" if name in self.dbg else "Internal"
        return self.nc.dram_tensor(name, list(shape), dt, kind=kind).ap()

    def dd(self, *key):
        d = self.ddeps.get(key)
        if d is None:
            d = self.ddeps[key] = Dep()
        return d

    def dr(self, name, n0, w):
        return [self.dd(name, i) for i in range(n0 // 128, (n0 + w + 127) // 128)]

    def _sync(self, E, R, W):
        need = {}

        def upd(tok):
            k, sem, val = tok
            if k not in need or need[k][1] < val:
                need[k] = (sem, val)

        for d in R:
            d = _d(d)
            if d.w:
                upd(d.w)
        for d in W:
            d = _d(d)
            if d.w:
                upd(d.w)
            for k, (sem, val) in d.r.items():
                if k != E.key:
                    upd((k, sem, val))
        for k, (sem, val) in need.items():
            if k == E.key and E is self.pe:
                continue
            if E.waited.get(k, 0) < val:
                E.be.wait_ge(sem, val)
                E.waited[k] = val

    def _mark(self, tok, R, W):
        k, sem, val = tok
        for d in R:
            _d(d).r[k] = (sem, val)
        for d in W:
            d = _d(d)
            d.w = tok
            d.r = {}

    def op(self, E, fn, R, W):
        self._sync(E, R, W)
        ins = fn()
        E.n += 1
        ins.then_inc(E.sem, 1)
        self._mark((E.key, E.sem, E.n), R, W)
        self._yield()

    def dma(self, out, in_, R, W, Q=None, **kw):
        Q = Q or self.sp
        self._sync(Q, R, W)
        sw = Q is self.pool
        if sw:
            s = NDS + self.drr_sw
            self.drr_sw = (self.drr_sw + 1) % NDS_SW
        else:
            s = self.drr
            self.drr = (s + 1) % NDS
        sem = self.dsem[s]
        k = ("d", s)
        if self.dcnt[s] > 0 and Q.waited.get(k, 0) < 16 * self.dcnt[s]:
            Q.be.wait_ge(sem, 16 * self.dcnt[s])
            Q.waited[k] = 16 * self.dcnt[s]
        Q.be.dma_start(out=out, in_=in_, **kw).then_inc(sem, 16)
        self.dcnt[s] += 1
        self._mark((k, sem, 16 * self.dcnt[s]), R, W)
        self._yield()

    def _yield(self):
        if self.yielders:
            y = self.yielders.get(threading.get_ident())
            if y:
                y()

    def run_interleaved(self, fns):
        n = len(fns)
        state = {"turn": 0, "done": [False] * n, "exc": None}
        cv = threading.Condition()

        def advance(i):
            for k in range(1, n + 1):
                nx = (i + k) % n
                if not state["done"][nx]:
                    state["turn"] = nx
                    break
            else:
                state["turn"] = -1
            cv.notify_all()

        def yielder(i):
            def y():
                with cv:
                    advance(i)
                    while state["turn"] != i:
                        cv.wait()
            return y

        def worker(i):
            with cv:
                while state["turn"] != i:
                    cv.wait()
            self.yielders[threading.get_ident()] = yielder(i)
            try:
                fns[i]()
            except BaseException as e:
                state["exc"] = e
            finally:
                self.yielders.pop(threading.get_ident(), None)
                with cv:
                    state["done"][i] = True
                    advance(i)

        ths = [threading.Thread(target=worker, args=(i,)) for i in range(n)]
        for th in ths:
            th.start()
        for th in ths:
            th.join()
        if state["exc"] is not None:
            raise state["exc"]

    def barrier(self):
        for E in self.engs:
            for E2 in self.engs:
                if E2 is not E and E2.n > 0 and E.waited.get(E2.key, 0) < E2.n:
                    E.be.wait_ge(E2.sem, E2.n)
                    E.waited[E2.key] = E2.n
            for s in range(NDS + NDS_SW):
                k = ("d", s)
                if self.dcnt[s] > 0 and E.waited.get(k, 0) < 16 * self.dcnt[s]:
                    E.be.wait_ge(self.dsem[s], 16 * self.dcnt[s])
                    E.waited[k] = 16 * self.dcnt[s]

    def mm(self, out, lhsT, rhs, start, stop, R, W):
        self.op(self.pe, lambda: self.nc.tensor.matmul(out, lhsT=lhsT, rhs=rhs, start=start, stop=stop), R, W)

    def tr(self, out, in_, ident, R, W):
        self.op(self.pe, lambda: self.nc.tensor.transpose(out, in_, ident), R, W)

    def actf(self, out, in_, func, R, W, bias=None, scale=None):
        kw = {}
        if bias is not None:
            kw["bias"] = bias
        if scale is not None:
            kw["scale"] = scale
        self.op(self.act, lambda: self.nc.scalar.activation(out=out, in_=in_, func=func, **kw), R, W)

    def ts(self, E, out, in0, s1, s2, op0, op1, R, W):
        if op1 is None:
            self.op(E, lambda: E.be.tensor_scalar(out=out, in0=in0, scalar1=s1, scalar2=None, op0=op0), R, W)
        else:
            self.op(E, lambda: E.be.tensor_scalar(out=out, in0=in0, scalar1=s1, scalar2=s2, op0=op0, op1=op1), R, W)

    def tt(self, E, out, in0, in1, op, R, W):
        self.op(E, lambda: E.be.tensor_tensor(out=out, in0=in0, in1=in1, op=op), R, W)

    def stt(self, E, out, in0, scalar, in1, op0, op1, R, W):
        self.op(E, lambda: E.be.scalar_tensor_tensor(out=out, in0=in0, scalar=scalar, in1=in1, op0=op0, op1=op1), R, W)

    def cp(self, E, out, in_, R, W):
        if E is self.act:
            self.op(E, lambda: self.nc.scalar.copy(out=out, in_=in_), R, W)
        else:
            self.op(E, lambda: E.be.tensor_copy(out=out, in_=in_), R, W)

    def recip(self, out, in_, R, W):
        self.op(self.dve, lambda: self.nc.vector.reciprocal(out=out, in_=in_), R, W)

    def memset(self, E, ap, v, W):
        self.op(E, lambda: E.be.memset(ap, v), [], W)


class PF:
    def __init__(self):
        self.cols = {}
        self.n = 0

    def add(self, name, nch):
        self.cols[name] = (self.n, nch)
        self.n += nch
        return self.cols[name][0]


def pf_layout():
    pf = PF()
    for nm, nch in [("norm_mix", 8), ("norm_ffn", 8), ("b_ada", 48), ("mu0", 15), ("mu1", 15),
                    ("w0_0", 4), ("w0_1", 4), ("a0_0", 4), ("a0_1", 4), ("kk", 4), ("ka", 4), ("rk", 4),
                    ("ln_g", 4), ("ln_b", 4), ("gconv", 60), ("gnorm", 1), ("alog", 1), ("dtb", 1),
                    ("cdw", 124), ("cdwb", 4), ("clng", 4), ("clnb", 4), ("norm_final", 8)]:
        pf.add(nm, nch)
    return pf


def _fm(v, nch):
    return np.ascontiguousarray(np.asarray(v, np.float32).reshape(nch, 128).T)


def pack_pf(inp, l):
    pf = pf_layout()
    out = np.zeros((128, pf.n), np.float32)

    def put(nm, arr):
        o, n = pf.cols[nm]
        out[:, o:o + n] = arr

    put("norm_mix", _fm(inp["norm_mix"][l], 8))
    put("norm_ffn", _fm(inp["norm_ffn"][l], 8))
    put("b_ada", _fm(inp["b_ada"][l], 48))
    put("mu0", _fm(inp["rwkv_mu"][l, 0], 15))
    put("mu1", _fm(inp["rwkv_mu"][l, 1], 15))
    for d in range(2):
        put(f"w0_{d}", _fm(inp["rwkv_w0"][l, d], 4))
        put(f"a0_{d}", _fm(inp["rwkv_a0"][l, d], 4))
    put("kk", _fm(inp["rwkv_kk"][l], 4))
    put("ka", _fm(inp["rwkv_ka"][l], 4))
    put("rk", _fm(inp["rwkv_rk"][l].reshape(-1), 4))
    put("ln_g", _fm(inp["rwkv_ln_g"][l], 4))
    put("ln_b", _fm(inp["rwkv_ln_b"][l], 4))
    gc = np.concatenate([_fm(inp["gdn_conv"][l, k], 12) for k in range(5)], axis=1)
    put("gconv", gc)
    put("gnorm", _fm(inp["gdn_norm"][l], 1))
    al = np.zeros((128, 1), np.float32); al[0:8, 0] = np.asarray(inp["gdn_A_log"][l]).reshape(-1)
    db = np.zeros((128, 1), np.float32); db[0:8, 0] = np.asarray(inp["gdn_dt_bias"][l]).reshape(-1)
    put("alog", al)
    put("dtb", db)
    cd = np.concatenate([_fm(inp["conf_dw"][l, k], 4) for k in range(31)], axis=1)
    put("cdw", cd)
    put("cdwb", _fm(inp["conf_dw_b"][l], 4))
    put("clng", _fm(inp["conf_ln_g"][l], 4))
    put("clnb", _fm(inp["conf_ln_b"][l], 4))
    put("norm_final", _fm(inp["norm_final"], 8))
    return out


EPS = 1e-6


class Prog(KB):
    def __init__(self, dbg=(), stop=None, nlayers=L):
        super().__init__(dbg)
        self.stop = stop
        self.nlayers = nlayers
        nc = self.nc
        self.pf = pf_layout()

        def inp(name, shape):
            return nc.dram_tensor(name, list(shape), F32, kind="ExternalInput").ap()

        self.x = inp("x", [S, TL, D])
        self.ctx = inp("ctx", [S, TC, D])
        self.cvecT = inp("cvecT", [D, 3])
        self.pfp = inp("pfp", [L, 128, self.pf.n])
        self.w_ada = inp("w_ada", [L, D, 6 * D])
        self.w_in = inp("w_in", [L, D, NIN])
        self.rw2 = inp("rwkv_w2", [L, 2, 64, 512])
        self.ra2 = inp("rwkv_a2", [L, 2, 64, 512])
        self.rg2 = inp("rwkv_g2", [L, 128, 512])
        self.w_branch = inp("w_branch", [L, 3, 512, D])
        self.w_out = inp("w_out", [L, D, D])
        self.w_router = inp("w_router", [D, 16])
        self.rbias = inp("rbias", [128, 16])
        self.weg = inp("w_e_gate", [L, 16, D, 512])
        self.weu = inp("w_e_up", [L, 16, D, 512])
        self.wed = inp("w_e_down", [L, 16, 512, D])
        self.y = nc.dram_tensor("y", [S, TL, D], F32, kind="ExternalOutput").ap()
        self.XT = self.dram("XT", [D, NT])
        self.XTv = self.XT.rearrange("(c p) n -> p c n", p=128)
        self.PT = self.dram("PT", [NIN, NT])
        self.ps = [self.psum([128, 512], F32) for _ in range(8)]
        self.ident = self.sb([128, 128], F32)
        self.identb = self.sb([128, 128], BF16)
        self.ones = self.sb([128, 128], F32)
        self.PFt = [self.sb([128, self.pf.n], F32) for _ in range(L)]
        self.scT = self.sb([128, 8, 3], F32)
        self.MOD = [self.sb([128, 48, 3], F32) for _ in range(L)]
        self.GM = [self.sb([128, 8, 3], F32) for _ in range(L)]
        self.GF = [self.sb([128, 8, 3], F32) for _ in range(L)]

    def pfv(self, l, name, c=None, n=1):
        o, nch = self.pf.cols[name]
        if c is None:
            return self.PFt[l][:, o:o + nch]
        return self.PFt[l][:, o + c:o + c + n]

    def consts(self):
        nc = self.nc
        P = self.pool
        self.memset(P, self.ident[:, :], 0.0, [self.ident])
        self.op(P, lambda: nc.gpsimd.affine_select(out=self.ident[:, :], in_=self.ident[:, :], pattern=[[-1, 128]],
                                                   compare_op=ALU.not_equal, fill=1.0, base=0, channel_multiplier=1),
                [self.ident], [self.ident])
        self.cp(P, self.identb[:, :], self.ident[:, :], [self.ident], [self.identb])
        self.memset(P, self.ones[:, :], 1.0, [self.ones])
        for l in range(L):
            self.dma(self.PFt[l][:, :], self.pfp[l], [], [self.PFt[l]])
        cv = self.sb([128, 8, 3], F32)
        self.dma(cv[:, :, :], self.cvecT.rearrange("(c p) r -> p c r", p=128), [], [cv])
        self.actf(self.scT[:, :, :], cv[:, :, :], AF.Silu, [cv], [self.scT])

    def phase0(self):
        with ExitStack() as es:
            xin = [self.sb([128, 1024], F32, es) for _ in range(2)]
            xo = [self.sb([128, 8, 128], F32, es) for _ in range(2)]
            i = 0
            for s in range(S):
                for j in range(T // 128):
                    n0 = s * T + j * 128
                    src = self.ctx[s, j * 128:(j + 1) * 128, :] if j < 2 else self.x[s, (j - 2) * 128:(j - 1) * 128, :]
                    a = xin[i % 2]
                    o = xo[i % 2]
                    self.dma(a[:, :], src, [], [a])
                    for c in range(8):
                        pb = self.ps[(i % 2) * 2 + c // 4]
                        self.tr(pb[:, (c % 4) * 128:(c % 4 + 1) * 128], a[:, c * 128:(c + 1) * 128], self.ident[:, :],
                                [a, self.ident], [pb])
                    for hf in range(2):
                        pb = self.ps[(i % 2) * 2 + hf]
                        self.cp(self.act if hf == 0 else self.dve, o[:, hf * 4:(hf + 1) * 4, :],
                                pb[:, :].rearrange("p (c t) -> p c t", c=4), [pb], [o])
                    self.dma(self.XTv[:, :, n0:n0 + 128], o[:, :, :], [o], self.dr("XT", n0, 128))
                    i += 1
            self.barrier()

    def phaseA(self, l):
        with ExitStack() as es:
            wa = [self.sb([128, 8, 768], F32, es) for _ in range(2)]
            pm = self.ps[0]
            wav = self.w_ada[l].rearrange("(k p) n -> p k n", p=128)
            for mg in range(8):
                w = wa[mg % 2]
                for q in range(4):
                    self.dma(w[:, q * 2:(q + 1) * 2, :], wav[:, q * 2:(q + 1) * 2, mg * 768:(mg + 1) * 768], [], [w])
                for m in range(6):
                    mm_ = mg * 6 + m
                    for k in range(8):
                        self.mm(pm[:, mm_ * 3:(mm_ + 1) * 3], w[:, k, m * 128:(m + 1) * 128], self.scT[:, k, :], k == 0, k == 7,
                                [w, self.scT], [pm])
            mod = self.MOD[l]
            self.tt(self.dve, mod[:, :, :], pm[:, 0:144].rearrange("p (m r) -> p m r", r=3),
                    self.pfv(l, "b_ada").unsqueeze(2).to_broadcast([128, 48, 3]), ALU.add, [pm, self.PFt[l]], [mod])
            for (G, mi, nm) in ((self.GM[l], 1, "norm_mix"), (self.GF[l], 4, "norm_ffn")):
                self.ts(self.dve, G[:, :, :], mod[:, mi * 8:(mi + 1) * 8, :], 1.0, None, ALU.add, None, [mod], [G])
                self.dump(f"G1{l}{mi}", G, G[:, :, :], [128, 8, 3])
                self.tt(self.dve, G[:, :, :], G[:, :, :], self.pfv(l, nm).unsqueeze(2).to_broadcast([128, 8, 3]), ALU.mult,
                        [G, self.PFt[l]], [G])
            self.dump(f"MOD{l}", mod, mod[:, :, :], [128, 48, 3])
            self.dump(f"GM{l}", self.GM[l], self.GM[l][:, :, :], [128, 8, 3])
            self.barrier()

    def tiles(self):
        out = []
        for s in range(S):
            out.append((s * T, TC, 2))
            for j in range(TL // 512):
                out.append((s * T + TC + j * 512, 512, s))
        return out

    def modulate_tile(self, es_bufs, n0, w, r, G, shift_col, mod, out_fn):
        xt, sq, rs, tmp = es_bufs
        psS = self.ps[7]
        self.dma(xt[:, :, :w], self.XTv[:, :, n0:n0 + w], self.dr("XT", n0, w), [xt])
        self.actf(sq[:, :, :w], xt[:, :, :w], AF.Square, [xt], [sq])
        for c in range(8):
            self.mm(psS[:, :w], self.ones[:, :], sq[:, c, :w], c == 0, c == 7, [self.ones, sq], [psS])
        self.ts(self.dve, rs[:, :w], psS[:, :w], 1.0 / D, EPS, ALU.mult, ALU.add, [psS], [rs])
        self.actf(rs[:, :w], rs[:, :w], AF.Sqrt, [rs], [rs])
        self.recip(rs[:, :w], rs[:, :w], [rs], [rs])
        return xt, rs

    def phaseB(self, l):
        with ExitStack() as es:
            hT = self.sb([128, 8, NT], BF16, es)
            with ExitStack() as es1:
                xts = [self.sb([128, 8, 512], F32, es1) for _ in range(2)]
                sq = self.sb([128, 8, 512], F32, es1)
                rs = self.sb([128, 512], F32, es1)
                tmps = [self.sb([128, 512], F32, es1) for _ in range(2)]
                for i, (n0, w, r) in enumerate(self.tiles()):
                    xt, _ = self.modulate_tile((xts[i % 2], sq, rs, None), n0, w, r, None, None, None, None)
                    for c in range(8):
                        tmp = tmps[c % 2]
                        self.stt(self.dve, tmp[:, :w], xt[:, c, :w], self.GM[l][:, c, r:r + 1], rs[:, :w], ALU.mult, ALU.mult,
                                 [xt, rs, self.GM[l]], [tmp])
                        self.actf(hT[:, c, n0:n0 + w], tmp[:, :w], AF.Identity, [tmp, self.MOD[l]], [hT],
                                  bias=self.MOD[l][:, c, r:r + 1], scale=1.0)
                if "HT" in self.dbg:
                    hd = self.dram("HT", [128, 8, NT], BF16)
                    self.dma(hd, hT[:, :, :], [hT], [self.dd("HTd")])
                self.barrier()
            wbf = [self.sb([128, 8, 1024], BF16, es) for _ in range(2)]
            ost = [self.sb([128, 512], F32, es) for _ in range(4)]
            no = 0
            def loadw(g):
                c0 = g * 1024
                cw = min(1024, NIN - c0)
                self.dma(wbf[g % 2][:, :, :cw], self.w_in[l][:, c0:c0 + cw].rearrange("(k p) n -> p k n", p=128), [], [wbf[g % 2]], Q=self.pool)
            loadw(0)
            for g in range(8):
                c0 = g * 1024
                cw = min(1024, NIN - c0)
                wb = wbf[g % 2]
                if g < 7:
                    loadw(g + 1)
                nm = (cw + 127) // 128
                for tt_ in range(NT // 512):
                    n0 = tt_ * 512
                    for m in range(nm):
                        mw = min(128, cw - m * 128)
                        pb = self.ps[no % 6]
                        for k in range(8):
                            self.mm(pb[:mw, :], wb[:, k, m * 128:m * 128 + mw], hT[:, k, n0:n0 + 512], k == 0, k == 7,
                                    [wb, hT], [pb])
                        o = ost[no % 4]
                        self.cp(self.act if no % 2 == 0 else self.dve, o[:mw, :], pb[:mw, :], [pb], [o])
                        self.dma(self.PT[c0 + m * 128:c0 + m * 128 + mw, n0:n0 + 512], o[:mw, :], [o],
                                 [self.dd("PT", (c0 + m * 128) // 128, j) for j in range(n0 // 128, n0 // 128 + 4)])
                        no += 1
            self.barrier()

    def dump(self, name, tile, ap, shape, dt=F32):
        if name in self.dbg:
            d = self.dram(name, shape, dt)
            self.dma(d, ap, [tile], [self.dd(name)])

    def finish(self):
        self.barrier()

    def build(self):
        self.consts()
        self.phase0()
        if self.stop == "0":
            return self.finish()
        for l in range(self.nlayers):
            self.phaseA(l)
            if self.stop == f"A{l}":
                return self.finish()
            self.phaseB(l)
            if self.stop == f"B{l}":
                return self.finish()
        self.finish()


def make_in_maps(inp):
    ncores = 8
    pfp = np.stack([pack_pf(inp, l) for l in range(L)])
    rbias = np.ascontiguousarray(np.broadcast_to(np.asarray(inp["router_bias"], np.float32)[None, :], (128, 16)))
    maps = []
    for i in range(ncores):
        cv = np.stack([inp["c"][2 * i], inp["c"][2 * i + 1], inp["c_ctx"]], axis=1).astype(np.float32)
        m = {
            "x": np.ascontiguousarray(inp["x"][2 * i:2 * i + 2]),
            "ctx": np.ascontiguousarray(inp["ctx"][2 * i:2 * i + 2]),
            "cvecT": np.ascontiguousarray(cv),
            "pfp": pfp, "rbias": rbias,
        }
        for k in ("w_ada", "w_in", "rwkv_w2", "rwkv_a2", "rwkv_g2", "w_branch", "w_out", "w_router",
                  "w_e_gate", "w_e_up", "w_e_down"):
            m[k] = np.ascontiguousarray(inp[k], dtype=np.float32)
        maps.append(m)
    return maps


def kernel(**inputs):
    inp = {k: np.asarray(v) for k, v in inputs.items()}
    prog = Prog2()
    prog.build()
    maps = make_in_maps(inp)
    res = run_bass_kernel_spmd(prog.nc, maps, core_ids=list(range(8)))
    return np.concatenate([r["y"] for r in res.results], axis=0).astype(np.float32)


CDEC = 0.6065306597126334
NJ = T // 128


def seg_bounds(j):
    return (j == 0 or j == 2), (j == 1 or j == NJ - 1)


class StopBuild(Exception):
    pass


class Prog2(Prog):
    cut = None

    def ck(self, n):
        if self.cut == n:
            raise StopBuild()

    def __init__(self, **kw):
        super().__init__(**kw)
        self.PTv = self.PT[0:8064, :].rearrange("(c p) n -> p c n", p=128)
        self.RKQ = self.dram("RKQ", [S, 2, NJ, 128, 4 * 256], BF16)
        self.RMAT = self.dram("RMAT", [S, 2, NJ, 128, 8 * 512], BF16)
        self.RBC = self.dram("RBC", [S, 2, NJ, 128, 1024], BF16)
        self.RV = self.dram("RV", [S, NJ, 128, 512], BF16)
        self.RPC = self.dram("RPC", [S, 2, NJ, 128, 8], F32)
        self.GAs = self.dram("GAs", [512, NT])
        self.BON = self.dram("BON", [512, NT])
        self.YA = self.dram("YA", [2, NT, 512])
        self.bones = self.sb([128, 128], F32)
        self.MU = self.sb([128, 256], F32)
        self.ML = self.sb([128, 256], F32)
        self.RM = self.sb([128, 128], F32)

    def consts(self):
        super().consts()
        nc = self.nc
        P = self.pool
        self.memset(P, self.bones[:, :], 0.0, [self.bones])
        self.memset(P, self.bones[0:64, 0:64], 1.0, [self.bones])
        self.memset(P, self.bones[64:128, 64:128], 1.0, [self.bones])
        self.memset(P, self.RM[:, :], 1.0, [self.RM])
        self.memset(P, self.RM[:, 0:1], 0.0, [self.RM])
        self.memset(P, self.RM[:, 64:65], 0.0, [self.RM])
        for (Mt, off, cmp_, sg) in ((self.MU, 0, ALU.is_gt, 1), (self.MU, 128, ALU.is_ge, 1), (self.ML, 0, ALU.is_gt, -1), (self.ML, 128, ALU.is_ge, -1)):
            sl = Mt[:, off:off + 128]
            self.memset(P, sl, 1.0, [Mt])
            self.op(P, lambda sl=sl, cmp_=cmp_, sg=sg: nc.gpsimd.affine_select(out=sl, in_=sl, pattern=[[sg, 128]], compare_op=cmp_, fill=0.0,
                                                                              base=0, channel_multiplier=-sg), [Mt], [Mt])
            self.memset(P, Mt[0:64, off + 64:off + 128], 0.0, [Mt])
            self.memset(P, Mt[64:128, off:off + 64], 0.0, [Mt])

    def load_halo(self, dst, c0, nch, s, j, hw, Q=None):
        n0 = s * T + j * 128
        lb, rb = seg_bounds(j)
        lo = 0 if lb else hw
        hi = 0 if rb else hw
        if lb:
            self.memset(self.pool, dst[:, :, 0:hw], 0.0, [dst])
        if rb:
            self.memset(self.pool, dst[:, :, 128 + hw:128 + 2 * hw], 0.0, [dst])
        deps = [self.dd("PT", c, i) for c in range(c0, c0 + nch) for i in range((n0 - lo) // 128, (n0 + 128 + hi - 1) // 128 + 1)]
        self.dma(dst[:, :, hw - lo:hw + 128 + hi], self.PTv[:, c0:c0 + nch, n0 - lo:n0 + 128 + hi], deps, [dst], Q=Q)

    def inverse(self, X1, NN, MAT, h, Wt, At, Bt, pA, pB_, pC):
        V, G = self.dve, self.pool
        W, A, B = Wt[0], At[0], Bt[0]
        self.tt(G, W[:, :], self.identb[:, :], X1[:, 0:128], ALU.subtract, [self.identb, X1], [W])
        self.mm(pA[:, 0:128], X1[:, 0:128], NN[:, :], True, True, [X1, NN], [pA])
        self.mm(pB_[:, 0:128], NN[:, :], X1[:, 0:128], True, True, [X1, NN], [pB_])
        self.cp(self.act, A[:, :], pA[:, 0:128], [pA], [A])
        self.cp(self.act, B[:, :], pB_[:, 0:128], [pB_], [B])
        for it in range(5):
            W2, A2, B2 = Wt[(it + 1) % 2], At[(it + 1) % 2], Bt[(it + 1) % 2]
            self.mm(pC[:, 0:128], A[:, :], W[:, :], True, True, [A, W], [pC])
            if it < 4:
                self.mm(pA[:, 0:128], B[:, :], A[:, :], True, True, [A, B], [pA])
                self.mm(pB_[:, 0:128], A[:, :], B[:, :], True, True, [A, B], [pB_])
            dstW = W2[:, :] if it < 4 else MAT[:, h, 0:128]
            self.tt(V, dstW, W[:, :], pC[:, 0:128], ALU.add, [W, pC], [W2 if it < 4 else MAT])
            if it < 4:
                self.cp(self.act, A2[:, :], pA[:, 0:128], [pA], [A2])
                self.cp(self.act, B2[:, :], pB_[:, 0:128], [pB_], [B2])
            W, A, B = W2, A2, B2

    def inverse_batch(self, X1a, NNa, MAT, h0, banks, Wt, At, Bt):
        V, G = self.dve, self.pool
        psW, psA, psB = banks
        hs = slice(h0, h0 + 4)

        def reg(p, i):
            return p[:, i * 128:(i + 1) * 128]

        def bv(p):
            return p[:, :].rearrange("p (h t) -> p h t", h=4)
        W, A, B = Wt[0], At[0], Bt[0]
        self.tt(G, W[:, :, :], self.identb[:, :].unsqueeze(1).to_broadcast([128, 4, 128]), X1a[:, hs, 0:128], ALU.subtract, [self.identb, X1a], [W])
        for i in range(4):
            self.mm(reg(psA, i), X1a[:, h0 + i, 0:128], NNa[:, h0 + i, :], True, True, [X1a, NNa], [psA])
        for i in range(4):
            self.mm(reg(psB, i), NNa[:, h0 + i, :], X1a[:, h0 + i, 0:128], True, True, [X1a, NNa], [psB])
        self.cp(self.act, A[:, :, :], bv(psA), [psA], [A])
        self.cp(self.act, B[:, :, :], bv(psB), [psB], [B])
        for it in range(5):
            W2, A2, B2 = Wt[(it + 1) % 2], At[(it + 1) % 2], Bt[(it + 1) % 2]
            for i in range(4):
                self.mm(reg(psW, i), A[:, i, :], W[:, i, :], True, True, [A, W], [psW])
            if it < 4:
                for i in range(4):
                    self.mm(reg(psA, i), B[:, i, :], A[:, i, :], True, True, [A, B], [psA])
                for i in range(4):
                    self.mm(reg(psB, i), A[:, i, :], B[:, i, :], True, True, [A, B], [psB])
                self.tt(V, W2[:, :, :], W[:, :, :], bv(psW), ALU.add, [W, psW], [W2])
                self.cp(self.act, A2[:, :, :], bv(psA), [psA], [A2])
                self.cp(self.act, B2[:, :, :], bv(psB), [psB], [B2])
            else:
                self.tt(V, MAT[:, hs, 0:128], W[:, :, :], bv(psW), ALU.add, [W, psW], [MAT])
            W, A, B = W2, A2, B2

    def rwkv_prep(self, l):
        nc = self.nc
        V, G = self.dve, self.pool
        with ExitStack() as es:
            def t(shape, dt=F32):
                return self.sb(shape, dt, es)
            wtmp = t([128, 512])
            w2b, a2b, g2b = t([128, 512], BF16), t([128, 512], BF16), t([128, 512], BF16)
            for (src, dstb) in ((self.rw2[l].rearrange("d r c -> (d r) c"), w2b), (self.ra2[l].rearrange("d r c -> (d r) c"), a2b), (self.rg2[l], g2b)):
                self.dma(wtmp[:, :], src, [], [wtmp])
                self.cp(V, dstb[:, :], wtmp[:, :], [wtmp], [dstb])
            PFl = self.PFt[l]
            c0t = t([128, 15])
            self.tt(V, c0t[:, :], self.pfv(l, "mu0"), self.pfv(l, "mu1"), ALU.add, [PFl], [c0t])
            self.ts(V, c0t[:, :], c0t[:, :], -1.0, 1.0, ALU.mult, ALU.add, [c0t], [c0t])
            omka = t([128, 4])
            self.ts(V, omka[:, :], self.pfv(l, "ka"), -1.0, 1.0, ALU.mult, ALU.add, [PFl], [omka])

            def bc(ap, n):
                return ap.unsqueeze(2).to_broadcast([128, n, 128])

            def stream(s):
                pa = t([128, 15, 130])
                sh = t([128, 15, 128])
                twb, xab, sgb = t([128, 128], BF16), t([128, 128], BF16), t([128, 128], BF16)
                SW = [t([128, 4, 128]) for _ in range(2)]
                AA = [t([128, 4, 128]) for _ in range(2)]
                ga = t([128, 4, 128])
                kx, kk, tq, bon = t([128, 4, 128]), t([128, 4, 128]), t([128, 4, 128]), t([128, 4, 128])
                CS, EX, tmpa, tmpb = (t([128, 4, 128]) for _ in range(4))
                e1, e2, e3 = (t([128, 4, 128]) for _ in range(3))
                pc = t([128, 8])
                KQ = t([128, 4, 256], BF16)
                BTb, CTb = t([128, 4, 128], BF16), t([128, 4, 128], BF16)
                vb = t([128, 4, 128], BF16)
                BC = t([128, 1024], BF16)
                Vt = t([128, 512], BF16)
                MAT = t([128, 8, 512], BF16)
                X1a = t([128, 8, 256], BF16)
                NNa = t([128, 8, 128], BF16)
                BTz, CTz = t([128, 8, 128], BF16), t([128, 8, 128], BF16)
                self.memset(G, BTz[:, :, :], 0.0, [BTz])
                self.memset(G, CTz[:, :, :], 0.0, [CTz])
                Wt = [t([128, 4, 128], BF16) for _ in range(2)]
                At = [t([128, 4, 128], BF16) for _ in range(2)]
                Bt = [t([128, 4, 128], BF16) for _ in range(2)]
                Wt2 = [t([128, 4, 128], BF16) for _ in range(2)]
                At2 = [t([128, 4, 128], BF16) for _ in range(2)]
                Bt2 = [t([128, 4, 128], BF16) for _ in range(2)]
                B = self.ps[4 * s:4 * s + 4]
                for j in range(NJ):
                    n0 = s * T + j * 128
                    self.ck(1000 + s * NJ + j)
                    self.load_halo(pa, 0, 15, s, j, 1)
                    self.ck(1)
                    self.tt(V, sh[:, :, :], pa[:, :, 1:129], bc(c0t[:, :], 15), ALU.mult, [pa, c0t], [sh])
                    for c in range(15):
                        self.stt(V, sh[:, c, :], pa[:, c, 0:128], self.pfv(l, "mu0", c), sh[:, c, :], ALU.mult, ALU.add, [pa, PFl, sh], [sh])
                        self.stt(V, sh[:, c, :], pa[:, c, 2:130], self.pfv(l, "mu1", c), sh[:, c, :], ALU.mult, ALU.add, [pa, PFl, sh], [sh])
                    self.ck(2)
                    r_, k_, v_ = sh[:, 0:4, :], sh[:, 4:8, :], sh[:, 8:12, :]
                    self.actf(twb[:, :], sh[:, 12, :], AF.Tanh, [sh], [twb])
                    self.cp(self.act, xab[:, :], sh[:, 13, :], [sh], [xab])
                    self.actf(sgb[:, :], sh[:, 14, :], AF.Sigmoid, [sh], [sgb])
                    self.ck(3)
                    for d in range(2):
                        for (wb_, xin, dst, bname, pb) in ((w2b, twb, SW[d], f"w0_{d}", B[0]), (a2b, xab, AA[d], f"a0_{d}", B[1])):
                            for m in range(4):
                                self.mm(pb[:, m * 128:(m + 1) * 128], wb_[d * 64:(d + 1) * 64, m * 128:(m + 1) * 128],
                                        xin[d * 64:(d + 1) * 64, :], True, True, [wb_, xin], [pb])
                            for m in range(4):
                                self.actf(dst[:, m, :], pb[:, m * 128:(m + 1) * 128], AF.Sigmoid, [pb, PFl], [dst],
                                          bias=self.pfv(l, bname, m), scale=1.0)
                    for m in range(4):
                        self.mm(B[2][:, m * 128:(m + 1) * 128], g2b[:, m * 128:(m + 1) * 128], sgb[:, :], True, True, [g2b, sgb], [B[2]])
                    self.cp(self.act, ga[:, :, :], B[2][:, :].rearrange("p (m t) -> p m t", m=4), [B[2]], [ga])
                    self.dma(self.GAs.rearrange("(m p) n -> p m n", p=128)[:, :, n0:n0 + 128], ga[:, :, :], [ga], [self.dd("GAs", n0 // 128)])
                    self.ck(4)
                    self.tt(V, kx[:, :, :], k_, bc(self.pfv(l, "kk"), 4), ALU.mult, [sh, PFl], [kx])
                    self.tt(G, tq[:, :, :], kx[:, :, :], kx[:, :, :], ALU.mult, [kx], [tq])
                    for m in range(4):
                        self.mm(B[3][:, m * 128:(m + 1) * 128], self.bones[:, :], tq[:, m, :], True, True, [self.bones, tq], [B[3]])
                    self.ts(V, tq[:, :, :], B[3][:, :].rearrange("p (m t) -> p m t", m=4), EPS, None, ALU.add, None, [B[3]], [tq])
                    self.actf(tq[:, :, :], tq[:, :, :], AF.Sqrt, [tq], [tq])
                    self.recip(tq[:, :, :], tq[:, :, :], [tq], [tq])
                    self.tt(V, kk[:, :, :], kx[:, :, :], tq[:, :, :], ALU.mult, [kx, tq], [kk])
                    self.ck(5)
                    self.tt(G, bon[:, :, :], r_, k_, ALU.mult, [sh], [bon])
                    self.tt(G, bon[:, :, :], bon[:, :, :], bc(self.pfv(l, "rk"), 4), ALU.mult, [bon, PFl], [bon])
                    for m in range(4):
                        self.mm(B[0][:, m * 128:(m + 1) * 128], self.bones[:, :], bon[:, m, :], True, True, [self.bones, bon], [B[0]])
                    self.tt(V, bon[:, :, :], B[0][:, :].rearrange("p (m t) -> p m t", m=4), v_, ALU.mult, [B[0], sh], [bon])
                    self.dma(self.BON.rearrange("(m p) n -> p m n", p=128)[:, :, n0:n0 + 128], bon[:, :, :], [bon], [self.dd("BON", n0 // 128)])
                    self.ck(6)
                    self.cp(G, vb[:, :, :], v_, [sh], [vb])
                    pbv = B[1][:, :].bitcast(BF16)
                    for m in range(4):
                        self.tr(pbv[:, m * 128:(m + 1) * 128], vb[:, m, :], self.identb[:, :], [vb, self.identb], [B[1]])
                    self.cp(self.act, Vt[:, :], pbv[:, 0:512], [B[1]], [Vt])
                    self.dma(self.RV[s, j], Vt[:, :], [Vt], [self.dd("RV", s, j)])
                    for d in range(2):
                        self.ck(7)
                        self.tt(V, tmpa[:, :, :], AA[d][:, :, :], bc(self.pfv(l, "ka"), 4), ALU.mult, [AA[d], PFl], [tmpa])
                        self.tt(V, tmpa[:, :, :], tmpa[:, :, :], bc(omka[:, :], 4), ALU.add, [tmpa, omka], [tmpa])
                        self.tt(V, tmpa[:, :, :], tmpa[:, :, :], k_, ALU.mult, [tmpa, sh], [tmpa])
                        self.tt(G, tmpb[:, :, :], kk[:, :, :], AA[d][:, :, :], ALU.mult, [kk, AA[d]], [tmpb])
                        self.ck(8)
                        for m in range(4):
                            self.op(V, lambda m=m: nc.vector.tensor_tensor_scan(out=CS[:, m, :], data0=self.RM[:, :], data1=SW[d][:, m, :],
                                                                               initial=0.0, op0=ALU.mult, op1=ALU.add),
                                    [self.RM, SW[d]], [CS])
                        CSv = CS[:, :, :].rearrange("p m (c t) -> p m c t", t=64)
                        if d == 0:
                            self.tt(V, EX[:, :, :], CS[:, :, :], SW[d][:, :, :], ALU.subtract, [CS, SW[d]], [EX])
                            incl = CS
                        else:
                            tot = CSv[:, :, :, 63:64].to_broadcast([128, 4, 2, 64])
                            self.tt(V, EX[:, :, :].rearrange("p m (c t) -> p m c t", t=64), tot, CSv, ALU.subtract, [CS], [EX])
                            self.tt(V, e3[:, :, :], EX[:, :, :], SW[d][:, :, :], ALU.add, [EX, SW[d]], [e3])
                            incl = e3
                        self.ck(9)
                        self.actf(e1[:, :, :], EX[:, :, :], AF.Exp, [EX], [e1], scale=-CDEC)
                        self.actf(e2[:, :, :], incl[:, :, :], AF.Exp, [incl], [e2], scale=CDEC)
                        self.actf(e3[:, :, :], incl[:, :, :], AF.Exp, [incl], [e3], scale=-CDEC)
                        self.actf(pc[:, :].rearrange("p (m c) -> p m c", c=2), CSv[:, :, :, 63], AF.Exp, [CS], [pc], scale=-CDEC)
                        self.dma(self.RPC[s, d, j], pc[:, :], [pc], [self.dd("RPC", s, d, j)])
                        self.tt(V, KQ[:, :, 0:128], kk[:, :, :], e1[:, :, :], ALU.mult, [kk, e1], [KQ])
                        self.tt(G, KQ[:, :, 128:256], r_, e3[:, :, :], ALU.mult, [sh, e3], [KQ])
                        self.tt(V, BTb[:, :, :], tmpb[:, :, :], e2[:, :, :], ALU.mult, [tmpb, e2], [BTb])
                        self.tt(G, CTb[:, :, :], tmpa[:, :, :], e2[:, :, :], ALU.mult, [tmpa, e2], [CTb])
                        self.dma(self.RKQ[s, d, j], KQ[:, :, :].rearrange("p m t -> p (m t)"), [KQ], [self.dd("RKQ", s, d, j)])
                        self.ck(10)
                        pbb = B[2][:, :].bitcast(BF16)
                        for m in range(4):
                            self.tr(pbb[:, m * 128:(m + 1) * 128], BTb[:, m, :], self.identb[:, :], [BTb, self.identb], [B[2]])
                            self.tr(pbb[:, 512 + m * 128:512 + (m + 1) * 128], CTb[:, m, :], self.identb[:, :], [CTb, self.identb], [B[2]])
                        self.cp(self.act, BC[:, :], pbb[:, :], [B[2]], [BC])
                        self.dma(self.RBC[s, d, j], BC[:, :], [BC], [self.dd("RBC", s, d, j)])
                        self.ck(11)
                        Ms, Mn = (self.MU, self.ML) if d == 0 else (self.ML, self.MU)
                        for e_ in range(2):
                            rows = slice(e_ * 64, e_ * 64 + 64)
                            self.cp(G, BTz[:, :, :].rearrange("p (m e) t -> p m e t", e=2)[rows, :, e_, :], BTb[rows, :, :], [BTb], [BTz])
                            self.cp(self.act, CTz[:, :, :].rearrange("p (m e) t -> p m e t", e=2)[rows, :, e_, :], CTb[rows, :, :], [CTb], [CTz])
                        Msb = Ms[:, :].unsqueeze(1).to_broadcast([128, 2, 256])
                        v2 = lambda p: p[:, :].rearrange("p (h t) -> p h t", h=2)
                        for h in range(8):
                            self.mm(B[h // 2][:, (h % 2) * 256:(h % 2 + 1) * 256], BTz[:, h, :], KQ[:, h // 2, :], True, True, [BTz, KQ], [B[h // 2]])
                        for q in range(4):
                            self.tt(V, X1a[:, 2 * q:2 * q + 2, :], v2(B[q]), Msb, ALU.mult, [B[q], Ms], [X1a])
                        for h in range(8):
                            self.mm(B[h // 2][:, (h % 2) * 256:(h % 2 + 1) * 256], CTz[:, h, :], KQ[:, h // 2, :], True, True, [CTz, KQ], [B[h // 2]])
                        for q in range(4):
                            self.tt(V, MAT[:, 2 * q:2 * q + 2, 256:512], v2(B[q]), Msb, ALU.mult, [B[q], Ms], [MAT])
                        for h in range(8):
                            self.mm(B[h // 4][:, (h % 4) * 128:(h % 4 + 1) * 128], KQ[:, h // 2, 0:128], BTz[:, h, :], True, True, [BTz, KQ], [B[h // 4]])
                        Mnb = Mn[:, 0:128].unsqueeze(1).to_broadcast([128, 4, 128])
                        for q in range(2):
                            self.tt(V, NNa[:, 4 * q:4 * q + 4, :], B[q][:, :].rearrange("p (h t) -> p h t", h=4), Mnb, ALU.mult, [B[q], Mn], [NNa])
                        self.cp(G, MAT[:, :, 128:256], X1a[:, :, 128:256], [X1a], [MAT])
                        self.inverse_batch(X1a, NNa, MAT, 0, (B[0], B[1], B[2]), Wt, At, Bt)
                        self.inverse_batch(X1a, NNa, MAT, 4, (B[3], B[1], B[2]), Wt2, At2, Bt2)
                        self.ck(12)
                        self.dma(self.RMAT[s, d, j], MAT[:, :, :].rearrange("p h t -> p (h t)"), [MAT], [self.dd("RMAT", s, d, j)])
            self.run_interleaved([lambda: stream(0), lambda: stream(1)])
            self.barrier()

    def gdn_setup(self):
        self.GKQ = self.dram("GKQ", [S, 2, NJ, 128, 4 * 256], BF16)
        self.GMAT = self.dram("GMAT", [S, 2, NJ, 128, 4 * 512], BF16)
        self.GBC = self.dram("GBC", [S, 2, NJ, 128, 1024], BF16)
        self.GV = self.dram("GV", [S, NJ, 128, 512], BF16)
        self.GPC = self.dram("GPC", [S, 2, NJ, 128, 8], F32)
        self.YB = self.dram("YB", [2, NT, 512])
        self.SEL = self.sb([16, 16, 128], F32)
        self.selc = self.sb([128, 1], F32)
        self.onec = self.sb([128, 1], F32)
        nc = self.nc
        P = self.pool
        for i in range(16):
            self.cp(P, self.SEL[0:16, i, :], self.ident[0:16, i:i + 1].to_broadcast([16, 128]), [self.ident], [self.SEL])
        self.memset(P, self.onec[:, :], 1.0, [self.onec])
        self.memset(P, self.selc[:, :], 1.0, [self.selc])
        self.op(P, lambda: nc.gpsimd.affine_select(out=self.selc[:, :], in_=self.selc[:, :], pattern=[[0, 1]], compare_op=ALU.is_ge, fill=0.0,
                                                   base=-4, channel_multiplier=1), [self.selc], [self.selc])

    def gdn_prep(self, l):
        nc = self.nc
        V, G = self.dve, self.pool
        ps = self.ps
        with ExitStack() as es:
            def t(shape, dt=F32):
                return self.sb(shape, dt, es)
            PFl = self.PFt[l]

            def bc(ap, n):
                return ap.unsqueeze(2).to_broadcast([128, n, 128])
            negA = t([128, 1])
            self.actf(negA[:, :], self.pfv(l, "alog"), AF.Exp, [PFl], [negA])
            self.ts(V, negA[:, :], negA[:, :], -1.0, None, ALU.mult, None, [negA], [negA])
            DWg = t([128, 60, 128], BF16)
            gw_ = self.pfv(l, "gconv")
            for q_ in range(60):
                self.ts(V, DWg[:, q_, :], self.identb[:, :], gw_[:, q_:q_ + 1], None, ALU.mult, None, [self.identb, PFl], [DWg])
            def stream(s):
                B = self.ps[4 * s:4 * s + 4]
                qkv = t([128, 12, 132], BF16)
                cv = t([128, 12, 128])
                sq = t([128, 8, 128])
                kq = t([128, 4, 256])
                kqb = t([128, 4, 256], BF16)
                kb = t([128, 4, 128], BF16)
                vb = t([128, 4, 128], BF16)
                ab, x1, Xg, SIG, gcf, gcr = (t([16, 128]) for _ in range(6))
                X2, E2 = t([16, 256]), t([16, 256])
                TOTb, Etot = t([16, 128]), t([16, 2])
                TS = t([128, 80])
                negg, sB, sC, dd_ = t([128, 8]), t([128, 8]), t([128, 8]), t([128, 8])
                gm4 = t([128, 4, 256])
                KQd = t([128, 4, 256], BF16)
                BCd = [t([128, 1024], BF16) for _ in range(2)]
                Vt = t([128, 512], BF16)
                MAT = t([128, 4, 512], BF16)
                pc = t([128, 8])
                X1a = t([128, 4, 256], BF16)
                NNa = t([128, 4, 128], BF16)
                Wt = [t([128, 4, 128], BF16) for _ in range(2)]
                At = [t([128, 4, 128], BF16) for _ in range(2)]
                Bt = [t([128, 4, 128], BF16) for _ in range(2)]
                abv = self.PT[3968:3984, :]
                for j in range(NJ):
                    n0 = s * T + j * 128
                    self.load_halo(qkv, 15, 12, s, j, 2, Q=self.pool)
                    for c in range(12):
                        pb = B[c // 4]
                        for k in range(5):
                            self.mm(pb[:, (c % 4) * 128:(c % 4 + 1) * 128], DWg[:, k * 12 + c, :], qkv[:, c, k:k + 128], k == 0, k == 4, [DWg, qkv], [pb])
                    for b3 in range(3):
                        self.actf(cv[:, 4 * b3:4 * b3 + 4, :], B[b3][:, :].rearrange("p (c t) -> p c t", c=4), AF.Silu, [B[b3]], [cv])
                    self.tt(G, sq[:, :, :], cv[:, 0:8, :], cv[:, 0:8, :], ALU.mult, [cv], [sq])
                    for c in range(8):
                        pb = B[c // 4]
                        self.mm(pb[:, (c % 4) * 128:(c % 4 + 1) * 128], self.ones[:, :], sq[:, c, :], True, True, [self.ones, sq], [pb])
                    for hf in range(2):
                        self.ts(V, sq[:, hf * 4:(hf + 1) * 4, :], B[hf][:, :].rearrange("p (c t) -> p c t", c=4), EPS, None, ALU.add, None, [B[hf]], [sq])
                    self.actf(sq[:, :, :], sq[:, :, :], AF.Sqrt, [sq], [sq])
                    self.recip(sq[:, :, :], sq[:, :, :], [sq], [sq])
                    self.tt(V, kq[:, :, 0:128], cv[:, 4:8, :], sq[:, 4:8, :], ALU.mult, [cv, sq], [kq])
                    self.stt(V, kq[:, :, 128:256], cv[:, 0:4, :], 128.0 ** -0.5, sq[:, 0:4, :], ALU.mult, ALU.mult, [cv, sq], [kq])
                    self.cp(G, kqb[:, :, :], kq[:, :, :], [kq], [kqb])
                    self.cp(G, kb[:, :, :], kq[:, :, 0:128], [kq], [kb])
                    self.cp(G, vb[:, :, :], cv[:, 8:12, :], [cv], [vb])
                    pbv = B[2][:, :].bitcast(BF16)
                    for m in range(4):
                        self.tr(pbv[:, m * 128:(m + 1) * 128], vb[:, m, :], self.identb[:, :], [vb, self.identb], [B[2]])
                    self.cp(self.act, Vt[:, :], pbv[:, 0:512], [B[2]], [Vt])
                    self.dma(self.GV[s, j], Vt[:, :], [Vt], [self.dd("GV", s, j)])
                    pbk = B[3][:, :].bitcast(BF16)
                    for m in range(4):
                        self.tr(pbk[:, m * 128:(m + 1) * 128], kb[:, m, :], self.identb[:, :], [kb, self.identb], [B[3]])
                    self.dma(ab[0:16, :], abv[:, n0:n0 + 128], [], [ab])
                    self.actf(x1[0:16, :], ab[0:16, :], AF.Exp, [ab, PFl], [x1], bias=self.pfv(l, "dtb")[0:16, :], scale=1.0)
                    self.actf(x1[0:16, :], x1[0:16, :], AF.Ln, [x1, self.onec], [x1], bias=self.onec[0:16, :], scale=1.0)
                    self.ts(V, Xg[0:16, :], x1[0:16, :], negA[0:16, :], None, ALU.mult, None, [x1, negA], [Xg])
                    self.actf(SIG[0:16, :], ab[0:16, :], AF.Sigmoid, [ab], [SIG])
                    self.op(V, lambda: nc.vector.tensor_tensor_scan(out=gcf[0:16, :], data0=self.RM[0:16, :], data1=Xg[0:16, :], initial=0.0,
                                                                    op0=ALU.mult, op1=ALU.add), [self.RM, Xg], [gcf])
                    gcfv = gcf[0:16, :].rearrange("p (c t) -> p c t", t=64)
                    totb = gcfv[:, :, 63:64].to_broadcast([16, 2, 64])
                    self.cp(V, TOTb[0:16, :].rearrange("p (c t) -> p c t", t=64), totb, [gcf], [TOTb])
                    self.tt(V, gcr[0:16, :], TOTb[0:16, :], gcf[0:16, :], ALU.subtract, [TOTb, gcf], [gcr])
                    self.tt(V, X2[0:16, 128:256], gcr[0:16, :], Xg[0:16, :], ALU.add, [gcr, Xg], [X2])
                    self.tt(V, X2[0:16, 128:256], X2[0:16, 128:256], gcf[0:16, :], ALU.subtract, [X2, gcf], [X2])
                    self.stt(V, X2[0:16, 128:256], X2[0:16, 128:256], self.selc[0:16, :], gcf[0:16, :], ALU.mult, ALU.add, [X2, self.selc, gcf], [X2])
                    self.tt(V, X2[0:16, 0:128], X2[0:16, 128:256], Xg[0:16, :], ALU.subtract, [X2, Xg], [X2])
                    self.actf(E2[0:16, :], X2[0:16, :], AF.Exp, [X2], [E2])
                    self.actf(Etot[0:16, :], gcfv[:, :, 63], AF.Exp, [gcf], [Etot])
                    pT = B[2]
                    for q_, src in enumerate((Xg[0:16, :], SIG[0:16, :], X2[0:16, 0:128], X2[0:16, 128:256], TOTb[0:16, :])):
                        self.tr(pT[:, q_ * 16:(q_ + 1) * 16], src, self.ident[0:16, 0:16], [Xg, SIG, X2, TOTb, self.ident], [pT])
                    self.cp(V, TS[:, :], pT[:, 0:80], [pT], [TS])
                    self.actf(negg[:, :], TS[:, 0:8], AF.Exp, [TS], [negg], scale=-1.0)
                    self.tt(V, dd_[:, :], TS[:, 64:72], TS[:, 32:40], ALU.subtract, [TS], [dd_])
                    self.actf(sB[:, :], dd_[:, :], AF.Exp, [dd_], [sB])
                    self.tt(V, sB[:, :], sB[:, :], TS[:, 24:32], ALU.mult, [sB, TS], [sB])
                    self.tt(V, dd_[:, :], TS[:, 64:72], TS[:, 48:56], ALU.subtract, [TS], [dd_])
                    self.actf(sC[:, :], dd_[:, :], AF.Exp, [dd_], [sC])
                    self.tt(V, sC[:, :], sC[:, :], TS[:, 24:32], ALU.mult, [sC, TS], [sC])
                    pbk4 = pbk[:, 0:512].rearrange("p (h t) -> p h t", h=4)
                    for d in range(2):
                        self.tt(V, BCd[d][:, 0:512].rearrange("p (h t) -> p h t", h=4), pbk4, sB[:, d * 4:d * 4 + 4].unsqueeze(2).to_broadcast([128, 4, 128]), ALU.mult, [B[3], sB], [BCd[d]])
                        self.tt(V, BCd[d][:, 512:1024].rearrange("p (h t) -> p h t", h=4), pbk4, sC[:, d * 4:d * 4 + 4].unsqueeze(2).to_broadcast([128, 4, 128]), ALU.mult, [B[3], sC], [BCd[d]])
                    for d in range(2):
                        Ms = self.MU if d == 0 else self.ML
                        BC = BCd[d]
                        for h in range(4):
                            i = d * 4 + h
                            self.mm(B[h // 2][:, (h % 2) * 256:(h % 2 + 1) * 256], self.SEL[0:16, i, :], X2[0:16, :], True, True, [self.SEL, X2], [B[h // 2]])
                            self.mm(B[2 + h // 2][:, (h % 2) * 256:(h % 2 + 1) * 256], self.SEL[0:16, i, :], E2[0:16, :], True, True, [self.SEL, E2], [B[2 + h // 2]])
                        v2 = lambda p: p[:, :].rearrange("p (h t) -> p h t", h=2)
                        for q in range(2):
                            self.tt(V, gm4[:, 2 * q:2 * q + 2, :], v2(B[q]), TS[:, 32 + d * 4 + 2 * q:32 + d * 4 + 2 * q + 2].unsqueeze(2).to_broadcast([128, 2, 256]),
                                    ALU.subtract, [B[q], TS], [gm4])
                            self.tt(V, KQd[:, 2 * q:2 * q + 2, :], kq[:, 2 * q:2 * q + 2, :], v2(B[2 + q]), ALU.mult, [kq, B[2 + q]], [KQd])
                        for h in range(4):
                            self.mm(B[2][:, h * 2:(h + 1) * 2], self.SEL[0:16, d * 4 + h, :], Etot[0:16, :], True, True, [self.SEL, Etot], [B[2]])
                        self.cp(self.act, pc[:, :], B[2][:, 0:8], [B[2]], [pc])
                        self.ts(G, gm4[:, :, :], gm4[:, :, :], 0.0, None, ALU.min, None, [gm4], [gm4])
                        self.actf(gm4[:, :, :], gm4[:, :, :], AF.Exp, [gm4], [gm4])
                        self.tt(G, gm4[:, :, :], gm4[:, :, :], Ms[:, :].unsqueeze(1).to_broadcast([128, 4, 256]), ALU.mult, [gm4, Ms], [gm4])
                        for h in range(4):
                            self.mm(B[h // 2][:, (h % 2) * 256:(h % 2 + 1) * 256], kb[:, h, :], kqb[:, h, :], True, True, [kb, kqb], [B[h // 2]])
                        for q in range(2):
                            self.tt(V, gm4[:, 2 * q:2 * q + 2, :], gm4[:, 2 * q:2 * q + 2, :], v2(B[q]), ALU.mult, [gm4, B[q]], [gm4])
                        self.tt(V, X1a[:, :, :], gm4[:, :, :], TS[:, 24 + d * 4:28 + d * 4].unsqueeze(2).to_broadcast([128, 4, 256]), ALU.mult, [gm4, TS], [X1a])
                        self.tt(V, MAT[:, :, 256:512], X1a[:, :, :], negg[:, d * 4:d * 4 + 4].unsqueeze(2).to_broadcast([128, 4, 256]), ALU.mult, [X1a, negg], [MAT])
                        self.cp(G, MAT[:, :, 128:256], X1a[:, :, 128:256], [X1a], [MAT])
                        pN = B[3][:, :].bitcast(BF16)
                        for h in range(4):
                            self.tr(pN[:, h * 128:(h + 1) * 128], X1a[:, h, 0:128], self.identb[:, :], [X1a, self.identb], [B[3]])
                        self.cp(self.act, NNa[:, :, :], pN[:, 0:512].rearrange("p (h t) -> p h t", h=4), [B[3]], [NNa])
                        self.inverse_batch(X1a, NNa, MAT, 0, (B[0], B[1], B[2]), Wt, At, Bt)
                        self.dma(self.GKQ[s, d, j], KQd[:, :, :].rearrange("p m t -> p (m t)"), [KQd], [self.dd("GKQ", s, d, j)])
                        self.dma(self.GMAT[s, d, j], MAT[:, :, :].rearrange("p h t -> p (h t)"), [MAT], [self.dd("GMAT", s, d, j)])
                        self.dma(self.GBC[s, d, j], BC[:, :], [BC], [self.dd("GBC", s, d, j)])
                        self.dma(self.GPC[s, d, j], pc[:, :], [pc], [self.dd("GPC", s, d, j)])
            self.run_interleaved([lambda: stream(0), lambda: stream(1)])
            self.barrier()

    def conf_setup(self):
        self.RCs = self.dram("RCs", [512, NT], BF16)
        self.RAs = self.dram("RAs", [512, NT], BF16)
        self.RBs = self.dram("RBs", [512, NT], BF16)

    def conformer(self, l):
        V, G = self.dve, self.pool
        ps = self.ps
        PFl = self.PFt[l]
        valv = self.PT[3984:3984 + 512, :].rearrange("(c p) n -> p c n", p=128)
        gatv = self.PT[4496:4496 + 512, :].rearrange("(c p) n -> p c n", p=128)
        RCv = self.RCs.rearrange("(c p) n -> p c n", p=128)
        with ExitStack() as es:
            def t(shape, dt=F32):
                return self.sb(shape, dt, es)
            DW = t([128, 124, 128], BF16)
            cw = self.pfv(l, "cdw")
            for q in range(124):
                self.ts(V, DW[:, q, :], self.identb[:, :], cw[:, q:q + 1], None, ALU.mult, None, [self.identb, PFl], [DW])
            upW = t([128, 2, 32, 94], BF16)
            upH = t([128, 2, 62, 64], BF16)
            upC = t([128, 4, 286], BF16)
            self.memset(G, upW[:, :, :, :], 0.0, [upW])
            self.memset(G, upH[:, :, :, :], 0.0, [upH])
            self.memset(G, upC[:, :, :], 0.0, [upC])
            vl = [t([128, TL]) for _ in range(2)]
            gl = [t([128, TL]) for _ in range(2)]
            o = t([128, 4, TL])
            sq = t([128, 4, 512])
            mu, rs, var = t([128, 512]), t([128, 512]), t([128, 512])
            ob = t([128, 4, 512], BF16)
            nb = 0
            for s in range(S):
                for (seg0, W_) in ((0, TC), (TC, TL)):
                    if seg0 == 0 and l == L - 1:
                        continue
                    n0 = s * T + seg0
                    for c in range(4):
                        v_, g_ = vl[c % 2], gl[c % 2]
                        self.dma(v_[:, :W_], valv[:, c, n0:n0 + W_], [], [v_])
                        self.dma(g_[:, :W_], gatv[:, c, n0:n0 + W_], [], [g_])
                        self.actf(g_[:, :W_], g_[:, :W_], AF.Sigmoid, [g_], [g_])
                        if seg0 == 0:
                            self.tt(V, upC[:, c, 15:15 + W_], v_[:, :W_], g_[:, :W_], ALU.mult, [v_, g_], [upC])
                        elif c < 2:
                            self.tt(V, upW[:, c, :, 15:79], v_[:, :].rearrange("p (r w) -> p r w", w=64), g_[:, :].rearrange("p (r w) -> p r w", w=64),
                                    ALU.mult, [v_, g_], [upW])
                        else:
                            self.tt(V, upH[:, c - 2, 15:47, :], v_[:, :].rearrange("p (r w) -> p r w", w=64), g_[:, :].rearrange("p (r w) -> p r w", w=64),
                                    ALU.mult, [v_, g_], [upH])
                    for c in range(4):
                        if seg0 == 0:
                            pb = ps[nb % 4]; nb += 1
                            for k in range(31):
                                self.mm(pb[:, 0:W_], DW[:, k * 4 + c, :], upC[:, c, k:k + W_], k == 0, k == 30, [DW, upC], [pb])
                            self.actf(o[:, c, 0:W_], pb[:, 0:W_], AF.Identity, [pb, PFl], [o], bias=self.pfv(l, "cdwb", c), scale=1.0)
                        else:
                            for rb_ in range(4):
                                pb = ps[nb % 4]; nb += 1
                                for k in range(31):
                                    if c < 2:
                                        rhs = upW[:, c, rb_ * 8:(rb_ + 1) * 8, k:k + 64]
                                        outp = pb[:, :].rearrange("p (r w) -> p r w", w=64)
                                    else:
                                        rhs = upH[:, c - 2, rb_ * 8 + k:rb_ * 8 + k + 8, :].rearrange("p r w -> p (r w)")
                                        outp = pb[:, :]
                                    self.mm(outp, DW[:, k * 4 + c, :], rhs, k == 0, k == 30, [DW, upW if c < 2 else upH], [pb])
                                self.actf(o[:, c, rb_ * 512:(rb_ + 1) * 512], pb[:, :], AF.Identity, [pb, PFl], [o], bias=self.pfv(l, "cdwb", c), scale=1.0)
                    for t0 in range(0, W_, 512):
                        w = min(512, W_ - t0)
                        self.tt(G, sq[:, :, :w], o[:, :, t0:t0 + w], o[:, :, t0:t0 + w], ALU.mult, [o], [sq])
                        for c in range(4):
                            self.mm(ps[4][:, :w], self.ones[:, :], o[:, c, t0:t0 + w], c == 0, c == 3, [self.ones, o], [ps[4]])
                        for c in range(4):
                            self.mm(ps[5][:, :w], self.ones[:, :], sq[:, c, :w], c == 0, c == 3, [self.ones, sq], [ps[5]])
                        self.ts(V, mu[:, :w], ps[4][:, :w], 1.0 / 512, None, ALU.mult, None, [ps[4]], [mu])
                        self.tt(V, var[:, :w], mu[:, :w], mu[:, :w], ALU.mult, [mu], [var])
                        self.stt(V, var[:, :w], ps[5][:, :w], 1.0 / 512, var[:, :w], ALU.mult, ALU.subtract, [ps[5], var], [var])
                        self.ts(V, var[:, :w], var[:, :w], EPS, None, ALU.add, None, [var], [var])
                        self.actf(var[:, :w], var[:, :w], AF.Sqrt, [var], [var])
                        self.recip(rs[:, :w], var[:, :w], [var], [rs])
                        for c in range(4):
                            self.tt(V, sq[:, c, :w], o[:, c, t0:t0 + w], mu[:, :w], ALU.subtract, [o, mu], [sq])
                            self.tt(G, sq[:, c, :w], sq[:, c, :w], rs[:, :w], ALU.mult, [sq, rs], [sq])
                            self.ts(V, sq[:, c, :w], sq[:, c, :w], self.pfv(l, "clng", c), self.pfv(l, "clnb", c), ALU.mult, ALU.add, [sq, PFl], [sq])
                        self.actf(ob[:, :, :w], sq[:, :, :w], AF.Silu, [sq], [ob])
                        self.dma(RCv[:, :, n0 + t0:n0 + t0 + w], ob[:, :, :w], [ob], [self.dd("RCs", (n0 + t0) // 128)])
            self.barrier()

    def red(self, E, out, in_, op, R, W):
        self.op(E, lambda: E.be.tensor_reduce(out=out, in_=in_, axis=AX.X, op=op), R, W)

    def merge(self, l):
        V, G = self.dve, self.pool
        ps = self.ps
        PFl = self.PFt[l]
        with ExitStack() as es:
            def t(shape, dt=F32):
                return self.sb(shape, dt, es)
            wbr = t([128, 12, 1024], BF16)
            wo = t([128, 8, 1024], BF16)
            for n in range(3):
                self.dma(wbr[:, n * 4:(n + 1) * 4, :], self.w_branch[l, n].rearrange("(k p) n -> p k n", p=128), [], [wbr], Q=self.pool)
            self.dma(wo[:, :, :], self.w_out[l].rearrange("(k p) n -> p k n", p=128), [], [wo], Q=self.pool)
            pgv = [self.PT[5008 + n * 1024:5008 + (n + 1) * 1024, :].rearrange("(c p) n -> p c n", p=128) for n in range(3)]
            fm4 = lambda A: A.rearrange("(c p) n -> p c n", p=128)
            def stream(s):
                B = self.ps[4 * s:4 * s + 4]
                y0, y1, ysq = t([128, 512]), t([128, 512]), t([128, 512])
                m1, m2, m3 = t([128, 8]), t([128, 8]), t([128, 8])
                raf, bon, ga, zt = (t([128, 4, 128]) for _ in range(4))
                Rb = [t([128, 4, 128], BF16) for _ in range(3)]
                pg = t([128, 8, 128])
                macc, tmpm = t([128, 8, 128]), t([128, 8, 128])
                mb = t([128, 8, 128], BF16)
                xt = t([128, 8, 128])
                for j in range(NJ):
                    if j < 2 and l == L - 1:
                        continue
                    n0 = s * T + j * 128
                    r = 2 if j < 2 else s
                    for br in range(2):
                        DY, nm, nh, dvv, eps_ = ((self.YA, "R", 8, 64, 64e-5), (self.YB, "G", 4, 128, EPS))[br]
                        self.dma(y0[:, :], DY[0, n0:n0 + 128, :], [self.dd(nm + "Y", 0, n0 // 128)], [y0])
                        self.dma(y1[:, :], DY[1, n0:n0 + 128, :], [self.dd(nm + "Y", 1, n0 // 128)], [y1])
                        self.tt(V, y0[:, :], y0[:, :], y1[:, :], ALU.add, [y0, y1], [y0])
                        yv = y0[:, :].rearrange("p (h d) -> p h d", d=dvv)
                        self.tt(G, ysq[:, :], y0[:, :], y0[:, :], ALU.mult, [y0], [ysq])
                        self.red(V, m2[:, 0:nh], ysq[:, :].rearrange("p (h d) -> p h d", d=dvv), ALU.add, [ysq], [m2])
                        if br == 0:
                            self.red(V, m1[:, 0:nh], yv, ALU.add, [y0], [m1])
                            self.ts(V, m1[:, 0:nh], m1[:, 0:nh], 1.0 / dvv, None, ALU.mult, None, [m1], [m1])
                            self.tt(V, m3[:, 0:nh], m1[:, 0:nh], m1[:, 0:nh], ALU.mult, [m1], [m3])
                            self.stt(V, m2[:, 0:nh], m2[:, 0:nh], 1.0 / dvv, m3[:, 0:nh], ALU.mult, ALU.subtract, [m2, m3], [m2])
                            self.ts(V, m2[:, 0:nh], m2[:, 0:nh], eps_, None, ALU.add, None, [m2], [m2])
                            self.tt(V, yv, yv, m1[:, 0:nh].unsqueeze(2).to_broadcast([128, nh, dvv]), ALU.subtract, [y0, m1], [y0])
                        else:
                            self.ts(V, m2[:, 0:nh], m2[:, 0:nh], 1.0 / dvv, eps_, ALU.mult, ALU.add, [m2], [m2])
                        self.actf(m2[:, 0:nh], m2[:, 0:nh], AF.Sqrt, [m2], [m2])
                        self.recip(m2[:, 0:nh], m2[:, 0:nh], [m2], [m2])
                        self.tt(V, yv, yv, m2[:, 0:nh].unsqueeze(2).to_broadcast([128, nh, dvv]), ALU.mult, [y0, m2], [y0])
                        pb = B[br]
                        for c in range(4):
                            self.tr(pb[:, c * 128:(c + 1) * 128], y0[:, c * 128:(c + 1) * 128], self.ident[:, :], [y0, self.ident], [pb])
                        pbv = pb[:, :].rearrange("p (c t) -> p c t", c=4)
                        if br == 0:
                            for c in range(4):
                                self.ts(V, raf[:, c, :], pbv[:, c, :], self.pfv(l, "ln_g", c), self.pfv(l, "ln_b", c), ALU.mult, ALU.add, [pb, PFl], [raf])
                            self.dma(bon[:, :, :], fm4(self.BON)[:, :, n0:n0 + 128], [self.dd("BON", n0 // 128)], [bon])
                            self.dma(ga[:, :, :], fm4(self.GAs)[:, :, n0:n0 + 128], [self.dd("GAs", n0 // 128)], [ga])
                            self.tt(V, raf[:, :, :], raf[:, :, :], bon[:, :, :], ALU.add, [raf, bon], [raf])
                            self.tt(V, Rb[0][:, :, :], raf[:, :, :], ga[:, :, :], ALU.mult, [raf, ga], [Rb[0]])
                        else:
                            self.dma(zt[:, :, :], self.PTv[:, 27:31, n0:n0 + 128], [], [zt])
                            self.actf(zt[:, :, :], zt[:, :, :], AF.Silu, [zt], [zt])
                            self.stt(V, Rb[1][:, :, :], pbv, self.pfv(l, "gnorm", 0), zt[:, :, :], ALU.mult, ALU.mult, [pb, PFl, zt], [Rb[1]])
                    self.dma(Rb[2][:, :, :], fm4(self.RCs)[:, :, n0:n0 + 128], [self.dd("RCs", n0 // 128)], [Rb[2]])
                    for n in range(3):
                        self.dma(pg[:, :, :], pgv[n][:, :, n0:n0 + 128], [], [pg])
                        self.actf(pg[:, :, :], pg[:, :, :], AF.Sigmoid, [pg], [pg])
                        for m in range(8):
                            pb = B[2 + m // 4]
                            for k in range(4):
                                self.mm(pb[:, (m % 4) * 128:(m % 4 + 1) * 128], wbr[:, n * 4 + k, m * 128:(m + 1) * 128], Rb[n][:, k, :], k == 0, k == 3,
                                        [wbr, Rb[n]], [pb])
                        for hf in range(2):
                            pbv = B[2 + hf][:, :].rearrange("p (c t) -> p c t", c=4)
                            dst = macc if n == 0 else tmpm
                            self.tt(V, dst[:, hf * 4:(hf + 1) * 4, :], pbv, pg[:, hf * 4:(hf + 1) * 4, :], ALU.mult, [B[2 + hf], pg], [dst])
                        if n > 0:
                            self.tt(G, macc[:, :, :], macc[:, :, :], tmpm[:, :, :], ALU.add, [macc, tmpm], [macc])
                    self.cp(G, mb[:, :, :], macc[:, :, :], [macc], [mb])
                    self.dma(xt[:, :, :], self.XTv[:, :, n0:n0 + 128], self.dr("XT", n0, 128), [xt])
                    for m in range(8):
                        pb = B[m // 4]
                        for k in range(8):
                            self.mm(pb[:, (m % 4) * 128:(m % 4 + 1) * 128], wo[:, k, m * 128:(m + 1) * 128], mb[:, k, :], k == 0, k == 7, [wo, mb], [pb])
                    for m in range(8):
                        pb = B[m // 4]
                        self.stt(V, xt[:, m, :], pb[:, (m % 4) * 128:(m % 4 + 1) * 128], self.MOD[l][:, 16 + m, r:r + 1], xt[:, m, :], ALU.mult, ALU.add,
                                 [pb, self.MOD[l], xt], [xt])
                    self.dma(self.XTv[:, :, n0:n0 + 128], xt[:, :, :], [xt], self.dr("XT", n0, 128))
                    if f"XM{l}" in self.dbg:
                        pass
            self.run_interleaved([lambda: stream(0), lambda: stream(1)])
            self.barrier()

    def moe(self, l):
        V, G = self.dve, self.pool
        ps = self.ps
        with ExitStack() as es:
            def t(shape, dt=F32, e_=None):
                return self.sb(shape, dt, e_ or es)
            hT = t([128, 8, NT], BF16)
            WTf = t([16, NT])
            wr = t([128, 8, 16])
            rb = t([128, 16])
            self.dma(wr[:, :, :], self.w_router.rearrange("(k p) e -> p k e", p=128), [], [wr])
            self.dma(rb[:, :], self.rbias, [], [rb])
            with ExitStack() as es1:
                xts = [t([128, 8, 512], F32, es1) for _ in range(2)]
                sq = t([128, 8, 512], F32, es1)
                rs = t([128, 512], F32, es1)
                hf = t([128, 8, 512], F32, es1)
                sc, sel, sel2, eq, cm, wts = (t([128, 16], F32, es1) for _ in range(6))
                m1, m2, gs, gsel = (t([128, 4], F32, es1) for _ in range(4))
                gmx, wsum = t([128, 1], F32, es1), t([128, 1], F32, es1)
                v4 = lambda a: a[:, :].rearrange("p (g j) -> p g j", j=4)
                b4 = lambda a: a[:, :].unsqueeze(2).to_broadcast([128, 4, 4])
                for i, (n0, w, r) in enumerate(self.tiles()):
                    xt, _ = self.modulate_tile((xts[i % 2], sq, rs, None), n0, w, r, None, None, None, None)
                    for c in range(8):
                        self.stt(V, hf[:, c, :w], xt[:, c, :w], self.GF[l][:, c, r:r + 1], rs[:, :w], ALU.mult, ALU.mult, [xt, rs, self.GF[l]], [hf])
                        self.actf(hf[:, c, :w], hf[:, c, :w], AF.Identity, [hf, self.MOD[l]], [hf], bias=self.MOD[l][:, 24 + c, r:r + 1], scale=1.0)
                    self.cp(G, hT[:, :, n0:n0 + w], hf[:, :, :w], [hf], [hT])
                    for q in range(w // 128):
                        pR = ps[6]
                        for c in range(8):
                            self.mm(pR[:, 0:16], hf[:, c, q * 128:(q + 1) * 128], wr[:, c, :], c == 0, c == 7, [hf, wr], [pR])
                        self.actf(sc[:, :], pR[:, 0:16], AF.Sigmoid, [pR], [sc])
                        self.tt(V, sel[:, :], sc[:, :], rb[:, :], ALU.add, [sc, rb], [sel])
                        self.red(V, m1[:, :], v4(sel), ALU.max, [sel], [m1])
                        self.tt(V, v4(eq), v4(sel), b4(m1), ALU.is_equal, [sel, m1], [eq])
                        self.stt(V, sel2[:, :], eq[:, :], -1e9, sel[:, :], ALU.mult, ALU.add, [eq, sel], [sel2])
                        self.red(V, m2[:, :], v4(sel2), ALU.max, [sel2], [m2])
                        self.tt(V, gs[:, :], m1[:, :], m2[:, :], ALU.add, [m1, m2], [gs])
                        self.red(V, gmx[:, :], gs[:, :], ALU.max, [gs], [gmx])
                        self.ts(V, gsel[:, :], gs[:, :], gmx[:, 0:1], None, ALU.is_equal, None, [gs, gmx], [gsel])
                        self.tt(V, v4(cm), v4(sel), b4(m2), ALU.is_ge, [sel, m2], [cm])
                        self.tt(V, v4(cm), v4(cm), b4(gsel), ALU.mult, [cm, gsel], [cm])
                        self.tt(V, wts[:, :], sc[:, :], cm[:, :], ALU.mult, [sc, cm], [wts])
                        self.red(V, wsum[:, :], wts[:, :], ALU.add, [wts], [wsum])
                        self.recip(wsum[:, :], wsum[:, :], [wsum], [wsum])
                        self.ts(V, wts[:, :], wts[:, :], wsum[:, 0:1], None, ALU.mult, None, [wts, wsum], [wts])
                        pT = ps[7]
                        self.tr(pT[0:16, 0:128], wts[:, :], self.ident[:, :], [wts, self.ident], [pT])
                        self.cp(self.act, WTf[0:16, n0 + q * 128:n0 + (q + 1) * 128], pT[0:16, 0:128], [pT], [WTf])
                self.barrier()
            TG = 1152
            TW = 384
            yacc = t([128, 8, TG])
            wgb = [t([128, 8, 512], BF16) for _ in range(2)]
            wub = [t([128, 8, 512], BF16) for _ in range(2)]
            wdb = [t([128, 4, 1024], BF16) for _ in range(2)]
            wtb = t([128, TW])
            sg = [t([128, TW]) for _ in range(2)]
            actb = t([128, 4, TW], BF16)
            xt2 = t([128, 8, 128])
            def loadw(e):
                for (src, dstt) in ((self.weg[l, e], wgb[e % 2]), (self.weu[l, e], wub[e % 2]), (self.wed[l, e], wdb[e % 2])):
                    self.dma(dstt[:, :, :], src.rearrange("(k p) n -> p k n", p=128), [], [dstt], Q=self.pool)
            loadw(0)
            for g in range(NT // TG):
                for e in range(16):
                    gb, ub, db = wgb[e % 2], wub[e % 2], wdb[e % 2]
                    if not (g == NT // TG - 1 and e == 15):
                        loadw((e + 1) % 16)
                    for tt_ in range(TG // TW):
                        n0 = g * TG + tt_ * TW
                        self.mm(ps[4][:, :TW], self.SEL[0:16, e, :], WTf[0:16, n0:n0 + TW], True, True, [self.SEL, WTf], [ps[4]])
                        self.cp(self.act, wtb[:, :], ps[4][:, :TW], [ps[4]], [wtb])
                        for hc in range(4):
                            pg_, pu_ = ps[(hc % 2) * 2], ps[(hc % 2) * 2 + 1]
                            for k in range(8):
                                self.mm(pg_[:, :TW], gb[:, k, hc * 128:(hc + 1) * 128], hT[:, k, n0:n0 + TW], k == 0, k == 7, [gb, hT], [pg_])
                            for k in range(8):
                                self.mm(pu_[:, :TW], ub[:, k, hc * 128:(hc + 1) * 128], hT[:, k, n0:n0 + TW], k == 0, k == 7, [ub, hT], [pu_])
                            sg_ = sg[hc % 2]
                            self.actf(sg_[:, :], pg_[:, :TW], AF.Silu, [pg_], [sg_])
                            self.tt(V, sg_[:, :], sg_[:, :], pu_[:, :TW], ALU.mult, [sg_, pu_], [sg_])
                            self.tt(G, actb[:, hc, :], sg_[:, :], wtb[:, :], ALU.mult, [sg_, wtb], [actb])
                        for m in range(8):
                            pd = ps[4 + m % 4]
                            for hc in range(4):
                                self.mm(pd[:, :TW], db[:, hc, m * 128:(m + 1) * 128], actb[:, hc, :], hc == 0, hc == 3, [db, actb], [pd])
                            dst = yacc[:, m, tt_ * TW:(tt_ + 1) * TW]
                            if e == 0:
                                self.cp(self.act, dst, pd[:, :TW], [pd], [yacc])
                            else:
                                self.tt(V, dst, dst, pd[:, :TW], ALU.add, [yacc, pd], [yacc])
                for p_ in range(TG // 128):
                    n0 = g * TG + p_ * 128
                    tq = n0 % T
                    r = 2 if tq < TC else n0 // T
                    self.dma(xt2[:, :, :], self.XTv[:, :, n0:n0 + 128], self.dr("XT", n0, 128), [xt2])
                    for m in range(8):
                        self.stt(V, xt2[:, m, :], yacc[:, m, p_ * 128:(p_ + 1) * 128], self.MOD[l][:, 40 + m, r:r + 1], xt2[:, m, :], ALU.mult, ALU.add,
                                 [yacc, self.MOD[l], xt2], [xt2])
                    self.dma(self.XTv[:, :, n0:n0 + 128], xt2[:, :, :], [xt2], self.dr("XT", n0, 128))
            self.barrier()

    def final(self):
        V, G = self.dve, self.pool
        ps = self.ps
        with ExitStack() as es:
            def t(shape, dt=F32):
                return self.sb(shape, dt, es)
            xts = [t([128, 8, 128]) for _ in range(2)]
            sq = t([128, 8, 128])
            rs = t([128, 128])
            tmp = t([128, 8, 128])
            os_ = [t([128, 1024]) for _ in range(2)]
            i = 0
            for s in range(S):
                for j in range(2, NJ):
                    n0 = s * T + j * 128
                    xt = xts[i % 2]; o = os_[i % 2]; i += 1
                    self.dma(xt[:, :, :], self.XTv[:, :, n0:n0 + 128], self.dr("XT", n0, 128), [xt])
                    self.actf(sq[:, :, :], xt[:, :, :], AF.Square, [xt], [sq])
                    for c in range(8):
                        self.mm(ps[0][:, 0:128], self.ones[:, :], sq[:, c, :], c == 0, c == 7, [self.ones, sq], [ps[0]])
                    self.ts(V, rs[:, :], ps[0][:, 0:128], 1.0 / D, EPS, ALU.mult, ALU.add, [ps[0]], [rs])
                    self.actf(rs[:, :], rs[:, :], AF.Sqrt, [rs], [rs])
                    self.recip(rs[:, :], rs[:, :], [rs], [rs])
                    for c in range(8):
                        self.stt(V, tmp[:, c, :], xt[:, c, :], self.pfv(0, "norm_final", c), rs[:, :], ALU.mult, ALU.mult, [xt, rs, self.PFt[0]], [tmp])
                    for c in range(8):
                        pb = ps[1 + c // 4]
                        self.tr(pb[:, (c % 4) * 128:(c % 4 + 1) * 128], tmp[:, c, :], self.ident[:, :], [tmp, self.ident], [pb])
                    self.cp(self.act, o[:, 0:512], ps[1][:, :], [ps[1]], [o])
                    self.cp(V, o[:, 512:1024], ps[2][:, :], [ps[2]], [o])
                    self.dma(self.y[s, (j - 2) * 128:(j - 1) * 128, :], o[:, :], [o], [self.dd("y", s, j)])
            self.barrier()

    def rwkv_scan(self, l, gdn=False):
        V, G = self.dve, self.pool
        ps = self.ps
        nh = 4 if gdn else 8
        dv = 512 // nh
        DKQ, DMAT, DBC, DV_, DPC, DY, nm = ((self.GKQ, self.GMAT, self.GBC, self.GV, self.GPC, self.YB, 'G') if gdn else (self.RKQ, self.RMAT, self.RBC, self.RV, self.RPC, self.YA, 'R'))
        with ExitStack() as es:
            def t(shape, dt=F32):
                return self.sb(shape, dt, es)
            chains = [(s, d) for s in range(S) for d in range(2)]
            order = {0: list(range(NJ)), 1: [1, 0] + list(range(NJ - 1, 1, -1))}
            bufs = []
            for _ in chains:
                ld = [(t([128, 4, 256], BF16), t([128, nh, 512], BF16), t([128, 1024], BF16), t([128, 512], BF16), t([128, 8])) for _ in range(2)]
                bufs.append(dict(ld=ld, H=t([128, 4, dv]), Hz=t([128, nh, dv], BF16), Rn=t([128, nh, dv], BF16),
                                 Ubz=[t([128, nh, dv], BF16) for _ in range(2)], Vz=[t([128, 512], BF16) for _ in range(2)],
                                 Yt=t([128, 512])))
            def chain(ci, s, d):
                for i in range(NJ):
                    j = order[d][i]
                    n0 = s * T + j * 128
                    b = bufs[ci]
                    KQ, MAT, BC, Vt, pc = b["ld"][i % 2]
                    H, Hz, Rn, Ubz, Vz, Yt = b["H"], b["Hz"], b["Rn"], b["Ubz"], b["Vz"], b["Yt"]
                    self.dma(KQ[:, :, :].rearrange("p m t -> p (m t)"), DKQ[s, d, j], [self.dd(nm + "KQ", s, d, j)], [KQ])
                    self.dma(MAT[:, :, :].rearrange("p h t -> p (h t)"), DMAT[s, d, j], [self.dd(nm + "MAT", s, d, j)], [MAT])
                    self.dma(BC[:, :], DBC[s, d, j], [self.dd(nm + "BC", s, d, j)], [BC])
                    self.dma(Vt[:, :], DV_[s, j], [self.dd(nm + "V", s, j)], [Vt])
                    self.dma(pc[:, :], DPC[s, d, j], [self.dd(nm + "PC", s, d, j)], [pc])
                    if i == 0:
                        self.memset(G, H[:, :, :], 0.0, [H])
                        self.memset(G, Hz[:, :, :], 0.0, [Hz])
                        self.memset(G, Rn[:, :, :], 0.0, [Rn])
                        for c in range(2):
                            self.memset(G, Ubz[c][:, :, :], 0.0, [Ubz[c]])
                            self.memset(G, Vz[c][:, :], 0.0, [Vz[c]])
                    for c in range(2):
                        self.cp(G, Vz[c][c * 64:c * 64 + 64, :], Vt[c * 64:c * 64 + 64, :], [Vt], [Vz[c]])
                    pA, pB = ps[2 * ci], ps[2 * ci + 1]
                    pAv = pA[:, :].rearrange("p (h v) -> p h v", v=dv)
                    pBv = pB[:, :].rearrange("p (h v) -> p h v", v=dv)
                    if not gdn:
                        pBe = pB[:, :].rearrange("p (m e v) -> p m e v", e=2, v=64)
                        Hze = Hz[:, :, :].rearrange("p (m e) v -> p m e v", e=2)
                    pcv = pc[:, :].rearrange("p (m c) -> p m c", c=2)
                    for c in ([0, 1] if d == 0 else [1, 0]):
                        cs = slice(c * 64, c * 64 + 64)
                        Ub = Ubz[c]
                        for h in range(nh):
                            m = h if gdn else h // 2
                            self.mm(pAv[:, h, :], KQ[:, m, 0:128], Hz[:, h, :], True, False, [KQ, Hz], [pA])
                            self.mm(pAv[:, h, :], MAT[:, h, 256:384], Vt[:, h * dv:(h + 1) * dv], False, True, [MAT, Vt], [pA])
                        self.ts(V, Rn[cs, :, :], pAv[cs, :, :], -1.0, None, ALU.mult, None, [pA], [Rn])
                        for h in range(nh):
                            self.mm(pBv[:, h, :], MAT[:, h, 0:128], Rn[:, h, :], True, True, [MAT, Rn], [pB])
                        self.cp(self.act, Ub[cs, :, :], pBv[cs, :, :], [pB], [Ub])
                        for h in range(nh):
                            m = h if gdn else h // 2
                            self.mm(pAv[:, h, :], KQ[:, m, 128:256], Hz[:, h, :], True, False, [KQ, Hz], [pA])
                            self.mm(pAv[:, h, :], MAT[:, h, 128:256], Ub[:, h, :], False, False, [MAT, Ub], [pA])
                            self.mm(pAv[:, h, :], MAT[:, h, 384:512], Vt[:, h * dv:(h + 1) * dv], False, True, [MAT, Vt], [pA])
                        self.cp(V, Yt[cs, :], pA[cs, :], [pA], [Yt])
                        for h in range(nh):
                            m = h if gdn else h // 2
                            self.mm(pBv[:, h, :], BC[:, m * 128:(m + 1) * 128], Ub[:, h, :], True, False, [BC, Ub], [pB])
                            self.mm(pBv[:, h, :], BC[:, 512 + m * 128:512 + (m + 1) * 128], Vz[c][:, h * dv:(h + 1) * dv], False, True, [BC, Vz[c]], [pB])
                        if gdn:
                            self.tt(V, H[:, :, :], H[:, :, :], pcv[:, :, c:c + 1].to_broadcast([128, 4, dv]), ALU.mult, [H, pc], [H])
                            self.tt(V, H[:, :, :], H[:, :, :], pBv[:, :, :], ALU.add, [H, pB], [H])
                            self.cp(self.act, Hz[:, :, :], H[:, :, :], [H], [Hz])
                        else:
                            for e in range(2):
                                rows = slice(e * 64, e * 64 + 64)
                                self.tt(V, H[rows, :, :], H[rows, :, :], pBe[rows, :, e, :], ALU.add, [H, pB], [H])
                            self.tt(V, H[:, :, :], H[:, :, :], pcv[:, :, c:c + 1].to_broadcast([128, 4, 64]), ALU.mult, [H, pc], [H])
                            for e in range(2):
                                rows = slice(e * 64, e * 64 + 64)
                                self.cp(self.act, Hze[rows, :, e, :], H[rows, :, :], [H], [Hz])
                    self.dma(DY[d, n0:n0 + 128, :], Yt[:, :], [Yt], [self.dd(nm + "Y", d, n0 // 128)])
            self.run_interleaved([(lambda ci=ci, s=s, d=d: chain(ci, s, d)) for ci, (s, d) in enumerate(chains)])
            self.barrier()

    def build(self):
        try:
            self.build_()
        except StopBuild:
            self.es2 = None
            self.finish()

    def build_(self):
        self.consts()
        self.gdn_setup()
        self.conf_setup()
        self.phase0()
        if self.stop == "0":
            return self.finish()
        for l in range(self.nlayers):
            self.phaseA(l)
            if self.stop == f"A{l}":
                return self.finish()
            self.phaseB(l)
            if self.stop == f"B{l}":
                return self.finish()
            self.rwkv_prep(l)
            if self.stop == f"C{l}":
                return self.finish()
            self.rwkv_scan(l)
            if self.stop == f"D{l}":
                return self.finish()
            self.gdn_prep(l)
            if self.stop == f"E{l}":
                return self.finish()
            self.rwkv_scan(l, gdn=True)
            if self.stop == f"F{l}":
                return self.finish()
            self.conformer(l)
            if self.stop == f"G{l}":
                return self.finish()
            self.merge(l)
            if self.stop == f"H{l}":
                return self.finish()
            self.moe(l)
            if self.stop == f"I{l}":
                return self.finish()
        self.final()
        self.finish()
```

```python
import threading
import numpy as np
from contextlib import ExitStack
import concourse.bass as bass
import concourse.mybir as mybir
from concourse.bass_utils import run_bass_kernel_spmd

F32 = mybir.dt.float32
BF16 = mybir.dt.bfloat16
ALU = mybir.AluOpType
AF = mybir.ActivationFunctionType
AX = mybir.AxisListType

D = 1024
S = 2
TC = 256
TL = 2048
T = TC + TL
NT = S * T
L = 2
NIN = 8080
NDS = 24
NDS_SW = 8


class Dep:
    __slots__ = ("w", "r")

    def __init__(self):
        self.w = None
        self.r = {}


class Tl:
    def __init__(self, t):
        self.t = t
        self.d = Dep()

    def __getitem__(self, k):
        return self.t[k]


class Eng:
    def __init__(self, name, be, sem):
        self.key = name
        self.be = be
        self.sem = sem
        self.n = 0
        self.waited = {}


def _d(x):
    return x.d if hasattr(x, "d") else x


class KB:
    def __init__(self, dbg=()):
        self.nc = nc = bass.Bass("TRN2", target_bir_lowering=False)
        self.es = ExitStack()
        self.dbg = set(dbg)
        e = self.es.enter_context
        self.pe = Eng("pe", nc.tensor, e(nc.semaphore("s_pe")))
        self.act = Eng("act", nc.scalar, e(nc.semaphore("s_act")))
        self.dve = Eng("dve", nc.vector, e(nc.semaphore("s_dve")))
        self.pool = Eng("pool", nc.gpsimd, e(nc.semaphore("s_pool")))
        self.sp = Eng("sp", nc.sync, e(nc.semaphore("s_sp")))
        self.engs = [self.pe, self.act, self.dve, self.pool, self.sp]
        self.dsem = [e(nc.semaphore(f"s_d{i}")) for i in range(NDS + NDS_SW)]
        self.dcnt = [0] * (NDS + NDS_SW)
        self.drr = 0
        self.drr_sw = 0
        self.ddeps = {}
        self.ntile = 0
        self.yielders = {}

    def sb(self, shape, dt=F32, es=None):
        self.ntile += 1
        t = (es or self.es).enter_context(self.nc.sbuf_tensor(f"t{self.ntile}", list(shape), dt))
        return Tl(t)

    def psum(self, shape, dt=F32, es=None):
        self.ntile += 1
        t = (es or self.es).enter_context(self.nc.psum_tensor(f"p{self.ntile}", list(shape), dt))
        return Tl(t)

    def dram(self, name, shape, dt=F32, kind=None):
        if kind is None:
            kind = "ExternalOutput" if name in self.dbg else "Internal"
        return self.nc.dram_tensor(name, list(shape), dt, kind=kind).ap()

    def dd(self, *key):
        d = self.ddeps.get(key)
        if d is None:
            d = self.ddeps[key] = Dep()
        return d

    def dr(self, name, n0, w):
        return [self.dd(name, i) for i in range(n0 // 128, (n0 + w + 127) // 128)]

    def _sync(self, E, R, W):
        need = {}

        def upd(tok):
            k, sem, val = tok
            if k not in need or need[k][1] < val:
                need[k] = (sem, val)

        for d in R:
            d = _d(d)
            if d.w:
                upd(d.w)
        for d in W:
            d = _d(d)
            if d.w:
                upd(d.w)
            for k, (sem, val) in d.r.items():
                if k != E.key:
                    upd((k, sem, val))
        for k, (sem, val) in need.items():
            if k == E.key and E is self.pe:
                continue
            if E.waited.get(k, 0) < val:
                E.be.wait_ge(sem, val)
                E.waited[k] = val

    def _mark(self, tok, R, W):
        k, sem, val = tok
        for d in R:
            _d(d).r[k] = (sem, val)
        for d in W:
            d = _d(d)
            d.w = tok
            d.r = {}

    def op(self, E, fn, R, W):
        self._sync(E, R, W)
        ins = fn()
        E.n += 1
        ins.then_inc(E.sem, 1)
        self._mark((E.key, E.sem, E.n), R, W)
        self._yield()

    def dma(self, out, in_, R, W, Q=None, **kw):
        Q = Q or self.sp
        self._sync(Q, R, W)
        sw = Q is self.pool
        if sw:
            s = NDS + self.drr_sw
            self.drr_sw = (self.drr_sw + 1) % NDS_SW
        else:
            s = self.drr
            self.drr = (s + 1) % NDS
        sem = self.dsem[s]
        k = ("d", s)
        if self.dcnt[s] > 0 and Q.waited.get(k, 0) < 16 * self.dcnt[s]:
            Q.be.wait_ge(sem, 16 * self.dcnt[s])
            Q.waited[k] = 16 * self.dcnt[s]
        Q.be.dma_start(out=out, in_=in_, **kw).then_inc(sem, 16)
        self.dcnt[s] += 1
        self._mark((k, sem, 16 * self.dcnt[s]), R, W)
        self._yield()

    def _yield(self):
        if self.yielders:
            y = self.yielders.get(threading.get_ident())
            if y:
                y()

    def run_interleaved(self, fns):
        n = len(fns)
        state = {"turn": 0, "done": [False] * n, "exc": None}
        cv = threading.Condition()

        def advance(i):
            for k in range(1, n + 1):
                nx = (i + k) % n
                if not state["done"][nx]:
                    state["turn"] = nx
                    break
            else:
                state["turn"] = -1
            cv.notify_all()

        def yielder(i):
            def y():
                with cv:
                    advance(i)
                    while state["turn"] != i:
                        cv.wait()
            return y

        def worker(i):
            with cv:
                while state["turn"] != i:
                    cv.wait()
            self.yielders[threading.get_ident()] = yielder(i)
            try:
                fns[i]()
            except BaseException as e:
                state["exc"] = e
            finally:
                self.yielders.pop(threading.get_ident(), None)
                with cv:
                    state["done"][i] = True
                    advance(i)

        ths = [threading.Thread(target=worker, args=(i,)) for i in range(n)]
        for th in ths:
            th.start()
        for th in ths:
            th.join()
        if state["exc"] is not None:
            raise state["exc"]

    def barrier(self):
        for E in self.engs:
            for E2 in self.engs:
                if E2 is not E and E2.n > 0 and E.waited.get(E2.key, 0) < E2.n:
                    E.be.wait_ge(E2.sem, E2.n)
                    E.waited[E2.key] = E2.n
            for s in range(NDS + NDS_SW):
                k = ("d", s)
                if self.dcnt[s] > 0 and E.waited.get(k, 0) < 16 * self.dcnt[s]:
                    E.be.wait_ge(self.dsem[s], 16 * self.dcnt[s])
                    E.waited[k] = 16 * self.dcnt[s]

    def mm(self, out, lhsT, rhs, start, stop, R, W):
        self.op(self.pe, lambda: self.nc.tensor.matmul(out, lhsT=lhsT, rhs=rhs, start=start, stop=stop), R, W)

    def tr(self, out, in_, ident, R, W):
        self.op(self.pe, lambda: self.nc.tensor.transpose(out, in_, ident), R, W)

    def actf(self, out, in_, func, R, W, bias=None, scale=None):
        kw = {}
        if bias is not None:
            kw["bias"] = bias
        if scale is not None:
            kw["scale"] = scale
        self.op(self.act, lambda: self.nc.scalar.activation(out=out, in_=in_, func=func, **kw), R, W)

    def ts(self, E, out, in0, s1, s2, op0, op1, R, W):
        if op1 is None:
            self.op(E, lambda: E.be.tensor_scalar(out=out, in0=in0, scalar1=s1, scalar2=None, op0=op0), R, W)
        else:
            self.op(E, lambda: E.be.tensor_scalar(out=out, in0=in0, scalar1=s1, scalar2=s2, op0=op0, op1=op1), R, W)

    def tt(self, E, out, in0, in1, op, R, W):
        self.op(E, lambda: E.be.tensor_tensor(out=out, in0=in0, in1=in1, op=op), R, W)

    def stt(self, E, out, in0, scalar, in1, op0, op1, R, W):
        self.op(E, lambda: E.be.scalar_tensor_tensor(out=out, in0=in0, scalar=scalar, in1=in1, op0=op0, op1=op1), R, W)

    def cp(self, E, out, in_, R, W):
        if E is self.act:
            self.op(E, lambda: self.nc.scalar.copy(out=out, in_=in_), R, W)
        else:
            self.op(E, lambda: E.be.tensor_copy(out=out, in_=in_), R, W)

    def recip(self, out, in_, R, W):
        self.op(self.dve, lambda: self.nc.vector.reciprocal(out=out, in_=in_), R, W)

    def memset(self, E, ap, v, W):
        self.op(E, lambda: E.be.memset(ap, v), [], W)


class PF:
    def __init__(self):
        self.cols = {}
        self.n = 0

    def add(self, name, nch):
        self.cols[name] = (self.n, nch)
        self.n += nch
        return self.cols[name][0]


def pf_layout():
    pf = PF()
    for nm, nch in [("norm_mix", 8), ("norm_ffn", 8), ("b_ada", 48), ("mu0", 15), ("mu1", 15),
                    ("w0_0", 4), ("w0_1", 4), ("a0_0", 4), ("a0_1", 4), ("kk", 4), ("ka", 4), ("rk", 4),
                    ("ln_g", 4), ("ln_b", 4), ("gconv", 60), ("gnorm", 1), ("alog", 1), ("dtb", 1),
                    ("cdw", 124), ("cdwb", 4), ("clng", 4), ("clnb", 4), ("norm_final", 8)]:
        pf.add(nm, nch)
    return pf


def _fm(v, nch):
    return np.ascontiguousarray(np.asarray(v, np.float32).reshape(nch, 128).T)


def pack_pf(inp, l):
    pf = pf_layout()
    out = np.zeros((128, pf.n), np.float32)

    def put(nm, arr):
        o, n = pf.cols[nm]
        out[:, o:o + n] = arr

    put("norm_mix", _fm(inp["norm_mix"][l], 8))
    put("norm_ffn", _fm(inp["norm_ffn"][l], 8))
    put("b_ada", _fm(inp["b_ada"][l], 48))
    put("mu0", _fm(inp["rwkv_mu"][l, 0], 15))
    put("mu1", _fm(inp["rwkv_mu"][l, 1], 15))
    for d in range(2):
        put(f"w0_{d}", _fm(inp["rwkv_w0"][l, d], 4))
        put(f"a0_{d}", _fm(inp["rwkv_a0"][l, d], 4))
    put("kk", _fm(inp["rwkv_kk"][l], 4))
    put("ka", _fm(inp["rwkv_ka"][l], 4))
    put("rk", _fm(inp["rwkv_rk"][l].reshape(-1), 4))
    put("ln_g", _fm(inp["rwkv_ln_g"][l], 4))
    put("ln_b", _fm(inp["rwkv_ln_b"][l], 4))
    gc = np.concatenate([_fm(inp["gdn_conv"][l, k], 12) for k in range(5)], axis=1)
    put("gconv", gc)
    put("gnorm", _fm(inp["gdn_norm"][l], 1))
    al = np.zeros((128, 1), np.float32); al[0:8, 0] = np.asarray(inp["gdn_A_log"][l]).reshape(-1)
    db = np.zeros((128, 1), np.float32); db[0:8, 0] = np.asarray(inp["gdn_dt_bias"][l]).reshape(-1)
    put("alog", al)
    put("dtb", db)
    cd = np.concatenate([_fm(inp["conf_dw"][l, k], 4) for k in range(31)], axis=1)
    put("cdw", cd)
    put("cdwb", _fm(inp["conf_dw_b"][l], 4))
    put("clng", _fm(inp["conf_ln_g"][l], 4))
    put("clnb", _fm(inp["conf_ln_b"][l], 4))
    put("norm_final", _fm(inp["norm_final"], 8))
    return out


EPS = 1e-6


class Prog(KB):
    def __init__(self, dbg=(), stop=None, nlayers=L):
        super().__init__(dbg)
        self.stop = stop
        self.nlayers = nlayers
        nc = self.nc
        self.pf = pf_layout()

        def inp(name, shape):
            return nc.dram_tensor(name, list(shape), F32, kind="ExternalInput").ap()

        self.x = inp("x", [S, TL, D])
        self.ctx = inp("ctx", [S, TC, D])
        self.cvecT = inp("cvecT", [D, 3])
        self.pfp = inp("pfp", [L, 128, self.pf.n])
        self.w_ada = inp("w_ada", [L, D, 6 * D])
        self.w_in = inp("w_in", [L, D, NIN])
        self.rw2 = inp("rwkv_w2", [L, 2, 64, 512])
        self.ra2 = inp("rwkv_a2", [L, 2, 64, 512])
        self.rg2 = inp("rwkv_g2", [L, 128, 512])
        self.w_branch = inp("w_branch", [L, 3, 512, D])
        self.w_out = inp("w_out", [L, D, D])
        self.w_router = inp("w_router", [D, 16])
        self.rbias = inp("rbias", [128, 16])
        self.weg = inp("w_e_gate", [L, 16, D, 512])
        self.weu = inp("w_e_up", [L, 16, D, 512])
        self.wed = inp("w_e_down", [L, 16, 512, D])
        self.y = nc.dram_tensor("y", [S, TL, D], F32, kind="ExternalOutput").ap()
        self.XT = self.dram("XT", [D, NT])
        self.XTv = self.XT.rearrange("(c p) n -> p c n", p=128)
        self.PT = self.dram("PT", [NIN, NT])
        self.ps = [self.psum([128, 512], F32) for _ in range(8)]
        self.ident = self.sb([128, 128], F32)
        self.identb = self.sb([128, 128], BF16)
        self.ones = self.sb([128, 128], F32)
        self.PFt = [self.sb([128, self.pf.n], F32) for _ in range(L)]
        self.scT = self.sb([128, 8, 3], F32)
        self.MOD = [self.sb([128, 48, 3], F32) for _ in range(L)]
        self.GM = [self.sb([128, 8, 3], F32) for _ in range(L)]
        self.GF = [self.sb([128, 8, 3], F32) for _ in range(L)]

    def pfv(self, l, name, c=None, n=1):
        o, nch = self.pf.cols[name]
        if c is None:
            return self.PFt[l][:, o:o + nch]
        return self.PFt[l][:, o + c:o + c + n]

    def consts(self):
        nc = self.nc
        P = self.pool
        self.memset(P, self.ident[:, :], 0.0, [self.ident])
        self.op(P, lambda: nc.gpsimd.affine_select(out=self.ident[:, :], in_=self.ident[:, :], pattern=[[-1, 128]],
                                                   compare_op=ALU.not_equal, fill=1.0, base=0, channel_multiplier=1),
                [self.ident], [self.ident])
        self.cp(P, self.identb[:, :], self.ident[:, :], [self.ident], [self.identb])
        self.memset(P, self.ones[:, :], 1.0, [self.ones])
        for l in range(L):
            self.dma(self.PFt[l][:, :], self.pfp[l], [], [self.PFt[l]])
        cv = self.sb([128, 8, 3], F32)
        self.dma(cv[:, :, :], self.cvecT.rearrange("(c p) r -> p c r", p=128), [], [cv])
        self.actf(self.scT[:, :, :], cv[:, :, :], AF.Silu, [cv], [self.scT])

    def phase0(self):
        with ExitStack() as es:
            xin = [self.sb([128, 1024], F32, es) for _ in range(2)]
            xo = [self.sb([128, 8, 128], F32, es) for _ in range(2)]
            i = 0
            for s in range(S):
                for j in range(T // 128):
                    n0 = s * T + j * 128
                    src = self.ctx[s, j * 128:(j + 1) * 128, :] if j < 2 else self.x[s, (j - 2) * 128:(j - 1) * 128, :]
                    a = xin[i % 2]
                    o = xo[i % 2]
                    self.dma(a[:, :], src, [], [a])
                    for c in range(8):
                        pb = self.ps[(i % 2) * 2 + c // 4]
                        self.tr(pb[:, (c % 4) * 128:(c % 4 + 1) * 128], a[:, c * 128:(c + 1) * 128], self.ident[:, :],
                                [a, self.ident], [pb])
                    for hf in range(2):
                        pb = self.ps[(i % 2) * 2 + hf]
                        self.cp(self.act if hf == 0 else self.dve, o[:, hf * 4:(hf + 1) * 4, :],
                                pb[:, :].rearrange("p (c t) -> p c t", c=4), [pb], [o])
                    self.dma(self.XTv[:, :, n0:n0 + 128], o[:, :, :], [o], self.dr("XT", n0, 128))
                    i += 1
            self.barrier()

    def phaseA(self, l):
        with ExitStack() as es:
            wa = [self.sb([128, 8, 768], F32, es) for _ in range(2)]
            pm = self.ps[0]
            wav = self.w_ada[l].rearrange("(k p) n -> p k n", p=128)
            for mg in range(8):
                w = wa[mg % 2]
                for q in range(4):
                    self.dma(w[:, q * 2:(q + 1) * 2, :], wav[:, q * 2:(q + 1) * 2, mg * 768:(mg + 1) * 768], [], [w])
                for m in range(6):
                    mm_ = mg * 6 + m
                    for k in range(8):
                        self.mm(pm[:, mm_ * 3:(mm_ + 1) * 3], w[:, k, m * 128:(m + 1) * 128], self.scT[:, k, :], k == 0, k == 7,
                                [w, self.scT], [pm])
            mod = self.MOD[l]
            self.tt(self.dve, mod[:, :, :], pm[:, 0:144].rearrange("p (m r) -> p m r", r=3),
                    self.pfv(l, "b_ada").unsqueeze(2).to_broadcast([128, 48, 3]), ALU.add, [pm, self.PFt[l]], [mod])
            for (G, mi, nm) in ((self.GM[l], 1, "norm_mix"), (self.GF[l], 4, "norm_ffn")):
                self.ts(self.dve, G[:, :, :], mod[:, mi * 8:(mi + 1) * 8, :], 1.0, None, ALU.add, None, [mod], [G])
                self.dump(f"G1{l}{mi}", G, G[:, :, :], [128, 8, 3])
                self.tt(self.dve, G[:, :, :], G[:, :, :], self.pfv(l, nm).unsqueeze(2).to_broadcast([128, 8, 3]), ALU.mult,
                        [G, self.PFt[l]], [G])
            self.dump(f"MOD{l}", mod, mod[:, :, :], [128, 48, 3])
            self.dump(f"GM{l}", self.GM[l], self.GM[l][:, :, :], [128, 8, 3])
            self.barrier()

    def tiles(self):
        out = []
        for s in range(S):
            out.append((s * T, TC, 2))
            for j in range(TL // 512):
                out.append((s * T + TC + j * 512, 512, s))
        return out

    def modulate_tile(self, es_bufs, n0, w, r, G, shift_col, mod, out_fn):
        xt, sq, rs, tmp = es_bufs
        psS = self.ps[7]
        self.dma(xt[:, :, :w], self.XTv[:, :, n0:n0 + w], self.dr("XT", n0, w), [xt])
        self.actf(sq[:, :, :w], xt[:, :, :w], AF.Square, [xt], [sq])
        for c in range(8):
            self.mm(psS[:, :w], self.ones[:, :], sq[:, c, :w], c == 0, c == 7, [self.ones, sq], [psS])
        self.ts(self.dve, rs[:, :w], psS[:, :w], 1.0 / D, EPS, ALU.mult, ALU.add, [psS], [rs])
        self.actf(rs[:, :w], rs[:, :w], AF.Sqrt, [rs], [rs])
        self.recip(rs[:, :w], rs[:, :w], [rs], [rs])
        return xt, rs

    def phaseB(self, l):
        with ExitStack() as es:
            hT = self.sb([128, 8, NT], BF16, es)
            with ExitStack() as es1:
                xts = [self.sb([128, 8, 512], F32, es1) for _ in range(2)]
                sq = self.sb([128, 8, 512], F32, es1)
                rs = self.sb([128, 512], F32, es1)
                tmps = [self.sb([128, 512], F32, es1) for _ in range(2)]
                for i, (n0, w, r) in enumerate(self.tiles()):
                    xt, _ = self.modulate_tile((xts[i % 2], sq, rs, None), n0, w, r, None, None, None, None)
                    for c in range(8):
                        tmp = tmps[c % 2]
                        self.stt(self.dve, tmp[:, :w], xt[:, c, :w], self.GM[l][:, c, r:r + 1], rs[:, :w], ALU.mult, ALU.mult,
                                 [xt, rs, self.GM[l]], [tmp])
                        self.actf(hT[:, c, n0:n0 + w], tmp[:, :w], AF.Identity, [tmp, self.MOD[l]], [hT],
                                  bias=self.MOD[l][:, c, r:r + 1], scale=1.0)
                if "HT" in self.dbg:
                    hd = self.dram("HT", [128, 8, NT], BF16)
                    self.dma(hd, hT[:, :, :], [hT], [self.dd("HTd")])
                self.barrier()
            wbf = [self.sb([128, 8, 1024], BF16, es) for _ in range(2)]
            ost = [self.sb([128, 512], F32, es) for _ in range(4)]
            no = 0
            def loadw(g):
                c0 = g * 1024
                cw = min(1024, NIN - c0)
                self.dma(wbf[g % 2][:, :, :cw], self.w_in[l][:, c0:c0 + cw].rearrange("(k p) n -> p k n", p=128), [], [wbf[g % 2]], Q=self.pool)
            loadw(0)
            for g in range(8):
                c0 = g * 1024
                cw = min(1024, NIN - c0)
                wb = wbf[g % 2]
                if g < 7:
                    loadw(g + 1)
                nm = (cw + 127) // 128
                for tt_ in range(NT // 512):
                    n0 = tt_ * 512
                    for m in range(nm):
                        mw = min(128, cw - m * 128)
                        pb = self.ps[no % 6]
                        for k in range(8):
                            self.mm(pb[:mw, :], wb[:, k, m * 128:m * 128 + mw], hT[:, k, n0:n0 + 512], k == 0, k == 7,
                                    [wb, hT], [pb])
                        o = ost[no % 4]
                        self.cp(self.act if no % 2 == 0 else self.dve, o[:mw, :], pb[:mw, :], [pb], [o])
                        self.dma(self.PT[c0 + m * 128:c0 + m * 128 + mw, n0:n0 + 512], o[:mw, :], [o],
                                 [self.dd("PT", (c0 + m * 128) // 128, j) for j in range(n0 // 128, n0 // 128 + 4)])
                        no += 1
            self.barrier()

    def dump(self, name, tile, ap, shape, dt=F32):
        if name in self.dbg:
            d = self.dram(name, shape, dt)
            self.dma(d, ap, [tile], [self.dd(name)])

    def finish(self):
        self.barrier()

    def build(self):
        self.consts()
        self.phase0()
        if self.stop == "0":
            return self.finish()
        for l in range(self.nlayers):
            self.phaseA(l)
            if self.stop == f"A{l}":
                return self.finish()
            self.phaseB(l)
            if self.stop == f"B{l}":
                return self.finish()
        self.finish()


def make_in_maps(inp):
    ncores = 8
    pfp = np.stack([pack_pf(inp, l) for l in range(L)])
    rbias = np.ascontiguousarray(np.broadcast_to(np.asarray(inp["router_bias"], np.float32)[None, :], (128, 16)))
    maps = []
    for i in range(ncores):
        cv = np.stack([inp["c"][2 * i], inp["c"][2 * i + 1], inp["c_ctx"]], axis=1).astype(np.float32)
        m = {
            "x": np.ascontiguousarray(inp["x"][2 * i:2 * i + 2]),
            "ctx": np.ascontiguousarray(inp["ctx"][2 * i:2 * i + 2]),
            "cvecT": np.ascontiguousarray(cv),
            "pfp": pfp, "rbias": rbias,
        }
        for k in ("w_ada", "w_in", "rwkv_w2", "rwkv_a2", "rwkv_g2", "w_branch", "w_out", "w_router",
                  "w_e_gate", "w_e_up", "w_e_down"):
            m[k] = np.ascontiguousarray(inp[k], dtype=np.float32)
        maps.append(m)
    return maps


def kernel(**inputs):
    inp = {k: np.asarray(v) for k, v in inputs.items()}
    prog = Prog2()
    prog.build()
    maps = make_in_maps(inp)
    res = run_bass_kernel_spmd(prog.nc, maps, core_ids=list(range(8)))
    return np.concatenate([r["y"] for r in res.results], axis=0).astype(np.float32)


CDEC = 0.6065306597126334
NJ = T // 128


def seg_bounds(j):
    return (j == 0 or j == 2), (j == 1 or j == NJ - 1)


class StopBuild(Exception):
    pass


class Prog2(Prog):
    cut = None

    def ck(self, n):
        if self.cut == n:
            raise StopBuild()

    def __init__(self, **kw):
        super().__init__(**kw)
        self.PTv = self.PT[0:8064, :].rearrange("(c p) n -> p c n", p=128)
        self.RKQ = self.dram("RKQ", [S, 2, NJ, 128, 4 * 256], BF16)
        self.RMAT = self.dram("RMAT", [S, 2, NJ, 128, 8 * 512], BF16)
        self.RBC = self.dram("RBC", [S, 2, NJ, 128, 1024], BF16)
        self.RV = self.dram("RV", [S, NJ, 128, 512], BF16)
        self.RPC = self.dram("RPC", [S, 2, NJ, 128, 8], F32)
        self.GAs = self.dram("GAs", [512, NT])
        self.BON = self.dram("BON", [512, NT])
        self.YA = self.dram("YA", [2, NT, 512])
        self.bones = self.sb([128, 128], F32)
        self.MU = self.sb([128, 256], F32)
        self.ML = self.sb([128, 256], F32)
        self.RM = self.sb([128, 128], F32)

    def consts(self):
        super().consts()
        nc = self.nc
        P = self.pool
        self.memset(P, self.bones[:, :], 0.0, [self.bones])
        self.memset(P, self.bones[0:64, 0:64], 1.0, [self.bones])
        self.memset(P, self.bones[64:128, 64:128], 1.0, [self.bones])
        self.memset(P, self.RM[:, :], 1.0, [self.RM])
        self.memset(P, self.RM[:, 0:1], 0.0, [self.RM])
        self.memset(P, self.RM[:, 64:65], 0.0, [self.RM])
        for (Mt, off, cmp_, sg) in ((self.MU, 0, ALU.is_gt, 1), (self.MU, 128, ALU.is_ge, 1), (self.ML, 0, ALU.is_gt, -1), (self.ML, 128, ALU.is_ge, -1)):
            sl = Mt[:, off:off + 128]
            self.memset(P, sl, 1.0, [Mt])
            self.op(P, lambda sl=sl, cmp_=cmp_, sg=sg: nc.gpsimd.affine_select(out=sl, in_=sl, pattern=[[sg, 128]], compare_op=cmp_, fill=0.0,
                                                                              base=0, channel_multiplier=-sg), [Mt], [Mt])
            self.memset(P, Mt[0:64, off + 64:off + 128], 0.0, [Mt])
            self.memset(P, Mt[64:128, off:off + 64], 0.0, [Mt])

    def load_halo(self, dst, c0, nch, s, j, hw, Q=None):
        n0 = s * T + j * 128
        lb, rb = seg_bounds(j)
        lo = 0 if lb else hw
        hi = 0 if rb else hw
        if lb:
            self.memset(self.pool, dst[:, :, 0:hw], 0.0, [dst])
        if rb:
            self.memset(self.pool, dst[:, :, 128 + hw:128 + 2 * hw], 0.0, [dst])
        deps = [self.dd("PT", c, i) for c in range(c0, c0 + nch) for i in range((n0 - lo) // 128, (n0 + 128 + hi - 1) // 128 + 1)]
        self.dma(dst[:, :, hw - lo:hw + 128 + hi], self.PTv[:, c0:c0 + nch, n0 - lo:n0 + 128 + hi], deps, [dst], Q=Q)

    def inverse(self, X1, NN, MAT, h, Wt, At, Bt, pA, pB_, pC):
        V, G = self.dve, self.pool
        W, A, B = Wt[0], At[0], Bt[0]
        self.tt(G, W[:, :], self.identb[:, :], X1[:, 0:128], ALU.subtract, [self.identb, X1], [W])
        self.mm(pA[:, 0:128], X1[:, 0:128], NN[:, :], True, True, [X1, NN], [pA])
        self.mm(pB_[:, 0:128], NN[:, :], X1[:, 0:128], True, True, [X1, NN], [pB_])
        self.cp(self.act, A[:, :], pA[:, 0:128], [pA], [A])
        self.cp(self.act, B[:, :], pB_[:, 0:128], [pB_], [B])
        for it in range(5):
            W2, A2, B2 = Wt[(it + 1) % 2], At[(it + 1) % 2], Bt[(it + 1) % 2]
            self.mm(pC[:, 0:128], A[:, :], W[:, :], True, True, [A, W], [pC])
            if it < 4:
                self.mm(pA[:, 0:128], B[:, :], A[:, :], True, True, [A, B], [pA])
                self.mm(pB_[:, 0:128], A[:, :], B[:, :], True, True, [A, B], [pB_])
            dstW = W2[:, :] if it < 4 else MAT[:, h, 0:128]
            self.tt(V, dstW, W[:, :], pC[:, 0:128], ALU.add, [W, pC], [W2 if it < 4 else MAT])
            if it < 4:
                self.cp(self.act, A2[:, :], pA[:, 0:128], [pA], [A2])
                self.cp(self.act, B2[:, :], pB_[:, 0:128], [pB_], [B2])
            W, A, B = W2, A2, B2

    def inverse_batch(self, X1a, NNa, MAT, h0, banks, Wt, At, Bt):
        V, G = self.dve, self.pool
        psW, psA, psB = banks
        hs = slice(h0, h0 + 4)

        def reg(p, i):
            return p[:, i * 128:(i + 1) * 128]

        def bv(p):
            return p[:, :].rearrange("p (h t) -> p h t", h=4)
        W, A, B = Wt[0], At[0], Bt[0]
        self.tt(G, W[:, :, :], self.identb[:, :].unsqueeze(1).to_broadcast([128, 4, 128]), X1a[:, hs, 0:128], ALU.subtract, [self.identb, X1a], [W])
        for i in range(4):
            self.mm(reg(psA, i), X1a[:, h0 + i, 0:128], NNa[:, h0 + i, :], True, True, [X1a, NNa], [psA])
        for i in range(4):
            self.mm(reg(psB, i), NNa[:, h0 + i, :], X1a[:, h0 + i, 0:128], True, True, [X1a, NNa], [psB])
        self.cp(self.act, A[:, :, :], bv(psA), [psA], [A])
        self.cp(self.act, B[:, :, :], bv(psB), [psB], [B])
        for it in range(5):
            W2, A2, B2 = Wt[(it + 1) % 2], At[(it + 1) % 2], Bt[(it + 1) % 2]
            for i in range(4):
                self.mm(reg(psW, i), A[:, i, :], W[:, i, :], True, True, [A, W], [psW])
            if it < 4:
                for i in range(4):
                    self.mm(reg(psA, i), B[:, i, :], A[:, i, :], True, True, [A, B], [psA])
                for i in range(4):
                    self.mm(reg(psB, i), A[:, i, :], B[:, i, :], True, True, [A, B], [psB])
                self.tt(V, W2[:, :, :], W[:, :, :], bv(psW), ALU.add, [W, psW], [W2])
                self.cp(self.act, A2[:, :, :], bv(psA), [psA], [A2])
                self.cp(self.act, B2[:, :, :], bv(psB), [psB], [B2])
            else:
                self.tt(V, MAT[:, hs, 0:128], W[:, :, :], bv(psW), ALU.add, [W, psW], [MAT])
            W, A, B = W2, A2, B2

    def rwkv_prep(self, l):
        nc = self.nc
        V, G = self.dve, self.pool
        with ExitStack() as es:
            def t(shape, dt=F32):
                return self.sb(shape, dt, es)
            wtmp = t([128, 512])
            w2b, a2b, g2b = t([128, 512], BF16), t([128, 512], BF16), t([128, 512], BF16)
            for (src, dstb) in ((self.rw2[l].rearrange("d r c -> (d r) c"), w2b), (self.ra2[l].rearrange("d r c -> (d r) c"), a2b), (self.rg2[l], g2b)):
                self.dma(wtmp[:, :], src, [], [wtmp])
                self.cp(V, dstb[:, :], wtmp[:, :], [wtmp], [dstb])
            PFl = self.PFt[l]
            c0t = t([128, 15])
            self.tt(V, c0t[:, :], self.pfv(l, "mu0"), self.pfv(l, "mu1"), ALU.add, [PFl], [c0t])
            self.ts(V, c0t[:, :], c0t[:, :], -1.0, 1.0, ALU.mult, ALU.add, [c0t], [c0t])
            omka = t([128, 4])
            self.ts(V, omka[:, :], self.pfv(l, "ka"), -1.0, 1.0, ALU.mult, ALU.add, [PFl], [omka])

            def bc(ap, n):
                return ap.unsqueeze(2).to_broadcast([128, n, 128])

            DWr = t([128, 45, 128], BF16)
            for c_ in range(15):
                for k_, col in enumerate((self.pfv(l, "mu0", c_), c0t[:, c_:c_ + 1], self.pfv(l, "mu1", c_))):
                    self.ts(V, DWr[:, k_ * 15 + c_, :], self.identb[:, :], col, None, ALU.mult, None, [self.identb, PFl, c0t], [DWr])
            def stream(s):
                pa = t([128, 15, 130], BF16)
                sh = t([128, 15, 128])
                twb, xab, sgb = t([128, 128], BF16), t([128, 128], BF16), t([128, 128], BF16)
                SW = [t([128, 4, 128]) for _ in range(2)]
                AA = [t([128, 4, 128]) for _ in range(2)]
                ga = t([128, 4, 128])
                kx, kk, tq, bon = t([128, 4, 128]), t([128, 4, 128]), t([128, 4, 128]), t([128, 4, 128])
                CS, EX, tmpa, tmpb = (t([128, 4, 128]) for _ in range(4))
                e1, e2, e3 = (t([128, 4, 128]) for _ in range(3))
                pc = t([128, 8])
                KQ = t([128, 4, 256], BF16)
                BTb, CTb = t([128, 4, 128], BF16), t([128, 4, 128], BF16)
                vb = t([128, 4, 128], BF16)
                BC = t([128, 1024], BF16)
                Vt = t([128, 512], BF16)
                MAT = t([128, 8, 512], BF16)
                X1a = t([128, 8, 256], BF16)
                NNa = t([128, 8, 128], BF16)
                BTz, CTz = t([128, 8, 128], BF16), t([128, 8, 128], BF16)
                self.memset(G, BTz[:, :, :], 0.0, [BTz])
                self.memset(G, CTz[:, :, :], 0.0, [CTz])
                Wt = [t([128, 4, 128], BF16) for _ in range(2)]
                At = [t([128, 4, 128], BF16) for _ in range(2)]
                Bt = [t([128, 4, 128], BF16) for _ in range(2)]
                Wt2 = [t([128, 4, 128], BF16) for _ in range(2)]
                At2 = [t([128, 4, 128], BF16) for _ in range(2)]
                Bt2 = [t([128, 4, 128], BF16) for _ in range(2)]
                B = self.ps[4 * s:4 * s + 4]
                for j in range(NJ):
                    n0 = s * T + j * 128
                    self.ck(1000 + s * NJ + j)
                    self.load_halo(pa, 0, 15, s, j, 1, Q=self.pool)
                    for c in range(15):
                        pb = B[c // 4]
                        for k in range(3):
                            self.mm(pb[:, (c % 4) * 128:(c % 4 + 1) * 128], DWr[:, k * 15 + c, :], pa[:, c, k:k + 128], k == 0, k == 2, [DWr, pa], [pb])
                    for b4_ in range(4):
                        n_ = 4 if b4_ < 3 else 3
                        self.cp(self.act if b4_ % 2 == 0 else V, sh[:, 4 * b4_:4 * b4_ + n_, :],
                                B[b4_][:, 0:n_ * 128].rearrange("p (c t) -> p c t", c=n_), [B[b4_]], [sh])
                    self.ck(2)
                    r_, k_, v_ = sh[:, 0:4, :], sh[:, 4:8, :], sh[:, 8:12, :]
                    self.actf(twb[:, :], sh[:, 12, :], AF.Tanh, [sh], [twb])
                    self.cp(self.act, xab[:, :], sh[:, 13, :], [sh], [xab])
                    self.actf(sgb[:, :], sh[:, 14, :], AF.Sigmoid, [sh], [sgb])
                    self.ck(3)
                    for d in range(2):
                        for (wb_, xin, dst, bname, pb) in ((w2b, twb, SW[d], f"w0_{d}", B[0]), (a2b, xab, AA[d], f"a0_{d}", B[1])):
                            for m in range(4):
                                self.mm(pb[:, m * 128:(m + 1) * 128], wb_[d * 64:(d + 1) * 64, m * 128:(m + 1) * 128],
                                        xin[d * 64:(d + 1) * 64, :], True, True, [wb_, xin], [pb])
                            for m in range(4):
                                self.actf(dst[:, m, :], pb[:, m * 128:(m + 1) * 128], AF.Sigmoid, [pb, PFl], [dst],
                                          bias=self.pfv(l, bname, m), scale=1.0)
                    for m in range(4):
                        self.mm(B[2][:, m * 128:(m + 1) * 128], g2b[:, m * 128:(m + 1) * 128], sgb[:, :], True, True, [g2b, sgb], [B[2]])
                    self.cp(self.act, ga[:, :, :], B[2][:, :].rearrange("p (m t) -> p m t", m=4), [B[2]], [ga])
                    self.dma(self.GAs.rearrange("(m p) n -> p m n", p=128)[:, :, n0:n0 + 128], ga[:, :, :], [ga], [self.dd("GAs", n0 // 128)])
                    self.ck(4)
                    self.tt(V, kx[:, :, :], k_, bc(self.pfv(l, "kk"), 4), ALU.mult, [sh, PFl], [kx])
                    self.tt(G, tq[:, :, :], kx[:, :, :], kx[:, :, :], ALU.mult, [kx], [tq])
                    for m in range(4):
                        self.mm(B[3][:, m * 128:(m + 1) * 128], self.bones[:, :], tq[:, m, :], True, True, [self.bones, tq], [B[3]])
                    self.ts(V, tq[:, :, :], B[3][:, :].rearrange("p (m t) -> p m t", m=4), EPS, None, ALU.add, None, [B[3]], [tq])
                    self.actf(tq[:, :, :], tq[:, :, :], AF.Sqrt, [tq], [tq])
                    self.recip(tq[:, :, :], tq[:, :, :], [tq], [tq])
                    self.tt(V, kk[:, :, :], kx[:, :, :], tq[:, :, :], ALU.mult, [kx, tq], [kk])
                    self.ck(5)
                    self.tt(G, bon[:, :, :], r_, k_, ALU.mult, [sh], [bon])
                    self.tt(G, bon[:, :, :], bon[:, :, :], bc(self.pfv(l, "rk"), 4), ALU.mult, [bon, PFl], [bon])
                    for m in range(4):
                        self.mm(B[0][:, m * 128:(m + 1) * 128], self.bones[:, :], bon[:, m, :], True, True, [self.bones, bon], [B[0]])
                    self.tt(V, bon[:, :, :], B[0][:, :].rearrange("p (m t) -> p m t", m=4), v_, ALU.mult, [B[0], sh], [bon])
                    self.dma(self.BON.rearrange("(m p) n -> p m n", p=128)[:, :, n0:n0 + 128], bon[:, :, :], [bon], [self.dd("BON", n0 // 128)])
                    self.ck(6)
                    self.cp(G, vb[:, :, :], v_, [sh], [vb])
                    pbv = B[1][:, :].bitcast(BF16)
                    for m in range(4):
                        self.tr(pbv[:, m * 128:(m + 1) * 128], vb[:, m, :], self.identb[:, :], [vb, self.identb], [B[1]])
                    self.cp(self.act, Vt[:, :], pbv[:, 0:512], [B[1]], [Vt])
                    self.dma(self.RV[s, j], Vt[:, :], [Vt], [self.dd("RV", s, j)])
                    for d in range(2):
                        self.ck(7)
                        self.tt(V, tmpa[:, :, :], AA[d][:, :, :], bc(self.pfv(l, "ka"), 4), ALU.mult, [AA[d], PFl], [tmpa])
                        self.tt(V, tmpa[:, :, :], tmpa[:, :, :], bc(omka[:, :], 4), ALU.add, [tmpa, omka], [tmpa])
                        self.tt(V, tmpa[:, :, :], tmpa[:, :, :], k_, ALU.mult, [tmpa, sh], [tmpa])
                        self.tt(G, tmpb[:, :, :], kk[:, :, :], AA[d][:, :, :], ALU.mult, [kk, AA[d]], [tmpb])
                        self.ck(8)
                        for m in range(4):
                            self.op(V, lambda m=m: nc.vector.tensor_tensor_scan(out=CS[:, m, :], data0=self.RM[:, :], data1=SW[d][:, m, :],
                                                                               initial=0.0, op0=ALU.mult, op1=ALU.add),
                                    [self.RM, SW[d]], [CS])
                        CSv = CS[:, :, :].rearrange("p m (c t) -> p m c t", t=64)
                        if d == 0:
                            self.tt(V, EX[:, :, :], CS[:, :, :], SW[d][:, :, :], ALU.subtract, [CS, SW[d]], [EX])
                            incl = CS
                        else:
                            tot = CSv[:, :, :, 63:64].to_broadcast([128, 4, 2, 64])
                            self.tt(V, EX[:, :, :].rearrange("p m (c t) -> p m c t", t=64), tot, CSv, ALU.subtract, [CS], [EX])
                            self.tt(V, e3[:, :, :], EX[:, :, :], SW[d][:, :, :], ALU.add, [EX, SW[d]], [e3])
                            incl = e3
                        self.ck(9)
                        self.actf(e1[:, :, :], EX[:, :, :], AF.Exp, [EX], [e1], scale=-CDEC)
                        self.actf(e2[:, :, :], incl[:, :, :], AF.Exp, [incl], [e2], scale=CDEC)
                        self.actf(e3[:, :, :], incl[:, :, :], AF.Exp, [incl], [e3], scale=-CDEC)
                        self.actf(pc[:, :].rearrange("p (m c) -> p m c", c=2), CSv[:, :, :, 63], AF.Exp, [CS], [pc], scale=-CDEC)
                        self.dma(self.RPC[s, d, j], pc[:, :], [pc], [self.dd("RPC", s, d, j)])
                        self.tt(V, KQ[:, :, 0:128], kk[:, :, :], e1[:, :, :], ALU.mult, [kk, e1], [KQ])
                        self.tt(G, KQ[:, :, 128:256], r_, e3[:, :, :], ALU.mult, [sh, e3], [KQ])
                        self.tt(V, BTb[:, :, :], tmpb[:, :, :], e2[:, :, :], ALU.mult, [tmpb, e2], [BTb])
                        self.tt(G, CTb[:, :, :], tmpa[:, :, :], e2[:, :, :], ALU.mult, [tmpa, e2], [CTb])
                        self.dma(self.RKQ[s, d, j], KQ[:, :, :].rearrange("p m t -> p (m t)"), [KQ], [self.dd("RKQ", s, d, j)])
                        self.ck(10)
                        pbb = B[2][:, :].bitcast(BF16)
                        for m in range(4):
                            self.tr(pbb[:, m * 128:(m + 1) * 128], BTb[:, m, :], self.identb[:, :], [BTb, self.identb], [B[2]])
                            self.tr(pbb[:, 512 + m * 128:512 + (m + 1) * 128], CTb[:, m, :], self.identb[:, :], [CTb, self.identb], [B[2]])
                        self.cp(self.act, BC[:, :], pbb[:, :], [B[2]], [BC])
                        self.dma(self.RBC[s, d, j], BC[:, :], [BC], [self.dd("RBC", s, d, j)])
                        self.ck(11)
                        Ms, Mn = (self.MU, self.ML) if d == 0 else (self.ML, self.MU)
                        for e_ in range(2):
                            rows = slice(e_ * 64, e_ * 64 + 64)
                            self.cp(G, BTz[:, :, :].rearrange("p (m e) t -> p m e t", e=2)[rows, :, e_, :], BTb[rows, :, :], [BTb], [BTz])
                            self.cp(self.act, CTz[:, :, :].rearrange("p (m e) t -> p m e t", e=2)[rows, :, e_, :], CTb[rows, :, :], [CTb], [CTz])
                        Msb = Ms[:, :].unsqueeze(1).to_broadcast([128, 2, 256])
                        v2 = lambda p: p[:, :].rearrange("p (h t) -> p h t", h=2)
                        for h in range(8):
                            self.mm(B[h // 2][:, (h % 2) * 256:(h % 2 + 1) * 256], BTz[:, h, :], KQ[:, h // 2, :], True, True, [BTz, KQ], [B[h // 2]])
                        for q in range(4):
                            self.tt(V, X1a[:, 2 * q:2 * q + 2, :], v2(B[q]), Msb, ALU.mult, [B[q], Ms], [X1a])
                        for h in range(8):
                            self.mm(B[h // 2][:, (h % 2) * 256:(h % 2 + 1) * 256], CTz[:, h, :], KQ[:, h // 2, :], True, True, [CTz, KQ], [B[h // 2]])
                        for q in range(4):
                            self.tt(V, MAT[:, 2 * q:2 * q + 2, 256:512], v2(B[q]), Msb, ALU.mult, [B[q], Ms], [MAT])
                        for h in range(8):
                            self.mm(B[h // 4][:, (h % 4) * 128:(h % 4 + 1) * 128], KQ[:, h // 2, 0:128], BTz[:, h, :], True, True, [BTz, KQ], [B[h // 4]])
                        Mnb = Mn[:, 0:128].unsqueeze(1).to_broadcast([128, 4, 128])
                        for q in range(2):
                            self.tt(V, NNa[:, 4 * q:4 * q + 4, :], B[q][:, :].rearrange("p (h t) -> p h t", h=4), Mnb, ALU.mult, [B[q], Mn], [NNa])
                        self.cp(G, MAT[:, :, 128:256], X1a[:, :, 128:256], [X1a], [MAT])
                        self.inverse_batch(X1a, NNa, MAT, 0, (B[0], B[1], B[2]), Wt, At, Bt)
                        self.inverse_batch(X1a, NNa, MAT, 4, (B[3], B[1], B[2]), Wt2, At2, Bt2)
                        self.ck(12)
                        self.dma(self.RMAT[s, d, j], MAT[:, :, :].rearrange("p h t -> p (h t)"), [MAT], [self.dd("RMAT", s, d, j)])
            self.run_interleaved([lambda: stream(0), lambda: stream(1)])
            self.barrier()

    def gdn_setup(self):
        self.GKQ = self.dram("GKQ", [S, 2, NJ, 128, 4 * 256], BF16)
        self.GMAT = self.dram("GMAT", [S, 2, NJ, 128, 4 * 512], BF16)
        self.GBC = self.dram("GBC", [S, 2, NJ, 128, 1024], BF16)
        self.GV = self.dram("GV", [S, NJ, 128, 512], BF16)
        self.GPC = self.dram("GPC", [S, 2, NJ, 128, 8], F32)
        self.YB = self.dram("YB", [2, NT, 512])
        self.SEL = self.sb([16, 16, 128], F32)
        self.selc = self.sb([128, 1], F32)
        self.onec = self.sb([128, 1], F32)
        nc = self.nc
        P = self.pool
        for i in range(16):
            self.cp(P, self.SEL[0:16, i, :], self.ident[0:16, i:i + 1].to_broadcast([16, 128]), [self.ident], [self.SEL])
        self.memset(P, self.onec[:, :], 1.0, [self.onec])
        self.memset(P, self.selc[:, :], 1.0, [self.selc])
        self.op(P, lambda: nc.gpsimd.affine_select(out=self.selc[:, :], in_=self.selc[:, :], pattern=[[0, 1]], compare_op=ALU.is_ge, fill=0.0,
                                                   base=-4, channel_multiplier=1), [self.selc], [self.selc])

    def gdn_prep(self, l):
        nc = self.nc
        V, G = self.dve, self.pool
        ps = self.ps
        with ExitStack() as es:
            def t(shape, dt=F32):
                return self.sb(shape, dt, es)
            PFl = self.PFt[l]

            def bc(ap, n):
                return ap.unsqueeze(2).to_broadcast([128, n, 128])
            negA = t([128, 1])
            self.actf(negA[:, :], self.pfv(l, "alog"), AF.Exp, [PFl], [negA])
            self.ts(V, negA[:, :], negA[:, :], -1.0, None, ALU.mult, None, [negA], [negA])
            DWg = t([128, 60, 128], BF16)
            gw_ = self.pfv(l, "gconv")
            for q_ in range(60):
                self.ts(V, DWg[:, q_, :], self.identb[:, :], gw_[:, q_:q_ + 1], None, ALU.mult, None, [self.identb, PFl], [DWg])
            def stream(s):
                B = self.ps[4 * s:4 * s + 4]
                qkv = t([128, 12, 132], BF16)
                cv = t([128, 12, 128])
                sq = t([128, 8, 128])
                kq = t([128, 4, 256])
                kqb = t([128, 4, 256], BF16)
                kb = t([128, 4, 128], BF16)
                vb = t([128, 4, 128], BF16)
                ab, x1, Xg, SIG, gcf, gcr = (t([16, 128]) for _ in range(6))
                X2, E2 = t([16, 256]), t([16, 256])
                TOTb, Etot = t([16, 128]), t([16, 2])
                TS = t([128, 80])
                negg, sB, sC, dd_ = t([128, 8]), t([128, 8]), t([128, 8]), t([128, 8])
                gm4 = t([128, 4, 256])
                KQd = t([128, 4, 256], BF16)
                BCd = [t([128, 1024], BF16) for _ in range(2)]
                Vt = t([128, 512], BF16)
                MAT = t([128, 4, 512], BF16)
                pc = t([128, 8])
                X1a = t([128, 4, 256], BF16)
                NNa = t([128, 4, 128], BF16)
                Wt = [t([128, 4, 128], BF16) for _ in range(2)]
                At = [t([128, 4, 128], BF16) for _ in range(2)]
                Bt = [t([128, 4, 128], BF16) for _ in range(2)]
                abv = self.PT[3968:3984, :]
                for j in range(NJ):
                    n0 = s * T + j * 128
                    self.load_halo(qkv, 15, 12, s, j, 2, Q=self.pool)
                    for c in range(12):
                        pb = B[c // 4]
                        for k in range(5):
                            self.mm(pb[:, (c % 4) * 128:(c % 4 + 1) * 128], DWg[:, k * 12 + c, :], qkv[:, c, k:k + 128], k == 0, k == 4, [DWg, qkv], [pb])
                    for b3 in range(3):
                        self.actf(cv[:, 4 * b3:4 * b3 + 4, :], B[b3][:, :].rearrange("p (c t) -> p c t", c=4), AF.Silu, [B[b3]], [cv])
                    self.tt(G, sq[:, :, :], cv[:, 0:8, :], cv[:, 0:8, :], ALU.mult, [cv], [sq])
                    for c in range(8):
                        pb = B[c // 4]
                        self.mm(pb[:, (c % 4) * 128:(c % 4 + 1) * 128], self.ones[:, :], sq[:, c, :], True, True, [self.ones, sq], [pb])
                    for hf in range(2):
                        self.ts(V, sq[:, hf * 4:(hf + 1) * 4, :], B[hf][:, :].rearrange("p (c t) -> p c t", c=4), EPS, None, ALU.add, None, [B[hf]], [sq])
                    self.actf(sq[:, :, :], sq[:, :, :], AF.Sqrt, [sq], [sq])
                    self.recip(sq[:, :, :], sq[:, :, :], [sq], [sq])
                    self.tt(V, kq[:, :, 0:128], cv[:, 4:8, :], sq[:, 4:8, :], ALU.mult, [cv, sq], [kq])
                    self.stt(V, kq[:, :, 128:256], cv[:, 0:4, :], 128.0 ** -0.5, sq[:, 0:4, :], ALU.mult, ALU.mult, [cv, sq], [kq])
                    self.cp(G, kqb[:, :, :], kq[:, :, :], [kq], [kqb])
                    self.cp(G, kb[:, :, :], kq[:, :, 0:128], [kq], [kb])
                    self.cp(G, vb[:, :, :], cv[:, 8:12, :], [cv], [vb])
                    pbv = B[2][:, :].bitcast(BF16)
                    for m in range(4):
                        self.tr(pbv[:, m * 128:(m + 1) * 128], vb[:, m, :], self.identb[:, :], [vb, self.identb], [B[2]])
                    self.cp(self.act, Vt[:, :], pbv[:, 0:512], [B[2]], [Vt])
                    self.dma(self.GV[s, j], Vt[:, :], [Vt], [self.dd("GV", s, j)])
                    pbk = B[3][:, :].bitcast(BF16)
                    for m in range(4):
                        self.tr(pbk[:, m * 128:(m + 1) * 128], kb[:, m, :], self.identb[:, :], [kb, self.identb], [B[3]])
                    self.dma(ab[0:16, :], abv[:, n0:n0 + 128], [], [ab])
                    self.actf(x1[0:16, :], ab[0:16, :], AF.Exp, [ab, PFl], [x1], bias=self.pfv(l, "dtb")[0:16, :], scale=1.0)
                    self.actf(x1[0:16, :], x1[0:16, :], AF.Ln, [x1, self.onec], [x1], bias=self.onec[0:16, :], scale=1.0)
                    self.ts(V, Xg[0:16, :], x1[0:16, :], negA[0:16, :], None, ALU.mult, None, [x1, negA], [Xg])
                    self.actf(SIG[0:16, :], ab[0:16, :], AF.Sigmoid, [ab], [SIG])
                    self.op(V, lambda: nc.vector.tensor_tensor_scan(out=gcf[0:16, :], data0=self.RM[0:16, :], data1=Xg[0:16, :], initial=0.0,
                                                                    op0=ALU.mult, op1=ALU.add), [self.RM, Xg], [gcf])
                    gcfv = gcf[0:16, :].rearrange("p (c t) -> p c t", t=64)
                    totb = gcfv[:, :, 63:64].to_broadcast([16, 2, 64])
                    self.cp(V, TOTb[0:16, :].rearrange("p (c t) -> p c t", t=64), totb, [gcf], [TOTb])
                    self.tt(V, gcr[0:16, :], TOTb[0:16, :], gcf[0:16, :], ALU.subtract, [TOTb, gcf], [gcr])
                    self.tt(V, X2[0:16, 128:256], gcr[0:16, :], Xg[0:16, :], ALU.add, [gcr, Xg], [X2])
                    self.tt(V, X2[0:16, 128:256], X2[0:16, 128:256], gcf[0:16, :], ALU.subtract, [X2, gcf], [X2])
                    self.stt(V, X2[0:16, 128:256], X2[0:16, 128:256], self.selc[0:16, :], gcf[0:16, :], ALU.mult, ALU.add, [X2, self.selc, gcf], [X2])
                    self.tt(V, X2[0:16, 0:128], X2[0:16, 128:256], Xg[0:16, :], ALU.subtract, [X2, Xg], [X2])
                    self.actf(E2[0:16, :], X2[0:16, :], AF.Exp, [X2], [E2])
                    self.actf(Etot[0:16, :], gcfv[:, :, 63], AF.Exp, [gcf], [Etot])
                    pT = B[2]
                    for q_, src in enumerate((Xg[0:16, :], SIG[0:16, :], X2[0:16, 0:128], X2[0:16, 128:256], TOTb[0:16, :])):
                        self.tr(pT[:, q_ * 16:(q_ + 1) * 16], src, self.ident[0:16, 0:16], [Xg, SIG, X2, TOTb, self.ident], [pT])
                    self.cp(V, TS[:, :], pT[:, 0:80], [pT], [TS])
                    self.actf(negg[:, :], TS[:, 0:8], AF.Exp, [TS], [negg], scale=-1.0)
                    self.tt(V, dd_[:, :], TS[:, 64:72], TS[:, 32:40], ALU.subtract, [TS], [dd_])
                    self.actf(sB[:, :], dd_[:, :], AF.Exp, [dd_], [sB])
                    self.tt(V, sB[:, :], sB[:, :], TS[:, 24:32], ALU.mult, [sB, TS], [sB])
                    self.tt(V, dd_[:, :], TS[:, 64:72], TS[:, 48:56], ALU.subtract, [TS], [dd_])
                    self.actf(sC[:, :], dd_[:, :], AF.Exp, [dd_], [sC])
                    self.tt(V, sC[:, :], sC[:, :], TS[:, 24:32], ALU.mult, [sC, TS], [sC])
                    pbk4 = pbk[:, 0:512].rearrange("p (h t) -> p h t", h=4)
                    for d in range(2):
                        self.tt(V, BCd[d][:, 0:512].rearrange("p (h t) -> p h t", h=4), pbk4, sB[:, d * 4:d * 4 + 4].unsqueeze(2).to_broadcast([128, 4, 128]), ALU.mult, [B[3], sB], [BCd[d]])
                        self.tt(V, BCd[d][:, 512:1024].rearrange("p (h t) -> p h t", h=4), pbk4, sC[:, d * 4:d * 4 + 4].unsqueeze(2).to_broadcast([128, 4, 128]), ALU.mult, [B[3], sC], [BCd[d]])
                    for d in range(2):
                        Ms = self.MU if d == 0 else self.ML
                        BC = BCd[d]
                        for h in range(4):
                            i = d * 4 + h
                            self.mm(B[h // 2][:, (h % 2) * 256:(h % 2 + 1) * 256], self.SEL[0:16, i, :], X2[0:16, :], True, True, [self.SEL, X2], [B[h // 2]])
                            self.mm(B[2 + h // 2][:, (h % 2) * 256:(h % 2 + 1) * 256], self.SEL[0:16, i, :], E2[0:16, :], True, True, [self.SEL, E2], [B[2 + h // 2]])
                        v2 = lambda p: p[:, :].rearrange("p (h t) -> p h t", h=2)
                        for q in range(2):
                            self.tt(V, gm4[:, 2 * q:2 * q + 2, :], v2(B[q]), TS[:, 32 + d * 4 + 2 * q:32 + d * 4 + 2 * q + 2].unsqueeze(2).to_broadcast([128, 2, 256]),
                                    ALU.subtract, [B[q], TS], [gm4])
                            self.tt(V, KQd[:, 2 * q:2 * q + 2, :], kq[:, 2 * q:2 * q + 2, :], v2(B[2 + q]), ALU.mult, [kq, B[2 + q]], [KQd])
                        for h in range(4):
                            self.mm(B[2][:, h * 2:(h + 1) * 2], self.SEL[0:16, d * 4 + h, :], Etot[0:16, :], True, True, [self.SEL, Etot], [B[2]])
                        self.cp(self.act, pc[:, :], B[2][:, 0:8], [B[2]], [pc])
                        self.ts(G, gm4[:, :, :], gm4[:, :, :], 0.0, None, ALU.min, None, [gm4], [gm4])
                        self.actf(gm4[:, :, :], gm4[:, :, :], AF.Exp, [gm4], [gm4])
                        self.tt(G, gm4[:, :, :], gm4[:, :, :], Ms[:, :].unsqueeze(1).to_broadcast([128, 4, 256]), ALU.mult, [gm4, Ms], [gm4])
                        for h in range(4):
                            self.mm(B[h // 2][:, (h % 2) * 256:(h % 2 + 1) * 256], kb[:, h, :], kqb[:, h, :], True, True, [kb, kqb], [B[h // 2]])
                        for q in range(2):
                            self.tt(V, gm4[:, 2 * q:2 * q + 2, :], gm4[:, 2 * q:2 * q + 2, :], v2(B[q]), ALU.mult, [gm4, B[q]], [gm4])
                        self.tt(V, X1a[:, :, :], gm4[:, :, :], TS[:, 24 + d * 4:28 + d * 4].unsqueeze(2).to_broadcast([128, 4, 256]), ALU.mult, [gm4, TS], [X1a])
                        self.tt(V, MAT[:, :, 256:512], X1a[:, :, :], negg[:, d * 4:d * 4 + 4].unsqueeze(2).to_broadcast([128, 4, 256]), ALU.mult, [X1a, negg], [MAT])
                        self.cp(G, MAT[:, :, 128:256], X1a[:, :, 128:256], [X1a], [MAT])
                        pN = B[3][:, :].bitcast(BF16)
                        for h in range(4):
                            self.tr(pN[:, h * 128:(h + 1) * 128], X1a[:, h, 0:128], self.identb[:, :], [X1a, self.identb], [B[3]])
                        self.cp(self.act, NNa[:, :, :], pN[:, 0:512].rearrange("p (h t) -> p h t", h=4), [B[3]], [NNa])
                        self.inverse_batch(X1a, NNa, MAT, 0, (B[0], B[1], B[2]), Wt, At, Bt)
                        self.dma(self.GKQ[s, d, j], KQd[:, :, :].rearrange("p m t -> p (m t)"), [KQd], [self.dd("GKQ", s, d, j)])
                        self.dma(self.GMAT[s, d, j], MAT[:, :, :].rearrange("p h t -> p (h t)"), [MAT], [self.dd("GMAT", s, d, j)])
                        self.dma(self.GBC[s, d, j], BC[:, :], [BC], [self.dd("GBC", s, d, j)])
                        self.dma(self.GPC[s, d, j], pc[:, :], [pc], [self.dd("GPC", s, d, j)])
            self.run_interleaved([lambda: stream(0), lambda: stream(1)])
            self.barrier()

    def conf_setup(self):
        self.RCs = self.dram("RCs", [512, NT], BF16)
        self.RAs = self.dram("RAs", [512, NT], BF16)
        self.RBs = self.dram("RBs", [512, NT], BF16)

    def conformer(self, l):
        V, G = self.dve, self.pool
        ps = self.ps
        PFl = self.PFt[l]
        valv = self.PT[3984:3984 + 512, :].rearrange("(c p) n -> p c n", p=128)
        gatv = self.PT[4496:4496 + 512, :].rearrange("(c p) n -> p c n", p=128)
        RCv = self.RCs.rearrange("(c p) n -> p c n", p=128)
        with ExitStack() as es:
            def t(shape, dt=F32):
                return self.sb(shape, dt, es)
            DW = t([128, 124, 128], BF16)
            cw = self.pfv(l, "cdw")
            for q in range(124):
                self.ts(V, DW[:, q, :], self.identb[:, :], cw[:, q:q + 1], None, ALU.mult, None, [self.identb, PFl], [DW])
            upW = t([128, 2, 32, 94], BF16)
            upH = t([128, 2, 62, 64], BF16)
            upC = t([128, 4, 286], BF16)
            self.memset(G, upW[:, :, :, :], 0.0, [upW])
            self.memset(G, upH[:, :, :, :], 0.0, [upH])
            self.memset(G, upC[:, :, :], 0.0, [upC])
            vl = [t([128, TL]) for _ in range(2)]
            gl = [t([128, TL]) for _ in range(2)]
            o = t([128, 4, TL])
            sq = t([128, 4, 512])
            mu, rs, var = t([128, 512]), t([128, 512]), t([128, 512])
            ob = t([128, 4, 512], BF16)
            nb = 0
            for s in range(S):
                for (seg0, W_) in ((0, TC), (TC, TL)):
                    if seg0 == 0 and l == L - 1:
                        continue
                    n0 = s * T + seg0
                    for c in range(4):
                        v_, g_ = vl[c % 2], gl[c % 2]
                        self.dma(v_[:, :W_], valv[:, c, n0:n0 + W_], [], [v_])
                        self.dma(g_[:, :W_], gatv[:, c, n0:n0 + W_], [], [g_])
                        self.actf(g_[:, :W_], g_[:, :W_], AF.Sigmoid, [g_], [g_])
                        if seg0 == 0:
                            self.tt(V, upC[:, c, 15:15 + W_], v_[:, :W_], g_[:, :W_], ALU.mult, [v_, g_], [upC])
                        elif c < 2:
                            self.tt(V, upW[:, c, :, 15:79], v_[:, :].rearrange("p (r w) -> p r w", w=64), g_[:, :].rearrange("p (r w) -> p r w", w=64),
                                    ALU.mult, [v_, g_], [upW])
                        else:
                            self.tt(V, upH[:, c - 2, 15:47, :], v_[:, :].rearrange("p (r w) -> p r w", w=64), g_[:, :].rearrange("p (r w) -> p r w", w=64),
                                    ALU.mult, [v_, g_], [upH])
                    for c in range(4):
                        if seg0 == 0:
                            pb = ps[nb % 4]; nb += 1
                            for k in range(31):
                                self.mm(pb[:, 0:W_], DW[:, k * 4 + c, :], upC[:, c, k:k + W_], k == 0, k == 30, [DW, upC], [pb])
                            self.actf(o[:, c, 0:W_], pb[:, 0:W_], AF.Identity, [pb, PFl], [o], bias=self.pfv(l, "cdwb", c), scale=1.0)
                        else:
                            for rb_ in range(4):
                                pb = ps[nb % 4]; nb += 1
                                for k in range(31):
                                    if c < 2:
                                        rhs = upW[:, c, rb_ * 8:(rb_ + 1) * 8, k:k + 64]
                                        outp = pb[:, :].rearrange("p (r w) -> p r w", w=64)
                                    else:
                                        rhs = upH[:, c - 2, rb_ * 8 + k:rb_ * 8 + k + 8, :].rearrange("p r w -> p (r w)")
                                        outp = pb[:, :]
                                    self.mm(outp, DW[:, k * 4 + c, :], rhs, k == 0, k == 30, [DW, upW if c < 2 else upH], [pb])
                                self.actf(o[:, c, rb_ * 512:(rb_ + 1) * 512], pb[:, :], AF.Identity, [pb, PFl], [o], bias=self.pfv(l, "cdwb", c), scale=1.0)
                    for t0 in range(0, W_, 512):
                        w = min(512, W_ - t0)
                        self.tt(G, sq[:, :, :w], o[:, :, t0:t0 + w], o[:, :, t0:t0 + w], ALU.mult, [o], [sq])
                        for c in range(4):
                            self.mm(ps[4][:, :w], self.ones[:, :], o[:, c, t0:t0 + w], c == 0, c == 3, [self.ones, o], [ps[4]])
                        for c in range(4):
                            self.mm(ps[5][:, :w], self.ones[:, :], sq[:, c, :w], c == 0, c == 3, [self.ones, sq], [ps[5]])
                        self.ts(V, mu[:, :w], ps[4][:, :w], 1.0 / 512, None, ALU.mult, None, [ps[4]], [mu])
                        self.tt(V, var[:, :w], mu[:, :w], mu[:, :w], ALU.mult, [mu], [var])
                        self.stt(V, var[:, :w], ps[5][:, :w], 1.0 / 512, var[:, :w], ALU.mult, ALU.subtract, [ps[5], var], [var])
                        self.ts(V, var[:, :w], var[:, :w], EPS, None, ALU.add, None, [var], [var])
                        self.actf(var[:, :w], var[:, :w], AF.Sqrt, [var], [var])
                        self.recip(rs[:, :w], var[:, :w], [var], [rs])
                        for c in range(4):
                            self.tt(V, sq[:, c, :w], o[:, c, t0:t0 + w], mu[:, :w], ALU.subtract, [o, mu], [sq])
                            self.tt(G, sq[:, c, :w], sq[:, c, :w], rs[:, :w], ALU.mult, [sq, rs], [sq])
                            self.ts(V, sq[:, c, :w], sq[:, c, :w], self.pfv(l, "clng", c), self.pfv(l, "clnb", c), ALU.mult, ALU.add, [sq, PFl], [sq])
                        self.actf(ob[:, :, :w], sq[:, :, :w], AF.Silu, [sq], [ob])
                        self.dma(RCv[:, :, n0 + t0:n0 + t0 + w], ob[:, :, :w], [ob], [self.dd("RCs", (n0 + t0) // 128)])
            self.barrier()

    def red(self, E, out, in_, op, R, W):
        self.op(E, lambda: E.be.tensor_reduce(out=out, in_=in_, axis=AX.X, op=op), R, W)

    def merge(self, l):
        V, G = self.dve, self.pool
        ps = self.ps
        PFl = self.PFt[l]
        with ExitStack() as es:
            def t(shape, dt=F32):
                return self.sb(shape, dt, es)
            wbr = t([128, 12, 1024], BF16)
            wo = t([128, 8, 1024], BF16)
            for n in range(3):
                self.dma(wbr[:, n * 4:(n + 1) * 4, :], self.w_branch[l, n].rearrange("(k p) n -> p k n", p=128), [], [wbr], Q=self.pool)
            self.dma(wo[:, :, :], self.w_out[l].rearrange("(k p) n -> p k n", p=128), [], [wo], Q=self.pool)
            pgv = [self.PT[5008 + n * 1024:5008 + (n + 1) * 1024, :].rearrange("(c p) n -> p c n", p=128) for n in range(3)]
            fm4 = lambda A: A.rearrange("(c p) n -> p c n", p=128)
            def stream(s):
                B = self.ps[4 * s:4 * s + 4]
                y0, y1, ysq = t([128, 512]), t([128, 512]), t([128, 512])
                m1, m2, m3 = t([128, 8]), t([128, 8]), t([128, 8])
                raf, bon, ga, zt = (t([128, 4, 128]) for _ in range(4))
                Rb = [t([128, 4, 128], BF16) for _ in range(3)]
                pg = t([128, 8, 128])
                macc, tmpm = t([128, 8, 128]), t([128, 8, 128])
                mb = t([128, 8, 128], BF16)
                xt = t([128, 8, 128])
                for j in range(NJ):
                    if j < 2 and l == L - 1:
                        continue
                    n0 = s * T + j * 128
                    r = 2 if j < 2 else s
                    for br in range(2):
                        DY, nm, nh, dvv, eps_ = ((self.YA, "R", 8, 64, 64e-5), (self.YB, "G", 4, 128, EPS))[br]
                        self.dma(y0[:, :], DY[0, n0:n0 + 128, :], [self.dd(nm + "Y", 0, n0 // 128)], [y0])
                        self.dma(y1[:, :], DY[1, n0:n0 + 128, :], [self.dd(nm + "Y", 1, n0 // 128)], [y1])
                        self.tt(V, y0[:, :], y0[:, :], y1[:, :], ALU.add, [y0, y1], [y0])
                        yv = y0[:, :].rearrange("p (h d) -> p h d", d=dvv)
                        self.tt(G, ysq[:, :], y0[:, :], y0[:, :], ALU.mult, [y0], [ysq])
                        self.red(V, m2[:, 0:nh], ysq[:, :].rearrange("p (h d) -> p h d", d=dvv), ALU.add, [ysq], [m2])
                        if br == 0:
                            self.red(V, m1[:, 0:nh], yv, ALU.add, [y0], [m1])
                            self.ts(V, m1[:, 0:nh], m1[:, 0:nh], 1.0 / dvv, None, ALU.mult, None, [m1], [m1])
                            self.tt(V, m3[:, 0:nh], m1[:, 0:nh], m1[:, 0:nh], ALU.mult, [m1], [m3])
                            self.stt(V, m2[:, 0:nh], m2[:, 0:nh], 1.0 / dvv, m3[:, 0:nh], ALU.mult, ALU.subtract, [m2, m3], [m2])
                            self.ts(V, m2[:, 0:nh], m2[:, 0:nh], eps_, None, ALU.add, None, [m2], [m2])
                            self.tt(V, yv, yv, m1[:, 0:nh].unsqueeze(2).to_broadcast([128, nh, dvv]), ALU.subtract, [y0, m1], [y0])
                        else:
                            self.ts(V, m2[:, 0:nh], m2[:, 0:nh], 1.0 / dvv, eps_, ALU.mult, ALU.add, [m2], [m2])
                        self.actf(m2[:, 0:nh], m2[:, 0:nh], AF.Sqrt, [m2], [m2])
                        self.recip(m2[:, 0:nh], m2[:, 0:nh], [m2], [m2])
                        self.tt(V, yv, yv, m2[:, 0:nh].unsqueeze(2).to_broadcast([128, nh, dvv]), ALU.mult, [y0, m2], [y0])
                        pb = B[br]
                        for c in range(4):
                            self.tr(pb[:, c * 128:(c + 1) * 128], y0[:, c * 128:(c + 1) * 128], self.ident[:, :], [y0, self.ident], [pb])
                        pbv = pb[:, :].rearrange("p (c t) -> p c t", c=4)
                        if br == 0:
                            for c in range(4):
                                self.ts(V, raf[:, c, :], pbv[:, c, :], self.pfv(l, "ln_g", c), self.pfv(l, "ln_b", c), ALU.mult, ALU.add, [pb, PFl], [raf])
                            self.dma(bon[:, :, :], fm4(self.BON)[:, :, n0:n0 + 128], [self.dd("BON", n0 // 128)], [bon])
                            self.dma(ga[:, :, :], fm4(self.GAs)[:, :, n0:n0 + 128], [self.dd("GAs", n0 // 128)], [ga])
                            self.tt(V, raf[:, :, :], raf[:, :, :], bon[:, :, :], ALU.add, [raf, bon], [raf])
                            self.tt(V, Rb[0][:, :, :], raf[:, :, :], ga[:, :, :], ALU.mult, [raf, ga], [Rb[0]])
                        else:
                            self.dma(zt[:, :, :], self.PTv[:, 27:31, n0:n0 + 128], [], [zt])
                            self.actf(zt[:, :, :], zt[:, :, :], AF.Silu, [zt], [zt])
                            self.stt(V, Rb[1][:, :, :], pbv, self.pfv(l, "gnorm", 0), zt[:, :, :], ALU.mult, ALU.mult, [pb, PFl, zt], [Rb[1]])
                    self.dma(Rb[2][:, :, :], fm4(self.RCs)[:, :, n0:n0 + 128], [self.dd("RCs", n0 // 128)], [Rb[2]])
                    for n in range(3):
                        self.dma(pg[:, :, :], pgv[n][:, :, n0:n0 + 128], [], [pg])
                        self.actf(pg[:, :, :], pg[:, :, :], AF.Sigmoid, [pg], [pg])
                        for m in range(8):
                            pb = B[2 + m // 4]
                            for k in range(4):
                                self.mm(pb[:, (m % 4) * 128:(m % 4 + 1) * 128], wbr[:, n * 4 + k, m * 128:(m + 1) * 128], Rb[n][:, k, :], k == 0, k == 3,
                                        [wbr, Rb[n]], [pb])
                        for hf in range(2):
                            pbv = B[2 + hf][:, :].rearrange("p (c t) -> p c t", c=4)
                            dst = macc if n == 0 else tmpm
                            self.tt(V, dst[:, hf * 4:(hf + 1) * 4, :], pbv, pg[:, hf * 4:(hf + 1) * 4, :], ALU.mult, [B[2 + hf], pg], [dst])
                        if n > 0:
                            self.tt(G, macc[:, :, :], macc[:, :, :], tmpm[:, :, :], ALU.add, [macc, tmpm], [macc])
                    self.cp(G, mb[:, :, :], macc[:, :, :], [macc], [mb])
                    self.dma(xt[:, :, :], self.XTv[:, :, n0:n0 + 128], self.dr("XT", n0, 128), [xt])
                    for m in range(8):
                        pb = B[m // 4]
                        for k in range(8):
                            self.mm(pb[:, (m % 4) * 128:(m % 4 + 1) * 128], wo[:, k, m * 128:(m + 1) * 128], mb[:, k, :], k == 0, k == 7, [wo, mb], [pb])
                    for m in range(8):
                        pb = B[m // 4]
                        self.stt(V, xt[:, m, :], pb[:, (m % 4) * 128:(m % 4 + 1) * 128], self.MOD[l][:, 16 + m, r:r + 1], xt[:, m, :], ALU.mult, ALU.add,
                                 [pb, self.MOD[l], xt], [xt])
                    self.dma(self.XTv[:, :, n0:n0 + 128], xt[:, :, :], [xt], self.dr("XT", n0, 128))
                    if f"XM{l}" in self.dbg:
                        pass
            self.run_interleaved([lambda: stream(0), lambda: stream(1)])
            self.barrier()

    def moe(self, l):
        V, G = self.dve, self.pool
        ps = self.ps
        with ExitStack() as es:
            def t(shape, dt=F32, e_=None):
                return self.sb(shape, dt, e_ or es)
            hT = t([128, 8, NT], BF16)
            WTf = t([16, NT])
            wr = t([128, 8, 16])
            rb = t([128, 16])
            self.dma(wr[:, :, :], self.w_router.rearrange("(k p) e -> p k e", p=128), [], [wr])
            self.dma(rb[:, :], self.rbias, [], [rb])
            with ExitStack() as es1:
                xts = [t([128, 8, 512], F32, es1) for _ in range(2)]
                sq = t([128, 8, 512], F32, es1)
                rs = t([128, 512], F32, es1)
                hf = t([128, 8, 512], F32, es1)
                sc, sel, sel2, eq, cm, wts = (t([128, 16], F32, es1) for _ in range(6))
                m1, m2, gs, gsel = (t([128, 4], F32, es1) for _ in range(4))
                gmx, wsum = t([128, 1], F32, es1), t([128, 1], F32, es1)
                v4 = lambda a: a[:, :].rearrange("p (g j) -> p g j", j=4)
                b4 = lambda a: a[:, :].unsqueeze(2).to_broadcast([128, 4, 4])
                for i, (n0, w, r) in enumerate(self.tiles()):
                    xt, _ = self.modulate_tile((xts[i % 2], sq, rs, None), n0, w, r, None, None, None, None)
                    for c in range(8):
                        self.stt(V, hf[:, c, :w], xt[:, c, :w], self.GF[l][:, c, r:r + 1], rs[:, :w], ALU.mult, ALU.mult, [xt, rs, self.GF[l]], [hf])
                        self.actf(hf[:, c, :w], hf[:, c, :w], AF.Identity, [hf, self.MOD[l]], [hf], bias=self.MOD[l][:, 24 + c, r:r + 1], scale=1.0)
                    self.cp(G, hT[:, :, n0:n0 + w], hf[:, :, :w], [hf], [hT])
                    for q in range(w // 128):
                        pR = ps[6]
                        for c in range(8):
                            self.mm(pR[:, 0:16], hf[:, c, q * 128:(q + 1) * 128], wr[:, c, :], c == 0, c == 7, [hf, wr], [pR])
                        self.actf(sc[:, :], pR[:, 0:16], AF.Sigmoid, [pR], [sc])
                        self.tt(V, sel[:, :], sc[:, :], rb[:, :], ALU.add, [sc, rb], [sel])
                        self.red(V, m1[:, :], v4(sel), ALU.max, [sel], [m1])
                        self.tt(V, v4(eq), v4(sel), b4(m1), ALU.is_equal, [sel, m1], [eq])
                        self.stt(V, sel2[:, :], eq[:, :], -1e9, sel[:, :], ALU.mult, ALU.add, [eq, sel], [sel2])
                        self.red(V, m2[:, :], v4(sel2), ALU.max, [sel2], [m2])
                        self.tt(V, gs[:, :], m1[:, :], m2[:, :], ALU.add, [m1, m2], [gs])
                        self.red(V, gmx[:, :], gs[:, :], ALU.max, [gs], [gmx])
                        self.ts(V, gsel[:, :], gs[:, :], gmx[:, 0:1], None, ALU.is_equal, None, [gs, gmx], [gsel])
                        self.tt(V, v4(cm), v4(sel), b4(m2), ALU.is_ge, [sel, m2], [cm])
                        self.tt(V, v4(cm), v4(cm), b4(gsel), ALU.mult, [cm, gsel], [cm])
                        self.tt(V, wts[:, :], sc[:, :], cm[:, :], ALU.mult, [sc, cm], [wts])
                        self.red(V, wsum[:, :], wts[:, :], ALU.add, [wts], [wsum])
                        self.recip(wsum[:, :], wsum[:, :], [wsum], [wsum])
                        self.ts(V, wts[:, :], wts[:, :], wsum[:, 0:1], None, ALU.mult, None, [wts, wsum], [wts])
                        pT = ps[7]
                        self.tr(pT[0:16, 0:128], wts[:, :], self.ident[:, :], [wts, self.ident], [pT])
                        self.cp(self.act, WTf[0:16, n0 + q * 128:n0 + (q + 1) * 128], pT[0:16, 0:128], [pT], [WTf])
                self.barrier()
            TG = 1152
            TW = 384
            yacc = t([128, 8, TG])
            wgb = [t([128, 8, 512], BF16) for _ in range(2)]
            wub = [t([128, 8, 512], BF16) for _ in range(2)]
            wdb = [t([128, 4, 1024], BF16) for _ in range(2)]
            wtb = t([128, TW])
            sg = [t([128, TW]) for _ in range(2)]
            actb = t([128, 4, TW], BF16)
            xt2 = t([128, 8, 128])
            def loadw(e):
                for (src, dstt) in ((self.weg[l, e], wgb[e % 2]), (self.weu[l, e], wub[e % 2]), (self.wed[l, e], wdb[e % 2])):
                    self.dma(dstt[:, :, :], src.rearrange("(k p) n -> p k n", p=128), [], [dstt], Q=self.pool)
            loadw(0)
            for g in range(NT // TG):
                for e in range(16):
                    gb, ub, db = wgb[e % 2], wub[e % 2], wdb[e % 2]
                    if not (g == NT // TG - 1 and e == 15):
                        loadw((e + 1) % 16)
                    for tt_ in range(TG // TW):
                        n0 = g * TG + tt_ * TW
                        self.mm(ps[4][:, :TW], self.SEL[0:16, e, :], WTf[0:16, n0:n0 + TW], True, True, [self.SEL, WTf], [ps[4]])
                        self.cp(self.act, wtb[:, :], ps[4][:, :TW], [ps[4]], [wtb])
                        for hc in range(4):
                            pg_, pu_ = ps[(hc % 2) * 2], ps[(hc % 2) * 2 + 1]
                            for k in range(8):
                                self.mm(pg_[:, :TW], gb[:, k, hc * 128:(hc + 1) * 128], hT[:, k, n0:n0 + TW], k == 0, k == 7, [gb, hT], [pg_])
                            for k in range(8):
                                self.mm(pu_[:, :TW], ub[:, k, hc * 128:(hc + 1) * 128], hT[:, k, n0:n0 + TW], k == 0, k == 7, [ub, hT], [pu_])
                            sg_ = sg[hc % 2]
                            self.actf(sg_[:, :], pg_[:, :TW], AF.Silu, [pg_], [sg_])
                            self.tt(V, sg_[:, :], sg_[:, :], pu_[:, :TW], ALU.mult, [sg_, pu_], [sg_])
                            self.tt(G, actb[:, hc, :], sg_[:, :], wtb[:, :], ALU.mult, [sg_, wtb], [actb])
                        for m in range(8):
                            pd = ps[4 + m % 4]
                            for hc in range(4):
                                self.mm(pd[:, :TW], db[:, hc, m * 128:(m + 1) * 128], actb[:, hc, :], hc == 0, hc == 3, [db, actb], [pd])
                            dst = yacc[:, m, tt_ * TW:(tt_ + 1) * TW]
                            if e == 0:
                                self.cp(self.act, dst, pd[:, :TW], [pd], [yacc])
                            else:
                                self.tt(V, dst, dst, pd[:, :TW], ALU.add, [yacc, pd], [yacc])
                for p_ in range(TG // 128):
                    n0 = g * TG + p_ * 128
                    tq = n0 % T
                    r = 2 if tq < TC else n0 // T
                    self.dma(xt2[:, :, :], self.XTv[:, :, n0:n0 + 128], self.dr("XT", n0, 128), [xt2])
                    for m in range(8):
                        self.stt(V, xt2[:, m, :], yacc[:, m, p_ * 128:(p_ + 1) * 128], self.MOD[l][:, 40 + m, r:r + 1], xt2[:, m, :], ALU.mult, ALU.add,
                                 [yacc, self.MOD[l], xt2], [xt2])
                    self.dma(self.XTv[:, :, n0:n0 + 128], xt2[:, :, :], [xt2], self.dr("XT", n0, 128))
            self.barrier()

    def final(self):
        V, G = self.dve, self.pool
        ps = self.ps
        with ExitStack() as es:
            def t(shape, dt=F32):
                return self.sb(shape, dt, es)
            xts = [t([128, 8, 128]) for _ in range(2)]
            sq = t([128, 8, 128])
            rs = t([128, 128])
            tmp = t([128, 8, 128])
            os_ = [t([128, 1024]) for _ in range(2)]
            i = 0
            for s in range(S):
                for j in range(2, NJ):
                    n0 = s * T + j * 128
                    xt = xts[i % 2]; o = os_[i % 2]; i += 1
                    self.dma(xt[:, :, :], self.XTv[:, :, n0:n0 + 128], self.dr("XT", n0, 128), [xt])
                    self.actf(sq[:, :, :], xt[:, :, :], AF.Square, [xt], [sq])
                    for c in range(8):
                        self.mm(ps[0][:, 0:128], self.ones[:, :], sq[:, c, :], c == 0, c == 7, [self.ones, sq], [ps[0]])
                    self.ts(V, rs[:, :], ps[0][:, 0:128], 1.0 / D, EPS, ALU.mult, ALU.add, [ps[0]], [rs])
                    self.actf(rs[:, :], rs[:, :], AF.Sqrt, [rs], [rs])
                    self.recip(rs[:, :], rs[:, :], [rs], [rs])
                    for c in range(8):
                        self.stt(V, tmp[:, c, :], xt[:, c, :], self.pfv(0, "norm_final", c), rs[:, :], ALU.mult, ALU.mult, [xt, rs, self.PFt[0]], [tmp])
                    for c in range(8):
                        pb = ps[1 + c // 4]
                        self.tr(pb[:, (c % 4) * 128:(c % 4 + 1) * 128], tmp[:, c, :], self.ident[:, :], [tmp, self.ident], [pb])
                    self.cp(self.act, o[:, 0:512], ps[1][:, :], [ps[1]], [o])
                    self.cp(V, o[:, 512:1024], ps[2][:, :], [ps[2]], [o])
                    self.dma(self.y[s, (j - 2) * 128:(j - 1) * 128, :], o[:, :], [o], [self.dd("y", s, j)])
            self.barrier()

    def rwkv_scan(self, l, gdn=False):
        V, G = self.dve, self.pool
        ps = self.ps
        nh = 4 if gdn else 8
        dv = 512 // nh
        DKQ, DMAT, DBC, DV_, DPC, DY, nm = ((self.GKQ, self.GMAT, self.GBC, self.GV, self.GPC, self.YB, 'G') if gdn else (self.RKQ, self.RMAT, self.RBC, self.RV, self.RPC, self.YA, 'R'))
        with ExitStack() as es:
            def t(shape, dt=F32):
                return self.sb(shape, dt, es)
            chains = [(s, d) for s in range(S) for d in range(2)]
            order = {0: list(range(NJ)), 1: [1, 0] + list(range(NJ - 1, 1, -1))}
            bufs = []
            for _ in chains:
                ld = [(t([128, 4, 256], BF16), t([128, nh, 512], BF16), t([128, 1024], BF16), t([128, 512], BF16), t([128, 8])) for _ in range(2)]
                bufs.append(dict(ld=ld, H=t([128, 4, dv]), Hz=t([128, nh, dv], BF16), Rn=t([128, nh, dv], BF16),
                                 Ubz=[t([128, nh, dv], BF16) for _ in range(2)], Vz=[t([128, 512], BF16) for _ in range(2)],
                                 Yt=t([128, 512])))
            def chain(ci, s, d):
                for i in range(NJ):
                    j = order[d][i]
                    n0 = s * T + j * 128
                    b = bufs[ci]
                    KQ, MAT, BC, Vt, pc = b["ld"][i % 2]
                    H, Hz, Rn, Ubz, Vz, Yt = b["H"], b["Hz"], b["Rn"], b["Ubz"], b["Vz"], b["Yt"]
                    self.dma(KQ[:, :, :].rearrange("p m t -> p (m t)"), DKQ[s, d, j], [self.dd(nm + "KQ", s, d, j)], [KQ])
                    self.dma(MAT[:, :, :].rearrange("p h t -> p (h t)"), DMAT[s, d, j], [self.dd(nm + "MAT", s, d, j)], [MAT])
                    self.dma(BC[:, :], DBC[s, d, j], [self.dd(nm + "BC", s, d, j)], [BC])
                    self.dma(Vt[:, :], DV_[s, j], [self.dd(nm + "V", s, j)], [Vt])
                    self.dma(pc[:, :], DPC[s, d, j], [self.dd(nm + "PC", s, d, j)], [pc])
                    if i == 0:
                        self.memset(G, H[:, :, :], 0.0, [H])
                        self.memset(G, Hz[:, :, :], 0.0, [Hz])
                        self.memset(G, Rn[:, :, :], 0.0, [Rn])
                        for c in range(2):
                            self.memset(G, Ubz[c][:, :, :], 0.0, [Ubz[c]])
                            self.memset(G, Vz[c][:, :], 0.0, [Vz[c]])
                    for c in range(2):
                        self.cp(G, Vz[c][c * 64:c * 64 + 64, :], Vt[c * 64:c * 64 + 64, :], [Vt], [Vz[c]])
                    pA, pB = ps[2 * ci], ps[2 * ci + 1]
                    pAv = pA[:, :].rearrange("p (h v) -> p h v", v=dv)
                    pBv = pB[:, :].rearrange("p (h v) -> p h v", v=dv)
                    if not gdn:
                        pBe = pB[:, :].rearrange("p (m e v) -> p m e v", e=2, v=64)
                        Hze = Hz[:, :, :].rearrange("p (m e) v -> p m e v", e=2)
                    pcv = pc[:, :].rearrange("p (m c) -> p m c", c=2)
                    for c in ([0, 1] if d == 0 else [1, 0]):
                        cs = slice(c * 64, c * 64 + 64)
                        Ub = Ubz[c]
                        for h in range(nh):
                            m = h if gdn else h // 2
                            self.mm(pAv[:, h, :], KQ[:, m, 0:128], Hz[:, h, :], True, False, [KQ, Hz], [pA])
                            self.mm(pAv[:, h, :], MAT[:, h, 256:384], Vt[:, h * dv:(h + 1) * dv], False, True, [MAT, Vt], [pA])
                        self.ts(V, Rn[cs, :, :], pAv[cs, :, :], -1.0, None, ALU.mult, None, [pA], [Rn])
                        for h in range(nh):
                            self.mm(pBv[:, h, :], MAT[:, h, 0:128], Rn[:, h, :], True, True, [MAT, Rn], [pB])
                        self.cp(self.act, Ub[cs, :, :], pBv[cs, :, :], [pB], [Ub])
                        for h in range(nh):
                            m = h if gdn else h // 2
                            self.mm(pAv[:, h, :], KQ[:, m, 128:256], Hz[:, h, :], True, False, [KQ, Hz], [pA])
                            self.mm(pAv[:, h, :], MAT[:, h, 128:256], Ub[:, h, :], False, False, [MAT, Ub], [pA])
                            self.mm(pAv[:, h, :], MAT[:, h, 384:512], Vt[:, h * dv:(h + 1) * dv], False, True, [MAT, Vt], [pA])
                        self.cp(V, Yt[cs, :], pA[cs, :], [pA], [Yt])
                        for h in range(nh):
                            m = h if gdn else h // 2
                            self.mm(pBv[:, h, :], BC[:, m * 128:(m + 1) * 128], Ub[:, h, :], True, False, [BC, Ub], [pB])
                            self.mm(pBv[:, h, :], BC[:, 512 + m * 128:512 + (m + 1) * 128], Vz[c][:, h * dv:(h + 1) * dv], False, True, [BC, Vz[c]], [pB])
                        if gdn:
                            self.tt(V, H[:, :, :], H[:, :, :], pcv[:, :, c:c + 1].to_broadcast([128, 4, dv]), ALU.mult, [H, pc], [H])
                            self.tt(V, H[:, :, :], H[:, :, :], pBv[:, :, :], ALU.add, [H, pB], [H])
                            self.cp(self.act, Hz[:, :, :], H[:, :, :], [H], [Hz])
                        else:
                            for e in range(2):
                                rows = slice(e * 64, e * 64 + 64)
                                self.tt(V, H[rows, :, :], H[rows, :, :], pBe[rows, :, e, :], ALU.add, [H, pB], [H])
                            self.tt(V, H[:, :, :], H[:, :, :], pcv[:, :, c:c + 1].to_broadcast([128, 4, 64]), ALU.mult, [H, pc], [H])
                            for e in range(2):
                                rows = slice(e * 64, e * 64 + 64)
                                self.cp(self.act, Hze[rows, :, e, :], H[rows, :, :], [H], [Hz])
                    self.dma(DY[d, n0:n0 + 128, :], Yt[:, :], [Yt], [self.dd(nm + "Y", d, n0 // 128)])
            self.run_interleaved([(lambda ci=ci, s=s, d=d: chain(ci, s, d)) for ci, (s, d) in enumerate(chains)])
            self.barrier()

    def build(self):
        try:
            self.build_()
        except StopBuild:
            self.es2 = None
            self.finish()

    def build_(self):
        self.consts()
        self.gdn_setup()
        self.conf_setup()
        self.phase0()
        if self.stop == "0":
            return self.finish()
        for l in range(self.nlayers):
            self.phaseA(l)
            if self.stop == f"A{l}":
                return self.finish()
            self.phaseB(l)
            if self.stop == f"B{l}":
                return self.finish()
            self.rwkv_prep(l)
            if self.stop == f"C{l}":
                return self.finish()
            self.rwkv_scan(l)
            if self.stop == f"D{l}":
                return self.finish()
            self.gdn_prep(l)
            if self.stop == f"E{l}":
                return self.finish()
            self.rwkv_scan(l, gdn=True)
            if self.stop == f"F{l}":
                return self.finish()
            self.conformer(l)
            if self.stop == f"G{l}":
                return self.finish()
            self.merge(l)
            if self.stop == f"H{l}":
                return self.finish()
            self.moe(l)
            if self.stop == f"I{l}":
                return self.finish()
        self.final()
        self.finish()
```

```python
import threading
import numpy as np
from contextlib import ExitStack
import concourse.bass as bass
import concourse.mybir as mybir
from concourse.bass_utils import run_bass_kernel_spmd

F32 = mybir.dt.float32
BF16 = mybir.dt.bfloat16
ALU = mybir.AluOpType
AF = mybir.ActivationFunctionType
AX = mybir.AxisListType

D = 1024
S = 2
TC = 256
TL = 2048
T = TC + TL
NT = S * T
L = 2
NIN = 8080
NDS = 24
NDS_SW = 8


class Dep:
    __slots__ = ("w", "r")

    def __init__(self):
        self.w = None
        self.r = {}


class Tl:
    def __init__(self, t):
        self.t = t
        self.d = Dep()

    def __getitem__(self, k):
        return self.t[k]


class Eng:
    def __init__(self, name, be, sem):
        self.key = name
        self.be = be
        self.sem = sem
        self.n = 0
        self.waited = {}


def _d(x):
    return x.d if hasattr(x, "d") else x


class KB:
    def __init__(self, dbg=()):
        self.nc = nc = bass.Bass("TRN2", target_bir_lowering=False)
        self.es = ExitStack()
        self.dbg = set(dbg)
        e = self.es.enter_context
        self.pe = Eng("pe", nc.tensor, e(nc.semaphore("s_pe")))
        self.act = Eng("act", nc.scalar, e(nc.semaphore("s_act")))
        self.dve = Eng("dve", nc.vector, e(nc.semaphore("s_dve")))
        self.pool = Eng("pool", nc.gpsimd, e(nc.semaphore("s_pool")))
        self.sp = Eng("sp", nc.sync, e(nc.semaphore("s_sp")))
        self.engs = [self.pe, self.act, self.dve, self.pool, self.sp]
        self.dsem = [e(nc.semaphore(f"s_d{i}")) for i in range(NDS + NDS_SW)]
        self.dcnt = [0] * (NDS + NDS_SW)
        self.drr = 0
        self.drr_sw = 0
        self.ddeps = {}
        self.ntile = 0
        self.yielders = {}

    def sb(self, shape, dt=F32, es=None):
        self.ntile += 1
        t = (es or self.es).enter_context(self.nc.sbuf_tensor(f"t{self.ntile}", list(shape), dt))
        return Tl(t)

    def psum(self, shape, dt=F32, es=None):
        self.ntile += 1
        t = (es or self.es).enter_context(self.nc.psum_tensor(f"p{self.ntile}", list(shape), dt))
        return Tl(t)

    def dram(self, name, shape, dt=F32, kind=None):
        if kind is None:
            kind = "ExternalOutput" if name in self.dbg else "Internal"
        return self.nc.dram_tensor(name, list(shape), dt, kind=kind).ap()

    def dd(self, *key):
        d = self.ddeps.get(key)
        if d is None:
            d = self.ddeps[key] = Dep()
        return d

    def dr(self, name, n0, w):
        return [self.dd(name, i) for i in range(n0 // 128, (n0 + w + 127) // 128)]

    def _sync(self, E, R, W):
        need = {}

        def upd(tok):
            k, sem, val = tok
            if k not in need or need[k][1] < val:
                need[k] = (sem, val)

        for d in R:
            d = _d(d)
            if d.w:
                upd(d.w)
        for d in W:
            d = _d(d)
            if d.w:
                upd(d.w)
            for k, (sem, val) in d.r.items():
                if k != E.key:
                    upd((k, sem, val))
        for k, (sem, val) in need.items():
            if k == E.key and E is self.pe:
                continue
            if E.waited.get(k, 0) < val:
                E.be.wait_ge(sem, val)
                E.waited[k] = val

    def _mark(self, tok, R, W):
        k, sem, val = tok
        for d in R:
            _d(d).r[k] = (sem, val)
        for d in W:
            d = _d(d)
            d.w = tok
            d.r = {}

    def op(self, E, fn, R, W):
        self._sync(E, R, W)
        ins = fn()
        E.n += 1
        ins.then_inc(E.sem, 1)
        self._mark((E.key, E.sem, E.n), R, W)
        self._yield()

    def dma(self, out, in_, R, W, Q=None, **kw):
        Q = Q or self.sp
        self._sync(Q, R, W)
        sw = Q is self.pool
        if sw:
            s = NDS + self.drr_sw
            self.drr_sw = (self.drr_sw + 1) % NDS_SW
        else:
            s = self.drr
            self.drr = (s + 1) % NDS
        sem = self.dsem[s]
        k = ("d", s)
        if self.dcnt[s] > 0 and Q.waited.get(k, 0) < 16 * self.dcnt[s]:
            Q.be.wait_ge(sem, 16 * self.dcnt[s])
            Q.waited[k] = 16 * self.dcnt[s]
        Q.be.dma_start(out=out, in_=in_, **kw).then_inc(sem, 16)
        self.dcnt[s] += 1
        self._mark((k, sem, 16 * self.dcnt[s]), R, W)
        self._yield()

    def _yield(self):
        if self.yielders:
            y = self.yielders.get(threading.get_ident())
            if y:
                y()

    def run_interleaved(self, fns):
        n = len(fns)
        state = {"turn": 0, "done": [False] * n, "exc": None}
        cv = threading.Condition()

        def advance(i):
            for k in range(1, n + 1):
                nx = (i + k) % n
                if not state["done"][nx]:
                    state["turn"] = nx
                    break
            else:
                state["turn"] = -1
            cv.notify_all()

        def yielder(i):
            def y():
                with cv:
                    advance(i)
                    while state["turn"] != i:
                        cv.wait()
            return y

        def worker(i):
            with cv:
                while state["turn"] != i:
                    cv.wait()
            self.yielders[threading.get_ident()] = yielder(i)
            try:
                fns[i]()
            except BaseException as e:
                state["exc"] = e
            finally:
                self.yielders.pop(threading.get_ident(), None)
                with cv:
                    state["done"][i] = True
                    advance(i)

        ths = [threading.Thread(target=worker, args=(i,)) for i in range(n)]
        for th in ths:
            th.start()
        for th in ths:
            th.join()
        if state["exc"] is not None:
            raise state["exc"]

    def barrier(self):
        for E in self.engs:
            for E2 in self.engs:
                if E2 is not E and E2.n > 0 and E.waited.get(E2.key, 0) < E2.n:
                    E.be.wait_ge(E2.sem, E2.n)
                    E.waited[E2.key] = E2.n
            for s in range(NDS + NDS_SW):
                k = ("d", s)
                if self.dcnt[s] > 0 and E.waited.get(k, 0) < 16 * self.dcnt[s]:
                    E.be.wait_ge(self.dsem[s], 16 * self.dcnt[s])
                    E.waited[k] = 16 * self.dcnt[s]

    def mm(self, out, lhsT, rhs, start, stop, R, W):
        self.op(self.pe, lambda: self.nc.tensor.matmul(out, lhsT=lhsT, rhs=rhs, start=start, stop=stop), R, W)

    def tr(self, out, in_, ident, R, W):
        self.op(self.pe, lambda: self.nc.tensor.transpose(out, in_, ident), R, W)

    def actf(self, out, in_, func, R, W, bias=None, scale=None):
        kw = {}
        if bias is not None:
            kw["bias"] = bias
        if scale is not None:
            kw["scale"] = scale
        self.op(self.act, lambda: self.nc.scalar.activation(out=out, in_=in_, func=func, **kw), R, W)

    def ts(self, E, out, in0, s1, s2, op0, op1, R, W):
        if op1 is None:
            self.op(E, lambda: E.be.tensor_scalar(out=out, in0=in0, scalar1=s1, scalar2=None, op0=op0), R, W)
        else:
            self.op(E, lambda: E.be.tensor_scalar(out=out, in0=in0, scalar1=s1, scalar2=s2, op0=op0, op1=op1), R, W)

    def tt(self, E, out, in0, in1, op, R, W):
        self.op(E, lambda: E.be.tensor_tensor(out=out, in0=in0, in1=in1, op=op), R, W)

    def stt(self, E, out, in0, scalar, in1, op0, op1, R, W):
        self.op(E, lambda: E.be.scalar_tensor_tensor(out=out, in0=in0, scalar=scalar, in1=in1, op0=op0, op1=op1), R, W)

    def cp(self, E, out, in_, R, W):
        if E is self.act:
            self.op(E, lambda: self.nc.scalar.copy(out=out, in_=in_), R, W)
        else:
            self.op(E, lambda: E.be.tensor_copy(out=out, in_=in_), R, W)

    def recip(self, out, in_, R, W):
        self.op(self.dve, lambda: self.nc.vector.reciprocal(out=out, in_=in_), R, W)

    def memset(self, E, ap, v, W):
        self.op(E, lambda: E.be.memset(ap, v), [], W)


class PF:
    def __init__(self):
        self.cols = {}
        self.n = 0

    def add(self, name, nch):
        self.cols[name] = (self.n, nch)
        self.n += nch
        return self.cols[name][0]


def pf_layout():
    pf = PF()
    for nm, nch in [("norm_mix", 8), ("norm_ffn", 8), ("b_ada", 48), ("mu0", 15), ("mu1", 15),
                    ("w0_0", 4), ("w0_1", 4), ("a0_0", 4), ("a0_1", 4), ("kk", 4), ("ka", 4), ("rk", 4),
                    ("ln_g", 4), ("ln_b", 4), ("gconv", 60), ("gnorm", 1), ("alog", 1), ("dtb", 1),
                    ("cdw", 124), ("cdwb", 4), ("clng", 4), ("clnb", 4), ("norm_final", 8)]:
        pf.add(nm, nch)
    return pf


def _fm(v, nch):
    return np.ascontiguousarray(np.asarray(v, np.float32).reshape(nch, 128).T)


def pack_pf(inp, l):
    pf = pf_layout()
    out = np.zeros((128, pf.n), np.float32)

    def put(nm, arr):
        o, n = pf.cols[nm]
        out[:, o:o + n] = arr

    put("norm_mix", _fm(inp["norm_mix"][l], 8))
    put("norm_ffn", _fm(inp["norm_ffn"][l], 8))
    put("b_ada", _fm(inp["b_ada"][l], 48))
    put("mu0", _fm(inp["rwkv_mu"][l, 0], 15))
    put("mu1", _fm(inp["rwkv_mu"][l, 1], 15))
    for d in range(2):
        put(f"w0_{d}", _fm(inp["rwkv_w0"][l, d], 4))
        put(f"a0_{d}", _fm(inp["rwkv_a0"][l, d], 4))
    put("kk", _fm(inp["rwkv_kk"][l], 4))
    put("ka", _fm(inp["rwkv_ka"][l], 4))
    put("rk", _fm(inp["rwkv_rk"][l].reshape(-1), 4))
    put("ln_g", _fm(inp["rwkv_ln_g"][l], 4))
    put("ln_b", _fm(inp["rwkv_ln_b"][l], 4))
    gc = np.concatenate([_fm(inp["gdn_conv"][l, k], 12) for k in range(5)], axis=1)
    put("gconv", gc)
    put("gnorm", _fm(inp["gdn_norm"][l], 1))
    al = np.zeros((128, 1), np.float32); al[0:8, 0] = np.asarray(inp["gdn_A_log"][l]).reshape(-1)
    db = np.zeros((128, 1), np.float32); db[0:8, 0] = np.asarray(inp["gdn_dt_bias"][l]).reshape(-1)
    put("alog", al)
    put("dtb", db)
    cd = np.concatenate([_fm(inp["conf_dw"][l, k], 4) for k in range(31)], axis=1)
    put("cdw", cd)
    put("cdwb", _fm(inp["conf_dw_b"][l], 4))
    put("clng", _fm(inp["conf_ln_g"][l], 4))
    put("clnb", _fm(inp["conf_ln_b"][l], 4))
    put("norm_final", _fm(inp["norm_final"], 8))
    return out


EPS = 1e-6


class Prog(KB):
    def __init__(self, dbg=(), stop=None, nlayers=L):
        super().__init__(dbg)
        self.stop = stop
        self.nlayers = nlayers
        nc = self.nc
        self.pf = pf_layout()

        def inp(name, shape):
            return nc.dram_tensor(name, list(shape), F32, kind="ExternalInput").ap()

        self.x = inp("x", [S, TL, D])
        self.ctx = inp("ctx", [S, TC, D])
        self.cvecT = inp("cvecT", [D, 3])
        self.pfp = inp("pfp", [L, 128, self.pf.n])
        self.w_ada = inp("w_ada", [L, D, 6 * D])
        self.w_in = inp("w_in", [L, D, NIN])
        self.rw2 = inp("rwkv_w2", [L, 2, 64, 512])
        self.ra2 = inp("rwkv_a2", [L, 2, 64, 512])
        self.rg2 = inp("rwkv_g2", [L, 128, 512])
        self.w_branch = inp("w_branch", [L, 3, 512, D])
        self.w_out = inp("w_out", [L, D, D])
        self.w_router = inp("w_router", [D, 16])
        self.rbias = inp("rbias", [128, 16])
        self.weg = inp("w_e_gate", [L, 16, D, 512])
        self.weu = inp("w_e_up", [L, 16, D, 512])
        self.wed = inp("w_e_down", [L, 16, 512, D])
        self.y = nc.dram_tensor("y", [S, TL, D], F32, kind="ExternalOutput").ap()
        self.XT = self.dram("XT", [D, NT])
        self.XTv = self.XT.rearrange("(c p) n -> p c n", p=128)
        self.PT = self.dram("PT", [NIN, NT])
        self.ps = [self.psum([128, 512], F32) for _ in range(8)]
        self.ident = self.sb([128, 128], F32)
        self.identb = self.sb([128, 128], BF16)
        self.ones = self.sb([128, 128], F32)
        self.PFt = [self.sb([128, self.pf.n], F32) for _ in range(L)]
        self.scT = self.sb([128, 8, 3], F32)
        self.MOD = [self.sb([128, 48, 3], F32) for _ in range(L)]
        self.GM = [self.sb([128, 8, 3], F32) for _ in range(L)]
        self.GF = [self.sb([128, 8, 3], F32) for _ in range(L)]

    def pfv(self, l, name, c=None, n=1):
        o, nch = self.pf.cols[name]
        if c is None:
            return self.PFt[l][:, o:o + nch]
        return self.PFt[l][:, o + c:o + c + n]

    def consts(self):
        nc = self.nc
        P = self.pool
        self.memset(P, self.ident[:, :], 0.0, [self.ident])
        self.op(P, lambda: nc.gpsimd.affine_select(out=self.ident[:, :], in_=self.ident[:, :], pattern=[[-1, 128]],
                                                   compare_op=ALU.not_equal, fill=1.0, base=0, channel_multiplier=1),
                [self.ident], [self.ident])
        self.cp(P, self.identb[:, :], self.ident[:, :], [self.ident], [self.identb])
        self.memset(P, self.ones[:, :], 1.0, [self.ones])
        for l in range(L):
            self.dma(self.PFt[l][:, :], self.pfp[l], [], [self.PFt[l]])
        cv = self.sb([128, 8, 3], F32)
        self.dma(cv[:, :, :], self.cvecT.rearrange("(c p) r -> p c r", p=128), [], [cv])
        self.actf(self.scT[:, :, :], cv[:, :, :], AF.Silu, [cv], [self.scT])

    def phase0(self):
        with ExitStack() as es:
            xin = [self.sb([128, 1024], F32, es) for _ in range(2)]
            xo = [self.sb([128, 8, 128], F32, es) for _ in range(2)]
            i = 0
            for s in range(S):
                for j in range(T // 128):
                    n0 = s * T + j * 128
                    src = self.ctx[s, j * 128:(j + 1) * 128, :] if j < 2 else self.x[s, (j - 2) * 128:(j - 1) * 128, :]
                    a = xin[i % 2]
                    o = xo[i % 2]
                    self.dma(a[:, :], src, [], [a])
                    for c in range(8):
                        pb = self.ps[(i % 2) * 2 + c // 4]
                        self.tr(pb[:, (c % 4) * 128:(c % 4 + 1) * 128], a[:, c * 128:(c + 1) * 128], self.ident[:, :],
                                [a, self.ident], [pb])
                    for hf in range(2):
                        pb = self.ps[(i % 2) * 2 + hf]
                        self.cp(self.act if hf == 0 else self.dve, o[:, hf * 4:(hf + 1) * 4, :],
                                pb[:, :].rearrange("p (c t) -> p c t", c=4), [pb], [o])
                    self.dma(self.XTv[:, :, n0:n0 + 128], o[:, :, :], [o], self.dr("XT", n0, 128))
                    i += 1
            self.barrier()

    def phaseA(self, l):
        with ExitStack() as es:
            wa = [self.sb([128, 8, 768], F32, es) for _ in range(2)]
            pm = self.ps[0]
            wav = self.w_ada[l].rearrange("(k p) n -> p k n", p=128)
            for mg in range(8):
                w = wa[mg % 2]
                for q in range(4):
                    self.dma(w[:, q * 2:(q + 1) * 2, :], wav[:, q * 2:(q + 1) * 2, mg * 768:(mg + 1) * 768], [], [w])
                for m in range(6):
                    mm_ = mg * 6 + m
                    for k in range(8):
                        self.mm(pm[:, mm_ * 3:(mm_ + 1) * 3], w[:, k, m * 128:(m + 1) * 128], self.scT[:, k, :], k == 0, k == 7,
                                [w, self.scT], [pm])
            mod = self.MOD[l]
            self.tt(self.dve, mod[:, :, :], pm[:, 0:144].rearrange("p (m r) -> p m r", r=3),
                    self.pfv(l, "b_ada").unsqueeze(2).to_broadcast([128, 48, 3]), ALU.add, [pm, self.PFt[l]], [mod])
            for (G, mi, nm) in ((self.GM[l], 1, "norm_mix"), (self.GF[l], 4, "norm_ffn")):
                self.ts(self.dve, G[:, :, :], mod[:, mi * 8:(mi + 1) * 8, :], 1.0, None, ALU.add, None, [mod], [G])
                self.dump(f"G1{l}{mi}", G, G[:, :, :], [128, 8, 3])
                self.tt(self.dve, G[:, :, :], G[:, :, :], self.pfv(l, nm).unsqueeze(2).to_broadcast([128, 8, 3]), ALU.mult,
                        [G, self.PFt[l]], [G])
            self.dump(f"MOD{l}", mod, mod[:, :, :], [128, 48, 3])
            self.dump(f"GM{l}", self.GM[l], self.GM[l][:, :, :], [128, 8, 3])
            self.barrier()

    def tiles(self):
        out = []
        for s in range(S):
            out.append((s * T, TC, 2))
            for j in range(TL // 512):
                out.append((s * T + TC + j * 512, 512, s))
        return out

    def modulate_tile(self, es_bufs, n0, w, r, G, shift_col, mod, out_fn):
        xt, sq, rs, tmp = es_bufs
        psS = self.ps[7]
        self.dma(xt[:, :, :w], self.XTv[:, :, n0:n0 + w], self.dr("XT", n0, w), [xt])
        self.actf(sq[:, :, :w], xt[:, :, :w], AF.Square, [xt], [sq])
        for c in range(8):
            self.mm(psS[:, :w], self.ones[:, :], sq[:, c, :w], c == 0, c == 7, [self.ones, sq], [psS])
        self.ts(self.dve, rs[:, :w], psS[:, :w], 1.0 / D, EPS, ALU.mult, ALU.add, [psS], [rs])
        self.actf(rs[:, :w], rs[:, :w], AF.Sqrt, [rs], [rs])
        self.recip(rs[:, :w], rs[:, :w], [rs], [rs])
        return xt, rs

    def phaseB(self, l):
        with ExitStack() as es:
            hT = self.sb([128, 8, NT], BF16, es)
            with ExitStack() as es1:
                xts = [self.sb([128, 8, 512], F32, es1) for _ in range(2)]
                sq = self.sb([128, 8, 512], F32, es1)
                rs = self.sb([128, 512], F32, es1)
                tmps = [self.sb([128, 512], F32, es1) for _ in range(2)]
                for i, (n0, w, r) in enumerate(self.tiles()):
                    xt, _ = self.modulate_tile((xts[i % 2], sq, rs, None), n0, w, r, None, None, None, None)
                    for c in range(8):
                        tmp = tmps[c % 2]
                        self.stt(self.dve, tmp[:, :w], xt[:, c, :w], self.GM[l][:, c, r:r + 1], rs[:, :w], ALU.mult, ALU.mult,
                                 [xt, rs, self.GM[l]], [tmp])
                        self.actf(hT[:, c, n0:n0 + w], tmp[:, :w], AF.Identity, [tmp, self.MOD[l]], [hT],
                                  bias=self.MOD[l][:, c, r:r + 1], scale=1.0)
                if "HT" in self.dbg:
                    hd = self.dram("HT", [128, 8, NT], BF16)
                    self.dma(hd, hT[:, :, :], [hT], [self.dd("HTd")])
                self.barrier()
            wbf = [self.sb([128, 8, 1024], BF16, es) for _ in range(2)]
            ost = [self.sb([128, 512], F32, es) for _ in range(4)]
            no = 0
            def loadw(g):
                c0 = g * 1024
                cw = min(1024, NIN - c0)
                self.dma(wbf[g % 2][:, :, :cw], self.w_in[l][:, c0:c0 + cw].rearrange("(k p) n -> p k n", p=128), [], [wbf[g % 2]], Q=self.pool)
            loadw(0)
            for g in range(8):
                c0 = g * 1024
                cw = min(1024, NIN - c0)
                wb = wbf[g % 2]
                if g < 7:
                    loadw(g + 1)
                nm = (cw + 127) // 128
                for tt_ in range(NT // 512):
                    n0 = tt_ * 512
                    for m in range(nm):
                        mw = min(128, cw - m * 128)
                        pb = self.ps[no % 6]
                        for k in range(8):
                            self.mm(pb[:mw, :], wb[:, k, m * 128:m * 128 + mw], hT[:, k, n0:n0 + 512], k == 0, k == 7,
                                    [wb, hT], [pb])
                        o = ost[no % 4]
                        self.cp(self.act if no % 2 == 0 else self.dve, o[:mw, :], pb[:mw, :], [pb], [o])
                        self.dma(self.PT[c0 + m * 128:c0 + m * 128 + mw, n0:n0 + 512], o[:mw, :], [o],
                                 [self.dd("PT", (c0 + m * 128) // 128, j) for j in range(n0 // 128, n0 // 128 + 4)])
                        no += 1
            self.barrier()

    def dump(self, name, tile, ap, shape, dt=F32):
        if name in self.dbg:
            d = self.dram(name, shape, dt)
            self.dma(d, ap, [tile], [self.dd(name)])

    def finish(self):
        self.barrier()

    def build(self):
        self.consts()
        self.phase0()
        if self.stop == "0":
            return self.finish()
        for l in range(self.nlayers):
            self.phaseA(l)
            if self.stop == f"A{l}":
                return self.finish()
            self.phaseB(l)
            if self.stop == f"B{l}":
                return self.finish()
        self.finish()


def make_in_maps(inp):
    ncores = 8
    pfp = np.stack([pack_pf(inp, l) for l in range(L)])
    rbias = np.ascontiguousarray(np.broadcast_to(np.asarray(inp["router_bias"], np.float32)[None, :], (128, 16)))
    maps = []
    for i in range(ncores):
        cv = np.stack([inp["c"][2 * i], inp["c"][2 * i + 1], inp["c_ctx"]], axis=1).astype(np.float32)
        m = {
            "x": np.ascontiguousarray(inp["x"][2 * i:2 * i + 2]),
            "ctx": np.ascontiguousarray(inp["ctx"][2 * i:2 * i + 2]),
            "cvecT": np.ascontiguousarray(cv),
            "pfp": pfp, "rbias": rbias,
        }
        for k in ("w_ada", "w_in", "rwkv_w2", "rwkv_a2", "rwkv_g2", "w_branch", "w_out", "w_router",
                  "w_e_gate", "w_e_up", "w_e_down"):
            m[k] = np.ascontiguousarray(inp[k], dtype=np.float32)
        maps.append(m)
    return maps


def kernel(**inputs):
    inp = {k: np.asarray(v) for k, v in inputs.items()}
    prog = Prog2()
    prog.build()
    maps = make_in_maps(inp)
    res = run_bass_kernel_spmd(prog.nc, maps, core_ids=list(range(8)))
    return np.concatenate([r["y"] for r in res.results], axis=0).astype(np.float32)


CDEC = 0.6065306597126334
NJ = T // 128


def seg_bounds(j):
    return (j == 0 or j == 2), (j == 1 or j == NJ - 1)


class StopBuild(Exception):
    pass


class Prog2(Prog):
    cut = None

    def ck(self, n):
        if self.cut == n:
            raise StopBuild()

    def __init__(self, **kw):
        super().__init__(**kw)
        self.PTv = self.PT[0:8064, :].rearrange("(c p) n -> p c n", p=128)
        self.RKQ = self.dram("RKQ", [S, 2, NJ, 128, 4 * 256], BF16)
        self.RMAT = self.dram("RMAT", [S, 2, NJ, 128, 8 * 512], BF16)
        self.RBC = self.dram("RBC", [S, 2, NJ, 128, 1024], BF16)
        self.RV = self.dram("RV", [S, NJ, 128, 512], BF16)
        self.RPC = self.dram("RPC", [S, 2, NJ, 128, 8], F32)
        self.GAs = self.dram("GAs", [512, NT])
        self.BON = self.dram("BON", [512, NT])
        self.YA = self.dram("YA", [2, NT, 512])
        self.bones = self.sb([128, 128], F32)
        self.MU = self.sb([128, 256], F32)
        self.ML = self.sb([128, 256], F32)
        self.RM = self.sb([128, 128], F32)

    def consts(self):
        super().consts()
        nc = self.nc
        P = self.pool
        self.memset(P, self.bones[:, :], 0.0, [self.bones])
        self.memset(P, self.bones[0:64, 0:64], 1.0, [self.bones])
        self.memset(P, self.bones[64:128, 64:128], 1.0, [self.bones])
        self.memset(P, self.RM[:, :], 1.0, [self.RM])
        self.memset(P, self.RM[:, 0:1], 0.0, [self.RM])
        self.memset(P, self.RM[:, 64:65], 0.0, [self.RM])
        for (Mt, off, cmp_, sg) in ((self.MU, 0, ALU.is_gt, 1), (self.MU, 128, ALU.is_ge, 1), (self.ML, 0, ALU.is_gt, -1), (self.ML, 128, ALU.is_ge, -1)):
            sl = Mt[:, off:off + 128]
            self.memset(P, sl, 1.0, [Mt])
            self.op(P, lambda sl=sl, cmp_=cmp_, sg=sg: nc.gpsimd.affine_select(out=sl, in_=sl, pattern=[[sg, 128]], compare_op=cmp_, fill=0.0,
                                                                              base=0, channel_multiplier=-sg), [Mt], [Mt])
            self.memset(P, Mt[0:64, off + 64:off + 128], 0.0, [Mt])
            self.memset(P, Mt[64:128, off:off + 64], 0.0, [Mt])

    def load_halo(self, dst, c0, nch, s, j, hw, Q=None):
        n0 = s * T + j * 128
        lb, rb = seg_bounds(j)
        lo = 0 if lb else hw
        hi = 0 if rb else hw
        if lb:
            self.memset(self.pool, dst[:, :, 0:hw], 0.0, [dst])
        if rb:
            self.memset(self.pool, dst[:, :, 128 + hw:128 + 2 * hw], 0.0, [dst])
        deps = [self.dd("PT", c, i) for c in range(c0, c0 + nch) for i in range((n0 - lo) // 128, (n0 + 128 + hi - 1) // 128 + 1)]
        self.dma(dst[:, :, hw - lo:hw + 128 + hi], self.PTv[:, c0:c0 + nch, n0 - lo:n0 + 128 + hi], deps, [dst], Q=Q)

    def inverse(self, X1, NN, MAT, h, Wt, At, Bt, pA, pB_, pC):
        V, G = self.dve, self.pool
        W, A, B = Wt[0], At[0], Bt[0]
        self.tt(G, W[:, :], self.identb[:, :], X1[:, 0:128], ALU.subtract, [self.identb, X1], [W])
        self.mm(pA[:, 0:128], X1[:, 0:128], NN[:, :], True, True, [X1, NN], [pA])
        self.mm(pB_[:, 0:128], NN[:, :], X1[:, 0:128], True, True, [X1, NN], [pB_])
        self.cp(self.act, A[:, :], pA[:, 0:128], [pA], [A])
        self.cp(self.act, B[:, :], pB_[:, 0:128], [pB_], [B])
        for it in range(5):
            W2, A2, B2 = Wt[(it + 1) % 2], At[(it + 1) % 2], Bt[(it + 1) % 2]
            self.mm(pC[:, 0:128], A[:, :], W[:, :], True, True, [A, W], [pC])
            if it < 4:
                self.mm(pA[:, 0:128], B[:, :], A[:, :], True, True, [A, B], [pA])
                self.mm(pB_[:, 0:128], A[:, :], B[:, :], True, True, [A, B], [pB_])
            dstW = W2[:, :] if it < 4 else MAT[:, h, 0:128]
            self.tt(V, dstW, W[:, :], pC[:, 0:128], ALU.add, [W, pC], [W2 if it < 4 else MAT])
            if it < 4:
                self.cp(self.act, A2[:, :], pA[:, 0:128], [pA], [A2])
                self.cp(self.act, B2[:, :], pB_[:, 0:128], [pB_], [B2])
            W, A, B = W2, A2, B2

    def inverse_batch(self, X1a, NNa, MAT, h0, banks, Wt, At, Bt):
        V, G = self.dve, self.pool
        psW, psA, psB = banks
        hs = slice(h0, h0 + 4)

        def reg(p, i):
            return p[:, i * 128:(i + 1) * 128]

        def bv(p):
            return p[:, :].rearrange("p (h t) -> p h t", h=4)
        W, A, B = Wt[0], At[0], Bt[0]
        self.tt(G, W[:, :, :], self.identb[:, :].unsqueeze(1).to_broadcast([128, 4, 128]), X1a[:, hs, 0:128], ALU.subtract, [self.identb, X1a], [W])
        for i in range(4):
            self.mm(reg(psA, i), X1a[:, h0 + i, 0:128], NNa[:, h0 + i, :], True, True, [X1a, NNa], [psA])
        for i in range(4):
            self.mm(reg(psB, i), NNa[:, h0 + i, :], X1a[:, h0 + i, 0:128], True, True, [X1a, NNa], [psB])
        self.cp(self.act, A[:, :, :], bv(psA), [psA], [A])
        self.cp(self.act, B[:, :, :], bv(psB), [psB], [B])
        for it in range(5):
            W2, A2, B2 = Wt[(it + 1) % 2], At[(it + 1) % 2], Bt[(it + 1) % 2]
            for i in range(4):
                self.mm(reg(psW, i), A[:, i, :], W[:, i, :], True, True, [A, W], [psW])
            if it < 4:
                for i in range(4):
                    self.mm(reg(psA, i), B[:, i, :], A[:, i, :], True, True, [A, B], [psA])
                for i in range(4):
                    self.mm(reg(psB, i), A[:, i, :], B[:, i, :], True, True, [A, B], [psB])
                self.tt(V, W2[:, :, :], W[:, :, :], bv(psW), ALU.add, [W, psW], [W2])
                self.cp(self.act, A2[:, :, :], bv(psA), [psA], [A2])
                self.cp(self.act, B2[:, :, :], bv(psB), [psB], [B2])
            else:
                self.tt(V, MAT[:, hs, 0:128], W[:, :, :], bv(psW), ALU.add, [W, psW], [MAT])
            W, A, B = W2, A2, B2

    def rwkv_prep(self, l):
        nc = self.nc
        V, G = self.dve, self.pool
        with ExitStack() as es:
            def t(shape, dt=F32):
                return self.sb(shape, dt, es)
            wtmp = t([128, 512])
            w2b, a2b, g2b = t([128, 512], BF16), t([128, 512], BF16), t([128, 512], BF16)
            for (src, dstb) in ((self.rw2[l].rearrange("d r c -> (d r) c"), w2b), (self.ra2[l].rearrange("d r c -> (d r) c"), a2b), (self.rg2[l], g2b)):
                self.dma(wtmp[:, :], src, [], [wtmp])
                self.cp(V, dstb[:, :], wtmp[:, :], [wtmp], [dstb])
            PFl = self.PFt[l]
            c0t = t([128, 15])
            self.tt(V, c0t[:, :], self.pfv(l, "mu0"), self.pfv(l, "mu1"), ALU.add, [PFl], [c0t])
            self.ts(V, c0t[:, :], c0t[:, :], -1.0, 1.0, ALU.mult, ALU.add, [c0t], [c0t])
            omka = t([128, 4])
            self.ts(V, omka[:, :], self.pfv(l, "ka"), -1.0, 1.0, ALU.mult, ALU.add, [PFl], [omka])

            def bc(ap, n):
                return ap.unsqueeze(2).to_broadcast([128, n, 128])

            DWr = t([128, 45, 128], BF16)
            for c_ in range(15):
                for k_, col in enumerate((self.pfv(l, "mu0", c_), c0t[:, c_:c_ + 1], self.pfv(l, "mu1", c_))):
                    self.ts(V, DWr[:, k_ * 15 + c_, :], self.identb[:, :], col, None, ALU.mult, None, [self.identb, PFl, c0t], [DWr])
            def stream(s):
                pa = t([128, 15, 130], BF16)
                sh = t([128, 15, 128])
                twb, xab, sgb = t([128, 128], BF16), t([128, 128], BF16), t([128, 128], BF16)
                SW = [t([128, 4, 128]) for _ in range(2)]
                AA = [t([128, 4, 128]) for _ in range(2)]
                ga = t([128, 4, 128])
                kx, kk, tq, bon = t([128, 4, 128]), t([128, 4, 128]), t([128, 4, 128]), t([128, 4, 128])
                CS, EX, tmpa, tmpb = (t([128, 4, 128]) for _ in range(4))
                e1, e2, e3 = (t([128, 4, 128]) for _ in range(3))
                pc = t([128, 8])
                KQ = t([128, 4, 256], BF16)
                BTb, CTb = t([128, 4, 128], BF16), t([128, 4, 128], BF16)
                vb = t([128, 4, 128], BF16)
                BC = t([128, 1024], BF16)
                Vt = t([128, 512], BF16)
                MAT = t([128, 8, 512], BF16)
                X1a = t([128, 8, 256], BF16)
                NNa = t([128, 8, 128], BF16)
                BTz, CTz = t([128, 8, 128], BF16), t([128, 8, 128], BF16)
                self.memset(G, BTz[:, :, :], 0.0, [BTz])
                self.memset(G, CTz[:, :, :], 0.0, [CTz])
                Wt = [t([128, 4, 128], BF16) for _ in range(2)]
                At = [t([128, 4, 128], BF16) for _ in range(2)]
                Bt = [t([128, 4, 128], BF16) for _ in range(2)]
                Wt2 = [t([128, 4, 128], BF16) for _ in range(2)]
                At2 = [t([128, 4, 128], BF16) for _ in range(2)]
                Bt2 = [t([128, 4, 128], BF16) for _ in range(2)]
                B = self.ps[4 * s:4 * s + 4]
                for j in range(NJ):
                    n0 = s * T + j * 128
                    self.ck(1000 + s * NJ + j)
                    self.load_halo(pa, 0, 15, s, j, 1, Q=self.pool)
                    for c in range(15):
                        pb = B[c // 4]
                        for k in range(3):
                            self.mm(pb[:, (c % 4) * 128:(c % 4 + 1) * 128], DWr[:, k * 15 + c, :], pa[:, c, k:k + 128], k == 0, k == 2, [DWr, pa], [pb])
                    for b4_ in range(4):
                        n_ = 4 if b4_ < 3 else 3
                        self.cp(self.act if b4_ % 2 == 0 else V, sh[:, 4 * b4_:4 * b4_ + n_, :],
                                B[b4_][:, 0:n_ * 128].rearrange("p (c t) -> p c t", c=n_), [B[b4_]], [sh])
                    self.ck(2)
                    r_, k_, v_ = sh[:, 0:4, :], sh[:, 4:8, :], sh[:, 8:12, :]
                    self.actf(twb[:, :], sh[:, 12, :], AF.Tanh, [sh], [twb])
                    self.cp(self.act, xab[:, :], sh[:, 13, :], [sh], [xab])
                    self.actf(sgb[:, :], sh[:, 14, :], AF.Sigmoid, [sh], [sgb])
                    self.ck(3)
                    for d in range(2):
                        for (wb_, xin, dst, bname, pb) in ((w2b, twb, SW[d], f"w0_{d}", B[0]), (a2b, xab, AA[d], f"a0_{d}", B[1])):
                            for m in range(4):
                                self.mm(pb[:, m * 128:(m + 1) * 128], wb_[d * 64:(d + 1) * 64, m * 128:(m + 1) * 128],
                                        xin[d * 64:(d + 1) * 64, :], True, True, [wb_, xin], [pb])
                            for m in range(4):
                                self.actf(dst[:, m, :], pb[:, m * 128:(m + 1) * 128], AF.Sigmoid, [pb, PFl], [dst],
                                          bias=self.pfv(l, bname, m), scale=1.0)
                    for m in range(4):
                        self.mm(B[2][:, m * 128:(m + 1) * 128], g2b[:, m * 128:(m + 1) * 128], sgb[:, :], True, True, [g2b, sgb], [B[2]])
                    self.cp(self.act, ga[:, :, :], B[2][:, :].rearrange("p (m t) -> p m t", m=4), [B[2]], [ga])
                    self.dma(self.GAs.rearrange("(m p) n -> p m n", p=128)[:, :, n0:n0 + 128], ga[:, :, :], [ga], [self.dd("GAs", n0 // 128)])
                    self.ck(4)
                    self.tt(V, kx[:, :, :], k_, bc(self.pfv(l, "kk"), 4), ALU.mult, [sh, PFl], [kx])
                    self.tt(G, tq[:, :, :], kx[:, :, :], kx[:, :, :], ALU.mult, [kx], [tq])
                    for m in range(4):
                        self.mm(B[3][:, m * 128:(m + 1) * 128], self.bones[:, :], tq[:, m, :], True, True, [self.bones, tq], [B[3]])
                    self.ts(V, tq[:, :, :], B[3][:, :].rearrange("p (m t) -> p m t", m=4), EPS, None, ALU.add, None, [B[3]], [tq])
                    self.actf(tq[:, :, :], tq[:, :, :], AF.Sqrt, [tq], [tq])
                    self.recip(tq[:, :, :], tq[:, :, :], [tq], [tq])
                    self.tt(V, kk[:, :, :], kx[:, :, :], tq[:, :, :], ALU.mult, [kx, tq], [kk])
                    self.ck(5)
                    self.tt(G, bon[:, :, :], r_, k_, ALU.mult, [sh], [bon])
                    self.tt(G, bon[:, :, :], bon[:, :, :], bc(self.pfv(l, "rk"), 4), ALU.mult, [bon, PFl], [bon])
                    for m in range(4):
                        self.mm(B[0][:, m * 128:(m + 1) * 128], self.bones[:, :], bon[:, m, :], True, True, [self.bones, bon], [B[0]])
                    self.tt(V, bon[:, :, :], B[0][:, :].rearrange("p (m t) -> p m t", m=4), v_, ALU.mult, [B[0], sh], [bon])
                    self.dma(self.BON.rearrange("(m p) n -> p m n", p=128)[:, :, n0:n0 + 128], bon[:, :, :], [bon], [self.dd("BON", n0 // 128)])
                    self.ck(6)
                    self.cp(G, vb[:, :, :], v_, [sh], [vb])
                    pbv = B[1][:, :].bitcast(BF16)
                    for m in range(4):
                        self.tr(pbv[:, m * 128:(m + 1) * 128], vb[:, m, :], self.identb[:, :], [vb, self.identb], [B[1]])
                    self.cp(self.act, Vt[:, :], pbv[:, 0:512], [B[1]], [Vt])
                    self.dma(self.RV[s, j], Vt[:, :], [Vt], [self.dd("RV", s, j)])
                    for d in range(2):
                        self.ck(7)
                        self.tt(V, tmpa[:, :, :], AA[d][:, :, :], bc(self.pfv(l, "ka"), 4), ALU.mult, [AA[d], PFl], [tmpa])
                        self.tt(V, tmpa[:, :, :], tmpa[:, :, :], bc(omka[:, :], 4), ALU.add, [tmpa, omka], [tmpa])
                        self.tt(V, tmpa[:, :, :], tmpa[:, :, :], k_, ALU.mult, [tmpa, sh], [tmpa])
                        self.tt(G, tmpb[:, :, :], kk[:, :, :], AA[d][:, :, :], ALU.mult, [kk, AA[d]], [tmpb])
                        self.ck(8)
                        for m in range(4):
                            self.op(V, lambda m=m: nc.vector.tensor_tensor_scan(out=CS[:, m, :], data0=self.RM[:, :], data1=SW[d][:, m, :],
                                                                               initial=0.0, op0=ALU.mult, op1=ALU.add),
                                    [self.RM, SW[d]], [CS])
                        CSv = CS[:, :, :].rearrange("p m (c t) -> p m c t", t=64)
                        if d == 0:
                            self.tt(V, EX[:, :, :], CS[:, :, :], SW[d][:, :, :], ALU.subtract, [CS, SW[d]], [EX])
                            incl = CS
                        else:
                            tot = CSv[:, :, :, 63:64].to_broadcast([128, 4, 2, 64])
                            self.tt(V, EX[:, :, :].rearrange("p m (c t) -> p m c t", t=64), tot, CSv, ALU.subtract, [CS], [EX])
                            self.tt(V, e3[:, :, :], EX[:, :, :], SW[d][:, :, :], ALU.add, [EX, SW[d]], [e3])
                            incl = e3
                        self.ck(9)
                        self.actf(e1[:, :, :], EX[:, :, :], AF.Exp, [EX], [e1], scale=-CDEC)
                        self.actf(e2[:, :, :], incl[:, :, :], AF.Exp, [incl], [e2], scale=CDEC)
                        self.actf(e3[:, :, :], incl[:, :, :], AF.Exp, [incl], [e3], scale=-CDEC)
                        self.actf(pc[:, :].rearrange("p (m c) -> p m c", c=2), CSv[:, :, :, 63], AF.Exp, [CS], [pc], scale=-CDEC)
                        self.dma(self.RPC[s, d, j], pc[:, :], [pc], [self.dd("RPC", s, d, j)])
                        self.tt(V, KQ[:, :, 0:128], kk[:, :, :], e1[:, :, :], ALU.mult, [kk, e1], [KQ])
                        self.tt(G, KQ[:, :, 128:256], r_, e3[:, :, :], ALU.mult, [sh, e3], [KQ])
                        self.tt(V, BTb[:, :, :], tmpb[:, :, :], e2[:, :, :], ALU.mult, [tmpb, e2], [BTb])
                        self.tt(G, CTb[:, :, :], tmpa[:, :, :], e2[:, :, :], ALU.mult, [tmpa, e2], [CTb])
                        self.dma(self.RKQ[s, d, j], KQ[:, :, :].rearrange("p m t -> p (m t)"), [KQ], [self.dd("RKQ", s, d, j)])
                        self.ck(10)
                        pbb = B[2][:, :].bitcast(BF16)
                        for m in range(4):
                            self.tr(pbb[:, m * 128:(m + 1) * 128], BTb[:, m, :], self.identb[:, :], [BTb, self.identb], [B[2]])
                            self.tr(pbb[:, 512 + m * 128:512 + (m + 1) * 128], CTb[:, m, :], self.identb[:, :], [CTb, self.identb], [B[2]])
                        self.cp(self.act, BC[:, :], pbb[:, :], [B[2]], [BC])
                        self.dma(self.RBC[s, d, j], BC[:, :], [BC], [self.dd("RBC", s, d, j)])
                        self.ck(11)
                        Ms, Mn = (self.MU, self.ML) if d == 0 else (self.ML, self.MU)
                        for e_ in range(2):
                            rows = slice(e_ * 64, e_ * 64 + 64)
                            self.cp(G, BTz[:, :, :].rearrange("p (m e) t -> p m e t", e=2)[rows, :, e_, :], BTb[rows, :, :], [BTb], [BTz])
                            self.cp(self.act, CTz[:, :, :].rearrange("p (m e) t -> p m e t", e=2)[rows, :, e_, :], CTb[rows, :, :], [CTb], [CTz])
                        Msb = Ms[:, :].unsqueeze(1).to_broadcast([128, 2, 256])
                        v2 = lambda p: p[:, :].rearrange("p (h t) -> p h t", h=2)
                        for h in range(8):
                            self.mm(B[h // 2][:, (h % 2) * 256:(h % 2 + 1) * 256], BTz[:, h, :], KQ[:, h // 2, :], True, True, [BTz, KQ], [B[h // 2]])
                        for q in range(4):
                            self.tt(V, X1a[:, 2 * q:2 * q + 2, :], v2(B[q]), Msb, ALU.mult, [B[q], Ms], [X1a])
                        for h in range(8):
                            self.mm(B[h // 2][:, (h % 2) * 256:(h % 2 + 1) * 256], CTz[:, h, :], KQ[:, h // 2, :], True, True, [CTz, KQ], [B[h // 2]])
                        for q in range(4):
                            self.tt(V, MAT[:, 2 * q:2 * q + 2, 256:512], v2(B[q]), Msb, ALU.mult, [B[q], Ms], [MAT])
                        for h in range(8):
                            self.mm(B[h // 4][:, (h % 4) * 128:(h % 4 + 1) * 128], KQ[:, h // 2, 0:128], BTz[:, h, :], True, True, [BTz, KQ], [B[h // 4]])
                        Mnb = Mn[:, 0:128].unsqueeze(1).to_broadcast([128, 4, 128])
                        for q in range(2):
                            self.tt(V, NNa[:, 4 * q:4 * q + 4, :], B[q][:, :].rearrange("p (h t) -> p h t", h=4), Mnb, ALU.mult, [B[q], Mn], [NNa])
                        self.cp(G, MAT[:, :, 128:256], X1a[:, :, 128:256], [X1a], [MAT])
                        self.inverse_batch(X1a, NNa, MAT, 0, (B[0], B[1], B[2]), Wt, At, Bt)
                        self.inverse_batch(X1a, NNa, MAT, 4, (B[3], B[1], B[2]), Wt2, At2, Bt2)
                        self.ck(12)
                        self.dma(self.RMAT[s, d, j], MAT[:, :, :].rearrange("p h t -> p (h t)"), [MAT], [self.dd("RMAT", s, d, j)])
            self.run_interleaved([lambda: stream(0), lambda: stream(1)])
            self.barrier()

    def gdn_setup(self):
        self.GKQ = self.dram("GKQ", [S, 2, NJ, 128, 4 * 256], BF16)
        self.GMAT = self.dram("GMAT", [S, 2, NJ, 128, 4 * 512], BF16)
        self.GBC = self.dram("GBC", [S, 2, NJ, 128, 1024], BF16)
        self.GV = self.dram("GV", [S, NJ, 128, 512], BF16)
        self.GPC = self.dram("GPC", [S, 2, NJ, 128, 8], F32)
        self.YB = self.dram("YB", [2, NT, 512])
        self.SEL = self.sb([16, 16, 128], F32)
        self.selc = self.sb([128, 1], F32)
        self.onec = self.sb([128, 1], F32)
        nc = self.nc
        P = self.pool
        for i in range(16):
            self.cp(P, self.SEL[0:16, i, :], self.ident[0:16, i:i + 1].to_broadcast([16, 128]), [self.ident], [self.SEL])
        self.memset(P, self.onec[:, :], 1.0, [self.onec])
        self.memset(P, self.selc[:, :], 1.0, [self.selc])
        self.op(P, lambda: nc.gpsimd.affine_select(out=self.selc[:, :], in_=self.selc[:, :], pattern=[[0, 1]], compare_op=ALU.is_ge, fill=0.0,
                                                   base=-4, channel_multiplier=1), [self.selc], [self.selc])

    def gdn_prep(self, l):
        nc = self.nc
        V, G = self.dve, self.pool
        ps = self.ps
        with ExitStack() as es:
            def t(shape, dt=F32):
                return self.sb(shape, dt, es)
            PFl = self.PFt[l]

            def bc(ap, n):
                return ap.unsqueeze(2).to_broadcast([128, n, 128])
            negA = t([128, 1])
            self.actf(negA[:, :], self.pfv(l, "alog"), AF.Exp, [PFl], [negA])
            self.ts(V, negA[:, :], negA[:, :], -1.0, None, ALU.mult, None, [negA], [negA])
            DWg = t([128, 60, 128], BF16)
            gw_ = self.pfv(l, "gconv")
            for q_ in range(60):
                self.ts(V, DWg[:, q_, :], self.identb[:, :], gw_[:, q_:q_ + 1], None, ALU.mult, None, [self.identb, PFl], [DWg])
            def stream(s):
                B = self.ps[4 * s:4 * s + 4]
                qkv = t([128, 12, 132], BF16)
                cv = t([128, 12, 128])
                sq = t([128, 8, 128])
                kq = t([128, 4, 256])
                kqb = t([128, 4, 256], BF16)
                kb = t([128, 4, 128], BF16)
                vb = t([128, 4, 128], BF16)
                ab, x1, Xg, SIG, gcf, gcr = (t([16, 128]) for _ in range(6))
                X2, E2 = t([16, 256]), t([16, 256])
                TOTb, Etot = t([16, 128]), t([16, 2])
                TS = t([128, 80])
                negg, sB, sC, dd_ = t([128, 8]), t([128, 8]), t([128, 8]), t([128, 8])
                gm4 = t([128, 4, 256])
                KQd = t([128, 4, 256], BF16)
                BCd = [t([128, 1024], BF16) for _ in range(2)]
                Vt = t([128, 512], BF16)
                MAT = t([128, 4, 512], BF16)
                pc = t([128, 8])
                X1a = t([128, 4, 256], BF16)
                NNa = t([128, 4, 128], BF16)
                Wt = [t([128, 4, 128], BF16) for _ in range(2)]
                At = [t([128, 4, 128], BF16) for _ in range(2)]
                Bt = [t([128, 4, 128], BF16) for _ in range(2)]
                abv = self.PT[3968:3984, :]
                for j in range(NJ):
                    n0 = s * T + j * 128
                    self.load_halo(qkv, 15, 12, s, j, 2, Q=self.pool)
                    for c in range(12):
                        pb = B[c // 4]
                        for k in range(5):
                            self.mm(pb[:, (c % 4) * 128:(c % 4 + 1) * 128], DWg[:, k * 12 + c, :], qkv[:, c, k:k + 128], k == 0, k == 4, [DWg, qkv], [pb])
                    for b3 in range(3):
                        self.actf(cv[:, 4 * b3:4 * b3 + 4, :], B[b3][:, :].rearrange("p (c t) -> p c t", c=4), AF.Silu, [B[b3]], [cv])
                    self.tt(G, sq[:, :, :], cv[:, 0:8, :], cv[:, 0:8, :], ALU.mult, [cv], [sq])
                    for c in range(8):
                        pb = B[c // 4]
                        self.mm(pb[:, (c % 4) * 128:(c % 4 + 1) * 128], self.ones[:, :], sq[:, c, :], True, True, [self.ones, sq], [pb])
                    for hf in range(2):
                        self.ts(V, sq[:, hf * 4:(hf + 1) * 4, :], B[hf][:, :].rearrange("p (c t) -> p c t", c=4), EPS, None, ALU.add, None, [B[hf]], [sq])
                    self.actf(sq[:, :, :], sq[:, :, :], AF.Sqrt, [sq], [sq])
                    self.recip(sq[:, :, :], sq[:, :, :], [sq], [sq])
                    self.tt(V, kq[:, :, 0:128], cv[:, 4:8, :], sq[:, 4:8, :], ALU.mult, [cv, sq], [kq])
                    self.stt(V, kq[:, :, 128:256], cv[:, 0:4, :], 128.0 ** -0.5, sq[:, 0:4, :], ALU.mult, ALU.mult, [cv, sq], [kq])
                    self.cp(G, kqb[:, :, :], kq[:, :, :], [kq], [kqb])
                    self.cp(G, kb[:, :, :], kq[:, :, 0:128], [kq], [kb])
                    self.cp(G, vb[:, :, :], cv[:, 8:12, :], [cv], [vb])
                    pbv = B[2][:, :].bitcast(BF16)
                    for m in range(4):
                        self.tr(pbv[:, m * 128:(m + 1) * 128], vb[:, m, :], self.identb[:, :], [vb, self.identb], [B[2]])
                    self.cp(self.act, Vt[:, :], pbv[:, 0:512], [B[2]], [Vt])
                    self.dma(self.GV[s, j], Vt[:, :], [Vt], [self.dd("GV", s, j)])
                    pbk = B[3][:, :].bitcast(BF16)
                    for m in range(4):
                        self.tr(pbk[:, m * 128:(m + 1) * 128], kb[:, m, :], self.identb[:, :], [kb, self.identb], [B[3]])
                    self.dma(ab[0:16, :], abv[:, n0:n0 + 128], [], [ab])
                    self.actf(x1[0:16, :], ab[0:16, :], AF.Exp, [ab, PFl], [x1], bias=self.pfv(l, "dtb")[0:16, :], scale=1.0)
                    self.actf(x1[0:16, :], x1[0:16, :], AF.Ln, [x1, self.onec], [x1], bias=self.onec[0:16, :], scale=1.0)
                    self.ts(V, Xg[0:16, :], x1[0:16, :], negA[0:16, :], None, ALU.mult, None, [x1, negA], [Xg])
                    self.actf(SIG[0:16, :], ab[0:16, :], AF.Sigmoid, [ab], [SIG])
                    self.op(V, lambda: nc.vector.tensor_tensor_scan(out=gcf[0:16, :], data0=self.RM[0:16, :], data1=Xg[0:16, :], initial=0.0,
                                                                    op0=ALU.mult, op1=ALU.add), [self.RM, Xg], [gcf])
                    gcfv = gcf[0:16, :].rearrange("p (c t) -> p c t", t=64)
                    totb = gcfv[:, :, 63:64].to_broadcast([16, 2, 64])
                    self.cp(V, TOTb[0:16, :].rearrange("p (c t) -> p c t", t=64), totb, [gcf], [TOTb])
                    self.tt(V, gcr[0:16, :], TOTb[0:16, :], gcf[0:16, :], ALU.subtract, [TOTb, gcf], [gcr])
                    self.tt(V, X2[0:16, 128:256], gcr[0:16, :], Xg[0:16, :], ALU.add, [gcr, Xg], [X2])
                    self.tt(V, X2[0:16, 128:256], X2[0:16, 128:256], gcf[0:16, :], ALU.subtract, [X2, gcf], [X2])
                    self.stt(V, X2[0:16, 128:256], X2[0:16, 128:256], self.selc[0:16, :], gcf[0:16, :], ALU.mult, ALU.add, [X2, self.selc, gcf], [X2])
                    self.tt(V, X2[0:16, 0:128], X2[0:16, 128:256], Xg[0:16, :], ALU.subtract, [X2, Xg], [X2])
                    self.actf(E2[0:16, :], X2[0:16, :], AF.Exp, [X2], [E2])
                    self.actf(Etot[0:16, :], gcfv[:, :, 63], AF.Exp, [gcf], [Etot])
                    pT = B[2]
                    for q_, src in enumerate((Xg[0:16, :], SIG[0:16, :], X2[0:16, 0:128], X2[0:16, 128:256], TOTb[0:16, :])):
                        self.tr(pT[:, q_ * 16:(q_ + 1) * 16], src, self.ident[0:16, 0:16], [Xg, SIG, X2, TOTb, self.ident], [pT])
                    self.cp(V, TS[:, :], pT[:, 0:80], [pT], [TS])
                    self.actf(negg[:, :], TS[:, 0:8], AF.Exp, [TS], [negg], scale=-1.0)
                    self.tt(V, dd_[:, :], TS[:, 64:72], TS[:, 32:40], ALU.subtract, [TS], [dd_])
                    self.actf(sB[:, :], dd_[:, :], AF.Exp, [dd_], [sB])
                    self.tt(V, sB[:, :], sB[:, :], TS[:, 24:32], ALU.mult, [sB, TS], [sB])
                    self.tt(V, dd_[:, :], TS[:, 64:72], TS[:, 48:56], ALU.subtract, [TS], [dd_])
                    self.actf(sC[:, :], dd_[:, :], AF.Exp, [dd_], [sC])
                    self.tt(V, sC[:, :], sC[:, :], TS[:, 24:32], ALU.mult, [sC, TS], [sC])
                    pbk4 = pbk[:, 0:512].rearrange("p (h t) -> p h t", h=4)
                    for d in range(2):
                        self.tt(V, BCd[d][:, 0:512].rearrange("p (h t) -> p h t", h=4), pbk4, sB[:, d * 4:d * 4 + 4].unsqueeze(2).to_broadcast([128, 4, 128]), ALU.mult, [B[3], sB], [BCd[d]])
                        self.tt(V, BCd[d][:, 512:1024].rearrange("p (h t) -> p h t", h=4), pbk4, sC[:, d * 4:d * 4 + 4].unsqueeze(2).to_broadcast([128, 4, 128]), ALU.mult, [B[3], sC], [BCd[d]])
                    for d in range(2):
                        Ms = self.MU if d == 0 else self.ML
                        BC = BCd[d]
                        for h in range(4):
                            i = d * 4 + h
                            self.mm(B[h // 2][:, (h % 2) * 256:(h % 2 + 1) * 256], self.SEL[0:16, i, :], X2[0:16, :], True, True, [self.SEL, X2], [B[h // 2]])
                            self.mm(B[2 + h // 2][:, (h % 2) * 256:(h % 2 + 1) * 256], self.SEL[0:16, i, :], E2[0:16, :], True, True, [self.SEL, E2], [B[2 + h // 2]])
                        v2 = lambda p: p[:, :].rearrange("p (h t) -> p h t", h=2)
                        for q in range(2):
                            self.tt(V, gm4[:, 2 * q:2 * q + 2, :], v2(B[q]), TS[:, 32 + d * 4 + 2 * q:32 + d * 4 + 2 * q + 2].unsqueeze(2).to_broadcast([128, 2, 256]),
                                    ALU.subtract, [B[q], TS], [gm4])
                            self.tt(V, KQd[:, 2 * q:2 * q + 2, :], kq[:, 2 * q:2 * q + 2, :], v2(B[2 + q]), ALU.mult, [kq, B[2 + q]], [KQd])
                        for h in range(4):
                            self.mm(B[2][:, h * 2:(h + 1) * 2], self.SEL[0:16, d * 4 + h, :], Etot[0:16, :], True, True, [self.SEL, Etot], [B[2]])
                        self.cp(self.act, pc[:, :], B[2][:, 0:8], [B[2]], [pc])
                        self.ts(G, gm4[:, :, :], gm4[:, :, :], 0.0, None, ALU.min, None, [gm4], [gm4])
                        self.actf(gm4[:, :, :], gm4[:, :, :], AF.Exp, [gm4], [gm4])
                        self.tt(G, gm4[:, :, :], gm4[:, :, :], Ms[:, :].unsqueeze(1).to_broadcast([128, 4, 256]), ALU.mult, [gm4, Ms], [gm4])
                        for h in range(4):
                            self.mm(B[h // 2][:, (h % 2) * 256:(h % 2 + 1) * 256], kb[:, h, :], kqb[:, h, :], True, True, [kb, kqb], [B[h // 2]])
                        for q in range(2):
                            self.tt(V, gm4[:, 2 * q:2 * q + 2, :], gm4[:, 2 * q:2 * q + 2, :], v2(B[q]), ALU.mult, [gm4, B[q]], [gm4])
                        self.tt(V, X1a[:, :, :], gm4[:, :, :], TS[:, 24 + d * 4:28 + d * 4].unsqueeze(2).to_broadcast([128, 4, 256]), ALU.mult, [gm4, TS], [X1a])
                        self.tt(V, MAT[:, :, 256:512], X1a[:, :, :], negg[:, d * 4:d * 4 + 4].unsqueeze(2).to_broadcast([128, 4, 256]), ALU.mult, [X1a, negg], [MAT])
                        self.cp(G, MAT[:, :, 128:256], X1a[:, :, 128:256], [X1a], [MAT])
                        pN = B[3][:, :].bitcast(BF16)
                        for h in range(4):
                            self.tr(pN[:, h * 128:(h + 1) * 128], X1a[:, h, 0:128], self.identb[:, :], [X1a, self.identb], [B[3]])
                        self.cp(self.act, NNa[:, :, :], pN[:, 0:512].rearrange("p (h t) -> p h t", h=4), [B[3]], [NNa])
                        self.inverse_batch(X1a, NNa, MAT, 0, (B[0], B[1], B[2]), Wt, At, Bt)
                        self.dma(self.GKQ[s, d, j], KQd[:, :, :].rearrange("p m t -> p (m t)"), [KQd], [self.dd("GKQ", s, d, j)])
                        self.dma(self.GMAT[s, d, j], MAT[:, :, :].rearrange("p h t -> p (h t)"), [MAT], [self.dd("GMAT", s, d, j)])
                        self.dma(self.GBC[s, d, j], BC[:, :], [BC], [self.dd("GBC", s, d, j)])
                        self.dma(self.GPC[s, d, j], pc[:, :], [pc], [self.dd("GPC", s, d, j)])
            self.run_interleaved([lambda: stream(0), lambda: stream(1)])
            self.barrier()

    def conf_setup(self):
        self.RCs = self.dram("RCs", [512, NT], BF16)
        self.RAs = self.dram("RAs", [512, NT], BF16)
        self.RBs = self.dram("RBs", [512, NT], BF16)

    def conformer(self, l):
        V, G = self.dve, self.pool
        ps = self.ps
        PFl = self.PFt[l]
        valv = self.PT[3984:3984 + 512, :].rearrange("(c p) n -> p c n", p=128)
        gatv = self.PT[4496:4496 + 512, :].rearrange("(c p) n -> p c n", p=128)
        RCv = self.RCs.rearrange("(c p) n -> p c n", p=128)
        with ExitStack() as es:
            def t(shape, dt=F32):
                return self.sb(shape, dt, es)
            DW = t([128, 124, 128], BF16)
            cw = self.pfv(l, "cdw")
            for q in range(124):
                self.ts(V, DW[:, q, :], self.identb[:, :], cw[:, q:q + 1], None, ALU.mult, None, [self.identb, PFl], [DW])
            upW = t([128, 2, 32, 94], BF16)
            upH = t([128, 2, 62, 64], BF16)
            upC = t([128, 4, 286], BF16)
            self.memset(G, upW[:, :, :, :], 0.0, [upW])
            self.memset(G, upH[:, :, :, :], 0.0, [upH])
            self.memset(G, upC[:, :, :], 0.0, [upC])
            vl = [t([128, TL]) for _ in range(2)]
            gl = [t([128, TL]) for _ in range(2)]
            o = t([128, 4, TL])
            sq = t([128, 4, 512])
            mu, rs, var = t([128, 512]), t([128, 512]), t([128, 512])
            ob = t([128, 4, 512], BF16)
            nb = 0
            for s in range(S):
                for (seg0, W_) in ((0, TC), (TC, TL)):
                    if seg0 == 0 and l == L - 1:
                        continue
                    n0 = s * T + seg0
                    for c in range(4):
                        v_, g_ = vl[c % 2], gl[c % 2]
                        self.dma(v_[:, :W_], valv[:, c, n0:n0 + W_], [], [v_])
                        self.dma(g_[:, :W_], gatv[:, c, n0:n0 + W_], [], [g_])
                        self.actf(g_[:, :W_], g_[:, :W_], AF.Sigmoid, [g_], [g_])
                        if seg0 == 0:
                            self.tt(V, upC[:, c, 15:15 + W_], v_[:, :W_], g_[:, :W_], ALU.mult, [v_, g_], [upC])
                        elif c < 2:
                            self.tt(V, upW[:, c, :, 15:79], v_[:, :].rearrange("p (r w) -> p r w", w=64), g_[:, :].rearrange("p (r w) -> p r w", w=64),
                                    ALU.mult, [v_, g_], [upW])
                        else:
                            self.tt(V, upH[:, c - 2, 15:47, :], v_[:, :].rearrange("p (r w) -> p r w", w=64), g_[:, :].rearrange("p (r w) -> p r w", w=64),
                                    ALU.mult, [v_, g_], [upH])
                    for c in range(4):
                        if seg0 == 0:
                            pb = ps[nb % 4]; nb += 1
                            for k in range(31):
                                self.mm(pb[:, 0:W_], DW[:, k * 4 + c, :], upC[:, c, k:k + W_], k == 0, k == 30, [DW, upC], [pb])
                            self.actf(o[:, c, 0:W_], pb[:, 0:W_], AF.Identity, [pb, PFl], [o], bias=self.pfv(l, "cdwb", c), scale=1.0)
                        else:
                            for rb_ in range(4):
                                pb = ps[nb % 4]; nb += 1
                                for k in range(31):
                                    if c < 2:
                                        rhs = upW[:, c, rb_ * 8:(rb_ + 1) * 8, k:k + 64]
                                        outp = pb[:, :].rearrange("p (r w) -> p r w", w=64)
                                    else:
                                        rhs = upH[:, c - 2, rb_ * 8 + k:rb_ * 8 + k + 8, :].rearrange("p r w -> p (r w)")
                                        outp = pb[:, :]
                                    self.mm(outp, DW[:, k * 4 + c, :], rhs, k == 0, k == 30, [DW, upW if c < 2 else upH], [pb])
                                self.actf(o[:, c, rb_ * 512:(rb_ + 1) * 512], pb[:, :], AF.Identity, [pb, PFl], [o], bias=self.pfv(l, "cdwb", c), scale=1.0)
                    for t0 in range(0, W_, 512):
                        w = min(512, W_ - t0)
                        self.tt(G, sq[:, :, :w], o[:, :, t0:t0 + w], o[:, :, t0:t0 + w], ALU.mult, [o], [sq])
                        for c in range(4):
                            self.mm(ps[4][:, :w], self.ones[:, :], o[:, c, t0:t0 + w], c == 0, c == 3, [self.ones, o], [ps[4]])
                        for c in range(4):
                            self.mm(ps[5][:, :w], self.ones[:, :], sq[:, c, :w], c == 0, c == 3, [self.ones, sq], [ps[5]])
                        self.ts(V, mu[:, :w], ps[4][:, :w], 1.0 / 512, None, ALU.mult, None, [ps[4]], [mu])
                        self.tt(V, var[:, :w], mu[:, :w], mu[:, :w], ALU.mult, [mu], [var])
                        self.stt(V, var[:, :w], ps[5][:, :w], 1.0 / 512, var[:, :w], ALU.mult, ALU.subtract, [ps[5], var], [var])
                        self.ts(V, var[:, :w], var[:, :w], EPS, None, ALU.add, None, [var], [var])
                        self.actf(var[:, :w], var[:, :w], AF.Sqrt, [var], [var])
                        self.recip(rs[:, :w], var[:, :w], [var], [rs])
                        for c in range(4):
                            self.tt(V, sq[:, c, :w], o[:, c, t0:t0 + w], mu[:, :w], ALU.subtract, [o, mu], [sq])
                            self.tt(G, sq[:, c, :w], sq[:, c, :w], rs[:, :w], ALU.mult, [sq, rs], [sq])
                            self.ts(V, sq[:, c, :w], sq[:, c, :w], self.pfv(l, "clng", c), self.pfv(l, "clnb", c), ALU.mult, ALU.add, [sq, PFl], [sq])
                        self.actf(ob[:, :, :w], sq[:, :, :w], AF.Silu, [sq], [ob])
                        self.dma(RCv[:, :, n0 + t0:n0 + t0 + w], ob[:, :, :w], [ob], [self.dd("RCs", (n0 + t0) // 128)])
            self.barrier()

    def red(self, E, out, in_, op, R, W):
        self.op(E, lambda: E.be.tensor_reduce(out=out, in_=in_, axis=AX.X, op=op), R, W)

    def merge(self, l):
        V, G = self.dve, self.pool
        ps = self.ps
        PFl = self.PFt[l]
        with ExitStack() as es:
            def t(shape, dt=F32):
                return self.sb(shape, dt, es)
            wbr = t([128, 12, 1024], BF16)
            wo = t([128, 8, 1024], BF16)
            for n in range(3):
                self.dma(wbr[:, n * 4:(n + 1) * 4, :], self.w_branch[l, n].rearrange("(k p) n -> p k n", p=128), [], [wbr], Q=self.pool)
            self.dma(wo[:, :, :], self.w_out[l].rearrange("(k p) n -> p k n", p=128), [], [wo], Q=self.pool)
            pgv = [self.PT[5008 + n * 1024:5008 + (n + 1) * 1024, :].rearrange("(c p) n -> p c n", p=128) for n in range(3)]
            fm4 = lambda A: A.rearrange("(c p) n -> p c n", p=128)
            def stream(s):
                B = self.ps[4 * s:4 * s + 4]
                y0, y1, ysq = t([128, 512]), t([128, 512]), t([128, 512])
                m1, m2, m3 = t([128, 8]), t([128, 8]), t([128, 8])
                raf, bon, ga, zt = (t([128, 4, 128]) for _ in range(4))
                Rb = [t([128, 4, 128], BF16) for _ in range(3)]
                pg = t([128, 8, 128])
                macc, tmpm = t([128, 8, 128]), t([128, 8, 128])
                mb = t([128, 8, 128], BF16)
                xt = t([128, 8, 128])
                for j in range(NJ):
                    if j < 2 and l == L - 1:
                        continue
                    n0 = s * T + j * 128
                    r = 2 if j < 2 else s
                    for br in range(2):
                        DY, nm, nh, dvv, eps_ = ((self.YA, "R", 8, 64, 64e-5), (self.YB, "G", 4, 128, EPS))[br]
                        self.dma(y0[:, :], DY[0, n0:n0 + 128, :], [self.dd(nm + "Y", 0, n0 // 128)], [y0])
                        self.dma(y1[:, :], DY[1, n0:n0 + 128, :], [self.dd(nm + "Y", 1, n0 // 128)], [y1])
                        self.tt(V, y0[:, :], y0[:, :], y1[:, :], ALU.add, [y0, y1], [y0])
                        yv = y0[:, :].rearrange("p (h d) -> p h d", d=dvv)
                        self.tt(G, ysq[:, :], y0[:, :], y0[:, :], ALU.mult, [y0], [ysq])
                        self.red(V, m2[:, 0:nh], ysq[:, :].rearrange("p (h d) -> p h d", d=dvv), ALU.add, [ysq], [m2])
                        if br == 0:
                            self.red(V, m1[:, 0:nh], yv, ALU.add, [y0], [m1])
                            self.ts(V, m1[:, 0:nh], m1[:, 0:nh], 1.0 / dvv, None, ALU.mult, None, [m1], [m1])
                            self.tt(V, m3[:, 0:nh], m1[:, 0:nh], m1[:, 0:nh], ALU.mult, [m1], [m3])
                            self.stt(V, m2[:, 0:nh], m2[:, 0:nh], 1.0 / dvv, m3[:, 0:nh], ALU.mult, ALU.subtract, [m2, m3], [m2])
                            self.ts(V, m2[:, 0:nh], m2[:, 0:nh], eps_, None, ALU.add, None, [m2], [m2])
                            self.tt(V, yv, yv, m1[:, 0:nh].unsqueeze(2).to_broadcast([128, nh, dvv]), ALU.subtract, [y0, m1], [y0])
                        else:
                            self.ts(V, m2[:, 0:nh], m2[:, 0:nh], 1.0 / dvv, eps_, ALU.mult, ALU.add, [m2], [m2])
                        self.actf(m2[:, 0:nh], m2[:, 0:nh], AF.Sqrt, [m2], [m2])
                        self.recip(m2[:, 0:nh], m2[:, 0:nh], [m2], [m2])
                        self.tt(V, yv, yv, m2[:, 0:nh].unsqueeze(2).to_broadcast([128, nh, dvv]), ALU.mult, [y0, m2], [y0])
                        pb = B[br]
                        for c in range(4):
                            self.tr(pb[:, c * 128:(c + 1) * 128], y0[:, c * 128:(c + 1) * 128], self.ident[:, :], [y0, self.ident], [pb])
                        pbv = pb[:, :].rearrange("p (c t) -> p c t", c=4)
                        if br == 0:
                            for c in range(4):
                                self.ts(V, raf[:, c, :], pbv[:, c, :], self.pfv(l, "ln_g", c), self.pfv(l, "ln_b", c), ALU.mult, ALU.add, [pb, PFl], [raf])
                            self.dma(bon[:, :, :], fm4(self.BON)[:, :, n0:n0 + 128], [self.dd("BON", n0 // 128)], [bon])
                            self.dma(ga[:, :, :], fm4(self.GAs)[:, :, n0:n0 + 128], [self.dd("GAs", n0 // 128)], [ga])
                            self.tt(V, raf[:, :, :], raf[:, :, :], bon[:, :, :], ALU.add, [raf, bon], [raf])
                            self.tt(V, Rb[0][:, :, :], raf[:, :, :], ga[:, :, :], ALU.mult, [raf, ga], [Rb[0]])
                        else:
                            self.dma(zt[:, :, :], self.PTv[:, 27:31, n0:n0 + 128], [], [zt])
                            self.actf(zt[:, :, :], zt[:, :, :], AF.Silu, [zt], [zt])
                            self.stt(V, Rb[1][:, :, :], pbv, self.pfv(l, "gnorm", 0), zt[:, :, :], ALU.mult, ALU.mult, [pb, PFl, zt], [Rb[1]])
                    self.dma(Rb[2][:, :, :], fm4(self.RCs)[:, :, n0:n0 + 128], [self.dd("RCs", n0 // 128)], [Rb[2]])
                    for n in range(3):
                        self.dma(pg[:, :, :], pgv[n][:, :, n0:n0 + 128], [], [pg])
                        self.actf(pg[:, :, :], pg[:, :, :], AF.Sigmoid, [pg], [pg])
                        for m in range(8):
                            pb = B[2 + m // 4]
                            for k in range(4):
                                self.mm(pb[:, (m % 4) * 128:(m % 4 + 1) * 128], wbr[:, n * 4 + k, m * 128:(m + 1) * 128], Rb[n][:, k, :], k == 0, k == 3,
                                        [wbr, Rb[n]], [pb])
                        for hf in range(2):
                            pbv = B[2 + hf][:, :].rearrange("p (c t) -> p c t", c=4)
                            dst = macc if n == 0 else tmpm
                            self.tt(V, dst[:, hf * 4:(hf + 1) * 4, :], pbv, pg[:, hf * 4:(hf + 1) * 4, :], ALU.mult, [B[2 + hf], pg], [dst])
                        if n > 0:
                            self.tt(G, macc[:, :, :], macc[:, :, :], tmpm[:, :, :], ALU.add, [macc, tmpm], [macc])
                    self.cp(G, mb[:, :, :], macc[:, :, :], [macc], [mb])
                    self.dma(xt[:, :, :], self.XTv[:, :, n0:n0 + 128], self.dr("XT", n0, 128), [xt])
                    for m in range(8):
                        pb = B[m // 4]
                        for k in range(8):
                            self.mm(pb[:, (m % 4) * 128:(m % 4 + 1) * 128], wo[:, k, m * 128:(m + 1) * 128], mb[:, k, :], k == 0, k == 7, [wo, mb], [pb])
                    for m in range(8):
                        pb = B[m // 4]
                        self.stt(V, xt[:, m, :], pb[:, (m % 4) * 128:(m % 4 + 1) * 128], self.MOD[l][:, 16 + m, r:r + 1], xt[:, m, :], ALU.mult, ALU.add,
                                 [pb, self.MOD[l], xt], [xt])
                    self.dma(self.XTv[:, :, n0:n0 + 128], xt[:, :, :], [xt], self.dr("XT", n0, 128))
                    if f"XM{l}" in self.dbg:
                        pass
            self.run_interleaved([lambda: stream(0), lambda: stream(1)])
            self.barrier()

    def moe(self, l):
        V, G = self.dve, self.pool
        ps = self.ps
        with ExitStack() as es:
            def t(shape, dt=F32, e_=None):
                return self.sb(shape, dt, e_ or es)
            hT = t([128, 8, NT], BF16)
            WTf = t([16, NT])
            wr = t([128, 8, 16])
            rb = t([128, 16])
            self.dma(wr[:, :, :], self.w_router.rearrange("(k p) e -> p k e", p=128), [], [wr])
            self.dma(rb[:, :], self.rbias, [], [rb])
            with ExitStack() as es1:
                xts = [t([128, 8, 512], F32, es1) for _ in range(2)]
                sq = t([128, 8, 512], F32, es1)
                rs = t([128, 512], F32, es1)
                hf = t([128, 8, 512], F32, es1)
                sc, sel, sel2, eq, cm, wts = (t([128, 16], F32, es1) for _ in range(6))
                m1, m2, gs, gsel = (t([128, 4], F32, es1) for _ in range(4))
                gmx, wsum = t([128, 1], F32, es1), t([128, 1], F32, es1)
                v4 = lambda a: a[:, :].rearrange("p (g j) -> p g j", j=4)
                b4 = lambda a: a[:, :].unsqueeze(2).to_broadcast([128, 4, 4])
                for i, (n0, w, r) in enumerate([t_ for t_ in self.tiles() if not (l == L - 1 and t_[2] == 2)]):
                    xt, _ = self.modulate_tile((xts[i % 2], sq, rs, None), n0, w, r, None, None, None, None)
                    for c in range(8):
                        self.stt(V, hf[:, c, :w], xt[:, c, :w], self.GF[l][:, c, r:r + 1], rs[:, :w], ALU.mult, ALU.mult, [xt, rs, self.GF[l]], [hf])
                        self.actf(hf[:, c, :w], hf[:, c, :w], AF.Identity, [hf, self.MOD[l]], [hf], bias=self.MOD[l][:, 24 + c, r:r + 1], scale=1.0)
                    self.cp(G, hT[:, :, n0:n0 + w], hf[:, :, :w], [hf], [hT])
                    for q in range(w // 128):
                        pR = ps[6]
                        for c in range(8):
                            self.mm(pR[:, 0:16], hf[:, c, q * 128:(q + 1) * 128], wr[:, c, :], c == 0, c == 7, [hf, wr], [pR])
                        self.actf(sc[:, :], pR[:, 0:16], AF.Sigmoid, [pR], [sc])
                        self.tt(V, sel[:, :], sc[:, :], rb[:, :], ALU.add, [sc, rb], [sel])
                        self.red(V, m1[:, :], v4(sel), ALU.max, [sel], [m1])
                        self.tt(V, v4(eq), v4(sel), b4(m1), ALU.is_equal, [sel, m1], [eq])
                        self.stt(V, sel2[:, :], eq[:, :], -1e9, sel[:, :], ALU.mult, ALU.add, [eq, sel], [sel2])
                        self.red(V, m2[:, :], v4(sel2), ALU.max, [sel2], [m2])
                        self.tt(V, gs[:, :], m1[:, :], m2[:, :], ALU.add, [m1, m2], [gs])
                        self.red(V, gmx[:, :], gs[:, :], ALU.max, [gs], [gmx])
                        self.ts(V, gsel[:, :], gs[:, :], gmx[:, 0:1], None, ALU.is_equal, None, [gs, gmx], [gsel])
                        self.tt(V, v4(cm), v4(sel), b4(m2), ALU.is_ge, [sel, m2], [cm])
                        self.tt(V, v4(cm), v4(cm), b4(gsel), ALU.mult, [cm, gsel], [cm])
                        self.tt(V, wts[:, :], sc[:, :], cm[:, :], ALU.mult, [sc, cm], [wts])
                        self.red(V, wsum[:, :], wts[:, :], ALU.add, [wts], [wsum])
                        self.recip(wsum[:, :], wsum[:, :], [wsum], [wsum])
                        self.ts(V, wts[:, :], wts[:, :], wsum[:, 0:1], None, ALU.mult, None, [wts, wsum], [wts])
                        pT = ps[7]
                        self.tr(pT[0:16, 0:128], wts[:, :], self.ident[:, :], [wts, self.ident], [pT])
                        self.cp(self.act, WTf[0:16, n0 + q * 128:n0 + (q + 1) * 128], pT[0:16, 0:128], [pT], [WTf])
                self.barrier()
            TG = 1152
            TW = 384
            yacc = t([128, 8, TG])
            wgb = [t([128, 8, 512], BF16) for _ in range(2)]
            wub = [t([128, 8, 512], BF16) for _ in range(2)]
            wdb = [t([128, 4, 1024], BF16) for _ in range(2)]
            wtb = t([128, TW])
            sg = [t([128, TW]) for _ in range(2)]
            actb = t([128, 4, TW], BF16)
            xt2 = t([128, 8, 128])
            def loadw(e):
                for (src, dstt) in ((self.weg[l, e], wgb[e % 2]), (self.weu[l, e], wub[e % 2]), (self.wed[l, e], wdb[e % 2])):
                    self.dma(dstt[:, :, :], src.rearrange("(k p) n -> p k n", p=128), [], [dstt], Q=self.pool)
            loadw(0)
            for g in range(NT // TG):
                for e in range(16):
                    gb, ub, db = wgb[e % 2], wub[e % 2], wdb[e % 2]
                    if not (g == NT // TG - 1 and e == 15):
                        loadw((e + 1) % 16)
                    skipc = (l == L - 1) and ((g * TG) % T == 0)
                    tl_ = [(256, 384), (640, 384), (1024, 128)] if skipc else [(i_ * TW, TW) for i_ in range(TG // TW)]
                    for (off_, w_) in tl_:
                        n0 = g * TG + off_
                        self.mm(ps[4][:, :w_], self.SEL[0:16, e, :], WTf[0:16, n0:n0 + w_], True, True, [self.SEL, WTf], [ps[4]])
                        self.cp(self.act, wtb[:, :w_], ps[4][:, :w_], [ps[4]], [wtb])
                        for hc in range(4):
                            pg_, pu_ = ps[(hc % 2) * 2], ps[(hc % 2) * 2 + 1]
                            for k in range(8):
                                self.mm(pg_[:, :w_], gb[:, k, hc * 128:(hc + 1) * 128], hT[:, k, n0:n0 + w_], k == 0, k == 7, [gb, hT], [pg_])
                            for k in range(8):
                                self.mm(pu_[:, :w_], ub[:, k, hc * 128:(hc + 1) * 128], hT[:, k, n0:n0 + w_], k == 0, k == 7, [ub, hT], [pu_])
                            sg_ = sg[hc % 2]
                            self.actf(sg_[:, :w_], pg_[:, :w_], AF.Silu, [pg_], [sg_])
                            self.tt(V, sg_[:, :w_], sg_[:, :w_], pu_[:, :w_], ALU.mult, [sg_, pu_], [sg_])
                            self.tt(G, actb[:, hc, :w_], sg_[:, :w_], wtb[:, :w_], ALU.mult, [sg_, wtb], [actb])
                        for m in range(8):
                            pd = ps[4 + m % 4]
                            for hc in range(4):
                                self.mm(pd[:, :w_], db[:, hc, m * 128:(m + 1) * 128], actb[:, hc, :w_], hc == 0, hc == 3, [db, actb], [pd])
                            dst = yacc[:, m, off_:off_ + w_]
                            if e == 0:
                                self.cp(self.act, dst, pd[:, :w_], [pd], [yacc])
                            else:
                                self.tt(V, dst, dst, pd[:, :w_], ALU.add, [yacc, pd], [yacc])
                for p_ in range(TG // 128):
                    n0 = g * TG + p_ * 128
                    tq = n0 % T
                    if tq < TC and l == L - 1:
                        continue
                    r = 2 if tq < TC else n0 // T
                    self.dma(xt2[:, :, :], self.XTv[:, :, n0:n0 + 128], self.dr("XT", n0, 128), [xt2])
                    for m in range(8):
                        self.stt(V, xt2[:, m, :], yacc[:, m, p_ * 128:(p_ + 1) * 128], self.MOD[l][:, 40 + m, r:r + 1], xt2[:, m, :], ALU.mult, ALU.add,
                                 [yacc, self.MOD[l], xt2], [xt2])
                    self.dma(self.XTv[:, :, n0:n0 + 128], xt2[:, :, :], [xt2], self.dr("XT", n0, 128))
            self.barrier()

    def final(self):
        V, G = self.dve, self.pool
        ps = self.ps
        with ExitStack() as es:
            def t(shape, dt=F32):
                return self.sb(shape, dt, es)
            xts = [t([128, 8, 128]) for _ in range(2)]
            sq = t([128, 8, 128])
            rs = t([128, 128])
            tmp = t([128, 8, 128])
            os_ = [t([128, 1024]) for _ in range(2)]
            i = 0
            for s in range(S):
                for j in range(2, NJ):
                    n0 = s * T + j * 128
                    xt = xts[i % 2]; o = os_[i % 2]; i += 1
                    self.dma(xt[:, :, :], self.XTv[:, :, n0:n0 + 128], self.dr("XT", n0, 128), [xt])
                    self.actf(sq[:, :, :], xt[:, :, :], AF.Square, [xt], [sq])
                    for c in range(8):
                        self.mm(ps[0][:, 0:128], self.ones[:, :], sq[:, c, :], c == 0, c == 7, [self.ones, sq], [ps[0]])
                    self.ts(V, rs[:, :], ps[0][:, 0:128], 1.0 / D, EPS, ALU.mult, ALU.add, [ps[0]], [rs])
                    self.actf(rs[:, :], rs[:, :], AF.Sqrt, [rs], [rs])
                    self.recip(rs[:, :], rs[:, :], [rs], [rs])
                    for c in range(8):
                        self.stt(V, tmp[:, c, :], xt[:, c, :], self.pfv(0, "norm_final", c), rs[:, :], ALU.mult, ALU.mult, [xt, rs, self.PFt[0]], [tmp])
                    for c in range(8):
                        pb = ps[1 + c // 4]
                        self.tr(pb[:, (c % 4) * 128:(c % 4 + 1) * 128], tmp[:, c, :], self.ident[:, :], [tmp, self.ident], [pb])
                    self.cp(self.act, o[:, 0:512], ps[1][:, :], [ps[1]], [o])
                    self.cp(V, o[:, 512:1024], ps[2][:, :], [ps[2]], [o])
                    self.dma(self.y[s, (j - 2) * 128:(j - 1) * 128, :], o[:, :], [o], [self.dd("y", s, j)])
            self.barrier()

    def rwkv_scan(self, l, gdn=False):
        V, G = self.dve, self.pool
        ps = self.ps
        nh = 4 if gdn else 8
        dv = 512 // nh
        DKQ, DMAT, DBC, DV_, DPC, DY, nm = ((self.GKQ, self.GMAT, self.GBC, self.GV, self.GPC, self.YB, 'G') if gdn else (self.RKQ, self.RMAT, self.RBC, self.RV, self.RPC, self.YA, 'R'))
        with ExitStack() as es:
            def t(shape, dt=F32):
                return self.sb(shape, dt, es)
            chains = [(s, d) for s in range(S) for d in range(2)]
            order = {0: list(range(NJ)), 1: [1, 0] + list(range(NJ - 1, 1, -1))}
            bufs = []
            for _ in chains:
                ld = [(t([128, 4, 256], BF16), t([128, nh, 512], BF16), t([128, 1024], BF16), t([128, 512], BF16), t([128, 8])) for _ in range(2)]
                bufs.append(dict(ld=ld, H=t([128, 4, dv]), Hz=t([128, nh, dv], BF16), Rn=t([128, nh, dv], BF16),
                                 Ubz=[t([128, nh, dv], BF16) for _ in range(2)], Vz=[t([128, 512], BF16) for _ in range(2)],
                                 Yt=t([128, 512])))
            def chain(ci, s, d):
                for i in range(NJ):
                    j = order[d][i]
                    n0 = s * T + j * 128
                    b = bufs[ci]
                    KQ, MAT, BC, Vt, pc = b["ld"][i % 2]
                    H, Hz, Rn, Ubz, Vz, Yt = b["H"], b["Hz"], b["Rn"], b["Ubz"], b["Vz"], b["Yt"]
                    self.dma(KQ[:, :, :].rearrange("p m t -> p (m t)"), DKQ[s, d, j], [self.dd(nm + "KQ", s, d, j)], [KQ])
                    self.dma(MAT[:, :, :].rearrange("p h t -> p (h t)"), DMAT[s, d, j], [self.dd(nm + "MAT", s, d, j)], [MAT])
                    self.dma(BC[:, :], DBC[s, d, j], [self.dd(nm + "BC", s, d, j)], [BC])
                    self.dma(Vt[:, :], DV_[s, j], [self.dd(nm + "V", s, j)], [Vt])
                    self.dma(pc[:, :], DPC[s, d, j], [self.dd(nm + "PC", s, d, j)], [pc])
                    if i == 0:
                        self.memset(G, H[:, :, :], 0.0, [H])
                        self.memset(G, Hz[:, :, :], 0.0, [Hz])
                        self.memset(G, Rn[:, :, :], 0.0, [Rn])
                        for c in range(2):
                            self.memset(G, Ubz[c][:, :, :], 0.0, [Ubz[c]])
                            self.memset(G, Vz[c][:, :], 0.0, [Vz[c]])
                    for c in range(2):
                        self.cp(G, Vz[c][c * 64:c * 64 + 64, :], Vt[c * 64:c * 64 + 64, :], [Vt], [Vz[c]])
                    pA, pB = ps[2 * ci], ps[2 * ci + 1]
                    pAv = pA[:, :].rearrange("p (h v) -> p h v", v=dv)
                    pBv = pB[:, :].rearrange("p (h v) -> p h v", v=dv)
                    if not gdn:
                        pBe = pB[:, :].rearrange("p (m e v) -> p m e v", e=2, v=64)
                        Hze = Hz[:, :, :].rearrange("p (m e) v -> p m e v", e=2)
                    pcv = pc[:, :].rearrange("p (m c) -> p m c", c=2)
                    for c in ([0, 1] if d == 0 else [1, 0]):
                        cs = slice(c * 64, c * 64 + 64)
                        Ub = Ubz[c]
                        for h in range(nh):
                            m = h if gdn else h // 2
                            self.mm(pAv[:, h, :], KQ[:, m, 0:128], Hz[:, h, :], True, False, [KQ, Hz], [pA])
                            self.mm(pAv[:, h, :], MAT[:, h, 256:384], Vt[:, h * dv:(h + 1) * dv], False, True, [MAT, Vt], [pA])
                        self.ts(V, Rn[cs, :, :], pAv[cs, :, :], -1.0, None, ALU.mult, None, [pA], [Rn])
                        for h in range(nh):
                            self.mm(pBv[:, h, :], MAT[:, h, 0:128], Rn[:, h, :], True, True, [MAT, Rn], [pB])
                        self.cp(self.act, Ub[cs, :, :], pBv[cs, :, :], [pB], [Ub])
                        for h in range(nh):
                            m = h if gdn else h // 2
                            self.mm(pAv[:, h, :], KQ[:, m, 128:256], Hz[:, h, :], True, False, [KQ, Hz], [pA])
                            self.mm(pAv[:, h, :], MAT[:, h, 128:256], Ub[:, h, :], False, False, [MAT, Ub], [pA])
                            self.mm(pAv[:, h, :], MAT[:, h, 384:512], Vt[:, h * dv:(h + 1) * dv], False, True, [MAT, Vt], [pA])
                        self.cp(V, Yt[cs, :], pA[cs, :], [pA], [Yt])
                        for h in range(nh):
                            m = h if gdn else h // 2
                            self.mm(pBv[:, h, :], BC[:, m * 128:(m + 1) * 128], Ub[:, h, :], True, False, [BC, Ub], [pB])
                            self.mm(pBv[:, h, :], BC[:, 512 + m * 128:512 + (m + 1) * 128], Vz[c][:, h * dv:(h + 1) * dv], False, True, [BC, Vz[c]], [pB])
                        if gdn:
                            self.tt(V, H[:, :, :], H[:, :, :], pcv[:, :, c:c + 1].to_broadcast([128, 4, dv]), ALU.mult, [H, pc], [H])
                            self.tt(V, H[:, :, :], H[:, :, :], pBv[:, :, :], ALU.add, [H, pB], [H])
                            self.cp(self.act, Hz[:, :, :], H[:, :, :], [H], [Hz])
                        else:
                            for e in range(2):
                                rows = slice(e * 64, e * 64 + 64)
                                self.tt(V, H[rows, :, :], H[rows, :, :], pBe[rows, :, e, :], ALU.add, [H, pB], [H])
                            self.tt(V, H[:, :, :], H[:, :, :], pcv[:, :, c:c + 1].to_broadcast([128, 4, 64]), ALU.mult, [H, pc], [H])
                            for e in range(2):
                                rows = slice(e * 64, e * 64 + 64)
                                self.cp(self.act, Hze[rows, :, e, :], H[rows, :, :], [H], [Hz])
                    self.dma(DY[d, n0:n0 + 128, :], Yt[:, :], [Yt], [self.dd(nm + "Y", d, n0 // 128)])
            self.run_interleaved([(lambda ci=ci, s=s, d=d: chain(ci, s, d)) for ci, (s, d) in enumerate(chains)])
            self.barrier()

    def build(self):
        try:
            self.build_()
        except StopBuild:
            self.es2 = None
            self.finish()

    def build_(self):
        self.consts()
        self.gdn_setup()
        self.conf_setup()
        self.phase0()
        if self.stop == "0":
            return self.finish()
        for l in range(self.nlayers):
            self.phaseA(l)
            if self.stop == f"A{l}":
                return self.finish()
            self.phaseB(l)
            if self.stop == f"B{l}":
                return self.finish()
            self.rwkv_prep(l)
            if self.stop == f"C{l}":
                return self.finish()
            self.rwkv_scan(l)
            if self.stop == f"D{l}":
                return self.finish()
            self.gdn_prep(l)
            if self.stop == f"E{l}":
                return self.finish()
            self.rwkv_scan(l, gdn=True)
            if self.stop == f"F{l}":
                return self.finish()
            self.conformer(l)
            if self.stop == f"G{l}":
                return self.finish()
            self.merge(l)
            if self.stop == f"H{l}":
                return self.finish()
            self.moe(l)
            if self.stop == f"I{l}":
                return self.finish()
        self.final()
        self.finish()
```
